# Optimizing a Trainium2 kernel written in Bass

```python
import math
import jax
import jax.numpy as jnp
from jax import lax
import numpy as np

D_MODEL = 1024
BATCH = 8
SEQ = 2048
DEPTH = 2

MEM_LEN = 256
N_MIXERS = 4
GROUP_WIDTH = D_MODEL // 4
MIX_WIDTH = N_MIXERS * GROUP_WIDTH
HEAD_DIM = 64
N_HEADS_GROUP = GROUP_WIDTH // HEAD_DIM
NORM_EPS = 1e-6
CONV_WIDTH = 4
LRU_BLOCKS = N_HEADS_GROUP
LRU_C = 8.0
SB_QBLOCK = 128
SSM_HEADS = N_HEADS_GROUP
SSM_HEAD_DIM = HEAD_DIM
SSM_INNER = SSM_HEADS * SSM_HEAD_DIM
SSM_NGROUPS = 2
SSM_STATE = 128
SSM_CHUNK = 128
SSM_CONV_CH = SSM_INNER + 2 * SSM_NGROUPS * SSM_STATE
MOBA_BLOCK = 256
MOBA_TOPK = 3
MOBA_QCHUNK = 32
ROPE_THETA = 10000.0
XATTN_HEADS = 4
XATTN_HEAD_DIM = D_MODEL // XATTN_HEADS
MOE_GROUPS = 4
MOE_EXPERTS_PER_GROUP = 4
MOE_EXPERTS = MOE_GROUPS * MOE_EXPERTS_PER_GROUP
MOE_TOPK = 2
MOE_FF = D_MODEL // 4
IN_SPLITS = (GROUP_WIDTH, GROUP_WIDTH, 3 * GROUP_WIDTH, SSM_INNER, SSM_CONV_CH, SSM_HEADS, 3 * GROUP_WIDTH)
IN_WIDTH = sum(IN_SPLITS)

kernel_name = 'hybrid_hymba_rglru_sb_ssd_moba_hmoe'


def rmsnorm(x, g):
    xf = x.astype(jnp.float32)
    y = xf * lax.rsqrt(jnp.mean(xf * xf, axis=-1, keepdims=True) + NORM_EPS)
    return (y * g.astype(jnp.float32)).astype(x.dtype)


def causal_depthwise_conv(x, w, b):
    y = lax.conv_general_dilated(x, w[:, None, :].astype(x.dtype), window_strides=(1,),
                                 padding=[(CONV_WIDTH - 1, 0)],
                                 dimension_numbers=('NWC', 'WIO', 'NWC'),
                                 feature_group_count=x.shape[-1])
    return y + b.astype(x.dtype)


def rope(x, positions):
    half = x.shape[-1] // 2
    inv_freq = ROPE_THETA ** (-jnp.arange(half, dtype=jnp.float32) / half)
    ang = positions.astype(jnp.float32)[:, None] * inv_freq[None, :]
    cos = jnp.cos(ang)[None, :, None, :].astype(x.dtype)
    sin = jnp.sin(ang)[None, :, None, :].astype(x.dtype)
    x1, x2 = x[..., :half], x[..., half:]
    return jnp.concatenate([x1 * cos - x2 * sin, x2 * cos + x1 * sin], axis=-1)


def rglru_mixer(xb, gate, conv_w, conv_b, wr, br, wi, bi, lam):
    B, S, W = xb.shape
    xc = causal_depthwise_conv(xb, conv_w, conv_b)
    xh = xc.reshape(B, S, LRU_BLOCKS, W // LRU_BLOCKS)
    r = jax.nn.sigmoid(jnp.einsum('bshi,hij->bshj', xh, wr).reshape(B, S, W) + br).astype(jnp.float32)
    i = jax.nn.sigmoid(jnp.einsum('bshi,hij->bshj', xh, wi).reshape(B, S, W) + bi)
    log_a = LRU_C * r * jax.nn.log_sigmoid(lam.astype(jnp.float32))
    a = jnp.exp(log_a)
    u = jnp.sqrt(-jnp.expm1(2.0 * log_a)) * (i * xc).astype(jnp.float32)

    def combine(left, right):
        a_l, b_l = left
        a_r, b_r = right
        return a_l * a_r, a_r * b_l + b_r

    _, h = lax.associative_scan(combine, (a, u), axis=1)
    return h.astype(xb.dtype) * jax.nn.gelu(gate)


def stick_breaking_attention(q, k, v):
    B, S, H, Dh = q.shape
    scale = Dh ** -0.5
    q = q.transpose(0, 2, 1, 3)
    k = k.transpose(0, 2, 1, 3)
    v = v.transpose(0, 2, 1, 3)
    outs = []
    for blk in range(S // SB_QBLOCK):
        t0 = blk * SB_QBLOCK
        t1 = t0 + SB_QBLOCK
        z = jnp.einsum('bhtd,bhsd->bhts', q[:, :, t0:t1], k[:, :, :t1]).astype(jnp.float32) * scale
        past = jnp.arange(t1)[None, :] < jnp.arange(t0, t1)[:, None]
        log_fail = jnp.where(past, jax.nn.log_sigmoid(-z), 0.0)
        after = lax.cumsum(log_fail, axis=3, reverse=True) - log_fail
        w = jnp.where(past, jnp.exp(jax.nn.log_sigmoid(z) + after), 0.0)
        outs.append(jnp.einsum('bhts,bhsd->bhtd', w.astype(v.dtype), v[:, :, :t1]))
    o = jnp.concatenate(outs, axis=2)
    return o.transpose(0, 2, 1, 3).reshape(B, S, H * Dh)


def segsum(x):
    T = x.shape[-1]
    xr = jnp.broadcast_to(x[..., :, None], x.shape + (T,))
    strict = jnp.tril(jnp.ones((T, T), dtype=bool), -1)
    cs = jnp.cumsum(jnp.where(strict, xr, 0.0), axis=-2)
    return jnp.where(jnp.tril(jnp.ones((T, T), dtype=bool)), cs, -jnp.inf)


def ssd_chunked(xs, dt, A, Bm, Cm):
    Bsz, S, H, P = xs.shape
    N = Bm.shape[-1]
    nc = S // SSM_CHUNK
    Xc = (xs * dt[..., None]).reshape(Bsz, nc, SSM_CHUNK, H, P)
    Ac = (dt * A).reshape(Bsz, nc, SSM_CHUNK, H).transpose(0, 3, 1, 2)
    Bc = Bm.reshape(Bsz, nc, SSM_CHUNK, H, N)
    Cc = Cm.reshape(Bsz, nc, SSM_CHUNK, H, N)
    A_cs = jnp.cumsum(Ac, axis=-1)
    Lmat = jnp.exp(segsum(Ac))
    y_diag = jnp.einsum('bhcls,bcshp->bclhp', jnp.einsum('bclhn,bcshn->bhcls', Cc, Bc) * Lmat, Xc)
    decay_states = jnp.exp(A_cs[..., -1:] - A_cs)
    states = jnp.einsum('bclhn,bhcl,bclhp->bchpn', Bc, decay_states, Xc)
    states = jnp.concatenate([jnp.zeros_like(states[:, :1]), states], axis=1)
    decay_chunk = jnp.exp(segsum(jnp.pad(A_cs[..., -1], ((0, 0), (0, 0), (1, 0)))))
    prev_states = jnp.einsum('bhzc,bchpn->bzhpn', decay_chunk, states)[:, :-1]
    y_off = jnp.einsum('bclhn,bchpn,bhcl->bclhp', Cc, prev_states, jnp.exp(A_cs))
    return (y_diag + y_off).reshape(Bsz, S, H, P)


def mamba2_mixer(z, xbc, dt_raw, conv_w, conv_b, dt_bias, a_log, d_skip):
    B, S, _ = z.shape
    f32 = jnp.float32
    xbc = jax.nn.silu(causal_depthwise_conv(xbc, conv_w, conv_b))
    xs, bm, cm = jnp.split(xbc, [SSM_INNER, SSM_INNER + SSM_NGROUPS * SSM_STATE], axis=-1)
    xs = xs.reshape(B, S, SSM_HEADS, SSM_HEAD_DIM).astype(f32)
    rep = SSM_HEADS // SSM_NGROUPS
    bm = jnp.repeat(bm.reshape(B, S, SSM_NGROUPS, SSM_STATE).astype(f32), rep, axis=2)
    cm = jnp.repeat(cm.reshape(B, S, SSM_NGROUPS, SSM_STATE).astype(f32), rep, axis=2)
    dt = jax.nn.softplus(dt_raw.astype(f32) + dt_bias.astype(f32))
    A = -jnp.exp(a_log.astype(f32))
    y = ssd_chunked(xs, dt, A, bm, cm) + d_skip.astype(f32)[:, None] * xs
    return y.reshape(B, S, SSM_INNER).astype(z.dtype) * jax.nn.silu(z)


def moba_attention(q, k, v):
    B, S, H, Dh = q.shape
    pos = jnp.arange(S)
    q = rope(q, pos).transpose(0, 2, 1, 3)
    k = rope(k, pos).transpose(0, 2, 1, 3)
    v = v.transpose(0, 2, 1, 3)
    nb = -(-S // MOBA_BLOCK)
    pad = nb * MOBA_BLOCK - S
    kb = jnp.pad(k, ((0, 0), (0, 0), (0, pad), (0, 0))).reshape(B, H, nb, MOBA_BLOCK, Dh)
    vb = jnp.pad(v, ((0, 0), (0, 0), (0, pad), (0, 0))).reshape(B, H, nb, MOBA_BLOCK, Dh)
    k_mean = jnp.mean(kb, axis=3)
    gate = jnp.einsum('bhtd,bhnd->bhtn', q, k_mean).astype(jnp.float32)
    fully_past = jnp.arange(nb)[None, :] < (pos // MOBA_BLOCK)[:, None]
    gate = jnp.where(fully_past, gate, -jnp.inf)
    n_sel = max(1, min(MOBA_TOPK, nb - 1))
    _, sel = lax.top_k(gate, n_sel)
    n_chunks = S // MOBA_QCHUNK
    q_c = q.reshape(B, H, n_chunks, MOBA_QCHUNK, Dh).transpose(2, 0, 1, 3, 4)
    sel_c = sel.reshape(B, H, n_chunks, MOBA_QCHUNK, n_sel).transpose(2, 0, 1, 3, 4)
    b_ix = jnp.arange(B)[:, None, None, None]
    h_ix = jnp.arange(H)[None, :, None, None]
    scale = Dh ** -0.5

    def attend_chunk(args):
        ci, qc, sc = args
        t0 = ci * MOBA_QCHUNK
        own = t0 // MOBA_BLOCK
        k_sel = kb[b_ix, h_ix, sc]
        v_sel = vb[b_ix, h_ix, sc]
        s_past = jnp.einsum('bhtd,bhtnjd->bhtnj', qc, k_sel).astype(jnp.float32) * scale
        s_past = jnp.where((sc < own)[..., None], s_past, -jnp.inf).reshape(B, H, MOBA_QCHUNK, n_sel * MOBA_BLOCK)
        k_own = lax.dynamic_index_in_dim(kb, own, axis=2, keepdims=False)
        v_own = lax.dynamic_index_in_dim(vb, own, axis=2, keepdims=False)
        s_own = jnp.einsum('bhtd,bhjd->bhtj', qc, k_own).astype(jnp.float32) * scale
        causal = (own * MOBA_BLOCK + jnp.arange(MOBA_BLOCK))[None, :] <= (t0 + jnp.arange(MOBA_QCHUNK))[:, None]
        s_own = jnp.where(causal, s_own, -jnp.inf)
        p = jax.nn.softmax(jnp.concatenate([s_past, s_own], axis=-1), axis=-1).astype(v.dtype)
        p_past = p[..., :n_sel * MOBA_BLOCK].reshape(B, H, MOBA_QCHUNK, n_sel, MOBA_BLOCK)
        return (jnp.einsum('bhtnj,bhtnjd->bhtd', p_past, v_sel)
                + jnp.einsum('bhtj,bhjd->bhtd', p[..., n_sel * MOBA_BLOCK:], v_own))

    o = lax.map(attend_chunk, (jnp.arange(n_chunks), q_c, sel_c))
    return o.transpose(1, 0, 3, 2, 4).reshape(B, S, H * Dh)


def hybrid_mixer(h, w_in, lru_conv_w, lru_conv_b, lru_wr, lru_br, lru_wi, lru_bi, lru_lambda,
                 ssm_conv_w, ssm_conv_b, ssm_dt_bias, ssm_a_log, ssm_d, group_norm_g, w_out):
    B, S, _ = h.shape
    proj = h @ w_in
    offs = [int(o) for o in np.cumsum(IN_SPLITS)[:-1]]
    lru_x, lru_gate, sb_qkv, ssm_z, ssm_xbc, ssm_dt, mb_qkv = jnp.split(proj, offs, axis=-1)

    def heads(t):
        return t.reshape(B, S, N_HEADS_GROUP, HEAD_DIM)

    y_a = rglru_mixer(lru_x, lru_gate, lru_conv_w, lru_conv_b, lru_wr, lru_br, lru_wi, lru_bi, lru_lambda)
    sq, sk, sv = jnp.split(sb_qkv, 3, axis=-1)
    y_b = stick_breaking_attention(heads(sq), heads(sk), heads(sv))
    y_c = mamba2_mixer(ssm_z, ssm_xbc, ssm_dt, ssm_conv_w, ssm_conv_b, ssm_dt_bias, ssm_a_log, ssm_d)
    mq, mk, mv = jnp.split(mb_qkv, 3, axis=-1)
    y_d = moba_attention(heads(mq), heads(mk), heads(mv))
    y = jnp.concatenate([y_a, y_b, y_c, y_d], axis=-1).reshape(B, S, N_MIXERS, GROUP_WIDTH)
    y = rmsnorm(y, group_norm_g.reshape(N_MIXERS, GROUP_WIDTH)).reshape(B, S, MIX_WIDTH)
    return y @ w_out


def memory_cross_attention(h, m, wq, wkv, wo):
    B, S, D = h.shape
    M = m.shape[1]
    q = (h @ wq).reshape(B, S, XATTN_HEADS, XATTN_HEAD_DIM)
    k, v = jnp.split(m @ wkv, 2, axis=-1)
    k = k.reshape(B, M, XATTN_HEADS, XATTN_HEAD_DIM)
    v = v.reshape(B, M, XATTN_HEADS, XATTN_HEAD_DIM)
    s = jnp.einsum('bshd,bmhd->bhsm', q, k).astype(jnp.float32) * XATTN_HEAD_DIM ** -0.5
    p = jax.nn.softmax(s, axis=-1).astype(v.dtype)
    o = jnp.einsum('bhsm,bmhd->bshd', p, v).reshape(B, S, D)
    return o @ wo


def hierarchical_moe(h, wg, bg, we, be, w1, w3, w2):
    B, S, D = h.shape
    t = h.reshape(B * S, D)
    f32 = jnp.float32
    pg = jax.nn.softmax((t @ wg).astype(f32) + bg.astype(f32), axis=-1)
    pg_top, g_idx = lax.top_k(pg, 1)
    g_onehot = jax.nn.one_hot(g_idx[:, 0], MOE_GROUPS, dtype=f32)
    le = ((t @ we).astype(f32) + be.astype(f32)).reshape(-1, MOE_GROUPS, MOE_EXPERTS_PER_GROUP)
    pe = jax.nn.softmax(jnp.einsum('tg,tge->te', g_onehot, le), axis=-1)
    pe_top, e_idx = lax.top_k(pe, MOE_TOPK)
    pe_top = pe_top / jnp.sum(pe_top, axis=-1, keepdims=True)
    w_group = jnp.einsum('tk,tke->te', pe_top, jax.nn.one_hot(e_idx, MOE_EXPERTS_PER_GROUP, dtype=f32))
    comb = (pg_top[:, :, None] * g_onehot[:, :, None] * w_group[:, None, :]).reshape(-1, MOE_EXPERTS)
    hid = jax.nn.silu(jnp.einsum('td,ndf->tnf', t, w1)) * jnp.einsum('td,ndf->tnf', t, w3)
    y = jnp.einsum('tnf,nfd->td', hid * comb.astype(t.dtype)[:, :, None], w2)
    return y.reshape(B, S, D)


def setup_inputs(seed: int = 0) -> dict:
    key = jax.random.key(seed)
    kit = iter(jax.random.split(key, 40))
    f32 = jnp.float32
    L, D = DEPTH, D_MODEL

    def nrm(shape, scale):
        return jax.random.normal(next(kit), shape, f32) * scale

    def gain(shape):
        return 1.0 + 0.02 * jax.random.normal(next(kit), shape, f32)

    x = nrm((BATCH, SEQ, D), 1.0)
    mem = nrm((BATCH, MEM_LEN, D), 1.0)
    mix_norm_g = gain((L, D))
    w_in = nrm((L, D, IN_WIDTH), D ** -0.5)
    lru_conv_w = nrm((L, CONV_WIDTH, GROUP_WIDTH), CONV_WIDTH ** -0.5)
    lru_conv_b = nrm((L, GROUP_WIDTH), 0.01)
    blk = GROUP_WIDTH // LRU_BLOCKS
    lru_wr = nrm((L, LRU_BLOCKS, blk, blk), blk ** -0.5)
    lru_br = nrm((L, GROUP_WIDTH), 0.1)
    lru_wi = nrm((L, LRU_BLOCKS, blk, blk), blk ** -0.5)
    lru_bi = nrm((L, GROUP_WIDTH), 0.1)
    a0 = jax.random.uniform(next(kit), (L, GROUP_WIDTH), f32, 0.9, 0.999) ** (1.0 / LRU_C)
    lru_lambda = jnp.log(a0) - jnp.log1p(-a0)
    ssm_conv_w = nrm((L, CONV_WIDTH, SSM_CONV_CH), CONV_WIDTH ** -0.5)
    ssm_conv_b = nrm((L, SSM_CONV_CH), 0.01)
    dt0 = jnp.exp(jax.random.uniform(next(kit), (L, SSM_HEADS), f32, math.log(1e-3), math.log(1e-1)))
    ssm_dt_bias = dt0 + jnp.log(-jnp.expm1(-dt0))
    ssm_a_log = jnp.log(jax.random.uniform(next(kit), (L, SSM_HEADS), f32, 1.0, 16.0))
    ssm_d = 1.0 + nrm((L, SSM_HEADS), 0.1)
    group_norm_g = gain((L, MIX_WIDTH))
    w_out = nrm((L, MIX_WIDTH, D), 0.5 * MIX_WIDTH ** -0.5)
    xattn_norm_g = gain((L, D))
    mem_norm_g = gain((L, D))
    xattn_wq = nrm((L, D, D), D ** -0.5)
    xattn_wkv = nrm((L, D, 2 * D), D ** -0.5)
    xattn_wo = nrm((L, D, D), 0.5 * D ** -0.5)
    ffn_norm_g = gain((L, D))
    router_group_w = nrm((L, D, MOE_GROUPS), D ** -0.5)
    router_group_b = nrm((L, MOE_GROUPS), 0.01)
    router_expert_w = nrm((L, D, MOE_EXPERTS), D ** -0.5)
    router_expert_b = nrm((L, MOE_EXPERTS), 0.01)
    expert_w1 = nrm((L, MOE_EXPERTS, D, MOE_FF), D ** -0.5)
    expert_w3 = nrm((L, MOE_EXPERTS, D, MOE_FF), D ** -0.5)
    expert_w2 = nrm((L, MOE_EXPERTS, MOE_FF, D), 0.5 * MOE_FF ** -0.5)
    final_norm_g = gain((D,))
    return {'x': x, 'mem': mem, 'mix_norm_g': mix_norm_g, 'w_in': w_in,
            'lru_conv_w': lru_conv_w, 'lru_conv_b': lru_conv_b, 'lru_wr': lru_wr, 'lru_br': lru_br,
            'lru_wi': lru_wi, 'lru_bi': lru_bi, 'lru_lambda': lru_lambda,
            'ssm_conv_w': ssm_conv_w, 'ssm_conv_b': ssm_conv_b, 'ssm_dt_bias': ssm_dt_bias,
            'ssm_a_log': ssm_a_log, 'ssm_d': ssm_d, 'group_norm_g': group_norm_g, 'w_out': w_out,
            'xattn_norm_g': xattn_norm_g, 'mem_norm_g': mem_norm_g, 'xattn_wq': xattn_wq,
            'xattn_wkv': xattn_wkv, 'xattn_wo': xattn_wo, 'ffn_norm_g': ffn_norm_g,
            'router_group_w': router_group_w, 'router_group_b': router_group_b,
            'router_expert_w': router_expert_w, 'router_expert_b': router_expert_b,
            'expert_w1': expert_w1, 'expert_w3': expert_w3, 'expert_w2': expert_w2,
            'final_norm_g': final_norm_g}


def reference(x, mem, mix_norm_g, w_in, lru_conv_w, lru_conv_b, lru_wr, lru_br, lru_wi, lru_bi,
              lru_lambda, ssm_conv_w, ssm_conv_b, ssm_dt_bias, ssm_a_log, ssm_d, group_norm_g, w_out,
              xattn_norm_g, mem_norm_g, xattn_wq, xattn_wkv, xattn_wo, ffn_norm_g,
              router_group_w, router_group_b, router_expert_w, router_expert_b,
              expert_w1, expert_w3, expert_w2, final_norm_g):
    for l in range(DEPTH):
        h = rmsnorm(x, mix_norm_g[l])
        x = x + hybrid_mixer(h, w_in[l], lru_conv_w[l], lru_conv_b[l], lru_wr[l], lru_br[l],
                             lru_wi[l], lru_bi[l], lru_lambda[l], ssm_conv_w[l], ssm_conv_b[l],
                             ssm_dt_bias[l], ssm_a_log[l], ssm_d[l], group_norm_g[l], w_out[l])
        x = x + memory_cross_attention(rmsnorm(x, xattn_norm_g[l]), rmsnorm(mem, mem_norm_g[l]),
                                       xattn_wq[l], xattn_wkv[l], xattn_wo[l])
        x = x + hierarchical_moe(rmsnorm(x, ffn_norm_g[l]), router_group_w[l], router_group_b[l],
                                 router_expert_w[l], router_expert_b[l],
                                 expert_w1[l], expert_w3[l], expert_w2[l])
    return rmsnorm(x, final_norm_g)
```

```python
import numpy as np
from contextlib import ExitStack
import concourse.bass as bass
import concourse.mybir as mybir
from concourse.bass_utils import run_bass_kernel_spmd

F32 = mybir.dt.float32
BF16 = mybir.dt.bfloat16
F32R = mybir.dt.float32r
AF = mybir.ActivationFunctionType
ALU = mybir.AluOpType
AX = mybir.AxisListType

S_ = 2048
D_ = 1024
L_ = 2
KC = 8
NTC = 4
TW = 512
NT = 16
NEG = -30000.0
EPS = 1e-6

COLS = {}
_c = 0
for _n, _w in [("mix_g", 8), ("xat_g", 8), ("mem_g", 8), ("ffn_g", 8), ("grp_g", 8),
               ("lru_cw", 8), ("lru_cb", 2), ("lru_br", 2), ("lru_bi", 2), ("lru_lam", 2),
               ("ssm_cw", 24), ("ssm_cb", 6), ("ssm_d", 2), ("fin_g", 8)]:
    COLS[_n] = (_c, _w)
    _c += _w
NCOL = _c
ROWS = {}
_c = 0
for _n, _w in [("dt_bias", 64), ("a_log", 64), ("bg", 4), ("be", 16)]:
    ROWS[_n] = (_c, _w)
    _c += _w
NROW = _c


class Sched:
    ENG = ("pe", "act", "dve", "pool", "sp")

    def __init__(self, nc):
        self.nc = nc
        self.ops = []
        self.lastw = {}
        self.rd = {}
        self.by_eng = {e: [] for e in self.ENG}
        self.extra = {e: set() for e in self.ENG}
        self.dma_since = []
        self.final = []
        self.psn = 0

    def add(self, eng, fn, reads=(), writes=(), dma=False):
        i = len(self.ops)
        reads = list(reads)
        writes = list(writes)
        deps = {}
        for r in reads:
            w = self.lastw.get(r)
            if w is not None:
                deps.setdefault(w, set()).add("raw")
        for w_ in writes:
            w = self.lastw.get(w_)
            if w is not None:
                deps.setdefault(w, set()).add("waw")
            rr = self.rd.get(w_)
            if rr:
                for r in rr[0].values():
                    deps.setdefault(r, set()).add("war")
                for r in rr[1]:
                    deps.setdefault(r, set()).add("war")
        ws = set(writes)
        for r in reads:
            if r not in ws:
                rr = self.rd.setdefault(r, ({}, []))
                if dma:
                    rr[1].append(i)
                else:
                    rr[0][eng] = i
        for w_ in writes:
            self.lastw[w_] = i
            self.rd[w_] = ({}, [])
        fdeps = set()
        for d, kinds in deps.items():
            od = self.ops[d]
            if od["eng"] == eng and not od["dma"] and not dma:
                if eng == "pe":
                    continue
                if "raw" not in kinds:
                    continue
            fdeps.add(d)
        fdeps |= self.extra[eng]
        self.extra[eng] = set()
        self.ops.append(dict(eng=eng, fn=fn, deps=fdeps, dma=dma, signal=dma, token=None))
        self.by_eng[eng].append(i)
        if dma and eng != "pool":
            self.dma_since.append(i)
        return i

    def barrier(self):
        toks = set()
        for e in self.ENG:
            for i in reversed(self.by_eng[e]):
                if not self.ops[i]["dma"]:
                    toks.add(i)
                    break
        toks |= set(self.dma_since)
        self.dma_since = []
        self.bar_toks = set(toks)
        for e in ("pe", "act", "dve", "sp"):
            self.extra[e] |= toks

    def mm(self, out, lhsT, rhs, start, stop, reads, writes):
        return self.add("pe", lambda e: e.matmul(out, lhsT, rhs, start=start, stop=stop), reads, writes)

    def tr(self, out, in_, ident, reads, writes):
        return self.add("pe", lambda e: e.transpose(out, in_, ident), reads, writes)

    def act(self, out, in_, func, reads, writes, bias=None, scale=None, accum=None):
        kw = {}
        if bias is not None:
            kw["bias"] = bias
        if scale is not None:
            kw["scale"] = scale
        if accum is not None:
            kw["accum_out"] = accum
        return self.add("act", lambda e: e.activation(out=out, in_=in_, func=func, **kw), reads, writes)

    def tt(self, out, in0, in1, op, reads, writes, eng="dve"):
        return self.add(eng, lambda e: e.tensor_tensor(out=out, in0=in0, in1=in1, op=op), reads, writes)

    def ts(self, out, in0, s1, s2, op0, op1, reads, writes, eng="dve"):
        if s2 is None:
            return self.add(eng, lambda e: e.tensor_scalar(out, in0, s1, None, op0), reads, writes)
        return self.add(eng, lambda e: e.tensor_scalar(out, in0, s1, s2, op0, op1), reads, writes)

    def stt(self, out, in0, scalar, in1, op0, op1, reads, writes, eng="dve"):
        return self.add(eng, lambda e: e.scalar_tensor_tensor(out=out, in0=in0, scalar=scalar, in1=in1,
                                                             op0=op0, op1=op1), reads, writes)

    def cp(self, out, in_, reads, writes, eng="dve"):
        if eng == "act":
            return self.add("act", lambda e: e.activation(out=out, in_=in_, func=AF.Copy), reads, writes)
        return self.add(eng, lambda e: e.tensor_copy(out=out, in_=in_), reads, writes)

    def dma(self, out, in_, reads, writes, q="sp", arena=False):
        i = self.add(q, lambda e: e.dma_start(out=out, in_=in_), reads, writes, dma=True)
        if arena:
            self.ops[i]["deps"] |= getattr(self, "bar_toks", set())
        return i

    def finalize(self, es):
        nc = self.nc
        for op in self.ops:
            for d in op["deps"]:
                self.ops[d]["signal"] = True
        for d in self.final:
            self.ops[d]["signal"] = True
        csem = {e: es.enter_context(nc.semaphore("c_" + e)) for e in ("pe", "act", "dve", "pool")}
        NP = 16
        qsem = {q: [es.enter_context(nc.semaphore(f"q_{q}{k}")) for k in range(NP)] for q in ("sp", "pool")}
        cnt = {e: 0 for e in csem}
        qn = {q: 0 for q in qsem}
        quse = {q: [0] * NP for q in qsem}
        qprev = {q: [None] * NP for q in qsem}
        for i, op in enumerate(self.ops):
            if op["dma"]:
                q = op["eng"]
                k = qn[q] % NP
                qn[q] += 1
                quse[q][k] += 1
                op["token"] = (qsem[q][k], 16 * quse[q][k])
                if qprev[q][k] is not None:
                    op["deps"].add(qprev[q][k])
                qprev[q][k] = i
            elif op["signal"]:
                e = op["eng"]
                cnt[e] += 1
                op["token"] = (csem[e], cnt[e])
        ops = self.ops
        final = list(self.final)

        def emit(engname, e):
            waited = {}
            for i in self.by_eng[engname]:
                op = ops[i]
                for d in sorted(op["deps"]):
                    sem, val = ops[d]["token"]
                    if waited.get(sem, 0) < val:
                        e.wait_ge(sem, val)
                        waited[sem] = val
                ins = op["fn"](e)
                if op["signal"]:
                    ins.then_inc(op["token"][0], 16 if op["dma"] else 1)
            if engname == "sp":
                for d in final:
                    sem, val = ops[d]["token"]
                    if waited.get(sem, 0) < val:
                        e.wait_ge(sem, val)
                        waited[sem] = val

        with nc.Block() as block:
            @block.tensor
            def _(e):
                emit("pe", e)

            @block.scalar
            def _(e):
                emit("act", e)

            @block.vector
            def _(e):
                emit("dve", e)

            @block.gpsimd
            def _(e):
                emit("pool", e)

            @block.sync
            def _(e):
                emit("sp", e)


def K(name, *idx):
    return (name,) + idx


def build_program(stop_after=None, dumps=(), n_layers=L_):
    nc = bass.Bass("TRN2", target_bir_lowering=False)
    S = Sched(nc)
    es = ExitStack()
    P = {}

    def din(name, shape):
        P[name] = nc.dram_tensor(name, list(shape), F32, kind="ExternalInput").ap()
        return P[name]

    x_d = din("x", [S_, D_])
    mem_d = din("mem", [256, D_])
    w_in_d = din("w_in", [L_, D_, 3076])
    w_out_d = din("w_out", [L_, D_, D_])
    wq_d = din("xattn_wq", [L_, D_, D_])
    wkv_d = din("xattn_wkv", [L_, D_, 2 * D_])
    wo_d = din("xattn_wo", [L_, D_, D_])
    w1_d = din("expert_w1", [L_, 16, D_, 256])
    w3_d = din("expert_w3", [L_, 16, D_, 256])
    w2_d = din("expert_w2", [L_, 16, 256, D_])
    wrt_d = din("router_w", [L_, D_, 20])
    lrubd_d = din("lru_wbd", [L_, 2, 2, 128, 128])
    colp_d = din("colp", [128, L_ * NCOL])
    rowp_d = din("rowp", [128, L_ * NROW])
    c128_d = din("c128", [128, 8 * 128])
    cmask_d = din("cmask", [128, 12 * 512])
    rope_d = din("rope", [128, 2 * S_])
    en_d = din("en", [16, 16 * 128])
    gate_c_d = din("gatec", [128, 3 * 128])
    out_d = nc.dram_tensor("out", [S_, D_], F32, kind="ExternalOutput").ap()
    dump_d = {}

    uid = [0]

    def sb(name, shape, dt=F32, stack=None):
        uid[0] += 1
        return (stack or es).enter_context(nc.sbuf_tensor(f"{name}_{uid[0]}", list(shape), dt))

    xT = sb("xT", [128, KC, S_])
    hT = sb("hT", [128, KC, S_], BF16)
    colp = sb("colp_s", [128, L_ * NCOL])
    rowp = sb("rowp_s", [128, L_ * NROW])
    c128 = sb("c128_s", [128, 8 * 128])
    c128b = sb("c128b_s", [128, 8 * 128], BF16)
    W = [sb(f"wslot{i}", [128, KC, 512], BF16) for i in range(3)]
    PS = [es.enter_context(nc.psum_tensor(f"ps{i}", [128, 512], F32)) for i in range(8)]
    wn = [0]

    ident = c128[:, 0:128]
    ones_f = c128[:, 128:256]
    tri_le = c128[:, 256:384]
    negu = c128[:, 384:512]
    negones = c128[:, 512:640]
    perm_f = c128[:, 640:768]
    ssdneg = c128[:, 768:896]
    ones_b = c128b[:, 128:256]
    ident_b = c128b[:, 0:128]

    cst = sb("cst", [128, 4])
    S.add("dve", lambda e: e.memset(cst[:, 0:1], EPS), [], [K("cst0")])
    S.add("dve", lambda e: e.memset(cst[:, 1:2], 1.0), [], [K("cst1")])
    S.add("dve", lambda e: e.memset(cst[:, 2:4], 0.0), [K("cst0"), K("cst1")], [K("cst")])
    S.dma(colp[:], colp_d[:, :], [], [K("colp")])
    S.dma(rowp[:], rowp_d[:, :], [], [K("rowp")])
    S.dma(c128[:], c128_d[:, :], [], [K("c128")])
    S.dma(c128b[:], c128_d[:, :], [], [K("c128b")], q="pool")

    def col(l, name, j=0, n=1):
        c0, w = COLS[name]
        return colp[:, l * NCOL + c0 + j: l * NCOL + c0 + j + n]

    def row(l, name, j=0, n=None):
        c0, w = ROWS[name]
        n = w if n is None else n
        return rowp[:, l * NROW + c0 + j: l * NROW + c0 + j + n]

    def ps_rot():
        k = S.psn % 4
        S.psn += 1
        return PS[k], K("ps", k)

    Wf = [w[:].rearrange("p a b -> p (a b)") for w in W]

    def load_w(src2d, nk, ncols, slot=None, c0=0, tot=None):
        tot = ncols if tot is None else tot
        if slot is None:
            k = wn[0] % 3
            wn[0] += 1
        else:
            k = slot
        v = Wf[k][:, 0:nk * tot].rearrange("p (a b) -> p a b", a=nk)
        step = 2 if nk >= 2 else 1
        for h in range(0, nk, step):
            S.dma(v[:, h:h + step, c0:c0 + ncols],
                  src2d[h * 128:(h + step) * 128, :].rearrange("(kc p) n -> p kc n", p=128),
                  [], [K("w", k)], q="pool")
        return v, [K("w", k)], k

    def dump(name, ap, reads, shape):
        if name in dumps:
            d = nc.dram_tensor("dbg_" + name, list(shape), ap.dtype, kind="ExternalOutput").ap()
            dump_d[name] = d
            i = S.dma(d, ap, reads, [K("dbg", name)])
            S.final.append(i)

    def load_fm(dst, src_d, ntile, dkey, st):
        xin = [sb(f"xin{j}", [128, D_], stack=st) for j in range(2)]
        for i in range(ntile):
            b = xin[i % 2]
            S.dma(b[:], src_d[i * 128:(i + 1) * 128, :], [], [K("xin", i % 2)])
            for half in range(2):
                ps, pk = ps_rot()
                for j in range(4):
                    kc = half * 4 + j
                    S.tr(ps[:, j * 128:(j + 1) * 128], b[:, kc * 128:(kc + 1) * 128], ident,
                         [K("xin", i % 2), K("c128")], [pk])
                S.cp(dst[:, half * 4:half * 4 + 4, i * 128:(i + 1) * 128],
                     ps[:].rearrange("p (a b) -> p a b", a=4), [pk],
                     [K(dkey, half * 4 + j, i // 4) for j in range(4)], eng=("dve" if half == 0 else "act"))

    with ExitStack() as st:
        load_fm(xT, x_d, NT, "xT", st)
    S.barrier()
    dump("xT0", xT[:], [K("xT", kc, tc) for kc in range(KC) for tc in range(NTC)], [128, KC, S_])

    def rmsnorm_fm(src, skey, gcol, dst, dkey, nk, T, st, dscale=1.0, hook=None, tag="n"):
        tw = min(T, TW)
        sq = [sb(f"sq_{tag}{j}", [128, nk, tw], BF16, stack=st) for j in range(2)]
        rs = [sb(f"rs_{tag}{j}", [128, tw], stack=st) for j in range(2)]
        for tc in range(T // tw):
            sl = slice(tc * tw, (tc + 1) * tw)
            q = sq[tc % 2]
            for kc in range(nk):
                S.act(q[:, kc, :], src[:, kc, sl], AF.Square, [K(skey, kc, tc)], [K("sq" + tag, tc % 2, kc)])
            ps, pk = ps_rot()
            for kc in range(nk):
                S.mm(ps[:, 0:tw], ones_b, q[:, kc, :], kc == 0, kc == nk - 1,
                     [K("sq" + tag, tc % 2, kc), K("c128b")], [pk])
            r = rs[tc % 2]
            S.act(r[:], ps[:, 0:tw], AF.Sqrt, [pk, K("cst")], [K("rs" + tag, tc % 2)],
                  bias=cst[:, 0:1], scale=1.0 / (nk * 128))
            S.add("dve", (lambda e, r=r: e.reciprocal(out=r[:], in_=r[:])),
                  [K("rs" + tag, tc % 2)], [K("rs" + tag, tc % 2)])
            if dst is not None:
                for kc in range(nk):
                    S.stt(dst[:, kc, sl], src[:, kc, sl], gcol(kc), r[:], ALU.mult, ALU.mult,
                          [K(skey, kc, tc), K("rs" + tag, tc % 2), K("colp")], [K(dkey, kc, tc)],
                          eng="dve")
            if hook is not None:
                hook(tc, r)

    ALLH = [K("hT", kc, tc) for kc in range(KC) for tc in range(NTC)]

    def hkeys(tc):
        return [K("hT", kc, tc) for kc in range(KC)]

    def proj_fm(wv, wkeys, c0, tc, ps, pk):
        for kc in range(KC):
            S.mm(ps[:, :], wv[:, kc, c0:c0 + 128], hT[:, kc, tc * TW:(tc + 1) * TW], kc == 0, kc == KC - 1,
                 wkeys + [K("hT", kc, tc)], [pk])

    def conv_chunk(ps, pk, tc, xw, xwk, cwcol, cbcol, dst, dkey):
        w_ = xw[tc % 2]
        wk = K(xwk, tc % 2)
        if tc == 0:
            S.add("dve", lambda e: e.memset(w_[:, 0:3], 0.0), [], [wk])
        else:
            S.cp(w_[:, 0:3], xw[(tc - 1) % 2][:, 512:515], [K(xwk, (tc - 1) % 2)], [wk])
        S.cp(w_[:, 3:515], ps[:, :], [pk], [wk], eng="act")
        S.ts(dst, w_[:, 3:515], cwcol(3), cbcol, ALU.mult, ALU.add, [wk, K("colp")], [dkey])
        for k in range(3):
            S.stt(dst, w_[:, k:k + 512], cwcol(k), dst, ALU.mult, ALU.add, [wk, dkey, K("colp")], [dkey])

    def group_out(l, g, YG, st):
        YB = sb("YB", [128, 2, S_], BF16, stack=st)
        rmsnorm_fm(YG, "YG", lambda kc: col(l, "grp_g", 2 * g + kc), YB, "YB", 2, S_, st, tag="g")
        dump(f"yn{l}_{g}", YB[:], [K("YB", c, t) for c in range(2) for t in range(NTC)], [128, 2, S_])
        for half in range(2):
            wv, wk, _ = load_w(w_out_d[l, g * 256:(g + 1) * 256, half * 512:(half + 1) * 512], 2, 512)
            for o4 in range(4):
                oc = half * 4 + o4
                for tc in range(NTC):
                    ps, pk = ps_rot()
                    for kc in range(2):
                        S.mm(ps[:, :], wv[:, kc, o4 * 128:(o4 + 1) * 128], YB[:, kc, tc * TW:(tc + 1) * TW],
                             kc == 0, kc == 1, wk + [K("YB", kc, tc)], [pk])
                    S.tt(xT[:, oc, tc * TW:(tc + 1) * TW], xT[:, oc, tc * TW:(tc + 1) * TW], ps[:, :], ALU.add,
                         [K("xT", oc, tc), pk], [K("xT", oc, tc)])

    ALLX = [K("xT", kc, tc) for kc in range(KC) for tc in range(NTC)]
    done = False
    for l in range(n_layers):
        with ExitStack() as st:
            rmsnorm_fm(xT, "xT", lambda kc: col(l, "mix_g", kc), hT, "hT", KC, S_, st, tag="a")
        S.barrier()
        if l == 0:
            dump("h0", hT[:], ALLH, [128, KC, S_])
        if stop_after == "norm0":
            break

        with ExitStack() as sty, ExitStack() as st:
            YG = sb("YG", [128, 2, S_], stack=sty)
            wv, wk, _ = load_w(w_in_d[l, :, 0:512], 8, 512)
            wbd = sb("wbd", [128, 4, 128], BF16, stack=st)
            S.dma(wbd[:], lrubd_d[l].rearrange("g c k m -> k (g c) m"), [], [K("wbd")], q="pool", arena=True)
            c8 = sb("c8", [128, 2], stack=st)
            dump(f"A_wbd{l}", wbd[:], [K("wbd")], [128, 4, 128])
            cy = sb("cy", [128, 2], stack=st)
            cz = sb("cz", [128, 2], stack=st)
            S.act(cy[:], col(l, "lru_lam", 0, 2), AF.Exp, [K("colp")], [K("cy")], scale=-1.0)
            S.ts(cz[:], cy[:], 2.0, None, ALU.add, None, [K("cy")], [K("cz")])
            S.add("dve", lambda e: e.reciprocal(out=cz[:], in_=cz[:]), [K("cz")], [K("cz")])
            S.tt(cz[:], cz[:], cy[:], ALU.mult, [K("cz"), K("cy")], [K("cz")])
            S.tt(cy[:], cz[:], cz[:], ALU.mult, [K("cz")], [K("cy")])
            S.ts(c8[:], cy[:], 1.0 / 9.0, 1.0 / 7.0, ALU.mult, ALU.add, [K("cy")], [K("c8")])
            for cc_ in (1.0 / 5.0, 1.0 / 3.0, 1.0):
                S.tt(c8[:], c8[:], cy[:], ALU.mult, [K("c8"), K("cy")], [K("c8")])
                S.ts(c8[:], c8[:], cc_, None, ALU.add, None, [K("c8")], [K("c8")])
            S.tt(c8[:], c8[:], cz[:], ALU.mult, [K("c8"), K("cz")], [K("c8")])
            S.ts(c8[:], c8[:], -16.0, None, ALU.mult, None, [K("c8")], [K("c8")])
            xw = [sb(f"xw{j}", [128, 515], stack=st) for j in range(2)]
            XC = sb("XC", [128, S_], stack=st)
            XCB = sb("XCB", [128, S_], BF16, stack=st)
            GG = sb("GG", [128, S_], stack=st)
            RA = sb("RA", [128, S_], stack=st)
            IU = sb("IU", [128, S_], stack=st)
            T2 = sb("T2", [128, S_], stack=st)
            XX = sb("XX", [128, S_], stack=st)
            for c in range(2):
                for tc in range(NTC):
                    sl = slice(tc * TW, (tc + 1) * TW)
                    ps, pk = ps_rot()
                    proj_fm(wv, wk, c * 128, tc, ps, pk)
                    conv_chunk(ps, pk, tc, xw, "xw", lambda k: col(l, "lru_cw", c * 4 + k), col(l, "lru_cb", c),
                               XC[:, sl], K("XC", tc))
                    ps, pk = ps_rot()
                    proj_fm(wv, wk, 256 + c * 128, tc, ps, pk)
                    S.cp(GG[:, sl], ps[:, :], [pk], [K("GG", tc)], eng="act")
                XCk = [K("XC", t) for t in range(NTC)]
                GGk = [K("GG", t) for t in range(NTC)]
                S.tt(T2[:], GG[:], GG[:], ALU.mult, GGk, [K("T2")])
                S.ts(T2[:], T2[:], 0.044715, 1.0, ALU.mult, ALU.add, [K("T2")], [K("T2")])
                S.tt(T2[:], T2[:], GG[:], ALU.mult, [K("T2")] + GGk, [K("T2")])
                S.act(T2[:], T2[:], AF.Sigmoid, [K("T2")], [K("T2")], scale=1.5957691216)
                S.tt(GG[:], GG[:], T2[:], ALU.mult, [K("T2")] + GGk, GGk)
                S.cp(XCB[:], XC[:], XCk, [K("XCB")], eng="act")
                for tc in range(NTC):
                    sl = slice(tc * TW, (tc + 1) * TW)
                    ps, pk = ps_rot()
                    S.mm(ps[:, :], wbd[:, c, :], XCB[:, sl], True, True, [K("wbd"), K("XCB")], [pk])
                    S.act(RA[:, sl], ps[:, :], AF.Sigmoid, [pk, K("colp")], [K("RA", tc)], bias=col(l, "lru_br", c))
                    ps, pk = ps_rot()
                    S.mm(ps[:, :], wbd[:, 2 + c, :], XCB[:, sl], True, True, [K("wbd"), K("XCB")], [pk])
                    S.act(IU[:, sl], ps[:, :], AF.Sigmoid, [pk, K("colp")], [K("IU", tc)], bias=col(l, "lru_bi", c))
                RAk = [K("RA", t) for t in range(NTC)]
                IUk = [K("IU", t) for t in range(NTC)]
                if c == 1:
                    dump(f"A_r{l}", RA[:], RAk, [128, S_])
                    dump(f"A_i{l}", IU[:], IUk, [128, S_])
                    dump(f"A_xcb{l}", XCB[:], [K("XCB")], [128, S_])
                    dump(f"A_xc{l}", XC[:], XCk, [128, S_])
                S.ts(XX[:], RA[:], c8[:, c:c + 1], 2.0, ALU.mult, ALU.mult, RAk + [K("c8")], [K("XX")])
                S.ts(T2[:], XX[:], 1.0 / 5040.0, 1.0 / 720.0, ALU.mult, ALU.add, [K("XX")], [K("T2")])
                for cc_ in (1.0 / 120.0, 1.0 / 24.0, 1.0 / 6.0, 0.5, 1.0):
                    S.tt(T2[:], T2[:], XX[:], ALU.mult, [K("T2"), K("XX")], [K("T2")])
                    S.ts(T2[:], T2[:], cc_, None, ALU.add, None, [K("T2")], [K("T2")])
                S.stt(T2[:], T2[:], -1.0, XX[:], ALU.mult, ALU.mult, [K("T2"), K("XX")], [K("T2")])
                S.act(T2[:], T2[:], AF.Sqrt, [K("T2")], [K("T2")])
                S.act(RA[:], XX[:], AF.Exp, [K("XX")], RAk, scale=0.5)
                S.tt(IU[:], IU[:], XC[:], ALU.mult, IUk + XCk, IUk)
                S.tt(IU[:], IU[:], T2[:], ALU.mult, IUk + [K("T2")], IUk)
                S.add("dve", lambda e: e.tensor_tensor_scan(out=XC[:], data0=RA[:], data1=IU[:], initial=0.0,
                                                            op0=ALU.mult, op1=ALU.add), RAk + IUk, XCk)
                S.tt(YG[:, c, :], XC[:], GG[:], ALU.mult, XCk + GGk, [K("YG", c, t) for t in range(NTC)])
            for nm_, t_ in (("XC", XC), ("GG", GG), ("RA", RA), ("IU", IU), ("T2", T2)):
                dump(f"A_{nm_}{l}", t_[:], [K(nm_, t) for t in range(NTC)] + [K(nm_)], [128, S_])
            dump(f"ya{l}", YG[:], [K("YG", c, t) for c in range(2) for t in range(NTC)], [128, 2, S_])
            st.close()
            S.barrier()
            group_out(l, 0, YG, sty)
        S.barrier()
        if stop_after == f"A{l}":
            done = True
            break

        with ExitStack() as sty, ExitStack() as st:
            YG = sb("YG", [128, 2, S_], stack=sty)
            QB = sb("QB", [128, 2, S_], BF16, stack=st)
            KB = sb("KB", [128, 2, S_], BF16, stack=st)
            VB = sb("VB", [128, NT, 256], BF16, stack=st)
            M01 = sb("M01", [128, 4, 512], stack=st)
            NM = sb("NM", [128, 4, 512], BF16, stack=st)
            S.dma(M01[:], cmask_d[:, 0:2048].rearrange("p (a b) -> p a b", a=4), [], [K("M01")])
            S.dma(NM[:], cmask_d[:, 2048:4096].rearrange("p (a b) -> p a b", a=4), [], [K("NM")], q="pool", arena=True)
            wv, wk, _ = load_w(w_in_d[l, :, 512:1024], 8, 512)
            wv2, wk2, _ = load_w(w_in_d[l, :, 1024:1280], 8, 256)
            for c2 in range(2):
                for tc in range(NTC):
                    sl = slice(tc * TW, (tc + 1) * TW)
                    ps, pk = ps_rot()
                    proj_fm(wv, wk, c2 * 128, tc, ps, pk)
                    S.act(QB[:, c2, sl], ps[:, :], AF.Copy, [pk], [K("QB", c2, tc)], scale=0.125)
                    ps, pk = ps_rot()
                    proj_fm(wv, wk, 256 + c2 * 128, tc, ps, pk)
                    S.cp(KB[:, c2, sl], ps[:, :], [pk], [K("KB", c2, tc)])
            for i in range(NT):
                ps, pk = ps_rot()
                for kc in range(KC):
                    S.mm(ps[:, 0:256], hT[:, kc, i * 128:(i + 1) * 128], wv2[:, kc, :], kc == 0, kc == KC - 1,
                         wk2 + [K("hT", kc, i // 4)], [pk])
                S.cp(VB[:, i, :], ps[:, 0:256], [pk], [K("VB", i)], eng=("dve" if i % 2 else "act"))
            Eb = [sb(f"Eb{j}", [128, 512], stack=st) for j in range(2)]
            SPb = [sb(f"SPb{j}", [128, 512], F32R, stack=st) for j in range(3)]
            Wb = [sb(f"Wb{j}", [128, 512], BF16, stack=st) for j in range(2)]
            Rb = [sb(f"Rb{j}", [128, 512], F32R, stack=st) for j in range(2)]
            NUr = sb("NUr", [128, 2, 128], F32R, stack=st)
            S.cp(NUr[:, 0, :], negu, [K("c128")], [K("NUr")])
            S.cp(NUr[:, 1, :], negones, [K("c128")], [K("NUr")])
            items = []
            grp = 0
            for h in range(4):
                for c in range(NTC):
                    nb = 4 * c + 4
                    for idx, b in enumerate(range(nb - 1, -1, -1)):
                        items.append(dict(h=h, c=c, idx=idx, b=b, grp=grp, last=(b == 0)))
                    grp += 1
            for i_, itm in enumerate(items):
                itm["i"] = i_
                itm["pr"] = slice((itm["h"] % 2) * 64, (itm["h"] % 2) * 64 + 64)
                itm["c2"] = itm["h"] // 2
                itm["d"] = itm["b"] - 4 * itm["c"]
                itm["ksl"] = slice(itm["b"] * 128, (itm["b"] + 1) * 128)
                itm["qsl"] = slice(itm["c"] * TW, (itm["c"] + 1) * TW)
                itm["qk"] = [K("KB", itm["c2"], itm["b"] // 4), K("QB", itm["c2"], itm["c"])]

            def sb_s1(m):
                i = m["i"]
                pr, c2 = m["pr"], m["c2"]
                psZ, pkZ = ps_rot()
                S.mm(psZ[:, :], KB[pr, c2, m["ksl"]], QB[pr, c2, m["qsl"]], True, True, m["qk"], [pkZ])
                E, Ek = Eb[i % 2], K("Eb", i % 2)
                SP, SPk = SPb[i % 3], K("SPb", i % 3)
                S.act(E[:], psZ[:, :], AF.Exp, [pkZ], [Ek])
                S.act(SP[:], E[:], AF.Ln, [Ek, K("cst")], [SPk], bias=cst[:, 1:2])
                if m["d"] >= 0:
                    S.tt(SP[:], SP[:].bitcast(F32), M01[:, m["d"], :], ALU.mult, [SPk, K("M01")], [SPk])

            def sb_s2(m):
                i = m["i"]
                pr, c2, d = m["pr"], m["c2"], m["d"]
                SP, SPk = SPb[i % 3], K("SPb", i % 3)
                Wt, Wk_ = Wb[i % 2], K("Wb", i % 2)
                R, Rk = Rb[m["grp"] % 2], K("Rb", m["grp"] % 2)
                psA, pkA = ps_rot()
                has_r = m["idx"] > 0
                S.mm(psA[:, :], KB[pr, c2, m["ksl"]], QB[pr, c2, m["qsl"]], True, False, m["qk"], [pkA])
                S.mm(psA[:, :], NUr[:, 0, :], SP[:], False, (not has_r) and d < 0, [SPk, K("NUr")], [pkA])
                if has_r:
                    S.mm(psA[:, :], NUr[:, 1, :], R[:], False, d < 0, [Rk, K("NUr")], [pkA])
                if d >= 0:
                    S.mm(psA[:, :], ident_b, NM[:, d, :], False, True, [K("NM"), K("c128b")], [pkA])
                S.act(Wt[:], psA[:, :], AF.Exp, [pkA], [Wk_])
                if not m["last"]:
                    if m["idx"] == 0:
                        S.cp(R[:], SP[:].bitcast(F32), [SPk], [Rk])
                    else:
                        S.tt(R[:], R[:].bitcast(F32), SP[:].bitcast(F32), ALU.add, [Rk, SPk], [Rk])

            def sb_s3(m):
                i = m["i"]
                pr, c2, h = m["pr"], m["c2"], m["h"]
                Wt, Wk_ = Wb[i % 2], K("Wb", i % 2)
                psO, pkO = PS[4 + m["grp"] % 2], K("psO", m["grp"] % 2)
                S.mm(psO[pr, :], VB[:, m["b"], h * 64:(h + 1) * 64], Wt[:], m["idx"] == 0, m["last"],
                     [K("VB", m["b"]), Wk_], [pkO])
                if m["last"]:
                    S.cp(YG[pr, c2, m["qsl"]], psO[pr, :], [pkO], [K("YG", c2, m["c"])], eng="act")

            n_it = len(items)
            for r in range(-2, n_it):
                if 0 <= r + 2 < n_it:
                    sb_s1(items[r + 2])
                if 0 <= r + 1 < n_it:
                    sb_s2(items[r + 1])
                if 0 <= r < n_it:
                    sb_s3(items[r])
            dump(f"yb{l}", YG[:], [K("YG", c, t) for c in range(2) for t in range(NTC)], [128, 2, S_])
            st.close()
            S.barrier()
            group_out(l, 1, YG, sty)
        S.barrier()
        if stop_after == f"B{l}":
            done = True
            break

        with ExitStack() as sty, ExitStack() as st:
            YG = sb("YG", [128, 2, S_], stack=sty)
            XS = sb("XS", [128, 2, S_], stack=st)
            BTb = sb("BTb", [128, 2, S_], BF16, stack=st)
            CTb = sb("CTb", [128, 2, S_], BF16, stack=st)
            BTM = sb("BTM", [128, NT, 256], BF16, stack=st)
            XTM = sb("XTM", [128, NT, 256], BF16, stack=st)
            wdt = sb("wdt", [128, KC, 4], BF16, stack=st)
            stc = ExitStack()
            xw = [sb(f"xwc{j}", [128, 515], stack=stc) for j in range(2)]
            CV = [sb(f"CV{j}", [128, 512], stack=stc) for j in range(2)]
            BF = [sb(f"BF{j}", [128, 512], stack=stc) for j in range(2)]
            S.dma(wdt[:], w_in_d[l, :, 2304:2308].rearrange("(kc p) n -> p kc n", p=128), [], [K("wdt")], q="pool", arena=True)
            wv1, wk1, _ = load_w(w_in_d[l, :, 1280:1792], 8, 512)
            wv2, wk2, _ = load_w(w_in_d[l, :, 1792:2304], 8, 512)
            n_cv = 0
            for j in range(6):
                if j < 2:
                    wv, wk, c0 = wv1, wk1, 256 + j * 128
                elif j < 4:
                    wv, wk, c0 = wv2, wk2, (j - 2) * 128
                else:
                    wv, wk, c0 = wv2, wk2, 256 + (j - 4) * 128
                for tc in range(NTC):
                    sl = slice(tc * TW, (tc + 1) * TW)
                    ps, pk = ps_rot()
                    proj_fm(wv, wk, c0, tc, ps, pk)
                    cv, cvk = CV[n_cv % 2], K("CV", n_cv % 2)
                    conv_chunk(ps, pk, tc, xw, "xwc", lambda k: col(l, "ssm_cw", j * 4 + k), col(l, "ssm_cb", j),
                               cv[:], cvk)
                    if j < 2:
                        S.act(XS[:, j, sl], cv[:], AF.Silu, [cvk], [K("XS", j, tc)])
                        ps, pk = ps_rot()
                        for q in range(4):
                            S.tr(ps[:, q * 128:(q + 1) * 128], XS[:, j, tc * TW + q * 128: tc * TW + (q + 1) * 128],
                                 ident, [K("XS", j, tc), K("c128")], [pk])
                        S.cp(XTM[:, tc * 4:tc * 4 + 4, j * 128:(j + 1) * 128],
                             ps[:].rearrange("p (a b) -> p a b", a=4), [pk], [K("XTM", tc, j)])
                    elif j < 4:
                        g = j - 2
                        bf, bfk = BF[n_cv % 2], K("BF", n_cv % 2)
                        S.act(bf[:], cv[:], AF.Silu, [cvk], [bfk])
                        S.cp(BTb[:, g, sl], bf[:], [bfk], [K("BTb", g, tc)])
                        ps, pk = ps_rot()
                        for q in range(4):
                            S.tr(ps[:, q * 128:(q + 1) * 128], bf[:, q * 128:(q + 1) * 128], ident,
                                 [bfk, K("c128")], [pk])
                        S.cp(BTM[:, tc * 4:tc * 4 + 4, g * 128:(g + 1) * 128],
                             ps[:].rearrange("p (a b) -> p a b", a=4), [pk], [K("BTM", tc, g)])
                    else:
                        S.act(CTb[:, j - 4, sl], cv[:], AF.Silu, [cvk], [K("CTb", j - 4, tc)])
                    n_cv += 1
            stc.close()
            S.barrier()
            DT = sb("DT", [128, 64], stack=st)
            AN = sb("AN", [128, 64], stack=st)
            AC = sb("AC", [128, 64], stack=st)
            ACS = sb("ACS", [128, 64], stack=st)
            TOT = sb("TOT", [128, 64], stack=st)
            DEC = sb("DEC", [128, 64], stack=st)
            ETOT = sb("ETOT", [128, 64], stack=st)
            DTD = sb("DTD", [128, 64], stack=st)
            psd, pkd = ps_rot()
            for i in range(NT):
                for kc in range(KC):
                    S.mm(psd[:, i * 4:(i + 1) * 4], hT[:, kc, i * 128:(i + 1) * 128], wdt[:, kc, :], kc == 0,
                         kc == KC - 1, [K("wdt"), K("hT", kc, i // 4)], [pkd])
            S.tt(DT[:], psd[:, 0:64], row(l, "dt_bias"), ALU.add, [pkd, K("rowp")], [K("DT")])
            S.act(DT[:], DT[:], AF.Exp, [K("DT")], [K("DT")])
            S.act(DT[:], DT[:], AF.Ln, [K("DT"), K("cst")], [K("DT")], bias=cst[:, 1:2])
            S.act(AN[:], row(l, "a_log"), AF.Exp, [K("rowp")], [K("AN")])
            S.ts(AN[:], AN[:], -1.0, None, ALU.mult, None, [K("AN")], [K("AN")])
            S.tt(AC[:], DT[:], AN[:], ALU.mult, [K("DT"), K("AN")], [K("AC")])
            ps, pk = ps_rot()
            S.mm(ps[:, 0:64], tri_le, AC[:], True, True, [K("AC"), K("c128")], [pk])
            S.cp(ACS[:], ps[:, 0:64], [pk], [K("ACS")])
            ps, pk = ps_rot()
            S.mm(ps[:, 0:64], ones_f, AC[:], True, True, [K("AC"), K("c128")], [pk])
            S.cp(TOT[:], ps[:, 0:64], [pk], [K("TOT")])
            S.tt(DEC[:], TOT[:], ACS[:], ALU.subtract, [K("TOT"), K("ACS")], [K("DEC")])
            S.act(DEC[:], DEC[:], AF.Exp, [K("DEC")], [K("DEC")])
            S.act(ETOT[:], TOT[:], AF.Exp, [K("TOT")], [K("ETOT")])
            S.tt(DTD[:], DT[:], DEC[:], ALU.mult, [K("DT"), K("DEC")], [K("DTD")])
            ST = sb("ST", [128, 4, 64], stack=st)
            STb = sb("STb", [128, 4, 64], BF16, stack=st)
            S.add("dve", lambda e: e.memset(ST[:], 0.0), [], [K("ST", h) for h in range(4)])
            S.add("dve", lambda e: e.memset(STb[:], 0.0), [], [K("STb", h) for h in range(4)])
            Rm = [sb(f"Rm{j}", [128, 128], stack=st) for j in range(2)]
            ARG = [sb(f"ARG{j}", [128, 128], stack=st) for j in range(2)]
            E1 = [sb(f"E1{j}", [128, 128], stack=st) for j in range(2)]
            GL = [sb(f"GL{j}", [128, 128], BF16, stack=st) for j in range(2)]
            CTp = [sb(f"CTp{j}", [128, 128], BF16, stack=st) for j in range(2)]
            XCh = [sb(f"XCh{j}", [128, 64], BF16, stack=st) for j in range(2)]
            XDh = [sb(f"XDh{j}", [128, 64], BF16, stack=st) for j in range(2)]
            n = 0
            for ci in range(NT):
                csl = slice(ci * 128, (ci + 1) * 128)
                tcx = ci // 4
                for g in range(2):
                    psG, pkG = ps_rot()
                    S.mm(psG[:, 0:128], BTb[:, g, csl], CTb[:, g, csl], True, True,
                         [K("BTb", g, tcx), K("CTb", g, tcx)], [pkG])
                    psY, pkY = PS[4 + (ci * 2 + g) % 2], K("psO", (ci * 2 + g) % 2)
                    for hh in range(2):
                        h = 2 * g + hh
                        pr = slice(hh * 64, hh * 64 + 64)
                        cidx = ci * 4 + h
                        k2 = n % 2
                        S.ts(Rm[k2][:], tri_le, AC[:, cidx:cidx + 1], None, ALU.mult, None,
                             [K("AC"), K("c128")], [K("Rm", k2)])
                        ps1, pk1 = ps_rot()
                        S.mm(ps1[:, 0:128], ones_f, Rm[k2][:], True, True, [K("Rm", k2), K("c128")], [pk1])
                        S.stt(ARG[k2][:], ps1[:, 0:128], ACS[:, cidx:cidx + 1], ssdneg, ALU.subtract, ALU.add,
                              [pk1, K("ACS"), K("c128")], [K("ARG", k2)])
                        S.act(ARG[k2][:], ARG[k2][:], AF.Exp, [K("ARG", k2)], [K("ARG", k2)])
                        S.tt(GL[k2][:], ARG[k2][:], psG[:, 0:128], ALU.mult, [K("ARG", k2), pkG], [K("GL", k2)])
                        S.act(E1[k2][:], ps1[:, 0:128], AF.Exp, [pk1], [K("E1", k2)])
                        S.tt(CTp[k2][:], CTb[:, g, csl], E1[k2][:], ALU.mult, [K("CTb", g, tcx), K("E1", k2)],
                             [K("CTp", k2)])
                        S.ts(XCh[k2][:], XTM[:, ci, h * 64:(h + 1) * 64], DT[:, cidx:cidx + 1], None, ALU.mult, None,
                             [K("XTM", tcx, g), K("DT")], [K("XCh", k2)])
                        S.mm(psY[pr, 0:128], XCh[k2][:], GL[k2][:], True, False, [K("XCh", k2), K("GL", k2)], [pkY])
                        S.mm(psY[pr, 0:128], STb[:, h, :], CTp[k2][:], False, True, [K("STb", h), K("CTp", k2)], [pkY])
                        S.ts(XDh[k2][:], XTM[:, ci, h * 64:(h + 1) * 64], DTD[:, cidx:cidx + 1], None, ALU.mult, None,
                             [K("XTM", tcx, g), K("DTD")], [K("XDh", k2)])
                        psS, pkS = ps_rot()
                        S.mm(psS[:, 0:64], BTM[:, ci, g * 128:(g + 1) * 128], XDh[k2][:], True, True,
                             [K("BTM", tcx, g), K("XDh", k2)], [pkS])
                        S.stt(ST[:, h, :], ST[:, h, :], ETOT[:, cidx:cidx + 1], psS[:, 0:64], ALU.mult, ALU.add,
                              [K("ST", h), K("ETOT"), pkS], [K("ST", h)])
                        S.cp(STb[:, h, :], ST[:, h, :], [K("ST", h)], [K("STb", h)], eng="act")
                        n += 1
                    S.cp(YG[:, g, csl], psY[:, 0:128], [pkY], [K("YG", g, tcx)], eng="act")
            ARG = [sb(f"ZT{j}", [128, 512], stack=st) for j in range(2)]
            for j in range(2):
                YGk = [K("YG", j, t) for t in range(NTC)]
                S.stt(YG[:, j, :], XS[:, j, :], col(l, "ssm_d", j), YG[:, j, :], ALU.mult, ALU.add,
                      [K("XS", j, t) for t in range(NTC)] + YGk + [K("colp")], YGk)
                for tc in range(NTC):
                    sl = slice(tc * TW, (tc + 1) * TW)
                    ps, pk = ps_rot()
                    proj_fm(wv1, wk1, j * 128, tc, ps, pk)
                    zt, ztk = ARG[tc % 2], K("ARGz", tc % 2)
                    S.act(zt[:], ps[:, :], AF.Silu, [pk], [ztk])
                    S.tt(YG[:, j, sl], YG[:, j, sl], zt[:], ALU.mult, [K("YG", j, tc), ztk], [K("YG", j, tc)])
            dump(f"yc{l}", YG[:], [K("YG", c, t) for c in range(2) for t in range(NTC)], [128, 2, S_])
            st.close()
            S.barrier()
            group_out(l, 2, YG, sty)
        S.barrier()
        if stop_after == f"C{l}":
            done = True
            break

        with ExitStack() as sty, ExitStack() as st:
            YG = sb("YG", [128, 2, S_], stack=sty)
            ROPEb = [sb(f"ROPE{j}", [128, 2, TW], stack=st) for j in range(2)]
            QB = sb("QB", [128, 2, S_], BF16, stack=st)
            KB = sb("KB", [128, 2, S_], BF16, stack=st)
            VB = sb("VB", [128, NT, 256], BF16, stack=st)
            CAUS = sb("CAUS", [128, 4, 512], BF16, stack=st)
            S.dma(CAUS[:], cmask_d[:, 4096:6144].rearrange("p (a b) -> p a b", a=4), [], [K("CAUS")], q="pool", arena=True)
            ENb = sb("ENb", [8, 8, 128], BF16, stack=st)
            S.dma(ENb[:], en_d[0:8, 0:1024].rearrange("p (a b) -> p a b", a=8), [], [K("ENb")], q="pool", arena=True)
            GC = sb("GC", [128, 3, 128], stack=st)
            S.dma(GC[:], gate_c_d[:, :].rearrange("p (a b) -> p a b", a=3), [], [K("GC")])
            KM = sb("KM", [128, 2, 8], stack=st)
            RAW = [sb(f"RAW{j}", [128, 512], stack=st) for j in range(2)]
            TMP = [sb(f"TMPr{j}", [128, 512], stack=st) for j in range(2)]
            KF = [sb(f"KF{j}", [128, 512], stack=st) for j in range(2)]
            wv, wk, _ = load_w(w_in_d[l, :, 2308:2820], 8, 512)
            wv2, wk2, _ = load_w(w_in_d[l, :, 2820:3076], 8, 256)
            nr = 0
            for c2 in range(2):
                for tc in range(NTC):
                    sl = slice(tc * TW, (tc + 1) * TW)
                    rj = (c2 * NTC + tc) % 2
                    ROPE = ROPEb[rj]
                    rsl = slice(0, TW)
                    S.dma(ROPE[:, 0, :], rope_d[:, tc * TW:(tc + 1) * TW], [], [K("ROPE", rj, 0)])
                    S.dma(ROPE[:, 1, :], rope_d[:, S_ + tc * TW:S_ + (tc + 1) * TW], [], [K("ROPE", rj, 1)])
                    for which in range(2):
                        ps, pk = ps_rot()
                        proj_fm(wv, wk, which * 256 + c2 * 128, tc, ps, pk)
                        raw, rawk = RAW[nr % 2], K("RAW", nr % 2)
                        tmp, tmpk = TMP[nr % 2], K("TMPr", nr % 2)
                        S.cp(raw[:], ps[:, :], [pk], [rawk], eng="act")
                        psw, pkw = ps_rot()
                        S.mm(psw[:, :], perm_f, raw[:], True, True, [rawk, K("c128")], [pkw])
                        S.tt(tmp[:], psw[:, :], ROPE[:, 1, rsl], ALU.mult, [pkw, K("ROPE", rj, 1)], [tmpk])
                        dst, dk = KF[nr % 2][:], K("KF", nr % 2)
                        S.tt(dst, raw[:], ROPE[:, 0, rsl], ALU.mult, [rawk, K("ROPE", rj, 0)], [dk])
                        S.tt(dst, dst, tmp[:], ALU.add, [dk, tmpk], [dk])
                        if which == 0:
                            S.act(QB[:, c2, sl], dst, AF.Copy, [dk], [K("QB", c2, tc)], scale=0.125)
                        else:
                            S.cp(KB[:, c2, sl], dst, [dk], [K("KB", c2, tc)], eng="act")
                            for hb in range(2):
                                nblk = tc * 2 + hb
                                S.add("dve", (lambda e, o=KM[:, c2, nblk:nblk + 1], i_=KF[nr % 2][:, hb * 256:(hb + 1) * 256]:
                                              e.reduce_sum(out=o, in_=i_, axis=AX.X)), [dk], [K("KM", c2, nblk)])
                        nr += 1
            for i in range(NT):
                ps, pk = ps_rot()
                for kc in range(KC):
                    S.mm(ps[:, 0:256], hT[:, kc, i * 128:(i + 1) * 128], wv2[:, kc, :], kc == 0, kc == KC - 1,
                         wk2 + [K("hT", kc, i // 4)], [pk])
                S.cp(VB[:, i, :], ps[:, 0:256], [pk], [K("VB", i)], eng=("dve" if i % 2 else "act"))
            KMb = sb("KMb", [128, 2, 8], BF16, stack=st)
            S.cp(KMb[:], KM[:], [K("KM", c2_, nb_) for c2_ in range(2) for nb_ in range(8)], [K("KMb")])
            PTb = [sb(f"PTb{j}", [128, 512], BF16, stack=st) for j in range(3)]
            RD = [sb(f"RD{j}", [128, 512], stack=st) for j in range(2)]
            cnt_ = dict(it=0, grp=0)

            def attend(h):
                it0 = cnt_["it"]
                grp0 = cnt_["grp"]
                hh, c2 = h % 2, h // 2
                pr = slice(hh * 64, hh * 64 + 64)
                its = []
                for c in range(NTC):
                    nkt = 4 * c + 4
                    for kt in range(nkt):
                        its.append(dict(c=c, kt=kt, nkt=nkt, grp=grp0 + c, i=it0 + len(its)))

                def m_s1(m):
                    c, kt = m["c"], m["kt"]
                    d = kt - 4 * c
                    qsl = slice(c * TW, (c + 1) * TW)
                    ksl = slice(kt * 128, (kt + 1) * 128)
                    psS, pkS = ps_rot()
                    S.mm(psS[:, :], KB[pr, c2, ksl], QB[pr, c2, qsl], True, False,
                         [K("KB", c2, kt // 4), K("QB", c2, c)], [pkS])
                    S.mm(psS[:, :], ENb[0:8, kt // 2, :], SELB[0:8, qsl], False, d < 0,
                         [K("ENb"), K("SELB", c)], [pkS])
                    if d >= 0:
                        S.mm(psS[:, :], ident_b, CAUS[:, d, :], False, True, [K("CAUS"), K("c128b")], [pkS])
                    pt, ptk = PTb[m["i"] % 3], K("PTb", m["i"] % 3)
                    S.act(pt[:], psS[:, :], AF.Exp, [pkS], [ptk])

                def m_s2(m):
                    c, kt, nkt = m["c"], m["kt"], m["nkt"]
                    qsl = slice(c * TW, (c + 1) * TW)
                    g2 = m["grp"] % 2
                    psO, pkO = PS[4 + g2], K("psO", g2)
                    psD, pkD = PS[6 + g2], K("psD", g2)
                    pt, ptk = PTb[m["i"] % 3], K("PTb", m["i"] % 3)
                    S.mm(psO[pr, :], VB[:, kt, h * 64:(h + 1) * 64], pt[:], kt == 0, kt == nkt - 1,
                         [K("VB", kt), ptk], [pkO])
                    S.mm(psD[:, :], ones_b, pt[:], kt == 0, kt == nkt - 1, [ptk, K("c128b")], [pkD])
                    if kt == nkt - 1:
                        rd, rdk = RD[g2], K("RD", g2)
                        S.add("dve", (lambda e, o=rd[pr, :], i_=psD[pr, :]: e.reciprocal(out=o, in_=i_)), [pkD], [rdk])
                        S.tt(YG[pr, c2, qsl], psO[pr, :], rd[pr, :], ALU.mult, [pkO, rdk], [K("YG", c2, c)])

                n_ = len(its)
                for r in range(-1, n_):
                    if r + 1 < n_:
                        m_s1(its[r + 1])
                    if r >= 0:
                        m_s2(its[r])
                cnt_["it"] = it0 + n_
                cnt_["grp"] = grp0 + NTC
            SELB = sb("SELB", [8, S_], BF16, stack=st)
            GT = sb("GT", [128, 128], stack=st)
            BT_ = sb("BIASTM", [128, 128], stack=st)
            MX = [sb(f"MX{j}", [128, 8], stack=st) for j in range(2)]
            KMk = [K("KM", c2_, nb_) for c2_ in range(2) for nb_ in range(8)]
            for h in range(4):
                hh, c2 = h % 2, h // 2
                pr = slice(hh * 64, hh * 64 + 64)
                psg, pkg = ps_rot()
                for i in range(NT):
                    S.mm(psg[:, i * 8:(i + 1) * 8], QB[pr, c2, i * 128:(i + 1) * 128], KMb[pr, c2, :], True, True,
                         [K("QB", c2, i // 4), K("KMb")], [pkg])
                S.stt(GT[:], psg[:, 0:128], 8.0 / 256.0, GC[:, 0, :], ALU.mult, ALU.add, [pkg, K("GC")], [K("GT")])
                for i in range(NT):
                    mx, mxk = MX[i % 2], K("MX", i % 2)
                    S.add("dve", (lambda e, o=mx[:], i_=GT[:, i * 8:(i + 1) * 8]: e.max(out=o, in_=i_)),
                          [K("GT")], [mxk])
                    bsl = BT_[:, i * 8:(i + 1) * 8]
                    S.ts(bsl, GT[:, i * 8:(i + 1) * 8], mx[:, 2:3], None, ALU.is_ge, None, [K("GT"), mxk],
                         [K("BIASTM", i)])
                    S.tt(bsl, bsl, GC[:, 1, i * 8:(i + 1) * 8], ALU.mult, [K("BIASTM", i), K("GC")], [K("BIASTM", i)])
                    S.tt(bsl, bsl, GC[:, 2, i * 8:(i + 1) * 8], ALU.add, [K("BIASTM", i), K("GC")], [K("BIASTM", i)])
                    S.ts(bsl, bsl, -1.0, -NEG, ALU.add, ALU.mult, [K("BIASTM", i)], [K("BIASTM", i)])
                for q4 in range(4):
                    pst, pkt = ps_rot()
                    for q in range(4):
                        i = q4 * 4 + q
                        S.tr(pst[0:8, q * 128:(q + 1) * 128], BT_[:, i * 8:(i + 1) * 8], ident,
                             [K("BIASTM", i), K("c128")], [pkt])
                    S.cp(SELB[0:8, q4 * 512:(q4 + 1) * 512], pst[0:8, :], [pkt], [K("SELB", q4)])
                attend(h)
            dump(f"yd{l}", YG[:], [K("YG", c, t) for c in range(2) for t in range(NTC)], [128, 2, S_])
            st.close()
            S.barrier()
            group_out(l, 3, YG, sty)
        S.barrier()
        dump(f"x1_{l}", xT[:], ALLX, [128, KC, S_])
        if stop_after == f"D{l}":
            done = True
            break

        with ExitStack() as st:
            with ExitStack() as st2:
                rmsnorm_fm(xT, "xT", lambda kc: col(l, "xat_g", kc), hT, "hT", KC, S_, st2, tag="x")
            S.barrier()
            memT = sb("memT", [128, KC, 256], stack=st)
            MTb = sb("MTb", [128, KC, 256], BF16, stack=st)
            with ExitStack() as st2:
                load_fm(memT, mem_d, 2, "memT", st2)
                rmsnorm_fm(memT, "memT", lambda kc: col(l, "mem_g", kc), MTb, "MTb", KC, 256, st2, tag="m")
            S.barrier()
            KTb = sb("KTb", [128, KC, 256], BF16, stack=st)
            VX = sb("VX", [128, 2, D_], BF16, stack=st)
            QX = sb("QX", [128, KC, S_], BF16, stack=st)
            MTk = [K("MTb", kc, 0) for kc in range(KC)]
            for half in range(2):
                wv, wk, _ = load_w(wkv_d[l, :, half * 512:(half + 1) * 512], 8, 512)
                for o4 in range(4):
                    oc = half * 4 + o4
                    ps, pk = ps_rot()
                    for kc in range(KC):
                        S.mm(ps[:, 0:256], wv[:, kc, o4 * 128:(o4 + 1) * 128], MTb[:, kc, :], kc == 0, kc == KC - 1,
                             wk + [K("MTb", kc, 0)], [pk])
                    S.cp(KTb[:, oc, :], ps[:, 0:256], [pk], [K("KTb", oc)])
            for half in range(2):
                wv, wk, _ = load_w(wkv_d[l, :, 1024 + half * 512:1024 + (half + 1) * 512], 8, 512)
                for mt in range(2):
                    ps, pk = ps_rot()
                    for kc in range(KC):
                        S.mm(ps[:, :], MTb[:, kc, mt * 128:(mt + 1) * 128], wv[:, kc, :], kc == 0, kc == KC - 1,
                             wk + [K("MTb", kc, 0)], [pk])
                    S.cp(VX[:, mt, half * 512:(half + 1) * 512], ps[:, :], [pk], [K("VX", mt, half)], eng="act")
            for half in range(2):
                wv, wk, _ = load_w(wq_d[l, :, half * 512:(half + 1) * 512], 8, 512)
                for o4 in range(4):
                    oc = half * 4 + o4
                    for tc in range(NTC):
                        ps, pk = ps_rot()
                        proj_fm(wv, wk, o4 * 128, tc, ps, pk)
                        S.act(QX[:, oc, tc * TW:(tc + 1) * TW], ps[:, :], AF.Copy, [pk], [K("QX", oc, tc)],
                              scale=1.0 / 16.0)
            dump(f"X_MTb{l}", MTb[:], MTk, [128, KC, 256])
            dump(f"X_KTb{l}", KTb[:], [K("KTb", oc) for oc in range(KC)], [128, KC, 256])
            dump(f"X_QX{l}", QX[:], [K("QX", oc, tc) for oc in range(KC) for tc in range(NTC)], [128, KC, S_])
            dump(f"X_VX{l}", VX[:], [K("VX", mt, hf) for mt in range(2) for hf in range(2)], [128, 2, D_])
            PT = [sb(f"PTx{j}", [128, 512], BF16, stack=st) for j in range(4)]
            RDx = [sb(f"RDx{j}", [128, 512], stack=st) for j in range(2)]
            it = 0
            for hd in range(4):
                for tc in range(NTC):
                    sl = slice(tc * TW, (tc + 1) * TW)
                    pts = []
                    for mt in range(2):
                        ps, pk = ps_rot()
                        for j in range(2):
                            S.mm(ps[:, :], KTb[:, 2 * hd + j, mt * 128:(mt + 1) * 128], QX[:, 2 * hd + j, sl],
                                 j == 0, j == 1, [K("KTb", 2 * hd + j), K("QX", 2 * hd + j, tc)], [pk])
                        pt, ptk = PT[it % 4], K("PTx", it % 4)
                        S.act(pt[:], ps[:, :], AF.Exp, [pk], [ptk])
                        pts.append((pt, ptk))
                        it += 1
                    psD, pkD = ps_rot()
                    for mt in range(2):
                        S.mm(psD[:, :], ones_b, pts[mt][0][:], mt == 0, mt == 1, [pts[mt][1], K("c128b")], [pkD])
                    rd, rdk = RDx[(hd * NTC + tc) % 2], K("RDx", (hd * NTC + tc) % 2)
                    S.add("dve", (lambda e, o=rd[:], i_=psD[:, :]: e.reciprocal(out=o, in_=i_)), [pkD], [rdk])
                    for j in range(2):
                        oc = 2 * hd + j
                        ps, pk = ps_rot()
                        for mt in range(2):
                            S.mm(ps[:, :], VX[:, mt, oc * 128:(oc + 1) * 128], pts[mt][0][:], mt == 0, mt == 1,
                                 [K("VX", mt, oc // 4), pts[mt][1]], [pk])
                        S.tt(hT[:, oc, sl], ps[:, :], rd[:], ALU.mult, [pk, rdk], [K("hT", oc, tc)])
            dump(f"X_OX{l}", hT[:], ALLH, [128, KC, S_])
            for half in range(2):
                wv, wk, _ = load_w(wo_d[l, :, half * 512:(half + 1) * 512], 8, 512)
                for o4 in range(4):
                    oc = half * 4 + o4
                    for tc in range(NTC):
                        ps, pk = ps_rot()
                        proj_fm(wv, wk, o4 * 128, tc, ps, pk)
                        S.tt(xT[:, oc, tc * TW:(tc + 1) * TW], xT[:, oc, tc * TW:(tc + 1) * TW], ps[:, :], ALU.add,
                             [K("xT", oc, tc), pk], [K("xT", oc, tc)])
        S.barrier()
        dump(f"x2_{l}", xT[:], ALLX, [128, KC, S_])
        if stop_after == f"X{l}":
            done = True
            break

        with ExitStack() as st:
            WR = sb("WR", [128, KC, 20], stack=st)
            S.dma(WR[:], wrt_d[l].rearrange("(kc p) n -> p kc n", p=128), [], [K("WR")])
            ENf = sb("ENf", [16, 16, 128], stack=st)
            S.dma(ENf[:], en_d[:, :].rearrange("p (a b) -> p a b", a=16), [], [K("ENf")])
            LG = sb("LG", [128, NT, 20], stack=st)
            st_hf = ExitStack()
            HF = sb("HF", [128, KC, TW], stack=st_hf)

            def router_hook(tc, r):
                sl = slice(tc * TW, (tc + 1) * TW)
                for kc in range(KC):
                    S.stt(HF[:, kc, :], xT[:, kc, sl], col(l, "ffn_g", kc), r[:], ALU.mult, ALU.mult,
                          [K("xT", kc, tc), K("rsf", tc % 2), K("colp")], [K("HF", kc)])
                ps, pk = ps_rot()
                for q in range(4):
                    for kc in range(KC):
                        S.mm(ps[:, q * 20:(q + 1) * 20], HF[:, kc, q * 128:(q + 1) * 128], WR[:, kc, :], kc == 0,
                             kc == KC - 1, [K("HF", kc), K("WR")], [pk])
                S.cp(LG[:, tc * 4:tc * 4 + 4, :], ps[:, 0:80].rearrange("p (a b) -> p a b", a=4), [pk],
                     [K("LG", tc)])

            with ExitStack() as st2:
                rmsnorm_fm(xT, "xT", lambda kc: col(l, "ffn_g", kc), hT, "hT", KC, S_, st2, hook=router_hook, tag="f")
            st_hf.close()
            S.barrier()
            COMB = sb("COMB", [128, NT, 16], stack=st)
            CT = sb("CT", [16, S_], stack=st)
            sm = {nm: sb("rt_" + nm, [128, w_], stack=st) for nm, w_ in
                  [("lg", 4), ("m", 1), ("nm", 1), ("eg", 4), ("sg", 1), ("pt", 1), ("oh", 4), ("le", 16), ("sel", 4),
                   ("a", 1), ("na", 1), ("eq", 4), ("sel2", 4), ("b", 1), ("t2", 4), ("ex", 4), ("dd", 1), ("den", 1),
                   ("wg", 4)]}

            def v(nm):
                return sm[nm][:]

            def kk(nm):
                return [K("rt", nm)]

            for i in range(NT):
                lgk = [K("LG", i // 4)]
                S.tt(v("lg"), LG[:, i, 0:4], row(l, "bg"), ALU.add, lgk + [K("rowp")], kk("lg"))
                S.add("dve", lambda e: e.reduce_max(out=v("m"), in_=v("lg"), axis=AX.X), kk("lg"), kk("m"))
                S.ts(v("nm"), v("m"), -1.0, None, ALU.mult, None, kk("m"), kk("nm"))
                S.act(v("eg"), v("lg"), AF.Exp, kk("lg") + kk("nm"), kk("eg") + kk("sg"), bias=v("nm"), accum=v("sg"))
                S.add("dve", lambda e: e.reciprocal(out=v("pt"), in_=v("sg")), kk("sg"), kk("pt"))
                S.ts(v("oh"), v("lg"), v("m"), None, ALU.is_ge, None, kk("lg") + kk("m"), kk("oh"))
                S.tt(v("le"), LG[:, i, 4:20], row(l, "be"), ALU.add, lgk + [K("rowp")], kk("le"))
                S.ts(v("sel"), sm["le"][:, 0:4], sm["oh"][:, 0:1], None, ALU.mult, None, kk("le") + kk("oh"), kk("sel"))
                for g in range(1, 4):
                    S.stt(v("sel"), sm["le"][:, 4 * g:4 * g + 4], sm["oh"][:, g:g + 1], v("sel"), ALU.mult, ALU.add,
                          kk("le") + kk("oh") + kk("sel"), kk("sel"))
                S.add("dve", lambda e: e.reduce_max(out=v("a"), in_=v("sel"), axis=AX.X), kk("sel"), kk("a"))
                S.ts(v("eq"), v("sel"), v("a"), None, ALU.is_ge, None, kk("sel") + kk("a"), kk("eq"))
                S.stt(v("sel2"), v("eq"), -1e30, v("sel"), ALU.mult, ALU.add, kk("eq") + kk("sel"), kk("sel2"))
                S.add("dve", lambda e: e.reduce_max(out=v("b"), in_=v("sel2"), axis=AX.X), kk("sel2"), kk("b"))
                S.ts(v("t2"), v("sel"), v("b"), None, ALU.is_ge, None, kk("sel") + kk("b"), kk("t2"))
                S.ts(v("na"), v("a"), -1.0, None, ALU.mult, None, kk("a"), kk("na"))
                S.act(v("ex"), v("sel"), AF.Exp, kk("sel") + kk("na"), kk("ex"), bias=v("na"))
                S.act(v("dd"), v("b"), AF.Exp, kk("b") + kk("na"), kk("dd"), bias=v("na"))
                S.ts(v("den"), v("dd"), 1.0, None, ALU.add, None, kk("dd"), kk("den"))
                S.add("dve", lambda e: e.reciprocal(out=v("den"), in_=v("den")), kk("den"), kk("den"))
                S.tt(v("wg"), v("ex"), v("t2"), ALU.mult, kk("ex") + kk("t2"), kk("wg"))
                S.ts(v("wg"), v("wg"), v("den"), v("pt"), ALU.mult, ALU.mult, kk("wg") + kk("den") + kk("pt"), kk("wg"))
                for g in range(4):
                    S.ts(COMB[:, i, 4 * g:4 * g + 4], v("wg"), sm["oh"][:, g:g + 1], None, ALU.mult, None,
                         kk("wg") + kk("oh"), [K("COMB", i, g)])
            for q4 in range(4):
                pst, pkt = ps_rot()
                for q in range(4):
                    i = q4 * 4 + q
                    S.tr(pst[0:16, q * 128:(q + 1) * 128], COMB[:, i, :], ident,
                         [K("COMB", i, g) for g in range(4)] + [K("c128")], [pkt])
                S.cp(CT[0:16, q4 * 512:(q4 + 1) * 512], pst[0:16, :], [pkt], [K("CT", q4)])
            dump(f"comb{l}", CT[:], [K("CT", q) for q in range(4)], [16, S_])
            HID = sb("HID", [128, 8, S_], BF16, stack=st)
            W2S = sb("W2S", [128, 8, D_], BF16, stack=st)
            CB = [sb(f"CB{j}", [128, 512], stack=st) for j in range(2)]
            SL = [sb(f"SL{j}", [128, 512], stack=st) for j in range(2)]
            n = 0
            for g in range(4):
                for el in range(4):
                    ex = 4 * g + el
                    S.dma(W2S[:, 2 * el:2 * el + 2, :], w2_d[l, ex].rearrange("(fc p) n -> p fc n", p=128), [],
                          [K("W2S", el)], q="pool", arena=True)
                    wv, wk1_, slot = load_w(w1_d[l, ex], 8, 256, c0=0, tot=512)
                    _, wk3_, _ = load_w(w3_d[l, ex], 8, 256, slot=slot, c0=256, tot=512)
                    wk = wk1_ + wk3_
                    for tc in range(NTC):
                        sl = slice(tc * TW, (tc + 1) * TW)
                        psC, pkC = ps_rot()
                        S.mm(psC[:, :], ENf[0:16, ex, :], CT[0:16, sl], True, True, [K("ENf"), K("CT", tc)], [pkC])
                        cb, cbk = CB[n % 2], K("CB", n % 2)
                        S.cp(cb[:], psC[:, :], [pkC], [cbk], eng="act")
                        for fc in range(2):
                            ps1, pk1 = ps_rot()
                            proj_fm(wv, wk, fc * 128, tc, ps1, pk1)
                            ps3, pk3 = ps_rot()
                            proj_fm(wv, wk, 256 + fc * 128, tc, ps3, pk3)
                            s_, sk = SL[(2 * n + fc) % 2], K("SL", (2 * n + fc) % 2)
                            S.act(s_[:], ps1[:, :], AF.Silu, [pk1], [sk])
                            S.tt(s_[:], s_[:], ps3[:, :], ALU.mult, [sk, pk3], [sk])
                            S.tt(HID[:, el * 2 + fc, sl], s_[:], cb[:], ALU.mult, [sk, cbk], [K("HID", el * 2 + fc, tc)])
                        n += 1
                for oc in range(KC):
                    for tc in range(NTC):
                        ps, pk = ps_rot()
                        for j in range(8):
                            S.mm(ps[:, :], W2S[:, j, oc * 128:(oc + 1) * 128], HID[:, j, tc * TW:(tc + 1) * TW],
                                 j == 0, j == 7, [K("W2S", j // 2), K("HID", j, tc)], [pk])
                        S.tt(xT[:, oc, tc * TW:(tc + 1) * TW], xT[:, oc, tc * TW:(tc + 1) * TW], ps[:, :], ALU.add,
                             [K("xT", oc, tc), pk], [K("xT", oc, tc)])
        S.barrier()
        dump(f"x3_{l}", xT[:], ALLX, [128, KC, S_])
        if stop_after == f"M{l}":
            done = True
            break

    if stop_after is None:
        with ExitStack() as st:
            HF = sb("HFo", [128, KC, TW], stack=st)
            OB = [sb(f"OB{j}", [128, D_], stack=st) for j in range(2)]
            nob = [0]

            def final_hook(tc, r):
                sl = slice(tc * TW, (tc + 1) * TW)
                for kc in range(KC):
                    S.stt(HF[:, kc, :], xT[:, kc, sl], col(0, "fin_g", kc), r[:], ALU.mult, ALU.mult,
                          [K("xT", kc, tc), K("rsz", tc % 2), K("colp")], [K("HFo", kc)])
                for q in range(4):
                    ob, obk = OB[nob[0] % 2], K("OB", nob[0] % 2)
                    nob[0] += 1
                    for half in range(2):
                        ps, pk = ps_rot()
                        for j in range(4):
                            S.tr(ps[:, j * 128:(j + 1) * 128], HF[:, half * 4 + j, q * 128:(q + 1) * 128], ident,
                                 [K("HFo", half * 4 + j), K("c128")], [pk])
                        S.cp(ob[:, half * 512:(half + 1) * 512], ps[:, :], [pk], [obk],
                             eng=("act" if half else "dve"))
                    r0 = tc * TW + q * 128
                    i = S.dma(out_d[r0:r0 + 128, :], ob[:], [obk], [K("out", r0)])
                    S.final.append(i)

            rmsnorm_fm(xT, "xT", lambda kc: col(0, "fin_g", kc), None, None, KC, S_, st, hook=final_hook, tag="z")

    S.finalize(es)
    es.close()
    return nc, dump_d


def make_consts():
    c128 = np.zeros((128, 8, 128), np.float32)
    j = np.arange(128)[:, None]
    s = np.arange(128)[None, :]
    c128[:, 0] = np.eye(128)
    c128[:, 1] = 1.0
    c128[:, 2] = (j <= s)
    c128[:, 3] = -1.0 * (j >= s)
    c128[:, 4] = -1.0
    partner = np.where((np.arange(128) % 64) < 32, np.arange(128) + 32, np.arange(128) - 32)
    perm = np.zeros((128, 128), np.float32)
    perm[partner, np.arange(128)] = 1.0
    c128[:, 5] = perm
    c128[:, 6] = NEG * (s < j)
    cm = np.zeros((128, 12, 512), np.float32)
    p = np.arange(128)[:, None]
    f = np.arange(512)[None, :]
    for d in range(4):
        valid = (f - p) > d * 128
        cm[:, d] = valid
        cm[:, 4 + d] = NEG * (~valid)
        cm[:, 8 + d] = NEG * ((d * 128 + p) > f)
    half = 32
    inv = (np.float32(10000.0) ** (-(np.arange(half, dtype=np.float32)) / np.float32(half))).astype(np.float32)
    ang = np.arange(S_, dtype=np.float32)[None, :] * inv[:, None]
    cos = np.cos(ang).astype(np.float32)
    sin = np.sin(ang).astype(np.float32)
    rope = np.zeros((128, 2, S_), np.float32)
    for pp in range(128):
        d = pp % 64
        rope[pp, 0] = cos[d % 32]
        rope[pp, 1] = -sin[d % 32] if d < 32 else sin[d % 32]
    en = np.zeros((16, 16, 128), np.float32)
    for n in range(16):
        en[n, n, :] = 1.0
    gc = np.zeros((128, 3, 16, 8), np.float32)
    for i in range(16):
        own = i // 2
        for n in range(8):
            gc[:, 0, i, n] = 0.0 if n < own else -1e30
            gc[:, 1, i, n] = 1.0 if n < own else 0.0
            gc[:, 2, i, n] = 1.0 if n == own else 0.0
    return dict(c128=c128.reshape(128, -1), cmask=cm.reshape(128, -1), rope=rope.reshape(128, -1),
                en=en.reshape(16, -1), gatec=gc.reshape(128, -1))


def colvec(v):
    v = np.asarray(v, np.float32)
    return np.ascontiguousarray(v.reshape(-1, 128).T)


def pack_params(inp):
    colp = np.zeros((128, L_, NCOL), np.float32)
    rowp = np.zeros((128, L_, NROW), np.float32)

    def put(l, name, arr):
        c0, w = COLS[name]
        assert arr.shape == (128, w), (name, arr.shape)
        colp[:, l, c0:c0 + w] = arr

    for l in range(L_):
        put(l, "mix_g", colvec(inp["mix_norm_g"][l]))
        put(l, "xat_g", colvec(inp["xattn_norm_g"][l]))
        put(l, "mem_g", colvec(inp["mem_norm_g"][l]))
        put(l, "ffn_g", colvec(inp["ffn_norm_g"][l]))
        put(l, "grp_g", colvec(inp["group_norm_g"][l]))
        cw = inp["lru_conv_w"][l]
        put(l, "lru_cw", np.concatenate([colvec(cw[k]) for k in range(4)], axis=1).reshape(128, 4, 2)
            .transpose(0, 2, 1).reshape(128, 8))
        put(l, "lru_cb", colvec(inp["lru_conv_b"][l]))
        put(l, "lru_br", colvec(inp["lru_br"][l]))
        put(l, "lru_bi", colvec(inp["lru_bi"][l]))
        put(l, "lru_lam", colvec(inp["lru_lambda"][l]))
        sw = inp["ssm_conv_w"][l]
        put(l, "ssm_cw", np.concatenate([colvec(sw[k]) for k in range(4)], axis=1).reshape(128, 4, 6)
            .transpose(0, 2, 1).reshape(128, 24))
        put(l, "ssm_cb", colvec(inp["ssm_conv_b"][l]))
        put(l, "ssm_d", colvec(np.repeat(inp["ssm_d"][l], 64)))
        put(l, "fin_g", colvec(inp["final_norm_g"]))
        r0, w = ROWS["dt_bias"]
        rowp[:, l, r0:r0 + w] = np.tile(inp["ssm_dt_bias"][l], 16)[None, :]
        r0, w = ROWS["a_log"]
        rowp[:, l, r0:r0 + w] = np.tile(inp["ssm_a_log"][l], 16)[None, :]
        r0, w = ROWS["bg"]
        rowp[:, l, r0:r0 + w] = inp["router_group_b"][l][None, :]
        r0, w = ROWS["be"]
        rowp[:, l, r0:r0 + w] = inp["router_expert_b"][l][None, :]
    wbd = np.zeros((L_, 2, 2, 128, 128), np.float32)
    for l in range(L_):
        for gi, nm in enumerate(("lru_wr", "lru_wi")):
            for c in range(2):
                for hh in range(2):
                    wbd[l, gi, c, hh * 64:(hh + 1) * 64, hh * 64:(hh + 1) * 64] = inp[nm][l, 2 * c + hh]
    router_w = np.concatenate([inp["router_group_w"], inp["router_expert_w"]], axis=2)
    return dict(colp=colp.reshape(128, -1), rowp=rowp.reshape(128, -1), lru_wbd=wbd,
                router_w=np.ascontiguousarray(router_w))


SHARED = ["w_in", "w_out", "xattn_wq", "xattn_wkv", "xattn_wo", "expert_w1", "expert_w3", "expert_w2"]


def make_in_maps(inputs, cores):
    inp = {k: np.asarray(v) for k, v in inputs.items()}
    consts = make_consts()
    packed = pack_params(inp)
    shared = {k: np.ascontiguousarray(inp[k], dtype=np.float32) for k in SHARED}
    maps = []
    for b in cores:
        m = dict(shared)
        m.update(consts)
        m.update(packed)
        m["x"] = np.ascontiguousarray(inp["x"][b])
        m["mem"] = np.ascontiguousarray(inp["mem"][b])
        maps.append(m)
    return maps


_CACHE = {}


def kernel(**inputs):
    if "nc" not in _CACHE:
        _CACHE["nc"] = build_program()[0]
    nc = _CACHE["nc"]
    maps = make_in_maps(inputs, list(range(8)))
    res = run_bass_kernel_spmd(nc, maps, core_ids=list(range(8)))
    return np.stack([r["out"] for r in res.results], axis=0).astype(np.float32)
```

```python
import numpy as np
from contextlib import ExitStack
import concourse.bass as bass
import concourse.mybir as mybir
from concourse.bass_utils import run_bass_kernel_spmd

F32 = mybir.dt.float32
BF16 = mybir.dt.bfloat16
F32R = mybir.dt.float32r
AF = mybir.ActivationFunctionType
ALU = mybir.AluOpType
AX = mybir.AxisListType

S_ = 2048
D_ = 1024
L_ = 2
KC = 8
NTC = 4
TW = 512
NT = 16
NEG = -30000.0
EPS = 1e-6

COLS = {}
_c = 0
for _n, _w in [("mix_g", 8), ("xat_g", 8), ("mem_g", 8), ("ffn_g", 8), ("grp_g", 8),
               ("lru_cw", 8), ("lru_cb", 2), ("lru_br", 2), ("lru_bi", 2), ("lru_lam", 2),
               ("ssm_cw", 24), ("ssm_cb", 6), ("ssm_d", 2), ("fin_g", 8)]:
    COLS[_n] = (_c, _w)
    _c += _w
NCOL = _c
ROWS = {}
_c = 0
for _n, _w in [("dt_bias", 64), ("a_log", 64), ("bg", 4), ("be", 16)]:
    ROWS[_n] = (_c, _w)
    _c += _w
NROW = _c


class Sched:
    ENG = ("pe", "act", "dve", "pool", "sp")

    def __init__(self, nc):
        self.nc = nc
        self.ops = []
        self.lastw = {}
        self.rd = {}
        self.by_eng = {e: [] for e in self.ENG}
        self.extra = {e: set() for e in self.ENG}
        self.dma_since = []
        self.final = []
        self.psn = 0

    def add(self, eng, fn, reads=(), writes=(), dma=False):
        i = len(self.ops)
        reads = list(reads)
        writes = list(writes)
        deps = {}
        for r in reads:
            w = self.lastw.get(r)
            if w is not None:
                deps.setdefault(w, set()).add("raw")
        for w_ in writes:
            w = self.lastw.get(w_)
            if w is not None:
                deps.setdefault(w, set()).add("waw")
            rr = self.rd.get(w_)
            if rr:
                for r in rr[0].values():
                    deps.setdefault(r, set()).add("war")
                for r in rr[1]:
                    deps.setdefault(r, set()).add("war")
        ws = set(writes)
        for r in reads:
            if r not in ws:
                rr = self.rd.setdefault(r, ({}, []))
                if dma:
                    rr[1].append(i)
                else:
                    rr[0][eng] = i
        for w_ in writes:
            self.lastw[w_] = i
            self.rd[w_] = ({}, [])
        fdeps = set()
        for d, kinds in deps.items():
            od = self.ops[d]
            if od["eng"] == eng and not od["dma"] and not dma:
                if eng == "pe":
                    continue
                if "raw" not in kinds:
                    continue
            fdeps.add(d)
        fdeps |= self.extra[eng]
        self.extra[eng] = set()
        self.ops.append(dict(eng=eng, fn=fn, deps=fdeps, dma=dma, signal=dma, token=None))
        self.by_eng[eng].append(i)
        if dma and eng != "pool":
            self.dma_since.append(i)
        return i

    def barrier(self):
        toks = set()
        for e in self.ENG:
            for i in reversed(self.by_eng[e]):
                if not self.ops[i]["dma"]:
                    toks.add(i)
                    break
        toks |= set(self.dma_since)
        self.dma_since = []
        self.bar_toks = set(toks)
        for e in ("pe", "act", "dve", "sp"):
            self.extra[e] |= toks

    def mm(self, out, lhsT, rhs, start, stop, reads, writes):
        return self.add("pe", lambda e: e.matmul(out, lhsT, rhs, start=start, stop=stop), reads, writes)

    def tr(self, out, in_, ident, reads, writes):
        return self.add("pe", lambda e: e.transpose(out, in_, ident), reads, writes)

    def act(self, out, in_, func, reads, writes, bias=None, scale=None, accum=None):
        kw = {}
        if bias is not None:
            kw["bias"] = bias
        if scale is not None:
            kw["scale"] = scale
        if accum is not None:
            kw["accum_out"] = accum
        return self.add("act", lambda e: e.activation(out=out, in_=in_, func=func, **kw), reads, writes)

    def tt(self, out, in0, in1, op, reads, writes, eng="dve"):
        return self.add(eng, lambda e: e.tensor_tensor(out=out, in0=in0, in1=in1, op=op), reads, writes)

    def ts(self, out, in0, s1, s2, op0, op1, reads, writes, eng="dve"):
        if s2 is None:
            return self.add(eng, lambda e: e.tensor_scalar(out, in0, s1, None, op0), reads, writes)
        return self.add(eng, lambda e: e.tensor_scalar(out, in0, s1, s2, op0, op1), reads, writes)

    def stt(self, out, in0, scalar, in1, op0, op1, reads, writes, eng="dve"):
        return self.add(eng, lambda e: e.scalar_tensor_tensor(out=out, in0=in0, scalar=scalar, in1=in1,
                                                             op0=op0, op1=op1), reads, writes)

    def cp(self, out, in_, reads, writes, eng="dve"):
        if eng == "act":
            return self.add("act", lambda e: e.activation(out=out, in_=in_, func=AF.Copy), reads, writes)
        return self.add(eng, lambda e: e.tensor_copy(out=out, in_=in_), reads, writes)

    def dma(self, out, in_, reads, writes, q="sp", arena=False):
        i = self.add(q, lambda e: e.dma_start(out=out, in_=in_), reads, writes, dma=True)
        if arena:
            self.ops[i]["deps"] |= getattr(self, "bar_toks", set())
        return i

    def finalize(self, es):
        nc = self.nc
        for op in self.ops:
            for d in op["deps"]:
                self.ops[d]["signal"] = True
        for d in self.final:
            self.ops[d]["signal"] = True
        csem = {e: es.enter_context(nc.semaphore("c_" + e)) for e in ("pe", "act", "dve", "pool")}
        NP = 16
        qsem = {q: [es.enter_context(nc.semaphore(f"q_{q}{k}")) for k in range(NP)] for q in ("sp", "pool")}
        cnt = {e: 0 for e in csem}
        qn = {q: 0 for q in qsem}
        quse = {q: [0] * NP for q in qsem}
        qprev = {q: [None] * NP for q in qsem}
        for i, op in enumerate(self.ops):
            if op["dma"]:
                q = op["eng"]
                k = qn[q] % NP
                qn[q] += 1
                quse[q][k] += 1
                op["token"] = (qsem[q][k], 16 * quse[q][k])
                if qprev[q][k] is not None:
                    op["deps"].add(qprev[q][k])
                qprev[q][k] = i
            elif op["signal"]:
                e = op["eng"]
                cnt[e] += 1
                op["token"] = (csem[e], cnt[e])
        ops = self.ops
        final = list(self.final)

        def emit(engname, e):
            waited = {}
            for i in self.by_eng[engname]:
                op = ops[i]
                for d in sorted(op["deps"]):
                    sem, val = ops[d]["token"]
                    if waited.get(sem, 0) < val:
                        e.wait_ge(sem, val)
                        waited[sem] = val
                ins = op["fn"](e)
                if op["signal"]:
                    ins.then_inc(op["token"][0], 16 if op["dma"] else 1)
            if engname == "sp":
                for d in final:
                    sem, val = ops[d]["token"]
                    if waited.get(sem, 0) < val:
                        e.wait_ge(sem, val)
                        waited[sem] = val

        with nc.Block() as block:
            @block.tensor
            def _(e):
                emit("pe", e)

            @block.scalar
            def _(e):
                emit("act", e)

            @block.vector
            def _(e):
                emit("dve", e)

            @block.gpsimd
            def _(e):
                emit("pool", e)

            @block.sync
            def _(e):
                emit("sp", e)


def K(name, *idx):
    return (name,) + idx


def build_program(stop_after=None, dumps=(), n_layers=L_):
    nc = bass.Bass("TRN2", target_bir_lowering=False)
    S = Sched(nc)
    es = ExitStack()
    P = {}

    def din(name, shape):
        P[name] = nc.dram_tensor(name, list(shape), F32, kind="ExternalInput").ap()
        return P[name]

    x_d = din("x", [S_, D_])
    mem_d = din("mem", [256, D_])
    w_in_d = din("w_in", [L_, D_, 3076])
    w_out_d = din("w_out", [L_, D_, D_])
    wq_d = din("xattn_wq", [L_, D_, D_])
    wkv_d = din("xattn_wkv", [L_, D_, 2 * D_])
    wo_d = din("xattn_wo", [L_, D_, D_])
    w1_d = din("expert_w1", [L_, 16, D_, 256])
    w3_d = din("expert_w3", [L_, 16, D_, 256])
    w2_d = din("expert_w2", [L_, 16, 256, D_])
    wrt_d = din("router_w", [L_, D_, 20])
    lrubd_d = din("lru_wbd", [L_, 2, 2, 128, 128])
    colp_d = din("colp", [128, L_ * NCOL])
    rowp_d = din("rowp", [128, L_ * NROW])
    c128_d = din("c128", [128, 8 * 128])
    cmask_d = din("cmask", [128, 12 * 512])
    rope_d = din("rope", [128, 2 * S_])
    en_d = din("en", [16, 16 * 128])
    blkoh_d = din("blkoh", [8, S_])
    gate_c_d = din("gatec", [128, 3 * 128])
    out_d = nc.dram_tensor("out", [S_, D_], F32, kind="ExternalOutput").ap()
    dump_d = {}

    uid = [0]

    def sb(name, shape, dt=F32, stack=None):
        uid[0] += 1
        return (stack or es).enter_context(nc.sbuf_tensor(f"{name}_{uid[0]}", list(shape), dt))

    xT = sb("xT", [128, KC, S_])
    hT = sb("hT", [128, KC, S_], BF16)
    colp = sb("colp_s", [128, L_ * NCOL])
    rowp = sb("rowp_s", [128, L_ * NROW])
    c128 = sb("c128_s", [128, 8 * 128])
    c128b = sb("c128b_s", [128, 2 * 128], BF16)
    W = [sb(f"wslot{i}", [128, KC, 512], BF16) for i in range(3)]
    PS = [es.enter_context(nc.psum_tensor(f"ps{i}", [128, 512], F32)) for i in range(8)]
    wn = [0]

    ident = c128[:, 0:128]
    ones_f = c128[:, 128:256]
    tri_le = c128[:, 256:384]
    negu = c128[:, 384:512]
    negones = c128[:, 512:640]
    perm_f = c128[:, 640:768]
    ssdneg = c128[:, 768:896]
    ones_b = c128b[:, 128:256]
    ident_b = c128b[:, 0:128]

    cst = sb("cst", [128, 4])
    S.add("dve", lambda e: e.memset(cst[:, 0:1], EPS), [], [K("cst0")])
    S.add("dve", lambda e: e.memset(cst[:, 1:2], 1.0), [], [K("cst1")])
    S.add("dve", lambda e: e.memset(cst[:, 2:4], 0.0), [K("cst0"), K("cst1")], [K("cst")])
    S.dma(colp[:], colp_d[:, :], [], [K("colp")])
    S.dma(rowp[:], rowp_d[:, :], [], [K("rowp")])
    S.dma(c128[:], c128_d[:, :], [], [K("c128")])
    S.dma(c128b[:], c128_d[:, 0:256], [], [K("c128b")], q="pool")

    def col(l, name, j=0, n=1):
        c0, w = COLS[name]
        return colp[:, l * NCOL + c0 + j: l * NCOL + c0 + j + n]

    def row(l, name, j=0, n=None):
        c0, w = ROWS[name]
        n = w if n is None else n
        return rowp[:, l * NROW + c0 + j: l * NROW + c0 + j + n]

    def ps_rot():
        k = S.psn % 4
        S.psn += 1
        return PS[k], K("ps", k)

    Wf = [w[:].rearrange("p a b -> p (a b)") for w in W]

    def load_w(src2d, nk, ncols, slot=None, c0=0, tot=None):
        tot = ncols if tot is None else tot
        if slot is None:
            k = wn[0] % 3
            wn[0] += 1
        else:
            k = slot
        v = Wf[k][:, 0:nk * tot].rearrange("p (a b) -> p a b", a=nk)
        step = 2 if nk >= 2 else 1
        for h in range(0, nk, step):
            S.dma(v[:, h:h + step, c0:c0 + ncols],
                  src2d[h * 128:(h + step) * 128, :].rearrange("(kc p) n -> p kc n", p=128),
                  [], [K("w", k)], q="pool")
        return v, [K("w", k)], k

    def dump(name, ap, reads, shape):
        if name in dumps:
            d = nc.dram_tensor("dbg_" + name, list(shape), ap.dtype, kind="ExternalOutput").ap()
            dump_d[name] = d
            i = S.dma(d, ap, reads, [K("dbg", name)])
            S.final.append(i)

    def load_fm(dst, src_d, ntile, dkey, st):
        xin = [sb(f"xin{j}", [128, D_], stack=st) for j in range(2)]
        for i in range(ntile):
            b = xin[i % 2]
            S.dma(b[:], src_d[i * 128:(i + 1) * 128, :], [], [K("xin", i % 2)])
            for half in range(2):
                ps, pk = ps_rot()
                for j in range(4):
                    kc = half * 4 + j
                    S.tr(ps[:, j * 128:(j + 1) * 128], b[:, kc * 128:(kc + 1) * 128], ident,
                         [K("xin", i % 2), K("c128")], [pk])
                S.cp(dst[:, half * 4:half * 4 + 4, i * 128:(i + 1) * 128],
                     ps[:].rearrange("p (a b) -> p a b", a=4), [pk],
                     [K(dkey, half * 4 + j, i // 4) for j in range(4)], eng=("dve" if half == 0 else "act"))

    with ExitStack() as st:
        load_fm(xT, x_d, NT, "xT", st)
    S.barrier()
    dump("xT0", xT[:], [K("xT", kc, tc) for kc in range(KC) for tc in range(NTC)], [128, KC, S_])

    def rmsnorm_fm(src, skey, gcol, dst, dkey, nk, T, st, dscale=1.0, hook=None, tag="n"):
        tw = min(T, TW)
        sq = [sb(f"sq_{tag}{j}", [128, nk, tw], BF16, stack=st) for j in range(2)]
        rs = [sb(f"rs_{tag}{j}", [128, tw], stack=st) for j in range(2)]
        for tc in range(T // tw):
            sl = slice(tc * tw, (tc + 1) * tw)
            q = sq[tc % 2]
            for kc in range(nk):
                S.act(q[:, kc, :], src[:, kc, sl], AF.Square, [K(skey, kc, tc)], [K("sq" + tag, tc % 2, kc)])
            ps, pk = ps_rot()
            for kc in range(nk):
                S.mm(ps[:, 0:tw], ones_b, q[:, kc, :], kc == 0, kc == nk - 1,
                     [K("sq" + tag, tc % 2, kc), K("c128b")], [pk])
            r = rs[tc % 2]
            S.act(r[:], ps[:, 0:tw], AF.Sqrt, [pk, K("cst")], [K("rs" + tag, tc % 2)],
                  bias=cst[:, 0:1], scale=1.0 / (nk * 128))
            S.add("dve", (lambda e, r=r: e.reciprocal(out=r[:], in_=r[:])),
                  [K("rs" + tag, tc % 2)], [K("rs" + tag, tc % 2)])
            if dst is not None:
                for kc in range(nk):
                    S.stt(dst[:, kc, sl], src[:, kc, sl], gcol(kc), r[:], ALU.mult, ALU.mult,
                          [K(skey, kc, tc), K("rs" + tag, tc % 2), K("colp")], [K(dkey, kc, tc)],
                          eng="dve")
            if hook is not None:
                hook(tc, r)

    ALLH = [K("hT", kc, tc) for kc in range(KC) for tc in range(NTC)]

    def hkeys(tc):
        return [K("hT", kc, tc) for kc in range(KC)]

    def proj_fm(wv, wkeys, c0, tc, ps, pk):
        for kc in range(KC):
            S.mm(ps[:, :], wv[:, kc, c0:c0 + 128], hT[:, kc, tc * TW:(tc + 1) * TW], kc == 0, kc == KC - 1,
                 wkeys + [K("hT", kc, tc)], [pk])

    def conv_chunk(ps, pk, tc, xw, xwk, cwcol, cbcol, dst, dkey):
        w_ = xw[tc % 2]
        wk = K(xwk, tc % 2)
        if tc == 0:
            S.add("dve", lambda e: e.memset(w_[:, 0:3], 0.0), [], [wk])
        else:
            S.cp(w_[:, 0:3], xw[(tc - 1) % 2][:, 512:515], [K(xwk, (tc - 1) % 2)], [wk])
        S.cp(w_[:, 3:515], ps[:, :], [pk], [wk], eng="act")
        S.ts(dst, w_[:, 3:515], cwcol(3), cbcol, ALU.mult, ALU.add, [wk, K("colp")], [dkey])
        for k in range(3):
            S.stt(dst, w_[:, k:k + 512], cwcol(k), dst, ALU.mult, ALU.add, [wk, dkey, K("colp")], [dkey])

    def group_out(l, g, YG, st):
        YB = sb("YB", [128, 2, S_], BF16, stack=st)
        rmsnorm_fm(YG, "YG", lambda kc: col(l, "grp_g", 2 * g + kc), YB, "YB", 2, S_, st, tag="g")
        dump(f"yn{l}_{g}", YB[:], [K("YB", c, t) for c in range(2) for t in range(NTC)], [128, 2, S_])
        for half in range(2):
            wv, wk, _ = load_w(w_out_d[l, g * 256:(g + 1) * 256, half * 512:(half + 1) * 512], 2, 512)
            for o4 in range(4):
                oc = half * 4 + o4
                for tc in range(NTC):
                    ps, pk = ps_rot()
                    for kc in range(2):
                        S.mm(ps[:, :], wv[:, kc, o4 * 128:(o4 + 1) * 128], YB[:, kc, tc * TW:(tc + 1) * TW],
                             kc == 0, kc == 1, wk + [K("YB", kc, tc)], [pk])
                    S.tt(xT[:, oc, tc * TW:(tc + 1) * TW], xT[:, oc, tc * TW:(tc + 1) * TW], ps[:, :], ALU.add,
                         [K("xT", oc, tc), pk], [K("xT", oc, tc)])

    ALLX = [K("xT", kc, tc) for kc in range(KC) for tc in range(NTC)]
    done = False
    for l in range(n_layers):
        with ExitStack() as st:
            rmsnorm_fm(xT, "xT", lambda kc: col(l, "mix_g", kc), hT, "hT", KC, S_, st, tag="a")
        S.barrier()
        if l == 0:
            dump("h0", hT[:], ALLH, [128, KC, S_])
        if stop_after == "norm0":
            break

        with ExitStack() as sty, ExitStack() as st:
            YG = sb("YG", [128, 2, S_], stack=sty)
            wv, wk, _ = load_w(w_in_d[l, :, 0:512], 8, 512)
            wbd = sb("wbd", [128, 4, 128], BF16, stack=st)
            S.dma(wbd[:], lrubd_d[l].rearrange("g c k m -> k (g c) m"), [], [K("wbd")], q="pool", arena=True)
            c8 = sb("c8", [128, 2], stack=st)
            dump(f"A_wbd{l}", wbd[:], [K("wbd")], [128, 4, 128])
            cy = sb("cy", [128, 2], stack=st)
            cz = sb("cz", [128, 2], stack=st)
            S.act(cy[:], col(l, "lru_lam", 0, 2), AF.Exp, [K("colp")], [K("cy")], scale=-1.0)
            S.ts(cz[:], cy[:], 2.0, None, ALU.add, None, [K("cy")], [K("cz")])
            S.add("dve", lambda e: e.reciprocal(out=cz[:], in_=cz[:]), [K("cz")], [K("cz")])
            S.tt(cz[:], cz[:], cy[:], ALU.mult, [K("cz"), K("cy")], [K("cz")])
            S.tt(cy[:], cz[:], cz[:], ALU.mult, [K("cz")], [K("cy")])
            S.ts(c8[:], cy[:], 1.0 / 9.0, 1.0 / 7.0, ALU.mult, ALU.add, [K("cy")], [K("c8")])
            for cc_ in (1.0 / 5.0, 1.0 / 3.0, 1.0):
                S.tt(c8[:], c8[:], cy[:], ALU.mult, [K("c8"), K("cy")], [K("c8")])
                S.ts(c8[:], c8[:], cc_, None, ALU.add, None, [K("c8")], [K("c8")])
            S.tt(c8[:], c8[:], cz[:], ALU.mult, [K("c8"), K("cz")], [K("c8")])
            S.ts(c8[:], c8[:], -16.0, None, ALU.mult, None, [K("c8")], [K("c8")])
            xw = [sb(f"xw{j}", [128, 515], stack=st) for j in range(2)]
            XC = sb("XC", [128, S_], stack=st)
            XCB = sb("XCB", [128, S_], BF16, stack=st)
            GG = sb("GG", [128, S_], stack=st)
            RA = sb("RA", [128, S_], stack=st)
            IU = sb("IU", [128, S_], stack=st)
            T2 = sb("T2", [128, S_], stack=st)
            XX = sb("XX", [128, S_], stack=st)
            for c in range(2):
                for tc in range(NTC):
                    sl = slice(tc * TW, (tc + 1) * TW)
                    ps, pk = ps_rot()
                    proj_fm(wv, wk, c * 128, tc, ps, pk)
                    conv_chunk(ps, pk, tc, xw, "xw", lambda k: col(l, "lru_cw", c * 4 + k), col(l, "lru_cb", c),
                               XC[:, sl], K("XC", tc))
                    ps, pk = ps_rot()
                    proj_fm(wv, wk, 256 + c * 128, tc, ps, pk)
                    S.cp(GG[:, sl], ps[:, :], [pk], [K("GG", tc)], eng="act")
                XCk = [K("XC", t) for t in range(NTC)]
                GGk = [K("GG", t) for t in range(NTC)]
                S.tt(T2[:], GG[:], GG[:], ALU.mult, GGk, [K("T2")])
                S.ts(T2[:], T2[:], 0.044715, 1.0, ALU.mult, ALU.add, [K("T2")], [K("T2")])
                S.tt(T2[:], T2[:], GG[:], ALU.mult, [K("T2")] + GGk, [K("T2")])
                S.act(T2[:], T2[:], AF.Sigmoid, [K("T2")], [K("T2")], scale=1.5957691216)
                S.tt(GG[:], GG[:], T2[:], ALU.mult, [K("T2")] + GGk, GGk)
                S.cp(XCB[:], XC[:], XCk, [K("XCB")], eng="act")
                for tc in range(NTC):
                    sl = slice(tc * TW, (tc + 1) * TW)
                    ps, pk = ps_rot()
                    S.mm(ps[:, :], wbd[:, c, :], XCB[:, sl], True, True, [K("wbd"), K("XCB")], [pk])
                    S.act(RA[:, sl], ps[:, :], AF.Sigmoid, [pk, K("colp")], [K("RA", tc)], bias=col(l, "lru_br", c))
                    ps, pk = ps_rot()
                    S.mm(ps[:, :], wbd[:, 2 + c, :], XCB[:, sl], True, True, [K("wbd"), K("XCB")], [pk])
                    S.act(IU[:, sl], ps[:, :], AF.Sigmoid, [pk, K("colp")], [K("IU", tc)], bias=col(l, "lru_bi", c))
                RAk = [K("RA", t) for t in range(NTC)]
                IUk = [K("IU", t) for t in range(NTC)]
                if c == 1:
                    dump(f"A_r{l}", RA[:], RAk, [128, S_])
                    dump(f"A_i{l}", IU[:], IUk, [128, S_])
                    dump(f"A_xcb{l}", XCB[:], [K("XCB")], [128, S_])
                    dump(f"A_xc{l}", XC[:], XCk, [128, S_])
                S.ts(XX[:], RA[:], c8[:, c:c + 1], 2.0, ALU.mult, ALU.mult, RAk + [K("c8")], [K("XX")])
                S.ts(T2[:], XX[:], 1.0 / 5040.0, 1.0 / 720.0, ALU.mult, ALU.add, [K("XX")], [K("T2")])
                for cc_ in (1.0 / 120.0, 1.0 / 24.0, 1.0 / 6.0, 0.5, 1.0):
                    S.tt(T2[:], T2[:], XX[:], ALU.mult, [K("T2"), K("XX")], [K("T2")])
                    S.ts(T2[:], T2[:], cc_, None, ALU.add, None, [K("T2")], [K("T2")])
                S.stt(T2[:], T2[:], -1.0, XX[:], ALU.mult, ALU.mult, [K("T2"), K("XX")], [K("T2")])
                S.act(T2[:], T2[:], AF.Sqrt, [K("T2")], [K("T2")])
                S.act(RA[:], XX[:], AF.Exp, [K("XX")], RAk, scale=0.5)
                S.tt(IU[:], IU[:], XC[:], ALU.mult, IUk + XCk, IUk)
                S.tt(IU[:], IU[:], T2[:], ALU.mult, IUk + [K("T2")], IUk)
                S.add("dve", lambda e: e.tensor_tensor_scan(out=XC[:], data0=RA[:], data1=IU[:], initial=0.0,
                                                            op0=ALU.mult, op1=ALU.add), RAk + IUk, XCk)
                S.tt(YG[:, c, :], XC[:], GG[:], ALU.mult, XCk + GGk, [K("YG", c, t) for t in range(NTC)])
            for nm_, t_ in (("XC", XC), ("GG", GG), ("RA", RA), ("IU", IU), ("T2", T2)):
                dump(f"A_{nm_}{l}", t_[:], [K(nm_, t) for t in range(NTC)] + [K(nm_)], [128, S_])
            dump(f"ya{l}", YG[:], [K("YG", c, t) for c in range(2) for t in range(NTC)], [128, 2, S_])
            st.close()
            S.barrier()
            group_out(l, 0, YG, sty)
        S.barrier()
        if stop_after == f"A{l}":
            done = True
            break

        with ExitStack() as sty, ExitStack() as st:
            YG = sb("YG", [128, 2, S_], stack=sty)
            QB = sb("QB", [128, 2, S_], BF16, stack=st)
            KZ = sb("KZ", [128, 4, S_], BF16, stack=st)
            S.add("dve", lambda e: e.memset(KZ[:], 0.0), [], [K("KZ", h_, t_) for h_ in range(4) for t_ in range(NTC)])
            VB = sb("VB", [128, NT, 256], BF16, stack=st)
            M01 = sb("M01", [128, 4, 512], stack=st)
            NM = sb("NM", [128, 4, 512], BF16, stack=st)
            S.dma(M01[:], cmask_d[:, 0:2048].rearrange("p (a b) -> p a b", a=4), [], [K("M01")])
            S.dma(NM[:], cmask_d[:, 2048:4096].rearrange("p (a b) -> p a b", a=4), [], [K("NM")], q="pool", arena=True)
            wv, wk, _ = load_w(w_in_d[l, :, 512:1024], 8, 512)
            wv2, wk2, _ = load_w(w_in_d[l, :, 1024:1280], 8, 256)
            for c2 in range(2):
                for tc in range(NTC):
                    sl = slice(tc * TW, (tc + 1) * TW)
                    ps, pk = ps_rot()
                    proj_fm(wv, wk, c2 * 128, tc, ps, pk)
                    S.act(QB[:, c2, sl], ps[:, :], AF.Copy, [pk], [K("QB", c2, tc)], scale=0.125)
                    ps, pk = ps_rot()
                    proj_fm(wv, wk, 256 + c2 * 128, tc, ps, pk)
                    S.cp(KZ[0:64, 2 * c2, sl], ps[0:64, :], [pk], [K("KZ", 2 * c2, tc)])
                    S.cp(KZ[64:128, 2 * c2 + 1, sl], ps[64:128, :], [pk], [K("KZ", 2 * c2 + 1, tc)], eng="act")
            for i in range(NT):
                ps, pk = ps_rot()
                for kc in range(KC):
                    S.mm(ps[:, 0:256], hT[:, kc, i * 128:(i + 1) * 128], wv2[:, kc, :], kc == 0, kc == KC - 1,
                         wk2 + [K("hT", kc, i // 4)], [pk])
                S.cp(VB[:, i, :], ps[:, 0:256], [pk], [K("VB", i)], eng=("dve" if i % 2 else "act"))
            Eb = [sb(f"Eb{j}", [128, 512], stack=st) for j in range(2)]
            SPb = [sb(f"SPb{j}", [128, 512], F32R, stack=st) for j in range(3)]
            Wb = [sb(f"Wb{j}", [128, 512], BF16, stack=st) for j in range(2)]
            Rb = [sb(f"Rb{j}", [128, 512], F32R, stack=st) for j in range(2)]
            NUr = sb("NUr", [128, 2, 128], F32R, stack=st)
            S.cp(NUr[:, 0, :], negu, [K("c128")], [K("NUr")])
            S.cp(NUr[:, 1, :], negones, [K("c128")], [K("NUr")])
            items = []
            grp = 0
            for h in range(4):
                for c in range(NTC):
                    nb = 4 * c + 4
                    for idx, b in enumerate(range(nb - 1, -1, -1)):
                        items.append(dict(h=h, c=c, idx=idx, b=b, grp=grp, last=(b == 0)))
                    grp += 1
            for i_, itm in enumerate(items):
                itm["i"] = i_
                itm["pr"] = slice((itm["h"] % 2) * 64, (itm["h"] % 2) * 64 + 64)
                itm["c2"] = itm["h"] // 2
                itm["d"] = itm["b"] - 4 * itm["c"]
                itm["ksl"] = slice(itm["b"] * 128, (itm["b"] + 1) * 128)
                itm["qsl"] = slice(itm["c"] * TW, (itm["c"] + 1) * TW)
                itm["qk"] = [K("KZ", itm["h"], itm["b"] // 4), K("QB", itm["c2"], itm["c"])]

            def sb_s1(m):
                i = m["i"]
                pr, c2 = m["pr"], m["c2"]
                psZ, pkZ = ps_rot()
                S.mm(psZ[:, :], KZ[:, m["h"], m["ksl"]], QB[:, c2, m["qsl"]], True, True, m["qk"], [pkZ])
                E, Ek = Eb[i % 2], K("Eb", i % 2)
                SP, SPk = SPb[i % 3], K("SPb", i % 3)
                S.act(E[:], psZ[:, :], AF.Exp, [pkZ], [Ek])
                S.act(SP[:], E[:], AF.Ln, [Ek, K("cst")], [SPk], bias=cst[:, 1:2])
                if m["d"] >= 0:
                    S.tt(SP[:], SP[:].bitcast(F32), M01[:, m["d"], :], ALU.mult, [SPk, K("M01")], [SPk])

            def sb_s2(m):
                i = m["i"]
                pr, c2, d = m["pr"], m["c2"], m["d"]
                SP, SPk = SPb[i % 3], K("SPb", i % 3)
                Wt, Wk_ = Wb[i % 2], K("Wb", i % 2)
                R, Rk = Rb[m["grp"] % 2], K("Rb", m["grp"] % 2)
                psA, pkA = ps_rot()
                has_r = m["idx"] > 0
                S.mm(psA[:, :], KZ[:, m["h"], m["ksl"]], QB[:, c2, m["qsl"]], True, False, m["qk"], [pkA])
                S.mm(psA[:, :], NUr[:, 0, :], SP[:], False, (not has_r) and d < 0, [SPk, K("NUr")], [pkA])
                if has_r:
                    S.mm(psA[:, :], NUr[:, 1, :], R[:], False, d < 0, [Rk, K("NUr")], [pkA])
                if d >= 0:
                    S.mm(psA[:, :], ident_b, NM[:, d, :], False, True, [K("NM"), K("c128b")], [pkA])
                S.act(Wt[:], psA[:, :], AF.Exp, [pkA], [Wk_])
                if not m["last"]:
                    if m["idx"] == 0:
                        S.cp(R[:], SP[:].bitcast(F32), [SPk], [Rk])
                    else:
                        S.tt(R[:], R[:].bitcast(F32), SP[:].bitcast(F32), ALU.add, [Rk, SPk], [Rk])

            def sb_s3(m):
                i = m["i"]
                pr, c2, h = m["pr"], m["c2"], m["h"]
                Wt, Wk_ = Wb[i % 2], K("Wb", i % 2)
                psO, pkO = PS[4 + m["grp"] % 2], K("psO", m["grp"] % 2)
                S.mm(psO[:, :], VB[:, m["b"], c2 * 128:(c2 + 1) * 128], Wt[:], m["idx"] == 0, m["last"],
                     [K("VB", m["b"]), Wk_], [pkO])
                if m["last"]:
                    S.cp(YG[pr, c2, m["qsl"]], psO[pr, :], [pkO], [K("YG", c2, m["c"])], eng="act")

            n_it = len(items)
            for r in range(-2, n_it):
                if 0 <= r + 2 < n_it:
                    sb_s1(items[r + 2])
                if 0 <= r + 1 < n_it:
                    sb_s2(items[r + 1])
                if 0 <= r < n_it:
                    sb_s3(items[r])
            dump(f"yb{l}", YG[:], [K("YG", c, t) for c in range(2) for t in range(NTC)], [128, 2, S_])
            st.close()
            S.barrier()
            group_out(l, 1, YG, sty)
        S.barrier()
        if stop_after == f"B{l}":
            done = True
            break

        with ExitStack() as sty, ExitStack() as st:
            YG = sb("YG", [128, 2, S_], stack=sty)
            XS = sb("XS", [128, 2, S_], stack=st)
            BTb = sb("BTb", [128, 2, S_], BF16, stack=st)
            CTb = sb("CTb", [128, 2, S_], BF16, stack=st)
            BTM = sb("BTM", [128, NT, 256], BF16, stack=st)
            XTM = sb("XTM", [128, NT, 256], BF16, stack=st)
            wdt = sb("wdt", [128, KC, 4], BF16, stack=st)
            stc = ExitStack()
            xw = [sb(f"xwc{j}", [128, 515], stack=stc) for j in range(2)]
            CV = [sb(f"CV{j}", [128, 512], stack=stc) for j in range(2)]
            BF = [sb(f"BF{j}", [128, 512], stack=stc) for j in range(2)]
            S.dma(wdt[:], w_in_d[l, :, 2304:2308].rearrange("(kc p) n -> p kc n", p=128), [], [K("wdt")], q="pool", arena=True)
            wv1, wk1, _ = load_w(w_in_d[l, :, 1280:1792], 8, 512)
            wv2, wk2, _ = load_w(w_in_d[l, :, 1792:2304], 8, 512)
            n_cv = 0
            for j in range(6):
                if j < 2:
                    wv, wk, c0 = wv1, wk1, 256 + j * 128
                elif j < 4:
                    wv, wk, c0 = wv2, wk2, (j - 2) * 128
                else:
                    wv, wk, c0 = wv2, wk2, 256 + (j - 4) * 128
                for tc in range(NTC):
                    sl = slice(tc * TW, (tc + 1) * TW)
                    ps, pk = ps_rot()
                    proj_fm(wv, wk, c0, tc, ps, pk)
                    cv, cvk = CV[n_cv % 2], K("CV", n_cv % 2)
                    conv_chunk(ps, pk, tc, xw, "xwc", lambda k: col(l, "ssm_cw", j * 4 + k), col(l, "ssm_cb", j),
                               cv[:], cvk)
                    if j < 2:
                        S.act(XS[:, j, sl], cv[:], AF.Silu, [cvk], [K("XS", j, tc)])
                        ps, pk = ps_rot()
                        for q in range(4):
                            S.tr(ps[:, q * 128:(q + 1) * 128], XS[:, j, tc * TW + q * 128: tc * TW + (q + 1) * 128],
                                 ident, [K("XS", j, tc), K("c128")], [pk])
                        S.cp(XTM[:, tc * 4:tc * 4 + 4, j * 128:(j + 1) * 128],
                             ps[:].rearrange("p (a b) -> p a b", a=4), [pk], [K("XTM", tc, j)])
                    elif j < 4:
                        g = j - 2
                        bf, bfk = BF[n_cv % 2], K("BF", n_cv % 2)
                        S.act(bf[:], cv[:], AF.Silu, [cvk], [bfk])
                        S.cp(BTb[:, g, sl], bf[:], [bfk], [K("BTb", g, tc)])
                        ps, pk = ps_rot()
                        for q in range(4):
                            S.tr(ps[:, q * 128:(q + 1) * 128], bf[:, q * 128:(q + 1) * 128], ident,
                                 [bfk, K("c128")], [pk])
                        S.cp(BTM[:, tc * 4:tc * 4 + 4, g * 128:(g + 1) * 128],
                             ps[:].rearrange("p (a b) -> p a b", a=4), [pk], [K("BTM", tc, g)])
                    else:
                        S.act(CTb[:, j - 4, sl], cv[:], AF.Silu, [cvk], [K("CTb", j - 4, tc)])
                    n_cv += 1
            stc.close()
            S.barrier()
            DT = sb("DT", [128, 64], stack=st)
            AN = sb("AN", [128, 64], stack=st)
            AC = sb("AC", [128, 64], stack=st)
            ACS = sb("ACS", [128, 64], stack=st)
            TOT = sb("TOT", [128, 64], stack=st)
            DEC = sb("DEC", [128, 64], stack=st)
            ETOT = sb("ETOT", [128, 64], stack=st)
            DTD = sb("DTD", [128, 64], stack=st)
            psd, pkd = ps_rot()
            for i in range(NT):
                for kc in range(KC):
                    S.mm(psd[:, i * 4:(i + 1) * 4], hT[:, kc, i * 128:(i + 1) * 128], wdt[:, kc, :], kc == 0,
                         kc == KC - 1, [K("wdt"), K("hT", kc, i // 4)], [pkd])
            S.tt(DT[:], psd[:, 0:64], row(l, "dt_bias"), ALU.add, [pkd, K("rowp")], [K("DT")])
            S.act(DT[:], DT[:], AF.Exp, [K("DT")], [K("DT")])
            S.act(DT[:], DT[:], AF.Ln, [K("DT"), K("cst")], [K("DT")], bias=cst[:, 1:2])
            S.act(AN[:], row(l, "a_log"), AF.Exp, [K("rowp")], [K("AN")])
            S.ts(AN[:], AN[:], -1.0, None, ALU.mult, None, [K("AN")], [K("AN")])
            S.tt(AC[:], DT[:], AN[:], ALU.mult, [K("DT"), K("AN")], [K("AC")])
            ps, pk = ps_rot()
            S.mm(ps[:, 0:64], tri_le, AC[:], True, True, [K("AC"), K("c128")], [pk])
            S.cp(ACS[:], ps[:, 0:64], [pk], [K("ACS")])
            ps, pk = ps_rot()
            S.mm(ps[:, 0:64], ones_f, AC[:], True, True, [K("AC"), K("c128")], [pk])
            S.cp(TOT[:], ps[:, 0:64], [pk], [K("TOT")])
            S.tt(DEC[:], TOT[:], ACS[:], ALU.subtract, [K("TOT"), K("ACS")], [K("DEC")])
            S.act(DEC[:], DEC[:], AF.Exp, [K("DEC")], [K("DEC")])
            S.act(ETOT[:], TOT[:], AF.Exp, [K("TOT")], [K("ETOT")])
            S.tt(DTD[:], DT[:], DEC[:], ALU.mult, [K("DT"), K("DEC")], [K("DTD")])
            ST = sb("ST", [128, 4, 64], stack=st)
            S.add("dve", lambda e: e.memset(ST[:], 0.0), [], [K("ST", h) for h in range(4)])
            Rm = [sb(f"Rm{j}", [128, 128], stack=st) for j in range(2)]
            ARG = [sb(f"ARG{j}", [128, 128], stack=st) for j in range(2)]
            E1 = [sb(f"E1{j}", [128, 128], stack=st) for j in range(2)]
            GL = [sb(f"GL{j}", [128, 128], BF16, stack=st) for j in range(2)]
            CTp = [sb(f"CTp{j}", [128, 128], BF16, stack=st) for j in range(2)]
            XCp = [[sb(f"XCp{a}{j}", [128, 128], BF16, stack=st) for j in range(2)] for a in range(2)]
            STp = sb("STp", [128, 4, 128], BF16, stack=st)
            for a in range(2):
                for j in range(2):
                    S.add("dve", (lambda e, t_=XCp[a][j]: e.memset(t_[:], 0.0)), [], [K("XCp", a, j)])
            S.add("dve", lambda e: e.memset(STp[:], 0.0), [], [K("STp", h) for h in range(4)])
            XDh = [sb(f"XDh{j}", [128, 64], BF16, stack=st) for j in range(2)]
            n = 0
            for ci in range(NT):
                csl = slice(ci * 128, (ci + 1) * 128)
                tcx = ci // 4
                for g in range(2):
                    psG, pkG = ps_rot()
                    S.mm(psG[:, 0:128], BTb[:, g, csl], CTb[:, g, csl], True, True,
                         [K("BTb", g, tcx), K("CTb", g, tcx)], [pkG])
                    psY, pkY = PS[4 + (ci * 2 + g) % 2], K("psO", (ci * 2 + g) % 2)
                    for hh in range(2):
                        h = 2 * g + hh
                        pr = slice(hh * 64, hh * 64 + 64)
                        cidx = ci * 4 + h
                        k2 = n % 2
                        S.ts(Rm[k2][:], tri_le, AC[:, cidx:cidx + 1], None, ALU.mult, None,
                             [K("AC"), K("c128")], [K("Rm", k2)])
                        ps1, pk1 = ps_rot()
                        S.mm(ps1[:, 0:128], ones_f, Rm[k2][:], True, True, [K("Rm", k2), K("c128")], [pk1])
                        S.stt(ARG[k2][:], ps1[:, 0:128], ACS[:, cidx:cidx + 1], ssdneg, ALU.subtract, ALU.add,
                              [pk1, K("ACS"), K("c128")], [K("ARG", k2)])
                        S.act(ARG[k2][:], ARG[k2][:], AF.Exp, [K("ARG", k2)], [K("ARG", k2)])
                        S.tt(GL[k2][:], ARG[k2][:], psG[:, 0:128], ALU.mult, [K("ARG", k2), pkG], [K("GL", k2)])
                        S.act(E1[k2][:], ps1[:, 0:128], AF.Exp, [pk1], [K("E1", k2)])
                        S.tt(CTp[k2][:], CTb[:, g, csl], E1[k2][:], ALU.mult, [K("CTb", g, tcx), K("E1", k2)],
                             [K("CTp", k2)])
                        rot = (ci * 2 + g) % 2
                        xcp = XCp[hh][rot]
                        S.ts(xcp[:, hh * 64:(hh + 1) * 64], XTM[:, ci, h * 64:(h + 1) * 64], DT[:, cidx:cidx + 1], None,
                             ALU.mult, None, [K("XTM", tcx, g), K("DT")], [K("XCp", hh, rot)])
                        S.mm(psY[:, 0:128], xcp[:], GL[k2][:], hh == 0, False, [K("XCp", hh, rot), K("GL", k2)], [pkY])
                        S.mm(psY[:, 0:128], STp[:, h, :], CTp[k2][:], False, hh == 1, [K("STp", h), K("CTp", k2)], [pkY])
                        S.ts(XDh[k2][:], XTM[:, ci, h * 64:(h + 1) * 64], DTD[:, cidx:cidx + 1], None, ALU.mult, None,
                             [K("XTM", tcx, g), K("DTD")], [K("XDh", k2)])
                        psS, pkS = ps_rot()
                        S.mm(psS[:, 0:64], BTM[:, ci, g * 128:(g + 1) * 128], XDh[k2][:], True, True,
                             [K("BTM", tcx, g), K("XDh", k2)], [pkS])
                        S.stt(ST[:, h, :], ST[:, h, :], ETOT[:, cidx:cidx + 1], psS[:, 0:64], ALU.mult, ALU.add,
                              [K("ST", h), K("ETOT"), pkS], [K("ST", h)])
                        S.cp(STp[:, h, hh * 64:(hh + 1) * 64], ST[:, h, :], [K("ST", h)], [K("STp", h)], eng="act")
                        n += 1
                    S.cp(YG[:, g, csl], psY[:, 0:128], [pkY], [K("YG", g, tcx)], eng="act")
            ARG = [sb(f"ZT{j}", [128, 512], stack=st) for j in range(2)]
            for j in range(2):
                YGk = [K("YG", j, t) for t in range(NTC)]
                S.stt(YG[:, j, :], XS[:, j, :], col(l, "ssm_d", j), YG[:, j, :], ALU.mult, ALU.add,
                      [K("XS", j, t) for t in range(NTC)] + YGk + [K("colp")], YGk)
                for tc in range(NTC):
                    sl = slice(tc * TW, (tc + 1) * TW)
                    ps, pk = ps_rot()
                    proj_fm(wv1, wk1, j * 128, tc, ps, pk)
                    zt, ztk = ARG[tc % 2], K("ARGz", tc % 2)
                    S.act(zt[:], ps[:, :], AF.Silu, [pk], [ztk])
                    S.tt(YG[:, j, sl], YG[:, j, sl], zt[:], ALU.mult, [K("YG", j, tc), ztk], [K("YG", j, tc)])
            dump(f"yc{l}", YG[:], [K("YG", c, t) for c in range(2) for t in range(NTC)], [128, 2, S_])
            st.close()
            S.barrier()
            group_out(l, 2, YG, sty)
        S.barrier()
        if stop_after == f"C{l}":
            done = True
            break

        with ExitStack() as sty, ExitStack() as st:
            YG = sb("YG", [128, 2, S_], stack=sty)
            QA = sb("QA", [128, 4, S_], BF16, stack=st)
            KA = sb("KA", [128, 4, S_], BF16, stack=st)
            allqa = [K("QA", h_, t_) for h_ in range(4) for t_ in range(NTC)]
            allka = [K("KA", h_, t_) for h_ in range(4) for t_ in range(NTC)]
            S.add("dve", lambda e: e.memset(QA[:], 0.0), [], allqa)
            S.add("dve", lambda e: e.memset(KA[:], 0.0), [], allka)
            for h_ in range(4):
                o_ = 64 if h_ % 2 == 0 else 0
                S.dma(KA[o_:o_ + 8, h_, :], blkoh_d[:, :], allka, [K("KAoh", h_)], q="pool", arena=True)
            VB = sb("VB", [128, NT, 256], BF16, stack=st)
            CAUS = sb("CAUS", [128, 4, 512], BF16, stack=st)
            S.dma(CAUS[:], cmask_d[:, 4096:6144].rearrange("p (a b) -> p a b", a=4), [], [K("CAUS")], q="pool", arena=True)
            GC = sb("GC", [128, 3, 128], stack=st)
            S.dma(GC[:], gate_c_d[:, :].rearrange("p (a b) -> p a b", a=3), [], [K("GC")])
            KM = sb("KM", [128, 2, 8], stack=st)
            KMb = sb("KMb", [128, 2, 8], BF16, stack=st)
            wv, wk, _ = load_w(w_in_d[l, :, 2308:2820], 8, 512)
            wv2, wk2, _ = load_w(w_in_d[l, :, 2820:3076], 8, 256)
            str_ = ExitStack()
            ROPEb = [sb(f"ROPE{j}", [128, 2, TW], stack=str_) for j in range(1)]
            RAW = [sb(f"RAW{j}", [128, 512], stack=str_) for j in range(2)]
            TMP = [sb(f"TMPr{j}", [128, 512], stack=str_) for j in range(2)]
            KF = [sb(f"KF{j}", [128, 512], stack=str_) for j in range(2)]
            nr = 0
            for c2 in range(2):
                for tc in range(NTC):
                    sl = slice(tc * TW, (tc + 1) * TW)
                    rj = 0
                    ROPE = ROPEb[rj]
                    rsl = slice(0, TW)
                    S.dma(ROPE[:, 0, :], rope_d[:, tc * TW:(tc + 1) * TW], [], [K("ROPE", rj, 0)])
                    S.dma(ROPE[:, 1, :], rope_d[:, S_ + tc * TW:S_ + (tc + 1) * TW], [], [K("ROPE", rj, 1)])
                    for which in range(2):
                        ps, pk = ps_rot()
                        proj_fm(wv, wk, which * 256 + c2 * 128, tc, ps, pk)
                        raw, rawk = RAW[nr % 2], K("RAW", nr % 2)
                        tmp, tmpk = TMP[nr % 2], K("TMPr", nr % 2)
                        S.cp(raw[:], ps[:, :], [pk], [rawk], eng="act")
                        psw, pkw = ps_rot()
                        S.mm(psw[:, :], perm_f, raw[:], True, True, [rawk, K("c128")], [pkw])
                        S.tt(tmp[:], psw[:, :], ROPE[:, 1, rsl], ALU.mult, [pkw, K("ROPE", rj, 1)], [tmpk])
                        kf = KF[nr % 2]
                        dst, dk = kf[:], K("KF", nr % 2)
                        S.tt(dst, raw[:], ROPE[:, 0, rsl], ALU.mult, [rawk, K("ROPE", rj, 0)], [dk])
                        S.tt(dst, dst, tmp[:], ALU.add, [dk, tmpk], [dk])
                        if which == 0:
                            S.act(QA[0:64, 2 * c2, sl], kf[0:64, :], AF.Copy, [dk], [K("QA", 2 * c2, tc)], scale=0.125)
                            S.act(QA[64:128, 2 * c2 + 1, sl], kf[64:128, :], AF.Copy, [dk], [K("QA", 2 * c2 + 1, tc)],
                                  scale=0.125)
                        else:
                            S.cp(KA[0:64, 2 * c2, sl], kf[0:64, :], [dk], [K("KA", 2 * c2, tc)], eng="act")
                            S.cp(KA[64:128, 2 * c2 + 1, sl], kf[64:128, :], [dk], [K("KA", 2 * c2 + 1, tc)], eng="act")
                            for hb in range(2):
                                nblk = tc * 2 + hb
                                S.add("dve", (lambda e, o=KM[:, c2, nblk:nblk + 1], i_=kf[:, hb * 256:(hb + 1) * 256]:
                                              e.reduce_sum(out=o, in_=i_, axis=AX.X)), [dk], [K("KM", c2, nblk)])
                        nr += 1
            for i in range(NT):
                ps, pk = ps_rot()
                for kc in range(KC):
                    S.mm(ps[:, 0:256], hT[:, kc, i * 128:(i + 1) * 128], wv2[:, kc, :], kc == 0, kc == KC - 1,
                         wk2 + [K("hT", kc, i // 4)], [pk])
                S.cp(VB[:, i, :], ps[:, 0:256], [pk], [K("VB", i)], eng=("dve" if i % 2 else "act"))
            S.cp(KMb[:], KM[:], [K("KM", c2_, nb_) for c2_ in range(2) for nb_ in range(8)], [K("KMb")])
            str_.close()
            S.barrier()
            PTb = [sb(f"PTb{j}", [128, 512], BF16, stack=st) for j in range(3)]
            RD = [sb(f"RD{j}", [128, 512], stack=st) for j in range(2)]
            GT = sb("GT", [128, 128], stack=st)
            BT_ = sb("BIASTM", [128, 128], stack=st)
            MX = [sb(f"MX{j}", [128, 8], stack=st) for j in range(2)]
            for h in range(4):
                hh, c2 = h % 2, h // 2
                pr = slice(hh * 64, hh * 64 + 64)
                o_ = 64 if hh == 0 else 0
                psg, pkg = ps_rot()
                for i in range(NT):
                    S.mm(psg[:, i * 8:(i + 1) * 8], QA[pr, h, i * 128:(i + 1) * 128], KMb[pr, c2, :], True, True,
                         [K("QA", h, i // 4), K("KMb")], [pkg])
                S.stt(GT[:], psg[:, 0:128], 8.0 / 256.0, GC[:, 0, :], ALU.mult, ALU.add, [pkg, K("GC")], [K("GT")])
                for i in range(NT):
                    mx, mxk = MX[i % 2], K("MX", i % 2)
                    S.add("dve", (lambda e, o=mx[:], i_=GT[:, i * 8:(i + 1) * 8]: e.max(out=o, in_=i_)),
                          [K("GT")], [mxk])
                    bsl = BT_[:, i * 8:(i + 1) * 8]
                    S.ts(bsl, GT[:, i * 8:(i + 1) * 8], mx[:, 2:3], None, ALU.is_ge, None, [K("GT"), mxk],
                         [K("BIASTM", i)])
                    S.tt(bsl, bsl, GC[:, 1, i * 8:(i + 1) * 8], ALU.mult, [K("BIASTM", i), K("GC")], [K("BIASTM", i)])
                    S.tt(bsl, bsl, GC[:, 2, i * 8:(i + 1) * 8], ALU.add, [K("BIASTM", i), K("GC")], [K("BIASTM", i)])
                    S.ts(bsl, bsl, -1.0, -NEG, ALU.add, ALU.mult, [K("BIASTM", i)], [K("BIASTM", i)])
                for q4 in range(4):
                    pst, pkt = ps_rot()
                    for q in range(4):
                        i = q4 * 4 + q
                        S.tr(pst[0:8, q * 128:(q + 1) * 128], BT_[:, i * 8:(i + 1) * 8], ident,
                             [K("BIASTM", i), K("c128")], [pkt])
                    S.cp(QA[o_:o_ + 8, h, q4 * 512:(q4 + 1) * 512], pst[0:8, :], [pkt], [K("QAsel", h, q4)])
            its = []
            grp = 0
            for h in range(4):
                for c in range(NTC):
                    nkt = 4 * c + 4
                    for kt in range(nkt):
                        its.append(dict(h=h, c=c, kt=kt, nkt=nkt, grp=grp, i=len(its)))
                    grp += 1

            def m_s1(m):
                h, c, kt = m["h"], m["c"], m["kt"]
                d = kt - 4 * c
                qsl = slice(c * TW, (c + 1) * TW)
                ksl = slice(kt * 128, (kt + 1) * 128)
                psS, pkS = ps_rot()
                S.mm(psS[:, :], KA[:, h, ksl], QA[:, h, qsl], True, d < 0,
                     [K("KA", h, kt // 4), K("KAoh", h), K("QA", h, c), K("QAsel", h, c)], [pkS])
                if d >= 0:
                    S.mm(psS[:, :], ident_b, CAUS[:, d, :], False, True, [K("CAUS"), K("c128b")], [pkS])
                pt, ptk = PTb[m["i"] % 3], K("PTb", m["i"] % 3)
                S.act(pt[:], psS[:, :], AF.Exp, [pkS], [ptk])

            def m_s2(m):
                h, c, kt, nkt = m["h"], m["c"], m["kt"], m["nkt"]
                hh, c2 = h % 2, h // 2
                pr = slice(hh * 64, hh * 64 + 64)
                qsl = slice(c * TW, (c + 1) * TW)
                g2 = m["grp"] % 2
                psO, pkO = PS[4 + g2], K("psO", g2)
                psD, pkD = PS[6 + g2], K("psD", g2)
                pt, ptk = PTb[m["i"] % 3], K("PTb", m["i"] % 3)
                S.mm(psO[:, :], VB[:, kt, c2 * 128:(c2 + 1) * 128], pt[:], kt == 0, kt == nkt - 1,
                     [K("VB", kt), ptk], [pkO])
                S.mm(psD[:, :], ones_b, pt[:], kt == 0, kt == nkt - 1, [ptk, K("c128b")], [pkD])
                if kt == nkt - 1:
                    rd, rdk = RD[g2], K("RD", g2)
                    S.add("dve", (lambda e, o=rd[pr, :], i_=psD[pr, :]: e.reciprocal(out=o, in_=i_)), [pkD], [rdk])
                    S.tt(YG[pr, c2, qsl], psO[pr, :], rd[pr, :], ALU.mult, [pkO, rdk], [K("YG", c2, c)])

            n_ = len(its)
            for r in range(-1, n_):
                if r + 1 < n_:
                    m_s1(its[r + 1])
                if r >= 0:
                    m_s2(its[r])
            dump(f"yd{l}", YG[:], [K("YG", c, t) for c in range(2) for t in range(NTC)], [128, 2, S_])
            st.close()
            S.barrier()
            group_out(l, 3, YG, sty)
        S.barrier()
        dump(f"x1_{l}", xT[:], ALLX, [128, KC, S_])
        if stop_after == f"D{l}":
            done = True
            break

        with ExitStack() as st:
            with ExitStack() as st2:
                rmsnorm_fm(xT, "xT", lambda kc: col(l, "xat_g", kc), hT, "hT", KC, S_, st2, tag="x")
            S.barrier()
            memT = sb("memT", [128, KC, 256], stack=st)
            MTb = sb("MTb", [128, KC, 256], BF16, stack=st)
            with ExitStack() as st2:
                load_fm(memT, mem_d, 2, "memT", st2)
                rmsnorm_fm(memT, "memT", lambda kc: col(l, "mem_g", kc), MTb, "MTb", KC, 256, st2, tag="m")
            S.barrier()
            KTb = sb("KTb", [128, KC, 256], BF16, stack=st)
            VX = sb("VX", [128, 2, D_], BF16, stack=st)
            QX = sb("QX", [128, KC, S_], BF16, stack=st)
            MTk = [K("MTb", kc, 0) for kc in range(KC)]
            for half in range(2):
                wv, wk, _ = load_w(wkv_d[l, :, half * 512:(half + 1) * 512], 8, 512)
                for o4 in range(4):
                    oc = half * 4 + o4
                    ps, pk = ps_rot()
                    for kc in range(KC):
                        S.mm(ps[:, 0:256], wv[:, kc, o4 * 128:(o4 + 1) * 128], MTb[:, kc, :], kc == 0, kc == KC - 1,
                             wk + [K("MTb", kc, 0)], [pk])
                    S.cp(KTb[:, oc, :], ps[:, 0:256], [pk], [K("KTb", oc)])
            for half in range(2):
                wv, wk, _ = load_w(wkv_d[l, :, 1024 + half * 512:1024 + (half + 1) * 512], 8, 512)
                for mt in range(2):
                    ps, pk = ps_rot()
                    for kc in range(KC):
                        S.mm(ps[:, :], MTb[:, kc, mt * 128:(mt + 1) * 128], wv[:, kc, :], kc == 0, kc == KC - 1,
                             wk + [K("MTb", kc, 0)], [pk])
                    S.cp(VX[:, mt, half * 512:(half + 1) * 512], ps[:, :], [pk], [K("VX", mt, half)], eng="act")
            for half in range(2):
                wv, wk, _ = load_w(wq_d[l, :, half * 512:(half + 1) * 512], 8, 512)
                for o4 in range(4):
                    oc = half * 4 + o4
                    for tc in range(NTC):
                        ps, pk = ps_rot()
                        proj_fm(wv, wk, o4 * 128, tc, ps, pk)
                        S.act(QX[:, oc, tc * TW:(tc + 1) * TW], ps[:, :], AF.Copy, [pk], [K("QX", oc, tc)],
                              scale=1.0 / 16.0)
            dump(f"X_MTb{l}", MTb[:], MTk, [128, KC, 256])
            dump(f"X_KTb{l}", KTb[:], [K("KTb", oc) for oc in range(KC)], [128, KC, 256])
            dump(f"X_QX{l}", QX[:], [K("QX", oc, tc) for oc in range(KC) for tc in range(NTC)], [128, KC, S_])
            dump(f"X_VX{l}", VX[:], [K("VX", mt, hf) for mt in range(2) for hf in range(2)], [128, 2, D_])
            PT = [sb(f"PTx{j}", [128, 512], BF16, stack=st) for j in range(4)]
            RDx = [sb(f"RDx{j}", [128, 512], stack=st) for j in range(2)]
            it = 0
            for hd in range(4):
                for tc in range(NTC):
                    sl = slice(tc * TW, (tc + 1) * TW)
                    pts = []
                    for mt in range(2):
                        ps, pk = ps_rot()
                        for j in range(2):
                            S.mm(ps[:, :], KTb[:, 2 * hd + j, mt * 128:(mt + 1) * 128], QX[:, 2 * hd + j, sl],
                                 j == 0, j == 1, [K("KTb", 2 * hd + j), K("QX", 2 * hd + j, tc)], [pk])
                        pt, ptk = PT[it % 4], K("PTx", it % 4)
                        S.act(pt[:], ps[:, :], AF.Exp, [pk], [ptk])
                        pts.append((pt, ptk))
                        it += 1
                    psD, pkD = ps_rot()
                    for mt in range(2):
                        S.mm(psD[:, :], ones_b, pts[mt][0][:], mt == 0, mt == 1, [pts[mt][1], K("c128b")], [pkD])
                    rd, rdk = RDx[(hd * NTC + tc) % 2], K("RDx", (hd * NTC + tc) % 2)
                    S.add("dve", (lambda e, o=rd[:], i_=psD[:, :]: e.reciprocal(out=o, in_=i_)), [pkD], [rdk])
                    for j in range(2):
                        oc = 2 * hd + j
                        ps, pk = ps_rot()
                        for mt in range(2):
                            S.mm(ps[:, :], VX[:, mt, oc * 128:(oc + 1) * 128], pts[mt][0][:], mt == 0, mt == 1,
                                 [K("VX", mt, oc // 4), pts[mt][1]], [pk])
                        S.tt(hT[:, oc, sl], ps[:, :], rd[:], ALU.mult, [pk, rdk], [K("hT", oc, tc)])
            dump(f"X_OX{l}", hT[:], ALLH, [128, KC, S_])
            for half in range(2):
                wv, wk, _ = load_w(wo_d[l, :, half * 512:(half + 1) * 512], 8, 512)
                for o4 in range(4):
                    oc = half * 4 + o4
                    for tc in range(NTC):
                        ps, pk = ps_rot()
                        proj_fm(wv, wk, o4 * 128, tc, ps, pk)
                        S.tt(xT[:, oc, tc * TW:(tc + 1) * TW], xT[:, oc, tc * TW:(tc + 1) * TW], ps[:, :], ALU.add,
                             [K("xT", oc, tc), pk], [K("xT", oc, tc)])
        S.barrier()
        dump(f"x2_{l}", xT[:], ALLX, [128, KC, S_])
        if stop_after == f"X{l}":
            done = True
            break

        with ExitStack() as st:
            WR = sb("WR", [128, KC, 20], stack=st)
            S.dma(WR[:], wrt_d[l].rearrange("(kc p) n -> p kc n", p=128), [], [K("WR")])
            ENf = sb("ENf", [16, 16, 128], stack=st)
            S.dma(ENf[:], en_d[:, :].rearrange("p (a b) -> p a b", a=16), [], [K("ENf")])
            LG = sb("LG", [128, NT, 20], stack=st)
            st_hf = ExitStack()
            HF = sb("HF", [128, KC, TW], stack=st_hf)

            def router_hook(tc, r):
                sl = slice(tc * TW, (tc + 1) * TW)
                for kc in range(KC):
                    S.stt(HF[:, kc, :], xT[:, kc, sl], col(l, "ffn_g", kc), r[:], ALU.mult, ALU.mult,
                          [K("xT", kc, tc), K("rsf", tc % 2), K("colp")], [K("HF", kc)])
                ps, pk = ps_rot()
                for q in range(4):
                    for kc in range(KC):
                        S.mm(ps[:, q * 20:(q + 1) * 20], HF[:, kc, q * 128:(q + 1) * 128], WR[:, kc, :], kc == 0,
                             kc == KC - 1, [K("HF", kc), K("WR")], [pk])
                S.cp(LG[:, tc * 4:tc * 4 + 4, :], ps[:, 0:80].rearrange("p (a b) -> p a b", a=4), [pk],
                     [K("LG", tc)])

            with ExitStack() as st2:
                rmsnorm_fm(xT, "xT", lambda kc: col(l, "ffn_g", kc), hT, "hT", KC, S_, st2, hook=router_hook, tag="f")
            st_hf.close()
            S.barrier()
            COMB = sb("COMB", [128, NT, 16], stack=st)
            CT = sb("CT", [16, S_], stack=st)
            sm = {nm: sb("rt_" + nm, [128, w_], stack=st) for nm, w_ in
                  [("lg", 4), ("m", 1), ("nm", 1), ("eg", 4), ("sg", 1), ("pt", 1), ("oh", 4), ("le", 16), ("sel", 4),
                   ("a", 1), ("na", 1), ("eq", 4), ("sel2", 4), ("b", 1), ("t2", 4), ("ex", 4), ("dd", 1), ("den", 1),
                   ("wg", 4)]}

            def v(nm):
                return sm[nm][:]

            def kk(nm):
                return [K("rt", nm)]

            for i in range(NT):
                lgk = [K("LG", i // 4)]
                S.tt(v("lg"), LG[:, i, 0:4], row(l, "bg"), ALU.add, lgk + [K("rowp")], kk("lg"))
                S.add("dve", lambda e: e.reduce_max(out=v("m"), in_=v("lg"), axis=AX.X), kk("lg"), kk("m"))
                S.ts(v("nm"), v("m"), -1.0, None, ALU.mult, None, kk("m"), kk("nm"))
                S.act(v("eg"), v("lg"), AF.Exp, kk("lg") + kk("nm"), kk("eg") + kk("sg"), bias=v("nm"), accum=v("sg"))
                S.add("dve", lambda e: e.reciprocal(out=v("pt"), in_=v("sg")), kk("sg"), kk("pt"))
                S.ts(v("oh"), v("lg"), v("m"), None, ALU.is_ge, None, kk("lg") + kk("m"), kk("oh"))
                S.tt(v("le"), LG[:, i, 4:20], row(l, "be"), ALU.add, lgk + [K("rowp")], kk("le"))
                S.ts(v("sel"), sm["le"][:, 0:4], sm["oh"][:, 0:1], None, ALU.mult, None, kk("le") + kk("oh"), kk("sel"))
                for g in range(1, 4):
                    S.stt(v("sel"), sm["le"][:, 4 * g:4 * g + 4], sm["oh"][:, g:g + 1], v("sel"), ALU.mult, ALU.add,
                          kk("le") + kk("oh") + kk("sel"), kk("sel"))
                S.add("dve", lambda e: e.reduce_max(out=v("a"), in_=v("sel"), axis=AX.X), kk("sel"), kk("a"))
                S.ts(v("eq"), v("sel"), v("a"), None, ALU.is_ge, None, kk("sel") + kk("a"), kk("eq"))
                S.stt(v("sel2"), v("eq"), -1e30, v("sel"), ALU.mult, ALU.add, kk("eq") + kk("sel"), kk("sel2"))
                S.add("dve", lambda e: e.reduce_max(out=v("b"), in_=v("sel2"), axis=AX.X), kk("sel2"), kk("b"))
                S.ts(v("t2"), v("sel"), v("b"), None, ALU.is_ge, None, kk("sel") + kk("b"), kk("t2"))
                S.ts(v("na"), v("a"), -1.0, None, ALU.mult, None, kk("a"), kk("na"))
                S.act(v("ex"), v("sel"), AF.Exp, kk("sel") + kk("na"), kk("ex"), bias=v("na"))
                S.act(v("dd"), v("b"), AF.Exp, kk("b") + kk("na"), kk("dd"), bias=v("na"))
                S.ts(v("den"), v("dd"), 1.0, None, ALU.add, None, kk("dd"), kk("den"))
                S.add("dve", lambda e: e.reciprocal(out=v("den"), in_=v("den")), kk("den"), kk("den"))
                S.tt(v("wg"), v("ex"), v("t2"), ALU.mult, kk("ex") + kk("t2"), kk("wg"))
                S.ts(v("wg"), v("wg"), v("den"), v("pt"), ALU.mult, ALU.mult, kk("wg") + kk("den") + kk("pt"), kk("wg"))
                for g in range(4):
                    S.ts(COMB[:, i, 4 * g:4 * g + 4], v("wg"), sm["oh"][:, g:g + 1], None, ALU.mult, None,
                         kk("wg") + kk("oh"), [K("COMB", i, g)])
            for q4 in range(4):
                pst, pkt = ps_rot()
                for q in range(4):
                    i = q4 * 4 + q
                    S.tr(pst[0:16, q * 128:(q + 1) * 128], COMB[:, i, :], ident,
                         [K("COMB", i, g) for g in range(4)] + [K("c128")], [pkt])
                S.cp(CT[0:16, q4 * 512:(q4 + 1) * 512], pst[0:16, :], [pkt], [K("CT", q4)])
            dump(f"comb{l}", CT[:], [K("CT", q) for q in range(4)], [16, S_])
            HID = sb("HID", [128, 8, S_], BF16, stack=st)
            W2S = sb("W2S", [128, 8, D_], BF16, stack=st)
            CB = [sb(f"CB{j}", [128, 512], stack=st) for j in range(2)]
            SL = [sb(f"SL{j}", [128, 512], stack=st) for j in range(2)]
            n = 0
            for g in range(4):
                for el in range(4):
                    ex = 4 * g + el
                    S.dma(W2S[:, 2 * el:2 * el + 2, :], w2_d[l, ex].rearrange("(fc p) n -> p fc n", p=128), [],
                          [K("W2S", el)], q="pool", arena=True)
                    wv, wk1_, slot = load_w(w1_d[l, ex], 8, 256, c0=0, tot=512)
                    _, wk3_, _ = load_w(w3_d[l, ex], 8, 256, slot=slot, c0=256, tot=512)
                    wk = wk1_ + wk3_
                    for tc in range(NTC):
                        sl = slice(tc * TW, (tc + 1) * TW)
                        psC, pkC = ps_rot()
                        S.mm(psC[:, :], ENf[0:16, ex, :], CT[0:16, sl], True, True, [K("ENf"), K("CT", tc)], [pkC])
                        cb, cbk = CB[n % 2], K("CB", n % 2)
                        S.cp(cb[:], psC[:, :], [pkC], [cbk], eng="act")
                        for fc in range(2):
                            ps1, pk1 = ps_rot()
                            proj_fm(wv, wk, fc * 128, tc, ps1, pk1)
                            ps3, pk3 = ps_rot()
                            proj_fm(wv, wk, 256 + fc * 128, tc, ps3, pk3)
                            s_, sk = SL[(2 * n + fc) % 2], K("SL", (2 * n + fc) % 2)
                            S.act(s_[:], ps1[:, :], AF.Silu, [pk1], [sk])
                            S.tt(s_[:], s_[:], ps3[:, :], ALU.mult, [sk, pk3], [sk])
                            S.tt(HID[:, el * 2 + fc, sl], s_[:], cb[:], ALU.mult, [sk, cbk], [K("HID", el * 2 + fc, tc)])
                        n += 1
                for oc in range(KC):
                    for tc in range(NTC):
                        ps, pk = ps_rot()
                        for j in range(8):
                            S.mm(ps[:, :], W2S[:, j, oc * 128:(oc + 1) * 128], HID[:, j, tc * TW:(tc + 1) * TW],
                                 j == 0, j == 7, [K("W2S", j // 2), K("HID", j, tc)], [pk])
                        S.tt(xT[:, oc, tc * TW:(tc + 1) * TW], xT[:, oc, tc * TW:(tc + 1) * TW], ps[:, :], ALU.add,
                             [K("xT", oc, tc), pk], [K("xT", oc, tc)])
        S.barrier()
        dump(f"x3_{l}", xT[:], ALLX, [128, KC, S_])
        if stop_after == f"M{l}":
            done = True
            break

    if stop_after is None:
        with ExitStack() as st:
            HF = sb("HFo", [128, KC, TW], stack=st)
            OB = [sb(f"OB{j}", [128, D_], stack=st) for j in range(2)]
            nob = [0]

            def final_hook(tc, r):
                sl = slice(tc * TW, (tc + 1) * TW)
                for kc in range(KC):
                    S.stt(HF[:, kc, :], xT[:, kc, sl], col(0, "fin_g", kc), r[:], ALU.mult, ALU.mult,
                          [K("xT", kc, tc), K("rsz", tc % 2), K("colp")], [K("HFo", kc)])
                for q in range(4):
                    ob, obk = OB[nob[0] % 2], K("OB", nob[0] % 2)
                    nob[0] += 1
                    for half in range(2):
                        ps, pk = ps_rot()
                        for j in range(4):
                            S.tr(ps[:, j * 128:(j + 1) * 128], HF[:, half * 4 + j, q * 128:(q + 1) * 128], ident,
                                 [K("HFo", half * 4 + j), K("c128")], [pk])
                        S.cp(ob[:, half * 512:(half + 1) * 512], ps[:, :], [pk], [obk],
                             eng=("act" if half else "dve"))
                    r0 = tc * TW + q * 128
                    i = S.dma(out_d[r0:r0 + 128, :], ob[:], [obk], [K("out", r0)])
                    S.final.append(i)

            rmsnorm_fm(xT, "xT", lambda kc: col(0, "fin_g", kc), None, None, KC, S_, st, hook=final_hook, tag="z")

    S.finalize(es)
    es.close()
    return nc, dump_d


def make_consts():
    c128 = np.zeros((128, 8, 128), np.float32)
    j = np.arange(128)[:, None]
    s = np.arange(128)[None, :]
    c128[:, 0] = np.eye(128)
    c128[:, 1] = 1.0
    c128[:, 2] = (j <= s)
    c128[:, 3] = -1.0 * (j >= s)
    c128[:, 4] = -1.0
    partner = np.where((np.arange(128) % 64) < 32, np.arange(128) + 32, np.arange(128) - 32)
    perm = np.zeros((128, 128), np.float32)
    perm[partner, np.arange(128)] = 1.0
    c128[:, 5] = perm
    c128[:, 6] = NEG * (s < j)
    cm = np.zeros((128, 12, 512), np.float32)
    p = np.arange(128)[:, None]
    f = np.arange(512)[None, :]
    for d in range(4):
        valid = (f - p) > d * 128
        cm[:, d] = valid
        cm[:, 4 + d] = NEG * (~valid)
        cm[:, 8 + d] = NEG * ((d * 128 + p) > f)
    half = 32
    inv = (np.float32(10000.0) ** (-(np.arange(half, dtype=np.float32)) / np.float32(half))).astype(np.float32)
    ang = np.arange(S_, dtype=np.float32)[None, :] * inv[:, None]
    cos = np.cos(ang).astype(np.float32)
    sin = np.sin(ang).astype(np.float32)
    rope = np.zeros((128, 2, S_), np.float32)
    for pp in range(128):
        d = pp % 64
        rope[pp, 0] = cos[d % 32]
        rope[pp, 1] = -sin[d % 32] if d < 32 else sin[d % 32]
    en = np.zeros((16, 16, 128), np.float32)
    for n in range(16):
        en[n, n, :] = 1.0
    gc = np.zeros((128, 3, 16, 8), np.float32)
    for i in range(16):
        own = i // 2
        for n in range(8):
            gc[:, 0, i, n] = 0.0 if n < own else -1e30
            gc[:, 1, i, n] = 1.0 if n < own else 0.0
            gc[:, 2, i, n] = 1.0 if n == own else 0.0
    blkoh = np.zeros((8, S_), np.float32)
    for n in range(8):
        blkoh[n, n * 256:(n + 1) * 256] = 1.0
    return dict(c128=c128.reshape(128, -1), cmask=cm.reshape(128, -1), rope=rope.reshape(128, -1),
                en=en.reshape(16, -1), gatec=gc.reshape(128, -1), blkoh=blkoh)


def colvec(v):
    v = np.asarray(v, np.float32)
    return np.ascontiguousarray(v.reshape(-1, 128).T)


def pack_params(inp):
    colp = np.zeros((128, L_, NCOL), np.float32)
    rowp = np.zeros((128, L_, NROW), np.float32)

    def put(l, name, arr):
        c0, w = COLS[name]
        assert arr.shape == (128, w), (name, arr.shape)
        colp[:, l, c0:c0 + w] = arr

    for l in range(L_):
        put(l, "mix_g", colvec(inp["mix_norm_g"][l]))
        put(l, "xat_g", colvec(inp["xattn_norm_g"][l]))
        put(l, "mem_g", colvec(inp["mem_norm_g"][l]))
        put(l, "ffn_g", colvec(inp["ffn_norm_g"][l]))
        put(l, "grp_g", colvec(inp["group_norm_g"][l]))
        cw = inp["lru_conv_w"][l]
        put(l, "lru_cw", np.concatenate([colvec(cw[k]) for k in range(4)], axis=1).reshape(128, 4, 2)
            .transpose(0, 2, 1).reshape(128, 8))
        put(l, "lru_cb", colvec(inp["lru_conv_b"][l]))
        put(l, "lru_br", colvec(inp["lru_br"][l]))
        put(l, "lru_bi", colvec(inp["lru_bi"][l]))
        put(l, "lru_lam", colvec(inp["lru_lambda"][l]))
        sw = inp["ssm_conv_w"][l]
        put(l, "ssm_cw", np.concatenate([colvec(sw[k]) for k in range(4)], axis=1).reshape(128, 4, 6)
            .transpose(0, 2, 1).reshape(128, 24))
        put(l, "ssm_cb", colvec(inp["ssm_conv_b"][l]))
        put(l, "ssm_d", colvec(np.repeat(inp["ssm_d"][l], 64)))
        put(l, "fin_g", colvec(inp["final_norm_g"]))
        r0, w = ROWS["dt_bias"]
        rowp[:, l, r0:r0 + w] = np.tile(inp["ssm_dt_bias"][l], 16)[None, :]
        r0, w = ROWS["a_log"]
        rowp[:, l, r0:r0 + w] = np.tile(inp["ssm_a_log"][l], 16)[None, :]
        r0, w = ROWS["bg"]
        rowp[:, l, r0:r0 + w] = inp["router_group_b"][l][None, :]
        r0, w = ROWS["be"]
        rowp[:, l, r0:r0 + w] = inp["router_expert_b"][l][None, :]
    wbd = np.zeros((L_, 2, 2, 128, 128), np.float32)
    for l in range(L_):
        for gi, nm in enumerate(("lru_wr", "lru_wi")):
            for c in range(2):
                for hh in range(2):
                    wbd[l, gi, c, hh * 64:(hh + 1) * 64, hh * 64:(hh + 1) * 64] = inp[nm][l, 2 * c + hh]
    router_w = np.concatenate([inp["router_group_w"], inp["router_expert_w"]], axis=2)
    return dict(colp=colp.reshape(128, -1), rowp=rowp.reshape(128, -1), lru_wbd=wbd,
                router_w=np.ascontiguousarray(router_w))


SHARED = ["w_in", "w_out", "xattn_wq", "xattn_wkv", "xattn_wo", "expert_w1", "expert_w3", "expert_w2"]


def make_in_maps(inputs, cores):
    inp = {k: np.asarray(v) for k, v in inputs.items()}
    consts = make_consts()
    packed = pack_params(inp)
    shared = {k: np.ascontiguousarray(inp[k], dtype=np.float32) for k in SHARED}
    maps = []
    for b in cores:
        m = dict(shared)
        m.update(consts)
        m.update(packed)
        m["x"] = np.ascontiguousarray(inp["x"][b])
        m["mem"] = np.ascontiguousarray(inp["mem"][b])
        maps.append(m)
    return maps


_CACHE = {}


def kernel(**inputs):
    if "nc" not in _CACHE:
        _CACHE["nc"] = build_program()[0]
    nc = _CACHE["nc"]
    maps = make_in_maps(inputs, list(range(8)))
    res = run_bass_kernel_spmd(nc, maps, core_ids=list(range(8)))
    return np.stack([r["out"] for r in res.results], axis=0).astype(np.float32)
```

```python
import numpy as np
from contextlib import ExitStack
import concourse.bass as bass
import concourse.mybir as mybir
from concourse.bass_utils import run_bass_kernel_spmd

F32 = mybir.dt.float32
BF16 = mybir.dt.bfloat16
F32R = mybir.dt.float32r
AF = mybir.ActivationFunctionType
ALU = mybir.AluOpType
AX = mybir.AxisListType

S_ = 2048
D_ = 1024
L_ = 2
KC = 8
NTC = 4
TW = 512
NT = 16
NEG = -30000.0
EPS = 1e-6

COLS = {}
_c = 0
for _n, _w in [("mix_g", 8), ("xat_g", 8), ("mem_g", 8), ("ffn_g", 8), ("grp_g", 8),
               ("lru_cw", 8), ("lru_cb", 2), ("lru_br", 2), ("lru_bi", 2), ("lru_lam", 2),
               ("ssm_cw", 24), ("ssm_cb", 6), ("ssm_d", 2), ("fin_g", 8)]:
    COLS[_n] = (_c, _w)
    _c += _w
NCOL = _c
ROWS = {}
_c = 0
for _n, _w in [("dt_bias", 64), ("a_log", 64), ("bg", 4), ("be", 16)]:
    ROWS[_n] = (_c, _w)
    _c += _w
NROW = _c


class Sched:
    ENG = ("pe", "act", "dve", "pool", "sp")

    def __init__(self, nc):
        self.nc = nc
        self.ops = []
        self.lastw = {}
        self.rd = {}
        self.by_eng = {e: [] for e in self.ENG}
        self.extra = {e: set() for e in self.ENG}
        self.dma_since = []
        self.final = []
        self.psn = 0

    def add(self, eng, fn, reads=(), writes=(), dma=False):
        i = len(self.ops)
        reads = list(reads)
        writes = list(writes)
        deps = {}
        for r in reads:
            w = self.lastw.get(r)
            if w is not None:
                deps.setdefault(w, set()).add("raw")
        for w_ in writes:
            w = self.lastw.get(w_)
            if w is not None:
                deps.setdefault(w, set()).add("waw")
            rr = self.rd.get(w_)
            if rr:
                for r in rr[0].values():
                    deps.setdefault(r, set()).add("war")
                for r in rr[1]:
                    deps.setdefault(r, set()).add("war")
        ws = set(writes)
        for r in reads:
            if r not in ws:
                rr = self.rd.setdefault(r, ({}, []))
                if dma:
                    rr[1].append(i)
                else:
                    rr[0][eng] = i
        for w_ in writes:
            self.lastw[w_] = i
            self.rd[w_] = ({}, [])
        fdeps = set()
        for d, kinds in deps.items():
            od = self.ops[d]
            if od["eng"] == eng and not od["dma"] and not dma:
                if eng == "pe":
                    continue
                if "raw" not in kinds:
                    continue
            fdeps.add(d)
        fdeps |= self.extra[eng]
        self.extra[eng] = set()
        self.ops.append(dict(eng=eng, fn=fn, deps=fdeps, dma=dma, signal=dma, token=None))
        self.by_eng[eng].append(i)
        if dma and eng != "pool":
            self.dma_since.append(i)
        return i

    def barrier(self):
        toks = set()
        for e in self.ENG:
            for i in reversed(self.by_eng[e]):
                if not self.ops[i]["dma"]:
                    toks.add(i)
                    break
        toks |= set(self.dma_since)
        self.dma_since = []
        self.bar_toks = set(toks)
        for e in ("pe", "act", "dve", "sp"):
            self.extra[e] |= toks

    def mm(self, out, lhsT, rhs, start, stop, reads, writes):
        return self.add("pe", lambda e: e.matmul(out, lhsT, rhs, start=start, stop=stop), reads, writes)

    def tr(self, out, in_, ident, reads, writes):
        return self.add("pe", lambda e: e.transpose(out, in_, ident), reads, writes)

    def act(self, out, in_, func, reads, writes, bias=None, scale=None, accum=None):
        kw = {}
        if bias is not None:
            kw["bias"] = bias
        if scale is not None:
            kw["scale"] = scale
        if accum is not None:
            kw["accum_out"] = accum
        return self.add("act", lambda e: e.activation(out=out, in_=in_, func=func, **kw), reads, writes)

    def tt(self, out, in0, in1, op, reads, writes, eng="dve"):
        return self.add(eng, lambda e: e.tensor_tensor(out=out, in0=in0, in1=in1, op=op), reads, writes)

    def ts(self, out, in0, s1, s2, op0, op1, reads, writes, eng="dve"):
        if s2 is None:
            return self.add(eng, lambda e: e.tensor_scalar(out, in0, s1, None, op0), reads, writes)
        return self.add(eng, lambda e: e.tensor_scalar(out, in0, s1, s2, op0, op1), reads, writes)

    def stt(self, out, in0, scalar, in1, op0, op1, reads, writes, eng="dve"):
        return self.add(eng, lambda e: e.scalar_tensor_tensor(out=out, in0=in0, scalar=scalar, in1=in1,
                                                             op0=op0, op1=op1), reads, writes)

    def cp(self, out, in_, reads, writes, eng="dve"):
        if eng == "act":
            return self.add("act", lambda e: e.activation(out=out, in_=in_, func=AF.Copy), reads, writes)
        return self.add(eng, lambda e: e.tensor_copy(out=out, in_=in_), reads, writes)

    def dma(self, out, in_, reads, writes, q="sp", arena=False):
        i = self.add(q, lambda e: e.dma_start(out=out, in_=in_), reads, writes, dma=True)
        if arena:
            self.ops[i]["deps"] |= getattr(self, "bar_toks", set())
        return i

    def finalize(self, es):
        nc = self.nc
        for op in self.ops:
            for d in op["deps"]:
                self.ops[d]["signal"] = True
        for d in self.final:
            self.ops[d]["signal"] = True
        csem = {e: es.enter_context(nc.semaphore("c_" + e)) for e in ("pe", "act", "dve", "pool")}
        NP = 16
        qsem = {q: [es.enter_context(nc.semaphore(f"q_{q}{k}")) for k in range(NP)] for q in ("sp", "pool")}
        cnt = {e: 0 for e in csem}
        qn = {q: 0 for q in qsem}
        quse = {q: [0] * NP for q in qsem}
        qprev = {q: [None] * NP for q in qsem}
        for i, op in enumerate(self.ops):
            if op["dma"]:
                q = op["eng"]
                k = qn[q] % NP
                qn[q] += 1
                quse[q][k] += 1
                op["token"] = (qsem[q][k], 16 * quse[q][k])
                if qprev[q][k] is not None:
                    op["deps"].add(qprev[q][k])
                qprev[q][k] = i
            elif op["signal"]:
                e = op["eng"]
                cnt[e] += 1
                op["token"] = (csem[e], cnt[e])
        ops = self.ops
        final = list(self.final)

        def emit(engname, e):
            waited = {}
            for i in self.by_eng[engname]:
                op = ops[i]
                for d in sorted(op["deps"]):
                    sem, val = ops[d]["token"]
                    if waited.get(sem, 0) < val:
                        e.wait_ge(sem, val)
                        waited[sem] = val
                ins = op["fn"](e)
                if op["signal"]:
                    ins.then_inc(op["token"][0], 16 if op["dma"] else 1)
            if engname == "sp":
                for d in final:
                    sem, val = ops[d]["token"]
                    if waited.get(sem, 0) < val:
                        e.wait_ge(sem, val)
                        waited[sem] = val

        with nc.Block() as block:
            @block.tensor
            def _(e):
                emit("pe", e)

            @block.scalar
            def _(e):
                emit("act", e)

            @block.vector
            def _(e):
                emit("dve", e)

            @block.gpsimd
            def _(e):
                emit("pool", e)

            @block.sync
            def _(e):
                emit("sp", e)


def K(name, *idx):
    return (name,) + idx


def build_program(stop_after=None, dumps=(), n_layers=L_):
    nc = bass.Bass("TRN2", target_bir_lowering=False)
    S = Sched(nc)
    es = ExitStack()
    P = {}

    def din(name, shape):
        P[name] = nc.dram_tensor(name, list(shape), F32, kind="ExternalInput").ap()
        return P[name]

    x_d = din("x", [S_, D_])
    mem_d = din("mem", [256, D_])
    w_in_d = din("w_in", [L_, D_, 3076])
    w_out_d = din("w_out", [L_, D_, D_])
    wq_d = din("xattn_wq", [L_, D_, D_])
    wkv_d = din("xattn_wkv", [L_, D_, 2 * D_])
    wo_d = din("xattn_wo", [L_, D_, D_])
    w1_d = din("expert_w1", [L_, 16, D_, 256])
    w3_d = din("expert_w3", [L_, 16, D_, 256])
    w2_d = din("expert_w2", [L_, 16, 256, D_])
    wrt_d = din("router_w", [L_, D_, 20])
    lrubd_d = din("lru_wbd", [L_, 2, 2, 128, 128])
    colp_d = din("colp", [128, L_ * NCOL])
    rowp_d = din("rowp", [128, L_ * NROW])
    c128_d = din("c128", [128, 8 * 128])
    cmask_d = din("cmask", [128, 12 * 512])
    rope_d = din("rope", [128, 2 * S_])
    en_d = din("en", [16, 16 * 128])
    blkoh_d = din("blkoh", [8, S_])
    gate_c_d = din("gatec", [128, 3 * 128])
    out_d = nc.dram_tensor("out", [S_, D_], F32, kind="ExternalOutput").ap()
    dump_d = {}

    uid = [0]

    def sb(name, shape, dt=F32, stack=None):
        uid[0] += 1
        return (stack or es).enter_context(nc.sbuf_tensor(f"{name}_{uid[0]}", list(shape), dt))

    xT = sb("xT", [128, KC, S_])
    hT = sb("hT", [128, KC, S_], BF16)
    colp = sb("colp_s", [128, L_ * NCOL])
    rowp = sb("rowp_s", [128, L_ * NROW])
    c128 = sb("c128_s", [128, 8 * 128])
    c128b = sb("c128b_s", [128, 2 * 128], BF16)
    W = [sb(f"wslot{i}", [128, KC, 512], BF16) for i in range(3)]
    PS = [es.enter_context(nc.psum_tensor(f"ps{i}", [128, 512], F32)) for i in range(8)]
    wn = [0]

    ident = c128[:, 0:128]
    ones_f = c128[:, 128:256]
    tri_le = c128[:, 256:384]
    negu = c128[:, 384:512]
    negones = c128[:, 512:640]
    perm_f = c128[:, 640:768]
    ssdneg = c128[:, 768:896]
    ones_b = c128b[:, 128:256]
    ident_b = c128b[:, 0:128]

    cst = sb("cst", [128, 4])
    S.add("dve", lambda e: e.memset(cst[:, 0:1], EPS), [], [K("cst0")])
    S.add("dve", lambda e: e.memset(cst[:, 1:2], 1.0), [], [K("cst1")])
    S.add("dve", lambda e: e.memset(cst[:, 2:4], 0.0), [K("cst0"), K("cst1")], [K("cst")])
    S.dma(colp[:], colp_d[:, :], [], [K("colp")])
    S.dma(rowp[:], rowp_d[:, :], [], [K("rowp")])
    S.dma(c128[:], c128_d[:, :], [], [K("c128")])
    S.dma(c128b[:], c128_d[:, 0:256], [], [K("c128b")], q="pool")

    def col(l, name, j=0, n=1):
        c0, w = COLS[name]
        return colp[:, l * NCOL + c0 + j: l * NCOL + c0 + j + n]

    def row(l, name, j=0, n=None):
        c0, w = ROWS[name]
        n = w if n is None else n
        return rowp[:, l * NROW + c0 + j: l * NROW + c0 + j + n]

    def ps_rot():
        k = S.psn % 4
        S.psn += 1
        return PS[k], K("ps", k)

    Wf = [w[:].rearrange("p a b -> p (a b)") for w in W]

    def load_w(src2d, nk, ncols, slot=None, c0=0, tot=None):
        tot = ncols if tot is None else tot
        if slot is None:
            k = wn[0] % 3
            wn[0] += 1
        else:
            k = slot
        v = Wf[k][:, 0:nk * tot].rearrange("p (a b) -> p a b", a=nk)
        step = 2 if nk >= 2 else 1
        for h in range(0, nk, step):
            S.dma(v[:, h:h + step, c0:c0 + ncols],
                  src2d[h * 128:(h + step) * 128, :].rearrange("(kc p) n -> p kc n", p=128),
                  [], [K("w", k)], q="pool")
        return v, [K("w", k)], k

    def dump(name, ap, reads, shape):
        if name in dumps:
            d = nc.dram_tensor("dbg_" + name, list(shape), ap.dtype, kind="ExternalOutput").ap()
            dump_d[name] = d
            i = S.dma(d, ap, reads, [K("dbg", name)])
            S.final.append(i)

    def load_fm(dst, src_d, ntile, dkey, st):
        xin = [sb(f"xin{j}", [128, D_], stack=st) for j in range(2)]
        for i in range(ntile):
            b = xin[i % 2]
            S.dma(b[:], src_d[i * 128:(i + 1) * 128, :], [], [K("xin", i % 2)])
            for half in range(2):
                ps, pk = ps_rot()
                for j in range(4):
                    kc = half * 4 + j
                    S.tr(ps[:, j * 128:(j + 1) * 128], b[:, kc * 128:(kc + 1) * 128], ident,
                         [K("xin", i % 2), K("c128")], [pk])
                S.cp(dst[:, half * 4:half * 4 + 4, i * 128:(i + 1) * 128],
                     ps[:].rearrange("p (a b) -> p a b", a=4), [pk],
                     [K(dkey, half * 4 + j, i // 4) for j in range(4)], eng=("dve" if half == 0 else "act"))

    with ExitStack() as st:
        load_fm(xT, x_d, NT, "xT", st)
    S.barrier()
    dump("xT0", xT[:], [K("xT", kc, tc) for kc in range(KC) for tc in range(NTC)], [128, KC, S_])

    def rmsnorm_fm(src, skey, gcol, dst, dkey, nk, T, st, dscale=1.0, hook=None, tag="n"):
        tw = min(T, TW)
        sq = [sb(f"sq_{tag}{j}", [128, nk, tw], BF16, stack=st) for j in range(2)]
        rs = [sb(f"rs_{tag}{j}", [128, tw], stack=st) for j in range(2)]
        for tc in range(T // tw):
            sl = slice(tc * tw, (tc + 1) * tw)
            q = sq[tc % 2]
            for kc in range(nk):
                S.act(q[:, kc, :], src[:, kc, sl], AF.Square, [K(skey, kc, tc)], [K("sq" + tag, tc % 2, kc)])
            ps, pk = ps_rot()
            for kc in range(nk):
                S.mm(ps[:, 0:tw], ones_b, q[:, kc, :], kc == 0, kc == nk - 1,
                     [K("sq" + tag, tc % 2, kc), K("c128b")], [pk])
            r = rs[tc % 2]
            S.act(r[:], ps[:, 0:tw], AF.Sqrt, [pk, K("cst")], [K("rs" + tag, tc % 2)],
                  bias=cst[:, 0:1], scale=1.0 / (nk * 128))
            S.add("dve", (lambda e, r=r: e.reciprocal(out=r[:], in_=r[:])),
                  [K("rs" + tag, tc % 2)], [K("rs" + tag, tc % 2)])
            if dst is not None:
                for kc in range(nk):
                    S.stt(dst[:, kc, sl], src[:, kc, sl], gcol(kc), r[:], ALU.mult, ALU.mult,
                          [K(skey, kc, tc), K("rs" + tag, tc % 2), K("colp")], [K(dkey, kc, tc)],
                          eng="dve")
            if hook is not None:
                hook(tc, r)

    ALLH = [K("hT", kc, tc) for kc in range(KC) for tc in range(NTC)]

    def hkeys(tc):
        return [K("hT", kc, tc) for kc in range(KC)]

    def proj_fm(wv, wkeys, c0, tc, ps, pk):
        for kc in range(KC):
            S.mm(ps[:, :], wv[:, kc, c0:c0 + 128], hT[:, kc, tc * TW:(tc + 1) * TW], kc == 0, kc == KC - 1,
                 wkeys + [K("hT", kc, tc)], [pk])

    def conv_chunk(ps, pk, tc, xw, xwk, cwcol, cbcol, dst, dkey):
        w_ = xw[tc % 2]
        wk = K(xwk, tc % 2)
        if tc == 0:
            S.add("dve", lambda e: e.memset(w_[:, 0:3], 0.0), [], [wk])
        else:
            S.cp(w_[:, 0:3], xw[(tc - 1) % 2][:, 512:515], [K(xwk, (tc - 1) % 2)], [wk])
        S.cp(w_[:, 3:515], ps[:, :], [pk], [wk], eng="act")
        S.ts(dst, w_[:, 3:515], cwcol(3), cbcol, ALU.mult, ALU.add, [wk, K("colp")], [dkey])
        for k in range(3):
            S.stt(dst, w_[:, k:k + 512], cwcol(k), dst, ALU.mult, ALU.add, [wk, dkey, K("colp")], [dkey])

    def group_out(l, g, YG, st):
        YB = sb("YB", [128, 2, S_], BF16, stack=st)
        rmsnorm_fm(YG, "YG", lambda kc: col(l, "grp_g", 2 * g + kc), YB, "YB", 2, S_, st, tag="g")
        dump(f"yn{l}_{g}", YB[:], [K("YB", c, t) for c in range(2) for t in range(NTC)], [128, 2, S_])
        for half in range(2):
            wv, wk, _ = load_w(w_out_d[l, g * 256:(g + 1) * 256, half * 512:(half + 1) * 512], 2, 512)
            for o4 in range(4):
                oc = half * 4 + o4
                for tc in range(NTC):
                    ps, pk = ps_rot()
                    for kc in range(2):
                        S.mm(ps[:, :], wv[:, kc, o4 * 128:(o4 + 1) * 128], YB[:, kc, tc * TW:(tc + 1) * TW],
                             kc == 0, kc == 1, wk + [K("YB", kc, tc)], [pk])
                    S.tt(xT[:, oc, tc * TW:(tc + 1) * TW], xT[:, oc, tc * TW:(tc + 1) * TW], ps[:, :], ALU.add,
                         [K("xT", oc, tc), pk], [K("xT", oc, tc)])

    ALLX = [K("xT", kc, tc) for kc in range(KC) for tc in range(NTC)]
    done = False
    for l in range(n_layers):
        with ExitStack() as st:
            rmsnorm_fm(xT, "xT", lambda kc: col(l, "mix_g", kc), hT, "hT", KC, S_, st, tag="a")
        S.barrier()
        if l == 0:
            dump("h0", hT[:], ALLH, [128, KC, S_])
        if stop_after == "norm0":
            break

        with ExitStack() as sty, ExitStack() as st:
            YG = sb("YG", [128, 2, S_], stack=sty)
            wv, wk, _ = load_w(w_in_d[l, :, 0:512], 8, 512)
            wbd = sb("wbd", [128, 4, 128], BF16, stack=st)
            S.dma(wbd[:], lrubd_d[l].rearrange("g c k m -> k (g c) m"), [], [K("wbd")], q="pool", arena=True)
            c8 = sb("c8", [128, 2], stack=st)
            dump(f"A_wbd{l}", wbd[:], [K("wbd")], [128, 4, 128])
            cy = sb("cy", [128, 2], stack=st)
            cz = sb("cz", [128, 2], stack=st)
            S.act(cy[:], col(l, "lru_lam", 0, 2), AF.Exp, [K("colp")], [K("cy")], scale=-1.0)
            S.ts(cz[:], cy[:], 2.0, None, ALU.add, None, [K("cy")], [K("cz")])
            S.add("dve", lambda e: e.reciprocal(out=cz[:], in_=cz[:]), [K("cz")], [K("cz")])
            S.tt(cz[:], cz[:], cy[:], ALU.mult, [K("cz"), K("cy")], [K("cz")])
            S.tt(cy[:], cz[:], cz[:], ALU.mult, [K("cz")], [K("cy")])
            S.ts(c8[:], cy[:], 1.0 / 9.0, 1.0 / 7.0, ALU.mult, ALU.add, [K("cy")], [K("c8")])
            for cc_ in (1.0 / 5.0, 1.0 / 3.0, 1.0):
                S.tt(c8[:], c8[:], cy[:], ALU.mult, [K("c8"), K("cy")], [K("c8")])
                S.ts(c8[:], c8[:], cc_, None, ALU.add, None, [K("c8")], [K("c8")])
            S.tt(c8[:], c8[:], cz[:], ALU.mult, [K("c8"), K("cz")], [K("c8")])
            S.ts(c8[:], c8[:], -16.0, None, ALU.mult, None, [K("c8")], [K("c8")])
            xw = [sb(f"xw{j}", [128, 515], stack=st) for j in range(2)]
            XC = sb("XC", [128, S_], stack=st)
            XCB = sb("XCB", [128, S_], BF16, stack=st)
            GG = sb("GG", [128, S_], stack=st)
            RA = sb("RA", [128, S_], stack=st)
            IU = sb("IU", [128, S_], stack=st)
            T2 = sb("T2", [128, S_], stack=st)
            XX = sb("XX", [128, S_], stack=st)
            for c in range(2):
                for tc in range(NTC):
                    sl = slice(tc * TW, (tc + 1) * TW)
                    ps, pk = ps_rot()
                    proj_fm(wv, wk, c * 128, tc, ps, pk)
                    conv_chunk(ps, pk, tc, xw, "xw", lambda k: col(l, "lru_cw", c * 4 + k), col(l, "lru_cb", c),
                               XC[:, sl], K("XC", tc))
                    ps, pk = ps_rot()
                    proj_fm(wv, wk, 256 + c * 128, tc, ps, pk)
                    S.cp(GG[:, sl], ps[:, :], [pk], [K("GG", tc)], eng="act")
                XCk = [K("XC", t) for t in range(NTC)]
                GGk = [K("GG", t) for t in range(NTC)]
                S.tt(T2[:], GG[:], GG[:], ALU.mult, GGk, [K("T2")])
                S.ts(T2[:], T2[:], 0.044715, 1.0, ALU.mult, ALU.add, [K("T2")], [K("T2")])
                S.tt(T2[:], T2[:], GG[:], ALU.mult, [K("T2")] + GGk, [K("T2")])
                S.act(T2[:], T2[:], AF.Sigmoid, [K("T2")], [K("T2")], scale=1.5957691216)
                S.tt(GG[:], GG[:], T2[:], ALU.mult, [K("T2")] + GGk, GGk)
                S.cp(XCB[:], XC[:], XCk, [K("XCB")], eng="act")
                for tc in range(NTC):
                    sl = slice(tc * TW, (tc + 1) * TW)
                    ps, pk = ps_rot()
                    S.mm(ps[:, :], wbd[:, c, :], XCB[:, sl], True, True, [K("wbd"), K("XCB")], [pk])
                    S.act(RA[:, sl], ps[:, :], AF.Sigmoid, [pk, K("colp")], [K("RA", tc)], bias=col(l, "lru_br", c))
                    ps, pk = ps_rot()
                    S.mm(ps[:, :], wbd[:, 2 + c, :], XCB[:, sl], True, True, [K("wbd"), K("XCB")], [pk])
                    S.act(IU[:, sl], ps[:, :], AF.Sigmoid, [pk, K("colp")], [K("IU", tc)], bias=col(l, "lru_bi", c))
                RAk = [K("RA", t) for t in range(NTC)]
                IUk = [K("IU", t) for t in range(NTC)]
                if c == 1:
                    dump(f"A_r{l}", RA[:], RAk, [128, S_])
                    dump(f"A_i{l}", IU[:], IUk, [128, S_])
                    dump(f"A_xcb{l}", XCB[:], [K("XCB")], [128, S_])
                    dump(f"A_xc{l}", XC[:], XCk, [128, S_])
                S.ts(XX[:], RA[:], c8[:, c:c + 1], 2.0, ALU.mult, ALU.mult, RAk + [K("c8")], [K("XX")])
                S.ts(T2[:], XX[:], 1.0 / 5040.0, 1.0 / 720.0, ALU.mult, ALU.add, [K("XX")], [K("T2")])
                for cc_ in (1.0 / 120.0, 1.0 / 24.0, 1.0 / 6.0, 0.5, 1.0):
                    S.tt(T2[:], T2[:], XX[:], ALU.mult, [K("T2"), K("XX")], [K("T2")])
                    S.ts(T2[:], T2[:], cc_, None, ALU.add, None, [K("T2")], [K("T2")])
                S.stt(T2[:], T2[:], -1.0, XX[:], ALU.mult, ALU.mult, [K("T2"), K("XX")], [K("T2")])
                S.act(T2[:], T2[:], AF.Sqrt, [K("T2")], [K("T2")])
                S.act(RA[:], XX[:], AF.Exp, [K("XX")], RAk, scale=0.5)
                S.tt(IU[:], IU[:], XC[:], ALU.mult, IUk + XCk, IUk)
                S.tt(IU[:], IU[:], T2[:], ALU.mult, IUk + [K("T2")], IUk)
                S.add("dve", lambda e: e.tensor_tensor_scan(out=XC[:], data0=RA[:], data1=IU[:], initial=0.0,
                                                            op0=ALU.mult, op1=ALU.add), RAk + IUk, XCk)
                S.tt(YG[:, c, :], XC[:], GG[:], ALU.mult, XCk + GGk, [K("YG", c, t) for t in range(NTC)])
            for nm_, t_ in (("XC", XC), ("GG", GG), ("RA", RA), ("IU", IU), ("T2", T2)):
                dump(f"A_{nm_}{l}", t_[:], [K(nm_, t) for t in range(NTC)] + [K(nm_)], [128, S_])
            dump(f"ya{l}", YG[:], [K("YG", c, t) for c in range(2) for t in range(NTC)], [128, 2, S_])
            st.close()
            S.barrier()
            group_out(l, 0, YG, sty)
        S.barrier()
        if stop_after == f"A{l}":
            done = True
            break

        with ExitStack() as sty, ExitStack() as st:
            YG = sb("YG", [128, 2, S_], stack=sty)
            QB = sb("QB", [128, 2, S_], BF16, stack=st)
            KZ = sb("KZ", [128, 4, S_], BF16, stack=st)
            S.add("dve", lambda e: e.memset(KZ[:], 0.0), [], [K("KZ", h_, t_) for h_ in range(4) for t_ in range(NTC)])
            VB = sb("VB", [128, NT, 256], BF16, stack=st)
            M01 = sb("M01", [128, 4, 512], stack=st)
            NM = sb("NM", [128, 4, 512], BF16, stack=st)
            S.dma(M01[:], cmask_d[:, 0:2048].rearrange("p (a b) -> p a b", a=4), [], [K("M01")])
            S.dma(NM[:], cmask_d[:, 2048:4096].rearrange("p (a b) -> p a b", a=4), [], [K("NM")], q="pool", arena=True)
            wv, wk, _ = load_w(w_in_d[l, :, 512:1024], 8, 512)
            wv2, wk2, _ = load_w(w_in_d[l, :, 1024:1280], 8, 256)
            for c2 in range(2):
                for tc in range(NTC):
                    sl = slice(tc * TW, (tc + 1) * TW)
                    ps, pk = ps_rot()
                    proj_fm(wv, wk, c2 * 128, tc, ps, pk)
                    S.act(QB[:, c2, sl], ps[:, :], AF.Copy, [pk], [K("QB", c2, tc)], scale=0.125)
                    ps, pk = ps_rot()
                    proj_fm(wv, wk, 256 + c2 * 128, tc, ps, pk)
                    S.cp(KZ[0:64, 2 * c2, sl], ps[0:64, :], [pk], [K("KZ", 2 * c2, tc)])
                    S.cp(KZ[64:128, 2 * c2 + 1, sl], ps[64:128, :], [pk], [K("KZ", 2 * c2 + 1, tc)], eng="act")
            for i in range(NT):
                ps, pk = ps_rot()
                for kc in range(KC):
                    S.mm(ps[:, 0:256], hT[:, kc, i * 128:(i + 1) * 128], wv2[:, kc, :], kc == 0, kc == KC - 1,
                         wk2 + [K("hT", kc, i // 4)], [pk])
                S.cp(VB[:, i, :], ps[:, 0:256], [pk], [K("VB", i)], eng=("dve" if i % 2 else "act"))
            Eb = [sb(f"Eb{j}", [128, 512], stack=st) for j in range(2)]
            SPb = [sb(f"SPb{j}", [128, 512], F32R, stack=st) for j in range(3)]
            Wb = [sb(f"Wb{j}", [128, 512], BF16, stack=st) for j in range(2)]
            Rb = [sb(f"Rb{j}", [128, 512], F32R, stack=st) for j in range(2)]
            NUr = sb("NUr", [128, 2, 128], F32R, stack=st)
            S.cp(NUr[:, 0, :], negu, [K("c128")], [K("NUr")])
            S.cp(NUr[:, 1, :], negones, [K("c128")], [K("NUr")])
            items = []
            grp = 0
            for h in range(4):
                for c in range(NTC):
                    nb = 4 * c + 4
                    for idx, b in enumerate(range(nb - 1, -1, -1)):
                        items.append(dict(h=h, c=c, idx=idx, b=b, grp=grp, last=(b == 0)))
                    grp += 1
            for i_, itm in enumerate(items):
                itm["i"] = i_
                itm["pr"] = slice((itm["h"] % 2) * 64, (itm["h"] % 2) * 64 + 64)
                itm["c2"] = itm["h"] // 2
                itm["d"] = itm["b"] - 4 * itm["c"]
                itm["ksl"] = slice(itm["b"] * 128, (itm["b"] + 1) * 128)
                itm["qsl"] = slice(itm["c"] * TW, (itm["c"] + 1) * TW)
                itm["qk"] = [K("KZ", itm["h"], itm["b"] // 4), K("QB", itm["c2"], itm["c"])]

            def sb_s1(m):
                i = m["i"]
                pr, c2 = m["pr"], m["c2"]
                psZ, pkZ = ps_rot()
                S.mm(psZ[:, :], KZ[:, m["h"], m["ksl"]], QB[:, c2, m["qsl"]], True, True, m["qk"], [pkZ])
                E, Ek = Eb[i % 2], K("Eb", i % 2)
                SP, SPk = SPb[i % 3], K("SPb", i % 3)
                S.act(E[:], psZ[:, :], AF.Exp, [pkZ], [Ek])
                S.act(SP[:], E[:], AF.Ln, [Ek, K("cst")], [SPk], bias=cst[:, 1:2])
                if m["d"] >= 0:
                    S.tt(SP[:], SP[:].bitcast(F32), M01[:, m["d"], :], ALU.mult, [SPk, K("M01")], [SPk])

            def sb_s2(m):
                i = m["i"]
                pr, c2, d = m["pr"], m["c2"], m["d"]
                SP, SPk = SPb[i % 3], K("SPb", i % 3)
                Wt, Wk_ = Wb[i % 2], K("Wb", i % 2)
                R, Rk = Rb[m["grp"] % 2], K("Rb", m["grp"] % 2)
                psA, pkA = ps_rot()
                has_r = m["idx"] > 0
                S.mm(psA[:, :], KZ[:, m["h"], m["ksl"]], QB[:, c2, m["qsl"]], True, False, m["qk"], [pkA])
                S.mm(psA[:, :], NUr[:, 0, :], SP[:], False, (not has_r) and d < 0, [SPk, K("NUr")], [pkA])
                if has_r:
                    S.mm(psA[:, :], NUr[:, 1, :], R[:], False, d < 0, [Rk, K("NUr")], [pkA])
                if d >= 0:
                    S.mm(psA[:, :], ident_b, NM[:, d, :], False, True, [K("NM"), K("c128b")], [pkA])
                S.act(Wt[:], psA[:, :], AF.Exp, [pkA], [Wk_])
                if not m["last"]:
                    if m["idx"] == 0:
                        S.cp(R[:], SP[:].bitcast(F32), [SPk], [Rk])
                    else:
                        S.tt(R[:], R[:].bitcast(F32), SP[:].bitcast(F32), ALU.add, [Rk, SPk], [Rk])

            def sb_s3(m):
                i = m["i"]
                pr, c2, h = m["pr"], m["c2"], m["h"]
                Wt, Wk_ = Wb[i % 2], K("Wb", i % 2)
                psO, pkO = PS[4 + m["grp"] % 2], K("psO", m["grp"] % 2)
                S.mm(psO[:, :], VB[:, m["b"], c2 * 128:(c2 + 1) * 128], Wt[:], m["idx"] == 0, m["last"],
                     [K("VB", m["b"]), Wk_], [pkO])
                if m["last"]:
                    S.cp(YG[pr, c2, m["qsl"]], psO[pr, :], [pkO], [K("YG", c2, m["c"])], eng="act")

            n_it = len(items)
            for r in range(-2, n_it):
                if 0 <= r + 2 < n_it:
                    sb_s1(items[r + 2])
                if 0 <= r + 1 < n_it:
                    sb_s2(items[r + 1])
                if 0 <= r < n_it:
                    sb_s3(items[r])
            dump(f"yb{l}", YG[:], [K("YG", c, t) for c in range(2) for t in range(NTC)], [128, 2, S_])
            st.close()
            S.barrier()
            group_out(l, 1, YG, sty)
        S.barrier()
        if stop_after == f"B{l}":
            done = True
            break

        with ExitStack() as sty, ExitStack() as st:
            YG = sb("YG", [128, 2, S_], stack=sty)
            XS = sb("XS", [128, 2, S_], stack=st)
            BTb = sb("BTb", [128, 2, S_], BF16, stack=st)
            CTb = sb("CTb", [128, 2, S_], BF16, stack=st)
            BTM = sb("BTM", [128, NT, 256], BF16, stack=st)
            XTM = sb("XTM", [128, NT, 256], BF16, stack=st)
            wdt = sb("wdt", [128, KC, 4], BF16, stack=st)
            stc = ExitStack()
            xw = [sb(f"xwc{j}", [128, 515], stack=stc) for j in range(2)]
            CV = [sb(f"CV{j}", [128, 512], stack=stc) for j in range(2)]
            BF = [sb(f"BF{j}", [128, 512], stack=stc) for j in range(2)]
            S.dma(wdt[:], w_in_d[l, :, 2304:2308].rearrange("(kc p) n -> p kc n", p=128), [], [K("wdt")], q="pool", arena=True)
            wv1, wk1, _ = load_w(w_in_d[l, :, 1280:1792], 8, 512)
            wv2, wk2, _ = load_w(w_in_d[l, :, 1792:2304], 8, 512)
            n_cv = 0
            for j in range(6):
                if j < 2:
                    wv, wk, c0 = wv1, wk1, 256 + j * 128
                elif j < 4:
                    wv, wk, c0 = wv2, wk2, (j - 2) * 128
                else:
                    wv, wk, c0 = wv2, wk2, 256 + (j - 4) * 128
                for tc in range(NTC):
                    sl = slice(tc * TW, (tc + 1) * TW)
                    ps, pk = ps_rot()
                    proj_fm(wv, wk, c0, tc, ps, pk)
                    cv, cvk = CV[n_cv % 2], K("CV", n_cv % 2)
                    conv_chunk(ps, pk, tc, xw, "xwc", lambda k: col(l, "ssm_cw", j * 4 + k), col(l, "ssm_cb", j),
                               cv[:], cvk)
                    if j < 2:
                        S.act(XS[:, j, sl], cv[:], AF.Silu, [cvk], [K("XS", j, tc)])
                        ps, pk = ps_rot()
                        for q in range(4):
                            S.tr(ps[:, q * 128:(q + 1) * 128], XS[:, j, tc * TW + q * 128: tc * TW + (q + 1) * 128],
                                 ident, [K("XS", j, tc), K("c128")], [pk])
                        S.cp(XTM[:, tc * 4:tc * 4 + 4, j * 128:(j + 1) * 128],
                             ps[:].rearrange("p (a b) -> p a b", a=4), [pk], [K("XTM", tc, j)])
                    elif j < 4:
                        g = j - 2
                        bf, bfk = BF[n_cv % 2], K("BF", n_cv % 2)
                        S.act(bf[:], cv[:], AF.Silu, [cvk], [bfk])
                        S.cp(BTb[:, g, sl], bf[:], [bfk], [K("BTb", g, tc)])
                        ps, pk = ps_rot()
                        for q in range(4):
                            S.tr(ps[:, q * 128:(q + 1) * 128], bf[:, q * 128:(q + 1) * 128], ident,
                                 [bfk, K("c128")], [pk])
                        S.cp(BTM[:, tc * 4:tc * 4 + 4, g * 128:(g + 1) * 128],
                             ps[:].rearrange("p (a b) -> p a b", a=4), [pk], [K("BTM", tc, g)])
                    else:
                        S.act(CTb[:, j - 4, sl], cv[:], AF.Silu, [cvk], [K("CTb", j - 4, tc)])
                    n_cv += 1
            stc.close()
            S.barrier()
            DT = sb("DT", [128, 64], stack=st)
            AN = sb("AN", [128, 64], stack=st)
            AC = sb("AC", [128, 64], stack=st)
            ACS = sb("ACS", [128, 64], stack=st)
            TOT = sb("TOT", [128, 64], stack=st)
            DEC = sb("DEC", [128, 64], stack=st)
            ETOT = sb("ETOT", [128, 64], stack=st)
            DTD = sb("DTD", [128, 64], stack=st)
            psd, pkd = ps_rot()
            for i in range(NT):
                for kc in range(KC):
                    S.mm(psd[:, i * 4:(i + 1) * 4], hT[:, kc, i * 128:(i + 1) * 128], wdt[:, kc, :], kc == 0,
                         kc == KC - 1, [K("wdt"), K("hT", kc, i // 4)], [pkd])
            S.tt(DT[:], psd[:, 0:64], row(l, "dt_bias"), ALU.add, [pkd, K("rowp")], [K("DT")])
            S.act(DT[:], DT[:], AF.Exp, [K("DT")], [K("DT")])
            S.act(DT[:], DT[:], AF.Ln, [K("DT"), K("cst")], [K("DT")], bias=cst[:, 1:2])
            S.act(AN[:], row(l, "a_log"), AF.Exp, [K("rowp")], [K("AN")])
            S.ts(AN[:], AN[:], -1.0, None, ALU.mult, None, [K("AN")], [K("AN")])
            S.tt(AC[:], DT[:], AN[:], ALU.mult, [K("DT"), K("AN")], [K("AC")])
            ps, pk = ps_rot()
            S.mm(ps[:, 0:64], tri_le, AC[:], True, True, [K("AC"), K("c128")], [pk])
            S.cp(ACS[:], ps[:, 0:64], [pk], [K("ACS")])
            ps, pk = ps_rot()
            S.mm(ps[:, 0:64], ones_f, AC[:], True, True, [K("AC"), K("c128")], [pk])
            S.cp(TOT[:], ps[:, 0:64], [pk], [K("TOT")])
            S.tt(DEC[:], TOT[:], ACS[:], ALU.subtract, [K("TOT"), K("ACS")], [K("DEC")])
            S.act(DEC[:], DEC[:], AF.Exp, [K("DEC")], [K("DEC")])
            S.act(ETOT[:], TOT[:], AF.Exp, [K("TOT")], [K("ETOT")])
            S.tt(DTD[:], DT[:], DEC[:], ALU.mult, [K("DT"), K("DEC")], [K("DTD")])
            ST = sb("ST", [128, 4, 64], stack=st)
            S.add("dve", lambda e: e.memset(ST[:], 0.0), [], [K("ST", h) for h in range(4)])
            Rm = [sb(f"Rm{j}", [128, 128], stack=st) for j in range(2)]
            ARG = [sb(f"ARG{j}", [128, 128], stack=st) for j in range(2)]
            E1 = [sb(f"E1{j}", [128, 128], stack=st) for j in range(2)]
            GL = [sb(f"GL{j}", [128, 128], BF16, stack=st) for j in range(2)]
            CTp = [sb(f"CTp{j}", [128, 128], BF16, stack=st) for j in range(2)]
            XCp = [[sb(f"XCp{a}{j}", [128, 128], BF16, stack=st) for j in range(2)] for a in range(2)]
            STp = sb("STp", [128, 4, 128], BF16, stack=st)
            for a in range(2):
                for j in range(2):
                    S.add("dve", (lambda e, t_=XCp[a][j]: e.memset(t_[:], 0.0)), [], [K("XCp", a, j)])
            S.add("dve", lambda e: e.memset(STp[:], 0.0), [], [K("STp", h) for h in range(4)])
            XDh = [sb(f"XDh{j}", [128, 64], BF16, stack=st) for j in range(2)]
            n = 0
            for ci in range(NT):
                csl = slice(ci * 128, (ci + 1) * 128)
                tcx = ci // 4
                for g in range(2):
                    psG, pkG = ps_rot()
                    S.mm(psG[:, 0:128], BTb[:, g, csl], CTb[:, g, csl], True, True,
                         [K("BTb", g, tcx), K("CTb", g, tcx)], [pkG])
                    psY, pkY = PS[4 + (ci * 2 + g) % 2], K("psO", (ci * 2 + g) % 2)
                    for hh in range(2):
                        h = 2 * g + hh
                        pr = slice(hh * 64, hh * 64 + 64)
                        cidx = ci * 4 + h
                        k2 = n % 2
                        S.ts(Rm[k2][:], tri_le, AC[:, cidx:cidx + 1], None, ALU.mult, None,
                             [K("AC"), K("c128")], [K("Rm", k2)])
                        ps1, pk1 = ps_rot()
                        S.mm(ps1[:, 0:128], ones_f, Rm[k2][:], True, True, [K("Rm", k2), K("c128")], [pk1])
                        S.stt(ARG[k2][:], ps1[:, 0:128], ACS[:, cidx:cidx + 1], ssdneg, ALU.subtract, ALU.add,
                              [pk1, K("ACS"), K("c128")], [K("ARG", k2)])
                        S.act(ARG[k2][:], ARG[k2][:], AF.Exp, [K("ARG", k2)], [K("ARG", k2)])
                        S.tt(GL[k2][:], ARG[k2][:], psG[:, 0:128], ALU.mult, [K("ARG", k2), pkG], [K("GL", k2)])
                        S.act(E1[k2][:], ps1[:, 0:128], AF.Exp, [pk1], [K("E1", k2)])
                        S.tt(CTp[k2][:], CTb[:, g, csl], E1[k2][:], ALU.mult, [K("CTb", g, tcx), K("E1", k2)],
                             [K("CTp", k2)])
                        rot = (ci * 2 + g) % 2
                        xcp = XCp[hh][rot]
                        S.ts(xcp[:, hh * 64:(hh + 1) * 64], XTM[:, ci, h * 64:(h + 1) * 64], DT[:, cidx:cidx + 1], None,
                             ALU.mult, None, [K("XTM", tcx, g), K("DT")], [K("XCp", hh, rot)])
                        S.mm(psY[:, 0:128], xcp[:], GL[k2][:], hh == 0, False, [K("XCp", hh, rot), K("GL", k2)], [pkY])
                        S.mm(psY[:, 0:128], STp[:, h, :], CTp[k2][:], False, hh == 1, [K("STp", h), K("CTp", k2)], [pkY])
                        S.ts(XDh[k2][:], XTM[:, ci, h * 64:(h + 1) * 64], DTD[:, cidx:cidx + 1], None, ALU.mult, None,
                             [K("XTM", tcx, g), K("DTD")], [K("XDh", k2)])
                        psS, pkS = ps_rot()
                        S.mm(psS[:, 0:64], BTM[:, ci, g * 128:(g + 1) * 128], XDh[k2][:], True, True,
                             [K("BTM", tcx, g), K("XDh", k2)], [pkS])
                        S.stt(ST[:, h, :], ST[:, h, :], ETOT[:, cidx:cidx + 1], psS[:, 0:64], ALU.mult, ALU.add,
                              [K("ST", h), K("ETOT"), pkS], [K("ST", h)])
                        S.cp(STp[:, h, hh * 64:(hh + 1) * 64], ST[:, h, :], [K("ST", h)], [K("STp", h)], eng="act")
                        n += 1
                    S.cp(YG[:, g, csl], psY[:, 0:128], [pkY], [K("YG", g, tcx)], eng="act")
            ARG = [sb(f"ZT{j}", [128, 512], stack=st) for j in range(1)]
            for j in range(2):
                YGk = [K("YG", j, t) for t in range(NTC)]
                S.stt(YG[:, j, :], XS[:, j, :], col(l, "ssm_d", j), YG[:, j, :], ALU.mult, ALU.add,
                      [K("XS", j, t) for t in range(NTC)] + YGk + [K("colp")], YGk)
                for tc in range(NTC):
                    sl = slice(tc * TW, (tc + 1) * TW)
                    ps, pk = ps_rot()
                    proj_fm(wv1, wk1, j * 128, tc, ps, pk)
                    zt, ztk = ARG[0], K("ARGz", 0)
                    S.act(zt[:], ps[:, :], AF.Silu, [pk], [ztk])
                    S.tt(YG[:, j, sl], YG[:, j, sl], zt[:], ALU.mult, [K("YG", j, tc), ztk], [K("YG", j, tc)])
            dump(f"yc{l}", YG[:], [K("YG", c, t) for c in range(2) for t in range(NTC)], [128, 2, S_])
            st.close()
            S.barrier()
            group_out(l, 2, YG, sty)
        S.barrier()
        if stop_after == f"C{l}":
            done = True
            break

        with ExitStack() as sty, ExitStack() as st:
            YG = sb("YG", [128, 2, S_], stack=sty)
            QA = sb("QA", [128, 4, S_], BF16, stack=st)
            KA = sb("KA", [128, 4, S_], BF16, stack=st)
            allqa = [K("QA", h_, t_) for h_ in range(4) for t_ in range(NTC)]
            allka = [K("KA", h_, t_) for h_ in range(4) for t_ in range(NTC)]
            S.add("dve", lambda e: e.memset(QA[:], 0.0), [], allqa)
            S.add("dve", lambda e: e.memset(KA[:], 0.0), [], allka)
            for h_ in range(4):
                o_ = 64 if h_ % 2 == 0 else 0
                S.dma(KA[o_:o_ + 8, h_, :], blkoh_d[:, :], allka, [K("KAoh", h_)], q="pool", arena=True)
            VB = sb("VB", [128, NT, 256], BF16, stack=st)
            CAUS = sb("CAUS", [128, 4, 512], BF16, stack=st)
            S.dma(CAUS[:], cmask_d[:, 4096:6144].rearrange("p (a b) -> p a b", a=4), [], [K("CAUS")], q="pool", arena=True)
            GC = sb("GC", [128, 3, 128], stack=st)
            S.dma(GC[:], gate_c_d[:, :].rearrange("p (a b) -> p a b", a=3), [], [K("GC")])
            KM = sb("KM", [128, 2, 8], stack=st)
            KMb = sb("KMb", [128, 2, 8], BF16, stack=st)
            wv, wk, _ = load_w(w_in_d[l, :, 2308:2820], 8, 512)
            wv2, wk2, _ = load_w(w_in_d[l, :, 2820:3076], 8, 256)
            str_ = ExitStack()
            ROPEb = [sb(f"ROPE{j}", [128, 2, TW], stack=str_) for j in range(1)]
            RAW = [sb(f"RAW{j}", [128, 512], stack=str_) for j in range(2)]
            TMP = [sb(f"TMPr{j}", [128, 512], stack=str_) for j in range(2)]
            KF = [sb(f"KF{j}", [128, 512], stack=str_) for j in range(2)]
            nr = 0
            for c2 in range(2):
                for tc in range(NTC):
                    sl = slice(tc * TW, (tc + 1) * TW)
                    rj = 0
                    ROPE = ROPEb[rj]
                    rsl = slice(0, TW)
                    S.dma(ROPE[:, 0, :], rope_d[:, tc * TW:(tc + 1) * TW], [], [K("ROPE", rj, 0)])
                    S.dma(ROPE[:, 1, :], rope_d[:, S_ + tc * TW:S_ + (tc + 1) * TW], [], [K("ROPE", rj, 1)])
                    for which in range(2):
                        ps, pk = ps_rot()
                        proj_fm(wv, wk, which * 256 + c2 * 128, tc, ps, pk)
                        raw, rawk = RAW[nr % 2], K("RAW", nr % 2)
                        tmp, tmpk = TMP[nr % 2], K("TMPr", nr % 2)
                        S.cp(raw[:], ps[:, :], [pk], [rawk], eng="act")
                        psw, pkw = ps_rot()
                        S.mm(psw[:, :], perm_f, raw[:], True, True, [rawk, K("c128")], [pkw])
                        S.tt(tmp[:], psw[:, :], ROPE[:, 1, rsl], ALU.mult, [pkw, K("ROPE", rj, 1)], [tmpk])
                        kf = KF[nr % 2]
                        dst, dk = kf[:], K("KF", nr % 2)
                        S.tt(dst, raw[:], ROPE[:, 0, rsl], ALU.mult, [rawk, K("ROPE", rj, 0)], [dk])
                        S.tt(dst, dst, tmp[:], ALU.add, [dk, tmpk], [dk])
                        if which == 0:
                            S.act(QA[0:64, 2 * c2, sl], kf[0:64, :], AF.Copy, [dk], [K("QA", 2 * c2, tc)], scale=0.125)
                            S.act(QA[64:128, 2 * c2 + 1, sl], kf[64:128, :], AF.Copy, [dk], [K("QA", 2 * c2 + 1, tc)],
                                  scale=0.125)
                        else:
                            S.cp(KA[0:64, 2 * c2, sl], kf[0:64, :], [dk], [K("KA", 2 * c2, tc)], eng="act")
                            S.cp(KA[64:128, 2 * c2 + 1, sl], kf[64:128, :], [dk], [K("KA", 2 * c2 + 1, tc)], eng="act")
                            for hb in range(2):
                                nblk = tc * 2 + hb
                                S.add("dve", (lambda e, o=KM[:, c2, nblk:nblk + 1], i_=kf[:, hb * 256:(hb + 1) * 256]:
                                              e.reduce_sum(out=o, in_=i_, axis=AX.X)), [dk], [K("KM", c2, nblk)])
                        nr += 1
            for i in range(NT):
                ps, pk = ps_rot()
                for kc in range(KC):
                    S.mm(ps[:, 0:256], hT[:, kc, i * 128:(i + 1) * 128], wv2[:, kc, :], kc == 0, kc == KC - 1,
                         wk2 + [K("hT", kc, i // 4)], [pk])
                S.cp(VB[:, i, :], ps[:, 0:256], [pk], [K("VB", i)], eng=("dve" if i % 2 else "act"))
            S.cp(KMb[:], KM[:], [K("KM", c2_, nb_) for c2_ in range(2) for nb_ in range(8)], [K("KMb")])
            str_.close()
            S.barrier()
            PTb = [sb(f"PTb{j}", [128, 512], BF16, stack=st) for j in range(3)]
            RD = [sb(f"RD{j}", [128, 512], stack=st) for j in range(2)]
            GT = sb("GT", [128, 128], stack=st)
            BT_ = sb("BIASTM", [128, 128], stack=st)
            MX = [sb(f"MX{j}", [128, 8], stack=st) for j in range(2)]
            for h in range(4):
                hh, c2 = h % 2, h // 2
                pr = slice(hh * 64, hh * 64 + 64)
                o_ = 64 if hh == 0 else 0
                psg, pkg = ps_rot()
                for i in range(NT):
                    S.mm(psg[:, i * 8:(i + 1) * 8], QA[pr, h, i * 128:(i + 1) * 128], KMb[pr, c2, :], True, True,
                         [K("QA", h, i // 4), K("KMb")], [pkg])
                S.stt(GT[:], psg[:, 0:128], 8.0 / 256.0, GC[:, 0, :], ALU.mult, ALU.add, [pkg, K("GC")], [K("GT")])
                for i in range(NT):
                    mx, mxk = MX[i % 2], K("MX", i % 2)
                    S.add("dve", (lambda e, o=mx[:], i_=GT[:, i * 8:(i + 1) * 8]: e.max(out=o, in_=i_)),
                          [K("GT")], [mxk])
                    bsl = BT_[:, i * 8:(i + 1) * 8]
                    S.ts(bsl, GT[:, i * 8:(i + 1) * 8], mx[:, 2:3], None, ALU.is_ge, None, [K("GT"), mxk],
                         [K("BIASTM", i)])
                    S.tt(bsl, bsl, GC[:, 1, i * 8:(i + 1) * 8], ALU.mult, [K("BIASTM", i), K("GC")], [K("BIASTM", i)])
                    S.tt(bsl, bsl, GC[:, 2, i * 8:(i + 1) * 8], ALU.add, [K("BIASTM", i), K("GC")], [K("BIASTM", i)])
                    S.ts(bsl, bsl, -1.0, -NEG, ALU.add, ALU.mult, [K("BIASTM", i)], [K("BIASTM", i)])
                for q4 in range(4):
                    pst, pkt = ps_rot()
                    for q in range(4):
                        i = q4 * 4 + q
                        S.tr(pst[0:8, q * 128:(q + 1) * 128], BT_[:, i * 8:(i + 1) * 8], ident,
                             [K("BIASTM", i), K("c128")], [pkt])
                    S.cp(QA[o_:o_ + 8, h, q4 * 512:(q4 + 1) * 512], pst[0:8, :], [pkt], [K("QAsel", h, q4)])
            its = []
            grp = 0
            for h in range(4):
                for c in range(NTC):
                    nkt = 4 * c + 4
                    for kt in range(nkt):
                        its.append(dict(h=h, c=c, kt=kt, nkt=nkt, grp=grp, i=len(its)))
                    grp += 1

            def m_s1(m):
                h, c, kt = m["h"], m["c"], m["kt"]
                d = kt - 4 * c
                qsl = slice(c * TW, (c + 1) * TW)
                ksl = slice(kt * 128, (kt + 1) * 128)
                psS, pkS = ps_rot()
                S.mm(psS[:, :], KA[:, h, ksl], QA[:, h, qsl], True, d < 0,
                     [K("KA", h, kt // 4), K("KAoh", h), K("QA", h, c), K("QAsel", h, c)], [pkS])
                if d >= 0:
                    S.mm(psS[:, :], ident_b, CAUS[:, d, :], False, True, [K("CAUS"), K("c128b")], [pkS])
                pt, ptk = PTb[m["i"] % 3], K("PTb", m["i"] % 3)
                S.act(pt[:], psS[:, :], AF.Exp, [pkS], [ptk])

            def m_s2(m):
                h, c, kt, nkt = m["h"], m["c"], m["kt"], m["nkt"]
                hh, c2 = h % 2, h // 2
                pr = slice(hh * 64, hh * 64 + 64)
                qsl = slice(c * TW, (c + 1) * TW)
                g2 = m["grp"] % 2
                psO, pkO = PS[4 + g2], K("psO", g2)
                psD, pkD = PS[6 + g2], K("psD", g2)
                pt, ptk = PTb[m["i"] % 3], K("PTb", m["i"] % 3)
                S.mm(psO[:, :], VB[:, kt, c2 * 128:(c2 + 1) * 128], pt[:], kt == 0, kt == nkt - 1,
                     [K("VB", kt), ptk], [pkO])
                S.mm(psD[:, :], ones_b, pt[:], kt == 0, kt == nkt - 1, [ptk, K("c128b")], [pkD])
                if kt == nkt - 1:
                    rd, rdk = RD[g2], K("RD", g2)
                    S.add("dve", (lambda e, o=rd[pr, :], i_=psD[pr, :]: e.reciprocal(out=o, in_=i_)), [pkD], [rdk])
                    S.tt(YG[pr, c2, qsl], psO[pr, :], rd[pr, :], ALU.mult, [pkO, rdk], [K("YG", c2, c)])

            n_ = len(its)
            for r in range(-1, n_):
                if r + 1 < n_:
                    m_s1(its[r + 1])
                if r >= 0:
                    m_s2(its[r])
            dump(f"yd{l}", YG[:], [K("YG", c, t) for c in range(2) for t in range(NTC)], [128, 2, S_])
            st.close()
            S.barrier()
            group_out(l, 3, YG, sty)
        S.barrier()
        dump(f"x1_{l}", xT[:], ALLX, [128, KC, S_])
        if stop_after == f"D{l}":
            done = True
            break

        with ExitStack() as st:
            with ExitStack() as st2:
                rmsnorm_fm(xT, "xT", lambda kc: col(l, "xat_g", kc), hT, "hT", KC, S_, st2, tag="x")
            S.barrier()
            memT = sb("memT", [128, KC, 256], stack=st)
            MTb = sb("MTb", [128, KC, 256], BF16, stack=st)
            with ExitStack() as st2:
                load_fm(memT, mem_d, 2, "memT", st2)
                rmsnorm_fm(memT, "memT", lambda kc: col(l, "mem_g", kc), MTb, "MTb", KC, 256, st2, tag="m")
            S.barrier()
            KTb = sb("KTb", [128, KC, 256], BF16, stack=st)
            VX = sb("VX", [128, 2, D_], BF16, stack=st)
            QX = sb("QX", [128, KC, S_], BF16, stack=st)
            MTk = [K("MTb", kc, 0) for kc in range(KC)]
            for half in range(2):
                wv, wk, _ = load_w(wkv_d[l, :, half * 512:(half + 1) * 512], 8, 512)
                for o4 in range(4):
                    oc = half * 4 + o4
                    ps, pk = ps_rot()
                    for kc in range(KC):
                        S.mm(ps[:, 0:256], wv[:, kc, o4 * 128:(o4 + 1) * 128], MTb[:, kc, :], kc == 0, kc == KC - 1,
                             wk + [K("MTb", kc, 0)], [pk])
                    S.cp(KTb[:, oc, :], ps[:, 0:256], [pk], [K("KTb", oc)])
            for half in range(2):
                wv, wk, _ = load_w(wkv_d[l, :, 1024 + half * 512:1024 + (half + 1) * 512], 8, 512)
                for mt in range(2):
                    ps, pk = ps_rot()
                    for kc in range(KC):
                        S.mm(ps[:, :], MTb[:, kc, mt * 128:(mt + 1) * 128], wv[:, kc, :], kc == 0, kc == KC - 1,
                             wk + [K("MTb", kc, 0)], [pk])
                    S.cp(VX[:, mt, half * 512:(half + 1) * 512], ps[:, :], [pk], [K("VX", mt, half)], eng="act")
            for half in range(2):
                wv, wk, _ = load_w(wq_d[l, :, half * 512:(half + 1) * 512], 8, 512)
                for o4 in range(4):
                    oc = half * 4 + o4
                    for tc in range(NTC):
                        ps, pk = ps_rot()
                        proj_fm(wv, wk, o4 * 128, tc, ps, pk)
                        S.act(QX[:, oc, tc * TW:(tc + 1) * TW], ps[:, :], AF.Copy, [pk], [K("QX", oc, tc)],
                              scale=1.0 / 16.0)
            dump(f"X_MTb{l}", MTb[:], MTk, [128, KC, 256])
            dump(f"X_KTb{l}", KTb[:], [K("KTb", oc) for oc in range(KC)], [128, KC, 256])
            dump(f"X_QX{l}", QX[:], [K("QX", oc, tc) for oc in range(KC) for tc in range(NTC)], [128, KC, S_])
            dump(f"X_VX{l}", VX[:], [K("VX", mt, hf) for mt in range(2) for hf in range(2)], [128, 2, D_])
            PT = [sb(f"PTx{j}", [128, 512], BF16, stack=st) for j in range(4)]
            RDx = [sb(f"RDx{j}", [128, 512], stack=st) for j in range(2)]
            it = 0
            for hd in range(4):
                for tc in range(NTC):
                    sl = slice(tc * TW, (tc + 1) * TW)
                    pts = []
                    for mt in range(2):
                        ps, pk = ps_rot()
                        for j in range(2):
                            S.mm(ps[:, :], KTb[:, 2 * hd + j, mt * 128:(mt + 1) * 128], QX[:, 2 * hd + j, sl],
                                 j == 0, j == 1, [K("KTb", 2 * hd + j), K("QX", 2 * hd + j, tc)], [pk])
                        pt, ptk = PT[it % 4], K("PTx", it % 4)
                        S.act(pt[:], ps[:, :], AF.Exp, [pk], [ptk])
                        pts.append((pt, ptk))
                        it += 1
                    psD, pkD = ps_rot()
                    for mt in range(2):
                        S.mm(psD[:, :], ones_b, pts[mt][0][:], mt == 0, mt == 1, [pts[mt][1], K("c128b")], [pkD])
                    rd, rdk = RDx[(hd * NTC + tc) % 2], K("RDx", (hd * NTC + tc) % 2)
                    S.add("dve", (lambda e, o=rd[:], i_=psD[:, :]: e.reciprocal(out=o, in_=i_)), [pkD], [rdk])
                    for j in range(2):
                        oc = 2 * hd + j
                        ps, pk = ps_rot()
                        for mt in range(2):
                            S.mm(ps[:, :], VX[:, mt, oc * 128:(oc + 1) * 128], pts[mt][0][:], mt == 0, mt == 1,
                                 [K("VX", mt, oc // 4), pts[mt][1]], [pk])
                        S.tt(hT[:, oc, sl], ps[:, :], rd[:], ALU.mult, [pk, rdk], [K("hT", oc, tc)])
            dump(f"X_OX{l}", hT[:], ALLH, [128, KC, S_])
            for half in range(2):
                wv, wk, _ = load_w(wo_d[l, :, half * 512:(half + 1) * 512], 8, 512)
                for o4 in range(4):
                    oc = half * 4 + o4
                    for tc in range(NTC):
                        ps, pk = ps_rot()
                        proj_fm(wv, wk, o4 * 128, tc, ps, pk)
                        S.tt(xT[:, oc, tc * TW:(tc + 1) * TW], xT[:, oc, tc * TW:(tc + 1) * TW], ps[:, :], ALU.add,
                             [K("xT", oc, tc), pk], [K("xT", oc, tc)])
        S.barrier()
        dump(f"x2_{l}", xT[:], ALLX, [128, KC, S_])
        if stop_after == f"X{l}":
            done = True
            break

        with ExitStack() as st:
            ENp = sb("ENp", [128, 16, 128], BF16, stack=st)
            S.add("dve", lambda e: e.memset(ENp[:], 0.0), [], [K("ENpz")])
            S.dma(ENp[0:16, :, :], en_d[:, :].rearrange("p (a b) -> p a b", a=16), [K("ENpz")], [K("ENf")], q="pool",
                  arena=True)
            CTh = sb("CTh", [128, S_], BF16, stack=st)
            CTl = sb("CTl", [128, S_], BF16, stack=st)
            S.add("dve", lambda e: e.memset(CTh[:], 0.0), [], [K("CThz")])
            S.add("dve", lambda e: e.memset(CTl[:], 0.0), [], [K("CTlz")])
            st_rt = ExitStack()
            WR = sb("WR", [128, KC, 20], stack=st_rt)
            S.dma(WR[:], wrt_d[l].rearrange("(kc p) n -> p kc n", p=128), [], [K("WR")])
            LG = sb("LG", [128, NT, 20], stack=st_rt)
            st_hf = ExitStack()
            HF = sb("HF", [128, KC, TW], stack=st_hf)

            def router_hook(tc, r):
                sl = slice(tc * TW, (tc + 1) * TW)
                for kc in range(KC):
                    S.stt(HF[:, kc, :], xT[:, kc, sl], col(l, "ffn_g", kc), r[:], ALU.mult, ALU.mult,
                          [K("xT", kc, tc), K("rsf", tc % 2), K("colp")], [K("HF", kc)])
                ps, pk = ps_rot()
                for q in range(4):
                    for kc in range(KC):
                        S.mm(ps[:, q * 20:(q + 1) * 20], HF[:, kc, q * 128:(q + 1) * 128], WR[:, kc, :], kc == 0,
                             kc == KC - 1, [K("HF", kc), K("WR")], [pk])
                S.cp(LG[:, tc * 4:tc * 4 + 4, :], ps[:, 0:80].rearrange("p (a b) -> p a b", a=4), [pk],
                     [K("LG", tc)])

            with ExitStack() as st2:
                rmsnorm_fm(xT, "xT", lambda kc: col(l, "ffn_g", kc), hT, "hT", KC, S_, st2, hook=router_hook, tag="f")
            st_hf.close()
            S.barrier()
            COMB = sb("COMB", [128, NT, 16], stack=st_rt)
            CT = sb("CT", [16, S_], stack=st_rt)
            sm = {nm: sb("rt_" + nm, [128, NT, w_] if w_ > 1 else [128, NT], stack=st_rt) for nm, w_ in
                  [("lg", 4), ("m", 1), ("eg", 4), ("sg", 1), ("oh", 4), ("le", 16), ("sel", 4), ("tmp", 4),
                   ("a", 1), ("eq", 4), ("sel2", 4), ("b", 1), ("t2", 4), ("ex", 4), ("dd", 1), ("wg", 4)]}

            def v(nm):
                return sm[nm][:]

            def kk(nm):
                return [K("rt", nm)]

            def B3(ap2, w_=4):
                return ap2.unsqueeze(2).to_broadcast([128, NT, w_])

            LGk = [K("LG", t) for t in range(NTC)]
            bgB = row(l, "bg").unsqueeze(1).to_broadcast([128, NT, 4])
            beB = row(l, "be").unsqueeze(1).to_broadcast([128, NT, 16])
            S.tt(v("lg"), LG[:, :, 0:4], bgB, ALU.add, LGk + [K("rowp")], kk("lg"))
            S.add("dve", lambda e: e.reduce_max(out=v("m"), in_=v("lg"), axis=AX.X), kk("lg"), kk("m"))
            S.tt(v("lg"), v("lg"), B3(v("m")), ALU.subtract, kk("lg") + kk("m"), kk("lg"))
            S.act(v("eg"), v("lg"), AF.Exp, kk("lg"), kk("eg"))
            S.add("dve", lambda e: e.reduce_sum(out=v("sg"), in_=v("eg"), axis=AX.X), kk("eg"), kk("sg"))
            S.add("dve", lambda e: e.reciprocal(out=v("sg"), in_=v("sg")), kk("sg"), kk("sg"))
            S.ts(v("oh"), v("lg"), 0.0, None, ALU.is_ge, None, kk("lg"), kk("oh"))
            S.tt(v("le"), LG[:, :, 4:20], beB, ALU.add, LGk + [K("rowp")], kk("le"))
            for g in range(4):
                ohg = sm["oh"][:, :, g:g + 1].to_broadcast([128, NT, 4])
                if g == 0:
                    S.tt(v("sel"), sm["le"][:, :, 0:4], ohg, ALU.mult, kk("le") + kk("oh"), kk("sel"))
                else:
                    S.tt(v("tmp"), sm["le"][:, :, 4 * g:4 * g + 4], ohg, ALU.mult, kk("le") + kk("oh"), kk("tmp"))
                    S.tt(v("sel"), v("sel"), v("tmp"), ALU.add, kk("sel") + kk("tmp"), kk("sel"))
            S.add("dve", lambda e: e.reduce_max(out=v("a"), in_=v("sel"), axis=AX.X), kk("sel"), kk("a"))
            S.tt(v("sel"), v("sel"), B3(v("a")), ALU.subtract, kk("sel") + kk("a"), kk("sel"))
            S.ts(v("eq"), v("sel"), 0.0, None, ALU.is_ge, None, kk("sel"), kk("eq"))
            S.stt(v("sel2"), v("eq"), -1e30, v("sel"), ALU.mult, ALU.add, kk("eq") + kk("sel"), kk("sel2"))
            S.add("dve", lambda e: e.reduce_max(out=v("b"), in_=v("sel2"), axis=AX.X), kk("sel2"), kk("b"))
            S.tt(v("t2"), v("sel"), B3(v("b")), ALU.is_ge, kk("sel") + kk("b"), kk("t2"))
            S.act(v("ex"), v("sel"), AF.Exp, kk("sel"), kk("ex"))
            S.act(v("dd"), v("b"), AF.Exp, kk("b"), kk("dd"))
            S.ts(v("dd"), v("dd"), 1.0, None, ALU.add, None, kk("dd"), kk("dd"))
            S.add("dve", lambda e: e.reciprocal(out=v("dd"), in_=v("dd")), kk("dd"), kk("dd"))
            S.tt(v("wg"), v("ex"), v("t2"), ALU.mult, kk("ex") + kk("t2"), kk("wg"))
            S.tt(v("wg"), v("wg"), B3(v("dd")), ALU.mult, kk("wg") + kk("dd"), kk("wg"))
            S.tt(v("wg"), v("wg"), B3(v("sg")), ALU.mult, kk("wg") + kk("sg"), kk("wg"))
            for g in range(4):
                ohg = sm["oh"][:, :, g:g + 1].to_broadcast([128, NT, 4])
                S.tt(COMB[:, :, 4 * g:4 * g + 4], v("wg"), ohg, ALU.mult, kk("wg") + kk("oh"),
                     [K("COMB", i, g) for i in range(NT)])
            for q4 in range(4):
                pst, pkt = ps_rot()
                for q in range(4):
                    i = q4 * 4 + q
                    S.tr(pst[0:16, q * 128:(q + 1) * 128], COMB[:, i, :], ident,
                         [K("COMB", i, g) for g in range(4)] + [K("c128")], [pkt])
                S.cp(CT[0:16, q4 * 512:(q4 + 1) * 512], pst[0:16, :], [pkt], [K("CT", q4)])
                S.cp(CTh[0:16, q4 * 512:(q4 + 1) * 512], CT[0:16, q4 * 512:(q4 + 1) * 512], [K("CT", q4), K("CThz")],
                     [K("CTh", q4)])
                S.tt(CT[0:16, q4 * 512:(q4 + 1) * 512], CT[0:16, q4 * 512:(q4 + 1) * 512],
                     CTh[0:16, q4 * 512:(q4 + 1) * 512], ALU.subtract, [K("CT", q4), K("CTh", q4)], [K("CT", q4)])
                S.cp(CTl[0:16, q4 * 512:(q4 + 1) * 512], CT[0:16, q4 * 512:(q4 + 1) * 512], [K("CT", q4), K("CTlz")],
                     [K("CTl", q4)])
            st_rt.close()
            S.barrier()
            HID = sb("HID", [128, 8, S_], BF16, stack=st)
            W2S = sb("W2S", [128, 8, D_], BF16, stack=st)
            CB = [sb(f"CB{j}", [128, 512], stack=st) for j in range(2)]
            SL = [sb(f"SL{j}", [128, 512], stack=st) for j in range(2)]
            n = 0
            for g in range(4):
                for el in range(4):
                    ex = 4 * g + el
                    wv, wk1_, slot = load_w(w1_d[l, ex], 8, 256, c0=0, tot=512)
                    _, wk3_, _ = load_w(w3_d[l, ex], 8, 256, slot=slot, c0=256, tot=512)
                    wk = wk1_ + wk3_
                    for tc in range(NTC):
                        sl = slice(tc * TW, (tc + 1) * TW)
                        psC, pkC = ps_rot()
                        S.mm(psC[:, :], ENp[:, ex, :], CTh[:, sl], True, False, [K("ENf"), K("CTh", tc), K("CThz")], [pkC])
                        S.mm(psC[:, :], ENp[:, ex, :], CTl[:, sl], False, True, [K("ENf"), K("CTl", tc), K("CTlz")], [pkC])
                        cb, cbk = CB[n % 2], K("CB", n % 2)
                        S.cp(cb[:], psC[:, :], [pkC], [cbk], eng="act")
                        for fc in range(2):
                            ps1, pk1 = ps_rot()
                            proj_fm(wv, wk, fc * 128, tc, ps1, pk1)
                            ps3, pk3 = ps_rot()
                            proj_fm(wv, wk, 256 + fc * 128, tc, ps3, pk3)
                            s_, sk = SL[(2 * n + fc) % 2], K("SL", (2 * n + fc) % 2)
                            S.act(s_[:], ps1[:, :], AF.Silu, [pk1], [sk])
                            S.tt(s_[:], s_[:], ps3[:, :], ALU.mult, [sk, pk3], [sk])
                            S.tt(HID[:, el * 2 + fc, sl], s_[:], cb[:], ALU.mult, [sk, cbk], [K("HID", el * 2 + fc, tc)])
                        n += 1
                    S.dma(W2S[:, 2 * el:2 * el + 2, :], w2_d[l, ex].rearrange("(fc p) n -> p fc n", p=128), [],
                          [K("W2S", el)], q="pool", arena=True)
                for oc in range(KC):
                    for tc in range(NTC):
                        ps, pk = ps_rot()
                        for j in range(8):
                            S.mm(ps[:, :], W2S[:, j, oc * 128:(oc + 1) * 128], HID[:, j, tc * TW:(tc + 1) * TW],
                                 j == 0, j == 7, [K("W2S", j // 2), K("HID", j, tc)], [pk])
                        S.tt(xT[:, oc, tc * TW:(tc + 1) * TW], xT[:, oc, tc * TW:(tc + 1) * TW], ps[:, :], ALU.add,
                             [K("xT", oc, tc), pk], [K("xT", oc, tc)])
        S.barrier()
        dump(f"x3_{l}", xT[:], ALLX, [128, KC, S_])
        if stop_after == f"M{l}":
            done = True
            break

    if stop_after is None:
        with ExitStack() as st:
            HF = sb("HFo", [128, KC, TW], stack=st)
            OB = [sb(f"OB{j}", [128, D_], stack=st) for j in range(2)]
            nob = [0]

            def final_hook(tc, r):
                sl = slice(tc * TW, (tc + 1) * TW)
                for kc in range(KC):
                    S.stt(HF[:, kc, :], xT[:, kc, sl], col(0, "fin_g", kc), r[:], ALU.mult, ALU.mult,
                          [K("xT", kc, tc), K("rsz", tc % 2), K("colp")], [K("HFo", kc)])
                for q in range(4):
                    ob, obk = OB[nob[0] % 2], K("OB", nob[0] % 2)
                    nob[0] += 1
                    for half in range(2):
                        ps, pk = ps_rot()
                        for j in range(4):
                            S.tr(ps[:, j * 128:(j + 1) * 128], HF[:, half * 4 + j, q * 128:(q + 1) * 128], ident,
                                 [K("HFo", half * 4 + j), K("c128")], [pk])
                        S.cp(ob[:, half * 512:(half + 1) * 512], ps[:, :], [pk], [obk],
                             eng=("act" if half else "dve"))
                    r0 = tc * TW + q * 128
                    i = S.dma(out_d[r0:r0 + 128, :], ob[:], [obk], [K("out", r0)])
                    S.final.append(i)

            rmsnorm_fm(xT, "xT", lambda kc: col(0, "fin_g", kc), None, None, KC, S_, st, hook=final_hook, tag="z")

    S.finalize(es)
    es.close()
    return nc, dump_d


def make_consts():
    c128 = np.zeros((128, 8, 128), np.float32)
    j = np.arange(128)[:, None]
    s = np.arange(128)[None, :]
    c128[:, 0] = np.eye(128)
    c128[:, 1] = 1.0
    c128[:, 2] = (j <= s)
    c128[:, 3] = -1.0 * (j >= s)
    c128[:, 4] = -1.0
    partner = np.where((np.arange(128) % 64) < 32, np.arange(128) + 32, np.arange(128) - 32)
    perm = np.zeros((128, 128), np.float32)
    perm[partner, np.arange(128)] = 1.0
    c128[:, 5] = perm
    c128[:, 6] = NEG * (s < j)
    cm = np.zeros((128, 12, 512), np.float32)
    p = np.arange(128)[:, None]
    f = np.arange(512)[None, :]
    for d in range(4):
        valid = (f - p) > d * 128
        cm[:, d] = valid
        cm[:, 4 + d] = NEG * (~valid)
        cm[:, 8 + d] = NEG * ((d * 128 + p) > f)
    half = 32
    inv = (np.float32(10000.0) ** (-(np.arange(half, dtype=np.float32)) / np.float32(half))).astype(np.float32)
    ang = np.arange(S_, dtype=np.float32)[None, :] * inv[:, None]
    cos = np.cos(ang).astype(np.float32)
    sin = np.sin(ang).astype(np.float32)
    rope = np.zeros((128, 2, S_), np.float32)
    for pp in range(128):
        d = pp % 64
        rope[pp, 0] = cos[d % 32]
        rope[pp, 1] = -sin[d % 32] if d < 32 else sin[d % 32]
    en = np.zeros((16, 16, 128), np.float32)
    for n in range(16):
        en[n, n, :] = 1.0
    gc = np.zeros((128, 3, 16, 8), np.float32)
    for i in range(16):
        own = i // 2
        for n in range(8):
            gc[:, 0, i, n] = 0.0 if n < own else -1e30
            gc[:, 1, i, n] = 1.0 if n < own else 0.0
            gc[:, 2, i, n] = 1.0 if n == own else 0.0
    blkoh = np.zeros((8, S_), np.float32)
    for n in range(8):
        blkoh[n, n * 256:(n + 1) * 256] = 1.0
    return dict(c128=c128.reshape(128, -1), cmask=cm.reshape(128, -1), rope=rope.reshape(128, -1),
                en=en.reshape(16, -1), gatec=gc.reshape(128, -1), blkoh=blkoh)


def colvec(v):
    v = np.asarray(v, np.float32)
    return np.ascontiguousarray(v.reshape(-1, 128).T)


def pack_params(inp):
    colp = np.zeros((128, L_, NCOL), np.float32)
    rowp = np.zeros((128, L_, NROW), np.float32)

    def put(l, name, arr):
        c0, w = COLS[name]
        assert arr.shape == (128, w), (name, arr.shape)
        colp[:, l, c0:c0 + w] = arr

    for l in range(L_):
        put(l, "mix_g", colvec(inp["mix_norm_g"][l]))
        put(l, "xat_g", colvec(inp["xattn_norm_g"][l]))
        put(l, "mem_g", colvec(inp["mem_norm_g"][l]))
        put(l, "ffn_g", colvec(inp["ffn_norm_g"][l]))
        put(l, "grp_g", colvec(inp["group_norm_g"][l]))
        cw = inp["lru_conv_w"][l]
        put(l, "lru_cw", np.concatenate([colvec(cw[k]) for k in range(4)], axis=1).reshape(128, 4, 2)
            .transpose(0, 2, 1).reshape(128, 8))
        put(l, "lru_cb", colvec(inp["lru_conv_b"][l]))
        put(l, "lru_br", colvec(inp["lru_br"][l]))
        put(l, "lru_bi", colvec(inp["lru_bi"][l]))
        put(l, "lru_lam", colvec(inp["lru_lambda"][l]))
        sw = inp["ssm_conv_w"][l]
        put(l, "ssm_cw", np.concatenate([colvec(sw[k]) for k in range(4)], axis=1).reshape(128, 4, 6)
            .transpose(0, 2, 1).reshape(128, 24))
        put(l, "ssm_cb", colvec(inp["ssm_conv_b"][l]))
        put(l, "ssm_d", colvec(np.repeat(inp["ssm_d"][l], 64)))
        put(l, "fin_g", colvec(inp["final_norm_g"]))
        r0, w = ROWS["dt_bias"]
        rowp[:, l, r0:r0 + w] = np.tile(inp["ssm_dt_bias"][l], 16)[None, :]
        r0, w = ROWS["a_log"]
        rowp[:, l, r0:r0 + w] = np.tile(inp["ssm_a_log"][l], 16)[None, :]
        r0, w = ROWS["bg"]
        rowp[:, l, r0:r0 + w] = inp["router_group_b"][l][None, :]
        r0, w = ROWS["be"]
        rowp[:, l, r0:r0 + w] = inp["router_expert_b"][l][None, :]
    wbd = np.zeros((L_, 2, 2, 128, 128), np.float32)
    for l in range(L_):
        for gi, nm in enumerate(("lru_wr", "lru_wi")):
            for c in range(2):
                for hh in range(2):
                    wbd[l, gi, c, hh * 64:(hh + 1) * 64, hh * 64:(hh + 1) * 64] = inp[nm][l, 2 * c + hh]
    router_w = np.concatenate([inp["router_group_w"], inp["router_expert_w"]], axis=2)
    return dict(colp=colp.reshape(128, -1), rowp=rowp.reshape(128, -1), lru_wbd=wbd,
                router_w=np.ascontiguousarray(router_w))


SHARED = ["w_in", "w_out", "xattn_wq", "xattn_wkv", "xattn_wo", "expert_w1", "expert_w3", "expert_w2"]


def make_in_maps(inputs, cores):
    inp = {k: np.asarray(v) for k, v in inputs.items()}
    consts = make_consts()
    packed = pack_params(inp)
    shared = {k: np.ascontiguousarray(inp[k], dtype=np.float32) for k in SHARED}
    maps = []
    for b in cores:
        m = dict(shared)
        m.update(consts)
        m.update(packed)
        m["x"] = np.ascontiguousarray(inp["x"][b])
        m["mem"] = np.ascontiguousarray(inp["mem"][b])
        maps.append(m)
    return maps


_CACHE = {}


def kernel(**inputs):
    if "nc" not in _CACHE:
        _CACHE["nc"] = build_program()[0]
    nc = _CACHE["nc"]
    maps = make_in_maps(inputs, list(range(8)))
    res = run_bass_kernel_spmd(nc, maps, core_ids=list(range(8)))
    return np.stack([r["out"] for r in res.results], axis=0).astype(np.float32)
```

```python
import numpy as np
from contextlib import ExitStack
import concourse.bass as bass
import concourse.mybir as mybir
from concourse.bass_utils import run_bass_kernel_spmd

F32 = mybir.dt.float32
BF16 = mybir.dt.bfloat16
F32R = mybir.dt.float32r
AF = mybir.ActivationFunctionType
ALU = mybir.AluOpType
AX = mybir.AxisListType

S_ = 2048
D_ = 1024
L_ = 2
KC = 8
NTC = 4
TW = 512
NT = 16
NEG = -30000.0
EPS = 1e-6

COLS = {}
_c = 0
for _n, _w in [("mix_g", 8), ("xat_g", 8), ("mem_g", 8), ("ffn_g", 8), ("grp_g", 8),
               ("lru_cw", 8), ("lru_cb", 2), ("lru_br", 2), ("lru_bi", 2), ("lru_lam", 2),
               ("ssm_cw", 24), ("ssm_cb", 6), ("ssm_d", 2), ("fin_g", 8)]:
    COLS[_n] = (_c, _w)
    _c += _w
NCOL = _c
ROWS = {}
_c = 0
for _n, _w in [("dt_bias", 64), ("a_log", 64), ("bg", 4), ("be", 16)]:
    ROWS[_n] = (_c, _w)
    _c += _w
NROW = _c


def _free(ap):
    n = 1
    for d in ap.shape[1:]:
        n *= int(d)
    return n


class Sched:
    ENG = ("pe", "act", "dve", "pool", "sp")
    GATED = ("pe", "act", "dve", "sp")

    def __init__(self, nc):
        self.nc = nc
        self.ops = []
        self.lastw = {}
        self.rd = {}
        self.by_eng = {e: [] for e in self.ENG}
        self.final = []
        self.psn = 0
        self.seg = 0

    def add(self, eng, fn, reads=(), writes=(), dma=False, cost=0.3, arena=False):
        i = len(self.ops)
        reads = list(reads)
        writes = list(writes)
        deps = {}
        for r in reads:
            w = self.lastw.get(r)
            if w is not None:
                deps.setdefault(w, set()).add("raw")
        for w_ in writes:
            w = self.lastw.get(w_)
            if w is not None:
                deps.setdefault(w, set()).add("waw")
            for r in self.rd.get(w_, ()):
                deps.setdefault(r, set()).add("war")
        ws = set(writes)
        for r in reads:
            if r not in ws:
                self.rd.setdefault(r, []).append(i)
        for w_ in writes:
            self.lastw[w_] = i
            self.rd[w_] = []
        deps.pop(i, None)
        fdeps = set()
        for d, kinds in deps.items():
            od = self.ops[d]
            if od["eng"] == eng and not od["dma"] and not dma:
                if eng == "pe":
                    continue
                if "raw" not in kinds:
                    continue
            fdeps.add(d)
        self.ops.append(dict(eng=eng, fn=fn, deps=fdeps, order=set(deps.keys()), dma=dma, signal=dma, token=None,
                             seg=self.seg, cost=cost, arena=arena))
        self.by_eng[eng].append(i)
        return i

    def barrier(self):
        self.seg += 1

    def schedule(self, W=24):
        ops = self.ops
        n = len(ops)
        succ = [[] for _ in range(n)]
        nun = [0] * n
        for i, op in enumerate(ops):
            nun[i] = len(op["order"])
            for d in op["order"]:
                succ[d].append(i)
        ready = [0.0] * n
        fin = [0.0] * n
        done = [False] * n
        head = {e: 0 for e in self.ENG}
        free = {e: 0.0 for e in self.ENG}
        lists = {e: list(self.by_eng[e]) for e in self.ENG}
        new_order = {e: [] for e in self.ENG}
        nseg = self.seg + 1
        rem = [0] * (nseg + 1)
        for i, op in enumerate(ops):
            if op["eng"] in self.GATED:
                rem[op["seg"]] += 1
        import os
        WDEF = {"pe": W, "act": 1, "dve": W}
        WE = {e_: int(os.environ.get("KSCHED_W_" + e_.upper(), WDEF[e_])) for e_ in ("pe", "act", "dve")}
        cur = 0
        seg_t0 = 0.0
        tmax = 0.0
        nsched = 0
        while nsched < n:
            while cur < nseg and rem[cur] == 0:
                cur += 1
                seg_t0 = tmax
            best = None
            for e in self.ENG:
                lst = lists[e]
                h = head[e]
                while h < len(lst) and done[lst[h]]:
                    h += 1
                head[e] = h
                w = WE.get(e, 1)
                seen = 0
                j = h
                while j < len(lst) and seen < w:
                    i = lst[j]
                    j += 1
                    if done[i]:
                        continue
                    op = ops[i]
                    if e != "pool":
                        if op["seg"] != cur:
                            break
                    elif op["arena"] and op["seg"] > cur:
                        break
                    seen += 1
                    if nun[i] == 0:
                        est = max(free[e], ready[i])
                        if e != "pool" or op["arena"]:
                            est = max(est, seg_t0)
                        key = (est, i)
                        if best is None or key < best[0]:
                            best = (key, e, i)
            if best is None:
                raise RuntimeError("scheduler stuck at seg %d" % cur)
            (est, _), e, i = best
            op = ops[i]
            if op["dma"]:
                free[e] = est + 0.06
                f = est + op["cost"]
            else:
                f = est + op["cost"]
                free[e] = f
            fin[i] = f
            tmax = max(tmax, f)
            done[i] = True
            nsched += 1
            new_order[e].append(i)
            if e in self.GATED:
                rem[op["seg"]] -= 1
            for s_ in succ[i]:
                nun[s_] -= 1
                lat = 0.0 if (ops[s_]["eng"] == e and not op["dma"]) else 0.1
                if f + lat > ready[s_]:
                    ready[s_] = f + lat
        self.by_eng = new_order
        self.sim_time = tmax
        import os
        if os.environ.get("KSCHED_DEBUG"):
            print("sched: ops", n, "sim_time_us", round(tmax, 1), flush=True)
        pos = {}
        for e in self.ENG:
            for k, i in enumerate(new_order[e]):
                pos[i] = k
        bar = [set() for _ in range(nseg + 1)]
        for s_ in range(1, nseg + 1):
            for e in ("pe", "act", "dve"):
                last = None
                for i in new_order[e]:
                    if ops[i]["seg"] < s_:
                        last = i
                    else:
                        break
                if last is not None:
                    bar[s_].add(last)
            for i in new_order["sp"]:
                if ops[i]["seg"] == s_ - 1 and ops[i]["dma"]:
                    bar[s_].add(i)
        for e in self.GATED:
            lastseg = 0
            for i in new_order[e]:
                sg = ops[i]["seg"]
                if sg > lastseg:
                    for t in range(lastseg + 1, sg + 1):
                        ops[i]["deps"] |= bar[t]
                    lastseg = sg
        for i in new_order["pool"]:
            if ops[i]["arena"]:
                ops[i]["deps"] |= bar[ops[i]["seg"]]
        for i, op in enumerate(ops):
            keep = set()
            bestp = {}
            for d in op["deps"]:
                if d == i:
                    continue
                od = ops[d]
                if od["dma"]:
                    keep.add(d)
                else:
                    pe_ = od["eng"]
                    if pe_ not in bestp or pos[d] > pos[bestp[pe_]]:
                        bestp[pe_] = d
            keep |= set(bestp.values())
            op["deps"] = keep

    def mm(self, out, lhsT, rhs, start, stop, reads, writes):
        nfree = _free(rhs)
        c = max(nfree, 64) / 2400.0 * (4.0 if rhs.dtype == F32 else 1.0) + 0.004
        return self.add("pe", lambda e: e.matmul(out, lhsT, rhs, start=start, stop=stop), reads, writes, cost=c)

    def tr(self, out, in_, ident, reads, writes):
        return self.add("pe", lambda e: e.transpose(out, in_, ident), reads, writes, cost=0.25)

    def act(self, out, in_, func, reads, writes, bias=None, scale=None, accum=None):
        kw = {}
        if bias is not None:
            kw["bias"] = bias
        if scale is not None:
            kw["scale"] = scale
        if accum is not None:
            kw["accum_out"] = accum
        c = 0.22 + _free(out) / 1400.0
        return self.add("act", lambda e: e.activation(out=out, in_=in_, func=func, **kw), reads, writes, cost=c)

    def _vc(self, out):
        return 0.08 + _free(out) / 960.0

    def tt(self, out, in0, in1, op, reads, writes, eng="dve"):
        return self.add(eng, lambda e: e.tensor_tensor(out=out, in0=in0, in1=in1, op=op), reads, writes,
                        cost=self._vc(out))

    def ts(self, out, in0, s1, s2, op0, op1, reads, writes, eng="dve"):
        if s2 is None:
            return self.add(eng, lambda e: e.tensor_scalar(out, in0, s1, None, op0), reads, writes, cost=self._vc(out))
        return self.add(eng, lambda e: e.tensor_scalar(out, in0, s1, s2, op0, op1), reads, writes, cost=self._vc(out))

    def stt(self, out, in0, scalar, in1, op0, op1, reads, writes, eng="dve"):
        return self.add(eng, lambda e: e.scalar_tensor_tensor(out=out, in0=in0, scalar=scalar, in1=in1,
                                                             op0=op0, op1=op1), reads, writes, cost=self._vc(out))

    def cp(self, out, in_, reads, writes, eng="dve"):
        if eng == "act":
            return self.add("act", lambda e: e.activation(out=out, in_=in_, func=AF.Copy), reads, writes,
                            cost=0.22 + _free(out) / 1400.0)
        return self.add(eng, lambda e: e.tensor_copy(out=out, in_=in_), reads, writes, cost=self._vc(out))

    def recip(self, out, in_, reads, writes):
        return self.add("dve", lambda e: e.reciprocal(out=out, in_=in_), reads, writes, cost=0.1 + _free(out) / 240.0)

    def dma(self, out, in_, reads, writes, q="sp", arena=False):
        nbytes = 128 * _free(out) * 4
        return self.add(q, lambda e: e.dma_start(out=out, in_=in_), reads, writes, dma=True,
                        cost=2.0 + nbytes / 150e3, arena=arena)

    def finalize(self, es):
        nc = self.nc
        self.schedule()
        for op in self.ops:
            for d in op["deps"]:
                self.ops[d]["signal"] = True
        for d in self.final:
            self.ops[d]["signal"] = True
        csem = {e: es.enter_context(nc.semaphore("c_" + e)) for e in ("pe", "act", "dve", "pool")}
        NP = 16
        qsem = {q: [es.enter_context(nc.semaphore(f"q_{q}{k}")) for k in range(NP)] for q in ("sp", "pool")}
        cnt = {e: 0 for e in csem}
        qn = {q: 0 for q in qsem}
        quse = {q: [0] * NP for q in qsem}
        qprev = {q: [None] * NP for q in qsem}
        for i, op in enumerate(self.ops):
            if op["dma"]:
                q = op["eng"]
                k = qn[q] % NP
                qn[q] += 1
                quse[q][k] += 1
                op["token"] = (qsem[q][k], 16 * quse[q][k])
                if qprev[q][k] is not None:
                    op["deps"].add(qprev[q][k])
                qprev[q][k] = i
        for e in csem:
            for i in self.by_eng[e]:
                op = self.ops[i]
                if (not op["dma"]) and op["signal"]:
                    cnt[e] += 1
                    op["token"] = (csem[e], cnt[e])
        ops = self.ops
        final = list(self.final)

        def emit(engname, e):
            waited = {}
            for i in self.by_eng[engname]:
                op = ops[i]
                for d in sorted(op["deps"]):
                    sem, val = ops[d]["token"]
                    if waited.get(sem, 0) < val:
                        e.wait_ge(sem, val)
                        waited[sem] = val
                ins = op["fn"](e)
                if op["signal"]:
                    ins.then_inc(op["token"][0], 16 if op["dma"] else 1)
            if engname == "sp":
                for d in final:
                    sem, val = ops[d]["token"]
                    if waited.get(sem, 0) < val:
                        e.wait_ge(sem, val)
                        waited[sem] = val

        with nc.Block() as block:
            @block.tensor
            def _(e):
                emit("pe", e)

            @block.scalar
            def _(e):
                emit("act", e)

            @block.vector
            def _(e):
                emit("dve", e)

            @block.gpsimd
            def _(e):
                emit("pool", e)

            @block.sync
            def _(e):
                emit("sp", e)


def K(name, *idx):
    return (name,) + idx


def build_program(stop_after=None, dumps=(), n_layers=L_):
    nc = bass.Bass("TRN2", target_bir_lowering=False)
    S = Sched(nc)
    es = ExitStack()
    P = {}

    def din(name, shape):
        P[name] = nc.dram_tensor(name, list(shape), F32, kind="ExternalInput").ap()
        return P[name]

    x_d = din("x", [S_, D_])
    mem_d = din("mem", [256, D_])
    w_in_d = din("w_in", [L_, D_, 3076])
    w_out_d = din("w_out", [L_, D_, D_])
    wq_d = din("xattn_wq", [L_, D_, D_])
    wkv_d = din("xattn_wkv", [L_, D_, 2 * D_])
    wo_d = din("xattn_wo", [L_, D_, D_])
    w1_d = din("expert_w1", [L_, 16, D_, 256])
    w3_d = din("expert_w3", [L_, 16, D_, 256])
    w2_d = din("expert_w2", [L_, 16, 256, D_])
    wrt_d = din("router_w", [L_, D_, 20])
    lrubd_d = din("lru_wbd", [L_, 2, 2, 128, 128])
    colp_d = din("colp", [128, L_ * NCOL])
    rowp_d = din("rowp", [128, L_ * NROW])
    c128_d = din("c128", [128, 8 * 128])
    cmask_d = din("cmask", [128, 12 * 512])
    rope_d = din("rope", [128, 2 * S_])
    en_d = din("en", [16, 16 * 128])
    blkoh_d = din("blkoh", [8, S_])
    gate_c_d = din("gatec", [128, 3 * 128])
    out_d = nc.dram_tensor("out", [S_, D_], F32, kind="ExternalOutput").ap()
    dump_d = {}

    uid = [0]

    def sb(name, shape, dt=F32, stack=None):
        uid[0] += 1
        return (stack or es).enter_context(nc.sbuf_tensor(f"{name}_{uid[0]}", list(shape), dt))

    xT = sb("xT", [128, KC, S_])
    hT = sb("hT", [128, KC, S_], BF16)
    colp = sb("colp_s", [128, L_ * NCOL])
    rowp = sb("rowp_s", [128, L_ * NROW])
    c128 = sb("c128_s", [128, 8 * 128])
    c128b = sb("c128b_s", [128, 2 * 128], BF16)
    W = [sb(f"wslot{i}", [128, KC, 512], BF16) for i in range(3)]
    PS = [es.enter_context(nc.psum_tensor(f"ps{i}", [128, 512], F32)) for i in range(8)]
    wn = [0]

    ident = c128[:, 0:128]
    ones_f = c128[:, 128:256]
    tri_le = c128[:, 256:384]
    negu = c128[:, 384:512]
    negones = c128[:, 512:640]
    perm_f = c128[:, 640:768]
    ssdneg = c128[:, 768:896]
    ones_b = c128b[:, 128:256]
    ident_b = c128b[:, 0:128]

    cst = sb("cst", [128, 4])
    S.add("dve", lambda e: e.memset(cst[:, 0:1], EPS), [], [K("cst0")])
    S.add("dve", lambda e: e.memset(cst[:, 1:2], 1.0), [], [K("cst1")])
    S.add("dve", lambda e: e.memset(cst[:, 2:4], 0.0), [K("cst0"), K("cst1")], [K("cst")])
    S.dma(colp[:], colp_d[:, :], [], [K("colp")])
    S.dma(rowp[:], rowp_d[:, :], [], [K("rowp")])
    S.dma(c128[:], c128_d[:, :], [], [K("c128")])
    S.dma(c128b[:], c128_d[:, 0:256], [], [K("c128b")], q="pool")

    def col(l, name, j=0, n=1):
        c0, w = COLS[name]
        return colp[:, l * NCOL + c0 + j: l * NCOL + c0 + j + n]

    def row(l, name, j=0, n=None):
        c0, w = ROWS[name]
        n = w if n is None else n
        return rowp[:, l * NROW + c0 + j: l * NROW + c0 + j + n]

    def ps_rot():
        k = S.psn % 4
        S.psn += 1
        return PS[k], K("ps", k)

    Wf = [w[:].rearrange("p a b -> p (a b)") for w in W]

    def load_w(src2d, nk, ncols, slot=None, c0=0, tot=None):
        tot = ncols if tot is None else tot
        if slot is None:
            k = wn[0] % 3
            wn[0] += 1
        else:
            k = slot
        v = Wf[k][:, 0:nk * tot].rearrange("p (a b) -> p a b", a=nk)
        step = 2 if nk >= 2 else 1
        for h in range(0, nk, step):
            S.dma(v[:, h:h + step, c0:c0 + ncols],
                  src2d[h * 128:(h + step) * 128, :].rearrange("(kc p) n -> p kc n", p=128),
                  [], [K("w", k)], q="pool")
        return v, [K("w", k)], k

    def dump(name, ap, reads, shape):
        if name in dumps:
            d = nc.dram_tensor("dbg_" + name, list(shape), ap.dtype, kind="ExternalOutput").ap()
            dump_d[name] = d
            i = S.dma(d, ap, reads, [K("dbg", name)])
            S.final.append(i)

    def load_fm(dst, src_d, ntile, dkey, st):
        xin = [sb(f"xin{j}", [128, D_], stack=st) for j in range(2)]
        for i in range(ntile):
            b = xin[i % 2]
            S.dma(b[:], src_d[i * 128:(i + 1) * 128, :], [], [K("xin", i % 2)])
            for half in range(2):
                ps, pk = ps_rot()
                for j in range(4):
                    kc = half * 4 + j
                    S.tr(ps[:, j * 128:(j + 1) * 128], b[:, kc * 128:(kc + 1) * 128], ident,
                         [K("xin", i % 2), K("c128")], [pk])
                S.cp(dst[:, half * 4:half * 4 + 4, i * 128:(i + 1) * 128],
                     ps[:].rearrange("p (a b) -> p a b", a=4), [pk],
                     [K(dkey, half * 4 + j, i // 4) for j in range(4)], eng=("dve" if half == 0 else "act"))

    with ExitStack() as st:
        load_fm(xT, x_d, NT, "xT", st)
    S.barrier()
    dump("xT0", xT[:], [K("xT", kc, tc) for kc in range(KC) for tc in range(NTC)], [128, KC, S_])

    def rmsnorm_fm(src, skey, gcol, dst, dkey, nk, T, st, dscale=1.0, hook=None, tag="n"):
        tw = min(T, TW)
        sq = [sb(f"sq_{tag}{j}", [128, nk, tw], BF16, stack=st) for j in range(2)]
        rs = [sb(f"rs_{tag}{j}", [128, tw], stack=st) for j in range(2)]
        for tc in range(T // tw):
            sl = slice(tc * tw, (tc + 1) * tw)
            q = sq[tc % 2]
            for kc in range(nk):
                S.act(q[:, kc, :], src[:, kc, sl], AF.Square, [K(skey, kc, tc)], [K("sq" + tag, tc % 2, kc)])
            ps, pk = ps_rot()
            for kc in range(nk):
                S.mm(ps[:, 0:tw], ones_b, q[:, kc, :], kc == 0, kc == nk - 1,
                     [K("sq" + tag, tc % 2, kc), K("c128b")], [pk])
            r = rs[tc % 2]
            S.act(r[:], ps[:, 0:tw], AF.Sqrt, [pk, K("cst")], [K("rs" + tag, tc % 2)],
                  bias=cst[:, 0:1], scale=1.0 / (nk * 128))
            S.add("dve", (lambda e, r=r: e.reciprocal(out=r[:], in_=r[:])),
                  [K("rs" + tag, tc % 2)], [K("rs" + tag, tc % 2)], cost=0.1 + tw / 240.0)
            if dst is not None:
                for kc in range(nk):
                    S.stt(dst[:, kc, sl], src[:, kc, sl], gcol(kc), r[:], ALU.mult, ALU.mult,
                          [K(skey, kc, tc), K("rs" + tag, tc % 2), K("colp")], [K(dkey, kc, tc)],
                          eng="dve")
            if hook is not None:
                hook(tc, r)

    ALLH = [K("hT", kc, tc) for kc in range(KC) for tc in range(NTC)]

    def hkeys(tc):
        return [K("hT", kc, tc) for kc in range(KC)]

    def proj_fm(wv, wkeys, c0, tc, ps, pk):
        for kc in range(KC):
            S.mm(ps[:, :], wv[:, kc, c0:c0 + 128], hT[:, kc, tc * TW:(tc + 1) * TW], kc == 0, kc == KC - 1,
                 wkeys + [K("hT", kc, tc)], [pk])

    def conv_chunk(ps, pk, tc, xw, xwk, cwcol, cbcol, dst, dkey):
        w_ = xw[tc % 2]
        wk = K(xwk, tc % 2)
        if tc == 0:
            S.add("dve", lambda e: e.memset(w_[:, 0:3], 0.0), [], [wk])
        else:
            S.cp(w_[:, 0:3], xw[(tc - 1) % 2][:, 512:515], [K(xwk, (tc - 1) % 2)], [wk])
        S.cp(w_[:, 3:515], ps[:, :], [pk], [wk], eng="act")
        S.ts(dst, w_[:, 3:515], cwcol(3), cbcol, ALU.mult, ALU.add, [wk, K("colp")], [dkey])
        for k in range(3):
            S.stt(dst, w_[:, k:k + 512], cwcol(k), dst, ALU.mult, ALU.add, [wk, dkey, K("colp")], [dkey])

    def group_out(l, g, YG, st):
        YB = sb("YB", [128, 2, S_], BF16, stack=st)
        rmsnorm_fm(YG, "YG", lambda kc: col(l, "grp_g", 2 * g + kc), YB, "YB", 2, S_, st, tag="g")
        dump(f"yn{l}_{g}", YB[:], [K("YB", c, t) for c in range(2) for t in range(NTC)], [128, 2, S_])
        for half in range(2):
            wv, wk, _ = load_w(w_out_d[l, g * 256:(g + 1) * 256, half * 512:(half + 1) * 512], 2, 512)
            for o4 in range(4):
                oc = half * 4 + o4
                for tc in range(NTC):
                    ps, pk = ps_rot()
                    for kc in range(2):
                        S.mm(ps[:, :], wv[:, kc, o4 * 128:(o4 + 1) * 128], YB[:, kc, tc * TW:(tc + 1) * TW],
                             kc == 0, kc == 1, wk + [K("YB", kc, tc)], [pk])
                    S.tt(xT[:, oc, tc * TW:(tc + 1) * TW], xT[:, oc, tc * TW:(tc + 1) * TW], ps[:, :], ALU.add,
                         [K("xT", oc, tc), pk], [K("xT", oc, tc)])

    ALLX = [K("xT", kc, tc) for kc in range(KC) for tc in range(NTC)]
    done = False
    for l in range(n_layers):
        with ExitStack() as st:
            rmsnorm_fm(xT, "xT", lambda kc: col(l, "mix_g", kc), hT, "hT", KC, S_, st, tag="a")
        S.barrier()
        if l == 0:
            dump("h0", hT[:], ALLH, [128, KC, S_])
        if stop_after == "norm0":
            break

        with ExitStack() as sty, ExitStack() as st:
            YG = sb("YG", [128, 2, S_], stack=sty)
            wv, wk, _ = load_w(w_in_d[l, :, 0:512], 8, 512)
            wbd = sb("wbd", [128, 4, 128], BF16, stack=st)
            S.dma(wbd[:], lrubd_d[l].rearrange("g c k m -> k (g c) m"), [], [K("wbd")], q="pool", arena=True)
            c8 = sb("c8", [128, 2], stack=st)
            dump(f"A_wbd{l}", wbd[:], [K("wbd")], [128, 4, 128])
            cy = sb("cy", [128, 2], stack=st)
            cz = sb("cz", [128, 2], stack=st)
            S.act(cy[:], col(l, "lru_lam", 0, 2), AF.Exp, [K("colp")], [K("cy")], scale=-1.0)
            S.ts(cz[:], cy[:], 2.0, None, ALU.add, None, [K("cy")], [K("cz")])
            S.add("dve", lambda e: e.reciprocal(out=cz[:], in_=cz[:]), [K("cz")], [K("cz")])
            S.tt(cz[:], cz[:], cy[:], ALU.mult, [K("cz"), K("cy")], [K("cz")])
            S.tt(cy[:], cz[:], cz[:], ALU.mult, [K("cz")], [K("cy")])
            S.ts(c8[:], cy[:], 1.0 / 9.0, 1.0 / 7.0, ALU.mult, ALU.add, [K("cy")], [K("c8")])
            for cc_ in (1.0 / 5.0, 1.0 / 3.0, 1.0):
                S.tt(c8[:], c8[:], cy[:], ALU.mult, [K("c8"), K("cy")], [K("c8")])
                S.ts(c8[:], c8[:], cc_, None, ALU.add, None, [K("c8")], [K("c8")])
            S.tt(c8[:], c8[:], cz[:], ALU.mult, [K("c8"), K("cz")], [K("c8")])
            S.ts(c8[:], c8[:], -16.0, None, ALU.mult, None, [K("c8")], [K("c8")])
            xw = [sb(f"xw{j}", [128, 515], stack=st) for j in range(2)]
            XC = sb("XC", [128, S_], stack=st)
            XCB = sb("XCB", [128, S_], BF16, stack=st)
            GG = sb("GG", [128, S_], stack=st)
            RA = sb("RA", [128, S_], stack=st)
            IU = sb("IU", [128, S_], stack=st)
            T2 = sb("T2", [128, S_], stack=st)
            XX = sb("XX", [128, S_], stack=st)
            for c in range(2):
                for tc in range(NTC):
                    sl = slice(tc * TW, (tc + 1) * TW)
                    ps, pk = ps_rot()
                    proj_fm(wv, wk, c * 128, tc, ps, pk)
                    conv_chunk(ps, pk, tc, xw, "xw", lambda k: col(l, "lru_cw", c * 4 + k), col(l, "lru_cb", c),
                               XC[:, sl], K("XC", tc))
                    ps, pk = ps_rot()
                    proj_fm(wv, wk, 256 + c * 128, tc, ps, pk)
                    S.cp(GG[:, sl], ps[:, :], [pk], [K("GG", tc)], eng="act")
                XCk = [K("XC", t) for t in range(NTC)]
                GGk = [K("GG", t) for t in range(NTC)]
                S.tt(T2[:], GG[:], GG[:], ALU.mult, GGk, [K("T2")])
                S.ts(T2[:], T2[:], 0.044715, 1.0, ALU.mult, ALU.add, [K("T2")], [K("T2")])
                S.tt(T2[:], T2[:], GG[:], ALU.mult, [K("T2")] + GGk, [K("T2")])
                S.act(T2[:], T2[:], AF.Sigmoid, [K("T2")], [K("T2")], scale=1.5957691216)
                S.tt(GG[:], GG[:], T2[:], ALU.mult, [K("T2")] + GGk, GGk)
                S.cp(XCB[:], XC[:], XCk, [K("XCB")], eng="act")
                for tc in range(NTC):
                    sl = slice(tc * TW, (tc + 1) * TW)
                    ps, pk = ps_rot()
                    S.mm(ps[:, :], wbd[:, c, :], XCB[:, sl], True, True, [K("wbd"), K("XCB")], [pk])
                    S.act(RA[:, sl], ps[:, :], AF.Sigmoid, [pk, K("colp")], [K("RA", tc)], bias=col(l, "lru_br", c))
                    ps, pk = ps_rot()
                    S.mm(ps[:, :], wbd[:, 2 + c, :], XCB[:, sl], True, True, [K("wbd"), K("XCB")], [pk])
                    S.act(IU[:, sl], ps[:, :], AF.Sigmoid, [pk, K("colp")], [K("IU", tc)], bias=col(l, "lru_bi", c))
                RAk = [K("RA", t) for t in range(NTC)]
                IUk = [K("IU", t) for t in range(NTC)]
                if c == 1:
                    dump(f"A_r{l}", RA[:], RAk, [128, S_])
                    dump(f"A_i{l}", IU[:], IUk, [128, S_])
                    dump(f"A_xcb{l}", XCB[:], [K("XCB")], [128, S_])
                    dump(f"A_xc{l}", XC[:], XCk, [128, S_])
                S.ts(XX[:], RA[:], c8[:, c:c + 1], 2.0, ALU.mult, ALU.mult, RAk + [K("c8")], [K("XX")])
                S.ts(T2[:], XX[:], 1.0 / 5040.0, 1.0 / 720.0, ALU.mult, ALU.add, [K("XX")], [K("T2")])
                for cc_ in (1.0 / 120.0, 1.0 / 24.0, 1.0 / 6.0, 0.5, 1.0):
                    S.tt(T2[:], T2[:], XX[:], ALU.mult, [K("T2"), K("XX")], [K("T2")])
                    S.ts(T2[:], T2[:], cc_, None, ALU.add, None, [K("T2")], [K("T2")])
                S.stt(T2[:], T2[:], -1.0, XX[:], ALU.mult, ALU.mult, [K("T2"), K("XX")], [K("T2")])
                S.act(T2[:], T2[:], AF.Sqrt, [K("T2")], [K("T2")])
                S.act(RA[:], XX[:], AF.Exp, [K("XX")], RAk, scale=0.5)
                S.tt(IU[:], IU[:], XC[:], ALU.mult, IUk + XCk, IUk)
                S.tt(IU[:], IU[:], T2[:], ALU.mult, IUk + [K("T2")], IUk)
                S.add("dve", lambda e: e.tensor_tensor_scan(out=XC[:], data0=RA[:], data1=IU[:], initial=0.0,
                                                            op0=ALU.mult, op1=ALU.add), RAk + IUk, XCk, cost=2.6)
                S.tt(YG[:, c, :], XC[:], GG[:], ALU.mult, XCk + GGk, [K("YG", c, t) for t in range(NTC)])
            for nm_, t_ in (("XC", XC), ("GG", GG), ("RA", RA), ("IU", IU), ("T2", T2)):
                dump(f"A_{nm_}{l}", t_[:], [K(nm_, t) for t in range(NTC)] + [K(nm_)], [128, S_])
            dump(f"ya{l}", YG[:], [K("YG", c, t) for c in range(2) for t in range(NTC)], [128, 2, S_])
            st.close()
            S.barrier()
            group_out(l, 0, YG, sty)
        S.barrier()
        if stop_after == f"A{l}":
            done = True
            break

        with ExitStack() as sty, ExitStack() as st:
            YG = sb("YG", [128, 2, S_], stack=sty)
            QB = sb("QB", [128, 2, S_], BF16, stack=st)
            KZ = sb("KZ", [128, 4, S_], BF16, stack=st)
            S.add("dve", lambda e: e.memset(KZ[:], 0.0), [], [K("KZ", h_, t_) for h_ in range(4) for t_ in range(NTC)], cost=4.5)
            VB = sb("VB", [128, NT, 256], BF16, stack=st)
            M01 = sb("M01", [128, 4, 512], stack=st)
            NM = sb("NM", [128, 4, 512], BF16, stack=st)
            S.dma(M01[:], cmask_d[:, 0:2048].rearrange("p (a b) -> p a b", a=4), [], [K("M01")])
            S.dma(NM[:], cmask_d[:, 2048:4096].rearrange("p (a b) -> p a b", a=4), [], [K("NM")], q="pool", arena=True)
            wv, wk, _ = load_w(w_in_d[l, :, 512:1024], 8, 512)
            wv2, wk2, _ = load_w(w_in_d[l, :, 1024:1280], 8, 256)
            for c2 in range(2):
                for tc in range(NTC):
                    sl = slice(tc * TW, (tc + 1) * TW)
                    ps, pk = ps_rot()
                    proj_fm(wv, wk, c2 * 128, tc, ps, pk)
                    S.act(QB[:, c2, sl], ps[:, :], AF.Copy, [pk], [K("QB", c2, tc)], scale=0.125)
                    ps, pk = ps_rot()
                    proj_fm(wv, wk, 256 + c2 * 128, tc, ps, pk)
                    S.cp(KZ[0:64, 2 * c2, sl], ps[0:64, :], [pk], [K("KZ", 2 * c2, tc)])
                    S.cp(KZ[64:128, 2 * c2 + 1, sl], ps[64:128, :], [pk], [K("KZ", 2 * c2 + 1, tc)], eng="act")
            for i in range(NT):
                ps, pk = ps_rot()
                for kc in range(KC):
                    S.mm(ps[:, 0:256], hT[:, kc, i * 128:(i + 1) * 128], wv2[:, kc, :], kc == 0, kc == KC - 1,
                         wk2 + [K("hT", kc, i // 4)], [pk])
                S.cp(VB[:, i, :], ps[:, 0:256], [pk], [K("VB", i)], eng=("dve" if i % 2 else "act"))
            Eb = [sb(f"Eb{j}", [128, 512], stack=st) for j in range(2)]
            SPb = [sb(f"SPb{j}", [128, 512], F32R, stack=st) for j in range(3)]
            Wb = [sb(f"Wb{j}", [128, 512], BF16, stack=st) for j in range(2)]
            Rb = [sb(f"Rb{j}", [128, 512], F32R, stack=st) for j in range(2)]
            NUr = sb("NUr", [128, 2, 128], F32R, stack=st)
            S.cp(NUr[:, 0, :], negu, [K("c128")], [K("NUr")])
            S.cp(NUr[:, 1, :], negones, [K("c128")], [K("NUr")])
            items = []
            grp = 0
            for h in range(4):
                for c in range(NTC):
                    nb = 4 * c + 4
                    for idx, b in enumerate(range(nb - 1, -1, -1)):
                        items.append(dict(h=h, c=c, idx=idx, b=b, grp=grp, last=(b == 0)))
                    grp += 1
            for i_, itm in enumerate(items):
                itm["i"] = i_
                itm["pr"] = slice((itm["h"] % 2) * 64, (itm["h"] % 2) * 64 + 64)
                itm["c2"] = itm["h"] // 2
                itm["d"] = itm["b"] - 4 * itm["c"]
                itm["ksl"] = slice(itm["b"] * 128, (itm["b"] + 1) * 128)
                itm["qsl"] = slice(itm["c"] * TW, (itm["c"] + 1) * TW)
                itm["qk"] = [K("KZ", itm["h"], itm["b"] // 4), K("QB", itm["c2"], itm["c"])]

            def sb_s1(m):
                i = m["i"]
                pr, c2 = m["pr"], m["c2"]
                psZ, pkZ = ps_rot()
                S.mm(psZ[:, :], KZ[:, m["h"], m["ksl"]], QB[:, c2, m["qsl"]], True, True, m["qk"], [pkZ])
                E, Ek = Eb[i % 2], K("Eb", i % 2)
                SP, SPk = SPb[i % 3], K("SPb", i % 3)
                S.act(E[:], psZ[:, :], AF.Exp, [pkZ], [Ek])
                S.act(SP[:], E[:], AF.Ln, [Ek, K("cst")], [SPk], bias=cst[:, 1:2])
                if m["d"] >= 0:
                    S.tt(SP[:], SP[:].bitcast(F32), M01[:, m["d"], :], ALU.mult, [SPk, K("M01")], [SPk])

            def sb_s2(m):
                i = m["i"]
                pr, c2, d = m["pr"], m["c2"], m["d"]
                SP, SPk = SPb[i % 3], K("SPb", i % 3)
                Wt, Wk_ = Wb[i % 2], K("Wb", i % 2)
                R, Rk = Rb[m["grp"] % 2], K("Rb", m["grp"] % 2)
                psA, pkA = ps_rot()
                has_r = m["idx"] > 0
                S.mm(psA[:, :], KZ[:, m["h"], m["ksl"]], QB[:, c2, m["qsl"]], True, False, m["qk"], [pkA])
                S.mm(psA[:, :], NUr[:, 0, :], SP[:], False, (not has_r) and d < 0, [SPk, K("NUr")], [pkA])
                if has_r:
                    S.mm(psA[:, :], NUr[:, 1, :], R[:], False, d < 0, [Rk, K("NUr")], [pkA])
                if d >= 0:
                    S.mm(psA[:, :], ident_b, NM[:, d, :], False, True, [K("NM"), K("c128b")], [pkA])
                S.act(Wt[:], psA[:, :], AF.Exp, [pkA], [Wk_])
                if not m["last"]:
                    if m["idx"] == 0:
                        S.cp(R[:], SP[:].bitcast(F32), [SPk], [Rk])
                    else:
                        S.tt(R[:], R[:].bitcast(F32), SP[:].bitcast(F32), ALU.add, [Rk, SPk], [Rk])

            def sb_s3(m):
                i = m["i"]
                pr, c2, h = m["pr"], m["c2"], m["h"]
                Wt, Wk_ = Wb[i % 2], K("Wb", i % 2)
                psO, pkO = PS[4 + m["grp"] % 2], K("psO", m["grp"] % 2)
                S.mm(psO[:, :], VB[:, m["b"], c2 * 128:(c2 + 1) * 128], Wt[:], m["idx"] == 0, m["last"],
                     [K("VB", m["b"]), Wk_], [pkO])
                if m["last"]:
                    S.cp(YG[pr, c2, m["qsl"]], psO[pr, :], [pkO], [K("YG", c2, m["c"])], eng="act")

            n_it = len(items)
            for r in range(-2, n_it):
                if 0 <= r + 2 < n_it:
                    sb_s1(items[r + 2])
                if 0 <= r + 1 < n_it:
                    sb_s2(items[r + 1])
                if 0 <= r < n_it:
                    sb_s3(items[r])
            dump(f"yb{l}", YG[:], [K("YG", c, t) for c in range(2) for t in range(NTC)], [128, 2, S_])
            st.close()
            S.barrier()
            group_out(l, 1, YG, sty)
        S.barrier()
        if stop_after == f"B{l}":
            done = True
            break

        with ExitStack() as sty, ExitStack() as st:
            YG = sb("YG", [128, 2, S_], stack=sty)
            XS = sb("XS", [128, 2, S_], stack=st)
            BTb = sb("BTb", [128, 2, S_], BF16, stack=st)
            CTb = sb("CTb", [128, 2, S_], BF16, stack=st)
            BTM = sb("BTM", [128, NT, 256], BF16, stack=st)
            XTM = sb("XTM", [128, NT, 256], BF16, stack=st)
            wdt = sb("wdt", [128, KC, 4], BF16, stack=st)
            stc = ExitStack()
            xw = [sb(f"xwc{j}", [128, 515], stack=stc) for j in range(2)]
            CV = [sb(f"CV{j}", [128, 512], stack=stc) for j in range(2)]
            BF = [sb(f"BF{j}", [128, 512], stack=stc) for j in range(2)]
            S.dma(wdt[:], w_in_d[l, :, 2304:2308].rearrange("(kc p) n -> p kc n", p=128), [], [K("wdt")], q="pool", arena=True)
            wv1, wk1, _ = load_w(w_in_d[l, :, 1280:1792], 8, 512)
            wv2, wk2, _ = load_w(w_in_d[l, :, 1792:2304], 8, 512)
            n_cv = 0
            for j in range(6):
                if j < 2:
                    wv, wk, c0 = wv1, wk1, 256 + j * 128
                elif j < 4:
                    wv, wk, c0 = wv2, wk2, (j - 2) * 128
                else:
                    wv, wk, c0 = wv2, wk2, 256 + (j - 4) * 128
                for tc in range(NTC):
                    sl = slice(tc * TW, (tc + 1) * TW)
                    ps, pk = ps_rot()
                    proj_fm(wv, wk, c0, tc, ps, pk)
                    cv, cvk = CV[n_cv % 2], K("CV", n_cv % 2)
                    conv_chunk(ps, pk, tc, xw, "xwc", lambda k: col(l, "ssm_cw", j * 4 + k), col(l, "ssm_cb", j),
                               cv[:], cvk)
                    if j < 2:
                        S.act(XS[:, j, sl], cv[:], AF.Silu, [cvk], [K("XS", j, tc)])
                        ps, pk = ps_rot()
                        for q in range(4):
                            S.tr(ps[:, q * 128:(q + 1) * 128], XS[:, j, tc * TW + q * 128: tc * TW + (q + 1) * 128],
                                 ident, [K("XS", j, tc), K("c128")], [pk])
                        S.cp(XTM[:, tc * 4:tc * 4 + 4, j * 128:(j + 1) * 128],
                             ps[:].rearrange("p (a b) -> p a b", a=4), [pk], [K("XTM", tc, j)])
                    elif j < 4:
                        g = j - 2
                        bf, bfk = BF[n_cv % 2], K("BF", n_cv % 2)
                        S.act(bf[:], cv[:], AF.Silu, [cvk], [bfk])
                        S.cp(BTb[:, g, sl], bf[:], [bfk], [K("BTb", g, tc)])
                        ps, pk = ps_rot()
                        for q in range(4):
                            S.tr(ps[:, q * 128:(q + 1) * 128], bf[:, q * 128:(q + 1) * 128], ident,
                                 [bfk, K("c128")], [pk])
                        S.cp(BTM[:, tc * 4:tc * 4 + 4, g * 128:(g + 1) * 128],
                             ps[:].rearrange("p (a b) -> p a b", a=4), [pk], [K("BTM", tc, g)])
                    else:
                        S.act(CTb[:, j - 4, sl], cv[:], AF.Silu, [cvk], [K("CTb", j - 4, tc)])
                    n_cv += 1
            stc.close()
            S.barrier()
            DT = sb("DT", [128, 64], stack=st)
            AN = sb("AN", [128, 64], stack=st)
            AC = sb("AC", [128, 64], stack=st)
            ACS = sb("ACS", [128, 64], stack=st)
            TOT = sb("TOT", [128, 64], stack=st)
            DEC = sb("DEC", [128, 64], stack=st)
            ETOT = sb("ETOT", [128, 64], stack=st)
            DTD = sb("DTD", [128, 64], stack=st)
            psd, pkd = ps_rot()
            for i in range(NT):
                for kc in range(KC):
                    S.mm(psd[:, i * 4:(i + 1) * 4], hT[:, kc, i * 128:(i + 1) * 128], wdt[:, kc, :], kc == 0,
                         kc == KC - 1, [K("wdt"), K("hT", kc, i // 4)], [pkd])
            S.tt(DT[:], psd[:, 0:64], row(l, "dt_bias"), ALU.add, [pkd, K("rowp")], [K("DT")])
            S.act(DT[:], DT[:], AF.Exp, [K("DT")], [K("DT")])
            S.act(DT[:], DT[:], AF.Ln, [K("DT"), K("cst")], [K("DT")], bias=cst[:, 1:2])
            S.act(AN[:], row(l, "a_log"), AF.Exp, [K("rowp")], [K("AN")])
            S.ts(AN[:], AN[:], -1.0, None, ALU.mult, None, [K("AN")], [K("AN")])
            S.tt(AC[:], DT[:], AN[:], ALU.mult, [K("DT"), K("AN")], [K("AC")])
            ps, pk = ps_rot()
            S.mm(ps[:, 0:64], tri_le, AC[:], True, True, [K("AC"), K("c128")], [pk])
            S.cp(ACS[:], ps[:, 0:64], [pk], [K("ACS")])
            ps, pk = ps_rot()
            S.mm(ps[:, 0:64], ones_f, AC[:], True, True, [K("AC"), K("c128")], [pk])
            S.cp(TOT[:], ps[:, 0:64], [pk], [K("TOT")])
            S.tt(DEC[:], TOT[:], ACS[:], ALU.subtract, [K("TOT"), K("ACS")], [K("DEC")])
            S.act(DEC[:], DEC[:], AF.Exp, [K("DEC")], [K("DEC")])
            S.act(ETOT[:], TOT[:], AF.Exp, [K("TOT")], [K("ETOT")])
            S.tt(DTD[:], DT[:], DEC[:], ALU.mult, [K("DT"), K("DEC")], [K("DTD")])
            ST = sb("ST", [128, 4, 64], stack=st)
            S.add("dve", lambda e: e.memset(ST[:], 0.0), [], [K("ST", h) for h in range(4)])
            Rm = [sb(f"Rm{j}", [128, 128], stack=st) for j in range(2)]
            ARG = [sb(f"ARG{j}", [128, 128], stack=st) for j in range(2)]
            E1 = [sb(f"E1{j}", [128, 128], stack=st) for j in range(2)]
            GL = [sb(f"GL{j}", [128, 128], BF16, stack=st) for j in range(2)]
            CTp = [sb(f"CTp{j}", [128, 128], BF16, stack=st) for j in range(2)]
            XCp = [[sb(f"XCp{a}{j}", [128, 128], BF16, stack=st) for j in range(2)] for a in range(2)]
            STp = sb("STp", [128, 4, 128], BF16, stack=st)
            for a in range(2):
                for j in range(2):
                    S.add("dve", (lambda e, t_=XCp[a][j]: e.memset(t_[:], 0.0)), [], [K("XCp", a, j)])
            S.add("dve", lambda e: e.memset(STp[:], 0.0), [], [K("STp", h) for h in range(4)])
            XDh = [sb(f"XDh{j}", [128, 64], BF16, stack=st) for j in range(2)]
            n = 0
            for ci in range(NT):
                csl = slice(ci * 128, (ci + 1) * 128)
                tcx = ci // 4
                for g in range(2):
                    psG, pkG = ps_rot()
                    S.mm(psG[:, 0:128], BTb[:, g, csl], CTb[:, g, csl], True, True,
                         [K("BTb", g, tcx), K("CTb", g, tcx)], [pkG])
                    psY, pkY = PS[4 + (ci * 2 + g) % 2], K("psO", (ci * 2 + g) % 2)
                    for hh in range(2):
                        h = 2 * g + hh
                        pr = slice(hh * 64, hh * 64 + 64)
                        cidx = ci * 4 + h
                        k2 = n % 2
                        S.ts(Rm[k2][:], tri_le, AC[:, cidx:cidx + 1], None, ALU.mult, None,
                             [K("AC"), K("c128")], [K("Rm", k2)])
                        ps1, pk1 = ps_rot()
                        S.mm(ps1[:, 0:128], ones_f, Rm[k2][:], True, True, [K("Rm", k2), K("c128")], [pk1])
                        S.stt(ARG[k2][:], ps1[:, 0:128], ACS[:, cidx:cidx + 1], ssdneg, ALU.subtract, ALU.add,
                              [pk1, K("ACS"), K("c128")], [K("ARG", k2)])
                        S.act(ARG[k2][:], ARG[k2][:], AF.Exp, [K("ARG", k2)], [K("ARG", k2)])
                        S.tt(GL[k2][:], ARG[k2][:], psG[:, 0:128], ALU.mult, [K("ARG", k2), pkG], [K("GL", k2)])
                        S.act(E1[k2][:], ps1[:, 0:128], AF.Exp, [pk1], [K("E1", k2)])
                        S.tt(CTp[k2][:], CTb[:, g, csl], E1[k2][:], ALU.mult, [K("CTb", g, tcx), K("E1", k2)],
                             [K("CTp", k2)])
                        rot = (ci * 2 + g) % 2
                        xcp = XCp[hh][rot]
                        S.ts(xcp[:, hh * 64:(hh + 1) * 64], XTM[:, ci, h * 64:(h + 1) * 64], DT[:, cidx:cidx + 1], None,
                             ALU.mult, None, [K("XTM", tcx, g), K("DT")], [K("XCp", hh, rot)])
                        S.mm(psY[:, 0:128], xcp[:], GL[k2][:], hh == 0, False, [K("XCp", hh, rot), K("GL", k2)], [pkY])
                        S.mm(psY[:, 0:128], STp[:, h, :], CTp[k2][:], False, hh == 1, [K("STp", h), K("CTp", k2)], [pkY])
                        S.ts(XDh[k2][:], XTM[:, ci, h * 64:(h + 1) * 64], DTD[:, cidx:cidx + 1], None, ALU.mult, None,
                             [K("XTM", tcx, g), K("DTD")], [K("XDh", k2)])
                        psS, pkS = ps_rot()
                        S.mm(psS[:, 0:64], BTM[:, ci, g * 128:(g + 1) * 128], XDh[k2][:], True, True,
                             [K("BTM", tcx, g), K("XDh", k2)], [pkS])
                        S.stt(ST[:, h, :], ST[:, h, :], ETOT[:, cidx:cidx + 1], psS[:, 0:64], ALU.mult, ALU.add,
                              [K("ST", h), K("ETOT"), pkS], [K("ST", h)])
                        S.cp(STp[:, h, hh * 64:(hh + 1) * 64], ST[:, h, :], [K("ST", h)], [K("STp", h)], eng="act")
                        n += 1
                    S.cp(YG[:, g, csl], psY[:, 0:128], [pkY], [K("YG", g, tcx)], eng="act")
            ARG = [sb(f"ZT{j}", [128, 512], stack=st) for j in range(1)]
            for j in range(2):
                YGk = [K("YG", j, t) for t in range(NTC)]
                S.stt(YG[:, j, :], XS[:, j, :], col(l, "ssm_d", j), YG[:, j, :], ALU.mult, ALU.add,
                      [K("XS", j, t) for t in range(NTC)] + YGk + [K("colp")], YGk)
                for tc in range(NTC):
                    sl = slice(tc * TW, (tc + 1) * TW)
                    ps, pk = ps_rot()
                    proj_fm(wv1, wk1, j * 128, tc, ps, pk)
                    zt, ztk = ARG[0], K("ARGz", 0)
                    S.act(zt[:], ps[:, :], AF.Silu, [pk], [ztk])
                    S.tt(YG[:, j, sl], YG[:, j, sl], zt[:], ALU.mult, [K("YG", j, tc), ztk], [K("YG", j, tc)])
            dump(f"yc{l}", YG[:], [K("YG", c, t) for c in range(2) for t in range(NTC)], [128, 2, S_])
            st.close()
            S.barrier()
            group_out(l, 2, YG, sty)
        S.barrier()
        if stop_after == f"C{l}":
            done = True
            break

        with ExitStack() as sty, ExitStack() as st:
            YG = sb("YG", [128, 2, S_], stack=sty)
            QA = sb("QA", [128, 4, S_], BF16, stack=st)
            KA = sb("KA", [128, 4, S_], BF16, stack=st)
            allqa = [K("QA", h_, t_) for h_ in range(4) for t_ in range(NTC)]
            allka = [K("KA", h_, t_) for h_ in range(4) for t_ in range(NTC)]
            S.add("dve", lambda e: e.memset(QA[:], 0.0), [], allqa, cost=4.5)
            S.add("dve", lambda e: e.memset(KA[:], 0.0), [], allka, cost=4.5)
            for h_ in range(4):
                o_ = 64 if h_ % 2 == 0 else 0
                S.dma(KA[o_:o_ + 8, h_, :], blkoh_d[:, :], allka, [K("KAoh", h_)], q="pool", arena=True)
            VB = sb("VB", [128, NT, 256], BF16, stack=st)
            CAUS = sb("CAUS", [128, 4, 512], BF16, stack=st)
            S.dma(CAUS[:], cmask_d[:, 4096:6144].rearrange("p (a b) -> p a b", a=4), [], [K("CAUS")], q="pool", arena=True)
            GC = sb("GC", [128, 3, 128], stack=st)
            S.dma(GC[:], gate_c_d[:, :].rearrange("p (a b) -> p a b", a=3), [], [K("GC")])
            KM = sb("KM", [128, 2, 8], stack=st)
            KMb = sb("KMb", [128, 2, 8], BF16, stack=st)
            wv, wk, _ = load_w(w_in_d[l, :, 2308:2820], 8, 512)
            wv2, wk2, _ = load_w(w_in_d[l, :, 2820:3076], 8, 256)
            str_ = ExitStack()
            ROPEb = [sb(f"ROPE{j}", [128, 2, TW], stack=str_) for j in range(1)]
            RAW = [sb(f"RAW{j}", [128, 512], stack=str_) for j in range(2)]
            TMP = [sb(f"TMPr{j}", [128, 512], stack=str_) for j in range(2)]
            KF = [sb(f"KF{j}", [128, 512], stack=str_) for j in range(2)]
            nr = 0
            for c2 in range(2):
                for tc in range(NTC):
                    sl = slice(tc * TW, (tc + 1) * TW)
                    rj = 0
                    ROPE = ROPEb[rj]
                    rsl = slice(0, TW)
                    S.dma(ROPE[:, 0, :], rope_d[:, tc * TW:(tc + 1) * TW], [], [K("ROPE", rj, 0)])
                    S.dma(ROPE[:, 1, :], rope_d[:, S_ + tc * TW:S_ + (tc + 1) * TW], [], [K("ROPE", rj, 1)])
                    for which in range(2):
                        ps, pk = ps_rot()
                        proj_fm(wv, wk, which * 256 + c2 * 128, tc, ps, pk)
                        raw, rawk = RAW[nr % 2], K("RAW", nr % 2)
                        tmp, tmpk = TMP[nr % 2], K("TMPr", nr % 2)
                        S.cp(raw[:], ps[:, :], [pk], [rawk], eng="act")
                        psw, pkw = ps_rot()
                        S.mm(psw[:, :], perm_f, raw[:], True, True, [rawk, K("c128")], [pkw])
                        S.tt(tmp[:], psw[:, :], ROPE[:, 1, rsl], ALU.mult, [pkw, K("ROPE", rj, 1)], [tmpk])
                        kf = KF[nr % 2]
                        dst, dk = kf[:], K("KF", nr % 2)
                        S.tt(dst, raw[:], ROPE[:, 0, rsl], ALU.mult, [rawk, K("ROPE", rj, 0)], [dk])
                        S.tt(dst, dst, tmp[:], ALU.add, [dk, tmpk], [dk])
                        if which == 0:
                            S.act(QA[0:64, 2 * c2, sl], kf[0:64, :], AF.Copy, [dk], [K("QA", 2 * c2, tc)], scale=0.125)
                            S.act(QA[64:128, 2 * c2 + 1, sl], kf[64:128, :], AF.Copy, [dk], [K("QA", 2 * c2 + 1, tc)],
                                  scale=0.125)
                        else:
                            S.cp(KA[0:64, 2 * c2, sl], kf[0:64, :], [dk], [K("KA", 2 * c2, tc)], eng="act")
                            S.cp(KA[64:128, 2 * c2 + 1, sl], kf[64:128, :], [dk], [K("KA", 2 * c2 + 1, tc)], eng="act")
                            for hb in range(2):
                                nblk = tc * 2 + hb
                                S.add("dve", (lambda e, o=KM[:, c2, nblk:nblk + 1], i_=kf[:, hb * 256:(hb + 1) * 256]:
                                              e.reduce_sum(out=o, in_=i_, axis=AX.X)), [dk], [K("KM", c2, nblk)])
                        nr += 1
            for i in range(NT):
                ps, pk = ps_rot()
                for kc in range(KC):
                    S.mm(ps[:, 0:256], hT[:, kc, i * 128:(i + 1) * 128], wv2[:, kc, :], kc == 0, kc == KC - 1,
                         wk2 + [K("hT", kc, i // 4)], [pk])
                S.cp(VB[:, i, :], ps[:, 0:256], [pk], [K("VB", i)], eng=("dve" if i % 2 else "act"))
            S.cp(KMb[:], KM[:], [K("KM", c2_, nb_) for c2_ in range(2) for nb_ in range(8)], [K("KMb")])
            str_.close()
            S.barrier()
            PTb = [sb(f"PTb{j}", [128, 512], BF16, stack=st) for j in range(3)]
            RD = [sb(f"RD{j}", [128, 512], stack=st) for j in range(2)]
            GT = sb("GT", [128, 128], stack=st)
            BT_ = sb("BIASTM", [128, 128], stack=st)
            MX = [sb(f"MX{j}", [128, 8], stack=st) for j in range(2)]
            for h in range(4):
                hh, c2 = h % 2, h // 2
                pr = slice(hh * 64, hh * 64 + 64)
                o_ = 64 if hh == 0 else 0
                psg, pkg = ps_rot()
                for i in range(NT):
                    S.mm(psg[:, i * 8:(i + 1) * 8], QA[pr, h, i * 128:(i + 1) * 128], KMb[pr, c2, :], True, True,
                         [K("QA", h, i // 4), K("KMb")], [pkg])
                S.stt(GT[:], psg[:, 0:128], 8.0 / 256.0, GC[:, 0, :], ALU.mult, ALU.add, [pkg, K("GC")], [K("GT")])
                for i in range(NT):
                    mx, mxk = MX[i % 2], K("MX", i % 2)
                    S.add("dve", (lambda e, o=mx[:], i_=GT[:, i * 8:(i + 1) * 8]: e.max(out=o, in_=i_)),
                          [K("GT")], [mxk])
                    bsl = BT_[:, i * 8:(i + 1) * 8]
                    S.ts(bsl, GT[:, i * 8:(i + 1) * 8], mx[:, 2:3], None, ALU.is_ge, None, [K("GT"), mxk],
                         [K("BIASTM", i)])
                    S.tt(bsl, bsl, GC[:, 1, i * 8:(i + 1) * 8], ALU.mult, [K("BIASTM", i), K("GC")], [K("BIASTM", i)])
                    S.tt(bsl, bsl, GC[:, 2, i * 8:(i + 1) * 8], ALU.add, [K("BIASTM", i), K("GC")], [K("BIASTM", i)])
                    S.ts(bsl, bsl, -1.0, -NEG, ALU.add, ALU.mult, [K("BIASTM", i)], [K("BIASTM", i)])
                for q4 in range(4):
                    pst, pkt = ps_rot()
                    for q in range(4):
                        i = q4 * 4 + q
                        S.tr(pst[0:8, q * 128:(q + 1) * 128], BT_[:, i * 8:(i + 1) * 8], ident,
                             [K("BIASTM", i), K("c128")], [pkt])
                    S.cp(QA[o_:o_ + 8, h, q4 * 512:(q4 + 1) * 512], pst[0:8, :], [pkt], [K("QAsel", h, q4)])
            its = []
            grp = 0
            for h in range(4):
                for c in range(NTC):
                    nkt = 4 * c + 4
                    for kt in range(nkt):
                        its.append(dict(h=h, c=c, kt=kt, nkt=nkt, grp=grp, i=len(its)))
                    grp += 1

            def m_s1(m):
                h, c, kt = m["h"], m["c"], m["kt"]
                d = kt - 4 * c
                qsl = slice(c * TW, (c + 1) * TW)
                ksl = slice(kt * 128, (kt + 1) * 128)
                psS, pkS = ps_rot()
                S.mm(psS[:, :], KA[:, h, ksl], QA[:, h, qsl], True, d < 0,
                     [K("KA", h, kt // 4), K("KAoh", h), K("QA", h, c), K("QAsel", h, c)], [pkS])
                if d >= 0:
                    S.mm(psS[:, :], ident_b, CAUS[:, d, :], False, True, [K("CAUS"), K("c128b")], [pkS])
                pt, ptk = PTb[m["i"] % 3], K("PTb", m["i"] % 3)
                S.act(pt[:], psS[:, :], AF.Exp, [pkS], [ptk])

            def m_s2(m):
                h, c, kt, nkt = m["h"], m["c"], m["kt"], m["nkt"]
                hh, c2 = h % 2, h // 2
                pr = slice(hh * 64, hh * 64 + 64)
                qsl = slice(c * TW, (c + 1) * TW)
                g2 = m["grp"] % 2
                psO, pkO = PS[4 + g2], K("psO", g2)
                psD, pkD = PS[6 + g2], K("psD", g2)
                pt, ptk = PTb[m["i"] % 3], K("PTb", m["i"] % 3)
                S.mm(psO[:, :], VB[:, kt, c2 * 128:(c2 + 1) * 128], pt[:], kt == 0, kt == nkt - 1,
                     [K("VB", kt), ptk], [pkO])
                S.mm(psD[:, :], ones_b, pt[:], kt == 0, kt == nkt - 1, [ptk, K("c128b")], [pkD])
                if kt == nkt - 1:
                    rd, rdk = RD[g2], K("RD", g2)
                    S.add("dve", (lambda e, o=rd[pr, :], i_=psD[pr, :]: e.reciprocal(out=o, in_=i_)), [pkD], [rdk], cost=2.2)
                    S.tt(YG[pr, c2, qsl], psO[pr, :], rd[pr, :], ALU.mult, [pkO, rdk], [K("YG", c2, c)])

            n_ = len(its)
            for r in range(-1, n_):
                if r + 1 < n_:
                    m_s1(its[r + 1])
                if r >= 0:
                    m_s2(its[r])
            dump(f"yd{l}", YG[:], [K("YG", c, t) for c in range(2) for t in range(NTC)], [128, 2, S_])
            st.close()
            S.barrier()
            group_out(l, 3, YG, sty)
        S.barrier()
        dump(f"x1_{l}", xT[:], ALLX, [128, KC, S_])
        if stop_after == f"D{l}":
            done = True
            break

        with ExitStack() as st:
            with ExitStack() as st2:
                rmsnorm_fm(xT, "xT", lambda kc: col(l, "xat_g", kc), hT, "hT", KC, S_, st2, tag="x")
            S.barrier()
            memT = sb("memT", [128, KC, 256], stack=st)
            MTb = sb("MTb", [128, KC, 256], BF16, stack=st)
            with ExitStack() as st2:
                load_fm(memT, mem_d, 2, "memT", st2)
                rmsnorm_fm(memT, "memT", lambda kc: col(l, "mem_g", kc), MTb, "MTb", KC, 256, st2, tag="m")
            S.barrier()
            KTb = sb("KTb", [128, KC, 256], BF16, stack=st)
            VX = sb("VX", [128, 2, D_], BF16, stack=st)
            QX = sb("QX", [128, KC, S_], BF16, stack=st)
            MTk = [K("MTb", kc, 0) for kc in range(KC)]
            for half in range(2):
                wv, wk, _ = load_w(wkv_d[l, :, half * 512:(half + 1) * 512], 8, 512)
                for o4 in range(4):
                    oc = half * 4 + o4
                    ps, pk = ps_rot()
                    for kc in range(KC):
                        S.mm(ps[:, 0:256], wv[:, kc, o4 * 128:(o4 + 1) * 128], MTb[:, kc, :], kc == 0, kc == KC - 1,
                             wk + [K("MTb", kc, 0)], [pk])
                    S.cp(KTb[:, oc, :], ps[:, 0:256], [pk], [K("KTb", oc)])
            for half in range(2):
                wv, wk, _ = load_w(wkv_d[l, :, 1024 + half * 512:1024 + (half + 1) * 512], 8, 512)
                for mt in range(2):
                    ps, pk = ps_rot()
                    for kc in range(KC):
                        S.mm(ps[:, :], MTb[:, kc, mt * 128:(mt + 1) * 128], wv[:, kc, :], kc == 0, kc == KC - 1,
                             wk + [K("MTb", kc, 0)], [pk])
                    S.cp(VX[:, mt, half * 512:(half + 1) * 512], ps[:, :], [pk], [K("VX", mt, half)], eng="act")
            for half in range(2):
                wv, wk, _ = load_w(wq_d[l, :, half * 512:(half + 1) * 512], 8, 512)
                for o4 in range(4):
                    oc = half * 4 + o4
                    for tc in range(NTC):
                        ps, pk = ps_rot()
                        proj_fm(wv, wk, o4 * 128, tc, ps, pk)
                        S.act(QX[:, oc, tc * TW:(tc + 1) * TW], ps[:, :], AF.Copy, [pk], [K("QX", oc, tc)],
                              scale=1.0 / 16.0)
            dump(f"X_MTb{l}", MTb[:], MTk, [128, KC, 256])
            dump(f"X_KTb{l}", KTb[:], [K("KTb", oc) for oc in range(KC)], [128, KC, 256])
            dump(f"X_QX{l}", QX[:], [K("QX", oc, tc) for oc in range(KC) for tc in range(NTC)], [128, KC, S_])
            dump(f"X_VX{l}", VX[:], [K("VX", mt, hf) for mt in range(2) for hf in range(2)], [128, 2, D_])
            PT = [sb(f"PTx{j}", [128, 512], BF16, stack=st) for j in range(4)]
            RDx = [sb(f"RDx{j}", [128, 512], stack=st) for j in range(2)]
            it = 0
            for hd in range(4):
                for tc in range(NTC):
                    sl = slice(tc * TW, (tc + 1) * TW)
                    pts = []
                    for mt in range(2):
                        ps, pk = ps_rot()
                        for j in range(2):
                            S.mm(ps[:, :], KTb[:, 2 * hd + j, mt * 128:(mt + 1) * 128], QX[:, 2 * hd + j, sl],
                                 j == 0, j == 1, [K("KTb", 2 * hd + j), K("QX", 2 * hd + j, tc)], [pk])
                        pt, ptk = PT[it % 4], K("PTx", it % 4)
                        S.act(pt[:], ps[:, :], AF.Exp, [pk], [ptk])
                        pts.append((pt, ptk))
                        it += 1
                    psD, pkD = ps_rot()
                    for mt in range(2):
                        S.mm(psD[:, :], ones_b, pts[mt][0][:], mt == 0, mt == 1, [pts[mt][1], K("c128b")], [pkD])
                    rd, rdk = RDx[(hd * NTC + tc) % 2], K("RDx", (hd * NTC + tc) % 2)
                    S.add("dve", (lambda e, o=rd[:], i_=psD[:, :]: e.reciprocal(out=o, in_=i_)), [pkD], [rdk], cost=2.2)
                    for j in range(2):
                        oc = 2 * hd + j
                        ps, pk = ps_rot()
                        for mt in range(2):
                            S.mm(ps[:, :], VX[:, mt, oc * 128:(oc + 1) * 128], pts[mt][0][:], mt == 0, mt == 1,
                                 [K("VX", mt, oc // 4), pts[mt][1]], [pk])
                        S.tt(hT[:, oc, sl], ps[:, :], rd[:], ALU.mult, [pk, rdk], [K("hT", oc, tc)])
            dump(f"X_OX{l}", hT[:], ALLH, [128, KC, S_])
            for half in range(2):
                wv, wk, _ = load_w(wo_d[l, :, half * 512:(half + 1) * 512], 8, 512)
                for o4 in range(4):
                    oc = half * 4 + o4
                    for tc in range(NTC):
                        ps, pk = ps_rot()
                        proj_fm(wv, wk, o4 * 128, tc, ps, pk)
                        S.tt(xT[:, oc, tc * TW:(tc + 1) * TW], xT[:, oc, tc * TW:(tc + 1) * TW], ps[:, :], ALU.add,
                             [K("xT", oc, tc), pk], [K("xT", oc, tc)])
        S.barrier()
        dump(f"x2_{l}", xT[:], ALLX, [128, KC, S_])
        if stop_after == f"X{l}":
            done = True
            break

        with ExitStack() as st:
            ENp = sb("ENp", [128, 16, 128], BF16, stack=st)
            S.add("dve", lambda e: e.memset(ENp[:], 0.0), [], [K("ENpz")])
            S.dma(ENp[0:16, :, :], en_d[:, :].rearrange("p (a b) -> p a b", a=16), [K("ENpz")], [K("ENf")], q="pool",
                  arena=True)
            CTh = sb("CTh", [128, S_], BF16, stack=st)
            CTl = sb("CTl", [128, S_], BF16, stack=st)
            S.add("dve", lambda e: e.memset(CTh[:], 0.0), [], [K("CThz")])
            S.add("dve", lambda e: e.memset(CTl[:], 0.0), [], [K("CTlz")])
            st_rt = ExitStack()
            WR = sb("WR", [128, KC, 20], stack=st_rt)
            S.dma(WR[:], wrt_d[l].rearrange("(kc p) n -> p kc n", p=128), [], [K("WR")])
            LG = sb("LG", [128, NT, 20], stack=st_rt)
            st_hf = ExitStack()
            HF = sb("HF", [128, KC, TW], stack=st_hf)

            def router_hook(tc, r):
                sl = slice(tc * TW, (tc + 1) * TW)
                for kc in range(KC):
                    S.stt(HF[:, kc, :], xT[:, kc, sl], col(l, "ffn_g", kc), r[:], ALU.mult, ALU.mult,
                          [K("xT", kc, tc), K("rsf", tc % 2), K("colp")], [K("HF", kc)])
                ps, pk = ps_rot()
                for q in range(4):
                    for kc in range(KC):
                        S.mm(ps[:, q * 20:(q + 1) * 20], HF[:, kc, q * 128:(q + 1) * 128], WR[:, kc, :], kc == 0,
                             kc == KC - 1, [K("HF", kc), K("WR")], [pk])
                S.cp(LG[:, tc * 4:tc * 4 + 4, :], ps[:, 0:80].rearrange("p (a b) -> p a b", a=4), [pk],
                     [K("LG", tc)])

            with ExitStack() as st2:
                rmsnorm_fm(xT, "xT", lambda kc: col(l, "ffn_g", kc), hT, "hT", KC, S_, st2, hook=router_hook, tag="f")
            st_hf.close()
            S.barrier()
            COMB = sb("COMB", [128, NT, 16], stack=st_rt)
            CT = sb("CT", [16, S_], stack=st_rt)
            sm = {nm: sb("rt_" + nm, [128, NT, w_] if w_ > 1 else [128, NT], stack=st_rt) for nm, w_ in
                  [("lg", 4), ("m", 1), ("eg", 4), ("sg", 1), ("oh", 4), ("le", 16), ("sel", 4), ("tmp", 4),
                   ("a", 1), ("eq", 4), ("sel2", 4), ("b", 1), ("t2", 4), ("ex", 4), ("dd", 1), ("wg", 4)]}

            def v(nm):
                return sm[nm][:]

            def kk(nm):
                return [K("rt", nm)]

            def B3(ap2, w_=4):
                return ap2.unsqueeze(2).to_broadcast([128, NT, w_])

            LGk = [K("LG", t) for t in range(NTC)]
            bgB = row(l, "bg").unsqueeze(1).to_broadcast([128, NT, 4])
            beB = row(l, "be").unsqueeze(1).to_broadcast([128, NT, 16])
            S.tt(v("lg"), LG[:, :, 0:4], bgB, ALU.add, LGk + [K("rowp")], kk("lg"))
            S.add("dve", lambda e: e.reduce_max(out=v("m"), in_=v("lg"), axis=AX.X), kk("lg"), kk("m"))
            S.tt(v("lg"), v("lg"), B3(v("m")), ALU.subtract, kk("lg") + kk("m"), kk("lg"))
            S.act(v("eg"), v("lg"), AF.Exp, kk("lg"), kk("eg"))
            S.add("dve", lambda e: e.reduce_sum(out=v("sg"), in_=v("eg"), axis=AX.X), kk("eg"), kk("sg"))
            S.add("dve", lambda e: e.reciprocal(out=v("sg"), in_=v("sg")), kk("sg"), kk("sg"))
            S.ts(v("oh"), v("lg"), 0.0, None, ALU.is_ge, None, kk("lg"), kk("oh"))
            S.tt(v("le"), LG[:, :, 4:20], beB, ALU.add, LGk + [K("rowp")], kk("le"))
            for g in range(4):
                ohg = sm["oh"][:, :, g:g + 1].to_broadcast([128, NT, 4])
                if g == 0:
                    S.tt(v("sel"), sm["le"][:, :, 0:4], ohg, ALU.mult, kk("le") + kk("oh"), kk("sel"))
                else:
                    S.tt(v("tmp"), sm["le"][:, :, 4 * g:4 * g + 4], ohg, ALU.mult, kk("le") + kk("oh"), kk("tmp"))
                    S.tt(v("sel"), v("sel"), v("tmp"), ALU.add, kk("sel") + kk("tmp"), kk("sel"))
            S.add("dve", lambda e: e.reduce_max(out=v("a"), in_=v("sel"), axis=AX.X), kk("sel"), kk("a"))
            S.tt(v("sel"), v("sel"), B3(v("a")), ALU.subtract, kk("sel") + kk("a"), kk("sel"))
            S.ts(v("eq"), v("sel"), 0.0, None, ALU.is_ge, None, kk("sel"), kk("eq"))
            S.stt(v("sel2"), v("eq"), -1e30, v("sel"), ALU.mult, ALU.add, kk("eq") + kk("sel"), kk("sel2"))
            S.add("dve", lambda e: e.reduce_max(out=v("b"), in_=v("sel2"), axis=AX.X), kk("sel2"), kk("b"))
            S.tt(v("t2"), v("sel"), B3(v("b")), ALU.is_ge, kk("sel") + kk("b"), kk("t2"))
            S.act(v("ex"), v("sel"), AF.Exp, kk("sel"), kk("ex"))
            S.act(v("dd"), v("b"), AF.Exp, kk("b"), kk("dd"))
            S.ts(v("dd"), v("dd"), 1.0, None, ALU.add, None, kk("dd"), kk("dd"))
            S.add("dve", lambda e: e.reciprocal(out=v("dd"), in_=v("dd")), kk("dd"), kk("dd"))
            S.tt(v("wg"), v("ex"), v("t2"), ALU.mult, kk("ex") + kk("t2"), kk("wg"))
            S.tt(v("wg"), v("wg"), B3(v("dd")), ALU.mult, kk("wg") + kk("dd"), kk("wg"))
            S.tt(v("wg"), v("wg"), B3(v("sg")), ALU.mult, kk("wg") + kk("sg"), kk("wg"))
            for g in range(4):
                ohg = sm["oh"][:, :, g:g + 1].to_broadcast([128, NT, 4])
                S.tt(COMB[:, :, 4 * g:4 * g + 4], v("wg"), ohg, ALU.mult, kk("wg") + kk("oh"),
                     [K("COMB", i, g) for i in range(NT)])
            for q4 in range(4):
                pst, pkt = ps_rot()
                for q in range(4):
                    i = q4 * 4 + q
                    S.tr(pst[0:16, q * 128:(q + 1) * 128], COMB[:, i, :], ident,
                         [K("COMB", i, g) for g in range(4)] + [K("c128")], [pkt])
                S.cp(CT[0:16, q4 * 512:(q4 + 1) * 512], pst[0:16, :], [pkt], [K("CT", q4)])
                S.cp(CTh[0:16, q4 * 512:(q4 + 1) * 512], CT[0:16, q4 * 512:(q4 + 1) * 512], [K("CT", q4), K("CThz")],
                     [K("CTh", q4)])
                S.tt(CT[0:16, q4 * 512:(q4 + 1) * 512], CT[0:16, q4 * 512:(q4 + 1) * 512],
                     CTh[0:16, q4 * 512:(q4 + 1) * 512], ALU.subtract, [K("CT", q4), K("CTh", q4)], [K("CT", q4)])
                S.cp(CTl[0:16, q4 * 512:(q4 + 1) * 512], CT[0:16, q4 * 512:(q4 + 1) * 512], [K("CT", q4), K("CTlz")],
                     [K("CTl", q4)])
            st_rt.close()
            S.barrier()
            HID = sb("HID", [128, 8, S_], BF16, stack=st)
            W2S = sb("W2S", [128, 8, D_], BF16, stack=st)
            CB = [sb(f"CB{j}", [128, 512], stack=st) for j in range(2)]
            SL = [sb(f"SL{j}", [128, 512], stack=st) for j in range(2)]
            n = 0
            for g in range(4):
                for el in range(4):
                    ex = 4 * g + el
                    wv, wk1_, slot = load_w(w1_d[l, ex], 8, 256, c0=0, tot=512)
                    _, wk3_, _ = load_w(w3_d[l, ex], 8, 256, slot=slot, c0=256, tot=512)
                    wk = wk1_ + wk3_
                    for tc in range(NTC):
                        sl = slice(tc * TW, (tc + 1) * TW)
                        psC, pkC = ps_rot()
                        S.mm(psC[:, :], ENp[:, ex, :], CTh[:, sl], True, False, [K("ENf"), K("CTh", tc), K("CThz")], [pkC])
                        S.mm(psC[:, :], ENp[:, ex, :], CTl[:, sl], False, True, [K("ENf"), K("CTl", tc), K("CTlz")], [pkC])
                        cb, cbk = CB[n % 2], K("CB", n % 2)
                        S.cp(cb[:], psC[:, :], [pkC], [cbk], eng="act")
                        for fc in range(2):
                            ps1, pk1 = ps_rot()
                            proj_fm(wv, wk, fc * 128, tc, ps1, pk1)
                            ps3, pk3 = ps_rot()
                            proj_fm(wv, wk, 256 + fc * 128, tc, ps3, pk3)
                            s_, sk = SL[(2 * n + fc) % 2], K("SL", (2 * n + fc) % 2)
                            S.act(s_[:], ps1[:, :], AF.Silu, [pk1], [sk])
                            S.tt(s_[:], s_[:], ps3[:, :], ALU.mult, [sk, pk3], [sk])
                            S.tt(HID[:, el * 2 + fc, sl], s_[:], cb[:], ALU.mult, [sk, cbk], [K("HID", el * 2 + fc, tc)])
                        n += 1
                    S.dma(W2S[:, 2 * el:2 * el + 2, :], w2_d[l, ex].rearrange("(fc p) n -> p fc n", p=128), [],
                          [K("W2S", el)], q="pool", arena=True)
                for oc in range(KC):
                    for tc in range(NTC):
                        ps, pk = ps_rot()
                        for j in range(8):
                            S.mm(ps[:, :], W2S[:, j, oc * 128:(oc + 1) * 128], HID[:, j, tc * TW:(tc + 1) * TW],
                                 j == 0, j == 7, [K("W2S", j // 2), K("HID", j, tc)], [pk])
                        S.tt(xT[:, oc, tc * TW:(tc + 1) * TW], xT[:, oc, tc * TW:(tc + 1) * TW], ps[:, :], ALU.add,
                             [K("xT", oc, tc), pk], [K("xT", oc, tc)])
        S.barrier()
        dump(f"x3_{l}", xT[:], ALLX, [128, KC, S_])
        if stop_after == f"M{l}":
            done = True
            break

    if stop_after is None:
        with ExitStack() as st:
            HF = sb("HFo", [128, KC, TW], stack=st)
            OB = [sb(f"OB{j}", [128, D_], stack=st) for j in range(2)]
            nob = [0]

            def final_hook(tc, r):
                sl = slice(tc * TW, (tc + 1) * TW)
                for kc in range(KC):
                    S.stt(HF[:, kc, :], xT[:, kc, sl], col(0, "fin_g", kc), r[:], ALU.mult, ALU.mult,
                          [K("xT", kc, tc), K("rsz", tc % 2), K("colp")], [K("HFo", kc)])
                for q in range(4):
                    ob, obk = OB[nob[0] % 2], K("OB", nob[0] % 2)
                    nob[0] += 1
                    for half in range(2):
                        ps, pk = ps_rot()
                        for j in range(4):
                            S.tr(ps[:, j * 128:(j + 1) * 128], HF[:, half * 4 + j, q * 128:(q + 1) * 128], ident,
                                 [K("HFo", half * 4 + j), K("c128")], [pk])
                        S.cp(ob[:, half * 512:(half + 1) * 512], ps[:, :], [pk], [obk],
                             eng=("act" if half else "dve"))
                    r0 = tc * TW + q * 128
                    i = S.dma(out_d[r0:r0 + 128, :], ob[:], [obk], [K("out", r0)])
                    S.final.append(i)

            rmsnorm_fm(xT, "xT", lambda kc: col(0, "fin_g", kc), None, None, KC, S_, st, hook=final_hook, tag="z")

    S.finalize(es)
    es.close()
    return nc, dump_d


def make_consts():
    c128 = np.zeros((128, 8, 128), np.float32)
    j = np.arange(128)[:, None]
    s = np.arange(128)[None, :]
    c128[:, 0] = np.eye(128)
    c128[:, 1] = 1.0
    c128[:, 2] = (j <= s)
    c128[:, 3] = -1.0 * (j >= s)
    c128[:, 4] = -1.0
    partner = np.where((np.arange(128) % 64) < 32, np.arange(128) + 32, np.arange(128) - 32)
    perm = np.zeros((128, 128), np.float32)
    perm[partner, np.arange(128)] = 1.0
    c128[:, 5] = perm
    c128[:, 6] = NEG * (s < j)
    cm = np.zeros((128, 12, 512), np.float32)
    p = np.arange(128)[:, None]
    f = np.arange(512)[None, :]
    for d in range(4):
        valid = (f - p) > d * 128
        cm[:, d] = valid
        cm[:, 4 + d] = NEG * (~valid)
        cm[:, 8 + d] = NEG * ((d * 128 + p) > f)
    half = 32
    inv = (np.float32(10000.0) ** (-(np.arange(half, dtype=np.float32)) / np.float32(half))).astype(np.float32)
    ang = np.arange(S_, dtype=np.float32)[None, :] * inv[:, None]
    cos = np.cos(ang).astype(np.float32)
    sin = np.sin(ang).astype(np.float32)
    rope = np.zeros((128, 2, S_), np.float32)
    for pp in range(128):
        d = pp % 64
        rope[pp, 0] = cos[d % 32]
        rope[pp, 1] = -sin[d % 32] if d < 32 else sin[d % 32]
    en = np.zeros((16, 16, 128), np.float32)
    for n in range(16):
        en[n, n, :] = 1.0
    gc = np.zeros((128, 3, 16, 8), np.float32)
    for i in range(16):
        own = i // 2
        for n in range(8):
            gc[:, 0, i, n] = 0.0 if n < own else -1e30
            gc[:, 1, i, n] = 1.0 if n < own else 0.0
            gc[:, 2, i, n] = 1.0 if n == own else 0.0
    blkoh = np.zeros((8, S_), np.float32)
    for n in range(8):
        blkoh[n, n * 256:(n + 1) * 256] = 1.0
    return dict(c128=c128.reshape(128, -1), cmask=cm.reshape(128, -1), rope=rope.reshape(128, -1),
                en=en.reshape(16, -1), gatec=gc.reshape(128, -1), blkoh=blkoh)


def colvec(v):
    v = np.asarray(v, np.float32)
    return np.ascontiguousarray(v.reshape(-1, 128).T)


def pack_params(inp):
    colp = np.zeros((128, L_, NCOL), np.float32)
    rowp = np.zeros((128, L_, NROW), np.float32)

    def put(l, name, arr):
        c0, w = COLS[name]
        assert arr.shape == (128, w), (name, arr.shape)
        colp[:, l, c0:c0 + w] = arr

    for l in range(L_):
        put(l, "mix_g", colvec(inp["mix_norm_g"][l]))
        put(l, "xat_g", colvec(inp["xattn_norm_g"][l]))
        put(l, "mem_g", colvec(inp["mem_norm_g"][l]))
        put(l, "ffn_g", colvec(inp["ffn_norm_g"][l]))
        put(l, "grp_g", colvec(inp["group_norm_g"][l]))
        cw = inp["lru_conv_w"][l]
        put(l, "lru_cw", np.concatenate([colvec(cw[k]) for k in range(4)], axis=1).reshape(128, 4, 2)
            .transpose(0, 2, 1).reshape(128, 8))
        put(l, "lru_cb", colvec(inp["lru_conv_b"][l]))
        put(l, "lru_br", colvec(inp["lru_br"][l]))
        put(l, "lru_bi", colvec(inp["lru_bi"][l]))
        put(l, "lru_lam", colvec(inp["lru_lambda"][l]))
        sw = inp["ssm_conv_w"][l]
        put(l, "ssm_cw", np.concatenate([colvec(sw[k]) for k in range(4)], axis=1).reshape(128, 4, 6)
            .transpose(0, 2, 1).reshape(128, 24))
        put(l, "ssm_cb", colvec(inp["ssm_conv_b"][l]))
        put(l, "ssm_d", colvec(np.repeat(inp["ssm_d"][l], 64)))
        put(l, "fin_g", colvec(inp["final_norm_g"]))
        r0, w = ROWS["dt_bias"]
        rowp[:, l, r0:r0 + w] = np.tile(inp["ssm_dt_bias"][l], 16)[None, :]
        r0, w = ROWS["a_log"]
        rowp[:, l, r0:r0 + w] = np.tile(inp["ssm_a_log"][l], 16)[None, :]
        r0, w = ROWS["bg"]
        rowp[:, l, r0:r0 + w] = inp["router_group_b"][l][None, :]
        r0, w = ROWS["be"]
        rowp[:, l, r0:r0 + w] = inp["router_expert_b"][l][None, :]
    wbd = np.zeros((L_, 2, 2, 128, 128), np.float32)
    for l in range(L_):
        for gi, nm in enumerate(("lru_wr", "lru_wi")):
            for c in range(2):
                for hh in range(2):
                    wbd[l, gi, c, hh * 64:(hh + 1) * 64, hh * 64:(hh + 1) * 64] = inp[nm][l, 2 * c + hh]
    router_w = np.concatenate([inp["router_group_w"], inp["router_expert_w"]], axis=2)
    return dict(colp=colp.reshape(128, -1), rowp=rowp.reshape(128, -1), lru_wbd=wbd,
                router_w=np.ascontiguousarray(router_w))


SHARED = ["w_in", "w_out", "xattn_wq", "xattn_wkv", "xattn_wo", "expert_w1", "expert_w3", "expert_w2"]


def make_in_maps(inputs, cores):
    inp = {k: np.asarray(v) for k, v in inputs.items()}
    consts = make_consts()
    packed = pack_params(inp)
    shared = {k: np.ascontiguousarray(inp[k], dtype=np.float32) for k in SHARED}
    maps = []
    for b in cores:
        m = dict(shared)
        m.update(consts)
        m.update(packed)
        m["x"] = np.ascontiguousarray(inp["x"][b])
        m["mem"] = np.ascontiguousarray(inp["mem"][b])
        maps.append(m)
    return maps


_CACHE = {}


def kernel(**inputs):
    if "nc" not in _CACHE:
        _CACHE["nc"] = build_program()[0]
    nc = _CACHE["nc"]
    maps = make_in_maps(inputs, list(range(8)))
    res = run_bass_kernel_spmd(nc, maps, core_ids=list(range(8)))
    return np.stack([r["out"] for r in res.results], axis=0).astype(np.float32)
```

```python
import numpy as np
from contextlib import ExitStack
import concourse.bass as bass
import concourse.mybir as mybir
from concourse.bass_utils import run_bass_kernel_spmd

F32 = mybir.dt.float32
BF16 = mybir.dt.bfloat16
F32R = mybir.dt.float32r
AF = mybir.ActivationFunctionType
ALU = mybir.AluOpType
AX = mybir.AxisListType

S_ = 2048
D_ = 1024
L_ = 2
KC = 8
NTC = 4
TW = 512
NT = 16
NEG = -30000.0
EPS = 1e-6

COLS = {}
_c = 0
for _n, _w in [("mix_g", 8), ("xat_g", 8), ("mem_g", 8), ("ffn_g", 8), ("grp_g", 8),
               ("lru_cw", 8), ("lru_cb", 2), ("lru_br", 2), ("lru_bi", 2), ("lru_lam", 2),
               ("ssm_cw", 24), ("ssm_cb", 6), ("ssm_d", 2), ("fin_g", 8)]:
    COLS[_n] = (_c, _w)
    _c += _w
NCOL = _c
ROWS = {}
_c = 0
for _n, _w in [("dt_bias", 64), ("a_log", 64), ("bg", 4), ("be", 16)]:
    ROWS[_n] = (_c, _w)
    _c += _w
NROW = _c


def _free(ap):
    n = 1
    for d in ap.shape[1:]:
        n *= int(d)
    return n


class Sched:
    ENG = ("pe", "act", "dve", "pool", "sp")
    GATED = ("pe", "act", "dve", "sp")

    def __init__(self, nc):
        self.nc = nc
        self.ops = []
        self.lastw = {}
        self.rd = {}
        self.by_eng = {e: [] for e in self.ENG}
        self.final = []
        self.psn = 0
        self.seg = 0

    def add(self, eng, fn, reads=(), writes=(), dma=False, cost=0.3, arena=False):
        i = len(self.ops)
        reads = list(reads)
        writes = list(writes)
        deps = {}
        for r in reads:
            w = self.lastw.get(r)
            if w is not None:
                deps.setdefault(w, set()).add("raw")
        for w_ in writes:
            w = self.lastw.get(w_)
            if w is not None:
                deps.setdefault(w, set()).add("waw")
            for r in self.rd.get(w_, ()):
                deps.setdefault(r, set()).add("war")
        ws = set(writes)
        for r in reads:
            if r not in ws:
                self.rd.setdefault(r, []).append(i)
        for w_ in writes:
            self.lastw[w_] = i
            self.rd[w_] = []
        deps.pop(i, None)
        fdeps = set()
        for d, kinds in deps.items():
            od = self.ops[d]
            if od["eng"] == eng and not od["dma"] and not dma:
                if eng == "pe":
                    continue
                if "raw" not in kinds:
                    continue
            fdeps.add(d)
        self.ops.append(dict(eng=eng, fn=fn, deps=fdeps, order=set(deps.keys()), dma=dma, signal=dma, token=None,
                             seg=self.seg, cost=cost, arena=arena))
        self.by_eng[eng].append(i)
        return i

    def barrier(self):
        self.seg += 1

    def schedule(self, W=24):
        ops = self.ops
        n = len(ops)
        succ = [[] for _ in range(n)]
        nun = [0] * n
        for i, op in enumerate(ops):
            nun[i] = len(op["order"])
            for d in op["order"]:
                succ[d].append(i)
        ready = [0.0] * n
        fin = [0.0] * n
        done = [False] * n
        head = {e: 0 for e in self.ENG}
        free = {e: 0.0 for e in self.ENG}
        lists = {e: list(self.by_eng[e]) for e in self.ENG}
        new_order = {e: [] for e in self.ENG}
        nseg = self.seg + 1
        rem = [0] * (nseg + 1)
        for i, op in enumerate(ops):
            if op["eng"] in self.GATED:
                rem[op["seg"]] += 1
        import os
        WDEF = {"pe": W, "act": 1, "dve": W}
        WE = {e_: int(os.environ.get("KSCHED_W_" + e_.upper(), WDEF[e_])) for e_ in ("pe", "act", "dve")}
        segbusy = {}
        segend = {}
        cur = 0
        seg_t0 = 0.0
        tmax = 0.0
        nsched = 0
        while nsched < n:
            while cur < nseg and rem[cur] == 0:
                cur += 1
                seg_t0 = tmax
            best = None
            for e in self.ENG:
                lst = lists[e]
                h = head[e]
                while h < len(lst) and done[lst[h]]:
                    h += 1
                head[e] = h
                w = WE.get(e, 1)
                seen = 0
                j = h
                while j < len(lst) and seen < w:
                    i = lst[j]
                    j += 1
                    if done[i]:
                        continue
                    op = ops[i]
                    if e != "pool":
                        if op["seg"] != cur:
                            break
                    elif op["arena"] and op["seg"] > cur:
                        break
                    seen += 1
                    if nun[i] == 0:
                        est = max(free[e], ready[i])
                        if e != "pool" or op["arena"]:
                            est = max(est, seg_t0)
                        key = (est, i)
                        if best is None or key < best[0]:
                            best = (key, e, i)
            if best is None:
                raise RuntimeError("scheduler stuck at seg %d" % cur)
            (est, _), e, i = best
            op = ops[i]
            if op["dma"]:
                free[e] = est + 0.06
                f = est + op["cost"]
            else:
                f = est + op["cost"]
                free[e] = f
            fin[i] = f
            tmax = max(tmax, f)
            if not op["dma"]:
                segbusy[(op["seg"], e)] = segbusy.get((op["seg"], e), 0.0) + op["cost"]
            segend[op["seg"]] = max(segend.get(op["seg"], 0.0), f)
            done[i] = True
            nsched += 1
            new_order[e].append(i)
            if e in self.GATED:
                rem[op["seg"]] -= 1
            for s_ in succ[i]:
                nun[s_] -= 1
                lat = 0.0 if (ops[s_]["eng"] == e and not op["dma"]) else 0.1
                if f + lat > ready[s_]:
                    ready[s_] = f + lat
        self.by_eng = new_order
        self.sim_time = tmax
        import os
        if os.environ.get("KSCHED_DEBUG"):
            print("sched: ops", n, "sim_time_us", round(tmax, 1), flush=True)
            prev = 0.0
            for sg in sorted(segend):
                print("  seg", sg, "dur", round(segend[sg] - prev, 1), {e_: round(segbusy.get((sg, e_), 0.0), 1)
                                                                         for e_ in ("pe", "act", "dve")})
                prev = segend[sg]
        pos = {}
        for e in self.ENG:
            for k, i in enumerate(new_order[e]):
                pos[i] = k
        bar = [set() for _ in range(nseg + 1)]
        for s_ in range(1, nseg + 1):
            for e in ("pe", "act", "dve"):
                last = None
                for i in new_order[e]:
                    if ops[i]["seg"] < s_:
                        last = i
                    else:
                        break
                if last is not None:
                    bar[s_].add(last)
            for i in new_order["sp"]:
                if ops[i]["seg"] == s_ - 1 and ops[i]["dma"]:
                    bar[s_].add(i)
        for e in self.GATED:
            lastseg = 0
            for i in new_order[e]:
                sg = ops[i]["seg"]
                if sg > lastseg:
                    for t in range(lastseg + 1, sg + 1):
                        ops[i]["deps"] |= bar[t]
                    lastseg = sg
        for i in new_order["pool"]:
            if ops[i]["arena"]:
                ops[i]["deps"] |= bar[ops[i]["seg"]]
        for i, op in enumerate(ops):
            keep = set()
            bestp = {}
            for d in op["deps"]:
                if d == i:
                    continue
                od = ops[d]
                if od["dma"]:
                    keep.add(d)
                else:
                    pe_ = od["eng"]
                    if pe_ not in bestp or pos[d] > pos[bestp[pe_]]:
                        bestp[pe_] = d
            keep |= set(bestp.values())
            op["deps"] = keep

    def mm(self, out, lhsT, rhs, start, stop, reads, writes):
        nfree = _free(rhs)
        c = max(nfree, 64) / 2400.0 * (4.0 if rhs.dtype == F32 else 1.0) + 0.004
        return self.add("pe", lambda e: e.matmul(out, lhsT, rhs, start=start, stop=stop), reads, writes, cost=c)

    def tr(self, out, in_, ident, reads, writes):
        return self.add("pe", lambda e: e.transpose(out, in_, ident), reads, writes, cost=0.25)

    def act(self, out, in_, func, reads, writes, bias=None, scale=None, accum=None):
        kw = {}
        if bias is not None:
            kw["bias"] = bias
        if scale is not None:
            kw["scale"] = scale
        if accum is not None:
            kw["accum_out"] = accum
        c = 0.22 + _free(out) / 1400.0
        return self.add("act", lambda e: e.activation(out=out, in_=in_, func=func, **kw), reads, writes, cost=c)

    def _vc(self, out):
        return 0.08 + _free(out) / 960.0

    def tt(self, out, in0, in1, op, reads, writes, eng="dve"):
        return self.add(eng, lambda e: e.tensor_tensor(out=out, in0=in0, in1=in1, op=op), reads, writes,
                        cost=self._vc(out))

    def ts(self, out, in0, s1, s2, op0, op1, reads, writes, eng="dve"):
        if s2 is None:
            return self.add(eng, lambda e: e.tensor_scalar(out, in0, s1, None, op0), reads, writes, cost=self._vc(out))
        return self.add(eng, lambda e: e.tensor_scalar(out, in0, s1, s2, op0, op1), reads, writes, cost=self._vc(out))

    def stt(self, out, in0, scalar, in1, op0, op1, reads, writes, eng="dve"):
        return self.add(eng, lambda e: e.scalar_tensor_tensor(out=out, in0=in0, scalar=scalar, in1=in1,
                                                             op0=op0, op1=op1), reads, writes, cost=self._vc(out))

    def cp(self, out, in_, reads, writes, eng="dve"):
        if eng == "act":
            return self.add("act", lambda e: e.activation(out=out, in_=in_, func=AF.Copy), reads, writes,
                            cost=0.22 + _free(out) / 1400.0)
        return self.add(eng, lambda e: e.tensor_copy(out=out, in_=in_), reads, writes, cost=self._vc(out))

    def recip(self, out, in_, reads, writes):
        return self.add("dve", lambda e: e.reciprocal(out=out, in_=in_), reads, writes, cost=0.1 + _free(out) / 240.0)

    def dma(self, out, in_, reads, writes, q="sp", arena=False):
        nbytes = 128 * _free(out) * 4
        return self.add(q, lambda e: e.dma_start(out=out, in_=in_), reads, writes, dma=True,
                        cost=2.0 + nbytes / 150e3, arena=arena)

    def finalize(self, es):
        nc = self.nc
        self.schedule()
        for op in self.ops:
            for d in op["deps"]:
                self.ops[d]["signal"] = True
        for d in self.final:
            self.ops[d]["signal"] = True
        csem = {e: es.enter_context(nc.semaphore("c_" + e)) for e in ("pe", "act", "dve", "pool")}
        NP = 16
        qsem = {q: [es.enter_context(nc.semaphore(f"q_{q}{k}")) for k in range(NP)] for q in ("sp", "pool")}
        cnt = {e: 0 for e in csem}
        qn = {q: 0 for q in qsem}
        quse = {q: [0] * NP for q in qsem}
        qprev = {q: [None] * NP for q in qsem}
        for i, op in enumerate(self.ops):
            if op["dma"]:
                q = op["eng"]
                k = qn[q] % NP
                qn[q] += 1
                quse[q][k] += 1
                op["token"] = (qsem[q][k], 16 * quse[q][k])
                if qprev[q][k] is not None:
                    op["deps"].add(qprev[q][k])
                qprev[q][k] = i
        for e in csem:
            for i in self.by_eng[e]:
                op = self.ops[i]
                if (not op["dma"]) and op["signal"]:
                    cnt[e] += 1
                    op["token"] = (csem[e], cnt[e])
        ops = self.ops
        final = list(self.final)

        def emit(engname, e):
            waited = {}
            for i in self.by_eng[engname]:
                op = ops[i]
                for d in sorted(op["deps"]):
                    sem, val = ops[d]["token"]
                    if waited.get(sem, 0) < val:
                        e.wait_ge(sem, val)
                        waited[sem] = val
                ins = op["fn"](e)
                if op["signal"]:
                    ins.then_inc(op["token"][0], 16 if op["dma"] else 1)
            if engname == "sp":
                for d in final:
                    sem, val = ops[d]["token"]
                    if waited.get(sem, 0) < val:
                        e.wait_ge(sem, val)
                        waited[sem] = val

        with nc.Block() as block:
            @block.tensor
            def _(e):
                emit("pe", e)

            @block.scalar
            def _(e):
                emit("act", e)

            @block.vector
            def _(e):
                emit("dve", e)

            @block.gpsimd
            def _(e):
                emit("pool", e)

            @block.sync
            def _(e):
                emit("sp", e)


def K(name, *idx):
    return (name,) + idx


def build_program(stop_after=None, dumps=(), n_layers=L_):
    nc = bass.Bass("TRN2", target_bir_lowering=False)
    S = Sched(nc)
    es = ExitStack()
    P = {}

    def din(name, shape):
        P[name] = nc.dram_tensor(name, list(shape), F32, kind="ExternalInput").ap()
        return P[name]

    x_d = din("x", [S_, D_])
    mem_d = din("mem", [256, D_])
    w_in_d = din("w_in", [L_, D_, 3076])
    w_out_d = din("w_out", [L_, D_, D_])
    wq_d = din("xattn_wq", [L_, D_, D_])
    wkv_d = din("xattn_wkv", [L_, D_, 2 * D_])
    wo_d = din("xattn_wo", [L_, D_, D_])
    w1_d = din("expert_w1", [L_, 16, D_, 256])
    w3_d = din("expert_w3", [L_, 16, D_, 256])
    w2_d = din("expert_w2", [L_, 16, 256, D_])
    wrt_d = din("router_w", [L_, D_, 20])
    lrubd_d = din("lru_wbd", [L_, 2, 2, 128, 128])
    colp_d = din("colp", [128, L_ * NCOL])
    rowp_d = din("rowp", [128, L_ * NROW])
    c128_d = din("c128", [128, 8 * 128])
    cmask_d = din("cmask", [128, 12 * 512])
    rope_d = din("rope", [128, 2 * S_])
    en_d = din("en", [16, 16 * 128])
    blkoh_d = din("blkoh", [8, S_])
    gate_c_d = din("gatec", [128, 3 * 128])
    out_d = nc.dram_tensor("out", [S_, D_], F32, kind="ExternalOutput").ap()
    dump_d = {}

    uid = [0]

    def sb(name, shape, dt=F32, stack=None):
        uid[0] += 1
        return (stack or es).enter_context(nc.sbuf_tensor(f"{name}_{uid[0]}", list(shape), dt))

    xT = sb("xT", [128, KC, S_])
    hT = sb("hT", [128, KC, S_], BF16)
    colp = sb("colp_s", [128, L_ * NCOL])
    rowp = sb("rowp_s", [128, L_ * NROW])
    c128 = sb("c128_s", [128, 8 * 128])
    c128b = sb("c128b_s", [128, 2 * 128], BF16)
    W = [sb(f"wslot{i}", [128, KC, 512], BF16) for i in range(3)]
    PS = [es.enter_context(nc.psum_tensor(f"ps{i}", [128, 512], F32)) for i in range(8)]
    wn = [0]

    ident = c128[:, 0:128]
    ones_f = c128[:, 128:256]
    tri_le = c128[:, 256:384]
    negu = c128[:, 384:512]
    negones = c128[:, 512:640]
    perm_f = c128[:, 640:768]
    ssdneg = c128[:, 768:896]
    ones_b = c128b[:, 128:256]
    ident_b = c128b[:, 0:128]

    cst = sb("cst", [128, 4])
    S.add("dve", lambda e: e.memset(cst[:, 0:1], EPS), [], [K("cst0")])
    S.add("dve", lambda e: e.memset(cst[:, 1:2], 1.0), [], [K("cst1")])
    S.add("dve", lambda e: e.memset(cst[:, 2:4], 0.0), [K("cst0"), K("cst1")], [K("cst")])
    S.dma(colp[:], colp_d[:, :], [], [K("colp")])
    S.dma(rowp[:], rowp_d[:, :], [], [K("rowp")])
    S.dma(c128[:], c128_d[:, :], [], [K("c128")])
    S.dma(c128b[:], c128_d[:, 0:256], [], [K("c128b")], q="pool")

    def col(l, name, j=0, n=1):
        c0, w = COLS[name]
        return colp[:, l * NCOL + c0 + j: l * NCOL + c0 + j + n]

    def row(l, name, j=0, n=None):
        c0, w = ROWS[name]
        n = w if n is None else n
        return rowp[:, l * NROW + c0 + j: l * NROW + c0 + j + n]

    def ps_rot():
        k = S.psn % 4
        S.psn += 1
        return PS[k], K("ps", k)

    Wf = [w[:].rearrange("p a b -> p (a b)") for w in W]

    def load_w(src2d, nk, ncols, slot=None, c0=0, tot=None):
        tot = ncols if tot is None else tot
        if slot is None:
            k = wn[0] % 3
            wn[0] += 1
        else:
            k = slot
        v = Wf[k][:, 0:nk * tot].rearrange("p (a b) -> p a b", a=nk)
        step = 2 if nk >= 2 else 1
        allk = [K("w", k, j_, hf_) for j_ in range(4) for hf_ in range(2)]
        for h in range(0, nk, step):
            if nk == 8 and tot == 512:
                halves = [hf_ for hf_ in range(2) if c0 < (hf_ + 1) * 256 and c0 + ncols > hf_ * 256]
                wkeys_ = [K("w", k, h // 2, hf_) for hf_ in halves]
            else:
                wkeys_ = allk
            S.dma(v[:, h:h + step, c0:c0 + ncols],
                  src2d[h * 128:(h + step) * 128, :].rearrange("(kc p) n -> p kc n", p=128),
                  [], wkeys_, q="pool")
        return v, allk, k

    def dump(name, ap, reads, shape):
        if name in dumps:
            d = nc.dram_tensor("dbg_" + name, list(shape), ap.dtype, kind="ExternalOutput").ap()
            dump_d[name] = d
            i = S.dma(d, ap, reads, [K("dbg", name)])
            S.final.append(i)

    def load_fm(dst, src_d, ntile, dkey, st):
        xin = [sb(f"xin{j}", [128, D_], stack=st) for j in range(2)]
        for i in range(ntile):
            b = xin[i % 2]
            S.dma(b[:], src_d[i * 128:(i + 1) * 128, :], [], [K("xin", i % 2)])
            for half in range(2):
                ps, pk = ps_rot()
                for j in range(4):
                    kc = half * 4 + j
                    S.tr(ps[:, j * 128:(j + 1) * 128], b[:, kc * 128:(kc + 1) * 128], ident,
                         [K("xin", i % 2), K("c128")], [pk])
                S.cp(dst[:, half * 4:half * 4 + 4, i * 128:(i + 1) * 128],
                     ps[:].rearrange("p (a b) -> p a b", a=4), [pk],
                     [K(dkey, half * 4 + j, i // 4) for j in range(4)], eng=("dve" if half == 0 else "act"))

    with ExitStack() as st:
        load_fm(xT, x_d, NT, "xT", st)
    S.barrier()
    dump("xT0", xT[:], [K("xT", kc, tc) for kc in range(KC) for tc in range(NTC)], [128, KC, S_])

    def rmsnorm_fm(src, skey, gcol, dst, dkey, nk, T, st, dscale=1.0, hook=None, tag="n"):
        tw = min(T, TW)
        sq = [sb(f"sq_{tag}{j}", [128, nk, tw], BF16, stack=st) for j in range(2)]
        rs = [sb(f"rs_{tag}{j}", [128, tw], stack=st) for j in range(2)]
        for tc in range(T // tw):
            sl = slice(tc * tw, (tc + 1) * tw)
            q = sq[tc % 2]
            for kc in range(nk):
                S.act(q[:, kc, :], src[:, kc, sl], AF.Square, [K(skey, kc, tc)], [K("sq" + tag, tc % 2, kc)])
            ps, pk = ps_rot()
            for kc in range(nk):
                S.mm(ps[:, 0:tw], ones_b, q[:, kc, :], kc == 0, kc == nk - 1,
                     [K("sq" + tag, tc % 2, kc), K("c128b")], [pk])
            r = rs[tc % 2]
            S.act(r[:], ps[:, 0:tw], AF.Sqrt, [pk, K("cst")], [K("rs" + tag, tc % 2)],
                  bias=cst[:, 0:1], scale=1.0 / (nk * 128))
            S.add("dve", (lambda e, r=r: e.reciprocal(out=r[:], in_=r[:])),
                  [K("rs" + tag, tc % 2)], [K("rs" + tag, tc % 2)], cost=0.1 + tw / 240.0)
            if dst is not None:
                for kc in range(nk):
                    S.stt(dst[:, kc, sl], src[:, kc, sl], gcol(kc), r[:], ALU.mult, ALU.mult,
                          [K(skey, kc, tc), K("rs" + tag, tc % 2), K("colp")], [K(dkey, kc, tc)],
                          eng="dve")
            if hook is not None:
                hook(tc, r)

    ALLH = [K("hT", kc, tc) for kc in range(KC) for tc in range(NTC)]

    def hkeys(tc):
        return [K("hT", kc, tc) for kc in range(KC)]

    def proj_fm(wv, wkeys, c0, tc, ps, pk):
        for kc in range(KC):
            S.mm(ps[:, :], wv[:, kc, c0:c0 + 128], hT[:, kc, tc * TW:(tc + 1) * TW], kc == 0, kc == KC - 1,
                 wkeys + [K("hT", kc, tc)], [pk])

    def conv_chunk(ps, pk, tc, xw, xwk, cwcol, cbcol, dst, dkey):
        w_ = xw[tc % 2]
        wk = K(xwk, tc % 2)
        if tc == 0:
            S.add("dve", lambda e: e.memset(w_[:, 0:3], 0.0), [], [wk])
        else:
            S.cp(w_[:, 0:3], xw[(tc - 1) % 2][:, 512:515], [K(xwk, (tc - 1) % 2)], [wk])
        S.cp(w_[:, 3:515], ps[:, :], [pk], [wk], eng="act")
        S.ts(dst, w_[:, 3:515], cwcol(3), cbcol, ALU.mult, ALU.add, [wk, K("colp")], [dkey])
        for k in range(3):
            S.stt(dst, w_[:, k:k + 512], cwcol(k), dst, ALU.mult, ALU.add, [wk, dkey, K("colp")], [dkey])

    def group_out(l, g, YG, st):
        YB = sb("YB", [128, 2, S_], BF16, stack=st)
        rmsnorm_fm(YG, "YG", lambda kc: col(l, "grp_g", 2 * g + kc), YB, "YB", 2, S_, st, tag="g")
        dump(f"yn{l}_{g}", YB[:], [K("YB", c, t) for c in range(2) for t in range(NTC)], [128, 2, S_])
        for half in range(2):
            wv, wk, _ = load_w(w_out_d[l, g * 256:(g + 1) * 256, half * 512:(half + 1) * 512], 2, 512)
            for o4 in range(4):
                oc = half * 4 + o4
                for tc in range(NTC):
                    ps, pk = ps_rot()
                    for kc in range(2):
                        S.mm(ps[:, :], wv[:, kc, o4 * 128:(o4 + 1) * 128], YB[:, kc, tc * TW:(tc + 1) * TW],
                             kc == 0, kc == 1, wk + [K("YB", kc, tc)], [pk])
                    S.tt(xT[:, oc, tc * TW:(tc + 1) * TW], xT[:, oc, tc * TW:(tc + 1) * TW], ps[:, :], ALU.add,
                         [K("xT", oc, tc), pk], [K("xT", oc, tc)])

    ALLX = [K("xT", kc, tc) for kc in range(KC) for tc in range(NTC)]
    done = False
    for l in range(n_layers):
        with ExitStack() as st:
            rmsnorm_fm(xT, "xT", lambda kc: col(l, "mix_g", kc), hT, "hT", KC, S_, st, tag="a")
        S.barrier()
        if l == 0:
            dump("h0", hT[:], ALLH, [128, KC, S_])
        if stop_after == "norm0":
            break

        with ExitStack() as sty, ExitStack() as st:
            YG = sb("YG", [128, 2, S_], stack=sty)
            wv, wk, _ = load_w(w_in_d[l, :, 0:512], 8, 512)
            wbd = sb("wbd", [128, 4, 128], BF16, stack=st)
            S.dma(wbd[:], lrubd_d[l].rearrange("g c k m -> k (g c) m"), [], [K("wbd")], q="pool", arena=True)
            c8 = sb("c8", [128, 2], stack=st)
            dump(f"A_wbd{l}", wbd[:], [K("wbd")], [128, 4, 128])
            cy = sb("cy", [128, 2], stack=st)
            cz = sb("cz", [128, 2], stack=st)
            S.act(cy[:], col(l, "lru_lam", 0, 2), AF.Exp, [K("colp")], [K("cy")], scale=-1.0)
            S.ts(cz[:], cy[:], 2.0, None, ALU.add, None, [K("cy")], [K("cz")])
            S.add("dve", lambda e: e.reciprocal(out=cz[:], in_=cz[:]), [K("cz")], [K("cz")])
            S.tt(cz[:], cz[:], cy[:], ALU.mult, [K("cz"), K("cy")], [K("cz")])
            S.tt(cy[:], cz[:], cz[:], ALU.mult, [K("cz")], [K("cy")])
            S.ts(c8[:], cy[:], 1.0 / 9.0, 1.0 / 7.0, ALU.mult, ALU.add, [K("cy")], [K("c8")])
            for cc_ in (1.0 / 5.0, 1.0 / 3.0, 1.0):
                S.tt(c8[:], c8[:], cy[:], ALU.mult, [K("c8"), K("cy")], [K("c8")])
                S.ts(c8[:], c8[:], cc_, None, ALU.add, None, [K("c8")], [K("c8")])
            S.tt(c8[:], c8[:], cz[:], ALU.mult, [K("c8"), K("cz")], [K("c8")])
            S.ts(c8[:], c8[:], -16.0, None, ALU.mult, None, [K("c8")], [K("c8")])
            xw = [sb(f"xw{j}", [128, 515], stack=st) for j in range(2)]
            XC = sb("XC", [128, S_], stack=st)
            XCB = sb("XCB", [128, S_], BF16, stack=st)
            GG = sb("GG", [128, S_], stack=st)
            RA = sb("RA", [128, S_], stack=st)
            IU = sb("IU", [128, S_], stack=st)
            T2 = sb("T2", [128, S_], stack=st)
            XX = sb("XX", [128, S_], stack=st)
            for c in range(2):
                for tc in range(NTC):
                    sl = slice(tc * TW, (tc + 1) * TW)
                    ps, pk = ps_rot()
                    proj_fm(wv, wk, c * 128, tc, ps, pk)
                    conv_chunk(ps, pk, tc, xw, "xw", lambda k: col(l, "lru_cw", c * 4 + k), col(l, "lru_cb", c),
                               XC[:, sl], K("XC", tc))
                    ps, pk = ps_rot()
                    proj_fm(wv, wk, 256 + c * 128, tc, ps, pk)
                    S.cp(GG[:, sl], ps[:, :], [pk], [K("GG", tc)], eng="act")
                XCk = [K("XC", t) for t in range(NTC)]
                GGk = [K("GG", t) for t in range(NTC)]
                S.tt(T2[:], GG[:], GG[:], ALU.mult, GGk, [K("T2")])
                S.ts(T2[:], T2[:], 0.044715, 1.0, ALU.mult, ALU.add, [K("T2")], [K("T2")])
                S.tt(T2[:], T2[:], GG[:], ALU.mult, [K("T2")] + GGk, [K("T2")])
                S.act(T2[:], T2[:], AF.Sigmoid, [K("T2")], [K("T2")], scale=1.5957691216)
                S.tt(GG[:], GG[:], T2[:], ALU.mult, [K("T2")] + GGk, GGk)
                S.cp(XCB[:], XC[:], XCk, [K("XCB")], eng="act")
                for tc in range(NTC):
                    sl = slice(tc * TW, (tc + 1) * TW)
                    ps, pk = ps_rot()
                    S.mm(ps[:, :], wbd[:, c, :], XCB[:, sl], True, True, [K("wbd"), K("XCB")], [pk])
                    S.act(RA[:, sl], ps[:, :], AF.Sigmoid, [pk, K("colp")], [K("RA", tc)], bias=col(l, "lru_br", c))
                    ps, pk = ps_rot()
                    S.mm(ps[:, :], wbd[:, 2 + c, :], XCB[:, sl], True, True, [K("wbd"), K("XCB")], [pk])
                    S.act(IU[:, sl], ps[:, :], AF.Sigmoid, [pk, K("colp")], [K("IU", tc)], bias=col(l, "lru_bi", c))
                RAk = [K("RA", t) for t in range(NTC)]
                IUk = [K("IU", t) for t in range(NTC)]
                if c == 1:
                    dump(f"A_r{l}", RA[:], RAk, [128, S_])
                    dump(f"A_i{l}", IU[:], IUk, [128, S_])
                    dump(f"A_xcb{l}", XCB[:], [K("XCB")], [128, S_])
                    dump(f"A_xc{l}", XC[:], XCk, [128, S_])
                S.ts(XX[:], RA[:], c8[:, c:c + 1], 2.0, ALU.mult, ALU.mult, RAk + [K("c8")], [K("XX")])
                S.ts(T2[:], XX[:], 1.0 / 5040.0, 1.0 / 720.0, ALU.mult, ALU.add, [K("XX")], [K("T2")])
                for cc_ in (1.0 / 120.0, 1.0 / 24.0, 1.0 / 6.0, 0.5, 1.0):
                    S.tt(T2[:], T2[:], XX[:], ALU.mult, [K("T2"), K("XX")], [K("T2")])
                    S.ts(T2[:], T2[:], cc_, None, ALU.add, None, [K("T2")], [K("T2")])
                S.stt(T2[:], T2[:], -1.0, XX[:], ALU.mult, ALU.mult, [K("T2"), K("XX")], [K("T2")])
                S.act(T2[:], T2[:], AF.Sqrt, [K("T2")], [K("T2")])
                S.act(RA[:], XX[:], AF.Exp, [K("XX")], RAk, scale=0.5)
                S.tt(IU[:], IU[:], XC[:], ALU.mult, IUk + XCk, IUk)
                S.tt(IU[:], IU[:], T2[:], ALU.mult, IUk + [K("T2")], IUk)
                S.add("dve", lambda e: e.tensor_tensor_scan(out=XC[:], data0=RA[:], data1=IU[:], initial=0.0,
                                                            op0=ALU.mult, op1=ALU.add), RAk + IUk, XCk, cost=2.6)
                S.tt(YG[:, c, :], XC[:], GG[:], ALU.mult, XCk + GGk, [K("YG", c, t) for t in range(NTC)])
            for nm_, t_ in (("XC", XC), ("GG", GG), ("RA", RA), ("IU", IU), ("T2", T2)):
                dump(f"A_{nm_}{l}", t_[:], [K(nm_, t) for t in range(NTC)] + [K(nm_)], [128, S_])
            dump(f"ya{l}", YG[:], [K("YG", c, t) for c in range(2) for t in range(NTC)], [128, 2, S_])
            st.close()
            S.barrier()
            group_out(l, 0, YG, sty)
        S.barrier()
        if stop_after == f"A{l}":
            done = True
            break

        with ExitStack() as sty, ExitStack() as st:
            YG = sb("YG", [128, 2, S_], stack=sty)
            QB = sb("QB", [128, 2, S_], BF16, stack=st)
            KZ = sb("KZ", [128, 4, S_], BF16, stack=st)
            S.add("dve", lambda e: e.memset(KZ[:], 0.0), [], [K("KZ", h_, t_) for h_ in range(4) for t_ in range(NTC)], cost=4.5)
            VB = sb("VB", [128, NT, 256], BF16, stack=st)
            M01 = sb("M01", [128, 4, 512], stack=st)
            NM = sb("NM", [128, 4, 512], BF16, stack=st)
            S.dma(M01[:], cmask_d[:, 0:2048].rearrange("p (a b) -> p a b", a=4), [], [K("M01")])
            S.dma(NM[:], cmask_d[:, 2048:4096].rearrange("p (a b) -> p a b", a=4), [], [K("NM")], q="pool", arena=True)
            wv, wk, _ = load_w(w_in_d[l, :, 512:1024], 8, 512)
            wv2, wk2, _ = load_w(w_in_d[l, :, 1024:1280], 8, 256)
            for c2 in range(2):
                for tc in range(NTC):
                    sl = slice(tc * TW, (tc + 1) * TW)
                    ps, pk = ps_rot()
                    proj_fm(wv, wk, c2 * 128, tc, ps, pk)
                    S.act(QB[:, c2, sl], ps[:, :], AF.Copy, [pk], [K("QB", c2, tc)], scale=0.125)
                    ps, pk = ps_rot()
                    proj_fm(wv, wk, 256 + c2 * 128, tc, ps, pk)
                    S.cp(KZ[0:64, 2 * c2, sl], ps[0:64, :], [pk], [K("KZ", 2 * c2, tc)])
                    S.cp(KZ[64:128, 2 * c2 + 1, sl], ps[64:128, :], [pk], [K("KZ", 2 * c2 + 1, tc)], eng="act")
            for i in range(NT):
                ps, pk = ps_rot()
                for kc in range(KC):
                    S.mm(ps[:, 0:256], hT[:, kc, i * 128:(i + 1) * 128], wv2[:, kc, :], kc == 0, kc == KC - 1,
                         wk2 + [K("hT", kc, i // 4)], [pk])
                S.cp(VB[:, i, :], ps[:, 0:256], [pk], [K("VB", i)], eng=("dve" if i % 2 else "act"))
            Eb = [sb(f"Eb{j}", [128, 512], stack=st) for j in range(2)]
            SPb = [sb(f"SPb{j}", [128, 512], F32R, stack=st) for j in range(3)]
            Wb = [sb(f"Wb{j}", [128, 512], BF16, stack=st) for j in range(2)]
            Rb = [sb(f"Rb{j}", [128, 512], F32R, stack=st) for j in range(2)]
            NUr = sb("NUr", [128, 2, 128], F32R, stack=st)
            S.cp(NUr[:, 0, :], negu, [K("c128")], [K("NUr")])
            S.cp(NUr[:, 1, :], negones, [K("c128")], [K("NUr")])
            items = []
            grp = 0
            for h in range(4):
                for c in range(NTC):
                    nb = 4 * c + 4
                    for idx, b in enumerate(range(nb - 1, -1, -1)):
                        items.append(dict(h=h, c=c, idx=idx, b=b, grp=grp, last=(b == 0)))
                    grp += 1
            for i_, itm in enumerate(items):
                itm["i"] = i_
                itm["pr"] = slice((itm["h"] % 2) * 64, (itm["h"] % 2) * 64 + 64)
                itm["c2"] = itm["h"] // 2
                itm["d"] = itm["b"] - 4 * itm["c"]
                itm["ksl"] = slice(itm["b"] * 128, (itm["b"] + 1) * 128)
                itm["qsl"] = slice(itm["c"] * TW, (itm["c"] + 1) * TW)
                itm["qk"] = [K("KZ", itm["h"], itm["b"] // 4), K("QB", itm["c2"], itm["c"])]

            def sb_s1(m):
                i = m["i"]
                pr, c2 = m["pr"], m["c2"]
                psZ, pkZ = ps_rot()
                S.mm(psZ[:, :], KZ[:, m["h"], m["ksl"]], QB[:, c2, m["qsl"]], True, True, m["qk"], [pkZ])
                E, Ek = Eb[i % 2], K("Eb", i % 2)
                SP, SPk = SPb[i % 3], K("SPb", i % 3)
                S.act(E[:], psZ[:, :], AF.Exp, [pkZ], [Ek])
                S.act(SP[:], E[:], AF.Ln, [Ek, K("cst")], [SPk], bias=cst[:, 1:2])
                if m["d"] >= 0:
                    S.tt(SP[:], SP[:].bitcast(F32), M01[:, m["d"], :], ALU.mult, [SPk, K("M01")], [SPk])

            def sb_s2(m):
                i = m["i"]
                pr, c2, d = m["pr"], m["c2"], m["d"]
                SP, SPk = SPb[i % 3], K("SPb", i % 3)
                Wt, Wk_ = Wb[i % 2], K("Wb", i % 2)
                R, Rk = Rb[m["grp"] % 2], K("Rb", m["grp"] % 2)
                psA, pkA = ps_rot()
                has_r = m["idx"] > 0
                S.mm(psA[:, :], KZ[:, m["h"], m["ksl"]], QB[:, c2, m["qsl"]], True, False, m["qk"], [pkA])
                S.mm(psA[:, :], NUr[:, 0, :], SP[:], False, (not has_r) and d < 0, [SPk, K("NUr")], [pkA])
                if has_r:
                    S.mm(psA[:, :], NUr[:, 1, :], R[:], False, d < 0, [Rk, K("NUr")], [pkA])
                if d >= 0:
                    S.mm(psA[:, :], ident_b, NM[:, d, :], False, True, [K("NM"), K("c128b")], [pkA])
                S.act(Wt[:], psA[:, :], AF.Exp, [pkA], [Wk_])
                if not m["last"]:
                    if m["idx"] == 0:
                        S.cp(R[:], SP[:].bitcast(F32), [SPk], [Rk])
                    else:
                        S.tt(R[:], R[:].bitcast(F32), SP[:].bitcast(F32), ALU.add, [Rk, SPk], [Rk])

            def sb_s3(m):
                i = m["i"]
                pr, c2, h = m["pr"], m["c2"], m["h"]
                Wt, Wk_ = Wb[i % 2], K("Wb", i % 2)
                psO, pkO = PS[4 + m["grp"] % 2], K("psO", m["grp"] % 2)
                S.mm(psO[:, :], VB[:, m["b"], c2 * 128:(c2 + 1) * 128], Wt[:], m["idx"] == 0, m["last"],
                     [K("VB", m["b"]), Wk_], [pkO])
                if m["last"]:
                    S.cp(YG[pr, c2, m["qsl"]], psO[pr, :], [pkO], [K("YG", c2, m["c"])], eng="act")

            n_it = len(items)
            for r in range(-2, n_it):
                if 0 <= r + 2 < n_it:
                    sb_s1(items[r + 2])
                if 0 <= r + 1 < n_it:
                    sb_s2(items[r + 1])
                if 0 <= r < n_it:
                    sb_s3(items[r])
            dump(f"yb{l}", YG[:], [K("YG", c, t) for c in range(2) for t in range(NTC)], [128, 2, S_])
            st.close()
            S.barrier()
            group_out(l, 1, YG, sty)
        S.barrier()
        if stop_after == f"B{l}":
            done = True
            break

        with ExitStack() as sty, ExitStack() as st:
            YG = sb("YG", [128, 2, S_], stack=sty)
            XS = sb("XS", [128, 2, S_], stack=st)
            BTb = sb("BTb", [128, 2, S_], BF16, stack=st)
            CTb = sb("CTb", [128, 2, S_], BF16, stack=st)
            BTM = sb("BTM", [128, NT, 256], BF16, stack=st)
            XTM = sb("XTM", [128, NT, 256], BF16, stack=st)
            wdt = sb("wdt", [128, KC, 4], BF16, stack=st)
            stc = ExitStack()
            xw = [sb(f"xwc{j}", [128, 515], stack=stc) for j in range(2)]
            CV = [sb(f"CV{j}", [128, 512], stack=stc) for j in range(2)]
            BF = [sb(f"BF{j}", [128, 512], stack=stc) for j in range(2)]
            S.dma(wdt[:], w_in_d[l, :, 2304:2308].rearrange("(kc p) n -> p kc n", p=128), [], [K("wdt")], q="pool", arena=True)
            wv1, wk1, _ = load_w(w_in_d[l, :, 1280:1792], 8, 512)
            wv2, wk2, _ = load_w(w_in_d[l, :, 1792:2304], 8, 512)
            n_cv = 0
            for j in range(6):
                if j < 2:
                    wv, wk, c0 = wv1, wk1, 256 + j * 128
                elif j < 4:
                    wv, wk, c0 = wv2, wk2, (j - 2) * 128
                else:
                    wv, wk, c0 = wv2, wk2, 256 + (j - 4) * 128
                for tc in range(NTC):
                    sl = slice(tc * TW, (tc + 1) * TW)
                    ps, pk = ps_rot()
                    proj_fm(wv, wk, c0, tc, ps, pk)
                    cv, cvk = CV[n_cv % 2], K("CV", n_cv % 2)
                    conv_chunk(ps, pk, tc, xw, "xwc", lambda k: col(l, "ssm_cw", j * 4 + k), col(l, "ssm_cb", j),
                               cv[:], cvk)
                    if j < 2:
                        S.act(XS[:, j, sl], cv[:], AF.Silu, [cvk], [K("XS", j, tc)])
                        ps, pk = ps_rot()
                        for q in range(4):
                            S.tr(ps[:, q * 128:(q + 1) * 128], XS[:, j, tc * TW + q * 128: tc * TW + (q + 1) * 128],
                                 ident, [K("XS", j, tc), K("c128")], [pk])
                        S.cp(XTM[:, tc * 4:tc * 4 + 4, j * 128:(j + 1) * 128],
                             ps[:].rearrange("p (a b) -> p a b", a=4), [pk], [K("XTM", tc, j)])
                    elif j < 4:
                        g = j - 2
                        bf, bfk = BF[n_cv % 2], K("BF", n_cv % 2)
                        S.act(bf[:], cv[:], AF.Silu, [cvk], [bfk])
                        S.cp(BTb[:, g, sl], bf[:], [bfk], [K("BTb", g, tc)])
                        ps, pk = ps_rot()
                        for q in range(4):
                            S.tr(ps[:, q * 128:(q + 1) * 128], bf[:, q * 128:(q + 1) * 128], ident,
                                 [bfk, K("c128")], [pk])
                        S.cp(BTM[:, tc * 4:tc * 4 + 4, g * 128:(g + 1) * 128],
                             ps[:].rearrange("p (a b) -> p a b", a=4), [pk], [K("BTM", tc, g)])
                    else:
                        S.act(CTb[:, j - 4, sl], cv[:], AF.Silu, [cvk], [K("CTb", j - 4, tc)])
                    n_cv += 1
            stc.close()
            S.barrier()
            DT = sb("DT", [128, 64], stack=st)
            AN = sb("AN", [128, 64], stack=st)
            AC = sb("AC", [128, 64], stack=st)
            ACS = sb("ACS", [128, 64], stack=st)
            TOT = sb("TOT", [128, 64], stack=st)
            DEC = sb("DEC", [128, 64], stack=st)
            ETOT = sb("ETOT", [128, 64], stack=st)
            DTD = sb("DTD", [128, 64], stack=st)
            psd, pkd = ps_rot()
            for i in range(NT):
                for kc in range(KC):
                    S.mm(psd[:, i * 4:(i + 1) * 4], hT[:, kc, i * 128:(i + 1) * 128], wdt[:, kc, :], kc == 0,
                         kc == KC - 1, [K("wdt"), K("hT", kc, i // 4)], [pkd])
            S.tt(DT[:], psd[:, 0:64], row(l, "dt_bias"), ALU.add, [pkd, K("rowp")], [K("DT")])
            S.act(DT[:], DT[:], AF.Exp, [K("DT")], [K("DT")])
            S.act(DT[:], DT[:], AF.Ln, [K("DT"), K("cst")], [K("DT")], bias=cst[:, 1:2])
            S.act(AN[:], row(l, "a_log"), AF.Exp, [K("rowp")], [K("AN")])
            S.ts(AN[:], AN[:], -1.0, None, ALU.mult, None, [K("AN")], [K("AN")])
            S.tt(AC[:], DT[:], AN[:], ALU.mult, [K("DT"), K("AN")], [K("AC")])
            ps, pk = ps_rot()
            S.mm(ps[:, 0:64], tri_le, AC[:], True, True, [K("AC"), K("c128")], [pk])
            S.cp(ACS[:], ps[:, 0:64], [pk], [K("ACS")])
            ps, pk = ps_rot()
            S.mm(ps[:, 0:64], ones_f, AC[:], True, True, [K("AC"), K("c128")], [pk])
            S.cp(TOT[:], ps[:, 0:64], [pk], [K("TOT")])
            S.tt(DEC[:], TOT[:], ACS[:], ALU.subtract, [K("TOT"), K("ACS")], [K("DEC")])
            S.act(DEC[:], DEC[:], AF.Exp, [K("DEC")], [K("DEC")])
            S.act(ETOT[:], TOT[:], AF.Exp, [K("TOT")], [K("ETOT")])
            S.tt(DTD[:], DT[:], DEC[:], ALU.mult, [K("DT"), K("DEC")], [K("DTD")])
            ST = sb("ST", [128, 4, 64], stack=st)
            S.add("dve", lambda e: e.memset(ST[:], 0.0), [], [K("ST", h) for h in range(4)])
            Rm = [sb(f"Rm{j}", [128, 128], stack=st) for j in range(2)]
            ARG = [sb(f"ARG{j}", [128, 128], stack=st) for j in range(2)]
            E1 = [sb(f"E1{j}", [128, 128], stack=st) for j in range(2)]
            GL = [sb(f"GL{j}", [128, 128], BF16, stack=st) for j in range(2)]
            CTp = [sb(f"CTp{j}", [128, 128], BF16, stack=st) for j in range(2)]
            XCp = [[sb(f"XCp{a}{j}", [128, 128], BF16, stack=st) for j in range(2)] for a in range(2)]
            STp = sb("STp", [128, 4, 128], BF16, stack=st)
            for a in range(2):
                for j in range(2):
                    S.add("dve", (lambda e, t_=XCp[a][j]: e.memset(t_[:], 0.0)), [], [K("XCp", a, j)])
            S.add("dve", lambda e: e.memset(STp[:], 0.0), [], [K("STp", h) for h in range(4)])
            XDh = [sb(f"XDh{j}", [128, 64], BF16, stack=st) for j in range(2)]
            n = 0
            for ci in range(NT):
                csl = slice(ci * 128, (ci + 1) * 128)
                tcx = ci // 4
                for g in range(2):
                    psG, pkG = ps_rot()
                    S.mm(psG[:, 0:128], BTb[:, g, csl], CTb[:, g, csl], True, True,
                         [K("BTb", g, tcx), K("CTb", g, tcx)], [pkG])
                    psY, pkY = PS[4 + (ci * 2 + g) % 2], K("psO", (ci * 2 + g) % 2)
                    for hh in range(2):
                        h = 2 * g + hh
                        pr = slice(hh * 64, hh * 64 + 64)
                        cidx = ci * 4 + h
                        k2 = n % 2
                        S.ts(Rm[k2][:], tri_le, AC[:, cidx:cidx + 1], None, ALU.mult, None,
                             [K("AC"), K("c128")], [K("Rm", k2)])
                        ps1, pk1 = ps_rot()
                        S.mm(ps1[:, 0:128], ones_f, Rm[k2][:], True, True, [K("Rm", k2), K("c128")], [pk1])
                        S.stt(ARG[k2][:], ps1[:, 0:128], ACS[:, cidx:cidx + 1], ssdneg, ALU.subtract, ALU.add,
                              [pk1, K("ACS"), K("c128")], [K("ARG", k2)])
                        S.act(ARG[k2][:], ARG[k2][:], AF.Exp, [K("ARG", k2)], [K("ARG", k2)])
                        S.tt(GL[k2][:], ARG[k2][:], psG[:, 0:128], ALU.mult, [K("ARG", k2), pkG], [K("GL", k2)])
                        S.act(E1[k2][:], ps1[:, 0:128], AF.Exp, [pk1], [K("E1", k2)])
                        S.tt(CTp[k2][:], CTb[:, g, csl], E1[k2][:], ALU.mult, [K("CTb", g, tcx), K("E1", k2)],
                             [K("CTp", k2)])
                        rot = (ci * 2 + g) % 2
                        xcp = XCp[hh][rot]
                        S.ts(xcp[:, hh * 64:(hh + 1) * 64], XTM[:, ci, h * 64:(h + 1) * 64], DT[:, cidx:cidx + 1], None,
                             ALU.mult, None, [K("XTM", tcx, g), K("DT")], [K("XCp", hh, rot)])
                        S.mm(psY[:, 0:128], xcp[:], GL[k2][:], hh == 0, False, [K("XCp", hh, rot), K("GL", k2)], [pkY])
                        S.mm(psY[:, 0:128], STp[:, h, :], CTp[k2][:], False, hh == 1, [K("STp", h), K("CTp", k2)], [pkY])
                        S.ts(XDh[k2][:], XTM[:, ci, h * 64:(h + 1) * 64], DTD[:, cidx:cidx + 1], None, ALU.mult, None,
                             [K("XTM", tcx, g), K("DTD")], [K("XDh", k2)])
                        psS, pkS = ps_rot()
                        S.mm(psS[:, 0:64], BTM[:, ci, g * 128:(g + 1) * 128], XDh[k2][:], True, True,
                             [K("BTM", tcx, g), K("XDh", k2)], [pkS])
                        S.stt(ST[:, h, :], ST[:, h, :], ETOT[:, cidx:cidx + 1], psS[:, 0:64], ALU.mult, ALU.add,
                              [K("ST", h), K("ETOT"), pkS], [K("ST", h)])
                        S.cp(STp[:, h, hh * 64:(hh + 1) * 64], ST[:, h, :], [K("ST", h)], [K("STp", h)], eng="act")
                        n += 1
                    S.cp(YG[:, g, csl], psY[:, 0:128], [pkY], [K("YG", g, tcx)], eng="act")
            ARG = [sb(f"ZT{j}", [128, 512], stack=st) for j in range(1)]
            for j in range(2):
                YGk = [K("YG", j, t) for t in range(NTC)]
                S.stt(YG[:, j, :], XS[:, j, :], col(l, "ssm_d", j), YG[:, j, :], ALU.mult, ALU.add,
                      [K("XS", j, t) for t in range(NTC)] + YGk + [K("colp")], YGk)
                for tc in range(NTC):
                    sl = slice(tc * TW, (tc + 1) * TW)
                    ps, pk = ps_rot()
                    proj_fm(wv1, wk1, j * 128, tc, ps, pk)
                    zt, ztk = ARG[0], K("ARGz", 0)
                    S.act(zt[:], ps[:, :], AF.Silu, [pk], [ztk])
                    S.tt(YG[:, j, sl], YG[:, j, sl], zt[:], ALU.mult, [K("YG", j, tc), ztk], [K("YG", j, tc)])
            dump(f"yc{l}", YG[:], [K("YG", c, t) for c in range(2) for t in range(NTC)], [128, 2, S_])
            st.close()
            S.barrier()
            group_out(l, 2, YG, sty)
        S.barrier()
        if stop_after == f"C{l}":
            done = True
            break

        with ExitStack() as sty, ExitStack() as st:
            YG = sb("YG", [128, 2, S_], stack=sty)
            QA = sb("QA", [128, 4, S_], BF16, stack=st)
            KA = sb("KA", [128, 4, S_], BF16, stack=st)
            allqa = [K("QA", h_, t_) for h_ in range(4) for t_ in range(NTC)]
            allka = [K("KA", h_, t_) for h_ in range(4) for t_ in range(NTC)]
            S.add("dve", lambda e: e.memset(QA[:], 0.0), [], allqa, cost=4.5)
            S.add("dve", lambda e: e.memset(KA[:], 0.0), [], allka, cost=4.5)
            for h_ in range(4):
                o_ = 64 if h_ % 2 == 0 else 0
                S.dma(KA[o_:o_ + 8, h_, :], blkoh_d[:, :], allka, [K("KAoh", h_)], q="pool", arena=True)
            VB = sb("VB", [128, NT, 256], BF16, stack=st)
            CAUS = sb("CAUS", [128, 4, 512], BF16, stack=st)
            S.dma(CAUS[:], cmask_d[:, 4096:6144].rearrange("p (a b) -> p a b", a=4), [], [K("CAUS")], q="pool", arena=True)
            GC = sb("GC", [128, 3, 128], stack=st)
            S.dma(GC[:], gate_c_d[:, :].rearrange("p (a b) -> p a b", a=3), [], [K("GC")])
            KM = sb("KM", [128, 2, 8], stack=st)
            KMb = sb("KMb", [128, 2, 8], BF16, stack=st)
            wv, wk, _ = load_w(w_in_d[l, :, 2308:2820], 8, 512)
            wv2, wk2, _ = load_w(w_in_d[l, :, 2820:3076], 8, 256)
            str_ = ExitStack()
            ROPEb = [sb(f"ROPE{j}", [128, 2, TW], stack=str_) for j in range(1)]
            RAW = [sb(f"RAW{j}", [128, 512], stack=str_) for j in range(2)]
            TMP = [sb(f"TMPr{j}", [128, 512], stack=str_) for j in range(2)]
            KF = [sb(f"KF{j}", [128, 512], stack=str_) for j in range(2)]
            nr = 0
            for c2 in range(2):
                for tc in range(NTC):
                    sl = slice(tc * TW, (tc + 1) * TW)
                    rj = 0
                    ROPE = ROPEb[rj]
                    rsl = slice(0, TW)
                    S.dma(ROPE[:, 0, :], rope_d[:, tc * TW:(tc + 1) * TW], [], [K("ROPE", rj, 0)])
                    S.dma(ROPE[:, 1, :], rope_d[:, S_ + tc * TW:S_ + (tc + 1) * TW], [], [K("ROPE", rj, 1)])
                    for which in range(2):
                        ps, pk = ps_rot()
                        proj_fm(wv, wk, which * 256 + c2 * 128, tc, ps, pk)
                        raw, rawk = RAW[nr % 2], K("RAW", nr % 2)
                        tmp, tmpk = TMP[nr % 2], K("TMPr", nr % 2)
                        S.cp(raw[:], ps[:, :], [pk], [rawk], eng="act")
                        psw, pkw = ps_rot()
                        S.mm(psw[:, :], perm_f, raw[:], True, True, [rawk, K("c128")], [pkw])
                        S.tt(tmp[:], psw[:, :], ROPE[:, 1, rsl], ALU.mult, [pkw, K("ROPE", rj, 1)], [tmpk])
                        kf = KF[nr % 2]
                        dst, dk = kf[:], K("KF", nr % 2)
                        S.tt(dst, raw[:], ROPE[:, 0, rsl], ALU.mult, [rawk, K("ROPE", rj, 0)], [dk])
                        S.tt(dst, dst, tmp[:], ALU.add, [dk, tmpk], [dk])
                        if which == 0:
                            S.act(QA[0:64, 2 * c2, sl], kf[0:64, :], AF.Copy, [dk], [K("QA", 2 * c2, tc)], scale=0.125)
                            S.act(QA[64:128, 2 * c2 + 1, sl], kf[64:128, :], AF.Copy, [dk], [K("QA", 2 * c2 + 1, tc)],
                                  scale=0.125)
                        else:
                            S.cp(KA[0:64, 2 * c2, sl], kf[0:64, :], [dk], [K("KA", 2 * c2, tc)], eng="act")
                            S.cp(KA[64:128, 2 * c2 + 1, sl], kf[64:128, :], [dk], [K("KA", 2 * c2 + 1, tc)], eng="act")
                            for hb in range(2):
                                nblk = tc * 2 + hb
                                S.add("dve", (lambda e, o=KM[:, c2, nblk:nblk + 1], i_=kf[:, hb * 256:(hb + 1) * 256]:
                                              e.reduce_sum(out=o, in_=i_, axis=AX.X)), [dk], [K("KM", c2, nblk)])
                        nr += 1
            for i in range(NT):
                ps, pk = ps_rot()
                for kc in range(KC):
                    S.mm(ps[:, 0:256], hT[:, kc, i * 128:(i + 1) * 128], wv2[:, kc, :], kc == 0, kc == KC - 1,
                         wk2 + [K("hT", kc, i // 4)], [pk])
                S.cp(VB[:, i, :], ps[:, 0:256], [pk], [K("VB", i)], eng=("dve" if i % 2 else "act"))
            S.cp(KMb[:], KM[:], [K("KM", c2_, nb_) for c2_ in range(2) for nb_ in range(8)], [K("KMb")])
            str_.close()
            S.barrier()
            PTb = [sb(f"PTb{j}", [128, 512], BF16, stack=st) for j in range(3)]
            RD = [sb(f"RD{j}", [128, 512], stack=st) for j in range(2)]
            GT = sb("GT", [128, 128], stack=st)
            BT_ = sb("BIASTM", [128, 128], stack=st)
            MX = [sb(f"MX{j}", [128, 8], stack=st) for j in range(2)]
            for h in range(4):
                hh, c2 = h % 2, h // 2
                pr = slice(hh * 64, hh * 64 + 64)
                o_ = 64 if hh == 0 else 0
                psg, pkg = ps_rot()
                for i in range(NT):
                    S.mm(psg[:, i * 8:(i + 1) * 8], QA[pr, h, i * 128:(i + 1) * 128], KMb[pr, c2, :], True, True,
                         [K("QA", h, i // 4), K("KMb")], [pkg])
                S.stt(GT[:], psg[:, 0:128], 8.0 / 256.0, GC[:, 0, :], ALU.mult, ALU.add, [pkg, K("GC")], [K("GT")])
                for i in range(NT):
                    mx, mxk = MX[i % 2], K("MX", i % 2)
                    S.add("dve", (lambda e, o=mx[:], i_=GT[:, i * 8:(i + 1) * 8]: e.max(out=o, in_=i_)),
                          [K("GT")], [mxk])
                    bsl = BT_[:, i * 8:(i + 1) * 8]
                    S.ts(bsl, GT[:, i * 8:(i + 1) * 8], mx[:, 2:3], None, ALU.is_ge, None, [K("GT"), mxk],
                         [K("BIASTM", i)])
                    S.tt(bsl, bsl, GC[:, 1, i * 8:(i + 1) * 8], ALU.mult, [K("BIASTM", i), K("GC")], [K("BIASTM", i)])
                    S.tt(bsl, bsl, GC[:, 2, i * 8:(i + 1) * 8], ALU.add, [K("BIASTM", i), K("GC")], [K("BIASTM", i)])
                    S.ts(bsl, bsl, -1.0, -NEG, ALU.add, ALU.mult, [K("BIASTM", i)], [K("BIASTM", i)])
                for q4 in range(4):
                    pst, pkt = ps_rot()
                    for q in range(4):
                        i = q4 * 4 + q
                        S.tr(pst[0:8, q * 128:(q + 1) * 128], BT_[:, i * 8:(i + 1) * 8], ident,
                             [K("BIASTM", i), K("c128")], [pkt])
                    S.cp(QA[o_:o_ + 8, h, q4 * 512:(q4 + 1) * 512], pst[0:8, :], [pkt], [K("QAsel", h, q4)])
            its = []
            grp = 0
            for h in range(4):
                for c in range(NTC):
                    nkt = 4 * c + 4
                    for kt in range(nkt):
                        its.append(dict(h=h, c=c, kt=kt, nkt=nkt, grp=grp, i=len(its)))
                    grp += 1

            def m_s1(m):
                h, c, kt = m["h"], m["c"], m["kt"]
                d = kt - 4 * c
                qsl = slice(c * TW, (c + 1) * TW)
                ksl = slice(kt * 128, (kt + 1) * 128)
                psS, pkS = ps_rot()
                S.mm(psS[:, :], KA[:, h, ksl], QA[:, h, qsl], True, d < 0,
                     [K("KA", h, kt // 4), K("KAoh", h), K("QA", h, c), K("QAsel", h, c)], [pkS])
                if d >= 0:
                    S.mm(psS[:, :], ident_b, CAUS[:, d, :], False, True, [K("CAUS"), K("c128b")], [pkS])
                pt, ptk = PTb[m["i"] % 3], K("PTb", m["i"] % 3)
                S.act(pt[:], psS[:, :], AF.Exp, [pkS], [ptk])

            def m_s2(m):
                h, c, kt, nkt = m["h"], m["c"], m["kt"], m["nkt"]
                hh, c2 = h % 2, h // 2
                pr = slice(hh * 64, hh * 64 + 64)
                qsl = slice(c * TW, (c + 1) * TW)
                g2 = m["grp"] % 2
                psO, pkO = PS[4 + g2], K("psO", g2)
                psD, pkD = PS[6 + g2], K("psD", g2)
                pt, ptk = PTb[m["i"] % 3], K("PTb", m["i"] % 3)
                S.mm(psO[:, :], VB[:, kt, c2 * 128:(c2 + 1) * 128], pt[:], kt == 0, kt == nkt - 1,
                     [K("VB", kt), ptk], [pkO])
                S.mm(psD[:, :], ones_b, pt[:], kt == 0, kt == nkt - 1, [ptk, K("c128b")], [pkD])
                if kt == nkt - 1:
                    rd, rdk = RD[g2], K("RD", g2)
                    S.add("dve", (lambda e, o=rd[pr, :], i_=psD[pr, :]: e.reciprocal(out=o, in_=i_)), [pkD], [rdk], cost=2.2)
                    S.tt(YG[pr, c2, qsl], psO[pr, :], rd[pr, :], ALU.mult, [pkO, rdk], [K("YG", c2, c)])

            n_ = len(its)
            for r in range(-1, n_):
                if r + 1 < n_:
                    m_s1(its[r + 1])
                if r >= 0:
                    m_s2(its[r])
            dump(f"yd{l}", YG[:], [K("YG", c, t) for c in range(2) for t in range(NTC)], [128, 2, S_])
            st.close()
            S.barrier()
            group_out(l, 3, YG, sty)
        S.barrier()
        dump(f"x1_{l}", xT[:], ALLX, [128, KC, S_])
        if stop_after == f"D{l}":
            done = True
            break

        with ExitStack() as st:
            with ExitStack() as st2:
                rmsnorm_fm(xT, "xT", lambda kc: col(l, "xat_g", kc), hT, "hT", KC, S_, st2, tag="x")
            S.barrier()
            memT = sb("memT", [128, KC, 256], stack=st)
            MTb = sb("MTb", [128, KC, 256], BF16, stack=st)
            with ExitStack() as st2:
                load_fm(memT, mem_d, 2, "memT", st2)
                rmsnorm_fm(memT, "memT", lambda kc: col(l, "mem_g", kc), MTb, "MTb", KC, 256, st2, tag="m")
            S.barrier()
            KTb = sb("KTb", [128, KC, 256], BF16, stack=st)
            VX = sb("VX", [128, 2, D_], BF16, stack=st)
            QX = sb("QX", [128, KC, S_], BF16, stack=st)
            MTk = [K("MTb", kc, 0) for kc in range(KC)]
            for half in range(2):
                wv, wk, _ = load_w(wkv_d[l, :, half * 512:(half + 1) * 512], 8, 512)
                for o4 in range(4):
                    oc = half * 4 + o4
                    ps, pk = ps_rot()
                    for kc in range(KC):
                        S.mm(ps[:, 0:256], wv[:, kc, o4 * 128:(o4 + 1) * 128], MTb[:, kc, :], kc == 0, kc == KC - 1,
                             wk + [K("MTb", kc, 0)], [pk])
                    S.cp(KTb[:, oc, :], ps[:, 0:256], [pk], [K("KTb", oc)])
            for half in range(2):
                wv, wk, _ = load_w(wkv_d[l, :, 1024 + half * 512:1024 + (half + 1) * 512], 8, 512)
                for mt in range(2):
                    ps, pk = ps_rot()
                    for kc in range(KC):
                        S.mm(ps[:, :], MTb[:, kc, mt * 128:(mt + 1) * 128], wv[:, kc, :], kc == 0, kc == KC - 1,
                             wk + [K("MTb", kc, 0)], [pk])
                    S.cp(VX[:, mt, half * 512:(half + 1) * 512], ps[:, :], [pk], [K("VX", mt, half)], eng="act")
            for half in range(2):
                wv, wk, _ = load_w(wq_d[l, :, half * 512:(half + 1) * 512], 8, 512)
                for o4 in range(4):
                    oc = half * 4 + o4
                    for tc in range(NTC):
                        ps, pk = ps_rot()
                        proj_fm(wv, wk, o4 * 128, tc, ps, pk)
                        S.act(QX[:, oc, tc * TW:(tc + 1) * TW], ps[:, :], AF.Copy, [pk], [K("QX", oc, tc)],
                              scale=1.0 / 16.0)
            dump(f"X_MTb{l}", MTb[:], MTk, [128, KC, 256])
            dump(f"X_KTb{l}", KTb[:], [K("KTb", oc) for oc in range(KC)], [128, KC, 256])
            dump(f"X_QX{l}", QX[:], [K("QX", oc, tc) for oc in range(KC) for tc in range(NTC)], [128, KC, S_])
            dump(f"X_VX{l}", VX[:], [K("VX", mt, hf) for mt in range(2) for hf in range(2)], [128, 2, D_])
            PT = [sb(f"PTx{j}", [128, 512], BF16, stack=st) for j in range(4)]
            RDx = [sb(f"RDx{j}", [128, 512], stack=st) for j in range(2)]
            it = 0
            for hd in range(4):
                for tc in range(NTC):
                    sl = slice(tc * TW, (tc + 1) * TW)
                    pts = []
                    for mt in range(2):
                        ps, pk = ps_rot()
                        for j in range(2):
                            S.mm(ps[:, :], KTb[:, 2 * hd + j, mt * 128:(mt + 1) * 128], QX[:, 2 * hd + j, sl],
                                 j == 0, j == 1, [K("KTb", 2 * hd + j), K("QX", 2 * hd + j, tc)], [pk])
                        pt, ptk = PT[it % 4], K("PTx", it % 4)
                        S.act(pt[:], ps[:, :], AF.Exp, [pk], [ptk])
                        pts.append((pt, ptk))
                        it += 1
                    psD, pkD = ps_rot()
                    for mt in range(2):
                        S.mm(psD[:, :], ones_b, pts[mt][0][:], mt == 0, mt == 1, [pts[mt][1], K("c128b")], [pkD])
                    rd, rdk = RDx[(hd * NTC + tc) % 2], K("RDx", (hd * NTC + tc) % 2)
                    S.add("dve", (lambda e, o=rd[:], i_=psD[:, :]: e.reciprocal(out=o, in_=i_)), [pkD], [rdk], cost=2.2)
                    for j in range(2):
                        oc = 2 * hd + j
                        ps, pk = ps_rot()
                        for mt in range(2):
                            S.mm(ps[:, :], VX[:, mt, oc * 128:(oc + 1) * 128], pts[mt][0][:], mt == 0, mt == 1,
                                 [K("VX", mt, oc // 4), pts[mt][1]], [pk])
                        S.tt(hT[:, oc, sl], ps[:, :], rd[:], ALU.mult, [pk, rdk], [K("hT", oc, tc)])
            dump(f"X_OX{l}", hT[:], ALLH, [128, KC, S_])
            for half in range(2):
                wv, wk, _ = load_w(wo_d[l, :, half * 512:(half + 1) * 512], 8, 512)
                for o4 in range(4):
                    oc = half * 4 + o4
                    for tc in range(NTC):
                        ps, pk = ps_rot()
                        proj_fm(wv, wk, o4 * 128, tc, ps, pk)
                        S.tt(xT[:, oc, tc * TW:(tc + 1) * TW], xT[:, oc, tc * TW:(tc + 1) * TW], ps[:, :], ALU.add,
                             [K("xT", oc, tc), pk], [K("xT", oc, tc)])
        S.barrier()
        dump(f"x2_{l}", xT[:], ALLX, [128, KC, S_])
        if stop_after == f"X{l}":
            done = True
            break

        with ExitStack() as st:
            ENp = sb("ENp", [128, 16, 128], BF16, stack=st)
            S.add("dve", lambda e: e.memset(ENp[:], 0.0), [], [K("ENpz")])
            S.dma(ENp[0:16, :, :], en_d[:, :].rearrange("p (a b) -> p a b", a=16), [K("ENpz")], [K("ENf")], q="pool",
                  arena=True)
            CTh = sb("CTh", [128, S_], BF16, stack=st)
            CTl = sb("CTl", [128, S_], BF16, stack=st)
            S.add("dve", lambda e: e.memset(CTh[:], 0.0), [], [K("CThz")])
            S.add("dve", lambda e: e.memset(CTl[:], 0.0), [], [K("CTlz")])
            st_rt = ExitStack()
            WR = sb("WR", [128, KC, 20], stack=st_rt)
            S.dma(WR[:], wrt_d[l].rearrange("(kc p) n -> p kc n", p=128), [], [K("WR")])
            LG = sb("LG", [128, NT, 20], stack=st_rt)
            st_hf = ExitStack()
            HF = sb("HF", [128, KC, TW], stack=st_hf)

            def router_hook(tc, r):
                sl = slice(tc * TW, (tc + 1) * TW)
                for kc in range(KC):
                    S.stt(HF[:, kc, :], xT[:, kc, sl], col(l, "ffn_g", kc), r[:], ALU.mult, ALU.mult,
                          [K("xT", kc, tc), K("rsf", tc % 2), K("colp")], [K("HF", kc)])
                ps, pk = ps_rot()
                for q in range(4):
                    for kc in range(KC):
                        S.mm(ps[:, q * 20:(q + 1) * 20], HF[:, kc, q * 128:(q + 1) * 128], WR[:, kc, :], kc == 0,
                             kc == KC - 1, [K("HF", kc), K("WR")], [pk])
                S.cp(LG[:, tc * 4:tc * 4 + 4, :], ps[:, 0:80].rearrange("p (a b) -> p a b", a=4), [pk],
                     [K("LG", tc)])

            with ExitStack() as st2:
                rmsnorm_fm(xT, "xT", lambda kc: col(l, "ffn_g", kc), hT, "hT", KC, S_, st2, hook=router_hook, tag="f")
            st_hf.close()
            S.barrier()
            COMB = sb("COMB", [128, NT, 16], stack=st_rt)
            CT = sb("CT", [16, S_], stack=st_rt)
            sm = {nm: sb("rt_" + nm, [128, NT, w_] if w_ > 1 else [128, NT], stack=st_rt) for nm, w_ in
                  [("lg", 4), ("m", 1), ("eg", 4), ("sg", 1), ("oh", 4), ("le", 16), ("sel", 4), ("tmp", 4),
                   ("a", 1), ("eq", 4), ("sel2", 4), ("b", 1), ("t2", 4), ("ex", 4), ("dd", 1), ("wg", 4)]}

            def v(nm):
                return sm[nm][:]

            def kk(nm):
                return [K("rt", nm)]

            def B3(ap2, w_=4):
                return ap2.unsqueeze(2).to_broadcast([128, NT, w_])

            LGk = [K("LG", t) for t in range(NTC)]
            bgB = row(l, "bg").unsqueeze(1).to_broadcast([128, NT, 4])
            beB = row(l, "be").unsqueeze(1).to_broadcast([128, NT, 16])
            S.tt(v("lg"), LG[:, :, 0:4], bgB, ALU.add, LGk + [K("rowp")], kk("lg"))
            S.add("dve", lambda e: e.reduce_max(out=v("m"), in_=v("lg"), axis=AX.X), kk("lg"), kk("m"))
            S.tt(v("lg"), v("lg"), B3(v("m")), ALU.subtract, kk("lg") + kk("m"), kk("lg"))
            S.act(v("eg"), v("lg"), AF.Exp, kk("lg"), kk("eg"))
            S.add("dve", lambda e: e.reduce_sum(out=v("sg"), in_=v("eg"), axis=AX.X), kk("eg"), kk("sg"))
            S.add("dve", lambda e: e.reciprocal(out=v("sg"), in_=v("sg")), kk("sg"), kk("sg"))
            S.ts(v("oh"), v("lg"), 0.0, None, ALU.is_ge, None, kk("lg"), kk("oh"))
            S.tt(v("le"), LG[:, :, 4:20], beB, ALU.add, LGk + [K("rowp")], kk("le"))
            for g in range(4):
                ohg = sm["oh"][:, :, g:g + 1].to_broadcast([128, NT, 4])
                if g == 0:
                    S.tt(v("sel"), sm["le"][:, :, 0:4], ohg, ALU.mult, kk("le") + kk("oh"), kk("sel"))
                else:
                    S.tt(v("tmp"), sm["le"][:, :, 4 * g:4 * g + 4], ohg, ALU.mult, kk("le") + kk("oh"), kk("tmp"))
                    S.tt(v("sel"), v("sel"), v("tmp"), ALU.add, kk("sel") + kk("tmp"), kk("sel"))
            S.add("dve", lambda e: e.reduce_max(out=v("a"), in_=v("sel"), axis=AX.X), kk("sel"), kk("a"))
            S.tt(v("sel"), v("sel"), B3(v("a")), ALU.subtract, kk("sel") + kk("a"), kk("sel"))
            S.ts(v("eq"), v("sel"), 0.0, None, ALU.is_ge, None, kk("sel"), kk("eq"))
            S.stt(v("sel2"), v("eq"), -1e30, v("sel"), ALU.mult, ALU.add, kk("eq") + kk("sel"), kk("sel2"))
            S.add("dve", lambda e: e.reduce_max(out=v("b"), in_=v("sel2"), axis=AX.X), kk("sel2"), kk("b"))
            S.tt(v("t2"), v("sel"), B3(v("b")), ALU.is_ge, kk("sel") + kk("b"), kk("t2"))
            S.act(v("ex"), v("sel"), AF.Exp, kk("sel"), kk("ex"))
            S.act(v("dd"), v("b"), AF.Exp, kk("b"), kk("dd"))
            S.ts(v("dd"), v("dd"), 1.0, None, ALU.add, None, kk("dd"), kk("dd"))
            S.add("dve", lambda e: e.reciprocal(out=v("dd"), in_=v("dd")), kk("dd"), kk("dd"))
            S.tt(v("wg"), v("ex"), v("t2"), ALU.mult, kk("ex") + kk("t2"), kk("wg"))
            S.tt(v("wg"), v("wg"), B3(v("dd")), ALU.mult, kk("wg") + kk("dd"), kk("wg"))
            S.tt(v("wg"), v("wg"), B3(v("sg")), ALU.mult, kk("wg") + kk("sg"), kk("wg"))
            for g in range(4):
                ohg = sm["oh"][:, :, g:g + 1].to_broadcast([128, NT, 4])
                S.tt(COMB[:, :, 4 * g:4 * g + 4], v("wg"), ohg, ALU.mult, kk("wg") + kk("oh"),
                     [K("COMB", i, g) for i in range(NT)])
            for q4 in range(4):
                pst, pkt = ps_rot()
                for q in range(4):
                    i = q4 * 4 + q
                    S.tr(pst[0:16, q * 128:(q + 1) * 128], COMB[:, i, :], ident,
                         [K("COMB", i, g) for g in range(4)] + [K("c128")], [pkt])
                S.cp(CT[0:16, q4 * 512:(q4 + 1) * 512], pst[0:16, :], [pkt], [K("CT", q4)])
                S.cp(CTh[0:16, q4 * 512:(q4 + 1) * 512], CT[0:16, q4 * 512:(q4 + 1) * 512], [K("CT", q4), K("CThz")],
                     [K("CTh", q4)])
                S.tt(CT[0:16, q4 * 512:(q4 + 1) * 512], CT[0:16, q4 * 512:(q4 + 1) * 512],
                     CTh[0:16, q4 * 512:(q4 + 1) * 512], ALU.subtract, [K("CT", q4), K("CTh", q4)], [K("CT", q4)])
                S.cp(CTl[0:16, q4 * 512:(q4 + 1) * 512], CT[0:16, q4 * 512:(q4 + 1) * 512], [K("CT", q4), K("CTlz")],
                     [K("CTl", q4)])
            st_rt.close()
            S.barrier()
            HID = sb("HID", [128, 8, S_], BF16, stack=st)
            W2S = sb("W2S", [128, 8, D_], BF16, stack=st)
            CB = [sb(f"CB{j}", [128, 512], stack=st) for j in range(2)]
            SL = [sb(f"SL{j}", [128, 512], stack=st) for j in range(2)]
            n = 0
            for g in range(4):
                for el in range(4):
                    ex = 4 * g + el
                    wv, wk1_, slot = load_w(w1_d[l, ex], 8, 256, c0=0, tot=512)
                    _, wk3_, _ = load_w(w3_d[l, ex], 8, 256, slot=slot, c0=256, tot=512)
                    wk = wk1_ + wk3_
                    for tc in range(NTC):
                        sl = slice(tc * TW, (tc + 1) * TW)
                        psC, pkC = ps_rot()
                        S.mm(psC[:, :], ENp[:, ex, :], CTh[:, sl], True, False, [K("ENf"), K("CTh", tc), K("CThz")], [pkC])
                        S.mm(psC[:, :], ENp[:, ex, :], CTl[:, sl], False, True, [K("ENf"), K("CTl", tc), K("CTlz")], [pkC])
                        cb, cbk = CB[n % 2], K("CB", n % 2)
                        S.cp(cb[:], psC[:, :], [pkC], [cbk], eng="act")
                        for fc in range(2):
                            ps1, pk1 = ps_rot()
                            proj_fm(wv, wk, fc * 128, tc, ps1, pk1)
                            ps3, pk3 = ps_rot()
                            proj_fm(wv, wk, 256 + fc * 128, tc, ps3, pk3)
                            s_, sk = SL[(2 * n + fc) % 2], K("SL", (2 * n + fc) % 2)
                            S.act(s_[:], ps1[:, :], AF.Silu, [pk1], [sk])
                            S.tt(s_[:], s_[:], ps3[:, :], ALU.mult, [sk, pk3], [sk])
                            S.tt(HID[:, el * 2 + fc, sl], s_[:], cb[:], ALU.mult, [sk, cbk], [K("HID", el * 2 + fc, tc)])
                        n += 1
                    S.dma(W2S[:, 2 * el:2 * el + 2, :], w2_d[l, ex].rearrange("(fc p) n -> p fc n", p=128), [],
                          [K("W2S", el)], q="pool", arena=True)
                for oc in range(KC):
                    for tc in range(NTC):
                        ps, pk = ps_rot()
                        for j in range(8):
                            S.mm(ps[:, :], W2S[:, j, oc * 128:(oc + 1) * 128], HID[:, j, tc * TW:(tc + 1) * TW],
                                 j == 0, j == 7, [K("W2S", j // 2), K("HID", j, tc)], [pk])
                        S.tt(xT[:, oc, tc * TW:(tc + 1) * TW], xT[:, oc, tc * TW:(tc + 1) * TW], ps[:, :], ALU.add,
                             [K("xT", oc, tc), pk], [K("xT", oc, tc)])
        S.barrier()
        dump(f"x3_{l}", xT[:], ALLX, [128, KC, S_])
        if stop_after == f"M{l}":
            done = True
            break

    if stop_after is None:
        with ExitStack() as st:
            HF = sb("HFo", [128, KC, TW], stack=st)
            OB = [sb(f"OB{j}", [128, D_], stack=st) for j in range(2)]
            nob = [0]

            def final_hook(tc, r):
                sl = slice(tc * TW, (tc + 1) * TW)
                for kc in range(KC):
                    S.stt(HF[:, kc, :], xT[:, kc, sl], col(0, "fin_g", kc), r[:], ALU.mult, ALU.mult,
                          [K("xT", kc, tc), K("rsz", tc % 2), K("colp")], [K("HFo", kc)])
                for q in range(4):
                    ob, obk = OB[nob[0] % 2], K("OB", nob[0] % 2)
                    nob[0] += 1
                    for half in range(2):
                        ps, pk = ps_rot()
                        for j in range(4):
                            S.tr(ps[:, j * 128:(j + 1) * 128], HF[:, half * 4 + j, q * 128:(q + 1) * 128], ident,
                                 [K("HFo", half * 4 + j), K("c128")], [pk])
                        S.cp(ob[:, half * 512:(half + 1) * 512], ps[:, :], [pk], [obk],
                             eng=("act" if half else "dve"))
                    r0 = tc * TW + q * 128
                    i = S.dma(out_d[r0:r0 + 128, :], ob[:], [obk], [K("out", r0)])
                    S.final.append(i)

            rmsnorm_fm(xT, "xT", lambda kc: col(0, "fin_g", kc), None, None, KC, S_, st, hook=final_hook, tag="z")

    S.finalize(es)
    es.close()
    return nc, dump_d


def make_consts():
    c128 = np.zeros((128, 8, 128), np.float32)
    j = np.arange(128)[:, None]
    s = np.arange(128)[None, :]
    c128[:, 0] = np.eye(128)
    c128[:, 1] = 1.0
    c128[:, 2] = (j <= s)
    c128[:, 3] = -1.0 * (j >= s)
    c128[:, 4] = -1.0
    partner = np.where((np.arange(128) % 64) < 32, np.arange(128) + 32, np.arange(128) - 32)
    perm = np.zeros((128, 128), np.float32)
    perm[partner, np.arange(128)] = 1.0
    c128[:, 5] = perm
    c128[:, 6] = NEG * (s < j)
    cm = np.zeros((128, 12, 512), np.float32)
    p = np.arange(128)[:, None]
    f = np.arange(512)[None, :]
    for d in range(4):
        valid = (f - p) > d * 128
        cm[:, d] = valid
        cm[:, 4 + d] = NEG * (~valid)
        cm[:, 8 + d] = NEG * ((d * 128 + p) > f)
    half = 32
    inv = (np.float32(10000.0) ** (-(np.arange(half, dtype=np.float32)) / np.float32(half))).astype(np.float32)
    ang = np.arange(S_, dtype=np.float32)[None, :] * inv[:, None]
    cos = np.cos(ang).astype(np.float32)
    sin = np.sin(ang).astype(np.float32)
    rope = np.zeros((128, 2, S_), np.float32)
    for pp in range(128):
        d = pp % 64
        rope[pp, 0] = cos[d % 32]
        rope[pp, 1] = -sin[d % 32] if d < 32 else sin[d % 32]
    en = np.zeros((16, 16, 128), np.float32)
    for n in range(16):
        en[n, n, :] = 1.0
    gc = np.zeros((128, 3, 16, 8), np.float32)
    for i in range(16):
        own = i // 2
        for n in range(8):
            gc[:, 0, i, n] = 0.0 if n < own else -1e30
            gc[:, 1, i, n] = 1.0 if n < own else 0.0
            gc[:, 2, i, n] = 1.0 if n == own else 0.0
    blkoh = np.zeros((8, S_), np.float32)
    for n in range(8):
        blkoh[n, n * 256:(n + 1) * 256] = 1.0
    return dict(c128=c128.reshape(128, -1), cmask=cm.reshape(128, -1), rope=rope.reshape(128, -1),
                en=en.reshape(16, -1), gatec=gc.reshape(128, -1), blkoh=blkoh)


def colvec(v):
    v = np.asarray(v, np.float32)
    return np.ascontiguousarray(v.reshape(-1, 128).T)


def pack_params(inp):
    colp = np.zeros((128, L_, NCOL), np.float32)
    rowp = np.zeros((128, L_, NROW), np.float32)

    def put(l, name, arr):
        c0, w = COLS[name]
        assert arr.shape == (128, w), (name, arr.shape)
        colp[:, l, c0:c0 + w] = arr

    for l in range(L_):
        put(l, "mix_g", colvec(inp["mix_norm_g"][l]))
        put(l, "xat_g", colvec(inp["xattn_norm_g"][l]))
        put(l, "mem_g", colvec(inp["mem_norm_g"][l]))
        put(l, "ffn_g", colvec(inp["ffn_norm_g"][l]))
        put(l, "grp_g", colvec(inp["group_norm_g"][l]))
        cw = inp["lru_conv_w"][l]
        put(l, "lru_cw", np.concatenate([colvec(cw[k]) for k in range(4)], axis=1).reshape(128, 4, 2)
            .transpose(0, 2, 1).reshape(128, 8))
        put(l, "lru_cb", colvec(inp["lru_conv_b"][l]))
        put(l, "lru_br", colvec(inp["lru_br"][l]))
        put(l, "lru_bi", colvec(inp["lru_bi"][l]))
        put(l, "lru_lam", colvec(inp["lru_lambda"][l]))
        sw = inp["ssm_conv_w"][l]
        put(l, "ssm_cw", np.concatenate([colvec(sw[k]) for k in range(4)], axis=1).reshape(128, 4, 6)
            .transpose(0, 2, 1).reshape(128, 24))
        put(l, "ssm_cb", colvec(inp["ssm_conv_b"][l]))
        put(l, "ssm_d", colvec(np.repeat(inp["ssm_d"][l], 64)))
        put(l, "fin_g", colvec(inp["final_norm_g"]))
        r0, w = ROWS["dt_bias"]
        rowp[:, l, r0:r0 + w] = np.tile(inp["ssm_dt_bias"][l], 16)[None, :]
        r0, w = ROWS["a_log"]
        rowp[:, l, r0:r0 + w] = np.tile(inp["ssm_a_log"][l], 16)[None, :]
        r0, w = ROWS["bg"]
        rowp[:, l, r0:r0 + w] = inp["router_group_b"][l][None, :]
        r0, w = ROWS["be"]
        rowp[:, l, r0:r0 + w] = inp["router_expert_b"][l][None, :]
    wbd = np.zeros((L_, 2, 2, 128, 128), np.float32)
    for l in range(L_):
        for gi, nm in enumerate(("lru_wr", "lru_wi")):
            for c in range(2):
                for hh in range(2):
                    wbd[l, gi, c, hh * 64:(hh + 1) * 64, hh * 64:(hh + 1) * 64] = inp[nm][l, 2 * c + hh]
    router_w = np.concatenate([inp["router_group_w"], inp["router_expert_w"]], axis=2)
    return dict(colp=colp.reshape(128, -1), rowp=rowp.reshape(128, -1), lru_wbd=wbd,
                router_w=np.ascontiguousarray(router_w))


SHARED = ["w_in", "w_out", "xattn_wq", "xattn_wkv", "xattn_wo", "expert_w1", "expert_w3", "expert_w2"]


def make_in_maps(inputs, cores):
    inp = {k: np.asarray(v) for k, v in inputs.items()}
    consts = make_consts()
    packed = pack_params(inp)
    shared = {k: np.ascontiguousarray(inp[k], dtype=np.float32) for k in SHARED}
    maps = []
    for b in cores:
        m = dict(shared)
        m.update(consts)
        m.update(packed)
        m["x"] = np.ascontiguousarray(inp["x"][b])
        m["mem"] = np.ascontiguousarray(inp["mem"][b])
        maps.append(m)
    return maps


_CACHE = {}


def kernel(**inputs):
    if "nc" not in _CACHE:
        _CACHE["nc"] = build_program()[0]
    nc = _CACHE["nc"]
    maps = make_in_maps(inputs, list(range(8)))
    res = run_bass_kernel_spmd(nc, maps, core_ids=list(range(8)))
    return np.stack([r["out"] for r in res.results], axis=0).astype(np.float32)
```

```python
import numpy as np
from contextlib import ExitStack
import concourse.bass as bass
import concourse.mybir as mybir
from concourse.bass_utils import run_bass_kernel_spmd

F32 = mybir.dt.float32
BF16 = mybir.dt.bfloat16
F32R = mybir.dt.float32r
AF = mybir.ActivationFunctionType
ALU = mybir.AluOpType
AX = mybir.AxisListType

S_ = 2048
D_ = 1024
L_ = 2
KC = 8
NTC = 4
TW = 512
NT = 16
NEG = -30000.0
EPS = 1e-6

COLS = {}
_c = 0
for _n, _w in [("mix_g", 8), ("xat_g", 8), ("mem_g", 8), ("ffn_g", 8), ("grp_g", 8),
               ("lru_cw", 8), ("lru_cb", 2), ("lru_br", 2), ("lru_bi", 2), ("lru_lam", 2),
               ("ssm_cw", 24), ("ssm_cb", 6), ("ssm_d", 2), ("fin_g", 8)]:
    COLS[_n] = (_c, _w)
    _c += _w
NCOL = _c
ROWS = {}
_c = 0
for _n, _w in [("dt_bias", 64), ("a_log", 64), ("bg", 4), ("be", 16)]:
    ROWS[_n] = (_c, _w)
    _c += _w
NROW = _c


def _free(ap):
    n = 1
    for d in ap.shape[1:]:
        n *= int(d)
    return n


class Sched:
    ENG = ("pe", "act", "dve", "pool", "sp")
    GATED = ("pe", "act", "dve", "sp")

    def __init__(self, nc):
        self.nc = nc
        self.ops = []
        self.lastw = {}
        self.rd = {}
        self.by_eng = {e: [] for e in self.ENG}
        self.final = []
        self.psn = 0
        self.seg = 0

    def add(self, eng, fn, reads=(), writes=(), dma=False, cost=0.3, arena=False):
        i = len(self.ops)
        reads = list(reads)
        writes = list(writes)
        deps = {}
        for r in reads:
            w = self.lastw.get(r)
            if w is not None:
                deps.setdefault(w, set()).add("raw")
        for w_ in writes:
            w = self.lastw.get(w_)
            if w is not None:
                deps.setdefault(w, set()).add("waw")
            for r in self.rd.get(w_, ()):
                deps.setdefault(r, set()).add("war")
        ws = set(writes)
        for r in reads:
            if r not in ws:
                self.rd.setdefault(r, []).append(i)
        for w_ in writes:
            self.lastw[w_] = i
            self.rd[w_] = []
        deps.pop(i, None)
        fdeps = set()
        for d, kinds in deps.items():
            od = self.ops[d]
            if od["eng"] == eng and not od["dma"] and not dma:
                if eng == "pe":
                    continue
                if "raw" not in kinds:
                    continue
            fdeps.add(d)
        self.ops.append(dict(eng=eng, fn=fn, deps=fdeps, order=set(deps.keys()), dma=dma, signal=dma, token=None,
                             seg=self.seg, cost=cost, arena=arena))
        self.by_eng[eng].append(i)
        return i

    def barrier(self):
        self.seg += 1

    def schedule(self, W=24):
        ops = self.ops
        n = len(ops)
        succ = [[] for _ in range(n)]
        nun = [0] * n
        for i, op in enumerate(ops):
            nun[i] = len(op["order"])
            for d in op["order"]:
                succ[d].append(i)
        ready = [0.0] * n
        fin = [0.0] * n
        done = [False] * n
        head = {e: 0 for e in self.ENG}
        free = {e: 0.0 for e in self.ENG}
        lists = {e: list(self.by_eng[e]) for e in self.ENG}
        new_order = {e: [] for e in self.ENG}
        nseg = self.seg + 1
        rem = [0] * (nseg + 1)
        for i, op in enumerate(ops):
            if op["eng"] in self.GATED:
                rem[op["seg"]] += 1
        import os
        WDEF = {"pe": W, "act": 1, "dve": W}
        WE = {e_: int(os.environ.get("KSCHED_W_" + e_.upper(), WDEF[e_])) for e_ in ("pe", "act", "dve")}
        segbusy = {}
        segend = {}
        cur = 0
        seg_t0 = 0.0
        tmax = 0.0
        nsched = 0
        while nsched < n:
            while cur < nseg and rem[cur] == 0:
                cur += 1
                seg_t0 = tmax
            best = None
            for e in self.ENG:
                lst = lists[e]
                h = head[e]
                while h < len(lst) and done[lst[h]]:
                    h += 1
                head[e] = h
                w = WE.get(e, 1)
                seen = 0
                j = h
                while j < len(lst) and seen < w:
                    i = lst[j]
                    j += 1
                    if done[i]:
                        continue
                    op = ops[i]
                    if e != "pool":
                        if op["seg"] != cur:
                            break
                    elif op["arena"] and op["seg"] > cur:
                        break
                    seen += 1
                    if nun[i] == 0:
                        est = max(free[e], ready[i])
                        if e != "pool" or op["arena"]:
                            est = max(est, seg_t0)
                        key = (est, i)
                        if best is None or key < best[0]:
                            best = (key, e, i)
            if best is None:
                raise RuntimeError("scheduler stuck at seg %d" % cur)
            (est, _), e, i = best
            op = ops[i]
            if op["dma"]:
                free[e] = est + 0.06
                f = est + op["cost"]
            else:
                f = est + op["cost"]
                free[e] = f
            fin[i] = f
            tmax = max(tmax, f)
            if not op["dma"]:
                segbusy[(op["seg"], e)] = segbusy.get((op["seg"], e), 0.0) + op["cost"]
            segend[op["seg"]] = max(segend.get(op["seg"], 0.0), f)
            done[i] = True
            nsched += 1
            new_order[e].append(i)
            if e in self.GATED:
                rem[op["seg"]] -= 1
            for s_ in succ[i]:
                nun[s_] -= 1
                lat = 0.0 if (ops[s_]["eng"] == e and not op["dma"]) else 0.1
                if f + lat > ready[s_]:
                    ready[s_] = f + lat
        self.by_eng = new_order
        self.sim_time = tmax
        import os
        if os.environ.get("KSCHED_DEBUG"):
            print("sched: ops", n, "sim_time_us", round(tmax, 1), flush=True)
            prev = 0.0
            for sg in sorted(segend):
                print("  seg", sg, "dur", round(segend[sg] - prev, 1), {e_: round(segbusy.get((sg, e_), 0.0), 1)
                                                                         for e_ in ("pe", "act", "dve")})
                prev = segend[sg]
        pos = {}
        for e in self.ENG:
            for k, i in enumerate(new_order[e]):
                pos[i] = k
        bar = [set() for _ in range(nseg + 1)]
        for s_ in range(1, nseg + 1):
            for e in ("pe", "act", "dve"):
                last = None
                for i in new_order[e]:
                    if ops[i]["seg"] < s_:
                        last = i
                    else:
                        break
                if last is not None:
                    bar[s_].add(last)
            for i in new_order["sp"]:
                if ops[i]["seg"] == s_ - 1 and ops[i]["dma"]:
                    bar[s_].add(i)
        for e in self.GATED:
            lastseg = 0
            for i in new_order[e]:
                sg = ops[i]["seg"]
                if sg > lastseg:
                    for t in range(lastseg + 1, sg + 1):
                        ops[i]["deps"] |= bar[t]
                    lastseg = sg
        for i in new_order["pool"]:
            if ops[i]["arena"]:
                ops[i]["deps"] |= bar[ops[i]["seg"]]
        for i, op in enumerate(ops):
            keep = set()
            bestp = {}
            for d in op["deps"]:
                if d == i:
                    continue
                od = ops[d]
                if od["dma"]:
                    keep.add(d)
                else:
                    pe_ = od["eng"]
                    if pe_ not in bestp or pos[d] > pos[bestp[pe_]]:
                        bestp[pe_] = d
            keep |= set(bestp.values())
            op["deps"] = keep

    def mm(self, out, lhsT, rhs, start, stop, reads, writes):
        nfree = _free(rhs)
        c = max(nfree, 64) / 2400.0 * (4.0 if rhs.dtype == F32 else 1.0) + 0.004
        return self.add("pe", lambda e: e.matmul(out, lhsT, rhs, start=start, stop=stop), reads, writes, cost=c)

    def tr(self, out, in_, ident, reads, writes):
        return self.add("pe", lambda e: e.transpose(out, in_, ident), reads, writes, cost=0.25)

    def act(self, out, in_, func, reads, writes, bias=None, scale=None, accum=None):
        kw = {}
        if bias is not None:
            kw["bias"] = bias
        if scale is not None:
            kw["scale"] = scale
        if accum is not None:
            kw["accum_out"] = accum
        c = 0.22 + _free(out) / 1400.0
        return self.add("act", lambda e: e.activation(out=out, in_=in_, func=func, **kw), reads, writes, cost=c)

    def _vc(self, out):
        return 0.08 + _free(out) / 960.0

    def tt(self, out, in0, in1, op, reads, writes, eng="dve"):
        return self.add(eng, lambda e: e.tensor_tensor(out=out, in0=in0, in1=in1, op=op), reads, writes,
                        cost=self._vc(out))

    def ts(self, out, in0, s1, s2, op0, op1, reads, writes, eng="dve"):
        if s2 is None:
            return self.add(eng, lambda e: e.tensor_scalar(out, in0, s1, None, op0), reads, writes, cost=self._vc(out))
        return self.add(eng, lambda e: e.tensor_scalar(out, in0, s1, s2, op0, op1), reads, writes, cost=self._vc(out))

    def stt(self, out, in0, scalar, in1, op0, op1, reads, writes, eng="dve"):
        return self.add(eng, lambda e: e.scalar_tensor_tensor(out=out, in0=in0, scalar=scalar, in1=in1,
                                                             op0=op0, op1=op1), reads, writes, cost=self._vc(out))

    def cp(self, out, in_, reads, writes, eng="dve"):
        if eng == "act":
            return self.add("act", lambda e: e.activation(out=out, in_=in_, func=AF.Copy), reads, writes,
                            cost=0.22 + _free(out) / 1400.0)
        return self.add(eng, lambda e: e.tensor_copy(out=out, in_=in_), reads, writes, cost=self._vc(out))

    def recip(self, out, in_, reads, writes):
        return self.add("dve", lambda e: e.reciprocal(out=out, in_=in_), reads, writes, cost=0.1 + _free(out) / 240.0)

    def dma(self, out, in_, reads, writes, q="sp", arena=False):
        nbytes = 128 * _free(out) * 4
        return self.add(q, lambda e: e.dma_start(out=out, in_=in_), reads, writes, dma=True,
                        cost=2.0 + nbytes / 150e3, arena=arena)

    def finalize(self, es):
        nc = self.nc
        self.schedule()
        for op in self.ops:
            for d in op["deps"]:
                self.ops[d]["signal"] = True
        for d in self.final:
            self.ops[d]["signal"] = True
        csem = {e: es.enter_context(nc.semaphore("c_" + e)) for e in ("pe", "act", "dve", "pool")}
        NP = 16
        qsem = {q: [es.enter_context(nc.semaphore(f"q_{q}{k}")) for k in range(NP)] for q in ("sp", "pool")}
        cnt = {e: 0 for e in csem}
        qn = {q: 0 for q in qsem}
        quse = {q: [0] * NP for q in qsem}
        qprev = {q: [None] * NP for q in qsem}
        for i, op in enumerate(self.ops):
            if op["dma"]:
                q = op["eng"]
                k = qn[q] % NP
                qn[q] += 1
                quse[q][k] += 1
                op["token"] = (qsem[q][k], 16 * quse[q][k])
                if qprev[q][k] is not None:
                    op["deps"].add(qprev[q][k])
                qprev[q][k] = i
        for e in csem:
            for i in self.by_eng[e]:
                op = self.ops[i]
                if (not op["dma"]) and op["signal"]:
                    cnt[e] += 1
                    op["token"] = (csem[e], cnt[e])
        ops = self.ops
        final = list(self.final)

        def emit(engname, e):
            waited = {}
            for i in self.by_eng[engname]:
                op = ops[i]
                for d in sorted(op["deps"]):
                    sem, val = ops[d]["token"]
                    if waited.get(sem, 0) < val:
                        e.wait_ge(sem, val)
                        waited[sem] = val
                ins = op["fn"](e)
                if op["signal"]:
                    ins.then_inc(op["token"][0], 16 if op["dma"] else 1)
            if engname == "sp":
                for d in final:
                    sem, val = ops[d]["token"]
                    if waited.get(sem, 0) < val:
                        e.wait_ge(sem, val)
                        waited[sem] = val

        with nc.Block() as block:
            @block.tensor
            def _(e):
                emit("pe", e)

            @block.scalar
            def _(e):
                emit("act", e)

            @block.vector
            def _(e):
                emit("dve", e)

            @block.gpsimd
            def _(e):
                emit("pool", e)

            @block.sync
            def _(e):
                emit("sp", e)


def K(name, *idx):
    return (name,) + idx


def build_program(stop_after=None, dumps=(), n_layers=L_):
    nc = bass.Bass("TRN2", target_bir_lowering=False)
    S = Sched(nc)
    es = ExitStack()
    P = {}

    def din(name, shape):
        P[name] = nc.dram_tensor(name, list(shape), F32, kind="ExternalInput").ap()
        return P[name]

    x_d = din("x", [S_, D_])
    mem_d = din("mem", [256, D_])
    w_in_d = din("w_in", [L_, D_, 3076])
    w_out_d = din("w_out", [L_, D_, D_])
    wq_d = din("xattn_wq", [L_, D_, D_])
    wkv_d = din("xattn_wkv", [L_, D_, 2 * D_])
    wo_d = din("xattn_wo", [L_, D_, D_])
    w1_d = din("expert_w1", [L_, 16, D_, 256])
    w3_d = din("expert_w3", [L_, 16, D_, 256])
    w2_d = din("expert_w2", [L_, 16, 256, D_])
    wrt_d = din("router_w", [L_, D_, 20])
    lrubd_d = din("lru_wbd", [L_, 2, 2, 128, 128])
    colp_d = din("colp", [128, L_ * NCOL])
    rowp_d = din("rowp", [128, L_ * NROW])
    c128_d = din("c128", [128, 8 * 128])
    cmask_d = din("cmask", [128, 12 * 512])
    rope_d = din("rope", [128, 2 * S_])
    en_d = din("en", [16, 16 * 128])
    blkoh_d = din("blkoh", [8, S_])
    gate_c_d = din("gatec", [128, 3 * 128])
    out_d = nc.dram_tensor("out", [S_, D_], F32, kind="ExternalOutput").ap()
    dump_d = {}

    uid = [0]

    def sb(name, shape, dt=F32, stack=None):
        uid[0] += 1
        return (stack or es).enter_context(nc.sbuf_tensor(f"{name}_{uid[0]}", list(shape), dt))

    xT = sb("xT", [128, KC, S_])
    hT = sb("hT", [128, KC, S_], BF16)
    colp = sb("colp_s", [128, L_ * NCOL])
    rowp = sb("rowp_s", [128, L_ * NROW])
    c128 = sb("c128_s", [128, 8 * 128])
    c128b = sb("c128b_s", [128, 2 * 128], BF16)
    W = [sb(f"wslot{i}", [128, KC, 512], BF16) for i in range(3)]
    PS = [es.enter_context(nc.psum_tensor(f"ps{i}", [128, 512], F32)) for i in range(8)]
    wn = [0]

    ident = c128[:, 0:128]
    ones_f = c128[:, 128:256]
    tri_le = c128[:, 256:384]
    negu = c128[:, 384:512]
    negones = c128[:, 512:640]
    perm_f = c128[:, 640:768]
    ssdneg = c128[:, 768:896]
    ones_b = c128b[:, 128:256]
    ident_b = c128b[:, 0:128]

    cst = sb("cst", [128, 4])
    S.add("dve", lambda e: e.memset(cst[:, 0:1], EPS), [], [K("cst0")])
    S.add("dve", lambda e: e.memset(cst[:, 1:2], 1.0), [], [K("cst1")])
    S.add("dve", lambda e: e.memset(cst[:, 2:4], 0.0), [K("cst0"), K("cst1")], [K("cst")])
    S.dma(colp[:], colp_d[:, :], [], [K("colp")])
    S.dma(rowp[:], rowp_d[:, :], [], [K("rowp")])
    S.dma(c128[:], c128_d[:, :], [], [K("c128")])
    S.dma(c128b[:], c128_d[:, 0:256], [], [K("c128b")], q="pool")

    def col(l, name, j=0, n=1):
        c0, w = COLS[name]
        return colp[:, l * NCOL + c0 + j: l * NCOL + c0 + j + n]

    def row(l, name, j=0, n=None):
        c0, w = ROWS[name]
        n = w if n is None else n
        return rowp[:, l * NROW + c0 + j: l * NROW + c0 + j + n]

    def ps_rot():
        k = S.psn % 4
        S.psn += 1
        return PS[k], K("ps", k)

    Wf = [w[:].rearrange("p a b -> p (a b)") for w in W]

    def load_w(src2d, nk, ncols, slot=None, c0=0, tot=None):
        tot = ncols if tot is None else tot
        if slot is None:
            k = wn[0] % 3
            wn[0] += 1
        else:
            k = slot
        v = Wf[k][:, 0:nk * tot].rearrange("p (a b) -> p a b", a=nk)
        step = 2 if nk >= 2 else 1
        allk = [K("w", k, j_, hf_) for j_ in range(4) for hf_ in range(2)]
        for h in range(0, nk, step):
            if nk == 8 and tot == 512:
                halves = [hf_ for hf_ in range(2) if c0 < (hf_ + 1) * 256 and c0 + ncols > hf_ * 256]
                wkeys_ = [K("w", k, h // 2, hf_) for hf_ in halves]
            else:
                wkeys_ = allk
            S.dma(v[:, h:h + step, c0:c0 + ncols],
                  src2d[h * 128:(h + step) * 128, :].rearrange("(kc p) n -> p kc n", p=128),
                  [], wkeys_, q="pool")
        return v, allk, k

    def dump(name, ap, reads, shape):
        if name in dumps:
            d = nc.dram_tensor("dbg_" + name, list(shape), ap.dtype, kind="ExternalOutput").ap()
            dump_d[name] = d
            i = S.dma(d, ap, reads, [K("dbg", name)])
            S.final.append(i)

    def load_fm(dst, src_d, ntile, dkey, st):
        xin = [sb(f"xin{j}", [128, D_], stack=st) for j in range(2)]
        for i in range(ntile):
            b = xin[i % 2]
            S.dma(b[:], src_d[i * 128:(i + 1) * 128, :], [], [K("xin", i % 2)])
            for half in range(2):
                ps, pk = ps_rot()
                for j in range(4):
                    kc = half * 4 + j
                    S.tr(ps[:, j * 128:(j + 1) * 128], b[:, kc * 128:(kc + 1) * 128], ident,
                         [K("xin", i % 2), K("c128")], [pk])
                S.cp(dst[:, half * 4:half * 4 + 4, i * 128:(i + 1) * 128],
                     ps[:].rearrange("p (a b) -> p a b", a=4), [pk],
                     [K(dkey, half * 4 + j, i // 4) for j in range(4)], eng=("dve" if half == 0 else "act"))

    with ExitStack() as st:
        load_fm(xT, x_d, NT, "xT", st)
    S.barrier()
    dump("xT0", xT[:], [K("xT", kc, tc) for kc in range(KC) for tc in range(NTC)], [128, KC, S_])

    def rmsnorm_fm(src, skey, gcol, dst, dkey, nk, T, st, dscale=1.0, hook=None, tag="n"):
        tw = min(T, TW)
        sq = [sb(f"sq_{tag}{j}", [128, nk, tw], BF16, stack=st) for j in range(2)]
        rs = [sb(f"rs_{tag}{j}", [128, tw], stack=st) for j in range(2)]
        for tc in range(T // tw):
            sl = slice(tc * tw, (tc + 1) * tw)
            q = sq[tc % 2]
            for kc in range(nk):
                S.act(q[:, kc, :], src[:, kc, sl], AF.Square, [K(skey, kc, tc)], [K("sq" + tag, tc % 2, kc)])
            ps, pk = ps_rot()
            for kc in range(nk):
                S.mm(ps[:, 0:tw], ones_b, q[:, kc, :], kc == 0, kc == nk - 1,
                     [K("sq" + tag, tc % 2, kc), K("c128b")], [pk])
            r = rs[tc % 2]
            S.act(r[:], ps[:, 0:tw], AF.Sqrt, [pk, K("cst")], [K("rs" + tag, tc % 2)],
                  bias=cst[:, 0:1], scale=1.0 / (nk * 128))
            S.add("dve", (lambda e, r=r: e.reciprocal(out=r[:], in_=r[:])),
                  [K("rs" + tag, tc % 2)], [K("rs" + tag, tc % 2)], cost=0.1 + tw / 240.0)
            if dst is not None:
                for kc in range(nk):
                    S.stt(dst[:, kc, sl], src[:, kc, sl], gcol(kc), r[:], ALU.mult, ALU.mult,
                          [K(skey, kc, tc), K("rs" + tag, tc % 2), K("colp")], [K(dkey, kc, tc)],
                          eng="dve")
            if hook is not None:
                hook(tc, r)

    ALLH = [K("hT", kc, tc) for kc in range(KC) for tc in range(NTC)]

    def hkeys(tc):
        return [K("hT", kc, tc) for kc in range(KC)]

    def proj_fm(wv, wkeys, c0, tc, ps, pk):
        for kc in range(KC):
            S.mm(ps[:, :], wv[:, kc, c0:c0 + 128], hT[:, kc, tc * TW:(tc + 1) * TW], kc == 0, kc == KC - 1,
                 wkeys + [K("hT", kc, tc)], [pk])

    def conv_chunk(ps, pk, tc, xw, xwk, cwcol, cbcol, dst, dkey):
        w_ = xw[tc % 2]
        wk = K(xwk, tc % 2)
        if tc == 0:
            S.add("dve", lambda e: e.memset(w_[:, 0:3], 0.0), [], [wk])
        else:
            S.cp(w_[:, 0:3], xw[(tc - 1) % 2][:, 512:515], [K(xwk, (tc - 1) % 2)], [wk])
        S.cp(w_[:, 3:515], ps[:, :], [pk], [wk], eng="act")
        S.ts(dst, w_[:, 3:515], cwcol(3), cbcol, ALU.mult, ALU.add, [wk, K("colp")], [dkey])
        for k in range(3):
            S.stt(dst, w_[:, k:k + 512], cwcol(k), dst, ALU.mult, ALU.add, [wk, dkey, K("colp")], [dkey])

    def group_out(l, g, YG, st):
        YB = sb("YB", [128, 2, S_], BF16, stack=st)
        rmsnorm_fm(YG, "YG", lambda kc: col(l, "grp_g", 2 * g + kc), YB, "YB", 2, S_, st, tag="g")
        dump(f"yn{l}_{g}", YB[:], [K("YB", c, t) for c in range(2) for t in range(NTC)], [128, 2, S_])
        for half in range(2):
            wv, wk, _ = load_w(w_out_d[l, g * 256:(g + 1) * 256, half * 512:(half + 1) * 512], 2, 512)
            for o4 in range(4):
                oc = half * 4 + o4
                for tc in range(NTC):
                    ps, pk = ps_rot()
                    for kc in range(2):
                        S.mm(ps[:, :], wv[:, kc, o4 * 128:(o4 + 1) * 128], YB[:, kc, tc * TW:(tc + 1) * TW],
                             kc == 0, kc == 1, wk + [K("YB", kc, tc)], [pk])
                    S.tt(xT[:, oc, tc * TW:(tc + 1) * TW], xT[:, oc, tc * TW:(tc + 1) * TW], ps[:, :], ALU.add,
                         [K("xT", oc, tc), pk], [K("xT", oc, tc)])

    ALLX = [K("xT", kc, tc) for kc in range(KC) for tc in range(NTC)]
    done = False
    for l in range(n_layers):
        with ExitStack() as st:
            rmsnorm_fm(xT, "xT", lambda kc: col(l, "mix_g", kc), hT, "hT", KC, S_, st, tag="a")
        S.barrier()
        if l == 0:
            dump("h0", hT[:], ALLH, [128, KC, S_])
        if stop_after == "norm0":
            break

        with ExitStack() as sty, ExitStack() as st:
            YG = sb("YG", [128, 2, S_], stack=sty)
            wv, wk, _ = load_w(w_in_d[l, :, 0:512], 8, 512)
            wbd = sb("wbd", [128, 4, 128], BF16, stack=st)
            S.dma(wbd[:], lrubd_d[l].rearrange("g c k m -> k (g c) m"), [], [K("wbd")], q="pool", arena=True)
            c8 = sb("c8", [128, 2], stack=st)
            dump(f"A_wbd{l}", wbd[:], [K("wbd")], [128, 4, 128])
            cy = sb("cy", [128, 2], stack=st)
            cz = sb("cz", [128, 2], stack=st)
            S.act(cy[:], col(l, "lru_lam", 0, 2), AF.Exp, [K("colp")], [K("cy")], scale=-1.0)
            S.ts(cz[:], cy[:], 2.0, None, ALU.add, None, [K("cy")], [K("cz")])
            S.add("dve", lambda e: e.reciprocal(out=cz[:], in_=cz[:]), [K("cz")], [K("cz")])
            S.tt(cz[:], cz[:], cy[:], ALU.mult, [K("cz"), K("cy")], [K("cz")])
            S.tt(cy[:], cz[:], cz[:], ALU.mult, [K("cz")], [K("cy")])
            S.ts(c8[:], cy[:], 1.0 / 9.0, 1.0 / 7.0, ALU.mult, ALU.add, [K("cy")], [K("c8")])
            for cc_ in (1.0 / 5.0, 1.0 / 3.0, 1.0):
                S.tt(c8[:], c8[:], cy[:], ALU.mult, [K("c8"), K("cy")], [K("c8")])
                S.ts(c8[:], c8[:], cc_, None, ALU.add, None, [K("c8")], [K("c8")])
            S.tt(c8[:], c8[:], cz[:], ALU.mult, [K("c8"), K("cz")], [K("c8")])
            S.ts(c8[:], c8[:], -16.0, None, ALU.mult, None, [K("c8")], [K("c8")])
            xw = [sb(f"xw{j}", [128, 515], stack=st) for j in range(2)]
            XC = sb("XC", [128, S_], stack=st)
            XCB = sb("XCB", [128, S_], BF16, stack=st)
            GG = sb("GG", [128, S_], stack=st)
            RA = sb("RA", [128, S_], stack=st)
            IU = sb("IU", [128, S_], stack=st)
            T2 = sb("T2", [128, S_], stack=st)
            XX = sb("XX", [128, S_], stack=st)
            for c in range(2):
                for tc in range(NTC):
                    sl = slice(tc * TW, (tc + 1) * TW)
                    ps, pk = ps_rot()
                    proj_fm(wv, wk, c * 128, tc, ps, pk)
                    conv_chunk(ps, pk, tc, xw, "xw", lambda k: col(l, "lru_cw", c * 4 + k), col(l, "lru_cb", c),
                               XC[:, sl], K("XC", tc))
                    ps, pk = ps_rot()
                    proj_fm(wv, wk, 256 + c * 128, tc, ps, pk)
                    S.cp(GG[:, sl], ps[:, :], [pk], [K("GG", tc)], eng="act")
                XCk = [K("XC", t) for t in range(NTC)]
                GGk = [K("GG", t) for t in range(NTC)]
                S.tt(T2[:], GG[:], GG[:], ALU.mult, GGk, [K("T2")])
                S.ts(T2[:], T2[:], 0.044715, 1.0, ALU.mult, ALU.add, [K("T2")], [K("T2")])
                S.tt(T2[:], T2[:], GG[:], ALU.mult, [K("T2")] + GGk, [K("T2")])
                S.act(T2[:], T2[:], AF.Sigmoid, [K("T2")], [K("T2")], scale=1.5957691216)
                S.tt(GG[:], GG[:], T2[:], ALU.mult, [K("T2")] + GGk, GGk)
                S.cp(XCB[:], XC[:], XCk, [K("XCB")], eng="act")
                for tc in range(NTC):
                    sl = slice(tc * TW, (tc + 1) * TW)
                    ps, pk = ps_rot()
                    S.mm(ps[:, :], wbd[:, c, :], XCB[:, sl], True, True, [K("wbd"), K("XCB")], [pk])
                    S.act(RA[:, sl], ps[:, :], AF.Sigmoid, [pk, K("colp")], [K("RA", tc)], bias=col(l, "lru_br", c))
                    ps, pk = ps_rot()
                    S.mm(ps[:, :], wbd[:, 2 + c, :], XCB[:, sl], True, True, [K("wbd"), K("XCB")], [pk])
                    S.act(IU[:, sl], ps[:, :], AF.Sigmoid, [pk, K("colp")], [K("IU", tc)], bias=col(l, "lru_bi", c))
                RAk = [K("RA", t) for t in range(NTC)]
                IUk = [K("IU", t) for t in range(NTC)]
                if c == 1:
                    dump(f"A_r{l}", RA[:], RAk, [128, S_])
                    dump(f"A_i{l}", IU[:], IUk, [128, S_])
                    dump(f"A_xcb{l}", XCB[:], [K("XCB")], [128, S_])
                    dump(f"A_xc{l}", XC[:], XCk, [128, S_])
                S.ts(XX[:], RA[:], c8[:, c:c + 1], 2.0, ALU.mult, ALU.mult, RAk + [K("c8")], [K("XX")])
                S.ts(T2[:], XX[:], 1.0 / 5040.0, 1.0 / 720.0, ALU.mult, ALU.add, [K("XX")], [K("T2")])
                for cc_ in (1.0 / 120.0, 1.0 / 24.0, 1.0 / 6.0, 0.5, 1.0):
                    S.tt(T2[:], T2[:], XX[:], ALU.mult, [K("T2"), K("XX")], [K("T2")])
                    S.ts(T2[:], T2[:], cc_, None, ALU.add, None, [K("T2")], [K("T2")])
                S.stt(T2[:], T2[:], -1.0, XX[:], ALU.mult, ALU.mult, [K("T2"), K("XX")], [K("T2")])
                S.act(T2[:], T2[:], AF.Sqrt, [K("T2")], [K("T2")])
                S.act(RA[:], XX[:], AF.Exp, [K("XX")], RAk, scale=0.5)
                S.tt(IU[:], IU[:], XC[:], ALU.mult, IUk + XCk, IUk)
                S.tt(IU[:], IU[:], T2[:], ALU.mult, IUk + [K("T2")], IUk)
                S.add("dve", lambda e: e.tensor_tensor_scan(out=XC[:], data0=RA[:], data1=IU[:], initial=0.0,
                                                            op0=ALU.mult, op1=ALU.add), RAk + IUk, XCk, cost=2.6)
                S.tt(YG[:, c, :], XC[:], GG[:], ALU.mult, XCk + GGk, [K("YG", c, t) for t in range(NTC)])
            for nm_, t_ in (("XC", XC), ("GG", GG), ("RA", RA), ("IU", IU), ("T2", T2)):
                dump(f"A_{nm_}{l}", t_[:], [K(nm_, t) for t in range(NTC)] + [K(nm_)], [128, S_])
            dump(f"ya{l}", YG[:], [K("YG", c, t) for c in range(2) for t in range(NTC)], [128, 2, S_])
            st.close()
            S.barrier()
            group_out(l, 0, YG, sty)
        S.barrier()
        if stop_after == f"A{l}":
            done = True
            break

        with ExitStack() as sty, ExitStack() as st:
            YG = sb("YG", [128, 2, S_], stack=sty)
            QB = sb("QB", [128, 2, S_], BF16, stack=st)
            KZ = sb("KZ", [128, 4, S_], BF16, stack=st)
            S.add("dve", lambda e: e.memset(KZ[:], 0.0), [], [K("KZ", h_, t_) for h_ in range(4) for t_ in range(NTC)], cost=4.5)
            VB = sb("VB", [128, NT, 256], BF16, stack=st)
            M01 = sb("M01", [128, 4, 512], stack=st)
            NM = sb("NM", [128, 4, 512], BF16, stack=st)
            S.dma(M01[:], cmask_d[:, 0:2048].rearrange("p (a b) -> p a b", a=4), [], [K("M01")])
            S.dma(NM[:], cmask_d[:, 2048:4096].rearrange("p (a b) -> p a b", a=4), [], [K("NM")], q="pool", arena=True)
            wv, wk, _ = load_w(w_in_d[l, :, 512:1024], 8, 512)
            wv2, wk2, _ = load_w(w_in_d[l, :, 1024:1280], 8, 256)
            for c2 in range(2):
                for tc in range(NTC):
                    sl = slice(tc * TW, (tc + 1) * TW)
                    ps, pk = ps_rot()
                    proj_fm(wv, wk, c2 * 128, tc, ps, pk)
                    S.act(QB[:, c2, sl], ps[:, :], AF.Copy, [pk], [K("QB", c2, tc)], scale=0.125)
                    ps, pk = ps_rot()
                    proj_fm(wv, wk, 256 + c2 * 128, tc, ps, pk)
                    S.cp(KZ[0:64, 2 * c2, sl], ps[0:64, :], [pk], [K("KZ", 2 * c2, tc)])
                    S.cp(KZ[64:128, 2 * c2 + 1, sl], ps[64:128, :], [pk], [K("KZ", 2 * c2 + 1, tc)], eng="act")
            for i in range(NT):
                ps, pk = ps_rot()
                for kc in range(KC):
                    S.mm(ps[:, 0:256], hT[:, kc, i * 128:(i + 1) * 128], wv2[:, kc, :], kc == 0, kc == KC - 1,
                         wk2 + [K("hT", kc, i // 4)], [pk])
                S.cp(VB[:, i, :], ps[:, 0:256], [pk], [K("VB", i)], eng=("dve" if i % 2 else "act"))
            Eb = [sb(f"Eb{j}", [128, 512], stack=st) for j in range(2)]
            SPb = [sb(f"SPb{j}", [128, 512], F32R, stack=st) for j in range(3)]
            Wb = [sb(f"Wb{j}", [128, 512], BF16, stack=st) for j in range(2)]
            Rb = [sb(f"Rb{j}", [128, 512], F32R, stack=st) for j in range(2)]
            NUr = sb("NUr", [128, 2, 128], F32R, stack=st)
            S.cp(NUr[:, 0, :], negu, [K("c128")], [K("NUr")])
            S.cp(NUr[:, 1, :], negones, [K("c128")], [K("NUr")])
            items = []
            grp = 0
            for h in range(4):
                for c in range(NTC):
                    nb = 4 * c + 4
                    for idx, b in enumerate(range(nb - 1, -1, -1)):
                        items.append(dict(h=h, c=c, idx=idx, b=b, grp=grp, last=(b == 0)))
                    grp += 1
            for i_, itm in enumerate(items):
                itm["i"] = i_
                itm["pr"] = slice((itm["h"] % 2) * 64, (itm["h"] % 2) * 64 + 64)
                itm["c2"] = itm["h"] // 2
                itm["d"] = itm["b"] - 4 * itm["c"]
                itm["ksl"] = slice(itm["b"] * 128, (itm["b"] + 1) * 128)
                itm["qsl"] = slice(itm["c"] * TW, (itm["c"] + 1) * TW)
                itm["qk"] = [K("KZ", itm["h"], itm["b"] // 4), K("QB", itm["c2"], itm["c"])]

            def sb_s1(m):
                i = m["i"]
                pr, c2 = m["pr"], m["c2"]
                off = 128 * max(m["d"], 0)
                cs = slice(off, TW)
                qs = slice(m["c"] * TW + off, (m["c"] + 1) * TW)
                if m["idx"] == 0:
                    R0, R0k = Rb[m["grp"] % 2], K("Rb", m["grp"] % 2)
                    S.ts(R0[:], M01[:, 0, :], 0.0, None, ALU.mult, None, [K("M01")], [R0k])
                psZ, pkZ = ps_rot()
                S.mm(psZ[:, cs], KZ[:, m["h"], m["ksl"]], QB[:, c2, qs], True, True, m["qk"], [pkZ])
                E, Ek = Eb[i % 2], K("Eb", i % 2)
                SP, SPk = SPb[i % 3], K("SPb", i % 3)
                S.act(E[:, cs], psZ[:, cs], AF.Exp, [pkZ], [Ek])
                S.act(SP[:, cs], E[:, cs], AF.Ln, [Ek, K("cst")], [SPk], bias=cst[:, 1:2])
                if m["d"] >= 0:
                    S.tt(SP[:, cs], SP[:, cs].bitcast(F32), M01[:, m["d"], cs], ALU.mult, [SPk, K("M01")], [SPk])

            def sb_s2(m):
                i = m["i"]
                pr, c2, d = m["pr"], m["c2"], m["d"]
                off = 128 * max(d, 0)
                cs = slice(off, TW)
                qs = slice(m["c"] * TW + off, (m["c"] + 1) * TW)
                SP, SPk = SPb[i % 3], K("SPb", i % 3)
                Wt, Wk_ = Wb[i % 2], K("Wb", i % 2)
                R, Rk = Rb[m["grp"] % 2], K("Rb", m["grp"] % 2)
                psA, pkA = ps_rot()
                has_r = m["idx"] > 0
                S.mm(psA[:, cs], KZ[:, m["h"], m["ksl"]], QB[:, c2, qs], True, False, m["qk"], [pkA])
                S.mm(psA[:, cs], NUr[:, 0, :], SP[:, cs], False, (not has_r) and d < 0, [SPk, K("NUr")], [pkA])
                if has_r:
                    S.mm(psA[:, cs], NUr[:, 1, :], R[:, cs], False, d < 0, [Rk, K("NUr")], [pkA])
                if d >= 0:
                    S.mm(psA[:, cs], ident_b, NM[:, d, cs], False, True, [K("NM"), K("c128b")], [pkA])
                S.act(Wt[:, cs], psA[:, cs], AF.Exp, [pkA], [Wk_])
                if not m["last"]:
                    S.tt(R[:, cs], R[:, cs].bitcast(F32), SP[:, cs].bitcast(F32), ALU.add, [Rk, SPk], [Rk])

            def sb_s3(m):
                i = m["i"]
                pr, c2, h = m["pr"], m["c2"], m["h"]
                off = 128 * max(m["d"], 0)
                cs = slice(off, TW)
                Wt, Wk_ = Wb[i % 2], K("Wb", i % 2)
                psO, pkO = PS[4 + m["grp"] % 2], K("psO", m["grp"] % 2)
                S.mm(psO[:, cs], VB[:, m["b"], c2 * 128:(c2 + 1) * 128], Wt[:, cs], m["idx"] == 0, m["last"],
                     [K("VB", m["b"]), Wk_], [pkO])
                if m["last"]:
                    S.cp(YG[pr, c2, m["qsl"]], psO[pr, :], [pkO], [K("YG", c2, m["c"])], eng="act")

            n_it = len(items)
            for r in range(-2, n_it):
                if 0 <= r + 2 < n_it:
                    sb_s1(items[r + 2])
                if 0 <= r + 1 < n_it:
                    sb_s2(items[r + 1])
                if 0 <= r < n_it:
                    sb_s3(items[r])
            dump(f"yb{l}", YG[:], [K("YG", c, t) for c in range(2) for t in range(NTC)], [128, 2, S_])
            st.close()
            S.barrier()
            group_out(l, 1, YG, sty)
        S.barrier()
        if stop_after == f"B{l}":
            done = True
            break

        with ExitStack() as sty, ExitStack() as st:
            YG = sb("YG", [128, 2, S_], stack=sty)
            XS = sb("XS", [128, 2, S_], stack=st)
            BTb = sb("BTb", [128, 2, S_], BF16, stack=st)
            CTb = sb("CTb", [128, 2, S_], BF16, stack=st)
            BTM = sb("BTM", [128, NT, 256], BF16, stack=st)
            XTM = sb("XTM", [128, NT, 256], BF16, stack=st)
            wdt = sb("wdt", [128, KC, 4], BF16, stack=st)
            stc = ExitStack()
            xw = [sb(f"xwc{j}", [128, 515], stack=stc) for j in range(2)]
            CV = [sb(f"CV{j}", [128, 512], stack=stc) for j in range(2)]
            BF = [sb(f"BF{j}", [128, 512], stack=stc) for j in range(2)]
            S.dma(wdt[:], w_in_d[l, :, 2304:2308].rearrange("(kc p) n -> p kc n", p=128), [], [K("wdt")], q="pool", arena=True)
            wv1, wk1, _ = load_w(w_in_d[l, :, 1280:1792], 8, 512)
            wv2, wk2, _ = load_w(w_in_d[l, :, 1792:2304], 8, 512)
            n_cv = 0
            for j in range(6):
                if j < 2:
                    wv, wk, c0 = wv1, wk1, 256 + j * 128
                elif j < 4:
                    wv, wk, c0 = wv2, wk2, (j - 2) * 128
                else:
                    wv, wk, c0 = wv2, wk2, 256 + (j - 4) * 128
                for tc in range(NTC):
                    sl = slice(tc * TW, (tc + 1) * TW)
                    ps, pk = ps_rot()
                    proj_fm(wv, wk, c0, tc, ps, pk)
                    cv, cvk = CV[n_cv % 2], K("CV", n_cv % 2)
                    conv_chunk(ps, pk, tc, xw, "xwc", lambda k: col(l, "ssm_cw", j * 4 + k), col(l, "ssm_cb", j),
                               cv[:], cvk)
                    if j < 2:
                        S.act(XS[:, j, sl], cv[:], AF.Silu, [cvk], [K("XS", j, tc)])
                        ps, pk = ps_rot()
                        for q in range(4):
                            S.tr(ps[:, q * 128:(q + 1) * 128], XS[:, j, tc * TW + q * 128: tc * TW + (q + 1) * 128],
                                 ident, [K("XS", j, tc), K("c128")], [pk])
                        S.cp(XTM[:, tc * 4:tc * 4 + 4, j * 128:(j + 1) * 128],
                             ps[:].rearrange("p (a b) -> p a b", a=4), [pk], [K("XTM", tc, j)])
                    elif j < 4:
                        g = j - 2
                        bf, bfk = BF[n_cv % 2], K("BF", n_cv % 2)
                        S.act(bf[:], cv[:], AF.Silu, [cvk], [bfk])
                        S.cp(BTb[:, g, sl], bf[:], [bfk], [K("BTb", g, tc)])
                        ps, pk = ps_rot()
                        for q in range(4):
                            S.tr(ps[:, q * 128:(q + 1) * 128], bf[:, q * 128:(q + 1) * 128], ident,
                                 [bfk, K("c128")], [pk])
                        S.cp(BTM[:, tc * 4:tc * 4 + 4, g * 128:(g + 1) * 128],
                             ps[:].rearrange("p (a b) -> p a b", a=4), [pk], [K("BTM", tc, g)])
                    else:
                        S.act(CTb[:, j - 4, sl], cv[:], AF.Silu, [cvk], [K("CTb", j - 4, tc)])
                    n_cv += 1
            stc.close()
            S.barrier()
            DT = sb("DT", [128, 64], stack=st)
            AN = sb("AN", [128, 64], stack=st)
            AC = sb("AC", [128, 64], stack=st)
            ACS = sb("ACS", [128, 64], stack=st)
            TOT = sb("TOT", [128, 64], stack=st)
            DEC = sb("DEC", [128, 64], stack=st)
            ETOT = sb("ETOT", [128, 64], stack=st)
            DTD = sb("DTD", [128, 64], stack=st)
            psd, pkd = ps_rot()
            for i in range(NT):
                for kc in range(KC):
                    S.mm(psd[:, i * 4:(i + 1) * 4], hT[:, kc, i * 128:(i + 1) * 128], wdt[:, kc, :], kc == 0,
                         kc == KC - 1, [K("wdt"), K("hT", kc, i // 4)], [pkd])
            S.tt(DT[:], psd[:, 0:64], row(l, "dt_bias"), ALU.add, [pkd, K("rowp")], [K("DT")])
            S.act(DT[:], DT[:], AF.Exp, [K("DT")], [K("DT")])
            S.act(DT[:], DT[:], AF.Ln, [K("DT"), K("cst")], [K("DT")], bias=cst[:, 1:2])
            S.act(AN[:], row(l, "a_log"), AF.Exp, [K("rowp")], [K("AN")])
            S.ts(AN[:], AN[:], -1.0, None, ALU.mult, None, [K("AN")], [K("AN")])
            S.tt(AC[:], DT[:], AN[:], ALU.mult, [K("DT"), K("AN")], [K("AC")])
            ps, pk = ps_rot()
            S.mm(ps[:, 0:64], tri_le, AC[:], True, True, [K("AC"), K("c128")], [pk])
            S.cp(ACS[:], ps[:, 0:64], [pk], [K("ACS")])
            ps, pk = ps_rot()
            S.mm(ps[:, 0:64], ones_f, AC[:], True, True, [K("AC"), K("c128")], [pk])
            S.cp(TOT[:], ps[:, 0:64], [pk], [K("TOT")])
            S.tt(DEC[:], TOT[:], ACS[:], ALU.subtract, [K("TOT"), K("ACS")], [K("DEC")])
            S.act(DEC[:], DEC[:], AF.Exp, [K("DEC")], [K("DEC")])
            S.act(ETOT[:], TOT[:], AF.Exp, [K("TOT")], [K("ETOT")])
            S.tt(DTD[:], DT[:], DEC[:], ALU.mult, [K("DT"), K("DEC")], [K("DTD")])
            ST = sb("ST", [128, 4, 64], stack=st)
            S.add("dve", lambda e: e.memset(ST[:], 0.0), [], [K("ST", h) for h in range(4)])
            Rm = [sb(f"Rm{j}", [128, 128], stack=st) for j in range(2)]
            ARG = [sb(f"ARG{j}", [128, 128], stack=st) for j in range(2)]
            E1 = [sb(f"E1{j}", [128, 128], stack=st) for j in range(2)]
            GL = [sb(f"GL{j}", [128, 128], BF16, stack=st) for j in range(2)]
            CTp = [sb(f"CTp{j}", [128, 128], BF16, stack=st) for j in range(2)]
            XCp = [[sb(f"XCp{a}{j}", [128, 128], BF16, stack=st) for j in range(2)] for a in range(2)]
            STp = sb("STp", [128, 4, 128], BF16, stack=st)
            for a in range(2):
                for j in range(2):
                    S.add("dve", (lambda e, t_=XCp[a][j]: e.memset(t_[:], 0.0)), [], [K("XCp", a, j)])
            S.add("dve", lambda e: e.memset(STp[:], 0.0), [], [K("STp", h) for h in range(4)])
            XDh = [sb(f"XDh{j}", [128, 64], BF16, stack=st) for j in range(2)]
            n = 0
            for ci in range(NT):
                csl = slice(ci * 128, (ci + 1) * 128)
                tcx = ci // 4
                for g in range(2):
                    psG, pkG = ps_rot()
                    S.mm(psG[:, 0:128], BTb[:, g, csl], CTb[:, g, csl], True, True,
                         [K("BTb", g, tcx), K("CTb", g, tcx)], [pkG])
                    psY, pkY = PS[4 + (ci * 2 + g) % 2], K("psO", (ci * 2 + g) % 2)
                    for hh in range(2):
                        h = 2 * g + hh
                        pr = slice(hh * 64, hh * 64 + 64)
                        cidx = ci * 4 + h
                        k2 = n % 2
                        S.ts(Rm[k2][:], tri_le, AC[:, cidx:cidx + 1], None, ALU.mult, None,
                             [K("AC"), K("c128")], [K("Rm", k2)])
                        ps1, pk1 = ps_rot()
                        S.mm(ps1[:, 0:128], ones_f, Rm[k2][:], True, True, [K("Rm", k2), K("c128")], [pk1])
                        S.stt(ARG[k2][:], ps1[:, 0:128], ACS[:, cidx:cidx + 1], ssdneg, ALU.subtract, ALU.add,
                              [pk1, K("ACS"), K("c128")], [K("ARG", k2)])
                        S.act(ARG[k2][:], ARG[k2][:], AF.Exp, [K("ARG", k2)], [K("ARG", k2)])
                        S.tt(GL[k2][:], ARG[k2][:], psG[:, 0:128], ALU.mult, [K("ARG", k2), pkG], [K("GL", k2)])
                        S.act(E1[k2][:], ps1[:, 0:128], AF.Exp, [pk1], [K("E1", k2)])
                        S.tt(CTp[k2][:], CTb[:, g, csl], E1[k2][:], ALU.mult, [K("CTb", g, tcx), K("E1", k2)],
                             [K("CTp", k2)])
                        rot = (ci * 2 + g) % 2
                        xcp = XCp[hh][rot]
                        S.ts(xcp[:, hh * 64:(hh + 1) * 64], XTM[:, ci, h * 64:(h + 1) * 64], DT[:, cidx:cidx + 1], None,
                             ALU.mult, None, [K("XTM", tcx, g), K("DT")], [K("XCp", hh, rot)])
                        S.mm(psY[:, 0:128], xcp[:], GL[k2][:], hh == 0, False, [K("XCp", hh, rot), K("GL", k2)], [pkY])
                        S.mm(psY[:, 0:128], STp[:, h, :], CTp[k2][:], False, hh == 1, [K("STp", h), K("CTp", k2)], [pkY])
                        S.ts(XDh[k2][:], XTM[:, ci, h * 64:(h + 1) * 64], DTD[:, cidx:cidx + 1], None, ALU.mult, None,
                             [K("XTM", tcx, g), K("DTD")], [K("XDh", k2)])
                        psS, pkS = ps_rot()
                        S.mm(psS[:, 0:64], BTM[:, ci, g * 128:(g + 1) * 128], XDh[k2][:], True, True,
                             [K("BTM", tcx, g), K("XDh", k2)], [pkS])
                        S.stt(ST[:, h, :], ST[:, h, :], ETOT[:, cidx:cidx + 1], psS[:, 0:64], ALU.mult, ALU.add,
                              [K("ST", h), K("ETOT"), pkS], [K("ST", h)])
                        S.cp(STp[:, h, hh * 64:(hh + 1) * 64], ST[:, h, :], [K("ST", h)], [K("STp", h)], eng="act")
                        n += 1
                    S.cp(YG[:, g, csl], psY[:, 0:128], [pkY], [K("YG", g, tcx)], eng="act")
            ARG = [sb(f"ZT{j}", [128, 512], stack=st) for j in range(1)]
            for j in range(2):
                YGk = [K("YG", j, t) for t in range(NTC)]
                S.stt(YG[:, j, :], XS[:, j, :], col(l, "ssm_d", j), YG[:, j, :], ALU.mult, ALU.add,
                      [K("XS", j, t) for t in range(NTC)] + YGk + [K("colp")], YGk)
                for tc in range(NTC):
                    sl = slice(tc * TW, (tc + 1) * TW)
                    ps, pk = ps_rot()
                    proj_fm(wv1, wk1, j * 128, tc, ps, pk)
                    zt, ztk = ARG[0], K("ARGz", 0)
                    S.act(zt[:], ps[:, :], AF.Silu, [pk], [ztk])
                    S.tt(YG[:, j, sl], YG[:, j, sl], zt[:], ALU.mult, [K("YG", j, tc), ztk], [K("YG", j, tc)])
            dump(f"yc{l}", YG[:], [K("YG", c, t) for c in range(2) for t in range(NTC)], [128, 2, S_])
            st.close()
            S.barrier()
            group_out(l, 2, YG, sty)
        S.barrier()
        if stop_after == f"C{l}":
            done = True
            break

        with ExitStack() as sty, ExitStack() as st:
            YG = sb("YG", [128, 2, S_], stack=sty)
            QA = sb("QA", [128, 4, S_], BF16, stack=st)
            KA = sb("KA", [128, 4, S_], BF16, stack=st)
            allqa = [K("QA", h_, t_) for h_ in range(4) for t_ in range(NTC)]
            allka = [K("KA", h_, t_) for h_ in range(4) for t_ in range(NTC)]
            S.add("dve", lambda e: e.memset(QA[:], 0.0), [], allqa, cost=4.5)
            S.add("dve", lambda e: e.memset(KA[:], 0.0), [], allka, cost=4.5)
            for h_ in range(4):
                o_ = 64 if h_ % 2 == 0 else 0
                S.dma(KA[o_:o_ + 8, h_, :], blkoh_d[:, :], allka, [K("KAoh", h_)], q="pool", arena=True)
            VB = sb("VB", [128, NT, 256], BF16, stack=st)
            CAUS = sb("CAUS", [128, 4, 512], BF16, stack=st)
            S.dma(CAUS[:], cmask_d[:, 4096:6144].rearrange("p (a b) -> p a b", a=4), [], [K("CAUS")], q="pool", arena=True)
            GC = sb("GC", [128, 3, 128], stack=st)
            S.dma(GC[:], gate_c_d[:, :].rearrange("p (a b) -> p a b", a=3), [], [K("GC")])
            KM = sb("KM", [128, 2, 8], stack=st)
            KMb = sb("KMb", [128, 2, 8], BF16, stack=st)
            wv, wk, _ = load_w(w_in_d[l, :, 2308:2820], 8, 512)
            wv2, wk2, _ = load_w(w_in_d[l, :, 2820:3076], 8, 256)
            str_ = ExitStack()
            ROPEb = [sb(f"ROPE{j}", [128, 2, TW], stack=str_) for j in range(1)]
            RAW = [sb(f"RAW{j}", [128, 512], stack=str_) for j in range(2)]
            TMP = [sb(f"TMPr{j}", [128, 512], stack=str_) for j in range(2)]
            KF = [sb(f"KF{j}", [128, 512], stack=str_) for j in range(2)]
            nr = 0
            for c2 in range(2):
                for tc in range(NTC):
                    sl = slice(tc * TW, (tc + 1) * TW)
                    rj = 0
                    ROPE = ROPEb[rj]
                    rsl = slice(0, TW)
                    S.dma(ROPE[:, 0, :], rope_d[:, tc * TW:(tc + 1) * TW], [], [K("ROPE", rj, 0)])
                    S.dma(ROPE[:, 1, :], rope_d[:, S_ + tc * TW:S_ + (tc + 1) * TW], [], [K("ROPE", rj, 1)])
                    for which in range(2):
                        ps, pk = ps_rot()
                        proj_fm(wv, wk, which * 256 + c2 * 128, tc, ps, pk)
                        raw, rawk = RAW[nr % 2], K("RAW", nr % 2)
                        tmp, tmpk = TMP[nr % 2], K("TMPr", nr % 2)
                        S.cp(raw[:], ps[:, :], [pk], [rawk], eng="act")
                        psw, pkw = ps_rot()
                        S.mm(psw[:, :], perm_f, raw[:], True, True, [rawk, K("c128")], [pkw])
                        S.tt(tmp[:], psw[:, :], ROPE[:, 1, rsl], ALU.mult, [pkw, K("ROPE", rj, 1)], [tmpk])
                        kf = KF[nr % 2]
                        dst, dk = kf[:], K("KF", nr % 2)
                        S.tt(dst, raw[:], ROPE[:, 0, rsl], ALU.mult, [rawk, K("ROPE", rj, 0)], [dk])
                        S.tt(dst, dst, tmp[:], ALU.add, [dk, tmpk], [dk])
                        if which == 0:
                            S.act(QA[0:64, 2 * c2, sl], kf[0:64, :], AF.Copy, [dk], [K("QA", 2 * c2, tc)], scale=0.125)
                            S.act(QA[64:128, 2 * c2 + 1, sl], kf[64:128, :], AF.Copy, [dk], [K("QA", 2 * c2 + 1, tc)],
                                  scale=0.125)
                        else:
                            S.cp(KA[0:64, 2 * c2, sl], kf[0:64, :], [dk], [K("KA", 2 * c2, tc)], eng="act")
                            S.cp(KA[64:128, 2 * c2 + 1, sl], kf[64:128, :], [dk], [K("KA", 2 * c2 + 1, tc)], eng="act")
                            for hb in range(2):
                                nblk = tc * 2 + hb
                                S.add("dve", (lambda e, o=KM[:, c2, nblk:nblk + 1], i_=kf[:, hb * 256:(hb + 1) * 256]:
                                              e.reduce_sum(out=o, in_=i_, axis=AX.X)), [dk], [K("KM", c2, nblk)])
                        nr += 1
            for i in range(NT):
                ps, pk = ps_rot()
                for kc in range(KC):
                    S.mm(ps[:, 0:256], hT[:, kc, i * 128:(i + 1) * 128], wv2[:, kc, :], kc == 0, kc == KC - 1,
                         wk2 + [K("hT", kc, i // 4)], [pk])
                S.cp(VB[:, i, :], ps[:, 0:256], [pk], [K("VB", i)], eng=("dve" if i % 2 else "act"))
            S.cp(KMb[:], KM[:], [K("KM", c2_, nb_) for c2_ in range(2) for nb_ in range(8)], [K("KMb")])
            str_.close()
            S.barrier()
            PTb = [sb(f"PTb{j}", [128, 512], BF16, stack=st) for j in range(3)]
            RD = [sb(f"RD{j}", [128, 512], stack=st) for j in range(2)]
            GT = sb("GT", [128, 128], stack=st)
            BT_ = sb("BIASTM", [128, 128], stack=st)
            MX = [sb(f"MX{j}", [128, 8], stack=st) for j in range(2)]
            for h in range(4):
                hh, c2 = h % 2, h // 2
                pr = slice(hh * 64, hh * 64 + 64)
                o_ = 64 if hh == 0 else 0
                psg, pkg = ps_rot()
                for i in range(NT):
                    S.mm(psg[:, i * 8:(i + 1) * 8], QA[pr, h, i * 128:(i + 1) * 128], KMb[pr, c2, :], True, True,
                         [K("QA", h, i // 4), K("KMb")], [pkg])
                S.stt(GT[:], psg[:, 0:128], 8.0 / 256.0, GC[:, 0, :], ALU.mult, ALU.add, [pkg, K("GC")], [K("GT")])
                for i in range(NT):
                    mx, mxk = MX[i % 2], K("MX", i % 2)
                    S.add("dve", (lambda e, o=mx[:], i_=GT[:, i * 8:(i + 1) * 8]: e.max(out=o, in_=i_)),
                          [K("GT")], [mxk])
                    bsl = BT_[:, i * 8:(i + 1) * 8]
                    S.ts(bsl, GT[:, i * 8:(i + 1) * 8], mx[:, 2:3], None, ALU.is_ge, None, [K("GT"), mxk],
                         [K("BIASTM", i)])
                    S.tt(bsl, bsl, GC[:, 1, i * 8:(i + 1) * 8], ALU.mult, [K("BIASTM", i), K("GC")], [K("BIASTM", i)])
                    S.tt(bsl, bsl, GC[:, 2, i * 8:(i + 1) * 8], ALU.add, [K("BIASTM", i), K("GC")], [K("BIASTM", i)])
                    S.ts(bsl, bsl, -1.0, -NEG, ALU.add, ALU.mult, [K("BIASTM", i)], [K("BIASTM", i)])
                for q4 in range(4):
                    pst, pkt = ps_rot()
                    for q in range(4):
                        i = q4 * 4 + q
                        S.tr(pst[0:8, q * 128:(q + 1) * 128], BT_[:, i * 8:(i + 1) * 8], ident,
                             [K("BIASTM", i), K("c128")], [pkt])
                    S.cp(QA[o_:o_ + 8, h, q4 * 512:(q4 + 1) * 512], pst[0:8, :], [pkt], [K("QAsel", h, q4)])
            its = []
            grp = 0
            for h in range(4):
                for c in range(NTC):
                    nkt = 4 * c + 4
                    for kt in range(nkt):
                        its.append(dict(h=h, c=c, kt=kt, nkt=nkt, grp=grp, i=len(its)))
                    grp += 1

            def m_s1(m):
                h, c, kt = m["h"], m["c"], m["kt"]
                d = kt - 4 * c
                qsl = slice(c * TW, (c + 1) * TW)
                ksl = slice(kt * 128, (kt + 1) * 128)
                off = 128 * max(d, 0)
                cs = slice(off, TW)
                qs = slice(c * TW + off, (c + 1) * TW)
                psS, pkS = ps_rot()
                S.mm(psS[:, cs], KA[:, h, ksl], QA[:, h, qs], True, d < 0,
                     [K("KA", h, kt // 4), K("KAoh", h), K("QA", h, c), K("QAsel", h, c)], [pkS])
                if d >= 0:
                    S.mm(psS[:, cs], ident_b, CAUS[:, d, cs], False, True, [K("CAUS"), K("c128b")], [pkS])
                pt, ptk = PTb[m["i"] % 3], K("PTb", m["i"] % 3)
                S.act(pt[:, cs], psS[:, cs], AF.Exp, [pkS], [ptk])

            def m_s2(m):
                h, c, kt, nkt = m["h"], m["c"], m["kt"], m["nkt"]
                hh, c2 = h % 2, h // 2
                pr = slice(hh * 64, hh * 64 + 64)
                qsl = slice(c * TW, (c + 1) * TW)
                g2 = m["grp"] % 2
                psO, pkO = PS[4 + g2], K("psO", g2)
                psD, pkD = PS[6 + g2], K("psD", g2)
                pt, ptk = PTb[m["i"] % 3], K("PTb", m["i"] % 3)
                off = 128 * max(kt - 4 * c, 0)
                cs = slice(off, TW)
                S.mm(psO[:, cs], VB[:, kt, c2 * 128:(c2 + 1) * 128], pt[:, cs], kt == 0, kt == nkt - 1,
                     [K("VB", kt), ptk], [pkO])
                S.mm(psD[:, cs], ones_b, pt[:, cs], kt == 0, kt == nkt - 1, [ptk, K("c128b")], [pkD])
                if kt == nkt - 1:
                    rd, rdk = RD[g2], K("RD", g2)
                    S.add("dve", (lambda e, o=rd[pr, :], i_=psD[pr, :]: e.reciprocal(out=o, in_=i_)), [pkD], [rdk], cost=2.2)
                    S.tt(YG[pr, c2, qsl], psO[pr, :], rd[pr, :], ALU.mult, [pkO, rdk], [K("YG", c2, c)])

            n_ = len(its)
            for r in range(-1, n_):
                if r + 1 < n_:
                    m_s1(its[r + 1])
                if r >= 0:
                    m_s2(its[r])
            dump(f"yd{l}", YG[:], [K("YG", c, t) for c in range(2) for t in range(NTC)], [128, 2, S_])
            st.close()
            S.barrier()
            group_out(l, 3, YG, sty)
        S.barrier()
        dump(f"x1_{l}", xT[:], ALLX, [128, KC, S_])
        if stop_after == f"D{l}":
            done = True
            break

        with ExitStack() as st:
            with ExitStack() as st2:
                rmsnorm_fm(xT, "xT", lambda kc: col(l, "xat_g", kc), hT, "hT", KC, S_, st2, tag="x")
            S.barrier()
            memT = sb("memT", [128, KC, 256], stack=st)
            MTb = sb("MTb", [128, KC, 256], BF16, stack=st)
            with ExitStack() as st2:
                load_fm(memT, mem_d, 2, "memT", st2)
                rmsnorm_fm(memT, "memT", lambda kc: col(l, "mem_g", kc), MTb, "MTb", KC, 256, st2, tag="m")
            S.barrier()
            KTb = sb("KTb", [128, KC, 256], BF16, stack=st)
            VX = sb("VX", [128, 2, D_], BF16, stack=st)
            QX = sb("QX", [128, KC, S_], BF16, stack=st)
            MTk = [K("MTb", kc, 0) for kc in range(KC)]
            for half in range(2):
                wv, wk, _ = load_w(wkv_d[l, :, half * 512:(half + 1) * 512], 8, 512)
                for o4 in range(4):
                    oc = half * 4 + o4
                    ps, pk = ps_rot()
                    for kc in range(KC):
                        S.mm(ps[:, 0:256], wv[:, kc, o4 * 128:(o4 + 1) * 128], MTb[:, kc, :], kc == 0, kc == KC - 1,
                             wk + [K("MTb", kc, 0)], [pk])
                    S.cp(KTb[:, oc, :], ps[:, 0:256], [pk], [K("KTb", oc)])
            for half in range(2):
                wv, wk, _ = load_w(wkv_d[l, :, 1024 + half * 512:1024 + (half + 1) * 512], 8, 512)
                for mt in range(2):
                    ps, pk = ps_rot()
                    for kc in range(KC):
                        S.mm(ps[:, :], MTb[:, kc, mt * 128:(mt + 1) * 128], wv[:, kc, :], kc == 0, kc == KC - 1,
                             wk + [K("MTb", kc, 0)], [pk])
                    S.cp(VX[:, mt, half * 512:(half + 1) * 512], ps[:, :], [pk], [K("VX", mt, half)], eng="act")
            for half in range(2):
                wv, wk, _ = load_w(wq_d[l, :, half * 512:(half + 1) * 512], 8, 512)
                for o4 in range(4):
                    oc = half * 4 + o4
                    for tc in range(NTC):
                        ps, pk = ps_rot()
                        proj_fm(wv, wk, o4 * 128, tc, ps, pk)
                        S.act(QX[:, oc, tc * TW:(tc + 1) * TW], ps[:, :], AF.Copy, [pk], [K("QX", oc, tc)],
                              scale=1.0 / 16.0)
            dump(f"X_MTb{l}", MTb[:], MTk, [128, KC, 256])
            dump(f"X_KTb{l}", KTb[:], [K("KTb", oc) for oc in range(KC)], [128, KC, 256])
            dump(f"X_QX{l}", QX[:], [K("QX", oc, tc) for oc in range(KC) for tc in range(NTC)], [128, KC, S_])
            dump(f"X_VX{l}", VX[:], [K("VX", mt, hf) for mt in range(2) for hf in range(2)], [128, 2, D_])
            PT = [sb(f"PTx{j}", [128, 512], BF16, stack=st) for j in range(4)]
            RDx = [sb(f"RDx{j}", [128, 512], stack=st) for j in range(2)]
            it = 0
            for hd in range(4):
                for tc in range(NTC):
                    sl = slice(tc * TW, (tc + 1) * TW)
                    pts = []
                    for mt in range(2):
                        ps, pk = ps_rot()
                        for j in range(2):
                            S.mm(ps[:, :], KTb[:, 2 * hd + j, mt * 128:(mt + 1) * 128], QX[:, 2 * hd + j, sl],
                                 j == 0, j == 1, [K("KTb", 2 * hd + j), K("QX", 2 * hd + j, tc)], [pk])
                        pt, ptk = PT[it % 4], K("PTx", it % 4)
                        S.act(pt[:], ps[:, :], AF.Exp, [pk], [ptk])
                        pts.append((pt, ptk))
                        it += 1
                    psD, pkD = ps_rot()
                    for mt in range(2):
                        S.mm(psD[:, :], ones_b, pts[mt][0][:], mt == 0, mt == 1, [pts[mt][1], K("c128b")], [pkD])
                    rd, rdk = RDx[(hd * NTC + tc) % 2], K("RDx", (hd * NTC + tc) % 2)
                    S.add("dve", (lambda e, o=rd[:], i_=psD[:, :]: e.reciprocal(out=o, in_=i_)), [pkD], [rdk], cost=2.2)
                    for j in range(2):
                        oc = 2 * hd + j
                        ps, pk = ps_rot()
                        for mt in range(2):
                            S.mm(ps[:, :], VX[:, mt, oc * 128:(oc + 1) * 128], pts[mt][0][:], mt == 0, mt == 1,
                                 [K("VX", mt, oc // 4), pts[mt][1]], [pk])
                        S.tt(hT[:, oc, sl], ps[:, :], rd[:], ALU.mult, [pk, rdk], [K("hT", oc, tc)])
            dump(f"X_OX{l}", hT[:], ALLH, [128, KC, S_])
            for half in range(2):
                wv, wk, _ = load_w(wo_d[l, :, half * 512:(half + 1) * 512], 8, 512)
                for o4 in range(4):
                    oc = half * 4 + o4
                    for tc in range(NTC):
                        ps, pk = ps_rot()
                        proj_fm(wv, wk, o4 * 128, tc, ps, pk)
                        S.tt(xT[:, oc, tc * TW:(tc + 1) * TW], xT[:, oc, tc * TW:(tc + 1) * TW], ps[:, :], ALU.add,
                             [K("xT", oc, tc), pk], [K("xT", oc, tc)])
        S.barrier()
        dump(f"x2_{l}", xT[:], ALLX, [128, KC, S_])
        if stop_after == f"X{l}":
            done = True
            break

        with ExitStack() as st:
            ENp = sb("ENp", [128, 16, 128], BF16, stack=st)
            S.add("dve", lambda e: e.memset(ENp[:], 0.0), [], [K("ENpz")])
            S.dma(ENp[0:16, :, :], en_d[:, :].rearrange("p (a b) -> p a b", a=16), [K("ENpz")], [K("ENf")], q="pool",
                  arena=True)
            CTh = sb("CTh", [128, S_], BF16, stack=st)
            CTl = sb("CTl", [128, S_], BF16, stack=st)
            S.add("dve", lambda e: e.memset(CTh[:], 0.0), [], [K("CThz")])
            S.add("dve", lambda e: e.memset(CTl[:], 0.0), [], [K("CTlz")])
            st_rt = ExitStack()
            WR = sb("WR", [128, KC, 20], stack=st_rt)
            S.dma(WR[:], wrt_d[l].rearrange("(kc p) n -> p kc n", p=128), [], [K("WR")])
            LG = sb("LG", [128, NT, 20], stack=st_rt)
            st_hf = ExitStack()
            HF = sb("HF", [128, KC, TW], stack=st_hf)

            def router_hook(tc, r):
                sl = slice(tc * TW, (tc + 1) * TW)
                for kc in range(KC):
                    S.stt(HF[:, kc, :], xT[:, kc, sl], col(l, "ffn_g", kc), r[:], ALU.mult, ALU.mult,
                          [K("xT", kc, tc), K("rsf", tc % 2), K("colp")], [K("HF", kc)])
                ps, pk = ps_rot()
                for q in range(4):
                    for kc in range(KC):
                        S.mm(ps[:, q * 20:(q + 1) * 20], HF[:, kc, q * 128:(q + 1) * 128], WR[:, kc, :], kc == 0,
                             kc == KC - 1, [K("HF", kc), K("WR")], [pk])
                S.cp(LG[:, tc * 4:tc * 4 + 4, :], ps[:, 0:80].rearrange("p (a b) -> p a b", a=4), [pk],
                     [K("LG", tc)])

            with ExitStack() as st2:
                rmsnorm_fm(xT, "xT", lambda kc: col(l, "ffn_g", kc), hT, "hT", KC, S_, st2, hook=router_hook, tag="f")
            st_hf.close()
            S.barrier()
            COMB = sb("COMB", [128, NT, 16], stack=st_rt)
            CT = sb("CT", [16, S_], stack=st_rt)
            sm = {nm: sb("rt_" + nm, [128, NT, w_] if w_ > 1 else [128, NT], stack=st_rt) for nm, w_ in
                  [("lg", 4), ("m", 1), ("eg", 4), ("sg", 1), ("oh", 4), ("le", 16), ("sel", 4), ("tmp", 4),
                   ("a", 1), ("eq", 4), ("sel2", 4), ("b", 1), ("t2", 4), ("ex", 4), ("dd", 1), ("wg", 4)]}

            def v(nm):
                return sm[nm][:]

            def kk(nm):
                return [K("rt", nm)]

            def B3(ap2, w_=4):
                return ap2.unsqueeze(2).to_broadcast([128, NT, w_])

            LGk = [K("LG", t) for t in range(NTC)]
            bgB = row(l, "bg").unsqueeze(1).to_broadcast([128, NT, 4])
            beB = row(l, "be").unsqueeze(1).to_broadcast([128, NT, 16])
            S.tt(v("lg"), LG[:, :, 0:4], bgB, ALU.add, LGk + [K("rowp")], kk("lg"))
            S.add("dve", lambda e: e.reduce_max(out=v("m"), in_=v("lg"), axis=AX.X), kk("lg"), kk("m"))
            S.tt(v("lg"), v("lg"), B3(v("m")), ALU.subtract, kk("lg") + kk("m"), kk("lg"))
            S.act(v("eg"), v("lg"), AF.Exp, kk("lg"), kk("eg"))
            S.add("dve", lambda e: e.reduce_sum(out=v("sg"), in_=v("eg"), axis=AX.X), kk("eg"), kk("sg"))
            S.add("dve", lambda e: e.reciprocal(out=v("sg"), in_=v("sg")), kk("sg"), kk("sg"))
            S.ts(v("oh"), v("lg"), 0.0, None, ALU.is_ge, None, kk("lg"), kk("oh"))
            S.tt(v("le"), LG[:, :, 4:20], beB, ALU.add, LGk + [K("rowp")], kk("le"))
            for g in range(4):
                ohg = sm["oh"][:, :, g:g + 1].to_broadcast([128, NT, 4])
                if g == 0:
                    S.tt(v("sel"), sm["le"][:, :, 0:4], ohg, ALU.mult, kk("le") + kk("oh"), kk("sel"))
                else:
                    S.tt(v("tmp"), sm["le"][:, :, 4 * g:4 * g + 4], ohg, ALU.mult, kk("le") + kk("oh"), kk("tmp"))
                    S.tt(v("sel"), v("sel"), v("tmp"), ALU.add, kk("sel") + kk("tmp"), kk("sel"))
            S.add("dve", lambda e: e.reduce_max(out=v("a"), in_=v("sel"), axis=AX.X), kk("sel"), kk("a"))
            S.tt(v("sel"), v("sel"), B3(v("a")), ALU.subtract, kk("sel") + kk("a"), kk("sel"))
            S.ts(v("eq"), v("sel"), 0.0, None, ALU.is_ge, None, kk("sel"), kk("eq"))
            S.stt(v("sel2"), v("eq"), -1e30, v("sel"), ALU.mult, ALU.add, kk("eq") + kk("sel"), kk("sel2"))
            S.add("dve", lambda e: e.reduce_max(out=v("b"), in_=v("sel2"), axis=AX.X), kk("sel2"), kk("b"))
            S.tt(v("t2"), v("sel"), B3(v("b")), ALU.is_ge, kk("sel") + kk("b"), kk("t2"))
            S.act(v("ex"), v("sel"), AF.Exp, kk("sel"), kk("ex"))
            S.act(v("dd"), v("b"), AF.Exp, kk("b"), kk("dd"))
            S.ts(v("dd"), v("dd"), 1.0, None, ALU.add, None, kk("dd"), kk("dd"))
            S.add("dve", lambda e: e.reciprocal(out=v("dd"), in_=v("dd")), kk("dd"), kk("dd"))
            S.tt(v("wg"), v("ex"), v("t2"), ALU.mult, kk("ex") + kk("t2"), kk("wg"))
            S.tt(v("wg"), v("wg"), B3(v("dd")), ALU.mult, kk("wg") + kk("dd"), kk("wg"))
            S.tt(v("wg"), v("wg"), B3(v("sg")), ALU.mult, kk("wg") + kk("sg"), kk("wg"))
            for g in range(4):
                ohg = sm["oh"][:, :, g:g + 1].to_broadcast([128, NT, 4])
                S.tt(COMB[:, :, 4 * g:4 * g + 4], v("wg"), ohg, ALU.mult, kk("wg") + kk("oh"),
                     [K("COMB", i, g) for i in range(NT)])
            for q4 in range(4):
                pst, pkt = ps_rot()
                for q in range(4):
                    i = q4 * 4 + q
                    S.tr(pst[0:16, q * 128:(q + 1) * 128], COMB[:, i, :], ident,
                         [K("COMB", i, g) for g in range(4)] + [K("c128")], [pkt])
                S.cp(CT[0:16, q4 * 512:(q4 + 1) * 512], pst[0:16, :], [pkt], [K("CT", q4)])
                S.cp(CTh[0:16, q4 * 512:(q4 + 1) * 512], CT[0:16, q4 * 512:(q4 + 1) * 512], [K("CT", q4), K("CThz")],
                     [K("CTh", q4)])
                S.tt(CT[0:16, q4 * 512:(q4 + 1) * 512], CT[0:16, q4 * 512:(q4 + 1) * 512],
                     CTh[0:16, q4 * 512:(q4 + 1) * 512], ALU.subtract, [K("CT", q4), K("CTh", q4)], [K("CT", q4)])
                S.cp(CTl[0:16, q4 * 512:(q4 + 1) * 512], CT[0:16, q4 * 512:(q4 + 1) * 512], [K("CT", q4), K("CTlz")],
                     [K("CTl", q4)])
            st_rt.close()
            S.barrier()
            HID = sb("HID", [128, 8, S_], BF16, stack=st)
            W2S = sb("W2S", [128, 8, D_], BF16, stack=st)
            CB = [sb(f"CB{j}", [128, 512], stack=st) for j in range(2)]
            SL = [sb(f"SL{j}", [128, 512], stack=st) for j in range(2)]
            n = 0
            for g in range(4):
                for el in range(4):
                    ex = 4 * g + el
                    wv, wk1_, slot = load_w(w1_d[l, ex], 8, 256, c0=0, tot=512)
                    _, wk3_, _ = load_w(w3_d[l, ex], 8, 256, slot=slot, c0=256, tot=512)
                    wk = wk1_ + wk3_
                    for tc in range(NTC):
                        sl = slice(tc * TW, (tc + 1) * TW)
                        psC, pkC = ps_rot()
                        S.mm(psC[:, :], ENp[:, ex, :], CTh[:, sl], True, False, [K("ENf"), K("CTh", tc), K("CThz")], [pkC])
                        S.mm(psC[:, :], ENp[:, ex, :], CTl[:, sl], False, True, [K("ENf"), K("CTl", tc), K("CTlz")], [pkC])
                        cb, cbk = CB[n % 2], K("CB", n % 2)
                        S.cp(cb[:], psC[:, :], [pkC], [cbk], eng="act")
                        for fc in range(2):
                            ps1, pk1 = ps_rot()
                            proj_fm(wv, wk, fc * 128, tc, ps1, pk1)
                            ps3, pk3 = ps_rot()
                            proj_fm(wv, wk, 256 + fc * 128, tc, ps3, pk3)
                            s_, sk = SL[(2 * n + fc) % 2], K("SL", (2 * n + fc) % 2)
                            S.act(s_[:], ps1[:, :], AF.Silu, [pk1], [sk])
                            S.tt(s_[:], s_[:], ps3[:, :], ALU.mult, [sk, pk3], [sk])
                            S.tt(HID[:, el * 2 + fc, sl], s_[:], cb[:], ALU.mult, [sk, cbk], [K("HID", el * 2 + fc, tc)])
                        n += 1
                    S.dma(W2S[:, 2 * el:2 * el + 2, :], w2_d[l, ex].rearrange("(fc p) n -> p fc n", p=128), [],
                          [K("W2S", el)], q="pool", arena=True)
                for oc in range(KC):
                    for tc in range(NTC):
                        ps, pk = ps_rot()
                        for j in range(8):
                            S.mm(ps[:, :], W2S[:, j, oc * 128:(oc + 1) * 128], HID[:, j, tc * TW:(tc + 1) * TW],
                                 j == 0, j == 7, [K("W2S", j // 2), K("HID", j, tc)], [pk])
                        S.tt(xT[:, oc, tc * TW:(tc + 1) * TW], xT[:, oc, tc * TW:(tc + 1) * TW], ps[:, :], ALU.add,
                             [K("xT", oc, tc), pk], [K("xT", oc, tc)])
        S.barrier()
        dump(f"x3_{l}", xT[:], ALLX, [128, KC, S_])
        if stop_after == f"M{l}":
            done = True
            break

    if stop_after is None:
        with ExitStack() as st:
            HF = sb("HFo", [128, KC, TW], stack=st)
            OB = [sb(f"OB{j}", [128, D_], stack=st) for j in range(2)]
            nob = [0]

            def final_hook(tc, r):
                sl = slice(tc * TW, (tc + 1) * TW)
                for kc in range(KC):
                    S.stt(HF[:, kc, :], xT[:, kc, sl], col(0, "fin_g", kc), r[:], ALU.mult, ALU.mult,
                          [K("xT", kc, tc), K("rsz", tc % 2), K("colp")], [K("HFo", kc)])
                for q in range(4):
                    ob, obk = OB[nob[0] % 2], K("OB", nob[0] % 2)
                    nob[0] += 1
                    for half in range(2):
                        ps, pk = ps_rot()
                        for j in range(4):
                            S.tr(ps[:, j * 128:(j + 1) * 128], HF[:, half * 4 + j, q * 128:(q + 1) * 128], ident,
                                 [K("HFo", half * 4 + j), K("c128")], [pk])
                        S.cp(ob[:, half * 512:(half + 1) * 512], ps[:, :], [pk], [obk],
                             eng=("act" if half else "dve"))
                    r0 = tc * TW + q * 128
                    i = S.dma(out_d[r0:r0 + 128, :], ob[:], [obk], [K("out", r0)])
                    S.final.append(i)

            rmsnorm_fm(xT, "xT", lambda kc: col(0, "fin_g", kc), None, None, KC, S_, st, hook=final_hook, tag="z")

    S.finalize(es)
    es.close()
    return nc, dump_d


def make_consts():
    c128 = np.zeros((128, 8, 128), np.float32)
    j = np.arange(128)[:, None]
    s = np.arange(128)[None, :]
    c128[:, 0] = np.eye(128)
    c128[:, 1] = 1.0
    c128[:, 2] = (j <= s)
    c128[:, 3] = -1.0 * (j >= s)
    c128[:, 4] = -1.0
    partner = np.where((np.arange(128) % 64) < 32, np.arange(128) + 32, np.arange(128) - 32)
    perm = np.zeros((128, 128), np.float32)
    perm[partner, np.arange(128)] = 1.0
    c128[:, 5] = perm
    c128[:, 6] = NEG * (s < j)
    cm = np.zeros((128, 12, 512), np.float32)
    p = np.arange(128)[:, None]
    f = np.arange(512)[None, :]
    for d in range(4):
        valid = (f - p) > d * 128
        cm[:, d] = valid
        cm[:, 4 + d] = NEG * (~valid)
        cm[:, 8 + d] = NEG * ((d * 128 + p) > f)
    half = 32
    inv = (np.float32(10000.0) ** (-(np.arange(half, dtype=np.float32)) / np.float32(half))).astype(np.float32)
    ang = np.arange(S_, dtype=np.float32)[None, :] * inv[:, None]
    cos = np.cos(ang).astype(np.float32)
    sin = np.sin(ang).astype(np.float32)
    rope = np.zeros((128, 2, S_), np.float32)
    for pp in range(128):
        d = pp % 64
        rope[pp, 0] = cos[d % 32]
        rope[pp, 1] = -sin[d % 32] if d < 32 else sin[d % 32]
    en = np.zeros((16, 16, 128), np.float32)
    for n in range(16):
        en[n, n, :] = 1.0
    gc = np.zeros((128, 3, 16, 8), np.float32)
    for i in range(16):
        own = i // 2
        for n in range(8):
            gc[:, 0, i, n] = 0.0 if n < own else -1e30
            gc[:, 1, i, n] = 1.0 if n < own else 0.0
            gc[:, 2, i, n] = 1.0 if n == own else 0.0
    blkoh = np.zeros((8, S_), np.float32)
    for n in range(8):
        blkoh[n, n * 256:(n + 1) * 256] = 1.0
    return dict(c128=c128.reshape(128, -1), cmask=cm.reshape(128, -1), rope=rope.reshape(128, -1),
                en=en.reshape(16, -1), gatec=gc.reshape(128, -1), blkoh=blkoh)


def colvec(v):
    v = np.asarray(v, np.float32)
    return np.ascontiguousarray(v.reshape(-1, 128).T)


def pack_params(inp):
    colp = np.zeros((128, L_, NCOL), np.float32)
    rowp = np.zeros((128, L_, NROW), np.float32)

    def put(l, name, arr):
        c0, w = COLS[name]
        assert arr.shape == (128, w), (name, arr.shape)
        colp[:, l, c0:c0 + w] = arr

    for l in range(L_):
        put(l, "mix_g", colvec(inp["mix_norm_g"][l]))
        put(l, "xat_g", colvec(inp["xattn_norm_g"][l]))
        put(l, "mem_g", colvec(inp["mem_norm_g"][l]))
        put(l, "ffn_g", colvec(inp["ffn_norm_g"][l]))
        put(l, "grp_g", colvec(inp["group_norm_g"][l]))
        cw = inp["lru_conv_w"][l]
        put(l, "lru_cw", np.concatenate([colvec(cw[k]) for k in range(4)], axis=1).reshape(128, 4, 2)
            .transpose(0, 2, 1).reshape(128, 8))
        put(l, "lru_cb", colvec(inp["lru_conv_b"][l]))
        put(l, "lru_br", colvec(inp["lru_br"][l]))
        put(l, "lru_bi", colvec(inp["lru_bi"][l]))
        put(l, "lru_lam", colvec(inp["lru_lambda"][l]))
        sw = inp["ssm_conv_w"][l]
        put(l, "ssm_cw", np.concatenate([colvec(sw[k]) for k in range(4)], axis=1).reshape(128, 4, 6)
            .transpose(0, 2, 1).reshape(128, 24))
        put(l, "ssm_cb", colvec(inp["ssm_conv_b"][l]))
        put(l, "ssm_d", colvec(np.repeat(inp["ssm_d"][l], 64)))
        put(l, "fin_g", colvec(inp["final_norm_g"]))
        r0, w = ROWS["dt_bias"]
        rowp[:, l, r0:r0 + w] = np.tile(inp["ssm_dt_bias"][l], 16)[None, :]
        r0, w = ROWS["a_log"]
        rowp[:, l, r0:r0 + w] = np.tile(inp["ssm_a_log"][l], 16)[None, :]
        r0, w = ROWS["bg"]
        rowp[:, l, r0:r0 + w] = inp["router_group_b"][l][None, :]
        r0, w = ROWS["be"]
        rowp[:, l, r0:r0 + w] = inp["router_expert_b"][l][None, :]
    wbd = np.zeros((L_, 2, 2, 128, 128), np.float32)
    for l in range(L_):
        for gi, nm in enumerate(("lru_wr", "lru_wi")):
            for c in range(2):
                for hh in range(2):
                    wbd[l, gi, c, hh * 64:(hh + 1) * 64, hh * 64:(hh + 1) * 64] = inp[nm][l, 2 * c + hh]
    router_w = np.concatenate([inp["router_group_w"], inp["router_expert_w"]], axis=2)
    return dict(colp=colp.reshape(128, -1), rowp=rowp.reshape(128, -1), lru_wbd=wbd,
                router_w=np.ascontiguousarray(router_w))


SHARED = ["w_in", "w_out", "xattn_wq", "xattn_wkv", "xattn_wo", "expert_w1", "expert_w3", "expert_w2"]


def make_in_maps(inputs, cores):
    inp = {k: np.asarray(v) for k, v in inputs.items()}
    consts = make_consts()
    packed = pack_params(inp)
    shared = {k: np.ascontiguousarray(inp[k], dtype=np.float32) for k in SHARED}
    maps = []
    for b in cores:
        m = dict(shared)
        m.update(consts)
        m.update(packed)
        m["x"] = np.ascontiguousarray(inp["x"][b])
        m["mem"] = np.ascontiguousarray(inp["mem"][b])
        maps.append(m)
    return maps


_CACHE = {}


def kernel(**inputs):
    if "nc" not in _CACHE:
        _CACHE["nc"] = build_program()[0]
    nc = _CACHE["nc"]
    maps = make_in_maps(inputs, list(range(8)))
    res = run_bass_kernel_spmd(nc, maps, core_ids=list(range(8)))
    return np.stack([r["out"] for r in res.results], axis=0).astype(np.float32)
```

```python
import numpy as np
from contextlib import ExitStack
import concourse.bass as bass
import concourse.mybir as mybir
from concourse.bass_utils import run_bass_kernel_spmd

F32 = mybir.dt.float32
BF16 = mybir.dt.bfloat16
F32R = mybir.dt.float32r
AF = mybir.ActivationFunctionType
ALU = mybir.AluOpType
AX = mybir.AxisListType

S_ = 2048
D_ = 1024
L_ = 2
KC = 8
NTC = 4
TW = 512
NT = 16
NEG = -30000.0
EPS = 1e-6

COLS = {}
_c = 0
for _n, _w in [("mix_g", 8), ("xat_g", 8), ("mem_g", 8), ("ffn_g", 8), ("grp_g", 8),
               ("lru_cw", 8), ("lru_cb", 2), ("lru_br", 2), ("lru_bi", 2), ("lru_lam", 2),
               ("ssm_cw", 24), ("ssm_cb", 6), ("ssm_d", 2), ("fin_g", 8)]:
    COLS[_n] = (_c, _w)
    _c += _w
NCOL = _c
ROWS = {}
_c = 0
for _n, _w in [("dt_bias", 64), ("a_log", 64), ("bg", 4), ("be", 16)]:
    ROWS[_n] = (_c, _w)
    _c += _w
NROW = _c


def _free(ap):
    n = 1
    for d in ap.shape[1:]:
        n *= int(d)
    return n


class Sched:
    ENG = ("pe", "act", "dve", "pool", "sp")
    GATED = ("pe", "act", "dve", "sp")

    def __init__(self, nc):
        self.nc = nc
        self.ops = []
        self.lastw = {}
        self.rd = {}
        self.by_eng = {e: [] for e in self.ENG}
        self.final = []
        self.psn = 0
        self.seg = 0

    def add(self, eng, fn, reads=(), writes=(), dma=False, cost=0.3, arena=False):
        i = len(self.ops)
        reads = list(reads)
        writes = list(writes)
        deps = {}
        for r in reads:
            w = self.lastw.get(r)
            if w is not None:
                deps.setdefault(w, set()).add("raw")
        for w_ in writes:
            w = self.lastw.get(w_)
            if w is not None:
                deps.setdefault(w, set()).add("waw")
            for r in self.rd.get(w_, ()):
                deps.setdefault(r, set()).add("war")
        ws = set(writes)
        for r in reads:
            if r not in ws:
                self.rd.setdefault(r, []).append(i)
        for w_ in writes:
            self.lastw[w_] = i
            self.rd[w_] = []
        deps.pop(i, None)
        fdeps = set()
        for d, kinds in deps.items():
            od = self.ops[d]
            if od["eng"] == eng and not od["dma"] and not dma:
                if eng == "pe":
                    continue
                if "raw" not in kinds:
                    continue
            fdeps.add(d)
        self.ops.append(dict(eng=eng, fn=fn, deps=fdeps, order=set(deps.keys()), dma=dma, signal=dma, token=None,
                             seg=self.seg, cost=cost, arena=arena))
        self.by_eng[eng].append(i)
        return i

    def barrier(self):
        self.seg += 1

    def schedule(self, W=24):
        ops = self.ops
        n = len(ops)
        succ = [[] for _ in range(n)]
        nun = [0] * n
        for i, op in enumerate(ops):
            nun[i] = len(op["order"])
            for d in op["order"]:
                succ[d].append(i)
        ready = [0.0] * n
        fin = [0.0] * n
        done = [False] * n
        head = {e: 0 for e in self.ENG}
        free = {e: 0.0 for e in self.ENG}
        lists = {e: list(self.by_eng[e]) for e in self.ENG}
        new_order = {e: [] for e in self.ENG}
        nseg = self.seg + 1
        rem = [0] * (nseg + 1)
        for i, op in enumerate(ops):
            if op["eng"] in self.GATED:
                rem[op["seg"]] += 1
        import os
        WDEF = {"pe": W, "act": 1, "dve": W}
        WE = {e_: int(os.environ.get("KSCHED_W_" + e_.upper(), WDEF[e_])) for e_ in ("pe", "act", "dve")}
        segbusy = {}
        segend = {}
        cur = 0
        seg_t0 = 0.0
        tmax = 0.0
        nsched = 0
        while nsched < n:
            while cur < nseg and rem[cur] == 0:
                cur += 1
                seg_t0 = tmax
            best = None
            for e in self.ENG:
                lst = lists[e]
                h = head[e]
                while h < len(lst) and done[lst[h]]:
                    h += 1
                head[e] = h
                w = WE.get(e, 1)
                seen = 0
                j = h
                while j < len(lst) and seen < w:
                    i = lst[j]
                    j += 1
                    if done[i]:
                        continue
                    op = ops[i]
                    if e != "pool":
                        if op["seg"] != cur:
                            break
                    elif op["arena"] and op["seg"] > cur:
                        break
                    seen += 1
                    if nun[i] == 0:
                        est = max(free[e], ready[i])
                        if e != "pool" or op["arena"]:
                            est = max(est, seg_t0)
                        key = (est, i)
                        if best is None or key < best[0]:
                            best = (key, e, i)
            if best is None:
                raise RuntimeError("scheduler stuck at seg %d" % cur)
            (est, _), e, i = best
            op = ops[i]
            if op["dma"]:
                free[e] = est + 0.06
                f = est + op["cost"]
            else:
                f = est + op["cost"]
                free[e] = f
            fin[i] = f
            tmax = max(tmax, f)
            if not op["dma"]:
                segbusy[(op["seg"], e)] = segbusy.get((op["seg"], e), 0.0) + op["cost"]
            segend[op["seg"]] = max(segend.get(op["seg"], 0.0), f)
            done[i] = True
            nsched += 1
            new_order[e].append(i)
            if e in self.GATED:
                rem[op["seg"]] -= 1
            for s_ in succ[i]:
                nun[s_] -= 1
                lat = 0.0 if (ops[s_]["eng"] == e and not op["dma"]) else 0.1
                if f + lat > ready[s_]:
                    ready[s_] = f + lat
        self.by_eng = new_order
        self.sim_time = tmax
        import os
        if os.environ.get("KSCHED_DEBUG"):
            print("sched: ops", n, "sim_time_us", round(tmax, 1), flush=True)
            prev = 0.0
            for sg in sorted(segend):
                print("  seg", sg, "dur", round(segend[sg] - prev, 1), {e_: round(segbusy.get((sg, e_), 0.0), 1)
                                                                         for e_ in ("pe", "act", "dve")})
                prev = segend[sg]
        pos = {}
        for e in self.ENG:
            for k, i in enumerate(new_order[e]):
                pos[i] = k
        bar = [set() for _ in range(nseg + 1)]
        for s_ in range(1, nseg + 1):
            for e in ("pe", "act", "dve"):
                last = None
                for i in new_order[e]:
                    if ops[i]["seg"] < s_:
                        last = i
                    else:
                        break
                if last is not None:
                    bar[s_].add(last)
            for i in new_order["sp"]:
                if ops[i]["seg"] == s_ - 1 and ops[i]["dma"]:
                    bar[s_].add(i)
        for e in self.GATED:
            lastseg = 0
            for i in new_order[e]:
                sg = ops[i]["seg"]
                if sg > lastseg:
                    for t in range(lastseg + 1, sg + 1):
                        ops[i]["deps"] |= bar[t]
                    lastseg = sg
        for i in new_order["pool"]:
            if ops[i]["arena"]:
                ops[i]["deps"] |= bar[ops[i]["seg"]]
        for i, op in enumerate(ops):
            keep = set()
            bestp = {}
            for d in op["deps"]:
                if d == i:
                    continue
                od = ops[d]
                if od["dma"]:
                    keep.add(d)
                else:
                    pe_ = od["eng"]
                    if pe_ not in bestp or pos[d] > pos[bestp[pe_]]:
                        bestp[pe_] = d
            keep |= set(bestp.values())
            op["deps"] = keep

    def mm(self, out, lhsT, rhs, start, stop, reads, writes):
        nfree = _free(rhs)
        c = max(nfree, 64) / 2400.0 * (4.0 if rhs.dtype == F32 else 1.0) + 0.004
        return self.add("pe", lambda e: e.matmul(out, lhsT, rhs, start=start, stop=stop), reads, writes, cost=c)

    def tr(self, out, in_, ident, reads, writes):
        return self.add("pe", lambda e: e.transpose(out, in_, ident), reads, writes, cost=0.25)

    def act(self, out, in_, func, reads, writes, bias=None, scale=None, accum=None):
        kw = {}
        if bias is not None:
            kw["bias"] = bias
        if scale is not None:
            kw["scale"] = scale
        if accum is not None:
            kw["accum_out"] = accum
        c = 0.22 + _free(out) / 1400.0
        return self.add("act", lambda e: e.activation(out=out, in_=in_, func=func, **kw), reads, writes, cost=c)

    def _vc(self, out):
        return 0.08 + _free(out) / 960.0

    def tt(self, out, in0, in1, op, reads, writes, eng="dve"):
        return self.add(eng, lambda e: e.tensor_tensor(out=out, in0=in0, in1=in1, op=op), reads, writes,
                        cost=self._vc(out))

    def ts(self, out, in0, s1, s2, op0, op1, reads, writes, eng="dve"):
        if s2 is None:
            return self.add(eng, lambda e: e.tensor_scalar(out, in0, s1, None, op0), reads, writes, cost=self._vc(out))
        return self.add(eng, lambda e: e.tensor_scalar(out, in0, s1, s2, op0, op1), reads, writes, cost=self._vc(out))

    def stt(self, out, in0, scalar, in1, op0, op1, reads, writes, eng="dve"):
        return self.add(eng, lambda e: e.scalar_tensor_tensor(out=out, in0=in0, scalar=scalar, in1=in1,
                                                             op0=op0, op1=op1), reads, writes, cost=self._vc(out))

    def cp(self, out, in_, reads, writes, eng="dve"):
        if eng == "act":
            return self.add("act", lambda e: e.activation(out=out, in_=in_, func=AF.Copy), reads, writes,
                            cost=0.22 + _free(out) / 1400.0)
        return self.add(eng, lambda e: e.tensor_copy(out=out, in_=in_), reads, writes, cost=self._vc(out))

    def recip(self, out, in_, reads, writes):
        return self.add("dve", lambda e: e.reciprocal(out=out, in_=in_), reads, writes, cost=0.1 + _free(out) / 240.0)

    def dma(self, out, in_, reads, writes, q="sp", arena=False):
        nbytes = 128 * _free(out) * 4
        return self.add(q, lambda e: e.dma_start(out=out, in_=in_), reads, writes, dma=True,
                        cost=2.0 + nbytes / 150e3, arena=arena)

    def finalize(self, es):
        nc = self.nc
        self.schedule()
        for op in self.ops:
            for d in op["deps"]:
                self.ops[d]["signal"] = True
        for d in self.final:
            self.ops[d]["signal"] = True
        csem = {e: es.enter_context(nc.semaphore("c_" + e)) for e in ("pe", "act", "dve", "pool")}
        NP = 16
        qsem = {q: [es.enter_context(nc.semaphore(f"q_{q}{k}")) for k in range(NP)] for q in ("sp", "pool")}
        cnt = {e: 0 for e in csem}
        qn = {q: 0 for q in qsem}
        quse = {q: [0] * NP for q in qsem}
        qprev = {q: [None] * NP for q in qsem}
        for i, op in enumerate(self.ops):
            if op["dma"]:
                q = op["eng"]
                k = qn[q] % NP
                qn[q] += 1
                quse[q][k] += 1
                op["token"] = (qsem[q][k], 16 * quse[q][k])
                if qprev[q][k] is not None:
                    op["deps"].add(qprev[q][k])
                qprev[q][k] = i
        for e in csem:
            for i in self.by_eng[e]:
                op = self.ops[i]
                if (not op["dma"]) and op["signal"]:
                    cnt[e] += 1
                    op["token"] = (csem[e], cnt[e])
        ops = self.ops
        final = list(self.final)

        def emit(engname, e):
            waited = {}
            for i in self.by_eng[engname]:
                op = ops[i]
                for d in sorted(op["deps"]):
                    sem, val = ops[d]["token"]
                    if waited.get(sem, 0) < val:
                        e.wait_ge(sem, val)
                        waited[sem] = val
                ins = op["fn"](e)
                if op["signal"]:
                    ins.then_inc(op["token"][0], 16 if op["dma"] else 1)
            if engname == "sp":
                for d in final:
                    sem, val = ops[d]["token"]
                    if waited.get(sem, 0) < val:
                        e.wait_ge(sem, val)
                        waited[sem] = val

        with nc.Block() as block:
            @block.tensor
            def _(e):
                emit("pe", e)

            @block.scalar
            def _(e):
                emit("act", e)

            @block.vector
            def _(e):
                emit("dve", e)

            @block.gpsimd
            def _(e):
                emit("pool", e)

            @block.sync
            def _(e):
                emit("sp", e)


def K(name, *idx):
    return (name,) + idx


def build_program(stop_after=None, dumps=(), n_layers=L_):
    nc = bass.Bass("TRN2", target_bir_lowering=False)
    S = Sched(nc)
    es = ExitStack()
    P = {}

    def din(name, shape):
        P[name] = nc.dram_tensor(name, list(shape), F32, kind="ExternalInput").ap()
        return P[name]

    x_d = din("x", [S_, D_])
    mem_d = din("mem", [256, D_])
    w_in_d = din("w_in", [L_, D_, 3076])
    w_out_d = din("w_out", [L_, D_, D_])
    wq_d = din("xattn_wq", [L_, D_, D_])
    wkv_d = din("xattn_wkv", [L_, D_, 2 * D_])
    wo_d = din("xattn_wo", [L_, D_, D_])
    w1_d = din("expert_w1", [L_, 16, D_, 256])
    w3_d = din("expert_w3", [L_, 16, D_, 256])
    w2_d = din("expert_w2", [L_, 16, 256, D_])
    wrt_d = din("router_w", [L_, D_, 20])
    lrubd_d = din("lru_wbd", [L_, 2, 2, 128, 128])
    colp_d = din("colp", [128, L_ * NCOL])
    rowp_d = din("rowp", [128, L_ * NROW])
    c128_d = din("c128", [128, 8 * 128])
    cmask_d = din("cmask", [128, 12 * 512])
    rope_d = din("rope", [128, 2 * S_])
    en_d = din("en", [16, 16 * 128])
    blkoh_d = din("blkoh", [8, S_])
    gate_c_d = din("gatec", [128, 3 * 128])
    out_d = nc.dram_tensor("out", [S_, D_], F32, kind="ExternalOutput").ap()
    dump_d = {}

    uid = [0]

    def sb(name, shape, dt=F32, stack=None):
        uid[0] += 1
        return (stack or es).enter_context(nc.sbuf_tensor(f"{name}_{uid[0]}", list(shape), dt))

    xT = sb("xT", [128, KC, S_])
    hT = sb("hT", [128, KC, S_], BF16)
    colp = sb("colp_s", [128, L_ * NCOL])
    rowp = sb("rowp_s", [128, L_ * NROW])
    c128 = sb("c128_s", [128, 8 * 128])
    c128b = sb("c128b_s", [128, 2 * 128], BF16)
    W = [sb(f"wslot{i}", [128, KC, 512], BF16) for i in range(3)]
    PS = [es.enter_context(nc.psum_tensor(f"ps{i}", [128, 512], F32)) for i in range(8)]
    wn = [0]

    ident = c128[:, 0:128]
    ones_f = c128[:, 128:256]
    tri_le = c128[:, 256:384]
    negu = c128[:, 384:512]
    negones = c128[:, 512:640]
    perm_f = c128[:, 640:768]
    ssdneg = c128[:, 768:896]
    ones_b = c128b[:, 128:256]
    ident_b = c128b[:, 0:128]

    cst = sb("cst", [128, 4])
    S.add("dve", lambda e: e.memset(cst[:, 0:1], EPS), [], [K("cst0")])
    S.add("dve", lambda e: e.memset(cst[:, 1:2], 1.0), [], [K("cst1")])
    S.add("dve", lambda e: e.memset(cst[:, 2:4], 0.0), [K("cst0"), K("cst1")], [K("cst")])
    S.dma(colp[:], colp_d[:, :], [], [K("colp")])
    S.dma(rowp[:], rowp_d[:, :], [], [K("rowp")])
    S.dma(c128[:], c128_d[:, :], [], [K("c128")])
    S.dma(c128b[:], c128_d[:, 0:256], [], [K("c128b")], q="pool")

    def col(l, name, j=0, n=1):
        c0, w = COLS[name]
        return colp[:, l * NCOL + c0 + j: l * NCOL + c0 + j + n]

    def row(l, name, j=0, n=None):
        c0, w = ROWS[name]
        n = w if n is None else n
        return rowp[:, l * NROW + c0 + j: l * NROW + c0 + j + n]

    def ps_rot():
        k = S.psn % 4
        S.psn += 1
        return PS[k], K("ps", k)

    Wf = [w[:].rearrange("p a b -> p (a b)") for w in W]

    def load_w(src2d, nk, ncols, slot=None, c0=0, tot=None):
        tot = ncols if tot is None else tot
        if slot is None:
            k = wn[0] % 3
            wn[0] += 1
        else:
            k = slot
        v = Wf[k][:, 0:nk * tot].rearrange("p (a b) -> p a b", a=nk)
        step = 2 if nk >= 2 else 1
        allk = [K("w", k, j_, hf_) for j_ in range(4) for hf_ in range(2)]
        for h in range(0, nk, step):
            if nk == 8 and tot == 512:
                halves = [hf_ for hf_ in range(2) if c0 < (hf_ + 1) * 256 and c0 + ncols > hf_ * 256]
                wkeys_ = [K("w", k, h // 2, hf_) for hf_ in halves]
            else:
                wkeys_ = allk
            S.dma(v[:, h:h + step, c0:c0 + ncols],
                  src2d[h * 128:(h + step) * 128, :].rearrange("(kc p) n -> p kc n", p=128),
                  [], wkeys_, q="pool")
        return v, allk, k

    def dump(name, ap, reads, shape):
        if name in dumps:
            d = nc.dram_tensor("dbg_" + name, list(shape), ap.dtype, kind="ExternalOutput").ap()
            dump_d[name] = d
            i = S.dma(d, ap, reads, [K("dbg", name)])
            S.final.append(i)

    def load_fm(dst, src_d, ntile, dkey, st):
        xin = [sb(f"xin{j}", [128, D_], stack=st) for j in range(2)]
        for i in range(ntile):
            b = xin[i % 2]
            S.dma(b[:], src_d[i * 128:(i + 1) * 128, :], [], [K("xin", i % 2)])
            for half in range(2):
                ps, pk = ps_rot()
                for j in range(4):
                    kc = half * 4 + j
                    S.tr(ps[:, j * 128:(j + 1) * 128], b[:, kc * 128:(kc + 1) * 128], ident,
                         [K("xin", i % 2), K("c128")], [pk])
                S.cp(dst[:, half * 4:half * 4 + 4, i * 128:(i + 1) * 128],
                     ps[:].rearrange("p (a b) -> p a b", a=4), [pk],
                     [K(dkey, half * 4 + j, i // 4) for j in range(4)], eng=("dve" if half == 0 else "act"))

    st0 = ExitStack()
    load_fm(xT, x_d, NT, "xT", st0)

    def rmsnorm_fm(src, skey, gcol, dst, dkey, nk, T, st, dscale=1.0, hook=None, tag="n"):
        tw = min(T, TW)
        sq = [sb(f"sq_{tag}{j}", [128, nk, tw], BF16, stack=st) for j in range(2)]
        rs = [sb(f"rs_{tag}{j}", [128, tw], stack=st) for j in range(2)]
        for tc in range(T // tw):
            sl = slice(tc * tw, (tc + 1) * tw)
            q = sq[tc % 2]
            for kc in range(nk):
                S.act(q[:, kc, :], src[:, kc, sl], AF.Square, [K(skey, kc, tc)], [K("sq" + tag, tc % 2, kc)])
            ps, pk = ps_rot()
            for kc in range(nk):
                S.mm(ps[:, 0:tw], ones_b, q[:, kc, :], kc == 0, kc == nk - 1,
                     [K("sq" + tag, tc % 2, kc), K("c128b")], [pk])
            r = rs[tc % 2]
            S.act(r[:], ps[:, 0:tw], AF.Sqrt, [pk, K("cst")], [K("rs" + tag, tc % 2)],
                  bias=cst[:, 0:1], scale=1.0 / (nk * 128))
            S.add("dve", (lambda e, r=r: e.reciprocal(out=r[:], in_=r[:])),
                  [K("rs" + tag, tc % 2)], [K("rs" + tag, tc % 2)], cost=0.1 + tw / 240.0)
            if dst is not None:
                for kc in range(nk):
                    S.stt(dst[:, kc, sl], src[:, kc, sl], gcol(kc), r[:], ALU.mult, ALU.mult,
                          [K(skey, kc, tc), K("rs" + tag, tc % 2), K("colp")], [K(dkey, kc, tc)],
                          eng="dve")
            if hook is not None:
                hook(tc, r)

    ALLH = [K("hT", kc, tc) for kc in range(KC) for tc in range(NTC)]

    def hkeys(tc):
        return [K("hT", kc, tc) for kc in range(KC)]

    def proj_fm(wv, wkeys, c0, tc, ps, pk):
        for kc in range(KC):
            S.mm(ps[:, :], wv[:, kc, c0:c0 + 128], hT[:, kc, tc * TW:(tc + 1) * TW], kc == 0, kc == KC - 1,
                 wkeys + [K("hT", kc, tc)], [pk])

    def conv_chunk(ps, pk, tc, xw, xwk, cwcol, cbcol, dst, dkey):
        w_ = xw[tc % 2]
        wk = K(xwk, tc % 2)
        if tc == 0:
            S.add("dve", lambda e: e.memset(w_[:, 0:3], 0.0), [], [wk])
        else:
            S.cp(w_[:, 0:3], xw[(tc - 1) % 2][:, 512:515], [K(xwk, (tc - 1) % 2)], [wk])
        S.cp(w_[:, 3:515], ps[:, :], [pk], [wk], eng="act")
        S.ts(dst, w_[:, 3:515], cwcol(3), cbcol, ALU.mult, ALU.add, [wk, K("colp")], [dkey])
        for k in range(3):
            S.stt(dst, w_[:, k:k + 512], cwcol(k), dst, ALU.mult, ALU.add, [wk, dkey, K("colp")], [dkey])

    def group_out(l, g, YG, st):
        YB = sb("YB", [128, 2, S_], BF16, stack=st)
        rmsnorm_fm(YG, "YG", lambda kc: col(l, "grp_g", 2 * g + kc), YB, "YB", 2, S_, st, tag="g")
        dump(f"yn{l}_{g}", YB[:], [K("YB", c, t) for c in range(2) for t in range(NTC)], [128, 2, S_])
        for half in range(2):
            wv, wk, _ = load_w(w_out_d[l, g * 256:(g + 1) * 256, half * 512:(half + 1) * 512], 2, 512)
            for o4 in range(4):
                oc = half * 4 + o4
                for tc in range(NTC):
                    ps, pk = ps_rot()
                    for kc in range(2):
                        S.mm(ps[:, :], wv[:, kc, o4 * 128:(o4 + 1) * 128], YB[:, kc, tc * TW:(tc + 1) * TW],
                             kc == 0, kc == 1, wk + [K("YB", kc, tc)], [pk])
                    S.tt(xT[:, oc, tc * TW:(tc + 1) * TW], xT[:, oc, tc * TW:(tc + 1) * TW], ps[:, :], ALU.add,
                         [K("xT", oc, tc), pk], [K("xT", oc, tc)])

    ALLX = [K("xT", kc, tc) for kc in range(KC) for tc in range(NTC)]
    done = False
    for l in range(n_layers):
        with ExitStack() as st:
            rmsnorm_fm(xT, "xT", lambda kc: col(l, "mix_g", kc), hT, "hT", KC, S_, (st0 if l == 0 else st), tag="a")
        if l == 0:
            st0.close()
        S.barrier()
        if l == 0:
            dump("h0", hT[:], ALLH, [128, KC, S_])
        if stop_after == "norm0":
            break

        with ExitStack() as sty, ExitStack() as st:
            YG = sb("YG", [128, 2, S_], stack=sty)
            wv, wk, _ = load_w(w_in_d[l, :, 0:512], 8, 512)
            wbd = sb("wbd", [128, 4, 128], BF16, stack=st)
            S.dma(wbd[:], lrubd_d[l].rearrange("g c k m -> k (g c) m"), [], [K("wbd")], q="pool", arena=True)
            c8 = sb("c8", [128, 2], stack=st)
            dump(f"A_wbd{l}", wbd[:], [K("wbd")], [128, 4, 128])
            cy = sb("cy", [128, 2], stack=st)
            cz = sb("cz", [128, 2], stack=st)
            S.act(cy[:], col(l, "lru_lam", 0, 2), AF.Exp, [K("colp")], [K("cy")], scale=-1.0)
            S.ts(cz[:], cy[:], 2.0, None, ALU.add, None, [K("cy")], [K("cz")])
            S.add("dve", lambda e: e.reciprocal(out=cz[:], in_=cz[:]), [K("cz")], [K("cz")])
            S.tt(cz[:], cz[:], cy[:], ALU.mult, [K("cz"), K("cy")], [K("cz")])
            S.tt(cy[:], cz[:], cz[:], ALU.mult, [K("cz")], [K("cy")])
            S.ts(c8[:], cy[:], 1.0 / 9.0, 1.0 / 7.0, ALU.mult, ALU.add, [K("cy")], [K("c8")])
            for cc_ in (1.0 / 5.0, 1.0 / 3.0, 1.0):
                S.tt(c8[:], c8[:], cy[:], ALU.mult, [K("c8"), K("cy")], [K("c8")])
                S.ts(c8[:], c8[:], cc_, None, ALU.add, None, [K("c8")], [K("c8")])
            S.tt(c8[:], c8[:], cz[:], ALU.mult, [K("c8"), K("cz")], [K("c8")])
            S.ts(c8[:], c8[:], -16.0, None, ALU.mult, None, [K("c8")], [K("c8")])
            xw = [sb(f"xw{j}", [128, 515], stack=st) for j in range(2)]
            XC = sb("XC", [128, S_], stack=st)
            XCB = sb("XCB", [128, S_], BF16, stack=st)
            GG = sb("GG", [128, S_], stack=st)
            RA = sb("RA", [128, S_], stack=st)
            IU = sb("IU", [128, S_], stack=st)
            T2 = sb("T2", [128, S_], stack=st)
            XX = sb("XX", [128, S_], stack=st)
            for c in range(2):
                for tc in range(NTC):
                    sl = slice(tc * TW, (tc + 1) * TW)
                    ps, pk = ps_rot()
                    proj_fm(wv, wk, c * 128, tc, ps, pk)
                    conv_chunk(ps, pk, tc, xw, "xw", lambda k: col(l, "lru_cw", c * 4 + k), col(l, "lru_cb", c),
                               XC[:, sl], K("XC", tc))
                    ps, pk = ps_rot()
                    proj_fm(wv, wk, 256 + c * 128, tc, ps, pk)
                    S.cp(GG[:, sl], ps[:, :], [pk], [K("GG", tc)], eng="act")
                XCk = [K("XC", t) for t in range(NTC)]
                GGk = [K("GG", t) for t in range(NTC)]
                S.tt(T2[:], GG[:], GG[:], ALU.mult, GGk, [K("T2")])
                S.ts(T2[:], T2[:], 0.044715, 1.0, ALU.mult, ALU.add, [K("T2")], [K("T2")])
                S.tt(T2[:], T2[:], GG[:], ALU.mult, [K("T2")] + GGk, [K("T2")])
                S.act(T2[:], T2[:], AF.Sigmoid, [K("T2")], [K("T2")], scale=1.5957691216)
                S.tt(GG[:], GG[:], T2[:], ALU.mult, [K("T2")] + GGk, GGk)
                S.cp(XCB[:], XC[:], XCk, [K("XCB")], eng="act")
                for tc in range(NTC):
                    sl = slice(tc * TW, (tc + 1) * TW)
                    ps, pk = ps_rot()
                    S.mm(ps[:, :], wbd[:, c, :], XCB[:, sl], True, True, [K("wbd"), K("XCB")], [pk])
                    S.act(RA[:, sl], ps[:, :], AF.Sigmoid, [pk, K("colp")], [K("RA", tc)], bias=col(l, "lru_br", c))
                    ps, pk = ps_rot()
                    S.mm(ps[:, :], wbd[:, 2 + c, :], XCB[:, sl], True, True, [K("wbd"), K("XCB")], [pk])
                    S.act(IU[:, sl], ps[:, :], AF.Sigmoid, [pk, K("colp")], [K("IU", tc)], bias=col(l, "lru_bi", c))
                RAk = [K("RA", t) for t in range(NTC)]
                IUk = [K("IU", t) for t in range(NTC)]
                if c == 1:
                    dump(f"A_r{l}", RA[:], RAk, [128, S_])
                    dump(f"A_i{l}", IU[:], IUk, [128, S_])
                    dump(f"A_xcb{l}", XCB[:], [K("XCB")], [128, S_])
                    dump(f"A_xc{l}", XC[:], XCk, [128, S_])
                S.ts(XX[:], RA[:], c8[:, c:c + 1], 2.0, ALU.mult, ALU.mult, RAk + [K("c8")], [K("XX")])
                S.ts(T2[:], XX[:], 1.0 / 120.0, None, ALU.mult, None, [K("XX")], [K("T2")])
                for cc_ in (1.0 / 24.0, 1.0 / 6.0, 0.5, 1.0):
                    S.stt(T2[:], T2[:], cc_, XX[:], ALU.add, ALU.mult, [K("T2"), K("XX")], [K("T2")])
                S.act(T2[:], T2[:], AF.Sqrt, [K("T2")], [K("T2")], scale=-1.0)
                S.act(RA[:], XX[:], AF.Exp, [K("XX")], RAk, scale=0.5)
                S.tt(IU[:], IU[:], XC[:], ALU.mult, IUk + XCk, IUk)
                S.tt(IU[:], IU[:], T2[:], ALU.mult, IUk + [K("T2")], IUk)
                S.add("dve", lambda e: e.tensor_tensor_scan(out=XC[:], data0=RA[:], data1=IU[:], initial=0.0,
                                                            op0=ALU.mult, op1=ALU.add), RAk + IUk, XCk, cost=2.6)
                S.tt(YG[:, c, :], XC[:], GG[:], ALU.mult, XCk + GGk, [K("YG", c, t) for t in range(NTC)])
            for nm_, t_ in (("XC", XC), ("GG", GG), ("RA", RA), ("IU", IU), ("T2", T2)):
                dump(f"A_{nm_}{l}", t_[:], [K(nm_, t) for t in range(NTC)] + [K(nm_)], [128, S_])
            dump(f"ya{l}", YG[:], [K("YG", c, t) for c in range(2) for t in range(NTC)], [128, 2, S_])
            st.close()
            S.barrier()
            group_out(l, 0, YG, sty)
        S.barrier()
        if stop_after == f"A{l}":
            done = True
            break

        with ExitStack() as sty, ExitStack() as st:
            YG = sb("YG", [128, 2, S_], stack=sty)
            QB = sb("QB", [128, 2, S_], BF16, stack=st)
            KZ = sb("KZ", [128, 4, S_], BF16, stack=st)
            S.add("dve", lambda e: e.memset(KZ[:], 0.0), [], [K("KZ", h_, t_) for h_ in range(4) for t_ in range(NTC)], cost=4.5)
            VB = sb("VB", [128, NT, 256], BF16, stack=st)
            M01 = sb("M01", [128, 4, 512], stack=st)
            NM = sb("NM", [128, 4, 512], BF16, stack=st)
            S.dma(M01[:], cmask_d[:, 0:2048].rearrange("p (a b) -> p a b", a=4), [], [K("M01")])
            S.dma(NM[:], cmask_d[:, 2048:4096].rearrange("p (a b) -> p a b", a=4), [], [K("NM")], q="pool", arena=True)
            wv, wk, _ = load_w(w_in_d[l, :, 512:1024], 8, 512)
            wv2, wk2, _ = load_w(w_in_d[l, :, 1024:1280], 8, 256)
            for c2 in range(2):
                for tc in range(NTC):
                    sl = slice(tc * TW, (tc + 1) * TW)
                    ps, pk = ps_rot()
                    proj_fm(wv, wk, c2 * 128, tc, ps, pk)
                    S.act(QB[:, c2, sl], ps[:, :], AF.Copy, [pk], [K("QB", c2, tc)], scale=0.125)
                    ps, pk = ps_rot()
                    proj_fm(wv, wk, 256 + c2 * 128, tc, ps, pk)
                    S.cp(KZ[0:64, 2 * c2, sl], ps[0:64, :], [pk], [K("KZ", 2 * c2, tc)])
                    S.cp(KZ[64:128, 2 * c2 + 1, sl], ps[64:128, :], [pk], [K("KZ", 2 * c2 + 1, tc)], eng="act")
            for i in range(NT):
                ps, pk = ps_rot()
                for kc in range(KC):
                    S.mm(ps[:, 0:256], hT[:, kc, i * 128:(i + 1) * 128], wv2[:, kc, :], kc == 0, kc == KC - 1,
                         wk2 + [K("hT", kc, i // 4)], [pk])
                S.cp(VB[:, i, :], ps[:, 0:256], [pk], [K("VB", i)], eng=("dve" if i % 2 else "act"))
            Eb = [sb(f"Eb{j}", [128, 512], stack=st) for j in range(2)]
            SPb = [sb(f"SPb{j}", [128, 512], F32R, stack=st) for j in range(3)]
            Wb = [sb(f"Wb{j}", [128, 512], BF16, stack=st) for j in range(2)]
            Rb = [sb(f"Rb{j}", [128, 512], F32R, stack=st) for j in range(2)]
            NUr = sb("NUr", [128, 2, 128], F32R, stack=st)
            S.cp(NUr[:, 0, :], negu, [K("c128")], [K("NUr")])
            S.cp(NUr[:, 1, :], negones, [K("c128")], [K("NUr")])
            items = []
            grp = 0
            for h in range(4):
                for c in range(NTC):
                    nb = 4 * c + 4
                    for idx, b in enumerate(range(nb - 1, -1, -1)):
                        items.append(dict(h=h, c=c, idx=idx, b=b, grp=grp, last=(b == 0)))
                    grp += 1
            for i_, itm in enumerate(items):
                itm["i"] = i_
                itm["pr"] = slice((itm["h"] % 2) * 64, (itm["h"] % 2) * 64 + 64)
                itm["c2"] = itm["h"] // 2
                itm["d"] = itm["b"] - 4 * itm["c"]
                itm["ksl"] = slice(itm["b"] * 128, (itm["b"] + 1) * 128)
                itm["qsl"] = slice(itm["c"] * TW, (itm["c"] + 1) * TW)
                itm["qk"] = [K("KZ", itm["h"], itm["b"] // 4), K("QB", itm["c2"], itm["c"])]

            def sb_s1(m):
                i = m["i"]
                pr, c2 = m["pr"], m["c2"]
                off = 128 * max(m["d"], 0)
                cs = slice(off, TW)
                qs = slice(m["c"] * TW + off, (m["c"] + 1) * TW)
                if m["idx"] == 0:
                    R0, R0k = Rb[m["grp"] % 2], K("Rb", m["grp"] % 2)
                    S.ts(R0[:], M01[:, 0, :], 0.0, None, ALU.mult, None, [K("M01")], [R0k])
                psZ, pkZ = ps_rot()
                S.mm(psZ[:, cs], KZ[:, m["h"], m["ksl"]], QB[:, c2, qs], True, True, m["qk"], [pkZ])
                E, Ek = Eb[i % 2], K("Eb", i % 2)
                SP, SPk = SPb[i % 3], K("SPb", i % 3)
                S.act(E[:, cs], psZ[:, cs], AF.Exp, [pkZ], [Ek])
                S.act(SP[:, cs], E[:, cs], AF.Ln, [Ek, K("cst")], [SPk], bias=cst[:, 1:2])
                if m["d"] >= 0:
                    S.tt(SP[:, cs], SP[:, cs].bitcast(F32), M01[:, m["d"], cs], ALU.mult, [SPk, K("M01")], [SPk])

            def sb_s2(m):
                i = m["i"]
                pr, c2, d = m["pr"], m["c2"], m["d"]
                off = 128 * max(d, 0)
                cs = slice(off, TW)
                qs = slice(m["c"] * TW + off, (m["c"] + 1) * TW)
                SP, SPk = SPb[i % 3], K("SPb", i % 3)
                Wt, Wk_ = Wb[i % 2], K("Wb", i % 2)
                R, Rk = Rb[m["grp"] % 2], K("Rb", m["grp"] % 2)
                psA, pkA = ps_rot()
                has_r = m["idx"] > 0
                S.mm(psA[:, cs], KZ[:, m["h"], m["ksl"]], QB[:, c2, qs], True, False, m["qk"], [pkA])
                S.mm(psA[:, cs], NUr[:, 0, :], SP[:, cs], False, (not has_r) and d < 0, [SPk, K("NUr")], [pkA])
                if has_r:
                    S.mm(psA[:, cs], NUr[:, 1, :], R[:, cs], False, d < 0, [Rk, K("NUr")], [pkA])
                if d >= 0:
                    S.mm(psA[:, cs], ident_b, NM[:, d, cs], False, True, [K("NM"), K("c128b")], [pkA])
                S.act(Wt[:, cs], psA[:, cs], AF.Exp, [pkA], [Wk_])
                if not m["last"]:
                    S.tt(R[:, cs], R[:, cs].bitcast(F32), SP[:, cs].bitcast(F32), ALU.add, [Rk, SPk], [Rk])

            def sb_s3(m):
                i = m["i"]
                pr, c2, h = m["pr"], m["c2"], m["h"]
                off = 128 * max(m["d"], 0)
                cs = slice(off, TW)
                Wt, Wk_ = Wb[i % 2], K("Wb", i % 2)
                psO, pkO = PS[4 + m["grp"] % 2], K("psO", m["grp"] % 2)
                S.mm(psO[:, cs], VB[:, m["b"], c2 * 128:(c2 + 1) * 128], Wt[:, cs], m["idx"] == 0, m["last"],
                     [K("VB", m["b"]), Wk_], [pkO])
                if m["last"]:
                    S.cp(YG[pr, c2, m["qsl"]], psO[pr, :], [pkO], [K("YG", c2, m["c"])], eng="act")

            n_it = len(items)
            for r in range(-2, n_it):
                if 0 <= r + 2 < n_it:
                    sb_s1(items[r + 2])
                if 0 <= r + 1 < n_it:
                    sb_s2(items[r + 1])
                if 0 <= r < n_it:
                    sb_s3(items[r])
            dump(f"yb{l}", YG[:], [K("YG", c, t) for c in range(2) for t in range(NTC)], [128, 2, S_])
            st.close()
            S.barrier()
            group_out(l, 1, YG, sty)
        S.barrier()
        if stop_after == f"B{l}":
            done = True
            break

        with ExitStack() as sty, ExitStack() as st:
            YG = sb("YG", [128, 2, S_], stack=sty)
            XS = sb("XS", [128, 2, S_], stack=st)
            BTb = sb("BTb", [128, 2, S_], BF16, stack=st)
            CTb = sb("CTb", [128, 2, S_], BF16, stack=st)
            BTM = sb("BTM", [128, NT, 256], BF16, stack=st)
            XTM = sb("XTM", [128, NT, 256], BF16, stack=st)
            wdt = sb("wdt", [128, KC, 4], BF16, stack=st)
            stc = ExitStack()
            xw = [sb(f"xwc{j}", [128, 515], stack=stc) for j in range(2)]
            CV = [sb(f"CV{j}", [128, 512], stack=stc) for j in range(2)]
            BF = [sb(f"BF{j}", [128, 512], stack=stc) for j in range(2)]
            S.dma(wdt[:], w_in_d[l, :, 2304:2308].rearrange("(kc p) n -> p kc n", p=128), [], [K("wdt")], q="pool", arena=True)
            wv1, wk1, _ = load_w(w_in_d[l, :, 1280:1792], 8, 512)
            wv2, wk2, _ = load_w(w_in_d[l, :, 1792:2304], 8, 512)
            n_cv = 0
            for j in range(6):
                if j < 2:
                    wv, wk, c0 = wv1, wk1, 256 + j * 128
                elif j < 4:
                    wv, wk, c0 = wv2, wk2, (j - 2) * 128
                else:
                    wv, wk, c0 = wv2, wk2, 256 + (j - 4) * 128
                for tc in range(NTC):
                    sl = slice(tc * TW, (tc + 1) * TW)
                    ps, pk = ps_rot()
                    proj_fm(wv, wk, c0, tc, ps, pk)
                    cv, cvk = CV[n_cv % 2], K("CV", n_cv % 2)
                    conv_chunk(ps, pk, tc, xw, "xwc", lambda k: col(l, "ssm_cw", j * 4 + k), col(l, "ssm_cb", j),
                               cv[:], cvk)
                    if j < 2:
                        S.act(XS[:, j, sl], cv[:], AF.Silu, [cvk], [K("XS", j, tc)])
                        ps, pk = ps_rot()
                        for q in range(4):
                            S.tr(ps[:, q * 128:(q + 1) * 128], XS[:, j, tc * TW + q * 128: tc * TW + (q + 1) * 128],
                                 ident, [K("XS", j, tc), K("c128")], [pk])
                        S.cp(XTM[:, tc * 4:tc * 4 + 4, j * 128:(j + 1) * 128],
                             ps[:].rearrange("p (a b) -> p a b", a=4), [pk], [K("XTM", tc, j)])
                    elif j < 4:
                        g = j - 2
                        bf, bfk = BF[n_cv % 2], K("BF", n_cv % 2)
                        S.act(bf[:], cv[:], AF.Silu, [cvk], [bfk])
                        S.cp(BTb[:, g, sl], bf[:], [bfk], [K("BTb", g, tc)])
                        ps, pk = ps_rot()
                        for q in range(4):
                            S.tr(ps[:, q * 128:(q + 1) * 128], bf[:, q * 128:(q + 1) * 128], ident,
                                 [bfk, K("c128")], [pk])
                        S.cp(BTM[:, tc * 4:tc * 4 + 4, g * 128:(g + 1) * 128],
                             ps[:].rearrange("p (a b) -> p a b", a=4), [pk], [K("BTM", tc, g)])
                    else:
                        S.act(CTb[:, j - 4, sl], cv[:], AF.Silu, [cvk], [K("CTb", j - 4, tc)])
                    n_cv += 1
            stc.close()
            S.barrier()
            DT = sb("DT", [128, 64], stack=st)
            AN = sb("AN", [128, 64], stack=st)
            AC = sb("AC", [128, 64], stack=st)
            ACS = sb("ACS", [128, 64], stack=st)
            TOT = sb("TOT", [128, 64], stack=st)
            DEC = sb("DEC", [128, 64], stack=st)
            ETOT = sb("ETOT", [128, 64], stack=st)
            DTD = sb("DTD", [128, 64], stack=st)
            psd, pkd = ps_rot()
            for i in range(NT):
                for kc in range(KC):
                    S.mm(psd[:, i * 4:(i + 1) * 4], hT[:, kc, i * 128:(i + 1) * 128], wdt[:, kc, :], kc == 0,
                         kc == KC - 1, [K("wdt"), K("hT", kc, i // 4)], [pkd])
            S.tt(DT[:], psd[:, 0:64], row(l, "dt_bias"), ALU.add, [pkd, K("rowp")], [K("DT")])
            S.act(DT[:], DT[:], AF.Exp, [K("DT")], [K("DT")])
            S.act(DT[:], DT[:], AF.Ln, [K("DT"), K("cst")], [K("DT")], bias=cst[:, 1:2])
            S.act(AN[:], row(l, "a_log"), AF.Exp, [K("rowp")], [K("AN")])
            S.ts(AN[:], AN[:], -1.0, None, ALU.mult, None, [K("AN")], [K("AN")])
            S.tt(AC[:], DT[:], AN[:], ALU.mult, [K("DT"), K("AN")], [K("AC")])
            ps, pk = ps_rot()
            S.mm(ps[:, 0:64], tri_le, AC[:], True, True, [K("AC"), K("c128")], [pk])
            S.cp(ACS[:], ps[:, 0:64], [pk], [K("ACS")])
            ps, pk = ps_rot()
            S.mm(ps[:, 0:64], ones_f, AC[:], True, True, [K("AC"), K("c128")], [pk])
            S.cp(TOT[:], ps[:, 0:64], [pk], [K("TOT")])
            S.tt(DEC[:], TOT[:], ACS[:], ALU.subtract, [K("TOT"), K("ACS")], [K("DEC")])
            S.act(DEC[:], DEC[:], AF.Exp, [K("DEC")], [K("DEC")])
            S.act(ETOT[:], TOT[:], AF.Exp, [K("TOT")], [K("ETOT")])
            S.tt(DTD[:], DT[:], DEC[:], ALU.mult, [K("DT"), K("DEC")], [K("DTD")])
            ST = sb("ST", [128, 4, 64], stack=st)
            S.add("dve", lambda e: e.memset(ST[:], 0.0), [], [K("ST", h) for h in range(4)])
            Rm = [sb(f"Rm{j}", [128, 128], stack=st) for j in range(2)]
            ARG = [sb(f"ARG{j}", [128, 128], stack=st) for j in range(2)]
            E1 = [sb(f"E1{j}", [128, 128], stack=st) for j in range(2)]
            GL = [sb(f"GL{j}", [128, 128], BF16, stack=st) for j in range(2)]
            CTp = [sb(f"CTp{j}", [128, 128], BF16, stack=st) for j in range(2)]
            XCp = [[sb(f"XCp{a}{j}", [128, 128], BF16, stack=st) for j in range(2)] for a in range(2)]
            STp = sb("STp", [128, 4, 128], BF16, stack=st)
            for a in range(2):
                for j in range(2):
                    S.add("dve", (lambda e, t_=XCp[a][j]: e.memset(t_[:], 0.0)), [], [K("XCp", a, j)])
            S.add("dve", lambda e: e.memset(STp[:], 0.0), [], [K("STp", h) for h in range(4)])
            XDh = [sb(f"XDh{j}", [128, 64], BF16, stack=st) for j in range(2)]
            n = 0
            for ci in range(NT):
                csl = slice(ci * 128, (ci + 1) * 128)
                tcx = ci // 4
                for g in range(2):
                    psG, pkG = ps_rot()
                    S.mm(psG[:, 0:128], BTb[:, g, csl], CTb[:, g, csl], True, True,
                         [K("BTb", g, tcx), K("CTb", g, tcx)], [pkG])
                    psY, pkY = PS[4 + (ci * 2 + g) % 2], K("psO", (ci * 2 + g) % 2)
                    for hh in range(2):
                        h = 2 * g + hh
                        pr = slice(hh * 64, hh * 64 + 64)
                        cidx = ci * 4 + h
                        k2 = n % 2
                        S.ts(Rm[k2][:], tri_le, AC[:, cidx:cidx + 1], None, ALU.mult, None,
                             [K("AC"), K("c128")], [K("Rm", k2)])
                        ps1, pk1 = ps_rot()
                        S.mm(ps1[:, 0:128], ones_f, Rm[k2][:], True, True, [K("Rm", k2), K("c128")], [pk1])
                        S.stt(ARG[k2][:], ps1[:, 0:128], ACS[:, cidx:cidx + 1], ssdneg, ALU.subtract, ALU.add,
                              [pk1, K("ACS"), K("c128")], [K("ARG", k2)])
                        S.act(ARG[k2][:], ARG[k2][:], AF.Exp, [K("ARG", k2)], [K("ARG", k2)])
                        S.tt(GL[k2][:], ARG[k2][:], psG[:, 0:128], ALU.mult, [K("ARG", k2), pkG], [K("GL", k2)])
                        S.act(E1[k2][:], ps1[:, 0:128], AF.Exp, [pk1], [K("E1", k2)])
                        S.tt(CTp[k2][:], CTb[:, g, csl], E1[k2][:], ALU.mult, [K("CTb", g, tcx), K("E1", k2)],
                             [K("CTp", k2)])
                        rot = (ci * 2 + g) % 2
                        xcp = XCp[hh][rot]
                        S.ts(xcp[:, hh * 64:(hh + 1) * 64], XTM[:, ci, h * 64:(h + 1) * 64], DT[:, cidx:cidx + 1], None,
                             ALU.mult, None, [K("XTM", tcx, g), K("DT")], [K("XCp", hh, rot)])
                        S.mm(psY[:, 0:128], xcp[:], GL[k2][:], hh == 0, False, [K("XCp", hh, rot), K("GL", k2)], [pkY])
                        S.mm(psY[:, 0:128], STp[:, h, :], CTp[k2][:], False, hh == 1, [K("STp", h), K("CTp", k2)], [pkY])
                        S.ts(XDh[k2][:], XTM[:, ci, h * 64:(h + 1) * 64], DTD[:, cidx:cidx + 1], None, ALU.mult, None,
                             [K("XTM", tcx, g), K("DTD")], [K("XDh", k2)])
                        psS, pkS = ps_rot()
                        S.mm(psS[:, 0:64], BTM[:, ci, g * 128:(g + 1) * 128], XDh[k2][:], True, True,
                             [K("BTM", tcx, g), K("XDh", k2)], [pkS])
                        S.stt(ST[:, h, :], ST[:, h, :], ETOT[:, cidx:cidx + 1], psS[:, 0:64], ALU.mult, ALU.add,
                              [K("ST", h), K("ETOT"), pkS], [K("ST", h)])
                        S.cp(STp[:, h, hh * 64:(hh + 1) * 64], ST[:, h, :], [K("ST", h)], [K("STp", h)], eng="act")
                        n += 1
                    S.cp(YG[:, g, csl], psY[:, 0:128], [pkY], [K("YG", g, tcx)], eng="act")
            ARG = [sb(f"ZT{j}", [128, 512], stack=st) for j in range(1)]
            for j in range(2):
                YGk = [K("YG", j, t) for t in range(NTC)]
                S.stt(YG[:, j, :], XS[:, j, :], col(l, "ssm_d", j), YG[:, j, :], ALU.mult, ALU.add,
                      [K("XS", j, t) for t in range(NTC)] + YGk + [K("colp")], YGk)
                for tc in range(NTC):
                    sl = slice(tc * TW, (tc + 1) * TW)
                    ps, pk = ps_rot()
                    proj_fm(wv1, wk1, j * 128, tc, ps, pk)
                    zt, ztk = ARG[0], K("ARGz", 0)
                    S.act(zt[:], ps[:, :], AF.Silu, [pk], [ztk])
                    S.tt(YG[:, j, sl], YG[:, j, sl], zt[:], ALU.mult, [K("YG", j, tc), ztk], [K("YG", j, tc)])
            dump(f"yc{l}", YG[:], [K("YG", c, t) for c in range(2) for t in range(NTC)], [128, 2, S_])
            st.close()
            S.barrier()
            group_out(l, 2, YG, sty)
        S.barrier()
        if stop_after == f"C{l}":
            done = True
            break

        with ExitStack() as sty, ExitStack() as st:
            YG = sb("YG", [128, 2, S_], stack=sty)
            QA = sb("QA", [128, 4, S_], BF16, stack=st)
            KA = sb("KA", [128, 4, S_], BF16, stack=st)
            allqa = [K("QA", h_, t_) for h_ in range(4) for t_ in range(NTC)]
            allka = [K("KA", h_, t_) for h_ in range(4) for t_ in range(NTC)]
            S.add("dve", lambda e: e.memset(QA[:], 0.0), [], allqa, cost=4.5)
            S.add("dve", lambda e: e.memset(KA[:], 0.0), [], allka, cost=4.5)
            for h_ in range(4):
                o_ = 64 if h_ % 2 == 0 else 0
                S.dma(KA[o_:o_ + 8, h_, :], blkoh_d[:, :], allka, [K("KAoh", h_)], q="pool", arena=True)
            VB = sb("VB", [128, NT, 256], BF16, stack=st)
            CAUS = sb("CAUS", [128, 4, 512], BF16, stack=st)
            S.dma(CAUS[:], cmask_d[:, 4096:6144].rearrange("p (a b) -> p a b", a=4), [], [K("CAUS")], q="pool", arena=True)
            GC = sb("GC", [128, 3, 128], stack=st)
            S.dma(GC[:], gate_c_d[:, :].rearrange("p (a b) -> p a b", a=3), [], [K("GC")])
            KM = sb("KM", [128, 2, 8], stack=st)
            KMb = sb("KMb", [128, 2, 8], BF16, stack=st)
            wv, wk, _ = load_w(w_in_d[l, :, 2308:2820], 8, 512)
            wv2, wk2, _ = load_w(w_in_d[l, :, 2820:3076], 8, 256)
            str_ = ExitStack()
            ROPEb = [sb(f"ROPE{j}", [128, 2, TW], stack=str_) for j in range(1)]
            RAW = [sb(f"RAW{j}", [128, 512], stack=str_) for j in range(2)]
            TMP = [sb(f"TMPr{j}", [128, 512], stack=str_) for j in range(2)]
            KF = [sb(f"KF{j}", [128, 512], stack=str_) for j in range(2)]
            nr = 0
            for c2 in range(2):
                for tc in range(NTC):
                    sl = slice(tc * TW, (tc + 1) * TW)
                    rj = 0
                    ROPE = ROPEb[rj]
                    rsl = slice(0, TW)
                    S.dma(ROPE[:, 0, :], rope_d[:, tc * TW:(tc + 1) * TW], [], [K("ROPE", rj, 0)])
                    S.dma(ROPE[:, 1, :], rope_d[:, S_ + tc * TW:S_ + (tc + 1) * TW], [], [K("ROPE", rj, 1)])
                    for which in range(2):
                        ps, pk = ps_rot()
                        proj_fm(wv, wk, which * 256 + c2 * 128, tc, ps, pk)
                        raw, rawk = RAW[nr % 2], K("RAW", nr % 2)
                        tmp, tmpk = TMP[nr % 2], K("TMPr", nr % 2)
                        S.cp(raw[:], ps[:, :], [pk], [rawk], eng="act")
                        psw, pkw = ps_rot()
                        S.mm(psw[:, :], perm_f, raw[:], True, True, [rawk, K("c128")], [pkw])
                        S.tt(tmp[:], psw[:, :], ROPE[:, 1, rsl], ALU.mult, [pkw, K("ROPE", rj, 1)], [tmpk])
                        kf = KF[nr % 2]
                        dst, dk = kf[:], K("KF", nr % 2)
                        S.tt(dst, raw[:], ROPE[:, 0, rsl], ALU.mult, [rawk, K("ROPE", rj, 0)], [dk])
                        S.tt(dst, dst, tmp[:], ALU.add, [dk, tmpk], [dk])
                        if which == 0:
                            S.act(QA[0:64, 2 * c2, sl], kf[0:64, :], AF.Copy, [dk], [K("QA", 2 * c2, tc)], scale=0.125)
                            S.act(QA[64:128, 2 * c2 + 1, sl], kf[64:128, :], AF.Copy, [dk], [K("QA", 2 * c2 + 1, tc)],
                                  scale=0.125)
                        else:
                            S.cp(KA[0:64, 2 * c2, sl], kf[0:64, :], [dk], [K("KA", 2 * c2, tc)], eng="act")
                            S.cp(KA[64:128, 2 * c2 + 1, sl], kf[64:128, :], [dk], [K("KA", 2 * c2 + 1, tc)], eng="act")
                            for hb in range(2):
                                nblk = tc * 2 + hb
                                S.add("dve", (lambda e, o=KM[:, c2, nblk:nblk + 1], i_=kf[:, hb * 256:(hb + 1) * 256]:
                                              e.reduce_sum(out=o, in_=i_, axis=AX.X)), [dk], [K("KM", c2, nblk)])
                        nr += 1
            for i in range(NT):
                ps, pk = ps_rot()
                for kc in range(KC):
                    S.mm(ps[:, 0:256], hT[:, kc, i * 128:(i + 1) * 128], wv2[:, kc, :], kc == 0, kc == KC - 1,
                         wk2 + [K("hT", kc, i // 4)], [pk])
                S.cp(VB[:, i, :], ps[:, 0:256], [pk], [K("VB", i)], eng=("dve" if i % 2 else "act"))
            S.cp(KMb[:], KM[:], [K("KM", c2_, nb_) for c2_ in range(2) for nb_ in range(8)], [K("KMb")])
            str_.close()
            S.barrier()
            PTb = [sb(f"PTb{j}", [128, 512], BF16, stack=st) for j in range(3)]
            RD = [sb(f"RD{j}", [128, 512], stack=st) for j in range(2)]
            GT = sb("GT", [128, 128], stack=st)
            BT_ = sb("BIASTM", [128, 128], stack=st)
            MX = [sb(f"MX{j}", [128, 8], stack=st) for j in range(2)]
            for h in range(4):
                hh, c2 = h % 2, h // 2
                pr = slice(hh * 64, hh * 64 + 64)
                o_ = 64 if hh == 0 else 0
                psg, pkg = ps_rot()
                for i in range(NT):
                    S.mm(psg[:, i * 8:(i + 1) * 8], QA[pr, h, i * 128:(i + 1) * 128], KMb[pr, c2, :], True, True,
                         [K("QA", h, i // 4), K("KMb")], [pkg])
                S.stt(GT[:], psg[:, 0:128], 8.0 / 256.0, GC[:, 0, :], ALU.mult, ALU.add, [pkg, K("GC")], [K("GT")])
                for i in range(NT):
                    mx, mxk = MX[i % 2], K("MX", i % 2)
                    S.add("dve", (lambda e, o=mx[:], i_=GT[:, i * 8:(i + 1) * 8]: e.max(out=o, in_=i_)),
                          [K("GT")], [mxk])
                    bsl = BT_[:, i * 8:(i + 1) * 8]
                    S.ts(bsl, GT[:, i * 8:(i + 1) * 8], mx[:, 2:3], None, ALU.is_ge, None, [K("GT"), mxk],
                         [K("BIASTM", i)])
                    S.tt(bsl, bsl, GC[:, 1, i * 8:(i + 1) * 8], ALU.mult, [K("BIASTM", i), K("GC")], [K("BIASTM", i)])
                    S.tt(bsl, bsl, GC[:, 2, i * 8:(i + 1) * 8], ALU.add, [K("BIASTM", i), K("GC")], [K("BIASTM", i)])
                    S.ts(bsl, bsl, -1.0, -NEG, ALU.add, ALU.mult, [K("BIASTM", i)], [K("BIASTM", i)])
                for q4 in range(4):
                    pst, pkt = ps_rot()
                    for q in range(4):
                        i = q4 * 4 + q
                        S.tr(pst[0:8, q * 128:(q + 1) * 128], BT_[:, i * 8:(i + 1) * 8], ident,
                             [K("BIASTM", i), K("c128")], [pkt])
                    S.cp(QA[o_:o_ + 8, h, q4 * 512:(q4 + 1) * 512], pst[0:8, :], [pkt], [K("QAsel", h, q4)])
            its = []
            grp = 0
            for h in range(4):
                for c in range(NTC):
                    nkt = 4 * c + 4
                    for kt in range(nkt):
                        its.append(dict(h=h, c=c, kt=kt, nkt=nkt, grp=grp, i=len(its)))
                    grp += 1

            def m_s1(m):
                h, c, kt = m["h"], m["c"], m["kt"]
                d = kt - 4 * c
                qsl = slice(c * TW, (c + 1) * TW)
                ksl = slice(kt * 128, (kt + 1) * 128)
                off = 128 * max(d, 0)
                cs = slice(off, TW)
                qs = slice(c * TW + off, (c + 1) * TW)
                psS, pkS = ps_rot()
                S.mm(psS[:, cs], KA[:, h, ksl], QA[:, h, qs], True, d < 0,
                     [K("KA", h, kt // 4), K("KAoh", h), K("QA", h, c), K("QAsel", h, c)], [pkS])
                if d >= 0:
                    S.mm(psS[:, cs], ident_b, CAUS[:, d, cs], False, True, [K("CAUS"), K("c128b")], [pkS])
                pt, ptk = PTb[m["i"] % 3], K("PTb", m["i"] % 3)
                S.act(pt[:, cs], psS[:, cs], AF.Exp, [pkS], [ptk])

            def m_s2(m):
                h, c, kt, nkt = m["h"], m["c"], m["kt"], m["nkt"]
                hh, c2 = h % 2, h // 2
                pr = slice(hh * 64, hh * 64 + 64)
                qsl = slice(c * TW, (c + 1) * TW)
                g2 = m["grp"] % 2
                psO, pkO = PS[4 + g2], K("psO", g2)
                psD, pkD = PS[6 + g2], K("psD", g2)
                pt, ptk = PTb[m["i"] % 3], K("PTb", m["i"] % 3)
                off = 128 * max(kt - 4 * c, 0)
                cs = slice(off, TW)
                S.mm(psO[:, cs], VB[:, kt, c2 * 128:(c2 + 1) * 128], pt[:, cs], kt == 0, kt == nkt - 1,
                     [K("VB", kt), ptk], [pkO])
                S.mm(psD[:, cs], ones_b, pt[:, cs], kt == 0, kt == nkt - 1, [ptk, K("c128b")], [pkD])
                if kt == nkt - 1:
                    rd, rdk = RD[g2], K("RD", g2)
                    S.add("dve", (lambda e, o=rd[pr, :], i_=psD[pr, :]: e.reciprocal(out=o, in_=i_)), [pkD], [rdk], cost=2.2)
                    S.tt(YG[pr, c2, qsl], psO[pr, :], rd[pr, :], ALU.mult, [pkO, rdk], [K("YG", c2, c)])

            n_ = len(its)
            for r in range(-1, n_):
                if r + 1 < n_:
                    m_s1(its[r + 1])
                if r >= 0:
                    m_s2(its[r])
            dump(f"yd{l}", YG[:], [K("YG", c, t) for c in range(2) for t in range(NTC)], [128, 2, S_])
            st.close()
            S.barrier()
            group_out(l, 3, YG, sty)
        S.barrier()
        dump(f"x1_{l}", xT[:], ALLX, [128, KC, S_])
        if stop_after == f"D{l}":
            done = True
            break

        with ExitStack() as st:
            with ExitStack() as st2:
                rmsnorm_fm(xT, "xT", lambda kc: col(l, "xat_g", kc), hT, "hT", KC, S_, st2, tag="x")
            S.barrier()
            memT = sb("memT", [128, KC, 256], stack=st)
            MTb = sb("MTb", [128, KC, 256], BF16, stack=st)
            with ExitStack() as st2:
                load_fm(memT, mem_d, 2, "memT", st2)
                rmsnorm_fm(memT, "memT", lambda kc: col(l, "mem_g", kc), MTb, "MTb", KC, 256, st2, tag="m")
            S.barrier()
            KTb = sb("KTb", [128, KC, 256], BF16, stack=st)
            VX = sb("VX", [128, 2, D_], BF16, stack=st)
            QX = sb("QX", [128, KC, S_], BF16, stack=st)
            MTk = [K("MTb", kc, 0) for kc in range(KC)]
            for half in range(2):
                wv, wk, _ = load_w(wkv_d[l, :, half * 512:(half + 1) * 512], 8, 512)
                for o4 in range(4):
                    oc = half * 4 + o4
                    ps, pk = ps_rot()
                    for kc in range(KC):
                        S.mm(ps[:, 0:256], wv[:, kc, o4 * 128:(o4 + 1) * 128], MTb[:, kc, :], kc == 0, kc == KC - 1,
                             wk + [K("MTb", kc, 0)], [pk])
                    S.cp(KTb[:, oc, :], ps[:, 0:256], [pk], [K("KTb", oc)])
            for half in range(2):
                wv, wk, _ = load_w(wkv_d[l, :, 1024 + half * 512:1024 + (half + 1) * 512], 8, 512)
                for mt in range(2):
                    ps, pk = ps_rot()
                    for kc in range(KC):
                        S.mm(ps[:, :], MTb[:, kc, mt * 128:(mt + 1) * 128], wv[:, kc, :], kc == 0, kc == KC - 1,
                             wk + [K("MTb", kc, 0)], [pk])
                    S.cp(VX[:, mt, half * 512:(half + 1) * 512], ps[:, :], [pk], [K("VX", mt, half)], eng="act")
            for half in range(2):
                wv, wk, _ = load_w(wq_d[l, :, half * 512:(half + 1) * 512], 8, 512)
                for o4 in range(4):
                    oc = half * 4 + o4
                    for tc in range(NTC):
                        ps, pk = ps_rot()
                        proj_fm(wv, wk, o4 * 128, tc, ps, pk)
                        S.act(QX[:, oc, tc * TW:(tc + 1) * TW], ps[:, :], AF.Copy, [pk], [K("QX", oc, tc)],
                              scale=1.0 / 16.0)
            dump(f"X_MTb{l}", MTb[:], MTk, [128, KC, 256])
            dump(f"X_KTb{l}", KTb[:], [K("KTb", oc) for oc in range(KC)], [128, KC, 256])
            dump(f"X_QX{l}", QX[:], [K("QX", oc, tc) for oc in range(KC) for tc in range(NTC)], [128, KC, S_])
            dump(f"X_VX{l}", VX[:], [K("VX", mt, hf) for mt in range(2) for hf in range(2)], [128, 2, D_])
            PT = [sb(f"PTx{j}", [128, 512], BF16, stack=st) for j in range(4)]
            RDx = [sb(f"RDx{j}", [128, 512], stack=st) for j in range(2)]
            it = 0
            for hd in range(4):
                for tc in range(NTC):
                    sl = slice(tc * TW, (tc + 1) * TW)
                    pts = []
                    for mt in range(2):
                        ps, pk = ps_rot()
                        for j in range(2):
                            S.mm(ps[:, :], KTb[:, 2 * hd + j, mt * 128:(mt + 1) * 128], QX[:, 2 * hd + j, sl],
                                 j == 0, j == 1, [K("KTb", 2 * hd + j), K("QX", 2 * hd + j, tc)], [pk])
                        pt, ptk = PT[it % 4], K("PTx", it % 4)
                        S.act(pt[:], ps[:, :], AF.Exp, [pk], [ptk])
                        pts.append((pt, ptk))
                        it += 1
                    psD, pkD = ps_rot()
                    for mt in range(2):
                        S.mm(psD[:, :], ones_b, pts[mt][0][:], mt == 0, mt == 1, [pts[mt][1], K("c128b")], [pkD])
                    rd, rdk = RDx[(hd * NTC + tc) % 2], K("RDx", (hd * NTC + tc) % 2)
                    S.add("dve", (lambda e, o=rd[:], i_=psD[:, :]: e.reciprocal(out=o, in_=i_)), [pkD], [rdk], cost=2.2)
                    for j in range(2):
                        oc = 2 * hd + j
                        ps, pk = ps_rot()
                        for mt in range(2):
                            S.mm(ps[:, :], VX[:, mt, oc * 128:(oc + 1) * 128], pts[mt][0][:], mt == 0, mt == 1,
                                 [K("VX", mt, oc // 4), pts[mt][1]], [pk])
                        S.tt(hT[:, oc, sl], ps[:, :], rd[:], ALU.mult, [pk, rdk], [K("hT", oc, tc)])
            dump(f"X_OX{l}", hT[:], ALLH, [128, KC, S_])
            for half in range(2):
                wv, wk, _ = load_w(wo_d[l, :, half * 512:(half + 1) * 512], 8, 512)
                for o4 in range(4):
                    oc = half * 4 + o4
                    for tc in range(NTC):
                        ps, pk = ps_rot()
                        proj_fm(wv, wk, o4 * 128, tc, ps, pk)
                        S.tt(xT[:, oc, tc * TW:(tc + 1) * TW], xT[:, oc, tc * TW:(tc + 1) * TW], ps[:, :], ALU.add,
                             [K("xT", oc, tc), pk], [K("xT", oc, tc)])
        S.barrier()
        dump(f"x2_{l}", xT[:], ALLX, [128, KC, S_])
        if stop_after == f"X{l}":
            done = True
            break

        with ExitStack() as st:
            ENp = sb("ENp", [128, 16, 128], BF16, stack=st)
            S.add("dve", lambda e: e.memset(ENp[:], 0.0), [], [K("ENpz")])
            S.dma(ENp[0:16, :, :], en_d[:, :].rearrange("p (a b) -> p a b", a=16), [K("ENpz")], [K("ENf")], q="pool",
                  arena=True)
            CTh = sb("CTh", [128, S_], BF16, stack=st)
            CTl = sb("CTl", [128, S_], BF16, stack=st)
            S.add("dve", lambda e: e.memset(CTh[:], 0.0), [], [K("CThz")])
            S.add("dve", lambda e: e.memset(CTl[:], 0.0), [], [K("CTlz")])
            st_rt = ExitStack()
            WR = sb("WR", [128, KC, 20], stack=st_rt)
            S.dma(WR[:], wrt_d[l].rearrange("(kc p) n -> p kc n", p=128), [], [K("WR")])
            LG = sb("LG", [128, NT, 20], stack=st_rt)
            st_hf = ExitStack()
            HF = sb("HF", [128, KC, TW], stack=st_hf)

            def router_hook(tc, r):
                sl = slice(tc * TW, (tc + 1) * TW)
                for kc in range(KC):
                    S.stt(HF[:, kc, :], xT[:, kc, sl], col(l, "ffn_g", kc), r[:], ALU.mult, ALU.mult,
                          [K("xT", kc, tc), K("rsf", tc % 2), K("colp")], [K("HF", kc)])
                ps, pk = ps_rot()
                for q in range(4):
                    for kc in range(KC):
                        S.mm(ps[:, q * 20:(q + 1) * 20], HF[:, kc, q * 128:(q + 1) * 128], WR[:, kc, :], kc == 0,
                             kc == KC - 1, [K("HF", kc), K("WR")], [pk])
                S.cp(LG[:, tc * 4:tc * 4 + 4, :], ps[:, 0:80].rearrange("p (a b) -> p a b", a=4), [pk],
                     [K("LG", tc)])

            with ExitStack() as st2:
                rmsnorm_fm(xT, "xT", lambda kc: col(l, "ffn_g", kc), hT, "hT", KC, S_, st2, hook=router_hook, tag="f")
            st_hf.close()
            S.barrier()
            COMB = sb("COMB", [128, NT, 16], stack=st_rt)
            CT = sb("CT", [16, S_], stack=st_rt)
            sm = {nm: sb("rt_" + nm, [128, NT, w_] if w_ > 1 else [128, NT], stack=st_rt) for nm, w_ in
                  [("lg", 4), ("m", 1), ("eg", 4), ("sg", 1), ("oh", 4), ("le", 16), ("sel", 4), ("tmp", 4),
                   ("a", 1), ("eq", 4), ("sel2", 4), ("b", 1), ("t2", 4), ("ex", 4), ("dd", 1), ("wg", 4)]}

            def v(nm):
                return sm[nm][:]

            def kk(nm):
                return [K("rt", nm)]

            def B3(ap2, w_=4):
                return ap2.unsqueeze(2).to_broadcast([128, NT, w_])

            LGk = [K("LG", t) for t in range(NTC)]
            bgB = row(l, "bg").unsqueeze(1).to_broadcast([128, NT, 4])
            beB = row(l, "be").unsqueeze(1).to_broadcast([128, NT, 16])
            S.tt(v("lg"), LG[:, :, 0:4], bgB, ALU.add, LGk + [K("rowp")], kk("lg"))
            S.add("dve", lambda e: e.reduce_max(out=v("m"), in_=v("lg"), axis=AX.X), kk("lg"), kk("m"))
            S.tt(v("lg"), v("lg"), B3(v("m")), ALU.subtract, kk("lg") + kk("m"), kk("lg"))
            S.act(v("eg"), v("lg"), AF.Exp, kk("lg"), kk("eg"))
            S.add("dve", lambda e: e.reduce_sum(out=v("sg"), in_=v("eg"), axis=AX.X), kk("eg"), kk("sg"))
            S.add("dve", lambda e: e.reciprocal(out=v("sg"), in_=v("sg")), kk("sg"), kk("sg"))
            S.ts(v("oh"), v("lg"), 0.0, None, ALU.is_ge, None, kk("lg"), kk("oh"))
            S.tt(v("le"), LG[:, :, 4:20], beB, ALU.add, LGk + [K("rowp")], kk("le"))
            for g in range(4):
                ohg = sm["oh"][:, :, g:g + 1].to_broadcast([128, NT, 4])
                if g == 0:
                    S.tt(v("sel"), sm["le"][:, :, 0:4], ohg, ALU.mult, kk("le") + kk("oh"), kk("sel"))
                else:
                    S.tt(v("tmp"), sm["le"][:, :, 4 * g:4 * g + 4], ohg, ALU.mult, kk("le") + kk("oh"), kk("tmp"))
                    S.tt(v("sel"), v("sel"), v("tmp"), ALU.add, kk("sel") + kk("tmp"), kk("sel"))
            S.add("dve", lambda e: e.reduce_max(out=v("a"), in_=v("sel"), axis=AX.X), kk("sel"), kk("a"))
            S.tt(v("sel"), v("sel"), B3(v("a")), ALU.subtract, kk("sel") + kk("a"), kk("sel"))
            S.ts(v("eq"), v("sel"), 0.0, None, ALU.is_ge, None, kk("sel"), kk("eq"))
            S.stt(v("sel2"), v("eq"), -1e30, v("sel"), ALU.mult, ALU.add, kk("eq") + kk("sel"), kk("sel2"))
            S.add("dve", lambda e: e.reduce_max(out=v("b"), in_=v("sel2"), axis=AX.X), kk("sel2"), kk("b"))
            S.tt(v("t2"), v("sel"), B3(v("b")), ALU.is_ge, kk("sel") + kk("b"), kk("t2"))
            S.act(v("ex"), v("sel"), AF.Exp, kk("sel"), kk("ex"))
            S.act(v("dd"), v("b"), AF.Exp, kk("b"), kk("dd"))
            S.ts(v("dd"), v("dd"), 1.0, None, ALU.add, None, kk("dd"), kk("dd"))
            S.add("dve", lambda e: e.reciprocal(out=v("dd"), in_=v("dd")), kk("dd"), kk("dd"))
            S.tt(v("wg"), v("ex"), v("t2"), ALU.mult, kk("ex") + kk("t2"), kk("wg"))
            S.tt(v("wg"), v("wg"), B3(v("dd")), ALU.mult, kk("wg") + kk("dd"), kk("wg"))
            S.tt(v("wg"), v("wg"), B3(v("sg")), ALU.mult, kk("wg") + kk("sg"), kk("wg"))
            for g in range(4):
                ohg = sm["oh"][:, :, g:g + 1].to_broadcast([128, NT, 4])
                S.tt(COMB[:, :, 4 * g:4 * g + 4], v("wg"), ohg, ALU.mult, kk("wg") + kk("oh"),
                     [K("COMB", i, g) for i in range(NT)])
            for q4 in range(4):
                pst, pkt = ps_rot()
                for q in range(4):
                    i = q4 * 4 + q
                    S.tr(pst[0:16, q * 128:(q + 1) * 128], COMB[:, i, :], ident,
                         [K("COMB", i, g) for g in range(4)] + [K("c128")], [pkt])
                S.cp(CT[0:16, q4 * 512:(q4 + 1) * 512], pst[0:16, :], [pkt], [K("CT", q4)])
                S.cp(CTh[0:16, q4 * 512:(q4 + 1) * 512], CT[0:16, q4 * 512:(q4 + 1) * 512], [K("CT", q4), K("CThz")],
                     [K("CTh", q4)])
                S.tt(CT[0:16, q4 * 512:(q4 + 1) * 512], CT[0:16, q4 * 512:(q4 + 1) * 512],
                     CTh[0:16, q4 * 512:(q4 + 1) * 512], ALU.subtract, [K("CT", q4), K("CTh", q4)], [K("CT", q4)])
                S.cp(CTl[0:16, q4 * 512:(q4 + 1) * 512], CT[0:16, q4 * 512:(q4 + 1) * 512], [K("CT", q4), K("CTlz")],
                     [K("CTl", q4)])
            st_rt.close()
            S.barrier()
            HID = sb("HID", [128, 8, S_], BF16, stack=st)
            W2S = sb("W2S", [128, 8, D_], BF16, stack=st)
            CB = [sb(f"CB{j}", [128, 512], stack=st) for j in range(2)]
            SL = [sb(f"SL{j}", [128, 512], stack=st) for j in range(2)]
            n = 0
            for g in range(4):
                for el in range(4):
                    ex = 4 * g + el
                    wv, wk1_, slot = load_w(w1_d[l, ex], 8, 256, c0=0, tot=512)
                    _, wk3_, _ = load_w(w3_d[l, ex], 8, 256, slot=slot, c0=256, tot=512)
                    wk = wk1_ + wk3_
                    for tc in range(NTC):
                        sl = slice(tc * TW, (tc + 1) * TW)
                        psC, pkC = ps_rot()
                        S.mm(psC[:, :], ENp[:, ex, :], CTh[:, sl], True, False, [K("ENf"), K("CTh", tc), K("CThz")], [pkC])
                        S.mm(psC[:, :], ENp[:, ex, :], CTl[:, sl], False, True, [K("ENf"), K("CTl", tc), K("CTlz")], [pkC])
                        cb, cbk = CB[n % 2], K("CB", n % 2)
                        S.cp(cb[:], psC[:, :], [pkC], [cbk], eng="act")
                        for fc in range(2):
                            ps1, pk1 = ps_rot()
                            proj_fm(wv, wk, fc * 128, tc, ps1, pk1)
                            ps3, pk3 = ps_rot()
                            proj_fm(wv, wk, 256 + fc * 128, tc, ps3, pk3)
                            s_, sk = SL[(2 * n + fc) % 2], K("SL", (2 * n + fc) % 2)
                            S.act(s_[:], ps1[:, :], AF.Silu, [pk1], [sk])
                            S.tt(s_[:], s_[:], ps3[:, :], ALU.mult, [sk, pk3], [sk])
                            S.tt(HID[:, el * 2 + fc, sl], s_[:], cb[:], ALU.mult, [sk, cbk], [K("HID", el * 2 + fc, tc)])
                        n += 1
                    S.dma(W2S[:, 2 * el:2 * el + 2, :], w2_d[l, ex].rearrange("(fc p) n -> p fc n", p=128), [],
                          [K("W2S", el)], q="pool", arena=True)
                for oc in range(KC):
                    for tc in range(NTC):
                        ps, pk = ps_rot()
                        for j in range(8):
                            S.mm(ps[:, :], W2S[:, j, oc * 128:(oc + 1) * 128], HID[:, j, tc * TW:(tc + 1) * TW],
                                 j == 0, j == 7, [K("W2S", j // 2), K("HID", j, tc)], [pk])
                        S.tt(xT[:, oc, tc * TW:(tc + 1) * TW], xT[:, oc, tc * TW:(tc + 1) * TW], ps[:, :], ALU.add,
                             [K("xT", oc, tc), pk], [K("xT", oc, tc)])
        S.barrier()
        dump(f"x3_{l}", xT[:], ALLX, [128, KC, S_])
        if stop_after == f"M{l}":
            done = True
            break

    if stop_after is None:
        with ExitStack() as st:
            HF = sb("HFo", [128, KC, TW], stack=st)
            OB = [sb(f"OB{j}", [128, D_], stack=st) for j in range(2)]
            nob = [0]

            def final_hook(tc, r):
                sl = slice(tc * TW, (tc + 1) * TW)
                for kc in range(KC):
                    S.stt(HF[:, kc, :], xT[:, kc, sl], col(0, "fin_g", kc), r[:], ALU.mult, ALU.mult,
                          [K("xT", kc, tc), K("rsz", tc % 2), K("colp")], [K("HFo", kc)])
                for q in range(4):
                    ob, obk = OB[nob[0] % 2], K("OB", nob[0] % 2)
                    nob[0] += 1
                    for half in range(2):
                        ps, pk = ps_rot()
                        for j in range(4):
                            S.tr(ps[:, j * 128:(j + 1) * 128], HF[:, half * 4 + j, q * 128:(q + 1) * 128], ident,
                                 [K("HFo", half * 4 + j), K("c128")], [pk])
                        S.cp(ob[:, half * 512:(half + 1) * 512], ps[:, :], [pk], [obk],
                             eng=("act" if half else "dve"))
                    r0 = tc * TW + q * 128
                    i = S.dma(out_d[r0:r0 + 128, :], ob[:], [obk], [K("out", r0)])
                    S.final.append(i)

            rmsnorm_fm(xT, "xT", lambda kc: col(0, "fin_g", kc), None, None, KC, S_, st, hook=final_hook, tag="z")

    S.finalize(es)
    es.close()
    return nc, dump_d


def make_consts():
    c128 = np.zeros((128, 8, 128), np.float32)
    j = np.arange(128)[:, None]
    s = np.arange(128)[None, :]
    c128[:, 0] = np.eye(128)
    c128[:, 1] = 1.0
    c128[:, 2] = (j <= s)
    c128[:, 3] = -1.0 * (j >= s)
    c128[:, 4] = -1.0
    partner = np.where((np.arange(128) % 64) < 32, np.arange(128) + 32, np.arange(128) - 32)
    perm = np.zeros((128, 128), np.float32)
    perm[partner, np.arange(128)] = 1.0
    c128[:, 5] = perm
    c128[:, 6] = NEG * (s < j)
    cm = np.zeros((128, 12, 512), np.float32)
    p = np.arange(128)[:, None]
    f = np.arange(512)[None, :]
    for d in range(4):
        valid = (f - p) > d * 128
        cm[:, d] = valid
        cm[:, 4 + d] = NEG * (~valid)
        cm[:, 8 + d] = NEG * ((d * 128 + p) > f)
    half = 32
    inv = (np.float32(10000.0) ** (-(np.arange(half, dtype=np.float32)) / np.float32(half))).astype(np.float32)
    ang = np.arange(S_, dtype=np.float32)[None, :] * inv[:, None]
    cos = np.cos(ang).astype(np.float32)
    sin = np.sin(ang).astype(np.float32)
    rope = np.zeros((128, 2, S_), np.float32)
    for pp in range(128):
        d = pp % 64
        rope[pp, 0] = cos[d % 32]
        rope[pp, 1] = -sin[d % 32] if d < 32 else sin[d % 32]
    en = np.zeros((16, 16, 128), np.float32)
    for n in range(16):
        en[n, n, :] = 1.0
    gc = np.zeros((128, 3, 16, 8), np.float32)
    for i in range(16):
        own = i // 2
        for n in range(8):
            gc[:, 0, i, n] = 0.0 if n < own else -1e30
            gc[:, 1, i, n] = 1.0 if n < own else 0.0
            gc[:, 2, i, n] = 1.0 if n == own else 0.0
    blkoh = np.zeros((8, S_), np.float32)
    for n in range(8):
        blkoh[n, n * 256:(n + 1) * 256] = 1.0
    return dict(c128=c128.reshape(128, -1), cmask=cm.reshape(128, -1), rope=rope.reshape(128, -1),
                en=en.reshape(16, -1), gatec=gc.reshape(128, -1), blkoh=blkoh)


def colvec(v):
    v = np.asarray(v, np.float32)
    return np.ascontiguousarray(v.reshape(-1, 128).T)


def pack_params(inp):
    colp = np.zeros((128, L_, NCOL), np.float32)
    rowp = np.zeros((128, L_, NROW), np.float32)

    def put(l, name, arr):
        c0, w = COLS[name]
        assert arr.shape == (128, w), (name, arr.shape)
        colp[:, l, c0:c0 + w] = arr

    for l in range(L_):
        put(l, "mix_g", colvec(inp["mix_norm_g"][l]))
        put(l, "xat_g", colvec(inp["xattn_norm_g"][l]))
        put(l, "mem_g", colvec(inp["mem_norm_g"][l]))
        put(l, "ffn_g", colvec(inp["ffn_norm_g"][l]))
        put(l, "grp_g", colvec(inp["group_norm_g"][l]))
        cw = inp["lru_conv_w"][l]
        put(l, "lru_cw", np.concatenate([colvec(cw[k]) for k in range(4)], axis=1).reshape(128, 4, 2)
            .transpose(0, 2, 1).reshape(128, 8))
        put(l, "lru_cb", colvec(inp["lru_conv_b"][l]))
        put(l, "lru_br", colvec(inp["lru_br"][l]))
        put(l, "lru_bi", colvec(inp["lru_bi"][l]))
        put(l, "lru_lam", colvec(inp["lru_lambda"][l]))
        sw = inp["ssm_conv_w"][l]
        put(l, "ssm_cw", np.concatenate([colvec(sw[k]) for k in range(4)], axis=1).reshape(128, 4, 6)
            .transpose(0, 2, 1).reshape(128, 24))
        put(l, "ssm_cb", colvec(inp["ssm_conv_b"][l]))
        put(l, "ssm_d", colvec(np.repeat(inp["ssm_d"][l], 64)))
        put(l, "fin_g", colvec(inp["final_norm_g"]))
        r0, w = ROWS["dt_bias"]
        rowp[:, l, r0:r0 + w] = np.tile(inp["ssm_dt_bias"][l], 16)[None, :]
        r0, w = ROWS["a_log"]
        rowp[:, l, r0:r0 + w] = np.tile(inp["ssm_a_log"][l], 16)[None, :]
        r0, w = ROWS["bg"]
        rowp[:, l, r0:r0 + w] = inp["router_group_b"][l][None, :]
        r0, w = ROWS["be"]
        rowp[:, l, r0:r0 + w] = inp["router_expert_b"][l][None, :]
    wbd = np.zeros((L_, 2, 2, 128, 128), np.float32)
    for l in range(L_):
        for gi, nm in enumerate(("lru_wr", "lru_wi")):
            for c in range(2):
                for hh in range(2):
                    wbd[l, gi, c, hh * 64:(hh + 1) * 64, hh * 64:(hh + 1) * 64] = inp[nm][l, 2 * c + hh]
    router_w = np.concatenate([inp["router_group_w"], inp["router_expert_w"]], axis=2)
    return dict(colp=colp.reshape(128, -1), rowp=rowp.reshape(128, -1), lru_wbd=wbd,
                router_w=np.ascontiguousarray(router_w))


SHARED = ["w_in", "w_out", "xattn_wq", "xattn_wkv", "xattn_wo", "expert_w1", "expert_w3", "expert_w2"]


def make_in_maps(inputs, cores):
    inp = {k: np.asarray(v) for k, v in inputs.items()}
    consts = make_consts()
    packed = pack_params(inp)
    shared = {k: np.ascontiguousarray(inp[k], dtype=np.float32) for k in SHARED}
    maps = []
    for b in cores:
        m = dict(shared)
        m.update(consts)
        m.update(packed)
        m["x"] = np.ascontiguousarray(inp["x"][b])
        m["mem"] = np.ascontiguousarray(inp["mem"][b])
        maps.append(m)
    return maps


_CACHE = {}


def kernel(**inputs):
    if "nc" not in _CACHE:
        _CACHE["nc"] = build_program()[0]
    nc = _CACHE["nc"]
    maps = make_in_maps(inputs, list(range(8)))
    res = run_bass_kernel_spmd(nc, maps, core_ids=list(range(8)))
    return np.stack([r["out"] for r in res.results], axis=0).astype(np.float32)
```

```python
import numpy as np
from contextlib import ExitStack
import concourse.bass as bass
import concourse.mybir as mybir
from concourse.bass_utils import run_bass_kernel_spmd

F32 = mybir.dt.float32
BF16 = mybir.dt.bfloat16
F32R = mybir.dt.float32r
AF = mybir.ActivationFunctionType
ALU = mybir.AluOpType
AX = mybir.AxisListType

S_ = 2048
D_ = 1024
L_ = 2
KC = 8
NTC = 4
TW = 512
NT = 16
NEG = -30000.0
EPS = 1e-6

COLS = {}
_c = 0
for _n, _w in [("mix_g", 8), ("xat_g", 8), ("mem_g", 8), ("ffn_g", 8), ("grp_g", 8),
               ("lru_cw", 8), ("lru_cb", 2), ("lru_br", 2), ("lru_bi", 2), ("lru_lam", 2),
               ("ssm_cw", 24), ("ssm_cb", 6), ("ssm_d", 2), ("fin_g", 8)]:
    COLS[_n] = (_c, _w)
    _c += _w
NCOL = _c
ROWS = {}
_c = 0
for _n, _w in [("dt_bias", 64), ("a_log", 64), ("bg", 4), ("be", 16)]:
    ROWS[_n] = (_c, _w)
    _c += _w
NROW = _c


def _free(ap):
    n = 1
    for d in ap.shape[1:]:
        n *= int(d)
    return n


class Sched:
    ENG = ("pe", "act", "dve", "pool", "sp")
    GATED = ("pe", "act", "dve", "sp")

    def __init__(self, nc):
        self.nc = nc
        self.ops = []
        self.lastw = {}
        self.rd = {}
        self.by_eng = {e: [] for e in self.ENG}
        self.final = []
        self.psn = 0
        self.seg = 0
        self.act_inorder = set()

    def add(self, eng, fn, reads=(), writes=(), dma=False, cost=0.3, arena=False):
        i = len(self.ops)
        reads = list(reads)
        writes = list(writes)
        deps = {}
        for r in reads:
            w = self.lastw.get(r)
            if w is not None:
                deps.setdefault(w, set()).add("raw")
        for w_ in writes:
            w = self.lastw.get(w_)
            if w is not None:
                deps.setdefault(w, set()).add("waw")
            for r in self.rd.get(w_, ()):
                deps.setdefault(r, set()).add("war")
        ws = set(writes)
        for r in reads:
            if r not in ws:
                self.rd.setdefault(r, []).append(i)
        for w_ in writes:
            self.lastw[w_] = i
            self.rd[w_] = []
        deps.pop(i, None)
        fdeps = set()
        for d, kinds in deps.items():
            od = self.ops[d]
            if od["eng"] == eng and not od["dma"] and not dma:
                if eng == "pe":
                    continue
                if "raw" not in kinds:
                    continue
            fdeps.add(d)
        self.ops.append(dict(eng=eng, fn=fn, deps=fdeps, order=set(deps.keys()), dma=dma, signal=dma, token=None,
                             seg=self.seg, cost=cost, arena=arena))
        self.by_eng[eng].append(i)
        return i

    def barrier(self):
        self.seg += 1

    def schedule(self, W=24):
        ops = self.ops
        n = len(ops)
        succ = [[] for _ in range(n)]
        nun = [0] * n
        for i, op in enumerate(ops):
            nun[i] = len(op["order"])
            for d in op["order"]:
                succ[d].append(i)
        ready = [0.0] * n
        fin = [0.0] * n
        done = [False] * n
        head = {e: 0 for e in self.ENG}
        free = {e: 0.0 for e in self.ENG}
        lists = {e: list(self.by_eng[e]) for e in self.ENG}
        new_order = {e: [] for e in self.ENG}
        nseg = self.seg + 1
        rem = [0] * (nseg + 1)
        for i, op in enumerate(ops):
            if op["eng"] in self.GATED:
                rem[op["seg"]] += 1
        import os
        WDEF = {"pe": W, "act": W, "dve": W}
        WE = {e_: int(os.environ.get("KSCHED_W_" + e_.upper(), WDEF[e_])) for e_ in ("pe", "act", "dve")}
        segbusy = {}
        segend = {}
        cur = 0
        seg_t0 = 0.0
        tmax = 0.0
        nsched = 0
        while nsched < n:
            while cur < nseg and rem[cur] == 0:
                cur += 1
                seg_t0 = tmax
            best = None
            for e in self.ENG:
                lst = lists[e]
                h = head[e]
                while h < len(lst) and done[lst[h]]:
                    h += 1
                head[e] = h
                w = WE.get(e, 1)
                if e == "act" and cur in self.act_inorder:
                    w = 1
                seen = 0
                j = h
                while j < len(lst) and seen < w:
                    i = lst[j]
                    j += 1
                    if done[i]:
                        continue
                    op = ops[i]
                    if e != "pool":
                        if op["seg"] != cur:
                            break
                    elif op["arena"] and op["seg"] > cur:
                        break
                    seen += 1
                    if nun[i] == 0:
                        est = max(free[e], ready[i])
                        if e != "pool" or op["arena"]:
                            est = max(est, seg_t0)
                        key = (est, i)
                        if best is None or key < best[0]:
                            best = (key, e, i)
            if best is None:
                raise RuntimeError("scheduler stuck at seg %d" % cur)
            (est, _), e, i = best
            op = ops[i]
            if op["dma"]:
                free[e] = est + 0.06
                f = est + op["cost"]
            else:
                f = est + op["cost"]
                free[e] = f
            fin[i] = f
            tmax = max(tmax, f)
            if not op["dma"]:
                segbusy[(op["seg"], e)] = segbusy.get((op["seg"], e), 0.0) + op["cost"]
            segend[op["seg"]] = max(segend.get(op["seg"], 0.0), f)
            done[i] = True
            nsched += 1
            new_order[e].append(i)
            if e in self.GATED:
                rem[op["seg"]] -= 1
            for s_ in succ[i]:
                nun[s_] -= 1
                lat = 0.0 if (ops[s_]["eng"] == e and not op["dma"]) else 0.1
                if f + lat > ready[s_]:
                    ready[s_] = f + lat
        self.by_eng = new_order
        self.sim_time = tmax
        import os
        if os.environ.get("KSCHED_DEBUG"):
            print("sched: ops", n, "sim_time_us", round(tmax, 1), flush=True)
            prev = 0.0
            for sg in sorted(segend):
                print("  seg", sg, "dur", round(segend[sg] - prev, 1), {e_: round(segbusy.get((sg, e_), 0.0), 1)
                                                                         for e_ in ("pe", "act", "dve")})
                prev = segend[sg]
        pos = {}
        for e in self.ENG:
            for k, i in enumerate(new_order[e]):
                pos[i] = k
        bar = [set() for _ in range(nseg + 1)]
        for s_ in range(1, nseg + 1):
            for e in ("pe", "act", "dve"):
                last = None
                for i in new_order[e]:
                    if ops[i]["seg"] < s_:
                        last = i
                    else:
                        break
                if last is not None:
                    bar[s_].add(last)
            for i in new_order["sp"]:
                if ops[i]["seg"] == s_ - 1 and ops[i]["dma"]:
                    bar[s_].add(i)
        for e in self.GATED:
            lastseg = 0
            for i in new_order[e]:
                sg = ops[i]["seg"]
                if sg > lastseg:
                    for t in range(lastseg + 1, sg + 1):
                        ops[i]["deps"] |= bar[t]
                    lastseg = sg
        for i in new_order["pool"]:
            if ops[i]["arena"]:
                ops[i]["deps"] |= bar[ops[i]["seg"]]
        for i, op in enumerate(ops):
            keep = set()
            bestp = {}
            for d in op["deps"]:
                if d == i:
                    continue
                od = ops[d]
                if od["dma"]:
                    keep.add(d)
                else:
                    pe_ = od["eng"]
                    if pe_ not in bestp or pos[d] > pos[bestp[pe_]]:
                        bestp[pe_] = d
            keep |= set(bestp.values())
            op["deps"] = keep

    def mm(self, out, lhsT, rhs, start, stop, reads, writes):
        nfree = _free(rhs)
        c = max(nfree, 64) / 2400.0 * (4.0 if rhs.dtype == F32 else 1.0) + 0.004
        return self.add("pe", lambda e: e.matmul(out, lhsT, rhs, start=start, stop=stop), reads, writes, cost=c)

    def tr(self, out, in_, ident, reads, writes):
        return self.add("pe", lambda e: e.transpose(out, in_, ident), reads, writes, cost=0.25)

    def act(self, out, in_, func, reads, writes, bias=None, scale=None, accum=None):
        kw = {}
        if bias is not None:
            kw["bias"] = bias
        if scale is not None:
            kw["scale"] = scale
        if accum is not None:
            kw["accum_out"] = accum
        c = 0.22 + _free(out) / 1400.0
        return self.add("act", lambda e: e.activation(out=out, in_=in_, func=func, **kw), reads, writes, cost=c)

    def _vc(self, out):
        return 0.08 + _free(out) / 960.0

    def tt(self, out, in0, in1, op, reads, writes, eng="dve"):
        return self.add(eng, lambda e: e.tensor_tensor(out=out, in0=in0, in1=in1, op=op), reads, writes,
                        cost=self._vc(out))

    def ts(self, out, in0, s1, s2, op0, op1, reads, writes, eng="dve"):
        if s2 is None:
            return self.add(eng, lambda e: e.tensor_scalar(out, in0, s1, None, op0), reads, writes, cost=self._vc(out))
        return self.add(eng, lambda e: e.tensor_scalar(out, in0, s1, s2, op0, op1), reads, writes, cost=self._vc(out))

    def stt(self, out, in0, scalar, in1, op0, op1, reads, writes, eng="dve"):
        return self.add(eng, lambda e: e.scalar_tensor_tensor(out=out, in0=in0, scalar=scalar, in1=in1,
                                                             op0=op0, op1=op1), reads, writes, cost=self._vc(out))

    def cp(self, out, in_, reads, writes, eng="dve"):
        if eng == "act":
            return self.add("act", lambda e: e.activation(out=out, in_=in_, func=AF.Copy), reads, writes,
                            cost=0.22 + _free(out) / 1400.0)
        return self.add(eng, lambda e: e.tensor_copy(out=out, in_=in_), reads, writes, cost=self._vc(out))

    def recip(self, out, in_, reads, writes):
        return self.add("dve", lambda e: e.reciprocal(out=out, in_=in_), reads, writes, cost=0.1 + _free(out) / 240.0)

    def dma(self, out, in_, reads, writes, q="sp", arena=False):
        nbytes = 128 * _free(out) * 4
        return self.add(q, lambda e: e.dma_start(out=out, in_=in_), reads, writes, dma=True,
                        cost=2.0 + nbytes / 150e3, arena=arena)

    def finalize(self, es):
        nc = self.nc
        self.schedule()
        for op in self.ops:
            for d in op["deps"]:
                self.ops[d]["signal"] = True
        for d in self.final:
            self.ops[d]["signal"] = True
        csem = {e: es.enter_context(nc.semaphore("c_" + e)) for e in ("pe", "act", "dve", "pool")}
        NP = 16
        qsem = {q: [es.enter_context(nc.semaphore(f"q_{q}{k}")) for k in range(NP)] for q in ("sp", "pool")}
        cnt = {e: 0 for e in csem}
        qn = {q: 0 for q in qsem}
        quse = {q: [0] * NP for q in qsem}
        qprev = {q: [None] * NP for q in qsem}
        for i, op in enumerate(self.ops):
            if op["dma"]:
                q = op["eng"]
                k = qn[q] % NP
                qn[q] += 1
                quse[q][k] += 1
                op["token"] = (qsem[q][k], 16 * quse[q][k])
                if qprev[q][k] is not None:
                    op["deps"].add(qprev[q][k])
                qprev[q][k] = i
        for e in csem:
            for i in self.by_eng[e]:
                op = self.ops[i]
                if (not op["dma"]) and op["signal"]:
                    cnt[e] += 1
                    op["token"] = (csem[e], cnt[e])
        ops = self.ops
        final = list(self.final)

        def emit(engname, e):
            waited = {}
            for i in self.by_eng[engname]:
                op = ops[i]
                for d in sorted(op["deps"]):
                    sem, val = ops[d]["token"]
                    if waited.get(sem, 0) < val:
                        e.wait_ge(sem, val)
                        waited[sem] = val
                ins = op["fn"](e)
                if op["signal"]:
                    ins.then_inc(op["token"][0], 16 if op["dma"] else 1)
            if engname == "sp":
                for d in final:
                    sem, val = ops[d]["token"]
                    if waited.get(sem, 0) < val:
                        e.wait_ge(sem, val)
                        waited[sem] = val

        with nc.Block() as block:
            @block.tensor
            def _(e):
                emit("pe", e)

            @block.scalar
            def _(e):
                emit("act", e)

            @block.vector
            def _(e):
                emit("dve", e)

            @block.gpsimd
            def _(e):
                emit("pool", e)

            @block.sync
            def _(e):
                emit("sp", e)


def K(name, *idx):
    return (name,) + idx


def build_program(stop_after=None, dumps=(), n_layers=L_):
    nc = bass.Bass("TRN2", target_bir_lowering=False)
    S = Sched(nc)
    es = ExitStack()
    P = {}

    def din(name, shape):
        P[name] = nc.dram_tensor(name, list(shape), F32, kind="ExternalInput").ap()
        return P[name]

    x_d = din("x", [S_, D_])
    mem_d = din("mem", [256, D_])
    w_in_d = din("w_in", [L_, D_, 3076])
    w_out_d = din("w_out", [L_, D_, D_])
    wq_d = din("xattn_wq", [L_, D_, D_])
    wkv_d = din("xattn_wkv", [L_, D_, 2 * D_])
    wo_d = din("xattn_wo", [L_, D_, D_])
    w1_d = din("expert_w1", [L_, 16, D_, 256])
    w3_d = din("expert_w3", [L_, 16, D_, 256])
    w2_d = din("expert_w2", [L_, 16, 256, D_])
    wrt_d = din("router_w", [L_, D_, 20])
    lrubd_d = din("lru_wbd", [L_, 2, 2, 128, 128])
    colp_d = din("colp", [128, L_ * NCOL])
    rowp_d = din("rowp", [128, L_ * NROW])
    c128_d = din("c128", [128, 8 * 128])
    cmask_d = din("cmask", [128, 12 * 512])
    rope_d = din("rope", [128, 2 * S_])
    en_d = din("en", [16, 16 * 128])
    blkoh_d = din("blkoh", [8, S_])
    gate_c_d = din("gatec", [128, 3 * 128])
    out_d = nc.dram_tensor("out", [S_, D_], F32, kind="ExternalOutput").ap()
    dump_d = {}

    uid = [0]

    def sb(name, shape, dt=F32, stack=None):
        uid[0] += 1
        return (stack or es).enter_context(nc.sbuf_tensor(f"{name}_{uid[0]}", list(shape), dt))

    xT = sb("xT", [128, KC, S_])
    hT = sb("hT", [128, KC, S_], BF16)
    colp = sb("colp_s", [128, L_ * NCOL])
    rowp = sb("rowp_s", [128, L_ * NROW])
    c128 = sb("c128_s", [128, 8 * 128])
    c128b = sb("c128b_s", [128, 2 * 128], BF16)
    W = [sb(f"wslot{i}", [128, KC, 512], BF16) for i in range(3)]
    PS = [es.enter_context(nc.psum_tensor(f"ps{i}", [128, 512], F32)) for i in range(8)]
    wn = [0]

    ident = c128[:, 0:128]
    ones_f = c128[:, 128:256]
    tri_le = c128[:, 256:384]
    negu = c128[:, 384:512]
    negones = c128[:, 512:640]
    perm_f = c128[:, 640:768]
    ssdneg = c128[:, 768:896]
    ones_b = c128b[:, 128:256]
    ident_b = c128b[:, 0:128]

    cst = sb("cst", [128, 4])
    S.add("dve", lambda e: e.memset(cst[:, 0:1], EPS), [], [K("cst0")])
    S.add("dve", lambda e: e.memset(cst[:, 1:2], 1.0), [], [K("cst1")])
    S.add("dve", lambda e: e.memset(cst[:, 2:4], 0.0), [K("cst0"), K("cst1")], [K("cst")])
    S.dma(colp[:], colp_d[:, :], [], [K("colp")])
    S.dma(rowp[:], rowp_d[:, :], [], [K("rowp")])
    S.dma(c128[:], c128_d[:, :], [], [K("c128")])
    S.dma(c128b[:], c128_d[:, 0:256], [], [K("c128b")], q="pool")

    def col(l, name, j=0, n=1):
        c0, w = COLS[name]
        return colp[:, l * NCOL + c0 + j: l * NCOL + c0 + j + n]

    def row(l, name, j=0, n=None):
        c0, w = ROWS[name]
        n = w if n is None else n
        return rowp[:, l * NROW + c0 + j: l * NROW + c0 + j + n]

    def ps_rot():
        k = S.psn % 4
        S.psn += 1
        return PS[k], K("ps", k)

    Wf = [w[:].rearrange("p a b -> p (a b)") for w in W]

    def load_w(src2d, nk, ncols, slot=None, c0=0, tot=None):
        tot = ncols if tot is None else tot
        if slot is None:
            k = wn[0] % 3
            wn[0] += 1
        else:
            k = slot
        v = Wf[k][:, 0:nk * tot].rearrange("p (a b) -> p a b", a=nk)
        step = 2 if nk >= 2 else 1
        allk = [K("w", k, j_, hf_) for j_ in range(4) for hf_ in range(2)]
        for h in range(0, nk, step):
            if nk == 8 and tot == 512:
                halves = [hf_ for hf_ in range(2) if c0 < (hf_ + 1) * 256 and c0 + ncols > hf_ * 256]
                wkeys_ = [K("w", k, h // 2, hf_) for hf_ in halves]
            else:
                wkeys_ = allk
            S.dma(v[:, h:h + step, c0:c0 + ncols],
                  src2d[h * 128:(h + step) * 128, :].rearrange("(kc p) n -> p kc n", p=128),
                  [], wkeys_, q="pool")
        return v, allk, k

    def dump(name, ap, reads, shape):
        if name in dumps:
            d = nc.dram_tensor("dbg_" + name, list(shape), ap.dtype, kind="ExternalOutput").ap()
            dump_d[name] = d
            i = S.dma(d, ap, reads, [K("dbg", name)])
            S.final.append(i)

    def load_fm(dst, src_d, ntile, dkey, st):
        xin = [sb(f"xin{j}", [128, D_], stack=st) for j in range(2)]
        for i in range(ntile):
            b = xin[i % 2]
            S.dma(b[:], src_d[i * 128:(i + 1) * 128, :], [], [K("xin", i % 2)])
            for half in range(2):
                ps, pk = ps_rot()
                for j in range(4):
                    kc = half * 4 + j
                    S.tr(ps[:, j * 128:(j + 1) * 128], b[:, kc * 128:(kc + 1) * 128], ident,
                         [K("xin", i % 2), K("c128")], [pk])
                S.cp(dst[:, half * 4:half * 4 + 4, i * 128:(i + 1) * 128],
                     ps[:].rearrange("p (a b) -> p a b", a=4), [pk],
                     [K(dkey, half * 4 + j, i // 4) for j in range(4)], eng=("dve" if half == 0 else "act"))

    st0 = ExitStack()
    load_fm(xT, x_d, NT, "xT", st0)

    def rmsnorm_fm(src, skey, gcol, dst, dkey, nk, T, st, dscale=1.0, hook=None, tag="n"):
        tw = min(T, TW)
        sq = [sb(f"sq_{tag}{j}", [128, nk, tw], BF16, stack=st) for j in range(2)]
        rs = [sb(f"rs_{tag}{j}", [128, tw], stack=st) for j in range(2)]
        for tc in range(T // tw):
            sl = slice(tc * tw, (tc + 1) * tw)
            q = sq[tc % 2]
            for kc in range(nk):
                S.act(q[:, kc, :], src[:, kc, sl], AF.Square, [K(skey, kc, tc)], [K("sq" + tag, tc % 2, kc)])
            ps, pk = ps_rot()
            for kc in range(nk):
                S.mm(ps[:, 0:tw], ones_b, q[:, kc, :], kc == 0, kc == nk - 1,
                     [K("sq" + tag, tc % 2, kc), K("c128b")], [pk])
            r = rs[tc % 2]
            S.act(r[:], ps[:, 0:tw], AF.Sqrt, [pk, K("cst")], [K("rs" + tag, tc % 2)],
                  bias=cst[:, 0:1], scale=1.0 / (nk * 128))
            S.add("dve", (lambda e, r=r: e.reciprocal(out=r[:], in_=r[:])),
                  [K("rs" + tag, tc % 2)], [K("rs" + tag, tc % 2)], cost=0.1 + tw / 240.0)
            if dst is not None:
                for kc in range(nk):
                    S.stt(dst[:, kc, sl], src[:, kc, sl], gcol(kc), r[:], ALU.mult, ALU.mult,
                          [K(skey, kc, tc), K("rs" + tag, tc % 2), K("colp")], [K(dkey, kc, tc)],
                          eng="dve")
            if hook is not None:
                hook(tc, r)

    ALLH = [K("hT", kc, tc) for kc in range(KC) for tc in range(NTC)]

    def hkeys(tc):
        return [K("hT", kc, tc) for kc in range(KC)]

    def proj_fm(wv, wkeys, c0, tc, ps, pk):
        for kc in range(KC):
            S.mm(ps[:, :], wv[:, kc, c0:c0 + 128], hT[:, kc, tc * TW:(tc + 1) * TW], kc == 0, kc == KC - 1,
                 wkeys + [K("hT", kc, tc)], [pk])

    def conv_chunk(ps, pk, tc, xw, xwk, cwcol, cbcol, dst, dkey):
        w_ = xw[tc % 2]
        wk = K(xwk, tc % 2)
        if tc == 0:
            S.add("dve", lambda e: e.memset(w_[:, 0:3], 0.0), [], [wk])
        else:
            S.cp(w_[:, 0:3], xw[(tc - 1) % 2][:, 512:515], [K(xwk, (tc - 1) % 2)], [wk])
        S.cp(w_[:, 3:515], ps[:, :], [pk], [wk], eng="act")
        S.ts(dst, w_[:, 3:515], cwcol(3), cbcol, ALU.mult, ALU.add, [wk, K("colp")], [dkey])
        for k in range(3):
            S.stt(dst, w_[:, k:k + 512], cwcol(k), dst, ALU.mult, ALU.add, [wk, dkey, K("colp")], [dkey])

    def group_out(l, g, YG, st):
        YB = sb("YB", [128, 2, S_], BF16, stack=st)
        rmsnorm_fm(YG, "YG", lambda kc: col(l, "grp_g", 2 * g + kc), YB, "YB", 2, S_, st, tag="g")
        dump(f"yn{l}_{g}", YB[:], [K("YB", c, t) for c in range(2) for t in range(NTC)], [128, 2, S_])
        for half in range(2):
            wv, wk, _ = load_w(w_out_d[l, g * 256:(g + 1) * 256, half * 512:(half + 1) * 512], 2, 512)
            for o4 in range(4):
                oc = half * 4 + o4
                for tc in range(NTC):
                    ps, pk = ps_rot()
                    for kc in range(2):
                        S.mm(ps[:, :], wv[:, kc, o4 * 128:(o4 + 1) * 128], YB[:, kc, tc * TW:(tc + 1) * TW],
                             kc == 0, kc == 1, wk + [K("YB", kc, tc)], [pk])
                    S.tt(xT[:, oc, tc * TW:(tc + 1) * TW], xT[:, oc, tc * TW:(tc + 1) * TW], ps[:, :], ALU.add,
                         [K("xT", oc, tc), pk], [K("xT", oc, tc)])

    ALLX = [K("xT", kc, tc) for kc in range(KC) for tc in range(NTC)]
    done = False
    for l in range(n_layers):
        with ExitStack() as st:
            rmsnorm_fm(xT, "xT", lambda kc: col(l, "mix_g", kc), hT, "hT", KC, S_, (st0 if l == 0 else st), tag="a")
        if l == 0:
            st0.close()
        S.barrier()
        if l == 0:
            dump("h0", hT[:], ALLH, [128, KC, S_])
        if stop_after == "norm0":
            break

        with ExitStack() as sty, ExitStack() as st:
            YG = sb("YG", [128, 2, S_], stack=sty)
            wv, wk, _ = load_w(w_in_d[l, :, 0:512], 8, 512)
            wbd = sb("wbd", [128, 4, 128], BF16, stack=st)
            S.dma(wbd[:], lrubd_d[l].rearrange("g c k m -> k (g c) m"), [], [K("wbd")], q="pool", arena=True)
            c8 = sb("c8", [128, 2], stack=st)
            dump(f"A_wbd{l}", wbd[:], [K("wbd")], [128, 4, 128])
            cy = sb("cy", [128, 2], stack=st)
            cz = sb("cz", [128, 2], stack=st)
            S.act(cy[:], col(l, "lru_lam", 0, 2), AF.Exp, [K("colp")], [K("cy")], scale=-1.0)
            S.ts(cz[:], cy[:], 2.0, None, ALU.add, None, [K("cy")], [K("cz")])
            S.add("dve", lambda e: e.reciprocal(out=cz[:], in_=cz[:]), [K("cz")], [K("cz")])
            S.tt(cz[:], cz[:], cy[:], ALU.mult, [K("cz"), K("cy")], [K("cz")])
            S.tt(cy[:], cz[:], cz[:], ALU.mult, [K("cz")], [K("cy")])
            S.ts(c8[:], cy[:], 1.0 / 9.0, 1.0 / 7.0, ALU.mult, ALU.add, [K("cy")], [K("c8")])
            for cc_ in (1.0 / 5.0, 1.0 / 3.0, 1.0):
                S.tt(c8[:], c8[:], cy[:], ALU.mult, [K("c8"), K("cy")], [K("c8")])
                S.ts(c8[:], c8[:], cc_, None, ALU.add, None, [K("c8")], [K("c8")])
            S.tt(c8[:], c8[:], cz[:], ALU.mult, [K("c8"), K("cz")], [K("c8")])
            S.ts(c8[:], c8[:], -16.0, None, ALU.mult, None, [K("c8")], [K("c8")])
            xw = [sb(f"xw{j}", [128, 515], stack=st) for j in range(2)]
            XC = sb("XC", [128, S_], stack=st)
            XCB = sb("XCB", [128, S_], BF16, stack=st)
            GG = sb("GG", [128, S_], stack=st)
            RA = sb("RA", [128, S_], stack=st)
            IU = sb("IU", [128, S_], stack=st)
            T2 = sb("T2", [128, S_], stack=st)
            XX = sb("XX", [128, S_], stack=st)
            for c in range(2):
                for tc in range(NTC):
                    sl = slice(tc * TW, (tc + 1) * TW)
                    ps, pk = ps_rot()
                    proj_fm(wv, wk, c * 128, tc, ps, pk)
                    conv_chunk(ps, pk, tc, xw, "xw", lambda k: col(l, "lru_cw", c * 4 + k), col(l, "lru_cb", c),
                               XC[:, sl], K("XC", tc))
                    ps, pk = ps_rot()
                    proj_fm(wv, wk, 256 + c * 128, tc, ps, pk)
                    S.cp(GG[:, sl], ps[:, :], [pk], [K("GG", tc)], eng="act")
                XCk = [K("XC", t) for t in range(NTC)]
                GGk = [K("GG", t) for t in range(NTC)]
                S.tt(T2[:], GG[:], GG[:], ALU.mult, GGk, [K("T2")])
                S.ts(T2[:], T2[:], 0.044715, 1.0, ALU.mult, ALU.add, [K("T2")], [K("T2")])
                S.tt(T2[:], T2[:], GG[:], ALU.mult, [K("T2")] + GGk, [K("T2")])
                S.act(T2[:], T2[:], AF.Sigmoid, [K("T2")], [K("T2")], scale=1.5957691216)
                S.tt(GG[:], GG[:], T2[:], ALU.mult, [K("T2")] + GGk, GGk)
                S.cp(XCB[:], XC[:], XCk, [K("XCB")], eng="act")
                for tc in range(NTC):
                    sl = slice(tc * TW, (tc + 1) * TW)
                    ps, pk = ps_rot()
                    S.mm(ps[:, :], wbd[:, c, :], XCB[:, sl], True, True, [K("wbd"), K("XCB")], [pk])
                    S.act(RA[:, sl], ps[:, :], AF.Sigmoid, [pk, K("colp")], [K("RA", tc)], bias=col(l, "lru_br", c))
                    ps, pk = ps_rot()
                    S.mm(ps[:, :], wbd[:, 2 + c, :], XCB[:, sl], True, True, [K("wbd"), K("XCB")], [pk])
                    S.act(IU[:, sl], ps[:, :], AF.Sigmoid, [pk, K("colp")], [K("IU", tc)], bias=col(l, "lru_bi", c))
                RAk = [K("RA", t) for t in range(NTC)]
                IUk = [K("IU", t) for t in range(NTC)]
                if c == 1:
                    dump(f"A_r{l}", RA[:], RAk, [128, S_])
                    dump(f"A_i{l}", IU[:], IUk, [128, S_])
                    dump(f"A_xcb{l}", XCB[:], [K("XCB")], [128, S_])
                    dump(f"A_xc{l}", XC[:], XCk, [128, S_])
                S.ts(XX[:], RA[:], c8[:, c:c + 1], 2.0, ALU.mult, ALU.mult, RAk + [K("c8")], [K("XX")])
                S.ts(T2[:], XX[:], 1.0 / 120.0, None, ALU.mult, None, [K("XX")], [K("T2")])
                for cc_ in (1.0 / 24.0, 1.0 / 6.0, 0.5, 1.0):
                    S.stt(T2[:], T2[:], cc_, XX[:], ALU.add, ALU.mult, [K("T2"), K("XX")], [K("T2")])
                S.act(T2[:], T2[:], AF.Sqrt, [K("T2")], [K("T2")], scale=-1.0)
                S.act(RA[:], XX[:], AF.Exp, [K("XX")], RAk, scale=0.5)
                S.tt(IU[:], IU[:], XC[:], ALU.mult, IUk + XCk, IUk)
                S.tt(IU[:], IU[:], T2[:], ALU.mult, IUk + [K("T2")], IUk)
                S.add("dve", lambda e: e.tensor_tensor_scan(out=XC[:], data0=RA[:], data1=IU[:], initial=0.0,
                                                            op0=ALU.mult, op1=ALU.add), RAk + IUk, XCk, cost=2.6)
                S.tt(YG[:, c, :], XC[:], GG[:], ALU.mult, XCk + GGk, [K("YG", c, t) for t in range(NTC)])
            for nm_, t_ in (("XC", XC), ("GG", GG), ("RA", RA), ("IU", IU), ("T2", T2)):
                dump(f"A_{nm_}{l}", t_[:], [K(nm_, t) for t in range(NTC)] + [K(nm_)], [128, S_])
            dump(f"ya{l}", YG[:], [K("YG", c, t) for c in range(2) for t in range(NTC)], [128, 2, S_])
            st.close()
            S.barrier()
            group_out(l, 0, YG, sty)
        S.barrier()
        if stop_after == f"A{l}":
            done = True
            break

        with ExitStack() as sty, ExitStack() as st:
            YG = sb("YG", [128, 2, S_], stack=sty)
            QB = sb("QB", [128, 2, S_], BF16, stack=st)
            KZ = sb("KZ", [128, 4, S_], BF16, stack=st)
            S.add("dve", lambda e: e.memset(KZ[:], 0.0), [], [K("KZ", h_, t_) for h_ in range(4) for t_ in range(NTC)], cost=4.5)
            VB = sb("VB", [128, NT, 256], BF16, stack=st)
            M01 = sb("M01", [128, 4, 512], stack=st)
            NM = sb("NM", [128, 4, 512], BF16, stack=st)
            S.dma(M01[:], cmask_d[:, 0:2048].rearrange("p (a b) -> p a b", a=4), [], [K("M01")])
            S.dma(NM[:], cmask_d[:, 2048:4096].rearrange("p (a b) -> p a b", a=4), [], [K("NM")], q="pool", arena=True)
            wv, wk, _ = load_w(w_in_d[l, :, 512:1024], 8, 512)
            wv2, wk2, _ = load_w(w_in_d[l, :, 1024:1280], 8, 256)
            for c2 in range(2):
                for tc in range(NTC):
                    sl = slice(tc * TW, (tc + 1) * TW)
                    ps, pk = ps_rot()
                    proj_fm(wv, wk, c2 * 128, tc, ps, pk)
                    S.act(QB[:, c2, sl], ps[:, :], AF.Copy, [pk], [K("QB", c2, tc)], scale=0.125)
                    ps, pk = ps_rot()
                    proj_fm(wv, wk, 256 + c2 * 128, tc, ps, pk)
                    S.cp(KZ[0:64, 2 * c2, sl], ps[0:64, :], [pk], [K("KZ", 2 * c2, tc)])
                    S.cp(KZ[64:128, 2 * c2 + 1, sl], ps[64:128, :], [pk], [K("KZ", 2 * c2 + 1, tc)], eng="act")
            for i in range(NT):
                ps, pk = ps_rot()
                for kc in range(KC):
                    S.mm(ps[:, 0:256], hT[:, kc, i * 128:(i + 1) * 128], wv2[:, kc, :], kc == 0, kc == KC - 1,
                         wk2 + [K("hT", kc, i // 4)], [pk])
                S.cp(VB[:, i, :], ps[:, 0:256], [pk], [K("VB", i)], eng=("dve" if i % 2 else "act"))
            Eb = [sb(f"Eb{j}", [128, 512], stack=st) for j in range(2)]
            SPb = [sb(f"SPb{j}", [128, 512], F32R, stack=st) for j in range(3)]
            Wb = [sb(f"Wb{j}", [128, 512], BF16, stack=st) for j in range(2)]
            Rb = [sb(f"Rb{j}", [128, 512], F32R, stack=st) for j in range(2)]
            NUr = sb("NUr", [128, 2, 128], F32R, stack=st)
            S.cp(NUr[:, 0, :], negu, [K("c128")], [K("NUr")])
            S.cp(NUr[:, 1, :], negones, [K("c128")], [K("NUr")])
            items = []
            grp = 0
            for h in range(4):
                for c in range(NTC):
                    nb = 4 * c + 4
                    for idx, b in enumerate(range(nb - 1, -1, -1)):
                        items.append(dict(h=h, c=c, idx=idx, b=b, grp=grp, last=(b == 0)))
                    grp += 1
            for i_, itm in enumerate(items):
                itm["i"] = i_
                itm["pr"] = slice((itm["h"] % 2) * 64, (itm["h"] % 2) * 64 + 64)
                itm["c2"] = itm["h"] // 2
                itm["d"] = itm["b"] - 4 * itm["c"]
                itm["ksl"] = slice(itm["b"] * 128, (itm["b"] + 1) * 128)
                itm["qsl"] = slice(itm["c"] * TW, (itm["c"] + 1) * TW)
                itm["qk"] = [K("KZ", itm["h"], itm["b"] // 4), K("QB", itm["c2"], itm["c"])]

            def sb_s1(m):
                i = m["i"]
                pr, c2 = m["pr"], m["c2"]
                off = 128 * max(m["d"], 0)
                cs = slice(off, TW)
                qs = slice(m["c"] * TW + off, (m["c"] + 1) * TW)
                if m["idx"] == 0:
                    R0, R0k = Rb[m["grp"] % 2], K("Rb", m["grp"] % 2)
                    S.ts(R0[:], M01[:, 0, :], 0.0, None, ALU.mult, None, [K("M01")], [R0k])
                psZ, pkZ = ps_rot()
                S.mm(psZ[:, cs], KZ[:, m["h"], m["ksl"]], QB[:, c2, qs], True, True, m["qk"], [pkZ])
                E, Ek = Eb[i % 2], K("Eb", i % 2)
                SP, SPk = SPb[i % 3], K("SPb", i % 3)
                S.act(E[:, cs], psZ[:, cs], AF.Exp, [pkZ], [Ek])
                S.act(SP[:, cs], E[:, cs], AF.Ln, [Ek, K("cst")], [SPk], bias=cst[:, 1:2])
                if m["d"] >= 0:
                    S.tt(SP[:, cs], SP[:, cs].bitcast(F32), M01[:, m["d"], cs], ALU.mult, [SPk, K("M01")], [SPk])

            def sb_s2(m):
                i = m["i"]
                pr, c2, d = m["pr"], m["c2"], m["d"]
                off = 128 * max(d, 0)
                cs = slice(off, TW)
                qs = slice(m["c"] * TW + off, (m["c"] + 1) * TW)
                SP, SPk = SPb[i % 3], K("SPb", i % 3)
                Wt, Wk_ = Wb[i % 2], K("Wb", i % 2)
                R, Rk = Rb[m["grp"] % 2], K("Rb", m["grp"] % 2)
                psA, pkA = ps_rot()
                has_r = m["idx"] > 0
                S.mm(psA[:, cs], KZ[:, m["h"], m["ksl"]], QB[:, c2, qs], True, False, m["qk"], [pkA])
                S.mm(psA[:, cs], NUr[:, 0, :], SP[:, cs], False, (not has_r) and d < 0, [SPk, K("NUr")], [pkA])
                if has_r:
                    S.mm(psA[:, cs], NUr[:, 1, :], R[:, cs], False, d < 0, [Rk, K("NUr")], [pkA])
                if d >= 0:
                    S.mm(psA[:, cs], ident_b, NM[:, d, cs], False, True, [K("NM"), K("c128b")], [pkA])
                S.act(Wt[:, cs], psA[:, cs], AF.Exp, [pkA], [Wk_])
                if not m["last"]:
                    S.tt(R[:, cs], R[:, cs].bitcast(F32), SP[:, cs].bitcast(F32), ALU.add, [Rk, SPk], [Rk])

            def sb_s3(m):
                i = m["i"]
                pr, c2, h = m["pr"], m["c2"], m["h"]
                off = 128 * max(m["d"], 0)
                cs = slice(off, TW)
                Wt, Wk_ = Wb[i % 2], K("Wb", i % 2)
                psO, pkO = PS[4 + m["grp"] % 2], K("psO", m["grp"] % 2)
                S.mm(psO[:, cs], VB[:, m["b"], c2 * 128:(c2 + 1) * 128], Wt[:, cs], m["idx"] == 0, m["last"],
                     [K("VB", m["b"]), Wk_], [pkO])
                if m["last"]:
                    S.cp(YG[pr, c2, m["qsl"]], psO[pr, :], [pkO], [K("YG", c2, m["c"])], eng="act")

            n_it = len(items)
            for r in range(-2, n_it):
                if 0 <= r + 2 < n_it:
                    sb_s1(items[r + 2])
                if 0 <= r + 1 < n_it:
                    sb_s2(items[r + 1])
                if 0 <= r < n_it:
                    sb_s3(items[r])
            dump(f"yb{l}", YG[:], [K("YG", c, t) for c in range(2) for t in range(NTC)], [128, 2, S_])
            st.close()
            S.barrier()
            group_out(l, 1, YG, sty)
        S.barrier()
        if stop_after == f"B{l}":
            done = True
            break

        S.act_inorder.update((S.seg, S.seg + 1, S.seg + 2))
        with ExitStack() as sty, ExitStack() as st:
            YG = sb("YG", [128, 2, S_], stack=sty)
            XS = sb("XS", [128, 2, S_], stack=st)
            BTb = sb("BTb", [128, 2, S_], BF16, stack=st)
            CTb = sb("CTb", [128, 2, S_], BF16, stack=st)
            BTM = sb("BTM", [128, NT, 256], BF16, stack=st)
            XTM = sb("XTM", [128, NT, 256], BF16, stack=st)
            wdt = sb("wdt", [128, KC, 4], BF16, stack=st)
            stc = ExitStack()
            xw = [sb(f"xwc{j}", [128, 515], stack=stc) for j in range(2)]
            CV = [sb(f"CV{j}", [128, 512], stack=stc) for j in range(2)]
            BF = [sb(f"BF{j}", [128, 512], stack=stc) for j in range(2)]
            S.dma(wdt[:], w_in_d[l, :, 2304:2308].rearrange("(kc p) n -> p kc n", p=128), [], [K("wdt")], q="pool", arena=True)
            wv1, wk1, _ = load_w(w_in_d[l, :, 1280:1792], 8, 512)
            wv2, wk2, _ = load_w(w_in_d[l, :, 1792:2304], 8, 512)
            n_cv = 0
            for j in range(6):
                if j < 2:
                    wv, wk, c0 = wv1, wk1, 256 + j * 128
                elif j < 4:
                    wv, wk, c0 = wv2, wk2, (j - 2) * 128
                else:
                    wv, wk, c0 = wv2, wk2, 256 + (j - 4) * 128
                for tc in range(NTC):
                    sl = slice(tc * TW, (tc + 1) * TW)
                    ps, pk = ps_rot()
                    proj_fm(wv, wk, c0, tc, ps, pk)
                    cv, cvk = CV[n_cv % 2], K("CV", n_cv % 2)
                    conv_chunk(ps, pk, tc, xw, "xwc", lambda k: col(l, "ssm_cw", j * 4 + k), col(l, "ssm_cb", j),
                               cv[:], cvk)
                    if j < 2:
                        S.act(XS[:, j, sl], cv[:], AF.Silu, [cvk], [K("XS", j, tc)])
                        ps, pk = ps_rot()
                        for q in range(4):
                            S.tr(ps[:, q * 128:(q + 1) * 128], XS[:, j, tc * TW + q * 128: tc * TW + (q + 1) * 128],
                                 ident, [K("XS", j, tc), K("c128")], [pk])
                        S.cp(XTM[:, tc * 4:tc * 4 + 4, j * 128:(j + 1) * 128],
                             ps[:].rearrange("p (a b) -> p a b", a=4), [pk], [K("XTM", tc, j)])
                    elif j < 4:
                        g = j - 2
                        bf, bfk = BF[n_cv % 2], K("BF", n_cv % 2)
                        S.act(bf[:], cv[:], AF.Silu, [cvk], [bfk])
                        S.cp(BTb[:, g, sl], bf[:], [bfk], [K("BTb", g, tc)])
                        ps, pk = ps_rot()
                        for q in range(4):
                            S.tr(ps[:, q * 128:(q + 1) * 128], bf[:, q * 128:(q + 1) * 128], ident,
                                 [bfk, K("c128")], [pk])
                        S.cp(BTM[:, tc * 4:tc * 4 + 4, g * 128:(g + 1) * 128],
                             ps[:].rearrange("p (a b) -> p a b", a=4), [pk], [K("BTM", tc, g)])
                    else:
                        S.act(CTb[:, j - 4, sl], cv[:], AF.Silu, [cvk], [K("CTb", j - 4, tc)])
                    n_cv += 1
            stc.close()
            S.barrier()
            DT = sb("DT", [128, 64], stack=st)
            AN = sb("AN", [128, 64], stack=st)
            AC = sb("AC", [128, 64], stack=st)
            ACS = sb("ACS", [128, 64], stack=st)
            TOT = sb("TOT", [128, 64], stack=st)
            DEC = sb("DEC", [128, 64], stack=st)
            ETOT = sb("ETOT", [128, 64], stack=st)
            DTD = sb("DTD", [128, 64], stack=st)
            psd, pkd = ps_rot()
            for i in range(NT):
                for kc in range(KC):
                    S.mm(psd[:, i * 4:(i + 1) * 4], hT[:, kc, i * 128:(i + 1) * 128], wdt[:, kc, :], kc == 0,
                         kc == KC - 1, [K("wdt"), K("hT", kc, i // 4)], [pkd])
            S.tt(DT[:], psd[:, 0:64], row(l, "dt_bias"), ALU.add, [pkd, K("rowp")], [K("DT")])
            S.act(DT[:], DT[:], AF.Exp, [K("DT")], [K("DT")])
            S.act(DT[:], DT[:], AF.Ln, [K("DT"), K("cst")], [K("DT")], bias=cst[:, 1:2])
            S.act(AN[:], row(l, "a_log"), AF.Exp, [K("rowp")], [K("AN")])
            S.ts(AN[:], AN[:], -1.0, None, ALU.mult, None, [K("AN")], [K("AN")])
            S.tt(AC[:], DT[:], AN[:], ALU.mult, [K("DT"), K("AN")], [K("AC")])
            ps, pk = ps_rot()
            S.mm(ps[:, 0:64], tri_le, AC[:], True, True, [K("AC"), K("c128")], [pk])
            S.cp(ACS[:], ps[:, 0:64], [pk], [K("ACS")])
            ps, pk = ps_rot()
            S.mm(ps[:, 0:64], ones_f, AC[:], True, True, [K("AC"), K("c128")], [pk])
            S.cp(TOT[:], ps[:, 0:64], [pk], [K("TOT")])
            S.tt(DEC[:], TOT[:], ACS[:], ALU.subtract, [K("TOT"), K("ACS")], [K("DEC")])
            S.act(DEC[:], DEC[:], AF.Exp, [K("DEC")], [K("DEC")])
            S.act(ETOT[:], TOT[:], AF.Exp, [K("TOT")], [K("ETOT")])
            S.tt(DTD[:], DT[:], DEC[:], ALU.mult, [K("DT"), K("DEC")], [K("DTD")])
            ST = sb("ST", [128, 4, 64], stack=st)
            S.add("dve", lambda e: e.memset(ST[:], 0.0), [], [K("ST", h) for h in range(4)])
            Rm = [sb(f"Rm{j}", [128, 128], stack=st) for j in range(2)]
            ARG = [sb(f"ARG{j}", [128, 128], stack=st) for j in range(2)]
            E1 = [sb(f"E1{j}", [128, 128], stack=st) for j in range(2)]
            GL = [sb(f"GL{j}", [128, 128], BF16, stack=st) for j in range(2)]
            CTp = [sb(f"CTp{j}", [128, 128], BF16, stack=st) for j in range(2)]
            XCp = [[sb(f"XCp{a}{j}", [128, 128], BF16, stack=st) for j in range(2)] for a in range(2)]
            STp = sb("STp", [128, 4, 128], BF16, stack=st)
            for a in range(2):
                for j in range(2):
                    S.add("dve", (lambda e, t_=XCp[a][j]: e.memset(t_[:], 0.0)), [], [K("XCp", a, j)])
            S.add("dve", lambda e: e.memset(STp[:], 0.0), [], [K("STp", h) for h in range(4)])
            XDh = [sb(f"XDh{j}", [128, 64], BF16, stack=st) for j in range(2)]
            n = 0
            for ci in range(NT):
                csl = slice(ci * 128, (ci + 1) * 128)
                tcx = ci // 4
                for g in range(2):
                    psG, pkG = ps_rot()
                    S.mm(psG[:, 0:128], BTb[:, g, csl], CTb[:, g, csl], True, True,
                         [K("BTb", g, tcx), K("CTb", g, tcx)], [pkG])
                    psY, pkY = PS[4 + (ci * 2 + g) % 2], K("psO", (ci * 2 + g) % 2)
                    for hh in range(2):
                        h = 2 * g + hh
                        pr = slice(hh * 64, hh * 64 + 64)
                        cidx = ci * 4 + h
                        k2 = n % 2
                        S.ts(Rm[k2][:], tri_le, AC[:, cidx:cidx + 1], None, ALU.mult, None,
                             [K("AC"), K("c128")], [K("Rm", k2)])
                        ps1, pk1 = ps_rot()
                        S.mm(ps1[:, 0:128], ones_f, Rm[k2][:], True, True, [K("Rm", k2), K("c128")], [pk1])
                        S.stt(ARG[k2][:], ps1[:, 0:128], ACS[:, cidx:cidx + 1], ssdneg, ALU.subtract, ALU.add,
                              [pk1, K("ACS"), K("c128")], [K("ARG", k2)])
                        S.act(ARG[k2][:], ARG[k2][:], AF.Exp, [K("ARG", k2)], [K("ARG", k2)])
                        S.tt(GL[k2][:], ARG[k2][:], psG[:, 0:128], ALU.mult, [K("ARG", k2), pkG], [K("GL", k2)])
                        S.act(E1[k2][:], ps1[:, 0:128], AF.Exp, [pk1], [K("E1", k2)])
                        S.tt(CTp[k2][:], CTb[:, g, csl], E1[k2][:], ALU.mult, [K("CTb", g, tcx), K("E1", k2)],
                             [K("CTp", k2)])
                        rot = (ci * 2 + g) % 2
                        xcp = XCp[hh][rot]
                        S.ts(xcp[:, hh * 64:(hh + 1) * 64], XTM[:, ci, h * 64:(h + 1) * 64], DT[:, cidx:cidx + 1], None,
                             ALU.mult, None, [K("XTM", tcx, g), K("DT")], [K("XCp", hh, rot)])
                        S.mm(psY[:, 0:128], xcp[:], GL[k2][:], hh == 0, False, [K("XCp", hh, rot), K("GL", k2)], [pkY])
                        S.mm(psY[:, 0:128], STp[:, h, :], CTp[k2][:], False, hh == 1, [K("STp", h), K("CTp", k2)], [pkY])
                        S.ts(XDh[k2][:], XTM[:, ci, h * 64:(h + 1) * 64], DTD[:, cidx:cidx + 1], None, ALU.mult, None,
                             [K("XTM", tcx, g), K("DTD")], [K("XDh", k2)])
                        psS, pkS = ps_rot()
                        S.mm(psS[:, 0:64], BTM[:, ci, g * 128:(g + 1) * 128], XDh[k2][:], True, True,
                             [K("BTM", tcx, g), K("XDh", k2)], [pkS])
                        S.stt(ST[:, h, :], ST[:, h, :], ETOT[:, cidx:cidx + 1], psS[:, 0:64], ALU.mult, ALU.add,
                              [K("ST", h), K("ETOT"), pkS], [K("ST", h)])
                        S.cp(STp[:, h, hh * 64:(hh + 1) * 64], ST[:, h, :], [K("ST", h)], [K("STp", h)], eng="act")
                        n += 1
                    S.cp(YG[:, g, csl], psY[:, 0:128], [pkY], [K("YG", g, tcx)], eng="act")
            ARG = [sb(f"ZT{j}", [128, 512], stack=st) for j in range(1)]
            for j in range(2):
                YGk = [K("YG", j, t) for t in range(NTC)]
                S.stt(YG[:, j, :], XS[:, j, :], col(l, "ssm_d", j), YG[:, j, :], ALU.mult, ALU.add,
                      [K("XS", j, t) for t in range(NTC)] + YGk + [K("colp")], YGk)
                for tc in range(NTC):
                    sl = slice(tc * TW, (tc + 1) * TW)
                    ps, pk = ps_rot()
                    proj_fm(wv1, wk1, j * 128, tc, ps, pk)
                    zt, ztk = ARG[0], K("ARGz", 0)
                    S.act(zt[:], ps[:, :], AF.Silu, [pk], [ztk])
                    S.tt(YG[:, j, sl], YG[:, j, sl], zt[:], ALU.mult, [K("YG", j, tc), ztk], [K("YG", j, tc)])
            dump(f"yc{l}", YG[:], [K("YG", c, t) for c in range(2) for t in range(NTC)], [128, 2, S_])
            st.close()
            S.barrier()
            group_out(l, 2, YG, sty)
        S.barrier()
        if stop_after == f"C{l}":
            done = True
            break

        with ExitStack() as sty, ExitStack() as st:
            YG = sb("YG", [128, 2, S_], stack=sty)
            QA = sb("QA", [128, 4, S_], BF16, stack=st)
            KA = sb("KA", [128, 4, S_], BF16, stack=st)
            allqa = [K("QA", h_, t_) for h_ in range(4) for t_ in range(NTC)]
            allka = [K("KA", h_, t_) for h_ in range(4) for t_ in range(NTC)]
            S.add("dve", lambda e: e.memset(QA[:], 0.0), [], allqa, cost=4.5)
            S.add("dve", lambda e: e.memset(KA[:], 0.0), [], allka, cost=4.5)
            for h_ in range(4):
                o_ = 64 if h_ % 2 == 0 else 0
                S.dma(KA[o_:o_ + 8, h_, :], blkoh_d[:, :], allka, [K("KAoh", h_)], q="pool", arena=True)
            VB = sb("VB", [128, NT, 256], BF16, stack=st)
            CAUS = sb("CAUS", [128, 4, 512], BF16, stack=st)
            S.dma(CAUS[:], cmask_d[:, 4096:6144].rearrange("p (a b) -> p a b", a=4), [], [K("CAUS")], q="pool", arena=True)
            GC = sb("GC", [128, 3, 128], stack=st)
            S.dma(GC[:], gate_c_d[:, :].rearrange("p (a b) -> p a b", a=3), [], [K("GC")])
            KM = sb("KM", [128, 2, 8], stack=st)
            KMb = sb("KMb", [128, 2, 8], BF16, stack=st)
            wv, wk, _ = load_w(w_in_d[l, :, 2308:2820], 8, 512)
            wv2, wk2, _ = load_w(w_in_d[l, :, 2820:3076], 8, 256)
            str_ = ExitStack()
            ROPEb = [sb(f"ROPE{j}", [128, 2, TW], stack=str_) for j in range(1)]
            RAW = [sb(f"RAW{j}", [128, 512], stack=str_) for j in range(2)]
            TMP = [sb(f"TMPr{j}", [128, 512], stack=str_) for j in range(2)]
            KF = [sb(f"KF{j}", [128, 512], stack=str_) for j in range(2)]
            nr = 0
            for c2 in range(2):
                for tc in range(NTC):
                    sl = slice(tc * TW, (tc + 1) * TW)
                    rj = 0
                    ROPE = ROPEb[rj]
                    rsl = slice(0, TW)
                    S.dma(ROPE[:, 0, :], rope_d[:, tc * TW:(tc + 1) * TW], [], [K("ROPE", rj, 0)])
                    S.dma(ROPE[:, 1, :], rope_d[:, S_ + tc * TW:S_ + (tc + 1) * TW], [], [K("ROPE", rj, 1)])
                    for which in range(2):
                        ps, pk = ps_rot()
                        proj_fm(wv, wk, which * 256 + c2 * 128, tc, ps, pk)
                        raw, rawk = RAW[nr % 2], K("RAW", nr % 2)
                        tmp, tmpk = TMP[nr % 2], K("TMPr", nr % 2)
                        S.cp(raw[:], ps[:, :], [pk], [rawk], eng="act")
                        psw, pkw = ps_rot()
                        S.mm(psw[:, :], perm_f, raw[:], True, True, [rawk, K("c128")], [pkw])
                        S.tt(tmp[:], psw[:, :], ROPE[:, 1, rsl], ALU.mult, [pkw, K("ROPE", rj, 1)], [tmpk])
                        kf = KF[nr % 2]
                        dst, dk = kf[:], K("KF", nr % 2)
                        S.tt(dst, raw[:], ROPE[:, 0, rsl], ALU.mult, [rawk, K("ROPE", rj, 0)], [dk])
                        S.tt(dst, dst, tmp[:], ALU.add, [dk, tmpk], [dk])
                        if which == 0:
                            S.act(QA[0:64, 2 * c2, sl], kf[0:64, :], AF.Copy, [dk], [K("QA", 2 * c2, tc)], scale=0.125)
                            S.act(QA[64:128, 2 * c2 + 1, sl], kf[64:128, :], AF.Copy, [dk], [K("QA", 2 * c2 + 1, tc)],
                                  scale=0.125)
                        else:
                            S.cp(KA[0:64, 2 * c2, sl], kf[0:64, :], [dk], [K("KA", 2 * c2, tc)], eng="act")
                            S.cp(KA[64:128, 2 * c2 + 1, sl], kf[64:128, :], [dk], [K("KA", 2 * c2 + 1, tc)], eng="act")
                            for hb in range(2):
                                nblk = tc * 2 + hb
                                S.add("dve", (lambda e, o=KM[:, c2, nblk:nblk + 1], i_=kf[:, hb * 256:(hb + 1) * 256]:
                                              e.reduce_sum(out=o, in_=i_, axis=AX.X)), [dk], [K("KM", c2, nblk)])
                        nr += 1
            for i in range(NT):
                ps, pk = ps_rot()
                for kc in range(KC):
                    S.mm(ps[:, 0:256], hT[:, kc, i * 128:(i + 1) * 128], wv2[:, kc, :], kc == 0, kc == KC - 1,
                         wk2 + [K("hT", kc, i // 4)], [pk])
                S.cp(VB[:, i, :], ps[:, 0:256], [pk], [K("VB", i)], eng=("dve" if i % 2 else "act"))
            S.cp(KMb[:], KM[:], [K("KM", c2_, nb_) for c2_ in range(2) for nb_ in range(8)], [K("KMb")])
            str_.close()
            S.barrier()
            PTb = [sb(f"PTb{j}", [128, 512], BF16, stack=st) for j in range(3)]
            RD = [sb(f"RD{j}", [128, 512], stack=st) for j in range(2)]
            GT = sb("GT", [128, 128], stack=st)
            BT_ = sb("BIASTM", [128, 128], stack=st)
            MX = [sb(f"MX{j}", [128, 8], stack=st) for j in range(2)]
            for h in range(4):
                hh, c2 = h % 2, h // 2
                pr = slice(hh * 64, hh * 64 + 64)
                o_ = 64 if hh == 0 else 0
                psg, pkg = ps_rot()
                for i in range(NT):
                    S.mm(psg[:, i * 8:(i + 1) * 8], QA[pr, h, i * 128:(i + 1) * 128], KMb[pr, c2, :], True, True,
                         [K("QA", h, i // 4), K("KMb")], [pkg])
                S.stt(GT[:], psg[:, 0:128], 8.0 / 256.0, GC[:, 0, :], ALU.mult, ALU.add, [pkg, K("GC")], [K("GT")])
                for i in range(NT):
                    mx, mxk = MX[i % 2], K("MX", i % 2)
                    S.add("dve", (lambda e, o=mx[:], i_=GT[:, i * 8:(i + 1) * 8]: e.max(out=o, in_=i_)),
                          [K("GT")], [mxk])
                    bsl = BT_[:, i * 8:(i + 1) * 8]
                    S.ts(bsl, GT[:, i * 8:(i + 1) * 8], mx[:, 2:3], None, ALU.is_ge, None, [K("GT"), mxk],
                         [K("BIASTM", i)])
                    S.tt(bsl, bsl, GC[:, 1, i * 8:(i + 1) * 8], ALU.mult, [K("BIASTM", i), K("GC")], [K("BIASTM", i)])
                    S.tt(bsl, bsl, GC[:, 2, i * 8:(i + 1) * 8], ALU.add, [K("BIASTM", i), K("GC")], [K("BIASTM", i)])
                    S.ts(bsl, bsl, -1.0, -NEG, ALU.add, ALU.mult, [K("BIASTM", i)], [K("BIASTM", i)])
                for q4 in range(4):
                    pst, pkt = ps_rot()
                    for q in range(4):
                        i = q4 * 4 + q
                        S.tr(pst[0:8, q * 128:(q + 1) * 128], BT_[:, i * 8:(i + 1) * 8], ident,
                             [K("BIASTM", i), K("c128")], [pkt])
                    S.cp(QA[o_:o_ + 8, h, q4 * 512:(q4 + 1) * 512], pst[0:8, :], [pkt], [K("QAsel", h, q4)])
            its = []
            grp = 0
            for h in range(4):
                for c in range(NTC):
                    nkt = 4 * c + 4
                    for kt in range(nkt):
                        its.append(dict(h=h, c=c, kt=kt, nkt=nkt, grp=grp, i=len(its)))
                    grp += 1

            def m_s1(m):
                h, c, kt = m["h"], m["c"], m["kt"]
                d = kt - 4 * c
                qsl = slice(c * TW, (c + 1) * TW)
                ksl = slice(kt * 128, (kt + 1) * 128)
                off = 128 * max(d, 0)
                cs = slice(off, TW)
                qs = slice(c * TW + off, (c + 1) * TW)
                psS, pkS = ps_rot()
                S.mm(psS[:, cs], KA[:, h, ksl], QA[:, h, qs], True, d < 0,
                     [K("KA", h, kt // 4), K("KAoh", h), K("QA", h, c), K("QAsel", h, c)], [pkS])
                if d >= 0:
                    S.mm(psS[:, cs], ident_b, CAUS[:, d, cs], False, True, [K("CAUS"), K("c128b")], [pkS])
                pt, ptk = PTb[m["i"] % 3], K("PTb", m["i"] % 3)
                S.act(pt[:, cs], psS[:, cs], AF.Exp, [pkS], [ptk])

            def m_s2(m):
                h, c, kt, nkt = m["h"], m["c"], m["kt"], m["nkt"]
                hh, c2 = h % 2, h // 2
                pr = slice(hh * 64, hh * 64 + 64)
                qsl = slice(c * TW, (c + 1) * TW)
                g2 = m["grp"] % 2
                psO, pkO = PS[4 + g2], K("psO", g2)
                psD, pkD = PS[6 + g2], K("psD", g2)
                pt, ptk = PTb[m["i"] % 3], K("PTb", m["i"] % 3)
                off = 128 * max(kt - 4 * c, 0)
                cs = slice(off, TW)
                S.mm(psO[:, cs], VB[:, kt, c2 * 128:(c2 + 1) * 128], pt[:, cs], kt == 0, kt == nkt - 1,
                     [K("VB", kt), ptk], [pkO])
                S.mm(psD[:, cs], ones_b, pt[:, cs], kt == 0, kt == nkt - 1, [ptk, K("c128b")], [pkD])
                if kt == nkt - 1:
                    rd, rdk = RD[g2], K("RD", g2)
                    S.add("dve", (lambda e, o=rd[pr, :], i_=psD[pr, :]: e.reciprocal(out=o, in_=i_)), [pkD], [rdk], cost=2.2)
                    S.tt(YG[pr, c2, qsl], psO[pr, :], rd[pr, :], ALU.mult, [pkO, rdk], [K("YG", c2, c)])

            n_ = len(its)
            for r in range(-1, n_):
                if r + 1 < n_:
                    m_s1(its[r + 1])
                if r >= 0:
                    m_s2(its[r])
            dump(f"yd{l}", YG[:], [K("YG", c, t) for c in range(2) for t in range(NTC)], [128, 2, S_])
            st.close()
            S.barrier()
            group_out(l, 3, YG, sty)
        S.barrier()
        dump(f"x1_{l}", xT[:], ALLX, [128, KC, S_])
        if stop_after == f"D{l}":
            done = True
            break

        with ExitStack() as st:
            with ExitStack() as st2:
                rmsnorm_fm(xT, "xT", lambda kc: col(l, "xat_g", kc), hT, "hT", KC, S_, st2, tag="x")
            S.barrier()
            memT = sb("memT", [128, KC, 256], stack=st)
            MTb = sb("MTb", [128, KC, 256], BF16, stack=st)
            with ExitStack() as st2:
                load_fm(memT, mem_d, 2, "memT", st2)
                rmsnorm_fm(memT, "memT", lambda kc: col(l, "mem_g", kc), MTb, "MTb", KC, 256, st2, tag="m")
            S.barrier()
            KTb = sb("KTb", [128, KC, 256], BF16, stack=st)
            VX = sb("VX", [128, 2, D_], BF16, stack=st)
            QX = sb("QX", [128, KC, S_], BF16, stack=st)
            MTk = [K("MTb", kc, 0) for kc in range(KC)]
            for half in range(2):
                wv, wk, _ = load_w(wkv_d[l, :, half * 512:(half + 1) * 512], 8, 512)
                for o4 in range(4):
                    oc = half * 4 + o4
                    ps, pk = ps_rot()
                    for kc in range(KC):
                        S.mm(ps[:, 0:256], wv[:, kc, o4 * 128:(o4 + 1) * 128], MTb[:, kc, :], kc == 0, kc == KC - 1,
                             wk + [K("MTb", kc, 0)], [pk])
                    S.cp(KTb[:, oc, :], ps[:, 0:256], [pk], [K("KTb", oc)])
            for half in range(2):
                wv, wk, _ = load_w(wkv_d[l, :, 1024 + half * 512:1024 + (half + 1) * 512], 8, 512)
                for mt in range(2):
                    ps, pk = ps_rot()
                    for kc in range(KC):
                        S.mm(ps[:, :], MTb[:, kc, mt * 128:(mt + 1) * 128], wv[:, kc, :], kc == 0, kc == KC - 1,
                             wk + [K("MTb", kc, 0)], [pk])
                    S.cp(VX[:, mt, half * 512:(half + 1) * 512], ps[:, :], [pk], [K("VX", mt, half)], eng="act")
            for half in range(2):
                wv, wk, _ = load_w(wq_d[l, :, half * 512:(half + 1) * 512], 8, 512)
                for o4 in range(4):
                    oc = half * 4 + o4
                    for tc in range(NTC):
                        ps, pk = ps_rot()
                        proj_fm(wv, wk, o4 * 128, tc, ps, pk)
                        S.act(QX[:, oc, tc * TW:(tc + 1) * TW], ps[:, :], AF.Copy, [pk], [K("QX", oc, tc)],
                              scale=1.0 / 16.0)
            dump(f"X_MTb{l}", MTb[:], MTk, [128, KC, 256])
            dump(f"X_KTb{l}", KTb[:], [K("KTb", oc) for oc in range(KC)], [128, KC, 256])
            dump(f"X_QX{l}", QX[:], [K("QX", oc, tc) for oc in range(KC) for tc in range(NTC)], [128, KC, S_])
            dump(f"X_VX{l}", VX[:], [K("VX", mt, hf) for mt in range(2) for hf in range(2)], [128, 2, D_])
            PT = [sb(f"PTx{j}", [128, 512], BF16, stack=st) for j in range(4)]
            RDx = [sb(f"RDx{j}", [128, 512], stack=st) for j in range(2)]
            it = 0
            for hd in range(4):
                for tc in range(NTC):
                    sl = slice(tc * TW, (tc + 1) * TW)
                    pts = []
                    for mt in range(2):
                        ps, pk = ps_rot()
                        for j in range(2):
                            S.mm(ps[:, :], KTb[:, 2 * hd + j, mt * 128:(mt + 1) * 128], QX[:, 2 * hd + j, sl],
                                 j == 0, j == 1, [K("KTb", 2 * hd + j), K("QX", 2 * hd + j, tc)], [pk])
                        pt, ptk = PT[it % 4], K("PTx", it % 4)
                        S.act(pt[:], ps[:, :], AF.Exp, [pk], [ptk])
                        pts.append((pt, ptk))
                        it += 1
                    psD, pkD = ps_rot()
                    for mt in range(2):
                        S.mm(psD[:, :], ones_b, pts[mt][0][:], mt == 0, mt == 1, [pts[mt][1], K("c128b")], [pkD])
                    rd, rdk = RDx[(hd * NTC + tc) % 2], K("RDx", (hd * NTC + tc) % 2)
                    S.add("dve", (lambda e, o=rd[:], i_=psD[:, :]: e.reciprocal(out=o, in_=i_)), [pkD], [rdk], cost=2.2)
                    for j in range(2):
                        oc = 2 * hd + j
                        ps, pk = ps_rot()
                        for mt in range(2):
                            S.mm(ps[:, :], VX[:, mt, oc * 128:(oc + 1) * 128], pts[mt][0][:], mt == 0, mt == 1,
                                 [K("VX", mt, oc // 4), pts[mt][1]], [pk])
                        S.tt(hT[:, oc, sl], ps[:, :], rd[:], ALU.mult, [pk, rdk], [K("hT", oc, tc)])
            dump(f"X_OX{l}", hT[:], ALLH, [128, KC, S_])
            for half in range(2):
                wv, wk, _ = load_w(wo_d[l, :, half * 512:(half + 1) * 512], 8, 512)
                for o4 in range(4):
                    oc = half * 4 + o4
                    for tc in range(NTC):
                        ps, pk = ps_rot()
                        proj_fm(wv, wk, o4 * 128, tc, ps, pk)
                        S.tt(xT[:, oc, tc * TW:(tc + 1) * TW], xT[:, oc, tc * TW:(tc + 1) * TW], ps[:, :], ALU.add,
                             [K("xT", oc, tc), pk], [K("xT", oc, tc)])
        S.barrier()
        dump(f"x2_{l}", xT[:], ALLX, [128, KC, S_])
        if stop_after == f"X{l}":
            done = True
            break

        with ExitStack() as st:
            ENp = sb("ENp", [128, 16, 128], BF16, stack=st)
            S.add("dve", lambda e: e.memset(ENp[:], 0.0), [], [K("ENpz")])
            S.dma(ENp[0:16, :, :], en_d[:, :].rearrange("p (a b) -> p a b", a=16), [K("ENpz")], [K("ENf")], q="pool",
                  arena=True)
            CTh = sb("CTh", [128, S_], BF16, stack=st)
            CTl = sb("CTl", [128, S_], BF16, stack=st)
            S.add("dve", lambda e: e.memset(CTh[:], 0.0), [], [K("CThz")])
            S.add("dve", lambda e: e.memset(CTl[:], 0.0), [], [K("CTlz")])
            st_rt = ExitStack()
            WR = sb("WR", [128, KC, 20], stack=st_rt)
            S.dma(WR[:], wrt_d[l].rearrange("(kc p) n -> p kc n", p=128), [], [K("WR")])
            LG = sb("LG", [128, NT, 20], stack=st_rt)
            st_hf = ExitStack()
            HF = sb("HF", [128, KC, TW], stack=st_hf)

            def router_hook(tc, r):
                sl = slice(tc * TW, (tc + 1) * TW)
                for kc in range(KC):
                    S.stt(HF[:, kc, :], xT[:, kc, sl], col(l, "ffn_g", kc), r[:], ALU.mult, ALU.mult,
                          [K("xT", kc, tc), K("rsf", tc % 2), K("colp")], [K("HF", kc)])
                ps, pk = ps_rot()
                for q in range(4):
                    for kc in range(KC):
                        S.mm(ps[:, q * 20:(q + 1) * 20], HF[:, kc, q * 128:(q + 1) * 128], WR[:, kc, :], kc == 0,
                             kc == KC - 1, [K("HF", kc), K("WR")], [pk])
                S.cp(LG[:, tc * 4:tc * 4 + 4, :], ps[:, 0:80].rearrange("p (a b) -> p a b", a=4), [pk],
                     [K("LG", tc)])

            with ExitStack() as st2:
                rmsnorm_fm(xT, "xT", lambda kc: col(l, "ffn_g", kc), hT, "hT", KC, S_, st2, hook=router_hook, tag="f")
            st_hf.close()
            S.barrier()
            COMB = sb("COMB", [128, NT, 16], stack=st_rt)
            CT = sb("CT", [16, S_], stack=st_rt)
            sm = {nm: sb("rt_" + nm, [128, NT, w_] if w_ > 1 else [128, NT], stack=st_rt) for nm, w_ in
                  [("lg", 4), ("m", 1), ("eg", 4), ("sg", 1), ("oh", 4), ("le", 16), ("sel", 4), ("tmp", 4),
                   ("a", 1), ("eq", 4), ("sel2", 4), ("b", 1), ("t2", 4), ("ex", 4), ("dd", 1), ("wg", 4)]}

            def v(nm):
                return sm[nm][:]

            def kk(nm):
                return [K("rt", nm)]

            def B3(ap2, w_=4):
                return ap2.unsqueeze(2).to_broadcast([128, NT, w_])

            LGk = [K("LG", t) for t in range(NTC)]
            bgB = row(l, "bg").unsqueeze(1).to_broadcast([128, NT, 4])
            beB = row(l, "be").unsqueeze(1).to_broadcast([128, NT, 16])
            S.tt(v("lg"), LG[:, :, 0:4], bgB, ALU.add, LGk + [K("rowp")], kk("lg"))
            S.add("dve", lambda e: e.reduce_max(out=v("m"), in_=v("lg"), axis=AX.X), kk("lg"), kk("m"))
            S.tt(v("lg"), v("lg"), B3(v("m")), ALU.subtract, kk("lg") + kk("m"), kk("lg"))
            S.act(v("eg"), v("lg"), AF.Exp, kk("lg"), kk("eg"))
            S.add("dve", lambda e: e.reduce_sum(out=v("sg"), in_=v("eg"), axis=AX.X), kk("eg"), kk("sg"))
            S.add("dve", lambda e: e.reciprocal(out=v("sg"), in_=v("sg")), kk("sg"), kk("sg"))
            S.ts(v("oh"), v("lg"), 0.0, None, ALU.is_ge, None, kk("lg"), kk("oh"))
            S.tt(v("le"), LG[:, :, 4:20], beB, ALU.add, LGk + [K("rowp")], kk("le"))
            for g in range(4):
                ohg = sm["oh"][:, :, g:g + 1].to_broadcast([128, NT, 4])
                if g == 0:
                    S.tt(v("sel"), sm["le"][:, :, 0:4], ohg, ALU.mult, kk("le") + kk("oh"), kk("sel"))
                else:
                    S.tt(v("tmp"), sm["le"][:, :, 4 * g:4 * g + 4], ohg, ALU.mult, kk("le") + kk("oh"), kk("tmp"))
                    S.tt(v("sel"), v("sel"), v("tmp"), ALU.add, kk("sel") + kk("tmp"), kk("sel"))
            S.add("dve", lambda e: e.reduce_max(out=v("a"), in_=v("sel"), axis=AX.X), kk("sel"), kk("a"))
            S.tt(v("sel"), v("sel"), B3(v("a")), ALU.subtract, kk("sel") + kk("a"), kk("sel"))
            S.ts(v("eq"), v("sel"), 0.0, None, ALU.is_ge, None, kk("sel"), kk("eq"))
            S.stt(v("sel2"), v("eq"), -1e30, v("sel"), ALU.mult, ALU.add, kk("eq") + kk("sel"), kk("sel2"))
            S.add("dve", lambda e: e.reduce_max(out=v("b"), in_=v("sel2"), axis=AX.X), kk("sel2"), kk("b"))
            S.tt(v("t2"), v("sel"), B3(v("b")), ALU.is_ge, kk("sel") + kk("b"), kk("t2"))
            S.act(v("ex"), v("sel"), AF.Exp, kk("sel"), kk("ex"))
            S.act(v("dd"), v("b"), AF.Exp, kk("b"), kk("dd"))
            S.ts(v("dd"), v("dd"), 1.0, None, ALU.add, None, kk("dd"), kk("dd"))
            S.add("dve", lambda e: e.reciprocal(out=v("dd"), in_=v("dd")), kk("dd"), kk("dd"))
            S.tt(v("wg"), v("ex"), v("t2"), ALU.mult, kk("ex") + kk("t2"), kk("wg"))
            S.tt(v("wg"), v("wg"), B3(v("dd")), ALU.mult, kk("wg") + kk("dd"), kk("wg"))
            S.tt(v("wg"), v("wg"), B3(v("sg")), ALU.mult, kk("wg") + kk("sg"), kk("wg"))
            for g in range(4):
                ohg = sm["oh"][:, :, g:g + 1].to_broadcast([128, NT, 4])
                S.tt(COMB[:, :, 4 * g:4 * g + 4], v("wg"), ohg, ALU.mult, kk("wg") + kk("oh"),
                     [K("COMB", i, g) for i in range(NT)])
            for q4 in range(4):
                pst, pkt = ps_rot()
                for q in range(4):
                    i = q4 * 4 + q
                    S.tr(pst[0:16, q * 128:(q + 1) * 128], COMB[:, i, :], ident,
                         [K("COMB", i, g) for g in range(4)] + [K("c128")], [pkt])
                S.cp(CT[0:16, q4 * 512:(q4 + 1) * 512], pst[0:16, :], [pkt], [K("CT", q4)])
                S.cp(CTh[0:16, q4 * 512:(q4 + 1) * 512], CT[0:16, q4 * 512:(q4 + 1) * 512], [K("CT", q4), K("CThz")],
                     [K("CTh", q4)])
                S.tt(CT[0:16, q4 * 512:(q4 + 1) * 512], CT[0:16, q4 * 512:(q4 + 1) * 512],
                     CTh[0:16, q4 * 512:(q4 + 1) * 512], ALU.subtract, [K("CT", q4), K("CTh", q4)], [K("CT", q4)])
                S.cp(CTl[0:16, q4 * 512:(q4 + 1) * 512], CT[0:16, q4 * 512:(q4 + 1) * 512], [K("CT", q4), K("CTlz")],
                     [K("CTl", q4)])
            st_rt.close()
            S.barrier()
            HID = sb("HID", [128, 8, S_], BF16, stack=st)
            W2S = sb("W2S", [128, 8, D_], BF16, stack=st)
            CB = [sb(f"CB{j}", [128, 512], stack=st) for j in range(2)]
            SL = [sb(f"SL{j}", [128, 512], stack=st) for j in range(2)]
            n = 0
            for g in range(4):
                for el in range(4):
                    ex = 4 * g + el
                    wv, wk1_, slot = load_w(w1_d[l, ex], 8, 256, c0=0, tot=512)
                    _, wk3_, _ = load_w(w3_d[l, ex], 8, 256, slot=slot, c0=256, tot=512)
                    wk = wk1_ + wk3_
                    for tc in range(NTC):
                        sl = slice(tc * TW, (tc + 1) * TW)
                        psC, pkC = ps_rot()
                        S.mm(psC[:, :], ENp[:, ex, :], CTh[:, sl], True, False, [K("ENf"), K("CTh", tc), K("CThz")], [pkC])
                        S.mm(psC[:, :], ENp[:, ex, :], CTl[:, sl], False, True, [K("ENf"), K("CTl", tc), K("CTlz")], [pkC])
                        cb, cbk = CB[n % 2], K("CB", n % 2)
                        S.cp(cb[:], psC[:, :], [pkC], [cbk], eng="act")
                        for fc in range(2):
                            ps1, pk1 = ps_rot()
                            proj_fm(wv, wk, fc * 128, tc, ps1, pk1)
                            ps3, pk3 = ps_rot()
                            proj_fm(wv, wk, 256 + fc * 128, tc, ps3, pk3)
                            s_, sk = SL[(2 * n + fc) % 2], K("SL", (2 * n + fc) % 2)
                            S.act(s_[:], ps1[:, :], AF.Silu, [pk1], [sk])
                            S.tt(s_[:], s_[:], ps3[:, :], ALU.mult, [sk, pk3], [sk])
                            S.tt(HID[:, el * 2 + fc, sl], s_[:], cb[:], ALU.mult, [sk, cbk], [K("HID", el * 2 + fc, tc)])
                        n += 1
                    S.dma(W2S[:, 2 * el:2 * el + 2, :], w2_d[l, ex].rearrange("(fc p) n -> p fc n", p=128), [],
                          [K("W2S", el)], q="pool", arena=True)
                for oc in range(KC):
                    for tc in range(NTC):
                        ps, pk = ps_rot()
                        for j in range(8):
                            S.mm(ps[:, :], W2S[:, j, oc * 128:(oc + 1) * 128], HID[:, j, tc * TW:(tc + 1) * TW],
                                 j == 0, j == 7, [K("W2S", j // 2), K("HID", j, tc)], [pk])
                        S.tt(xT[:, oc, tc * TW:(tc + 1) * TW], xT[:, oc, tc * TW:(tc + 1) * TW], ps[:, :], ALU.add,
                             [K("xT", oc, tc), pk], [K("xT", oc, tc)])
        S.barrier()
        dump(f"x3_{l}", xT[:], ALLX, [128, KC, S_])
        if stop_after == f"M{l}":
            done = True
            break

    if stop_after is None:
        with ExitStack() as st:
            HF = sb("HFo", [128, KC, TW], stack=st)
            OB = [sb(f"OB{j}", [128, D_], stack=st) for j in range(2)]
            nob = [0]

            def final_hook(tc, r):
                sl = slice(tc * TW, (tc + 1) * TW)
                for kc in range(KC):
                    S.stt(HF[:, kc, :], xT[:, kc, sl], col(0, "fin_g", kc), r[:], ALU.mult, ALU.mult,
                          [K("xT", kc, tc), K("rsz", tc % 2), K("colp")], [K("HFo", kc)])
                for q in range(4):
                    ob, obk = OB[nob[0] % 2], K("OB", nob[0] % 2)
                    nob[0] += 1
                    for half in range(2):
                        ps, pk = ps_rot()
                        for j in range(4):
                            S.tr(ps[:, j * 128:(j + 1) * 128], HF[:, half * 4 + j, q * 128:(q + 1) * 128], ident,
                                 [K("HFo", half * 4 + j), K("c128")], [pk])
                        S.cp(ob[:, half * 512:(half + 1) * 512], ps[:, :], [pk], [obk],
                             eng=("act" if half else "dve"))
                    r0 = tc * TW + q * 128
                    i = S.dma(out_d[r0:r0 + 128, :], ob[:], [obk], [K("out", r0)])
                    S.final.append(i)

            rmsnorm_fm(xT, "xT", lambda kc: col(0, "fin_g", kc), None, None, KC, S_, st, hook=final_hook, tag="z")

    S.finalize(es)
    es.close()
    return nc, dump_d


def make_consts():
    c128 = np.zeros((128, 8, 128), np.float32)
    j = np.arange(128)[:, None]
    s = np.arange(128)[None, :]
    c128[:, 0] = np.eye(128)
    c128[:, 1] = 1.0
    c128[:, 2] = (j <= s)
    c128[:, 3] = -1.0 * (j >= s)
    c128[:, 4] = -1.0
    partner = np.where((np.arange(128) % 64) < 32, np.arange(128) + 32, np.arange(128) - 32)
    perm = np.zeros((128, 128), np.float32)
    perm[partner, np.arange(128)] = 1.0
    c128[:, 5] = perm
    c128[:, 6] = NEG * (s < j)
    cm = np.zeros((128, 12, 512), np.float32)
    p = np.arange(128)[:, None]
    f = np.arange(512)[None, :]
    for d in range(4):
        valid = (f - p) > d * 128
        cm[:, d] = valid
        cm[:, 4 + d] = NEG * (~valid)
        cm[:, 8 + d] = NEG * ((d * 128 + p) > f)
    half = 32
    inv = (np.float32(10000.0) ** (-(np.arange(half, dtype=np.float32)) / np.float32(half))).astype(np.float32)
    ang = np.arange(S_, dtype=np.float32)[None, :] * inv[:, None]
    cos = np.cos(ang).astype(np.float32)
    sin = np.sin(ang).astype(np.float32)
    rope = np.zeros((128, 2, S_), np.float32)
    for pp in range(128):
        d = pp % 64
        rope[pp, 0] = cos[d % 32]
        rope[pp, 1] = -sin[d % 32] if d < 32 else sin[d % 32]
    en = np.zeros((16, 16, 128), np.float32)
    for n in range(16):
        en[n, n, :] = 1.0
    gc = np.zeros((128, 3, 16, 8), np.float32)
    for i in range(16):
        own = i // 2
        for n in range(8):
            gc[:, 0, i, n] = 0.0 if n < own else -1e30
            gc[:, 1, i, n] = 1.0 if n < own else 0.0
            gc[:, 2, i, n] = 1.0 if n == own else 0.0
    blkoh = np.zeros((8, S_), np.float32)
    for n in range(8):
        blkoh[n, n * 256:(n + 1) * 256] = 1.0
    return dict(c128=c128.reshape(128, -1), cmask=cm.reshape(128, -1), rope=rope.reshape(128, -1),
                en=en.reshape(16, -1), gatec=gc.reshape(128, -1), blkoh=blkoh)


def colvec(v):
    v = np.asarray(v, np.float32)
    return np.ascontiguousarray(v.reshape(-1, 128).T)


def pack_params(inp):
    colp = np.zeros((128, L_, NCOL), np.float32)
    rowp = np.zeros((128, L_, NROW), np.float32)

    def put(l, name, arr):
        c0, w = COLS[name]
        assert arr.shape == (128, w), (name, arr.shape)
        colp[:, l, c0:c0 + w] = arr

    for l in range(L_):
        put(l, "mix_g", colvec(inp["mix_norm_g"][l]))
        put(l, "xat_g", colvec(inp["xattn_norm_g"][l]))
        put(l, "mem_g", colvec(inp["mem_norm_g"][l]))
        put(l, "ffn_g", colvec(inp["ffn_norm_g"][l]))
        put(l, "grp_g", colvec(inp["group_norm_g"][l]))
        cw = inp["lru_conv_w"][l]
        put(l, "lru_cw", np.concatenate([colvec(cw[k]) for k in range(4)], axis=1).reshape(128, 4, 2)
            .transpose(0, 2, 1).reshape(128, 8))
        put(l, "lru_cb", colvec(inp["lru_conv_b"][l]))
        put(l, "lru_br", colvec(inp["lru_br"][l]))
        put(l, "lru_bi", colvec(inp["lru_bi"][l]))
        put(l, "lru_lam", colvec(inp["lru_lambda"][l]))
        sw = inp["ssm_conv_w"][l]
        put(l, "ssm_cw", np.concatenate([colvec(sw[k]) for k in range(4)], axis=1).reshape(128, 4, 6)
            .transpose(0, 2, 1).reshape(128, 24))
        put(l, "ssm_cb", colvec(inp["ssm_conv_b"][l]))
        put(l, "ssm_d", colvec(np.repeat(inp["ssm_d"][l], 64)))
        put(l, "fin_g", colvec(inp["final_norm_g"]))
        r0, w = ROWS["dt_bias"]
        rowp[:, l, r0:r0 + w] = np.tile(inp["ssm_dt_bias"][l], 16)[None, :]
        r0, w = ROWS["a_log"]
        rowp[:, l, r0:r0 + w] = np.tile(inp["ssm_a_log"][l], 16)[None, :]
        r0, w = ROWS["bg"]
        rowp[:, l, r0:r0 + w] = inp["router_group_b"][l][None, :]
        r0, w = ROWS["be"]
        rowp[:, l, r0:r0 + w] = inp["router_expert_b"][l][None, :]
    wbd = np.zeros((L_, 2, 2, 128, 128), np.float32)
    for l in range(L_):
        for gi, nm in enumerate(("lru_wr", "lru_wi")):
            for c in range(2):
                for hh in range(2):
                    wbd[l, gi, c, hh * 64:(hh + 1) * 64, hh * 64:(hh + 1) * 64] = inp[nm][l, 2 * c + hh]
    router_w = np.concatenate([inp["router_group_w"], inp["router_expert_w"]], axis=2)
    return dict(colp=colp.reshape(128, -1), rowp=rowp.reshape(128, -1), lru_wbd=wbd,
                router_w=np.ascontiguousarray(router_w))


SHARED = ["w_in", "w_out", "xattn_wq", "xattn_wkv", "xattn_wo", "expert_w1", "expert_w3", "expert_w2"]


def make_in_maps(inputs, cores):
    inp = {k: np.asarray(v) for k, v in inputs.items()}
    consts = make_consts()
    packed = pack_params(inp)
    shared = {k: np.ascontiguousarray(inp[k], dtype=np.float32) for k in SHARED}
    maps = []
    for b in cores:
        m = dict(shared)
        m.update(consts)
        m.update(packed)
        m["x"] = np.ascontiguousarray(inp["x"][b])
        m["mem"] = np.ascontiguousarray(inp["mem"][b])
        maps.append(m)
    return maps


_CACHE = {}


def kernel(**inputs):
    if "nc" not in _CACHE:
        _CACHE["nc"] = build_program()[0]
    nc = _CACHE["nc"]
    maps = make_in_maps(inputs, list(range(8)))
    res = run_bass_kernel_spmd(nc, maps, core_ids=list(range(8)))
    return np.stack([r["out"] for r in res.results], axis=0).astype(np.float32)
```

```python
import numpy as np
from contextlib import ExitStack
import concourse.bass as bass
import concourse.mybir as mybir
from concourse.bass_utils import run_bass_kernel_spmd

F32 = mybir.dt.float32
BF16 = mybir.dt.bfloat16
F32R = mybir.dt.float32r
AF = mybir.ActivationFunctionType
ALU = mybir.AluOpType
AX = mybir.AxisListType

S_ = 2048
D_ = 1024
L_ = 2
KC = 8
NTC = 4
TW = 512
NT = 16
NEG = -30000.0
EPS = 1e-6

COLS = {}
_c = 0
for _n, _w in [("mix_g", 8), ("xat_g", 8), ("mem_g", 8), ("ffn_g", 8), ("grp_g", 8),
               ("lru_cw", 8), ("lru_cb", 2), ("lru_br", 2), ("lru_bi", 2), ("lru_lam", 2),
               ("ssm_cw", 24), ("ssm_cb", 6), ("ssm_d", 2), ("fin_g", 8)]:
    COLS[_n] = (_c, _w)
    _c += _w
NCOL = _c
ROWS = {}
_c = 0
for _n, _w in [("dt_bias", 64), ("a_log", 64), ("bg", 4), ("be", 16)]:
    ROWS[_n] = (_c, _w)
    _c += _w
NROW = _c


def _free(ap):
    n = 1
    for d in ap.shape[1:]:
        n *= int(d)
    return n


DEBUG_LINES = False


class Sched:
    ENG = ("pe", "act", "dve", "pool", "sp")
    GATED = ("pe", "act", "dve", "sp")

    def __init__(self, nc):
        self.nc = nc
        self.ops = []
        self.lastw = {}
        self.rd = {}
        self.by_eng = {e: [] for e in self.ENG}
        self.final = []
        self.psn = 0
        self.seg = 0
        self.act_inorder = set()

    def add(self, eng, fn, reads=(), writes=(), dma=False, cost=0.3, arena=False):
        i = len(self.ops)
        reads = list(reads)
        writes = list(writes)
        deps = {}
        for r in reads:
            w = self.lastw.get(r)
            if w is not None:
                deps.setdefault(w, set()).add("raw")
        for w_ in writes:
            w = self.lastw.get(w_)
            if w is not None:
                deps.setdefault(w, set()).add("waw")
            for r in self.rd.get(w_, ()):
                deps.setdefault(r, set()).add("war")
        ws = set(writes)
        for r in reads:
            if r not in ws:
                self.rd.setdefault(r, []).append(i)
        for w_ in writes:
            self.lastw[w_] = i
            self.rd[w_] = []
        deps.pop(i, None)
        fdeps = set()
        for d, kinds in deps.items():
            od = self.ops[d]
            if od["eng"] == eng and not od["dma"] and not dma:
                if eng == "pe":
                    continue
                if "raw" not in kinds:
                    continue
            fdeps.add(d)
        ln_ = 0
        if DEBUG_LINES:
            import sys as _sys
            f_ = _sys._getframe(1)
            while f_ is not None and f_.f_code.co_name != "build_program":
                f_ = f_.f_back
            ln_ = f_.f_lineno if f_ is not None else 0
        self.ops.append(dict(eng=eng, fn=fn, deps=fdeps, order=set(deps.keys()), dma=dma, signal=dma, token=None,
                             seg=self.seg, cost=cost, arena=arena, line=ln_))
        self.by_eng[eng].append(i)
        return i

    def barrier(self):
        self.seg += 1

    def schedule(self, W=24):
        ops = self.ops
        n = len(ops)
        succ = [[] for _ in range(n)]
        nun = [0] * n
        for i, op in enumerate(ops):
            nun[i] = len(op["order"])
            for d in op["order"]:
                succ[d].append(i)
        ready = [0.0] * n
        fin = [0.0] * n
        done = [False] * n
        head = {e: 0 for e in self.ENG}
        free = {e: 0.0 for e in self.ENG}
        lists = {e: list(self.by_eng[e]) for e in self.ENG}
        new_order = {e: [] for e in self.ENG}
        nseg = self.seg + 1
        rem = [0] * (nseg + 1)
        for i, op in enumerate(ops):
            if op["eng"] in self.GATED:
                rem[op["seg"]] += 1
        import os
        WDEF = {"pe": W, "act": W, "dve": W}
        WE = {e_: int(os.environ.get("KSCHED_W_" + e_.upper(), WDEF[e_])) for e_ in ("pe", "act", "dve")}
        segbusy = {}
        segend = {}
        cur = 0
        seg_t0 = 0.0
        tmax = 0.0
        nsched = 0
        while nsched < n:
            while cur < nseg and rem[cur] == 0:
                cur += 1
                seg_t0 = tmax
            best = None
            for e in self.ENG:
                lst = lists[e]
                h = head[e]
                while h < len(lst) and done[lst[h]]:
                    h += 1
                head[e] = h
                w = WE.get(e, 1)
                if e == "act" and cur in self.act_inorder:
                    w = 1
                seen = 0
                j = h
                while j < len(lst) and seen < w:
                    i = lst[j]
                    j += 1
                    if done[i]:
                        continue
                    op = ops[i]
                    if e != "pool":
                        if op["seg"] != cur:
                            break
                    elif op["arena"] and op["seg"] > cur:
                        break
                    seen += 1
                    if nun[i] == 0:
                        est = max(free[e], ready[i])
                        if e != "pool" or op["arena"]:
                            est = max(est, seg_t0)
                        key = (est, i)
                        if best is None or key < best[0]:
                            best = (key, e, i)
            if best is None:
                raise RuntimeError("scheduler stuck at seg %d" % cur)
            (est, _), e, i = best
            op = ops[i]
            if op["dma"]:
                free[e] = est + 0.06
                f = est + op["cost"]
            else:
                f = est + op["cost"]
                free[e] = f
            fin[i] = f
            tmax = max(tmax, f)
            if not op["dma"]:
                segbusy[(op["seg"], e)] = segbusy.get((op["seg"], e), 0.0) + op["cost"]
            segend[op["seg"]] = max(segend.get(op["seg"], 0.0), f)
            done[i] = True
            nsched += 1
            new_order[e].append(i)
            if e in self.GATED:
                rem[op["seg"]] -= 1
            for s_ in succ[i]:
                nun[s_] -= 1
                lat = 0.0 if (ops[s_]["eng"] == e and not op["dma"]) else 0.1
                if f + lat > ready[s_]:
                    ready[s_] = f + lat
        self.by_eng = new_order
        self.sim_time = tmax
        import os
        if os.environ.get("KSCHED_DEBUG"):
            print("sched: ops", n, "sim_time_us", round(tmax, 1), flush=True)
            prev = 0.0
            for sg in sorted(segend):
                print("  seg", sg, "dur", round(segend[sg] - prev, 1), {e_: round(segbusy.get((sg, e_), 0.0), 1)
                                                                         for e_ in ("pe", "act", "dve")})
                prev = segend[sg]
        pos = {}
        for e in self.ENG:
            for k, i in enumerate(new_order[e]):
                pos[i] = k
        bar = [set() for _ in range(nseg + 1)]
        for s_ in range(1, nseg + 1):
            for e in ("pe", "act", "dve"):
                last = None
                for i in new_order[e]:
                    if ops[i]["seg"] < s_:
                        last = i
                    else:
                        break
                if last is not None:
                    bar[s_].add(last)
            for i in new_order["sp"]:
                if ops[i]["seg"] == s_ - 1 and ops[i]["dma"]:
                    bar[s_].add(i)
        for e in self.GATED:
            lastseg = 0
            for i in new_order[e]:
                sg = ops[i]["seg"]
                if sg > lastseg:
                    for t in range(lastseg + 1, sg + 1):
                        ops[i]["deps"] |= bar[t]
                    lastseg = sg
        for i in new_order["pool"]:
            if ops[i]["arena"]:
                ops[i]["deps"] |= bar[ops[i]["seg"]]
        for i, op in enumerate(ops):
            keep = set()
            bestp = {}
            for d in op["deps"]:
                if d == i:
                    continue
                od = ops[d]
                if od["dma"]:
                    keep.add(d)
                else:
                    pe_ = od["eng"]
                    if pe_ not in bestp or pos[d] > pos[bestp[pe_]]:
                        bestp[pe_] = d
            keep |= set(bestp.values())
            op["deps"] = keep

    def mm(self, out, lhsT, rhs, start, stop, reads, writes):
        nfree = _free(rhs)
        c = max(nfree, 64) / 2400.0 * (4.0 if rhs.dtype == F32 else 1.0) + 0.004
        return self.add("pe", lambda e: e.matmul(out, lhsT, rhs, start=start, stop=stop), reads, writes, cost=c)

    def tr(self, out, in_, ident, reads, writes):
        return self.add("pe", lambda e: e.transpose(out, in_, ident), reads, writes, cost=0.25)

    def act(self, out, in_, func, reads, writes, bias=None, scale=None, accum=None):
        kw = {}
        if bias is not None:
            kw["bias"] = bias
        if scale is not None:
            kw["scale"] = scale
        if accum is not None:
            kw["accum_out"] = accum
        c = 0.22 + _free(out) / 1400.0
        return self.add("act", lambda e: e.activation(out=out, in_=in_, func=func, **kw), reads, writes, cost=c)

    def _vc(self, out):
        return 0.08 + _free(out) / 960.0

    def tt(self, out, in0, in1, op, reads, writes, eng="dve"):
        return self.add(eng, lambda e: e.tensor_tensor(out=out, in0=in0, in1=in1, op=op), reads, writes,
                        cost=self._vc(out))

    def ts(self, out, in0, s1, s2, op0, op1, reads, writes, eng="dve"):
        if s2 is None:
            return self.add(eng, lambda e: e.tensor_scalar(out, in0, s1, None, op0), reads, writes, cost=self._vc(out))
        return self.add(eng, lambda e: e.tensor_scalar(out, in0, s1, s2, op0, op1), reads, writes, cost=self._vc(out))

    def stt(self, out, in0, scalar, in1, op0, op1, reads, writes, eng="dve"):
        return self.add(eng, lambda e: e.scalar_tensor_tensor(out=out, in0=in0, scalar=scalar, in1=in1,
                                                             op0=op0, op1=op1), reads, writes, cost=self._vc(out))

    def cp(self, out, in_, reads, writes, eng="dve"):
        if eng == "act":
            return self.add("act", lambda e: e.activation(out=out, in_=in_, func=AF.Copy), reads, writes,
                            cost=0.22 + _free(out) / 1400.0)
        return self.add(eng, lambda e: e.tensor_copy(out=out, in_=in_), reads, writes, cost=self._vc(out))

    def recip(self, out, in_, reads, writes):
        return self.add("dve", lambda e: e.reciprocal(out=out, in_=in_), reads, writes, cost=0.1 + _free(out) / 240.0)

    def dma(self, out, in_, reads, writes, q="sp", arena=False):
        nbytes = 128 * _free(out) * 4
        return self.add(q, lambda e: e.dma_start(out=out, in_=in_), reads, writes, dma=True,
                        cost=2.0 + nbytes / 150e3, arena=arena)

    def finalize(self, es):
        nc = self.nc
        self.schedule()
        for op in self.ops:
            for d in op["deps"]:
                self.ops[d]["signal"] = True
        for d in self.final:
            self.ops[d]["signal"] = True
        csem = {e: es.enter_context(nc.semaphore("c_" + e)) for e in ("pe", "act", "dve", "pool")}
        NP = 16
        qsem = {q: [es.enter_context(nc.semaphore(f"q_{q}{k}")) for k in range(NP)] for q in ("sp", "pool")}
        cnt = {e: 0 for e in csem}
        qn = {q: 0 for q in qsem}
        quse = {q: [0] * NP for q in qsem}
        qprev = {q: [None] * NP for q in qsem}
        for i, op in enumerate(self.ops):
            if op["dma"]:
                q = op["eng"]
                k = qn[q] % NP
                qn[q] += 1
                quse[q][k] += 1
                op["token"] = (qsem[q][k], 16 * quse[q][k])
                if qprev[q][k] is not None:
                    op["deps"].add(qprev[q][k])
                qprev[q][k] = i
        for e in csem:
            for i in self.by_eng[e]:
                op = self.ops[i]
                if (not op["dma"]) and op["signal"]:
                    cnt[e] += 1
                    op["token"] = (csem[e], cnt[e])
        ops = self.ops
        final = list(self.final)

        def emit(engname, e):
            waited = {}
            for i in self.by_eng[engname]:
                op = ops[i]
                for d in sorted(op["deps"]):
                    sem, val = ops[d]["token"]
                    if waited.get(sem, 0) < val:
                        e.wait_ge(sem, val)
                        waited[sem] = val
                ins = op["fn"](e)
                if op["signal"]:
                    ins.then_inc(op["token"][0], 16 if op["dma"] else 1)
            if engname == "sp":
                for d in final:
                    sem, val = ops[d]["token"]
                    if waited.get(sem, 0) < val:
                        e.wait_ge(sem, val)
                        waited[sem] = val

        with nc.Block() as block:
            @block.tensor
            def _(e):
                emit("pe", e)

            @block.scalar
            def _(e):
                emit("act", e)

            @block.vector
            def _(e):
                emit("dve", e)

            @block.gpsimd
            def _(e):
                emit("pool", e)

            @block.sync
            def _(e):
                emit("sp", e)


def K(name, *idx):
    return (name,) + idx


def build_program(stop_after=None, dumps=(), n_layers=L_):
    nc = bass.Bass("TRN2", target_bir_lowering=False)
    S = Sched(nc)
    es = ExitStack()
    P = {}

    def din(name, shape):
        P[name] = nc.dram_tensor(name, list(shape), F32, kind="ExternalInput").ap()
        return P[name]

    x_d = din("x", [S_, D_])
    mem_d = din("mem", [256, D_])
    w_in_d = din("w_in", [L_, D_, 3076])
    w_out_d = din("w_out", [L_, D_, D_])
    wq_d = din("xattn_wq", [L_, D_, D_])
    wkv_d = din("xattn_wkv", [L_, D_, 2 * D_])
    wo_d = din("xattn_wo", [L_, D_, D_])
    w1_d = din("expert_w1", [L_, 16, D_, 256])
    w3_d = din("expert_w3", [L_, 16, D_, 256])
    w2_d = din("expert_w2", [L_, 16, 256, D_])
    wrt_d = din("router_w", [L_, D_, 20])
    lrubd_d = din("lru_wbd", [L_, 2, 2, 128, 128])
    colp_d = din("colp", [128, L_ * NCOL])
    rowp_d = din("rowp", [128, L_ * NROW])
    c128_d = din("c128", [128, 8 * 128])
    cmask_d = din("cmask", [128, 12 * 512])
    rope_d = din("rope", [128, 2 * S_])
    en_d = din("en", [16, 16 * 128])
    blkoh_d = din("blkoh", [8, S_])
    gate_c_d = din("gatec", [128, 3 * 128])
    out_d = nc.dram_tensor("out", [S_, D_], F32, kind="ExternalOutput").ap()
    dump_d = {}

    uid = [0]

    def sb(name, shape, dt=F32, stack=None):
        uid[0] += 1
        return (stack or es).enter_context(nc.sbuf_tensor(f"{name}_{uid[0]}", list(shape), dt))

    xT = sb("xT", [128, KC, S_])
    hT = sb("hT", [128, KC, S_], BF16)
    colp = sb("colp_s", [128, L_ * NCOL])
    rowp = sb("rowp_s", [128, L_ * NROW])
    c128 = sb("c128_s", [128, 8 * 128])
    c128b = sb("c128b_s", [128, 2 * 128], BF16)
    W = [sb(f"wslot{i}", [128, KC, 512], BF16) for i in range(3)]
    PS = [es.enter_context(nc.psum_tensor(f"ps{i}", [128, 512], F32)) for i in range(8)]
    wn = [0]

    ident = c128[:, 0:128]
    ones_f = c128[:, 128:256]
    tri_le = c128[:, 256:384]
    negu = c128[:, 384:512]
    negones = c128[:, 512:640]
    perm_f = c128[:, 640:768]
    ssdneg = c128[:, 768:896]
    ones_b = c128b[:, 128:256]
    ident_b = c128b[:, 0:128]

    cst = sb("cst", [128, 4])
    S.add("dve", lambda e: e.memset(cst[:, 0:1], EPS), [], [K("cst0")])
    S.add("dve", lambda e: e.memset(cst[:, 1:2], 1.0), [], [K("cst1")])
    S.add("dve", lambda e: e.memset(cst[:, 2:4], 0.0), [K("cst0"), K("cst1")], [K("cst")])
    S.dma(colp[:], colp_d[:, :], [], [K("colp")])
    S.dma(rowp[:], rowp_d[:, :], [], [K("rowp")])
    S.dma(c128[:], c128_d[:, :], [], [K("c128")])
    S.dma(c128b[:], c128_d[:, 0:256], [], [K("c128b")], q="pool")

    def col(l, name, j=0, n=1):
        c0, w = COLS[name]
        return colp[:, l * NCOL + c0 + j: l * NCOL + c0 + j + n]

    def row(l, name, j=0, n=None):
        c0, w = ROWS[name]
        n = w if n is None else n
        return rowp[:, l * NROW + c0 + j: l * NROW + c0 + j + n]

    def ps_rot():
        k = S.psn % 4
        S.psn += 1
        return PS[k], K("ps", k)

    Wf = [w[:].rearrange("p a b -> p (a b)") for w in W]

    def load_w(src2d, nk, ncols, slot=None, c0=0, tot=None):
        tot = ncols if tot is None else tot
        if slot is None:
            k = wn[0] % 3
            wn[0] += 1
        else:
            k = slot
        v = Wf[k][:, 0:nk * tot].rearrange("p (a b) -> p a b", a=nk)
        step = 2 if nk >= 2 else 1
        allk = [K("w", k, j_, hf_) for j_ in range(4) for hf_ in range(2)]
        for h in range(0, nk, step):
            if nk == 8 and tot == 512:
                halves = [hf_ for hf_ in range(2) if c0 < (hf_ + 1) * 256 and c0 + ncols > hf_ * 256]
                wkeys_ = [K("w", k, h // 2, hf_) for hf_ in halves]
            else:
                wkeys_ = allk
            S.dma(v[:, h:h + step, c0:c0 + ncols],
                  src2d[h * 128:(h + step) * 128, :].rearrange("(kc p) n -> p kc n", p=128),
                  [], wkeys_, q="pool")
        return v, allk, k

    def dump(name, ap, reads, shape):
        if name in dumps:
            d = nc.dram_tensor("dbg_" + name, list(shape), ap.dtype, kind="ExternalOutput").ap()
            dump_d[name] = d
            i = S.dma(d, ap, reads, [K("dbg", name)])
            S.final.append(i)

    def load_fm(dst, src_d, ntile, dkey, st):
        xin = [sb(f"xin{j}", [128, D_], stack=st) for j in range(2)]
        for i in range(ntile):
            b = xin[i % 2]
            S.dma(b[:], src_d[i * 128:(i + 1) * 128, :], [], [K("xin", i % 2)])
            for half in range(2):
                ps, pk = ps_rot()
                for j in range(4):
                    kc = half * 4 + j
                    S.tr(ps[:, j * 128:(j + 1) * 128], b[:, kc * 128:(kc + 1) * 128], ident,
                         [K("xin", i % 2), K("c128")], [pk])
                S.cp(dst[:, half * 4:half * 4 + 4, i * 128:(i + 1) * 128],
                     ps[:].rearrange("p (a b) -> p a b", a=4), [pk],
                     [K(dkey, half * 4 + j, i // 4) for j in range(4)], eng=("dve" if half == 0 else "act"))

    st0 = ExitStack()
    load_fm(xT, x_d, NT, "xT", st0)

    def rmsnorm_fm(src, skey, gcol, dst, dkey, nk, T, st, dscale=1.0, hook=None, tag="n"):
        tw = min(T, TW)
        sq = [sb(f"sq_{tag}{j}", [128, nk, tw], BF16, stack=st) for j in range(2)]
        rs = [sb(f"rs_{tag}{j}", [128, tw], stack=st) for j in range(2)]
        for tc in range(T // tw):
            sl = slice(tc * tw, (tc + 1) * tw)
            q = sq[tc % 2]
            for kc in range(nk):
                S.act(q[:, kc, :], src[:, kc, sl], AF.Square, [K(skey, kc, tc)], [K("sq" + tag, tc % 2, kc)])
            ps, pk = ps_rot()
            for kc in range(nk):
                S.mm(ps[:, 0:tw], ones_b, q[:, kc, :], kc == 0, kc == nk - 1,
                     [K("sq" + tag, tc % 2, kc), K("c128b")], [pk])
            r = rs[tc % 2]
            S.act(r[:], ps[:, 0:tw], AF.Sqrt, [pk, K("cst")], [K("rs" + tag, tc % 2)],
                  bias=cst[:, 0:1], scale=1.0 / (nk * 128))
            S.add("dve", (lambda e, r=r: e.reciprocal(out=r[:], in_=r[:])),
                  [K("rs" + tag, tc % 2)], [K("rs" + tag, tc % 2)], cost=0.1 + tw / 240.0)
            if dst is not None:
                for kc in range(nk):
                    S.stt(dst[:, kc, sl], src[:, kc, sl], gcol(kc), r[:], ALU.mult, ALU.mult,
                          [K(skey, kc, tc), K("rs" + tag, tc % 2), K("colp")], [K(dkey, kc, tc)],
                          eng="dve")
            if hook is not None:
                hook(tc, r)

    ALLH = [K("hT", kc, tc) for kc in range(KC) for tc in range(NTC)]

    def hkeys(tc):
        return [K("hT", kc, tc) for kc in range(KC)]

    def proj_fm(wv, wkeys, c0, tc, ps, pk):
        for kc in range(KC):
            S.mm(ps[:, :], wv[:, kc, c0:c0 + 128], hT[:, kc, tc * TW:(tc + 1) * TW], kc == 0, kc == KC - 1,
                 wkeys + [K("hT", kc, tc)], [pk])

    def conv_chunk(ps, pk, tc, xw, xwk, cwcol, cbcol, dst, dkey):
        w_ = xw[tc % 2]
        wk = K(xwk, tc % 2)
        if tc == 0:
            S.add("dve", lambda e: e.memset(w_[:, 0:3], 0.0), [], [wk])
        else:
            S.cp(w_[:, 0:3], xw[(tc - 1) % 2][:, 512:515], [K(xwk, (tc - 1) % 2)], [wk])
        S.cp(w_[:, 3:515], ps[:, :], [pk], [wk], eng="act")
        S.ts(dst, w_[:, 3:515], cwcol(3), cbcol, ALU.mult, ALU.add, [wk, K("colp")], [dkey])
        for k in range(3):
            S.stt(dst, w_[:, k:k + 512], cwcol(k), dst, ALU.mult, ALU.add, [wk, dkey, K("colp")], [dkey])

    def group_out(l, g, YG, st):
        YB = sb("YB", [128, 2, S_], BF16, stack=st)
        rmsnorm_fm(YG, "YG", lambda kc: col(l, "grp_g", 2 * g + kc), YB, "YB", 2, S_, st, tag="g")
        dump(f"yn{l}_{g}", YB[:], [K("YB", c, t) for c in range(2) for t in range(NTC)], [128, 2, S_])
        for half in range(2):
            wv, wk, _ = load_w(w_out_d[l, g * 256:(g + 1) * 256, half * 512:(half + 1) * 512], 2, 512)
            for o4 in range(4):
                oc = half * 4 + o4
                for tc in range(NTC):
                    ps, pk = ps_rot()
                    for kc in range(2):
                        S.mm(ps[:, :], wv[:, kc, o4 * 128:(o4 + 1) * 128], YB[:, kc, tc * TW:(tc + 1) * TW],
                             kc == 0, kc == 1, wk + [K("YB", kc, tc)], [pk])
                    S.tt(xT[:, oc, tc * TW:(tc + 1) * TW], xT[:, oc, tc * TW:(tc + 1) * TW], ps[:, :], ALU.add,
                         [K("xT", oc, tc), pk], [K("xT", oc, tc)])

    ALLX = [K("xT", kc, tc) for kc in range(KC) for tc in range(NTC)]
    done = False
    for l in range(n_layers):
        with ExitStack() as st:
            rmsnorm_fm(xT, "xT", lambda kc: col(l, "mix_g", kc), hT, "hT", KC, S_, (st0 if l == 0 else st), tag="a")
        if l == 0:
            st0.close()
        S.barrier()
        if l == 0:
            dump("h0", hT[:], ALLH, [128, KC, S_])
        if stop_after == "norm0":
            break

        with ExitStack() as sty, ExitStack() as st:
            YG = sb("YG", [128, 2, S_], stack=sty)
            wv, wk, _ = load_w(w_in_d[l, :, 0:512], 8, 512)
            wbd = sb("wbd", [128, 4, 128], BF16, stack=st)
            S.dma(wbd[:], lrubd_d[l].rearrange("g c k m -> k (g c) m"), [], [K("wbd")], q="pool", arena=True)
            c8 = sb("c8", [128, 2], stack=st)
            dump(f"A_wbd{l}", wbd[:], [K("wbd")], [128, 4, 128])
            cy = sb("cy", [128, 2], stack=st)
            cz = sb("cz", [128, 2], stack=st)
            S.act(cy[:], col(l, "lru_lam", 0, 2), AF.Exp, [K("colp")], [K("cy")], scale=-1.0)
            S.ts(cz[:], cy[:], 2.0, None, ALU.add, None, [K("cy")], [K("cz")])
            S.add("dve", lambda e: e.reciprocal(out=cz[:], in_=cz[:]), [K("cz")], [K("cz")])
            S.tt(cz[:], cz[:], cy[:], ALU.mult, [K("cz"), K("cy")], [K("cz")])
            S.tt(cy[:], cz[:], cz[:], ALU.mult, [K("cz")], [K("cy")])
            S.ts(c8[:], cy[:], 1.0 / 9.0, 1.0 / 7.0, ALU.mult, ALU.add, [K("cy")], [K("c8")])
            for cc_ in (1.0 / 5.0, 1.0 / 3.0, 1.0):
                S.tt(c8[:], c8[:], cy[:], ALU.mult, [K("c8"), K("cy")], [K("c8")])
                S.ts(c8[:], c8[:], cc_, None, ALU.add, None, [K("c8")], [K("c8")])
            S.tt(c8[:], c8[:], cz[:], ALU.mult, [K("c8"), K("cz")], [K("c8")])
            S.ts(c8[:], c8[:], -16.0, None, ALU.mult, None, [K("c8")], [K("c8")])
            xw = [sb(f"xw{j}", [128, 515], stack=st) for j in range(2)]
            XC = sb("XC", [128, S_], stack=st)
            XCB = sb("XCB", [128, S_], BF16, stack=st)
            GG = sb("GG", [128, S_], stack=st)
            RA = sb("RA", [128, S_], stack=st)
            IU = sb("IU", [128, S_], stack=st)
            T2 = sb("T2", [128, S_], stack=st)
            XX = sb("XX", [128, S_], stack=st)
            for c in range(2):
                for tc in range(NTC):
                    sl = slice(tc * TW, (tc + 1) * TW)
                    ps, pk = ps_rot()
                    proj_fm(wv, wk, c * 128, tc, ps, pk)
                    conv_chunk(ps, pk, tc, xw, "xw", lambda k: col(l, "lru_cw", c * 4 + k), col(l, "lru_cb", c),
                               XC[:, sl], K("XC", tc))
                    ps, pk = ps_rot()
                    proj_fm(wv, wk, 256 + c * 128, tc, ps, pk)
                    S.cp(GG[:, sl], ps[:, :], [pk], [K("GG", tc)], eng="act")
                XCk = [K("XC", t) for t in range(NTC)]
                GGk = [K("GG", t) for t in range(NTC)]
                S.tt(T2[:], GG[:], GG[:], ALU.mult, GGk, [K("T2")])
                S.ts(T2[:], T2[:], 0.044715, 1.0, ALU.mult, ALU.add, [K("T2")], [K("T2")])
                S.tt(T2[:], T2[:], GG[:], ALU.mult, [K("T2")] + GGk, [K("T2")])
                S.act(T2[:], T2[:], AF.Sigmoid, [K("T2")], [K("T2")], scale=1.5957691216)
                S.tt(GG[:], GG[:], T2[:], ALU.mult, [K("T2")] + GGk, GGk)
                S.cp(XCB[:], XC[:], XCk, [K("XCB")], eng="act")
                for tc in range(NTC):
                    sl = slice(tc * TW, (tc + 1) * TW)
                    ps, pk = ps_rot()
                    S.mm(ps[:, :], wbd[:, c, :], XCB[:, sl], True, True, [K("wbd"), K("XCB")], [pk])
                    S.act(RA[:, sl], ps[:, :], AF.Sigmoid, [pk, K("colp")], [K("RA", tc)], bias=col(l, "lru_br", c))
                    ps, pk = ps_rot()
                    S.mm(ps[:, :], wbd[:, 2 + c, :], XCB[:, sl], True, True, [K("wbd"), K("XCB")], [pk])
                    S.act(IU[:, sl], ps[:, :], AF.Sigmoid, [pk, K("colp")], [K("IU", tc)], bias=col(l, "lru_bi", c))
                RAk = [K("RA", t) for t in range(NTC)]
                IUk = [K("IU", t) for t in range(NTC)]
                if c == 1:
                    dump(f"A_r{l}", RA[:], RAk, [128, S_])
                    dump(f"A_i{l}", IU[:], IUk, [128, S_])
                    dump(f"A_xcb{l}", XCB[:], [K("XCB")], [128, S_])
                    dump(f"A_xc{l}", XC[:], XCk, [128, S_])
                S.ts(XX[:], RA[:], c8[:, c:c + 1], 2.0, ALU.mult, ALU.mult, RAk + [K("c8")], [K("XX")])
                S.ts(T2[:], XX[:], 1.0 / 120.0, None, ALU.mult, None, [K("XX")], [K("T2")])
                for cc_ in (1.0 / 24.0, 1.0 / 6.0, 0.5, 1.0):
                    S.stt(T2[:], T2[:], cc_, XX[:], ALU.add, ALU.mult, [K("T2"), K("XX")], [K("T2")])
                S.act(T2[:], T2[:], AF.Sqrt, [K("T2")], [K("T2")], scale=-1.0)
                S.act(RA[:], XX[:], AF.Exp, [K("XX")], RAk, scale=0.5)
                S.tt(IU[:], IU[:], XC[:], ALU.mult, IUk + XCk, IUk)
                S.tt(IU[:], IU[:], T2[:], ALU.mult, IUk + [K("T2")], IUk)
                S.add("dve", lambda e: e.tensor_tensor_scan(out=XC[:], data0=RA[:], data1=IU[:], initial=0.0,
                                                            op0=ALU.mult, op1=ALU.add), RAk + IUk, XCk, cost=2.6)
                S.tt(YG[:, c, :], XC[:], GG[:], ALU.mult, XCk + GGk, [K("YG", c, t) for t in range(NTC)])
            for nm_, t_ in (("XC", XC), ("GG", GG), ("RA", RA), ("IU", IU), ("T2", T2)):
                dump(f"A_{nm_}{l}", t_[:], [K(nm_, t) for t in range(NTC)] + [K(nm_)], [128, S_])
            dump(f"ya{l}", YG[:], [K("YG", c, t) for c in range(2) for t in range(NTC)], [128, 2, S_])
            st.close()
            S.barrier()
            group_out(l, 0, YG, sty)
        S.barrier()
        if stop_after == f"A{l}":
            done = True
            break

        with ExitStack() as sty, ExitStack() as st:
            YG = sb("YG", [128, 2, S_], stack=sty)
            QB = sb("QB", [128, 2, S_], BF16, stack=st)
            KZ = sb("KZ", [128, 4, S_], BF16, stack=st)
            S.add("dve", lambda e: e.memset(KZ[:], 0.0), [], [K("KZ", h_, t_) for h_ in range(4) for t_ in range(NTC)], cost=4.5)
            VB = sb("VB", [128, NT, 256], BF16, stack=st)
            M01 = sb("M01", [128, 4, 512], stack=st)
            NM = sb("NM", [128, 4, 512], BF16, stack=st)
            S.dma(M01[:], cmask_d[:, 0:2048].rearrange("p (a b) -> p a b", a=4), [], [K("M01")])
            S.dma(NM[:], cmask_d[:, 2048:4096].rearrange("p (a b) -> p a b", a=4), [], [K("NM")], q="pool", arena=True)
            wv, wk, _ = load_w(w_in_d[l, :, 512:1024], 8, 512)
            wv2, wk2, _ = load_w(w_in_d[l, :, 1024:1280], 8, 256)
            for c2 in range(2):
                for tc in range(NTC):
                    sl = slice(tc * TW, (tc + 1) * TW)
                    ps, pk = ps_rot()
                    proj_fm(wv, wk, c2 * 128, tc, ps, pk)
                    S.act(QB[:, c2, sl], ps[:, :], AF.Copy, [pk], [K("QB", c2, tc)], scale=0.125)
                    ps, pk = ps_rot()
                    proj_fm(wv, wk, 256 + c2 * 128, tc, ps, pk)
                    S.cp(KZ[0:64, 2 * c2, sl], ps[0:64, :], [pk], [K("KZ", 2 * c2, tc)])
                    S.cp(KZ[64:128, 2 * c2 + 1, sl], ps[64:128, :], [pk], [K("KZ", 2 * c2 + 1, tc)], eng="act")
            for i in range(NT):
                ps, pk = ps_rot()
                for kc in range(KC):
                    S.mm(ps[:, 0:256], hT[:, kc, i * 128:(i + 1) * 128], wv2[:, kc, :], kc == 0, kc == KC - 1,
                         wk2 + [K("hT", kc, i // 4)], [pk])
                S.cp(VB[:, i, :], ps[:, 0:256], [pk], [K("VB", i)], eng=("dve" if i % 2 else "act"))
            Eb = [sb(f"Eb{j}", [128, 512], stack=st) for j in range(2)]
            SPb = [sb(f"SPb{j}", [128, 512], F32R, stack=st) for j in range(3)]
            Wb = [sb(f"Wb{j}", [128, 512], BF16, stack=st) for j in range(2)]
            Rb = [sb(f"Rb{j}", [128, 512], F32R, stack=st) for j in range(2)]
            NUr = sb("NUr", [128, 2, 128], F32R, stack=st)
            S.cp(NUr[:, 0, :], negu, [K("c128")], [K("NUr")])
            S.cp(NUr[:, 1, :], negones, [K("c128")], [K("NUr")])
            items = []
            grp = 0
            for h in range(4):
                for c in range(NTC):
                    nb = 4 * c + 4
                    for idx, b in enumerate(range(nb - 1, -1, -1)):
                        items.append(dict(h=h, c=c, idx=idx, b=b, grp=grp, last=(b == 0)))
                    grp += 1
            for i_, itm in enumerate(items):
                itm["i"] = i_
                itm["pr"] = slice((itm["h"] % 2) * 64, (itm["h"] % 2) * 64 + 64)
                itm["c2"] = itm["h"] // 2
                itm["d"] = itm["b"] - 4 * itm["c"]
                itm["ksl"] = slice(itm["b"] * 128, (itm["b"] + 1) * 128)
                itm["qsl"] = slice(itm["c"] * TW, (itm["c"] + 1) * TW)
                itm["qk"] = [K("KZ", itm["h"], itm["b"] // 4), K("QB", itm["c2"], itm["c"])]

            def sb_s1(m):
                i = m["i"]
                pr, c2 = m["pr"], m["c2"]
                off = 128 * max(m["d"], 0)
                cs = slice(off, TW)
                qs = slice(m["c"] * TW + off, (m["c"] + 1) * TW)
                if m["idx"] == 0:
                    R0, R0k = Rb[m["grp"] % 2], K("Rb", m["grp"] % 2)
                    S.ts(R0[:], M01[:, 0, :], 0.0, None, ALU.mult, None, [K("M01")], [R0k])
                psZ, pkZ = ps_rot()
                S.mm(psZ[:, cs], KZ[:, m["h"], m["ksl"]], QB[:, c2, qs], True, True, m["qk"], [pkZ])
                E, Ek = Eb[i % 2], K("Eb", i % 2)
                SP, SPk = SPb[i % 3], K("SPb", i % 3)
                S.act(E[:, cs], psZ[:, cs], AF.Exp, [pkZ], [Ek])
                S.act(SP[:, cs], E[:, cs], AF.Ln, [Ek, K("cst")], [SPk], bias=cst[:, 1:2])
                if m["d"] >= 0:
                    S.tt(SP[:, cs], SP[:, cs].bitcast(F32), M01[:, m["d"], cs], ALU.mult, [SPk, K("M01")], [SPk])

            def sb_s2(m):
                i = m["i"]
                pr, c2, d = m["pr"], m["c2"], m["d"]
                off = 128 * max(d, 0)
                cs = slice(off, TW)
                qs = slice(m["c"] * TW + off, (m["c"] + 1) * TW)
                SP, SPk = SPb[i % 3], K("SPb", i % 3)
                Wt, Wk_ = Wb[i % 2], K("Wb", i % 2)
                R, Rk = Rb[m["grp"] % 2], K("Rb", m["grp"] % 2)
                psA, pkA = ps_rot()
                has_r = m["idx"] > 0
                S.mm(psA[:, cs], KZ[:, m["h"], m["ksl"]], QB[:, c2, qs], True, False, m["qk"], [pkA])
                S.mm(psA[:, cs], NUr[:, 0, :], SP[:, cs], False, (not has_r) and d < 0, [SPk, K("NUr")], [pkA])
                if has_r:
                    S.mm(psA[:, cs], NUr[:, 1, :], R[:, cs], False, d < 0, [Rk, K("NUr")], [pkA])
                if d >= 0:
                    S.mm(psA[:, cs], ident_b, NM[:, d, cs], False, True, [K("NM"), K("c128b")], [pkA])
                S.act(Wt[:, cs], psA[:, cs], AF.Exp, [pkA], [Wk_])
                if not m["last"]:
                    S.tt(R[:, cs], R[:, cs].bitcast(F32), SP[:, cs].bitcast(F32), ALU.add, [Rk, SPk], [Rk])

            def sb_s3(m):
                i = m["i"]
                pr, c2, h = m["pr"], m["c2"], m["h"]
                off = 128 * max(m["d"], 0)
                cs = slice(off, TW)
                Wt, Wk_ = Wb[i % 2], K("Wb", i % 2)
                psO, pkO = PS[4 + m["grp"] % 2], K("psO", m["grp"] % 2)
                S.mm(psO[:, cs], VB[:, m["b"], c2 * 128:(c2 + 1) * 128], Wt[:, cs], m["idx"] == 0, m["last"],
                     [K("VB", m["b"]), Wk_], [pkO])
                if m["last"]:
                    S.cp(YG[pr, c2, m["qsl"]], psO[pr, :], [pkO], [K("YG", c2, m["c"])], eng="act")

            n_it = len(items)
            for r in range(-2, n_it):
                if 0 <= r + 2 < n_it:
                    sb_s1(items[r + 2])
                if 0 <= r + 1 < n_it:
                    sb_s2(items[r + 1])
                if 0 <= r < n_it:
                    sb_s3(items[r])
            dump(f"yb{l}", YG[:], [K("YG", c, t) for c in range(2) for t in range(NTC)], [128, 2, S_])
            st.close()
            S.barrier()
            group_out(l, 1, YG, sty)
        S.barrier()
        if stop_after == f"B{l}":
            done = True
            break

        with ExitStack() as sty, ExitStack() as st:
            YG = sb("YG", [128, 2, S_], stack=sty)
            XS = sb("XS", [128, 2, S_], stack=st)
            BTb = sb("BTb", [128, 2, S_], BF16, stack=st)
            CTb = sb("CTb", [128, 2, S_], BF16, stack=st)
            BTM = sb("BTM", [128, NT, 256], BF16, stack=st)
            XTM = sb("XTM", [128, NT, 256], BF16, stack=st)
            wdt = sb("wdt", [128, KC, 4], BF16, stack=st)
            stc = ExitStack()
            xw = [sb(f"xwc{j}", [128, 515], stack=stc) for j in range(2)]
            CV = [sb(f"CV{j}", [128, 512], stack=stc) for j in range(2)]
            BF = [sb(f"BF{j}", [128, 512], stack=stc) for j in range(2)]
            S.dma(wdt[:], w_in_d[l, :, 2304:2308].rearrange("(kc p) n -> p kc n", p=128), [], [K("wdt")], q="pool", arena=True)
            wv1, wk1, _ = load_w(w_in_d[l, :, 1280:1792], 8, 512)
            wv2, wk2, _ = load_w(w_in_d[l, :, 1792:2304], 8, 512)
            n_cv = 0
            for j in range(6):
                if j < 2:
                    wv, wk, c0 = wv1, wk1, 256 + j * 128
                elif j < 4:
                    wv, wk, c0 = wv2, wk2, (j - 2) * 128
                else:
                    wv, wk, c0 = wv2, wk2, 256 + (j - 4) * 128
                for tc in range(NTC):
                    sl = slice(tc * TW, (tc + 1) * TW)
                    ps, pk = ps_rot()
                    proj_fm(wv, wk, c0, tc, ps, pk)
                    cv, cvk = CV[n_cv % 2], K("CV", n_cv % 2)
                    conv_chunk(ps, pk, tc, xw, "xwc", lambda k: col(l, "ssm_cw", j * 4 + k), col(l, "ssm_cb", j),
                               cv[:], cvk)
                    if j < 2:
                        S.act(XS[:, j, sl], cv[:], AF.Silu, [cvk], [K("XS", j, tc)])
                        ps, pk = ps_rot()
                        for q in range(4):
                            S.tr(ps[:, q * 128:(q + 1) * 128], XS[:, j, tc * TW + q * 128: tc * TW + (q + 1) * 128],
                                 ident, [K("XS", j, tc), K("c128")], [pk])
                        S.cp(XTM[:, tc * 4:tc * 4 + 4, j * 128:(j + 1) * 128],
                             ps[:].rearrange("p (a b) -> p a b", a=4), [pk], [K("XTM", tc, j)])
                    elif j < 4:
                        g = j - 2
                        bf, bfk = BF[n_cv % 2], K("BF", n_cv % 2)
                        S.act(bf[:], cv[:], AF.Silu, [cvk], [bfk])
                        S.cp(BTb[:, g, sl], bf[:], [bfk], [K("BTb", g, tc)])
                        ps, pk = ps_rot()
                        for q in range(4):
                            S.tr(ps[:, q * 128:(q + 1) * 128], bf[:, q * 128:(q + 1) * 128], ident,
                                 [bfk, K("c128")], [pk])
                        S.cp(BTM[:, tc * 4:tc * 4 + 4, g * 128:(g + 1) * 128],
                             ps[:].rearrange("p (a b) -> p a b", a=4), [pk], [K("BTM", tc, g)])
                    else:
                        S.act(CTb[:, j - 4, sl], cv[:], AF.Silu, [cvk], [K("CTb", j - 4, tc)])
                    n_cv += 1
            stc.close()
            S.barrier()
            DT = sb("DT", [128, 64], stack=st)
            AN = sb("AN", [128, 64], stack=st)
            AC = sb("AC", [128, 64], stack=st)
            ACS = sb("ACS", [128, 64], stack=st)
            TOT = sb("TOT", [128, 64], stack=st)
            DEC = sb("DEC", [128, 64], stack=st)
            ETOT = sb("ETOT", [128, 64], stack=st)
            DTD = sb("DTD", [128, 64], stack=st)
            psd, pkd = ps_rot()
            for i in range(NT):
                for kc in range(KC):
                    S.mm(psd[:, i * 4:(i + 1) * 4], hT[:, kc, i * 128:(i + 1) * 128], wdt[:, kc, :], kc == 0,
                         kc == KC - 1, [K("wdt"), K("hT", kc, i // 4)], [pkd])
            S.tt(DT[:], psd[:, 0:64], row(l, "dt_bias"), ALU.add, [pkd, K("rowp")], [K("DT")])
            S.act(DT[:], DT[:], AF.Exp, [K("DT")], [K("DT")])
            S.act(DT[:], DT[:], AF.Ln, [K("DT"), K("cst")], [K("DT")], bias=cst[:, 1:2])
            S.act(AN[:], row(l, "a_log"), AF.Exp, [K("rowp")], [K("AN")])
            S.ts(AN[:], AN[:], -1.0, None, ALU.mult, None, [K("AN")], [K("AN")])
            S.tt(AC[:], DT[:], AN[:], ALU.mult, [K("DT"), K("AN")], [K("AC")])
            ps, pk = ps_rot()
            S.mm(ps[:, 0:64], tri_le, AC[:], True, True, [K("AC"), K("c128")], [pk])
            S.cp(ACS[:], ps[:, 0:64], [pk], [K("ACS")])
            ps, pk = ps_rot()
            S.mm(ps[:, 0:64], ones_f, AC[:], True, True, [K("AC"), K("c128")], [pk])
            S.cp(TOT[:], ps[:, 0:64], [pk], [K("TOT")])
            S.tt(DEC[:], TOT[:], ACS[:], ALU.subtract, [K("TOT"), K("ACS")], [K("DEC")])
            S.act(DEC[:], DEC[:], AF.Exp, [K("DEC")], [K("DEC")])
            S.act(ETOT[:], TOT[:], AF.Exp, [K("TOT")], [K("ETOT")])
            S.tt(DTD[:], DT[:], DEC[:], ALU.mult, [K("DT"), K("DEC")], [K("DTD")])
            ST = sb("ST", [128, 4, 64], stack=st)
            S.add("dve", lambda e: e.memset(ST[:], 0.0), [], [K("ST", h) for h in range(4)])
            Rm = [sb(f"Rm{j}", [128, 128], stack=st) for j in range(2)]
            ARG = [sb(f"ARG{j}", [128, 128], stack=st) for j in range(2)]
            E1 = [sb(f"E1{j}", [128, 128], stack=st) for j in range(2)]
            GL = [sb(f"GL{j}", [128, 128], BF16, stack=st) for j in range(2)]
            CTp = [sb(f"CTp{j}", [128, 128], BF16, stack=st) for j in range(2)]
            XCp = [[sb(f"XCp{a}{j}", [128, 128], BF16, stack=st) for j in range(2)] for a in range(2)]
            STp = sb("STp", [128, 4, 128], BF16, stack=st)
            for a in range(2):
                for j in range(2):
                    S.add("dve", (lambda e, t_=XCp[a][j]: e.memset(t_[:], 0.0)), [], [K("XCp", a, j)])
            S.add("dve", lambda e: e.memset(STp[:], 0.0), [], [K("STp", h) for h in range(4)])
            XDh = [sb(f"XDh{j}", [128, 64], BF16, stack=st) for j in range(2)]
            n = 0
            for ci in range(NT):
                csl = slice(ci * 128, (ci + 1) * 128)
                tcx = ci // 4
                for g in range(2):
                    psG, pkG = ps_rot()
                    S.mm(psG[:, 0:128], BTb[:, g, csl], CTb[:, g, csl], True, True,
                         [K("BTb", g, tcx), K("CTb", g, tcx)], [pkG])
                    psY, pkY = PS[4 + (ci * 2 + g) % 2], K("psO", (ci * 2 + g) % 2)
                    for hh in range(2):
                        h = 2 * g + hh
                        pr = slice(hh * 64, hh * 64 + 64)
                        cidx = ci * 4 + h
                        k2 = n % 2
                        S.ts(Rm[k2][:], tri_le, AC[:, cidx:cidx + 1], None, ALU.mult, None,
                             [K("AC"), K("c128")], [K("Rm", k2)])
                        ps1, pk1 = ps_rot()
                        S.mm(ps1[:, 0:128], ones_f, Rm[k2][:], True, True, [K("Rm", k2), K("c128")], [pk1])
                        S.stt(ARG[k2][:], ps1[:, 0:128], ACS[:, cidx:cidx + 1], ssdneg, ALU.subtract, ALU.add,
                              [pk1, K("ACS"), K("c128")], [K("ARG", k2)])
                        S.act(ARG[k2][:], ARG[k2][:], AF.Exp, [K("ARG", k2)], [K("ARG", k2)])
                        S.tt(GL[k2][:], ARG[k2][:], psG[:, 0:128], ALU.mult, [K("ARG", k2), pkG], [K("GL", k2)])
                        S.act(E1[k2][:], ps1[:, 0:128], AF.Exp, [pk1, K("ARG", k2)], [K("E1", k2)])
                        S.tt(CTp[k2][:], CTb[:, g, csl], E1[k2][:], ALU.mult, [K("CTb", g, tcx), K("E1", k2)],
                             [K("CTp", k2)])
                        rot = (ci * 2 + g) % 2
                        xcp = XCp[hh][rot]
                        S.ts(xcp[:, hh * 64:(hh + 1) * 64], XTM[:, ci, h * 64:(h + 1) * 64], DT[:, cidx:cidx + 1], None,
                             ALU.mult, None, [K("XTM", tcx, g), K("DT")], [K("XCp", hh, rot)])
                        S.mm(psY[:, 0:128], xcp[:], GL[k2][:], hh == 0, False, [K("XCp", hh, rot), K("GL", k2)], [pkY])
                        S.mm(psY[:, 0:128], STp[:, h, :], CTp[k2][:], False, hh == 1, [K("STp", h), K("CTp", k2)], [pkY])
                        S.ts(XDh[k2][:], XTM[:, ci, h * 64:(h + 1) * 64], DTD[:, cidx:cidx + 1], None, ALU.mult, None,
                             [K("XTM", tcx, g), K("DTD")], [K("XDh", k2)])
                        psS, pkS = ps_rot()
                        S.mm(psS[:, 0:64], BTM[:, ci, g * 128:(g + 1) * 128], XDh[k2][:], True, True,
                             [K("BTM", tcx, g), K("XDh", k2)], [pkS])
                        S.stt(ST[:, h, :], ST[:, h, :], ETOT[:, cidx:cidx + 1], psS[:, 0:64], ALU.mult, ALU.add,
                              [K("ST", h), K("ETOT"), pkS], [K("ST", h)])
                        S.cp(STp[:, h, hh * 64:(hh + 1) * 64], ST[:, h, :], [K("ST", h)], [K("STp", h)], eng="act")
                        n += 1
                    S.cp(YG[:, g, csl], psY[:, 0:128], [pkY], [K("YG", g, tcx)], eng="act")
            ARG = [sb(f"ZT{j}", [128, 512], stack=st) for j in range(1)]
            for j in range(2):
                YGk = [K("YG", j, t) for t in range(NTC)]
                S.stt(YG[:, j, :], XS[:, j, :], col(l, "ssm_d", j), YG[:, j, :], ALU.mult, ALU.add,
                      [K("XS", j, t) for t in range(NTC)] + YGk + [K("colp")], YGk)
                for tc in range(NTC):
                    sl = slice(tc * TW, (tc + 1) * TW)
                    ps, pk = ps_rot()
                    proj_fm(wv1, wk1, j * 128, tc, ps, pk)
                    zt, ztk = ARG[0], K("ARGz", 0)
                    S.act(zt[:], ps[:, :], AF.Silu, [pk], [ztk])
                    S.tt(YG[:, j, sl], YG[:, j, sl], zt[:], ALU.mult, [K("YG", j, tc), ztk], [K("YG", j, tc)])
            dump(f"yc{l}", YG[:], [K("YG", c, t) for c in range(2) for t in range(NTC)], [128, 2, S_])
            st.close()
            S.barrier()
            group_out(l, 2, YG, sty)
        S.barrier()
        if stop_after == f"C{l}":
            done = True
            break

        with ExitStack() as sty, ExitStack() as st:
            YG = sb("YG", [128, 2, S_], stack=sty)
            QA = sb("QA", [128, 4, S_], BF16, stack=st)
            KA = sb("KA", [128, 4, S_], BF16, stack=st)
            allqa = [K("QA", h_, t_) for h_ in range(4) for t_ in range(NTC)]
            allka = [K("KA", h_, t_) for h_ in range(4) for t_ in range(NTC)]
            S.add("dve", lambda e: e.memset(QA[:], 0.0), [], allqa, cost=4.5)
            S.add("dve", lambda e: e.memset(KA[:], 0.0), [], allka, cost=4.5)
            for h_ in range(4):
                o_ = 64 if h_ % 2 == 0 else 0
                S.dma(KA[o_:o_ + 8, h_, :], blkoh_d[:, :], allka, [K("KAoh", h_)], q="pool", arena=True)
            VB = sb("VB", [128, NT, 256], BF16, stack=st)
            CAUS = sb("CAUS", [128, 4, 512], BF16, stack=st)
            S.dma(CAUS[:], cmask_d[:, 4096:6144].rearrange("p (a b) -> p a b", a=4), [], [K("CAUS")], q="pool", arena=True)
            GC = sb("GC", [128, 3, 128], stack=st)
            S.dma(GC[:], gate_c_d[:, :].rearrange("p (a b) -> p a b", a=3), [], [K("GC")])
            KM = sb("KM", [128, 2, 8], stack=st)
            KMb = sb("KMb", [128, 2, 8], BF16, stack=st)
            wv, wk, _ = load_w(w_in_d[l, :, 2308:2820], 8, 512)
            wv2, wk2, _ = load_w(w_in_d[l, :, 2820:3076], 8, 256)
            str_ = ExitStack()
            ROPEb = [sb(f"ROPE{j}", [128, 2, TW], stack=str_) for j in range(1)]
            RAW = [sb(f"RAW{j}", [128, 512], stack=str_) for j in range(2)]
            TMP = [sb(f"TMPr{j}", [128, 512], stack=str_) for j in range(2)]
            KF = [sb(f"KF{j}", [128, 512], stack=str_) for j in range(2)]
            nr = 0
            for c2 in range(2):
                for tc in range(NTC):
                    sl = slice(tc * TW, (tc + 1) * TW)
                    rj = 0
                    ROPE = ROPEb[rj]
                    rsl = slice(0, TW)
                    S.dma(ROPE[:, 0, :], rope_d[:, tc * TW:(tc + 1) * TW], [], [K("ROPE", rj, 0)])
                    S.dma(ROPE[:, 1, :], rope_d[:, S_ + tc * TW:S_ + (tc + 1) * TW], [], [K("ROPE", rj, 1)])
                    for which in range(2):
                        ps, pk = ps_rot()
                        proj_fm(wv, wk, which * 256 + c2 * 128, tc, ps, pk)
                        raw, rawk = RAW[nr % 2], K("RAW", nr % 2)
                        tmp, tmpk = TMP[nr % 2], K("TMPr", nr % 2)
                        S.cp(raw[:], ps[:, :], [pk], [rawk], eng="act")
                        psw, pkw = ps_rot()
                        S.mm(psw[:, :], perm_f, raw[:], True, True, [rawk, K("c128")], [pkw])
                        S.tt(tmp[:], psw[:, :], ROPE[:, 1, rsl], ALU.mult, [pkw, K("ROPE", rj, 1)], [tmpk])
                        kf = KF[nr % 2]
                        dst, dk = kf[:], K("KF", nr % 2)
                        S.tt(dst, raw[:], ROPE[:, 0, rsl], ALU.mult, [rawk, K("ROPE", rj, 0)], [dk])
                        S.tt(dst, dst, tmp[:], ALU.add, [dk, tmpk], [dk])
                        if which == 0:
                            S.act(QA[0:64, 2 * c2, sl], kf[0:64, :], AF.Copy, [dk], [K("QA", 2 * c2, tc)], scale=0.125)
                            S.act(QA[64:128, 2 * c2 + 1, sl], kf[64:128, :], AF.Copy, [dk], [K("QA", 2 * c2 + 1, tc)],
                                  scale=0.125)
                        else:
                            S.cp(KA[0:64, 2 * c2, sl], kf[0:64, :], [dk], [K("KA", 2 * c2, tc)], eng="act")
                            S.cp(KA[64:128, 2 * c2 + 1, sl], kf[64:128, :], [dk], [K("KA", 2 * c2 + 1, tc)], eng="act")
                            for hb in range(2):
                                nblk = tc * 2 + hb
                                S.add("dve", (lambda e, o=KM[:, c2, nblk:nblk + 1], i_=kf[:, hb * 256:(hb + 1) * 256]:
                                              e.reduce_sum(out=o, in_=i_, axis=AX.X)), [dk], [K("KM", c2, nblk)])
                        nr += 1
            for i in range(NT):
                ps, pk = ps_rot()
                for kc in range(KC):
                    S.mm(ps[:, 0:256], hT[:, kc, i * 128:(i + 1) * 128], wv2[:, kc, :], kc == 0, kc == KC - 1,
                         wk2 + [K("hT", kc, i // 4)], [pk])
                S.cp(VB[:, i, :], ps[:, 0:256], [pk], [K("VB", i)], eng=("dve" if i % 2 else "act"))
            S.cp(KMb[:], KM[:], [K("KM", c2_, nb_) for c2_ in range(2) for nb_ in range(8)], [K("KMb")])
            str_.close()
            S.barrier()
            PTb = [sb(f"PTb{j}", [128, 512], BF16, stack=st) for j in range(3)]
            RD = [sb(f"RD{j}", [128, 512], stack=st) for j in range(2)]
            GT = sb("GT", [128, 128], stack=st)
            BT_ = sb("BIASTM", [128, 128], stack=st)
            MX = [sb(f"MX{j}", [128, 8], stack=st) for j in range(2)]
            for h in range(4):
                hh, c2 = h % 2, h // 2
                pr = slice(hh * 64, hh * 64 + 64)
                o_ = 64 if hh == 0 else 0
                psg, pkg = ps_rot()
                for i in range(NT):
                    S.mm(psg[:, i * 8:(i + 1) * 8], QA[pr, h, i * 128:(i + 1) * 128], KMb[pr, c2, :], True, True,
                         [K("QA", h, i // 4), K("KMb")], [pkg])
                S.stt(GT[:], psg[:, 0:128], 8.0 / 256.0, GC[:, 0, :], ALU.mult, ALU.add, [pkg, K("GC")], [K("GT")])
                for i in range(NT):
                    mx, mxk = MX[i % 2], K("MX", i % 2)
                    S.add("dve", (lambda e, o=mx[:], i_=GT[:, i * 8:(i + 1) * 8]: e.max(out=o, in_=i_)),
                          [K("GT")], [mxk])
                    bsl = BT_[:, i * 8:(i + 1) * 8]
                    S.ts(bsl, GT[:, i * 8:(i + 1) * 8], mx[:, 2:3], None, ALU.is_ge, None, [K("GT"), mxk],
                         [K("BIASTM", i)])
                    S.tt(bsl, bsl, GC[:, 1, i * 8:(i + 1) * 8], ALU.mult, [K("BIASTM", i), K("GC")], [K("BIASTM", i)])
                    S.tt(bsl, bsl, GC[:, 2, i * 8:(i + 1) * 8], ALU.add, [K("BIASTM", i), K("GC")], [K("BIASTM", i)])
                    S.ts(bsl, bsl, -1.0, -NEG, ALU.add, ALU.mult, [K("BIASTM", i)], [K("BIASTM", i)])
                for q4 in range(4):
                    pst, pkt = ps_rot()
                    for q in range(4):
                        i = q4 * 4 + q
                        S.tr(pst[0:8, q * 128:(q + 1) * 128], BT_[:, i * 8:(i + 1) * 8], ident,
                             [K("BIASTM", i), K("c128")], [pkt])
                    S.cp(QA[o_:o_ + 8, h, q4 * 512:(q4 + 1) * 512], pst[0:8, :], [pkt], [K("QAsel", h, q4)])
            its = []
            grp = 0
            for h in range(4):
                for c in range(NTC):
                    nkt = 4 * c + 4
                    for kt in range(nkt):
                        its.append(dict(h=h, c=c, kt=kt, nkt=nkt, grp=grp, i=len(its)))
                    grp += 1

            def m_s1(m):
                h, c, kt = m["h"], m["c"], m["kt"]
                d = kt - 4 * c
                qsl = slice(c * TW, (c + 1) * TW)
                ksl = slice(kt * 128, (kt + 1) * 128)
                off = 128 * max(d, 0)
                cs = slice(off, TW)
                qs = slice(c * TW + off, (c + 1) * TW)
                psS, pkS = ps_rot()
                S.mm(psS[:, cs], KA[:, h, ksl], QA[:, h, qs], True, d < 0,
                     [K("KA", h, kt // 4), K("KAoh", h), K("QA", h, c), K("QAsel", h, c)], [pkS])
                if d >= 0:
                    S.mm(psS[:, cs], ident_b, CAUS[:, d, cs], False, True, [K("CAUS"), K("c128b")], [pkS])
                pt, ptk = PTb[m["i"] % 3], K("PTb", m["i"] % 3)
                S.act(pt[:, cs], psS[:, cs], AF.Exp, [pkS], [ptk])

            def m_s2(m):
                h, c, kt, nkt = m["h"], m["c"], m["kt"], m["nkt"]
                hh, c2 = h % 2, h // 2
                pr = slice(hh * 64, hh * 64 + 64)
                qsl = slice(c * TW, (c + 1) * TW)
                g2 = m["grp"] % 2
                psO, pkO = PS[4 + g2], K("psO", g2)
                psD, pkD = PS[6 + g2], K("psD", g2)
                pt, ptk = PTb[m["i"] % 3], K("PTb", m["i"] % 3)
                off = 128 * max(kt - 4 * c, 0)
                cs = slice(off, TW)
                S.mm(psO[:, cs], VB[:, kt, c2 * 128:(c2 + 1) * 128], pt[:, cs], kt == 0, kt == nkt - 1,
                     [K("VB", kt), ptk], [pkO])
                S.mm(psD[:, cs], ones_b, pt[:, cs], kt == 0, kt == nkt - 1, [ptk, K("c128b")], [pkD])
                if kt == nkt - 1:
                    rd, rdk = RD[g2], K("RD", g2)
                    S.add("dve", (lambda e, o=rd[pr, :], i_=psD[pr, :]: e.reciprocal(out=o, in_=i_)), [pkD], [rdk], cost=2.2)
                    S.tt(YG[pr, c2, qsl], psO[pr, :], rd[pr, :], ALU.mult, [pkO, rdk], [K("YG", c2, c)])

            n_ = len(its)
            for r in range(-1, n_):
                if r + 1 < n_:
                    m_s1(its[r + 1])
                if r >= 0:
                    m_s2(its[r])
            dump(f"yd{l}", YG[:], [K("YG", c, t) for c in range(2) for t in range(NTC)], [128, 2, S_])
            st.close()
            S.barrier()
            group_out(l, 3, YG, sty)
        S.barrier()
        dump(f"x1_{l}", xT[:], ALLX, [128, KC, S_])
        if stop_after == f"D{l}":
            done = True
            break

        with ExitStack() as st:
            with ExitStack() as st2:
                rmsnorm_fm(xT, "xT", lambda kc: col(l, "xat_g", kc), hT, "hT", KC, S_, st2, tag="x")
            S.barrier()
            memT = sb("memT", [128, KC, 256], stack=st)
            MTb = sb("MTb", [128, KC, 256], BF16, stack=st)
            with ExitStack() as st2:
                load_fm(memT, mem_d, 2, "memT", st2)
                rmsnorm_fm(memT, "memT", lambda kc: col(l, "mem_g", kc), MTb, "MTb", KC, 256, st2, tag="m")
            S.barrier()
            KTb = sb("KTb", [128, KC, 256], BF16, stack=st)
            VX = sb("VX", [128, 2, D_], BF16, stack=st)
            QX = sb("QX", [128, KC, S_], BF16, stack=st)
            MTk = [K("MTb", kc, 0) for kc in range(KC)]
            for half in range(2):
                wv, wk, _ = load_w(wkv_d[l, :, half * 512:(half + 1) * 512], 8, 512)
                for o4 in range(4):
                    oc = half * 4 + o4
                    ps, pk = ps_rot()
                    for kc in range(KC):
                        S.mm(ps[:, 0:256], wv[:, kc, o4 * 128:(o4 + 1) * 128], MTb[:, kc, :], kc == 0, kc == KC - 1,
                             wk + [K("MTb", kc, 0)], [pk])
                    S.cp(KTb[:, oc, :], ps[:, 0:256], [pk], [K("KTb", oc)])
            for half in range(2):
                wv, wk, _ = load_w(wkv_d[l, :, 1024 + half * 512:1024 + (half + 1) * 512], 8, 512)
                for mt in range(2):
                    ps, pk = ps_rot()
                    for kc in range(KC):
                        S.mm(ps[:, :], MTb[:, kc, mt * 128:(mt + 1) * 128], wv[:, kc, :], kc == 0, kc == KC - 1,
                             wk + [K("MTb", kc, 0)], [pk])
                    S.cp(VX[:, mt, half * 512:(half + 1) * 512], ps[:, :], [pk], [K("VX", mt, half)], eng="act")
            for half in range(2):
                wv, wk, _ = load_w(wq_d[l, :, half * 512:(half + 1) * 512], 8, 512)
                for o4 in range(4):
                    oc = half * 4 + o4
                    for tc in range(NTC):
                        ps, pk = ps_rot()
                        proj_fm(wv, wk, o4 * 128, tc, ps, pk)
                        S.act(QX[:, oc, tc * TW:(tc + 1) * TW], ps[:, :], AF.Copy, [pk], [K("QX", oc, tc)],
                              scale=1.0 / 16.0)
            dump(f"X_MTb{l}", MTb[:], MTk, [128, KC, 256])
            dump(f"X_KTb{l}", KTb[:], [K("KTb", oc) for oc in range(KC)], [128, KC, 256])
            dump(f"X_QX{l}", QX[:], [K("QX", oc, tc) for oc in range(KC) for tc in range(NTC)], [128, KC, S_])
            dump(f"X_VX{l}", VX[:], [K("VX", mt, hf) for mt in range(2) for hf in range(2)], [128, 2, D_])
            PT = [sb(f"PTx{j}", [128, 512], BF16, stack=st) for j in range(4)]
            RDx = [sb(f"RDx{j}", [128, 512], stack=st) for j in range(2)]
            it = 0
            for hd in range(4):
                for tc in range(NTC):
                    sl = slice(tc * TW, (tc + 1) * TW)
                    pts = []
                    for mt in range(2):
                        ps, pk = ps_rot()
                        for j in range(2):
                            S.mm(ps[:, :], KTb[:, 2 * hd + j, mt * 128:(mt + 1) * 128], QX[:, 2 * hd + j, sl],
                                 j == 0, j == 1, [K("KTb", 2 * hd + j), K("QX", 2 * hd + j, tc)], [pk])
                        pt, ptk = PT[it % 4], K("PTx", it % 4)
                        S.act(pt[:], ps[:, :], AF.Exp, [pk], [ptk])
                        pts.append((pt, ptk))
                        it += 1
                    psD, pkD = ps_rot()
                    for mt in range(2):
                        S.mm(psD[:, :], ones_b, pts[mt][0][:], mt == 0, mt == 1, [pts[mt][1], K("c128b")], [pkD])
                    rd, rdk = RDx[(hd * NTC + tc) % 2], K("RDx", (hd * NTC + tc) % 2)
                    S.add("dve", (lambda e, o=rd[:], i_=psD[:, :]: e.reciprocal(out=o, in_=i_)), [pkD], [rdk], cost=2.2)
                    for j in range(2):
                        oc = 2 * hd + j
                        ps, pk = ps_rot()
                        for mt in range(2):
                            S.mm(ps[:, :], VX[:, mt, oc * 128:(oc + 1) * 128], pts[mt][0][:], mt == 0, mt == 1,
                                 [K("VX", mt, oc // 4), pts[mt][1]], [pk])
                        S.tt(hT[:, oc, sl], ps[:, :], rd[:], ALU.mult, [pk, rdk], [K("hT", oc, tc)])
            dump(f"X_OX{l}", hT[:], ALLH, [128, KC, S_])
            for half in range(2):
                wv, wk, _ = load_w(wo_d[l, :, half * 512:(half + 1) * 512], 8, 512)
                for o4 in range(4):
                    oc = half * 4 + o4
                    for tc in range(NTC):
                        ps, pk = ps_rot()
                        proj_fm(wv, wk, o4 * 128, tc, ps, pk)
                        S.tt(xT[:, oc, tc * TW:(tc + 1) * TW], xT[:, oc, tc * TW:(tc + 1) * TW], ps[:, :], ALU.add,
                             [K("xT", oc, tc), pk], [K("xT", oc, tc)])
        S.barrier()
        dump(f"x2_{l}", xT[:], ALLX, [128, KC, S_])
        if stop_after == f"X{l}":
            done = True
            break

        with ExitStack() as st:
            ENp = sb("ENp", [128, 16, 128], BF16, stack=st)
            S.add("dve", lambda e: e.memset(ENp[:], 0.0), [], [K("ENpz")])
            S.dma(ENp[0:16, :, :], en_d[:, :].rearrange("p (a b) -> p a b", a=16), [K("ENpz")], [K("ENf")], q="pool",
                  arena=True)
            CTh = sb("CTh", [128, S_], BF16, stack=st)
            CTl = sb("CTl", [128, S_], BF16, stack=st)
            S.add("dve", lambda e: e.memset(CTh[:], 0.0), [], [K("CThz")])
            S.add("dve", lambda e: e.memset(CTl[:], 0.0), [], [K("CTlz")])
            st_rt = ExitStack()
            WR = sb("WR", [128, KC, 20], stack=st_rt)
            S.dma(WR[:], wrt_d[l].rearrange("(kc p) n -> p kc n", p=128), [], [K("WR")])
            LG = sb("LG", [128, NT, 20], stack=st_rt)
            st_hf = ExitStack()
            HF = sb("HF", [128, KC, TW], stack=st_hf)

            def router_hook(tc, r):
                sl = slice(tc * TW, (tc + 1) * TW)
                for kc in range(KC):
                    S.stt(HF[:, kc, :], xT[:, kc, sl], col(l, "ffn_g", kc), r[:], ALU.mult, ALU.mult,
                          [K("xT", kc, tc), K("rsf", tc % 2), K("colp")], [K("HF", kc)])
                ps, pk = ps_rot()
                for q in range(4):
                    for kc in range(KC):
                        S.mm(ps[:, q * 20:(q + 1) * 20], HF[:, kc, q * 128:(q + 1) * 128], WR[:, kc, :], kc == 0,
                             kc == KC - 1, [K("HF", kc), K("WR")], [pk])
                S.cp(LG[:, tc * 4:tc * 4 + 4, :], ps[:, 0:80].rearrange("p (a b) -> p a b", a=4), [pk],
                     [K("LG", tc)])

            with ExitStack() as st2:
                rmsnorm_fm(xT, "xT", lambda kc: col(l, "ffn_g", kc), hT, "hT", KC, S_, st2, hook=router_hook, tag="f")
            st_hf.close()
            S.barrier()
            COMB = sb("COMB", [128, NT, 16], stack=st_rt)
            CT = sb("CT", [16, S_], stack=st_rt)
            sm = {nm: sb("rt_" + nm, [128, NT, w_] if w_ > 1 else [128, NT], stack=st_rt) for nm, w_ in
                  [("lg", 4), ("m", 1), ("eg", 4), ("sg", 1), ("oh", 4), ("le", 16), ("sel", 4), ("tmp", 4),
                   ("a", 1), ("eq", 4), ("sel2", 4), ("b", 1), ("t2", 4), ("ex", 4), ("dd", 1), ("wg", 4)]}

            def v(nm):
                return sm[nm][:]

            def kk(nm):
                return [K("rt", nm)]

            def B3(ap2, w_=4):
                return ap2.unsqueeze(2).to_broadcast([128, NT, w_])

            LGk = [K("LG", t) for t in range(NTC)]
            bgB = row(l, "bg").unsqueeze(1).to_broadcast([128, NT, 4])
            beB = row(l, "be").unsqueeze(1).to_broadcast([128, NT, 16])
            S.tt(v("lg"), LG[:, :, 0:4], bgB, ALU.add, LGk + [K("rowp")], kk("lg"))
            S.add("dve", lambda e: e.reduce_max(out=v("m"), in_=v("lg"), axis=AX.X), kk("lg"), kk("m"))
            S.tt(v("lg"), v("lg"), B3(v("m")), ALU.subtract, kk("lg") + kk("m"), kk("lg"))
            S.act(v("eg"), v("lg"), AF.Exp, kk("lg"), kk("eg"))
            S.add("dve", lambda e: e.reduce_sum(out=v("sg"), in_=v("eg"), axis=AX.X), kk("eg"), kk("sg"))
            S.add("dve", lambda e: e.reciprocal(out=v("sg"), in_=v("sg")), kk("sg"), kk("sg"))
            S.ts(v("oh"), v("lg"), 0.0, None, ALU.is_ge, None, kk("lg"), kk("oh"))
            S.tt(v("le"), LG[:, :, 4:20], beB, ALU.add, LGk + [K("rowp")], kk("le"))
            for g in range(4):
                ohg = sm["oh"][:, :, g:g + 1].to_broadcast([128, NT, 4])
                if g == 0:
                    S.tt(v("sel"), sm["le"][:, :, 0:4], ohg, ALU.mult, kk("le") + kk("oh"), kk("sel"))
                else:
                    S.tt(v("tmp"), sm["le"][:, :, 4 * g:4 * g + 4], ohg, ALU.mult, kk("le") + kk("oh"), kk("tmp"))
                    S.tt(v("sel"), v("sel"), v("tmp"), ALU.add, kk("sel") + kk("tmp"), kk("sel"))
            S.add("dve", lambda e: e.reduce_max(out=v("a"), in_=v("sel"), axis=AX.X), kk("sel"), kk("a"))
            S.tt(v("sel"), v("sel"), B3(v("a")), ALU.subtract, kk("sel") + kk("a"), kk("sel"))
            S.ts(v("eq"), v("sel"), 0.0, None, ALU.is_ge, None, kk("sel"), kk("eq"))
            S.stt(v("sel2"), v("eq"), -1e30, v("sel"), ALU.mult, ALU.add, kk("eq") + kk("sel"), kk("sel2"))
            S.add("dve", lambda e: e.reduce_max(out=v("b"), in_=v("sel2"), axis=AX.X), kk("sel2"), kk("b"))
            S.tt(v("t2"), v("sel"), B3(v("b")), ALU.is_ge, kk("sel") + kk("b"), kk("t2"))
            S.act(v("ex"), v("sel"), AF.Exp, kk("sel"), kk("ex"))
            S.act(v("dd"), v("b"), AF.Exp, kk("b"), kk("dd"))
            S.ts(v("dd"), v("dd"), 1.0, None, ALU.add, None, kk("dd"), kk("dd"))
            S.add("dve", lambda e: e.reciprocal(out=v("dd"), in_=v("dd")), kk("dd"), kk("dd"))
            S.tt(v("wg"), v("ex"), v("t2"), ALU.mult, kk("ex") + kk("t2"), kk("wg"))
            S.tt(v("wg"), v("wg"), B3(v("dd")), ALU.mult, kk("wg") + kk("dd"), kk("wg"))
            S.tt(v("wg"), v("wg"), B3(v("sg")), ALU.mult, kk("wg") + kk("sg"), kk("wg"))
            for g in range(4):
                ohg = sm["oh"][:, :, g:g + 1].to_broadcast([128, NT, 4])
                S.tt(COMB[:, :, 4 * g:4 * g + 4], v("wg"), ohg, ALU.mult, kk("wg") + kk("oh"),
                     [K("COMB", i, g) for i in range(NT)])
            for q4 in range(4):
                pst, pkt = ps_rot()
                for q in range(4):
                    i = q4 * 4 + q
                    S.tr(pst[0:16, q * 128:(q + 1) * 128], COMB[:, i, :], ident,
                         [K("COMB", i, g) for g in range(4)] + [K("c128")], [pkt])
                S.cp(CT[0:16, q4 * 512:(q4 + 1) * 512], pst[0:16, :], [pkt], [K("CT", q4)])
                S.cp(CTh[0:16, q4 * 512:(q4 + 1) * 512], CT[0:16, q4 * 512:(q4 + 1) * 512], [K("CT", q4), K("CThz")],
                     [K("CTh", q4)])
                S.tt(CT[0:16, q4 * 512:(q4 + 1) * 512], CT[0:16, q4 * 512:(q4 + 1) * 512],
                     CTh[0:16, q4 * 512:(q4 + 1) * 512], ALU.subtract, [K("CT", q4), K("CTh", q4)], [K("CT", q4)])
                S.cp(CTl[0:16, q4 * 512:(q4 + 1) * 512], CT[0:16, q4 * 512:(q4 + 1) * 512], [K("CT", q4), K("CTlz")],
                     [K("CTl", q4)])
            st_rt.close()
            S.barrier()
            HID = sb("HID", [128, 8, S_], BF16, stack=st)
            W2S = sb("W2S", [128, 8, D_], BF16, stack=st)
            CB = [sb(f"CB{j}", [128, 512], stack=st) for j in range(2)]
            SL = [sb(f"SL{j}", [128, 512], stack=st) for j in range(2)]
            n = 0
            for g in range(4):
                for el in range(4):
                    ex = 4 * g + el
                    wv, wk1_, slot = load_w(w1_d[l, ex], 8, 256, c0=0, tot=512)
                    _, wk3_, _ = load_w(w3_d[l, ex], 8, 256, slot=slot, c0=256, tot=512)
                    wk = wk1_ + wk3_
                    for tc in range(NTC):
                        sl = slice(tc * TW, (tc + 1) * TW)
                        psC, pkC = ps_rot()
                        S.mm(psC[:, :], ENp[:, ex, :], CTh[:, sl], True, False, [K("ENf"), K("CTh", tc), K("CThz")], [pkC])
                        S.mm(psC[:, :], ENp[:, ex, :], CTl[:, sl], False, True, [K("ENf"), K("CTl", tc), K("CTlz")], [pkC])
                        cb, cbk = CB[n % 2], K("CB", n % 2)
                        S.cp(cb[:], psC[:, :], [pkC], [cbk], eng="act")
                        for fc in range(2):
                            ps1, pk1 = ps_rot()
                            proj_fm(wv, wk, fc * 128, tc, ps1, pk1)
                            ps3, pk3 = ps_rot()
                            proj_fm(wv, wk, 256 + fc * 128, tc, ps3, pk3)
                            s_, sk = SL[(2 * n + fc) % 2], K("SL", (2 * n + fc) % 2)
                            S.act(s_[:], ps1[:, :], AF.Silu, [pk1], [sk])
                            S.tt(s_[:], s_[:], ps3[:, :], ALU.mult, [sk, pk3], [sk])
                            S.tt(HID[:, el * 2 + fc, sl], s_[:], cb[:], ALU.mult, [sk, cbk], [K("HID", el * 2 + fc, tc)])
                        n += 1
                    S.dma(W2S[:, 2 * el:2 * el + 2, :], w2_d[l, ex].rearrange("(fc p) n -> p fc n", p=128), [],
                          [K("W2S", el)], q="pool", arena=True)
                for oc in range(KC):
                    for tc in range(NTC):
                        ps, pk = ps_rot()
                        for j in range(8):
                            S.mm(ps[:, :], W2S[:, j, oc * 128:(oc + 1) * 128], HID[:, j, tc * TW:(tc + 1) * TW],
                                 j == 0, j == 7, [K("W2S", j // 2), K("HID", j, tc)], [pk])
                        S.tt(xT[:, oc, tc * TW:(tc + 1) * TW], xT[:, oc, tc * TW:(tc + 1) * TW], ps[:, :], ALU.add,
                             [K("xT", oc, tc), pk], [K("xT", oc, tc)])
        S.barrier()
        dump(f"x3_{l}", xT[:], ALLX, [128, KC, S_])
        if stop_after == f"M{l}":
            done = True
            break

    if stop_after is None:
        with ExitStack() as st:
            HF = sb("HFo", [128, KC, TW], stack=st)
            OB = [sb(f"OB{j}", [128, D_], stack=st) for j in range(2)]
            nob = [0]

            def final_hook(tc, r):
                sl = slice(tc * TW, (tc + 1) * TW)
                for kc in range(KC):
                    S.stt(HF[:, kc, :], xT[:, kc, sl], col(0, "fin_g", kc), r[:], ALU.mult, ALU.mult,
                          [K("xT", kc, tc), K("rsz", tc % 2), K("colp")], [K("HFo", kc)])
                for q in range(4):
                    ob, obk = OB[nob[0] % 2], K("OB", nob[0] % 2)
                    nob[0] += 1
                    for half in range(2):
                        ps, pk = ps_rot()
                        for j in range(4):
                            S.tr(ps[:, j * 128:(j + 1) * 128], HF[:, half * 4 + j, q * 128:(q + 1) * 128], ident,
                                 [K("HFo", half * 4 + j), K("c128")], [pk])
                        S.cp(ob[:, half * 512:(half + 1) * 512], ps[:, :], [pk], [obk],
                             eng=("act" if half else "dve"))
                    r0 = tc * TW + q * 128
                    i = S.dma(out_d[r0:r0 + 128, :], ob[:], [obk], [K("out", r0)])
                    S.final.append(i)

            rmsnorm_fm(xT, "xT", lambda kc: col(0, "fin_g", kc), None, None, KC, S_, st, hook=final_hook, tag="z")

    S.finalize(es)
    es.close()
    return nc, dump_d


def make_consts():
    c128 = np.zeros((128, 8, 128), np.float32)
    j = np.arange(128)[:, None]
    s = np.arange(128)[None, :]
    c128[:, 0] = np.eye(128)
    c128[:, 1] = 1.0
    c128[:, 2] = (j <= s)
    c128[:, 3] = -1.0 * (j >= s)
    c128[:, 4] = -1.0
    partner = np.where((np.arange(128) % 64) < 32, np.arange(128) + 32, np.arange(128) - 32)
    perm = np.zeros((128, 128), np.float32)
    perm[partner, np.arange(128)] = 1.0
    c128[:, 5] = perm
    c128[:, 6] = NEG * (s < j)
    cm = np.zeros((128, 12, 512), np.float32)
    p = np.arange(128)[:, None]
    f = np.arange(512)[None, :]
    for d in range(4):
        valid = (f - p) > d * 128
        cm[:, d] = valid
        cm[:, 4 + d] = NEG * (~valid)
        cm[:, 8 + d] = NEG * ((d * 128 + p) > f)
    half = 32
    inv = (np.float32(10000.0) ** (-(np.arange(half, dtype=np.float32)) / np.float32(half))).astype(np.float32)
    ang = np.arange(S_, dtype=np.float32)[None, :] * inv[:, None]
    cos = np.cos(ang).astype(np.float32)
    sin = np.sin(ang).astype(np.float32)
    rope = np.zeros((128, 2, S_), np.float32)
    for pp in range(128):
        d = pp % 64
        rope[pp, 0] = cos[d % 32]
        rope[pp, 1] = -sin[d % 32] if d < 32 else sin[d % 32]
    en = np.zeros((16, 16, 128), np.float32)
    for n in range(16):
        en[n, n, :] = 1.0
    gc = np.zeros((128, 3, 16, 8), np.float32)
    for i in range(16):
        own = i // 2
        for n in range(8):
            gc[:, 0, i, n] = 0.0 if n < own else -1e30
            gc[:, 1, i, n] = 1.0 if n < own else 0.0
            gc[:, 2, i, n] = 1.0 if n == own else 0.0
    blkoh = np.zeros((8, S_), np.float32)
    for n in range(8):
        blkoh[n, n * 256:(n + 1) * 256] = 1.0
    return dict(c128=c128.reshape(128, -1), cmask=cm.reshape(128, -1), rope=rope.reshape(128, -1),
                en=en.reshape(16, -1), gatec=gc.reshape(128, -1), blkoh=blkoh)


def colvec(v):
    v = np.asarray(v, np.float32)
    return np.ascontiguousarray(v.reshape(-1, 128).T)


def pack_params(inp):
    colp = np.zeros((128, L_, NCOL), np.float32)
    rowp = np.zeros((128, L_, NROW), np.float32)

    def put(l, name, arr):
        c0, w = COLS[name]
        assert arr.shape == (128, w), (name, arr.shape)
        colp[:, l, c0:c0 + w] = arr

    for l in range(L_):
        put(l, "mix_g", colvec(inp["mix_norm_g"][l]))
        put(l, "xat_g", colvec(inp["xattn_norm_g"][l]))
        put(l, "mem_g", colvec(inp["mem_norm_g"][l]))
        put(l, "ffn_g", colvec(inp["ffn_norm_g"][l]))
        put(l, "grp_g", colvec(inp["group_norm_g"][l]))
        cw = inp["lru_conv_w"][l]
        put(l, "lru_cw", np.concatenate([colvec(cw[k]) for k in range(4)], axis=1).reshape(128, 4, 2)
            .transpose(0, 2, 1).reshape(128, 8))
        put(l, "lru_cb", colvec(inp["lru_conv_b"][l]))
        put(l, "lru_br", colvec(inp["lru_br"][l]))
        put(l, "lru_bi", colvec(inp["lru_bi"][l]))
        put(l, "lru_lam", colvec(inp["lru_lambda"][l]))
        sw = inp["ssm_conv_w"][l]
        put(l, "ssm_cw", np.concatenate([colvec(sw[k]) for k in range(4)], axis=1).reshape(128, 4, 6)
            .transpose(0, 2, 1).reshape(128, 24))
        put(l, "ssm_cb", colvec(inp["ssm_conv_b"][l]))
        put(l, "ssm_d", colvec(np.repeat(inp["ssm_d"][l], 64)))
        put(l, "fin_g", colvec(inp["final_norm_g"]))
        r0, w = ROWS["dt_bias"]
        rowp[:, l, r0:r0 + w] = np.tile(inp["ssm_dt_bias"][l], 16)[None, :]
        r0, w = ROWS["a_log"]
        rowp[:, l, r0:r0 + w] = np.tile(inp["ssm_a_log"][l], 16)[None, :]
        r0, w = ROWS["bg"]
        rowp[:, l, r0:r0 + w] = inp["router_group_b"][l][None, :]
        r0, w = ROWS["be"]
        rowp[:, l, r0:r0 + w] = inp["router_expert_b"][l][None, :]
    wbd = np.zeros((L_, 2, 2, 128, 128), np.float32)
    for l in range(L_):
        for gi, nm in enumerate(("lru_wr", "lru_wi")):
            for c in range(2):
                for hh in range(2):
                    wbd[l, gi, c, hh * 64:(hh + 1) * 64, hh * 64:(hh + 1) * 64] = inp[nm][l, 2 * c + hh]
    router_w = np.concatenate([inp["router_group_w"], inp["router_expert_w"]], axis=2)
    return dict(colp=colp.reshape(128, -1), rowp=rowp.reshape(128, -1), lru_wbd=wbd,
                router_w=np.ascontiguousarray(router_w))


SHARED = ["w_in", "w_out", "xattn_wq", "xattn_wkv", "xattn_wo", "expert_w1", "expert_w3", "expert_w2"]


def make_in_maps(inputs, cores):
    inp = {k: np.asarray(v) for k, v in inputs.items()}
    consts = make_consts()
    packed = pack_params(inp)
    shared = {k: np.ascontiguousarray(inp[k], dtype=np.float32) for k in SHARED}
    maps = []
    for b in cores:
        m = dict(shared)
        m.update(consts)
        m.update(packed)
        m["x"] = np.ascontiguousarray(inp["x"][b])
        m["mem"] = np.ascontiguousarray(inp["mem"][b])
        maps.append(m)
    return maps


_CACHE = {}


def kernel(**inputs):
    if "nc" not in _CACHE:
        _CACHE["nc"] = build_program()[0]
    nc = _CACHE["nc"]
    maps = make_in_maps(inputs, list(range(8)))
    res = run_bass_kernel_spmd(nc, maps, core_ids=list(range(8)))
    return np.stack([r["out"] for r in res.results], axis=0).astype(np.float32)
```

```python
import numpy as np
from contextlib import ExitStack
import concourse.bass as bass
import concourse.mybir as mybir
from concourse.bass_utils import run_bass_kernel_spmd

F32 = mybir.dt.float32
BF16 = mybir.dt.bfloat16
F32R = mybir.dt.float32r
AF = mybir.ActivationFunctionType
ALU = mybir.AluOpType
AX = mybir.AxisListType

S_ = 2048
D_ = 1024
L_ = 2
KC = 8
NTC = 4
TW = 512
NT = 16
NEG = -30000.0
EPS = 1e-6

COLS = {}
_c = 0
for _n, _w in [("mix_g", 8), ("xat_g", 8), ("mem_g", 8), ("ffn_g", 8), ("grp_g", 8),
               ("lru_cw", 8), ("lru_cb", 2), ("lru_br", 2), ("lru_bi", 2), ("lru_lam", 2),
               ("ssm_cw", 24), ("ssm_cb", 6), ("ssm_d", 2), ("fin_g", 8)]:
    COLS[_n] = (_c, _w)
    _c += _w
NCOL = _c
ROWS = {}
_c = 0
for _n, _w in [("dt_bias", 64), ("a_log", 64), ("bg", 4), ("be", 16)]:
    ROWS[_n] = (_c, _w)
    _c += _w
NROW = _c


def _free(ap):
    n = 1
    for d in ap.shape[1:]:
        n *= int(d)
    return n


DEBUG_LINES = False


class Sched:
    ENG = ("pe", "act", "dve", "pool", "sp")
    GATED = ("pe", "act", "dve", "sp")

    def __init__(self, nc):
        self.nc = nc
        self.ops = []
        self.lastw = {}
        self.rd = {}
        self.by_eng = {e: [] for e in self.ENG}
        self.final = []
        self.psn = 0
        self.seg = 0
        self.act_inorder = set()

    def add(self, eng, fn, reads=(), writes=(), dma=False, cost=0.3, arena=False):
        i = len(self.ops)
        reads = list(reads)
        writes = list(writes)
        deps = {}
        for r in reads:
            w = self.lastw.get(r)
            if w is not None:
                deps.setdefault(w, set()).add("raw")
        for w_ in writes:
            w = self.lastw.get(w_)
            if w is not None:
                deps.setdefault(w, set()).add("waw")
            for r in self.rd.get(w_, ()):
                deps.setdefault(r, set()).add("war")
        ws = set(writes)
        for r in reads:
            if r not in ws:
                self.rd.setdefault(r, []).append(i)
        for w_ in writes:
            self.lastw[w_] = i
            self.rd[w_] = []
        deps.pop(i, None)
        fdeps = set()
        for d, kinds in deps.items():
            od = self.ops[d]
            if od["eng"] == eng and not od["dma"] and not dma:
                if eng == "pe":
                    continue
                if "raw" not in kinds:
                    continue
            fdeps.add(d)
        ln_ = 0
        if DEBUG_LINES:
            import sys as _sys
            f_ = _sys._getframe(1)
            while f_ is not None and f_.f_code.co_name != "build_program":
                f_ = f_.f_back
            ln_ = f_.f_lineno if f_ is not None else 0
        self.ops.append(dict(eng=eng, fn=fn, deps=fdeps, order=set(deps.keys()), dma=dma, signal=dma, token=None,
                             seg=self.seg, cost=cost, arena=arena, line=ln_))
        self.by_eng[eng].append(i)
        return i

    def barrier(self):
        self.seg += 1

    def schedule(self, W=24):
        ops = self.ops
        n = len(ops)
        succ = [[] for _ in range(n)]
        nun = [0] * n
        for i, op in enumerate(ops):
            nun[i] = len(op["order"])
            for d in op["order"]:
                succ[d].append(i)
        ready = [0.0] * n
        fin = [0.0] * n
        done = [False] * n
        head = {e: 0 for e in self.ENG}
        free = {e: 0.0 for e in self.ENG}
        lists = {e: list(self.by_eng[e]) for e in self.ENG}
        new_order = {e: [] for e in self.ENG}
        nseg = self.seg + 1
        rem = [0] * (nseg + 1)
        for i, op in enumerate(ops):
            if op["eng"] in self.GATED:
                rem[op["seg"]] += 1
        import os
        WDEF = {"pe": W, "act": W, "dve": W}
        WE = {e_: int(os.environ.get("KSCHED_W_" + e_.upper(), WDEF[e_])) for e_ in ("pe", "act", "dve")}
        segbusy = {}
        segend = {}
        cur = 0
        seg_t0 = 0.0
        tmax = 0.0
        nsched = 0
        while nsched < n:
            while cur < nseg and rem[cur] == 0:
                cur += 1
                seg_t0 = tmax
            best = None
            for e in self.ENG:
                lst = lists[e]
                h = head[e]
                while h < len(lst) and done[lst[h]]:
                    h += 1
                head[e] = h
                w = WE.get(e, 1)
                if e == "act" and cur in self.act_inorder:
                    w = 1
                seen = 0
                j = h
                while j < len(lst) and seen < w:
                    i = lst[j]
                    j += 1
                    if done[i]:
                        continue
                    op = ops[i]
                    if e != "pool":
                        if op["seg"] != cur:
                            break
                    elif op["arena"] and op["seg"] > cur:
                        break
                    seen += 1
                    if nun[i] == 0:
                        est = max(free[e], ready[i])
                        if e != "pool" or op["arena"]:
                            est = max(est, seg_t0)
                        key = (est, i)
                        if best is None or key < best[0]:
                            best = (key, e, i)
            if best is None:
                raise RuntimeError("scheduler stuck at seg %d" % cur)
            (est, _), e, i = best
            op = ops[i]
            if op["dma"]:
                free[e] = est + 0.06
                f = est + op["cost"]
            else:
                f = est + op["cost"]
                free[e] = f
            fin[i] = f
            tmax = max(tmax, f)
            if not op["dma"]:
                segbusy[(op["seg"], e)] = segbusy.get((op["seg"], e), 0.0) + op["cost"]
            segend[op["seg"]] = max(segend.get(op["seg"], 0.0), f)
            done[i] = True
            nsched += 1
            new_order[e].append(i)
            if e in self.GATED:
                rem[op["seg"]] -= 1
            for s_ in succ[i]:
                nun[s_] -= 1
                lat = 0.0 if (ops[s_]["eng"] == e and not op["dma"]) else 0.1
                if f + lat > ready[s_]:
                    ready[s_] = f + lat
        self.by_eng = new_order
        self.sim_time = tmax
        import os
        if os.environ.get("KSCHED_DEBUG"):
            print("sched: ops", n, "sim_time_us", round(tmax, 1), flush=True)
            prev = 0.0
            for sg in sorted(segend):
                print("  seg", sg, "dur", round(segend[sg] - prev, 1), {e_: round(segbusy.get((sg, e_), 0.0), 1)
                                                                         for e_ in ("pe", "act", "dve")})
                prev = segend[sg]
        pos = {}
        for e in self.ENG:
            for k, i in enumerate(new_order[e]):
                pos[i] = k
        bar = [set() for _ in range(nseg + 1)]
        for s_ in range(1, nseg + 1):
            for e in ("pe", "act", "dve"):
                last = None
                for i in new_order[e]:
                    if ops[i]["seg"] < s_:
                        last = i
                    else:
                        break
                if last is not None:
                    bar[s_].add(last)
            for i in new_order["sp"]:
                if ops[i]["seg"] == s_ - 1 and ops[i]["dma"]:
                    bar[s_].add(i)
        for e in self.GATED:
            lastseg = 0
            for i in new_order[e]:
                sg = ops[i]["seg"]
                if sg > lastseg:
                    for t in range(lastseg + 1, sg + 1):
                        ops[i]["deps"] |= bar[t]
                    lastseg = sg
        for i in new_order["pool"]:
            if ops[i]["arena"]:
                ops[i]["deps"] |= bar[ops[i]["seg"]]
        for i, op in enumerate(ops):
            keep = set()
            bestp = {}
            for d in op["deps"]:
                if d == i:
                    continue
                od = ops[d]
                if od["dma"]:
                    keep.add(d)
                else:
                    pe_ = od["eng"]
                    if pe_ not in bestp or pos[d] > pos[bestp[pe_]]:
                        bestp[pe_] = d
            keep |= set(bestp.values())
            op["deps"] = keep

    def mm(self, out, lhsT, rhs, start, stop, reads, writes):
        nfree = _free(rhs)
        c = max(nfree, 64) / 2400.0 * (4.0 if rhs.dtype == F32 else 1.0) + 0.004
        return self.add("pe", lambda e: e.matmul(out, lhsT, rhs, start=start, stop=stop), reads, writes, cost=c)

    def tr(self, out, in_, ident, reads, writes):
        return self.add("pe", lambda e: e.transpose(out, in_, ident), reads, writes, cost=0.25)

    def act(self, out, in_, func, reads, writes, bias=None, scale=None, accum=None):
        kw = {}
        if bias is not None:
            kw["bias"] = bias
        if scale is not None:
            kw["scale"] = scale
        if accum is not None:
            kw["accum_out"] = accum
        c = 0.22 + _free(out) / 1400.0
        return self.add("act", lambda e: e.activation(out=out, in_=in_, func=func, **kw), reads, writes, cost=c)

    def _vc(self, out):
        return 0.08 + _free(out) / 960.0

    def tt(self, out, in0, in1, op, reads, writes, eng="dve"):
        return self.add(eng, lambda e: e.tensor_tensor(out=out, in0=in0, in1=in1, op=op), reads, writes,
                        cost=self._vc(out))

    def ts(self, out, in0, s1, s2, op0, op1, reads, writes, eng="dve"):
        if s2 is None:
            return self.add(eng, lambda e: e.tensor_scalar(out, in0, s1, None, op0), reads, writes, cost=self._vc(out))
        return self.add(eng, lambda e: e.tensor_scalar(out, in0, s1, s2, op0, op1), reads, writes, cost=self._vc(out))

    def stt(self, out, in0, scalar, in1, op0, op1, reads, writes, eng="dve"):
        return self.add(eng, lambda e: e.scalar_tensor_tensor(out=out, in0=in0, scalar=scalar, in1=in1,
                                                             op0=op0, op1=op1), reads, writes, cost=self._vc(out))

    def cp(self, out, in_, reads, writes, eng="dve"):
        if eng == "act":
            return self.add("act", lambda e: e.activation(out=out, in_=in_, func=AF.Copy), reads, writes,
                            cost=0.22 + _free(out) / 1400.0)
        return self.add(eng, lambda e: e.tensor_copy(out=out, in_=in_), reads, writes, cost=self._vc(out))

    def recip(self, out, in_, reads, writes):
        return self.add("dve", lambda e: e.reciprocal(out=out, in_=in_), reads, writes, cost=0.1 + _free(out) / 240.0)

    def dma(self, out, in_, reads, writes, q="sp", arena=False):
        nbytes = 128 * _free(out) * 4
        return self.add(q, lambda e: e.dma_start(out=out, in_=in_), reads, writes, dma=True,
                        cost=2.0 + nbytes / 150e3, arena=arena)

    def finalize(self, es):
        nc = self.nc
        self.schedule()
        for op in self.ops:
            for d in op["deps"]:
                self.ops[d]["signal"] = True
        for d in self.final:
            self.ops[d]["signal"] = True
        csem = {e: es.enter_context(nc.semaphore("c_" + e)) for e in ("pe", "act", "dve", "pool")}
        NP = 16
        qsem = {q: [es.enter_context(nc.semaphore(f"q_{q}{k}")) for k in range(NP)] for q in ("sp", "pool")}
        cnt = {e: 0 for e in csem}
        qn = {q: 0 for q in qsem}
        quse = {q: [0] * NP for q in qsem}
        qprev = {q: [None] * NP for q in qsem}
        for i, op in enumerate(self.ops):
            if op["dma"]:
                q = op["eng"]
                k = qn[q] % NP
                qn[q] += 1
                quse[q][k] += 1
                op["token"] = (qsem[q][k], 16 * quse[q][k])
                if qprev[q][k] is not None:
                    op["deps"].add(qprev[q][k])
                qprev[q][k] = i
        for e in csem:
            for i in self.by_eng[e]:
                op = self.ops[i]
                if (not op["dma"]) and op["signal"]:
                    cnt[e] += 1
                    op["token"] = (csem[e], cnt[e])
        ops = self.ops
        final = list(self.final)

        def emit(engname, e):
            waited = {}
            for i in self.by_eng[engname]:
                op = ops[i]
                for d in sorted(op["deps"]):
                    sem, val = ops[d]["token"]
                    if waited.get(sem, 0) < val:
                        e.wait_ge(sem, val)
                        waited[sem] = val
                ins = op["fn"](e)
                if op["signal"]:
                    ins.then_inc(op["token"][0], 16 if op["dma"] else 1)
            if engname == "sp":
                for d in final:
                    sem, val = ops[d]["token"]
                    if waited.get(sem, 0) < val:
                        e.wait_ge(sem, val)
                        waited[sem] = val

        with nc.Block() as block:
            @block.tensor
            def _(e):
                emit("pe", e)

            @block.scalar
            def _(e):
                emit("act", e)

            @block.vector
            def _(e):
                emit("dve", e)

            @block.gpsimd
            def _(e):
                emit("pool", e)

            @block.sync
            def _(e):
                emit("sp", e)


def K(name, *idx):
    return (name,) + idx


def build_program(stop_after=None, dumps=(), n_layers=L_):
    nc = bass.Bass("TRN2", target_bir_lowering=False)
    S = Sched(nc)
    es = ExitStack()
    P = {}

    def din(name, shape):
        P[name] = nc.dram_tensor(name, list(shape), F32, kind="ExternalInput").ap()
        return P[name]

    x_d = din("x", [S_, D_])
    mem_d = din("mem", [256, D_])
    w_in_d = din("w_in", [L_, D_, 3076])
    w_out_d = din("w_out", [L_, D_, D_])
    wq_d = din("xattn_wq", [L_, D_, D_])
    wkv_d = din("xattn_wkv", [L_, D_, 2 * D_])
    wo_d = din("xattn_wo", [L_, D_, D_])
    w1_d = din("expert_w1", [L_, 16, D_, 256])
    w3_d = din("expert_w3", [L_, 16, D_, 256])
    w2_d = din("expert_w2", [L_, 16, 256, D_])
    wrt_d = din("router_w", [L_, D_, 20])
    lrubd_d = din("lru_wbd", [L_, 2, 2, 128, 128])
    colp_d = din("colp", [128, L_ * NCOL])
    rowp_d = din("rowp", [128, L_ * NROW])
    c128_d = din("c128", [128, 8 * 128])
    cmask_d = din("cmask", [128, 12 * 512])
    rope_d = din("rope", [128, 2 * S_])
    en_d = din("en", [16, 16 * 128])
    blkoh_d = din("blkoh", [8, S_])
    gate_c_d = din("gatec", [128, 3 * 128])
    out_d = nc.dram_tensor("out", [S_, D_], F32, kind="ExternalOutput").ap()
    dump_d = {}

    uid = [0]

    def sb(name, shape, dt=F32, stack=None):
        uid[0] += 1
        return (stack or es).enter_context(nc.sbuf_tensor(f"{name}_{uid[0]}", list(shape), dt))

    xT = sb("xT", [128, KC, S_])
    hT = sb("hT", [128, KC, S_], BF16)
    colp = sb("colp_s", [128, L_ * NCOL])
    rowp = sb("rowp_s", [128, L_ * NROW])
    c128 = sb("c128_s", [128, 8 * 128])
    c128b = sb("c128b_s", [128, 2 * 128], BF16)
    W = [sb(f"wslot{i}", [128, KC, 512], BF16) for i in range(3)]
    PS = [es.enter_context(nc.psum_tensor(f"ps{i}", [128, 512], F32)) for i in range(8)]
    wn = [0]

    ident = c128[:, 0:128]
    ones_f = c128[:, 128:256]
    tri_le = c128[:, 256:384]
    negu = c128[:, 384:512]
    negones = c128[:, 512:640]
    perm_f = c128[:, 640:768]
    ssdneg = c128[:, 768:896]
    ones_b = c128b[:, 128:256]
    ident_b = c128b[:, 0:128]

    cst = sb("cst", [128, 4])
    S.add("dve", lambda e: e.memset(cst[:, 0:1], EPS), [], [K("cst0")])
    S.add("dve", lambda e: e.memset(cst[:, 1:2], 1.0), [], [K("cst1")])
    S.add("dve", lambda e: e.memset(cst[:, 2:4], 0.0), [K("cst0"), K("cst1")], [K("cst")])
    S.dma(colp[:], colp_d[:, :], [], [K("colp")])
    S.dma(rowp[:], rowp_d[:, :], [], [K("rowp")])
    S.dma(c128[:], c128_d[:, :], [], [K("c128")])
    S.dma(c128b[:], c128_d[:, 0:256], [], [K("c128b")], q="pool")

    def col(l, name, j=0, n=1):
        c0, w = COLS[name]
        return colp[:, l * NCOL + c0 + j: l * NCOL + c0 + j + n]

    def row(l, name, j=0, n=None):
        c0, w = ROWS[name]
        n = w if n is None else n
        return rowp[:, l * NROW + c0 + j: l * NROW + c0 + j + n]

    nrot = [8]

    def ps_rot():
        k = S.psn % nrot[0]
        S.psn += 1
        return PS[k], K("ps", k)

    Wf = [w[:].rearrange("p a b -> p (a b)") for w in W]

    def load_w(src2d, nk, ncols, slot=None, c0=0, tot=None):
        tot = ncols if tot is None else tot
        if slot is None:
            k = wn[0] % 3
            wn[0] += 1
        else:
            k = slot
        v = Wf[k][:, 0:nk * tot].rearrange("p (a b) -> p a b", a=nk)
        step = 2 if nk >= 2 else 1
        allk = [K("w", k, j_, hf_) for j_ in range(4) for hf_ in range(2)]
        for h in range(0, nk, step):
            if nk == 8 and tot == 512:
                halves = [hf_ for hf_ in range(2) if c0 < (hf_ + 1) * 256 and c0 + ncols > hf_ * 256]
                wkeys_ = [K("w", k, h // 2, hf_) for hf_ in halves]
            else:
                wkeys_ = allk
            S.dma(v[:, h:h + step, c0:c0 + ncols],
                  src2d[h * 128:(h + step) * 128, :].rearrange("(kc p) n -> p kc n", p=128),
                  [], wkeys_, q="pool")
        return v, allk, k

    def dump(name, ap, reads, shape):
        if name in dumps:
            d = nc.dram_tensor("dbg_" + name, list(shape), ap.dtype, kind="ExternalOutput").ap()
            dump_d[name] = d
            i = S.dma(d, ap, reads, [K("dbg", name)])
            S.final.append(i)

    def load_fm(dst, src_d, ntile, dkey, st):
        xin = [sb(f"xin{j}", [128, D_], stack=st) for j in range(2)]
        for i in range(ntile):
            b = xin[i % 2]
            S.dma(b[:], src_d[i * 128:(i + 1) * 128, :], [], [K("xin", i % 2)])
            for half in range(2):
                ps, pk = ps_rot()
                for j in range(4):
                    kc = half * 4 + j
                    S.tr(ps[:, j * 128:(j + 1) * 128], b[:, kc * 128:(kc + 1) * 128], ident,
                         [K("xin", i % 2), K("c128")], [pk])
                S.cp(dst[:, half * 4:half * 4 + 4, i * 128:(i + 1) * 128],
                     ps[:].rearrange("p (a b) -> p a b", a=4), [pk],
                     [K(dkey, half * 4 + j, i // 4) for j in range(4)], eng=("dve" if half == 0 else "act"))

    st0 = ExitStack()
    load_fm(xT, x_d, NT, "xT", st0)

    def rmsnorm_fm(src, skey, gcol, dst, dkey, nk, T, st, dscale=1.0, hook=None, tag="n"):
        tw = min(T, TW)
        sq = [sb(f"sq_{tag}{j}", [128, nk, tw], BF16, stack=st) for j in range(2)]
        rs = [sb(f"rs_{tag}{j}", [128, tw], stack=st) for j in range(2)]
        for tc in range(T // tw):
            sl = slice(tc * tw, (tc + 1) * tw)
            q = sq[tc % 2]
            for kc in range(nk):
                S.act(q[:, kc, :], src[:, kc, sl], AF.Square, [K(skey, kc, tc)], [K("sq" + tag, tc % 2, kc)])
            ps, pk = ps_rot()
            for kc in range(nk):
                S.mm(ps[:, 0:tw], ones_b, q[:, kc, :], kc == 0, kc == nk - 1,
                     [K("sq" + tag, tc % 2, kc), K("c128b")], [pk])
            r = rs[tc % 2]
            S.act(r[:], ps[:, 0:tw], AF.Sqrt, [pk, K("cst")], [K("rs" + tag, tc % 2)],
                  bias=cst[:, 0:1], scale=1.0 / (nk * 128))
            S.add("dve", (lambda e, r=r: e.reciprocal(out=r[:], in_=r[:])),
                  [K("rs" + tag, tc % 2)], [K("rs" + tag, tc % 2)], cost=0.1 + tw / 240.0)
            if dst is not None:
                for kc in range(nk):
                    S.stt(dst[:, kc, sl], src[:, kc, sl], gcol(kc), r[:], ALU.mult, ALU.mult,
                          [K(skey, kc, tc), K("rs" + tag, tc % 2), K("colp")], [K(dkey, kc, tc)],
                          eng="dve")
            if hook is not None:
                hook(tc, r)

    ALLH = [K("hT", kc, tc) for kc in range(KC) for tc in range(NTC)]

    def hkeys(tc):
        return [K("hT", kc, tc) for kc in range(KC)]

    def proj_fm(wv, wkeys, c0, tc, ps, pk):
        for kc in range(KC):
            S.mm(ps[:, :], wv[:, kc, c0:c0 + 128], hT[:, kc, tc * TW:(tc + 1) * TW], kc == 0, kc == KC - 1,
                 wkeys + [K("hT", kc, tc)], [pk])

    def conv_chunk(ps, pk, tc, xw, xwk, cwcol, cbcol, dst, dkey):
        w_ = xw[tc % 2]
        wk = K(xwk, tc % 2)
        if tc == 0:
            S.add("dve", lambda e: e.memset(w_[:, 0:3], 0.0), [], [wk])
        else:
            S.cp(w_[:, 0:3], xw[(tc - 1) % 2][:, 512:515], [K(xwk, (tc - 1) % 2)], [wk])
        S.cp(w_[:, 3:515], ps[:, :], [pk], [wk], eng="act")
        S.ts(dst, w_[:, 3:515], cwcol(3), cbcol, ALU.mult, ALU.add, [wk, K("colp")], [dkey])
        for k in range(3):
            S.stt(dst, w_[:, k:k + 512], cwcol(k), dst, ALU.mult, ALU.add, [wk, dkey, K("colp")], [dkey])

    def group_out(l, g, YG, st):
        YB = sb("YB", [128, 2, S_], BF16, stack=st)
        rmsnorm_fm(YG, "YG", lambda kc: col(l, "grp_g", 2 * g + kc), YB, "YB", 2, S_, st, tag="g")
        dump(f"yn{l}_{g}", YB[:], [K("YB", c, t) for c in range(2) for t in range(NTC)], [128, 2, S_])
        for half in range(2):
            wv, wk, _ = load_w(w_out_d[l, g * 256:(g + 1) * 256, half * 512:(half + 1) * 512], 2, 512)
            for o4 in range(4):
                oc = half * 4 + o4
                for tc in range(NTC):
                    ps, pk = ps_rot()
                    for kc in range(2):
                        S.mm(ps[:, :], wv[:, kc, o4 * 128:(o4 + 1) * 128], YB[:, kc, tc * TW:(tc + 1) * TW],
                             kc == 0, kc == 1, wk + [K("YB", kc, tc)], [pk])
                    S.tt(xT[:, oc, tc * TW:(tc + 1) * TW], xT[:, oc, tc * TW:(tc + 1) * TW], ps[:, :], ALU.add,
                         [K("xT", oc, tc), pk], [K("xT", oc, tc)])

    ALLX = [K("xT", kc, tc) for kc in range(KC) for tc in range(NTC)]
    done = False
    for l in range(n_layers):
        with ExitStack() as st:
            rmsnorm_fm(xT, "xT", lambda kc: col(l, "mix_g", kc), hT, "hT", KC, S_, (st0 if l == 0 else st), tag="a")
        if l == 0:
            st0.close()
        S.barrier()
        if l == 0:
            dump("h0", hT[:], ALLH, [128, KC, S_])
        if stop_after == "norm0":
            break

        with ExitStack() as sty, ExitStack() as st:
            YG = sb("YG", [128, 2, S_], stack=sty)
            wv, wk, _ = load_w(w_in_d[l, :, 0:512], 8, 512)
            wbd = sb("wbd", [128, 4, 128], BF16, stack=st)
            S.dma(wbd[:], lrubd_d[l].rearrange("g c k m -> k (g c) m"), [], [K("wbd")], q="pool", arena=True)
            c8 = sb("c8", [128, 2], stack=st)
            dump(f"A_wbd{l}", wbd[:], [K("wbd")], [128, 4, 128])
            cy = sb("cy", [128, 2], stack=st)
            cz = sb("cz", [128, 2], stack=st)
            S.act(cy[:], col(l, "lru_lam", 0, 2), AF.Exp, [K("colp")], [K("cy")], scale=-1.0)
            S.ts(cz[:], cy[:], 2.0, None, ALU.add, None, [K("cy")], [K("cz")])
            S.add("dve", lambda e: e.reciprocal(out=cz[:], in_=cz[:]), [K("cz")], [K("cz")])
            S.tt(cz[:], cz[:], cy[:], ALU.mult, [K("cz"), K("cy")], [K("cz")])
            S.tt(cy[:], cz[:], cz[:], ALU.mult, [K("cz")], [K("cy")])
            S.ts(c8[:], cy[:], 1.0 / 9.0, 1.0 / 7.0, ALU.mult, ALU.add, [K("cy")], [K("c8")])
            for cc_ in (1.0 / 5.0, 1.0 / 3.0, 1.0):
                S.tt(c8[:], c8[:], cy[:], ALU.mult, [K("c8"), K("cy")], [K("c8")])
                S.ts(c8[:], c8[:], cc_, None, ALU.add, None, [K("c8")], [K("c8")])
            S.tt(c8[:], c8[:], cz[:], ALU.mult, [K("c8"), K("cz")], [K("c8")])
            S.ts(c8[:], c8[:], -16.0, None, ALU.mult, None, [K("c8")], [K("c8")])
            xw = [sb(f"xw{j}", [128, 515], stack=st) for j in range(2)]
            XC = sb("XC", [128, S_], stack=st)
            XCB = sb("XCB", [128, S_], BF16, stack=st)
            GG = sb("GG", [128, S_], stack=st)
            RA = sb("RA", [128, S_], stack=st)
            IU = sb("IU", [128, S_], stack=st)
            T2 = sb("T2", [128, S_], stack=st)
            XX = sb("XX", [128, S_], stack=st)
            for c in range(2):
                for tc in range(NTC):
                    sl = slice(tc * TW, (tc + 1) * TW)
                    ps, pk = ps_rot()
                    proj_fm(wv, wk, c * 128, tc, ps, pk)
                    conv_chunk(ps, pk, tc, xw, "xw", lambda k: col(l, "lru_cw", c * 4 + k), col(l, "lru_cb", c),
                               XC[:, sl], K("XC", tc))
                    ps, pk = ps_rot()
                    proj_fm(wv, wk, 256 + c * 128, tc, ps, pk)
                    S.cp(GG[:, sl], ps[:, :], [pk], [K("GG", tc)], eng="act")
                XCk = [K("XC", t) for t in range(NTC)]
                GGk = [K("GG", t) for t in range(NTC)]
                S.tt(T2[:], GG[:], GG[:], ALU.mult, GGk, [K("T2")])
                S.ts(T2[:], T2[:], 0.044715, 1.0, ALU.mult, ALU.add, [K("T2")], [K("T2")])
                S.tt(T2[:], T2[:], GG[:], ALU.mult, [K("T2")] + GGk, [K("T2")])
                S.act(T2[:], T2[:], AF.Sigmoid, [K("T2")], [K("T2")], scale=1.5957691216)
                S.tt(GG[:], GG[:], T2[:], ALU.mult, [K("T2")] + GGk, GGk)
                S.cp(XCB[:], XC[:], XCk, [K("XCB")], eng="act")
                for tc in range(NTC):
                    sl = slice(tc * TW, (tc + 1) * TW)
                    ps, pk = ps_rot()
                    S.mm(ps[:, :], wbd[:, c, :], XCB[:, sl], True, True, [K("wbd"), K("XCB")], [pk])
                    S.act(RA[:, sl], ps[:, :], AF.Sigmoid, [pk, K("colp")], [K("RA", tc)], bias=col(l, "lru_br", c))
                    ps, pk = ps_rot()
                    S.mm(ps[:, :], wbd[:, 2 + c, :], XCB[:, sl], True, True, [K("wbd"), K("XCB")], [pk])
                    S.act(IU[:, sl], ps[:, :], AF.Sigmoid, [pk, K("colp")], [K("IU", tc)], bias=col(l, "lru_bi", c))
                RAk = [K("RA", t) for t in range(NTC)]
                IUk = [K("IU", t) for t in range(NTC)]
                if c == 1:
                    dump(f"A_r{l}", RA[:], RAk, [128, S_])
                    dump(f"A_i{l}", IU[:], IUk, [128, S_])
                    dump(f"A_xcb{l}", XCB[:], [K("XCB")], [128, S_])
                    dump(f"A_xc{l}", XC[:], XCk, [128, S_])
                S.ts(XX[:], RA[:], c8[:, c:c + 1], 2.0, ALU.mult, ALU.mult, RAk + [K("c8")], [K("XX")])
                S.ts(T2[:], XX[:], 1.0 / 120.0, None, ALU.mult, None, [K("XX")], [K("T2")])
                for cc_ in (1.0 / 24.0, 1.0 / 6.0, 0.5, 1.0):
                    S.stt(T2[:], T2[:], cc_, XX[:], ALU.add, ALU.mult, [K("T2"), K("XX")], [K("T2")])
                S.act(T2[:], T2[:], AF.Sqrt, [K("T2")], [K("T2")], scale=-1.0)
                S.act(RA[:], XX[:], AF.Exp, [K("XX")], RAk, scale=0.5)
                S.tt(IU[:], IU[:], XC[:], ALU.mult, IUk + XCk, IUk)
                S.tt(IU[:], IU[:], T2[:], ALU.mult, IUk + [K("T2")], IUk)
                S.add("dve", lambda e: e.tensor_tensor_scan(out=XC[:], data0=RA[:], data1=IU[:], initial=0.0,
                                                            op0=ALU.mult, op1=ALU.add), RAk + IUk, XCk, cost=2.6)
                S.tt(YG[:, c, :], XC[:], GG[:], ALU.mult, XCk + GGk, [K("YG", c, t) for t in range(NTC)])
            for nm_, t_ in (("XC", XC), ("GG", GG), ("RA", RA), ("IU", IU), ("T2", T2)):
                dump(f"A_{nm_}{l}", t_[:], [K(nm_, t) for t in range(NTC)] + [K(nm_)], [128, S_])
            dump(f"ya{l}", YG[:], [K("YG", c, t) for c in range(2) for t in range(NTC)], [128, 2, S_])
            st.close()
            S.barrier()
            group_out(l, 0, YG, sty)
        S.barrier()
        if stop_after == f"A{l}":
            done = True
            break

        nrot[0] = 4
        with ExitStack() as sty, ExitStack() as st:
            YG = sb("YG", [128, 2, S_], stack=sty)
            QB = sb("QB", [128, 2, S_], BF16, stack=st)
            KZ = sb("KZ", [128, 4, S_], BF16, stack=st)
            S.add("dve", lambda e: e.memset(KZ[:], 0.0), [], [K("KZ", h_, t_) for h_ in range(4) for t_ in range(NTC)], cost=4.5)
            VB = sb("VB", [128, NT, 256], BF16, stack=st)
            M01 = sb("M01", [128, 4, 512], stack=st)
            NM = sb("NM", [128, 4, 512], BF16, stack=st)
            S.dma(M01[:], cmask_d[:, 0:2048].rearrange("p (a b) -> p a b", a=4), [], [K("M01")])
            S.dma(NM[:], cmask_d[:, 2048:4096].rearrange("p (a b) -> p a b", a=4), [], [K("NM")], q="pool", arena=True)
            wv, wk, _ = load_w(w_in_d[l, :, 512:1024], 8, 512)
            wv2, wk2, _ = load_w(w_in_d[l, :, 1024:1280], 8, 256)
            for c2 in range(2):
                for tc in range(NTC):
                    sl = slice(tc * TW, (tc + 1) * TW)
                    ps, pk = ps_rot()
                    proj_fm(wv, wk, c2 * 128, tc, ps, pk)
                    S.act(QB[:, c2, sl], ps[:, :], AF.Copy, [pk], [K("QB", c2, tc)], scale=0.125)
                    ps, pk = ps_rot()
                    proj_fm(wv, wk, 256 + c2 * 128, tc, ps, pk)
                    S.cp(KZ[0:64, 2 * c2, sl], ps[0:64, :], [pk], [K("KZ", 2 * c2, tc)])
                    S.cp(KZ[64:128, 2 * c2 + 1, sl], ps[64:128, :], [pk], [K("KZ", 2 * c2 + 1, tc)], eng="act")
            for i in range(NT):
                ps, pk = ps_rot()
                for kc in range(KC):
                    S.mm(ps[:, 0:256], hT[:, kc, i * 128:(i + 1) * 128], wv2[:, kc, :], kc == 0, kc == KC - 1,
                         wk2 + [K("hT", kc, i // 4)], [pk])
                S.cp(VB[:, i, :], ps[:, 0:256], [pk], [K("VB", i)], eng=("dve" if i % 2 else "act"))
            Eb = [sb(f"Eb{j}", [128, 512], stack=st) for j in range(2)]
            SPb = [sb(f"SPb{j}", [128, 512], F32R, stack=st) for j in range(3)]
            Wb = [sb(f"Wb{j}", [128, 512], BF16, stack=st) for j in range(2)]
            Rb = [sb(f"Rb{j}", [128, 512], F32R, stack=st) for j in range(2)]
            NUr = sb("NUr", [128, 2, 128], F32R, stack=st)
            S.cp(NUr[:, 0, :], negu, [K("c128")], [K("NUr")])
            S.cp(NUr[:, 1, :], negones, [K("c128")], [K("NUr")])
            items = []
            grp = 0
            for h in range(4):
                for c in range(NTC):
                    nb = 4 * c + 4
                    for idx, b in enumerate(range(nb - 1, -1, -1)):
                        items.append(dict(h=h, c=c, idx=idx, b=b, grp=grp, last=(b == 0)))
                    grp += 1
            for i_, itm in enumerate(items):
                itm["i"] = i_
                itm["pr"] = slice((itm["h"] % 2) * 64, (itm["h"] % 2) * 64 + 64)
                itm["c2"] = itm["h"] // 2
                itm["d"] = itm["b"] - 4 * itm["c"]
                itm["ksl"] = slice(itm["b"] * 128, (itm["b"] + 1) * 128)
                itm["qsl"] = slice(itm["c"] * TW, (itm["c"] + 1) * TW)
                itm["qk"] = [K("KZ", itm["h"], itm["b"] // 4), K("QB", itm["c2"], itm["c"])]

            def sb_s1(m):
                i = m["i"]
                pr, c2 = m["pr"], m["c2"]
                off = 128 * max(m["d"], 0)
                cs = slice(off, TW)
                qs = slice(m["c"] * TW + off, (m["c"] + 1) * TW)
                if m["idx"] == 0:
                    R0, R0k = Rb[m["grp"] % 2], K("Rb", m["grp"] % 2)
                    S.ts(R0[:], M01[:, 0, :], 0.0, None, ALU.mult, None, [K("M01")], [R0k])
                psZ, pkZ = ps_rot()
                S.mm(psZ[:, cs], KZ[:, m["h"], m["ksl"]], QB[:, c2, qs], True, True, m["qk"], [pkZ])
                E, Ek = Eb[i % 2], K("Eb", i % 2)
                SP, SPk = SPb[i % 3], K("SPb", i % 3)
                S.act(E[:, cs], psZ[:, cs], AF.Exp, [pkZ], [Ek])
                S.act(SP[:, cs], E[:, cs], AF.Ln, [Ek, K("cst")], [SPk], bias=cst[:, 1:2])
                if m["d"] >= 0:
                    S.tt(SP[:, cs], SP[:, cs].bitcast(F32), M01[:, m["d"], cs], ALU.mult, [SPk, K("M01")], [SPk])

            def sb_s2(m):
                i = m["i"]
                pr, c2, d = m["pr"], m["c2"], m["d"]
                off = 128 * max(d, 0)
                cs = slice(off, TW)
                qs = slice(m["c"] * TW + off, (m["c"] + 1) * TW)
                SP, SPk = SPb[i % 3], K("SPb", i % 3)
                Wt, Wk_ = Wb[i % 2], K("Wb", i % 2)
                R, Rk = Rb[m["grp"] % 2], K("Rb", m["grp"] % 2)
                psA, pkA = ps_rot()
                has_r = m["idx"] > 0
                S.mm(psA[:, cs], KZ[:, m["h"], m["ksl"]], QB[:, c2, qs], True, False, m["qk"], [pkA])
                S.mm(psA[:, cs], NUr[:, 0, :], SP[:, cs], False, (not has_r) and d < 0, [SPk, K("NUr")], [pkA])
                if has_r:
                    S.mm(psA[:, cs], NUr[:, 1, :], R[:, cs], False, d < 0, [Rk, K("NUr")], [pkA])
                if d >= 0:
                    S.mm(psA[:, cs], ident_b, NM[:, d, cs], False, True, [K("NM"), K("c128b")], [pkA])
                S.act(Wt[:, cs], psA[:, cs], AF.Exp, [pkA], [Wk_])
                if not m["last"]:
                    S.tt(R[:, cs], R[:, cs].bitcast(F32), SP[:, cs].bitcast(F32), ALU.add, [Rk, SPk], [Rk])

            def sb_s3(m):
                i = m["i"]
                pr, c2, h = m["pr"], m["c2"], m["h"]
                off = 128 * max(m["d"], 0)
                cs = slice(off, TW)
                Wt, Wk_ = Wb[i % 2], K("Wb", i % 2)
                psO, pkO = PS[4 + m["grp"] % 2], K("psO", m["grp"] % 2)
                S.mm(psO[:, cs], VB[:, m["b"], c2 * 128:(c2 + 1) * 128], Wt[:, cs], m["idx"] == 0, m["last"],
                     [K("VB", m["b"]), Wk_], [pkO])
                if m["last"]:
                    S.cp(YG[pr, c2, m["qsl"]], psO[pr, :], [pkO], [K("YG", c2, m["c"])], eng="act")

            n_it = len(items)
            for r in range(-2, n_it):
                if 0 <= r + 2 < n_it:
                    sb_s1(items[r + 2])
                if 0 <= r + 1 < n_it:
                    sb_s2(items[r + 1])
                if 0 <= r < n_it:
                    sb_s3(items[r])
            dump(f"yb{l}", YG[:], [K("YG", c, t) for c in range(2) for t in range(NTC)], [128, 2, S_])
            st.close()
            S.barrier()
            nrot[0] = 8
            group_out(l, 1, YG, sty)
        S.barrier()
        if stop_after == f"B{l}":
            done = True
            break

        nrot[0] = 4
        with ExitStack() as sty, ExitStack() as st:
            YG = sb("YG", [128, 2, S_], stack=sty)
            XS = sb("XS", [128, 2, S_], stack=st)
            BTb = sb("BTb", [128, 2, S_], BF16, stack=st)
            CTb = sb("CTb", [128, 2, S_], BF16, stack=st)
            BTM = sb("BTM", [128, NT, 256], BF16, stack=st)
            XTM = sb("XTM", [128, NT, 256], BF16, stack=st)
            wdt = sb("wdt", [128, KC, 4], BF16, stack=st)
            stc = ExitStack()
            xw = [sb(f"xwc{j}", [128, 515], stack=stc) for j in range(2)]
            CV = [sb(f"CV{j}", [128, 512], stack=stc) for j in range(2)]
            BF = [sb(f"BF{j}", [128, 512], stack=stc) for j in range(2)]
            S.dma(wdt[:], w_in_d[l, :, 2304:2308].rearrange("(kc p) n -> p kc n", p=128), [], [K("wdt")], q="pool", arena=True)
            wv1, wk1, _ = load_w(w_in_d[l, :, 1280:1792], 8, 512)
            wv2, wk2, _ = load_w(w_in_d[l, :, 1792:2304], 8, 512)
            n_cv = 0
            for j in range(6):
                if j < 2:
                    wv, wk, c0 = wv1, wk1, 256 + j * 128
                elif j < 4:
                    wv, wk, c0 = wv2, wk2, (j - 2) * 128
                else:
                    wv, wk, c0 = wv2, wk2, 256 + (j - 4) * 128
                for tc in range(NTC):
                    sl = slice(tc * TW, (tc + 1) * TW)
                    ps, pk = ps_rot()
                    proj_fm(wv, wk, c0, tc, ps, pk)
                    cv, cvk = CV[n_cv % 2], K("CV", n_cv % 2)
                    conv_chunk(ps, pk, tc, xw, "xwc", lambda k: col(l, "ssm_cw", j * 4 + k), col(l, "ssm_cb", j),
                               cv[:], cvk)
                    if j < 2:
                        S.act(XS[:, j, sl], cv[:], AF.Silu, [cvk], [K("XS", j, tc)])
                        ps, pk = ps_rot()
                        for q in range(4):
                            S.tr(ps[:, q * 128:(q + 1) * 128], XS[:, j, tc * TW + q * 128: tc * TW + (q + 1) * 128],
                                 ident, [K("XS", j, tc), K("c128")], [pk])
                        S.cp(XTM[:, tc * 4:tc * 4 + 4, j * 128:(j + 1) * 128],
                             ps[:].rearrange("p (a b) -> p a b", a=4), [pk], [K("XTM", tc, j)])
                    elif j < 4:
                        g = j - 2
                        bf, bfk = BF[n_cv % 2], K("BF", n_cv % 2)
                        S.act(bf[:], cv[:], AF.Silu, [cvk], [bfk])
                        S.cp(BTb[:, g, sl], bf[:], [bfk], [K("BTb", g, tc)])
                        ps, pk = ps_rot()
                        for q in range(4):
                            S.tr(ps[:, q * 128:(q + 1) * 128], bf[:, q * 128:(q + 1) * 128], ident,
                                 [bfk, K("c128")], [pk])
                        S.cp(BTM[:, tc * 4:tc * 4 + 4, g * 128:(g + 1) * 128],
                             ps[:].rearrange("p (a b) -> p a b", a=4), [pk], [K("BTM", tc, g)])
                    else:
                        S.act(CTb[:, j - 4, sl], cv[:], AF.Silu, [cvk], [K("CTb", j - 4, tc)])
                    n_cv += 1
            stc.close()
            S.barrier()
            DT = sb("DT", [128, 64], stack=st)
            AN = sb("AN", [128, 64], stack=st)
            AC = sb("AC", [128, 64], stack=st)
            ACS = sb("ACS", [128, 64], stack=st)
            TOT = sb("TOT", [128, 64], stack=st)
            DEC = sb("DEC", [128, 64], stack=st)
            ETOT = sb("ETOT", [128, 64], stack=st)
            DTD = sb("DTD", [128, 64], stack=st)
            psd, pkd = ps_rot()
            for i in range(NT):
                for kc in range(KC):
                    S.mm(psd[:, i * 4:(i + 1) * 4], hT[:, kc, i * 128:(i + 1) * 128], wdt[:, kc, :], kc == 0,
                         kc == KC - 1, [K("wdt"), K("hT", kc, i // 4)], [pkd])
            S.tt(DT[:], psd[:, 0:64], row(l, "dt_bias"), ALU.add, [pkd, K("rowp")], [K("DT")])
            S.act(DT[:], DT[:], AF.Exp, [K("DT")], [K("DT")])
            S.act(DT[:], DT[:], AF.Ln, [K("DT"), K("cst")], [K("DT")], bias=cst[:, 1:2])
            S.act(AN[:], row(l, "a_log"), AF.Exp, [K("rowp")], [K("AN")])
            S.ts(AN[:], AN[:], -1.0, None, ALU.mult, None, [K("AN")], [K("AN")])
            S.tt(AC[:], DT[:], AN[:], ALU.mult, [K("DT"), K("AN")], [K("AC")])
            ps, pk = ps_rot()
            S.mm(ps[:, 0:64], tri_le, AC[:], True, True, [K("AC"), K("c128")], [pk])
            S.cp(ACS[:], ps[:, 0:64], [pk], [K("ACS")])
            ps, pk = ps_rot()
            S.mm(ps[:, 0:64], ones_f, AC[:], True, True, [K("AC"), K("c128")], [pk])
            S.cp(TOT[:], ps[:, 0:64], [pk], [K("TOT")])
            S.tt(DEC[:], TOT[:], ACS[:], ALU.subtract, [K("TOT"), K("ACS")], [K("DEC")])
            S.act(DEC[:], DEC[:], AF.Exp, [K("DEC")], [K("DEC")])
            S.act(ETOT[:], TOT[:], AF.Exp, [K("TOT")], [K("ETOT")])
            S.tt(DTD[:], DT[:], DEC[:], ALU.mult, [K("DT"), K("DEC")], [K("DTD")])
            ST = sb("ST", [128, 4, 64], stack=st)
            S.add("dve", lambda e: e.memset(ST[:], 0.0), [], [K("ST", h) for h in range(4)])
            Rm = [sb(f"Rm{j}", [128, 128], stack=st) for j in range(2)]
            ARG = [sb(f"ARG{j}", [128, 128], stack=st) for j in range(2)]
            E1 = [sb(f"E1{j}", [128, 128], stack=st) for j in range(2)]
            GL = [sb(f"GL{j}", [128, 128], BF16, stack=st) for j in range(2)]
            CTp = [sb(f"CTp{j}", [128, 128], BF16, stack=st) for j in range(2)]
            XCp = [[sb(f"XCp{a}{j}", [128, 128], BF16, stack=st) for j in range(2)] for a in range(2)]
            STp = sb("STp", [128, 4, 128], BF16, stack=st)
            for a in range(2):
                for j in range(2):
                    S.add("dve", (lambda e, t_=XCp[a][j]: e.memset(t_[:], 0.0)), [], [K("XCp", a, j)])
            S.add("dve", lambda e: e.memset(STp[:], 0.0), [], [K("STp", h) for h in range(4)])
            XDh = [sb(f"XDh{j}", [128, 64], BF16, stack=st) for j in range(2)]
            n = 0
            for ci in range(NT):
                csl = slice(ci * 128, (ci + 1) * 128)
                tcx = ci // 4
                for g in range(2):
                    psG, pkG = ps_rot()
                    S.mm(psG[:, 0:128], BTb[:, g, csl], CTb[:, g, csl], True, True,
                         [K("BTb", g, tcx), K("CTb", g, tcx)], [pkG])
                    psY, pkY = PS[4 + (ci * 2 + g) % 2], K("psO", (ci * 2 + g) % 2)
                    for hh in range(2):
                        h = 2 * g + hh
                        pr = slice(hh * 64, hh * 64 + 64)
                        cidx = ci * 4 + h
                        k2 = n % 2
                        S.ts(Rm[k2][:], tri_le, AC[:, cidx:cidx + 1], None, ALU.mult, None,
                             [K("AC"), K("c128")], [K("Rm", k2)])
                        ps1, pk1 = ps_rot()
                        S.mm(ps1[:, 0:128], ones_f, Rm[k2][:], True, True, [K("Rm", k2), K("c128")], [pk1])
                        S.stt(ARG[k2][:], ps1[:, 0:128], ACS[:, cidx:cidx + 1], ssdneg, ALU.subtract, ALU.add,
                              [pk1, K("ACS"), K("c128")], [K("ARG", k2)])
                        S.act(ARG[k2][:], ARG[k2][:], AF.Exp, [K("ARG", k2)], [K("ARG", k2)])
                        S.tt(GL[k2][:], ARG[k2][:], psG[:, 0:128], ALU.mult, [K("ARG", k2), pkG], [K("GL", k2)])
                        S.act(E1[k2][:], ps1[:, 0:128], AF.Exp, [pk1, K("ARG", k2)], [K("E1", k2)])
                        S.tt(CTp[k2][:], CTb[:, g, csl], E1[k2][:], ALU.mult, [K("CTb", g, tcx), K("E1", k2)],
                             [K("CTp", k2)])
                        rot = (ci * 2 + g) % 2
                        xcp = XCp[hh][rot]
                        S.ts(xcp[:, hh * 64:(hh + 1) * 64], XTM[:, ci, h * 64:(h + 1) * 64], DT[:, cidx:cidx + 1], None,
                             ALU.mult, None, [K("XTM", tcx, g), K("DT")], [K("XCp", hh, rot)])
                        S.mm(psY[:, 0:128], xcp[:], GL[k2][:], hh == 0, False, [K("XCp", hh, rot), K("GL", k2)], [pkY])
                        S.mm(psY[:, 0:128], STp[:, h, :], CTp[k2][:], False, hh == 1, [K("STp", h), K("CTp", k2)], [pkY])
                        S.ts(XDh[k2][:], XTM[:, ci, h * 64:(h + 1) * 64], DTD[:, cidx:cidx + 1], None, ALU.mult, None,
                             [K("XTM", tcx, g), K("DTD")], [K("XDh", k2)])
                        psS, pkS = ps_rot()
                        S.mm(psS[:, 0:64], BTM[:, ci, g * 128:(g + 1) * 128], XDh[k2][:], True, True,
                             [K("BTM", tcx, g), K("XDh", k2)], [pkS])
                        S.stt(ST[:, h, :], ST[:, h, :], ETOT[:, cidx:cidx + 1], psS[:, 0:64], ALU.mult, ALU.add,
                              [K("ST", h), K("ETOT"), pkS], [K("ST", h)])
                        S.cp(STp[:, h, hh * 64:(hh + 1) * 64], ST[:, h, :], [K("ST", h)], [K("STp", h)], eng="act")
                        n += 1
                    S.cp(YG[:, g, csl], psY[:, 0:128], [pkY], [K("YG", g, tcx)], eng="act")
            ARG = [sb(f"ZT{j}", [128, 512], stack=st) for j in range(1)]
            for j in range(2):
                YGk = [K("YG", j, t) for t in range(NTC)]
                S.stt(YG[:, j, :], XS[:, j, :], col(l, "ssm_d", j), YG[:, j, :], ALU.mult, ALU.add,
                      [K("XS", j, t) for t in range(NTC)] + YGk + [K("colp")], YGk)
                for tc in range(NTC):
                    sl = slice(tc * TW, (tc + 1) * TW)
                    ps, pk = ps_rot()
                    proj_fm(wv1, wk1, j * 128, tc, ps, pk)
                    zt, ztk = ARG[0], K("ARGz", 0)
                    S.act(zt[:], ps[:, :], AF.Silu, [pk], [ztk])
                    S.tt(YG[:, j, sl], YG[:, j, sl], zt[:], ALU.mult, [K("YG", j, tc), ztk], [K("YG", j, tc)])
            dump(f"yc{l}", YG[:], [K("YG", c, t) for c in range(2) for t in range(NTC)], [128, 2, S_])
            st.close()
            S.barrier()
            nrot[0] = 8
            group_out(l, 2, YG, sty)
        S.barrier()
        if stop_after == f"C{l}":
            done = True
            break

        nrot[0] = 4
        with ExitStack() as sty, ExitStack() as st:
            YG = sb("YG", [128, 2, S_], stack=sty)
            QA = sb("QA", [128, 4, S_], BF16, stack=st)
            KA = sb("KA", [128, 4, S_], BF16, stack=st)
            allqa = [K("QA", h_, t_) for h_ in range(4) for t_ in range(NTC)]
            allka = [K("KA", h_, t_) for h_ in range(4) for t_ in range(NTC)]
            S.add("dve", lambda e: e.memset(QA[:], 0.0), [], allqa, cost=4.5)
            S.add("dve", lambda e: e.memset(KA[:], 0.0), [], allka, cost=4.5)
            for h_ in range(4):
                o_ = 64 if h_ % 2 == 0 else 0
                S.dma(KA[o_:o_ + 8, h_, :], blkoh_d[:, :], allka, [K("KAoh", h_)], q="pool", arena=True)
            VB = sb("VB", [128, NT, 256], BF16, stack=st)
            CAUS = sb("CAUS", [128, 4, 512], BF16, stack=st)
            S.dma(CAUS[:], cmask_d[:, 4096:6144].rearrange("p (a b) -> p a b", a=4), [], [K("CAUS")], q="pool", arena=True)
            GC = sb("GC", [128, 3, 128], stack=st)
            S.dma(GC[:], gate_c_d[:, :].rearrange("p (a b) -> p a b", a=3), [], [K("GC")])
            KM = sb("KM", [128, 2, 8], stack=st)
            KMb = sb("KMb", [128, 2, 8], BF16, stack=st)
            wv, wk, _ = load_w(w_in_d[l, :, 2308:2820], 8, 512)
            wv2, wk2, _ = load_w(w_in_d[l, :, 2820:3076], 8, 256)
            str_ = ExitStack()
            ROPEb = [sb(f"ROPE{j}", [128, 2, TW], stack=str_) for j in range(1)]
            RAW = [sb(f"RAW{j}", [128, 512], stack=str_) for j in range(2)]
            TMP = [sb(f"TMPr{j}", [128, 512], stack=str_) for j in range(2)]
            KF = [sb(f"KF{j}", [128, 512], stack=str_) for j in range(2)]
            nr = 0
            for c2 in range(2):
                for tc in range(NTC):
                    sl = slice(tc * TW, (tc + 1) * TW)
                    rj = 0
                    ROPE = ROPEb[rj]
                    rsl = slice(0, TW)
                    S.dma(ROPE[:, 0, :], rope_d[:, tc * TW:(tc + 1) * TW], [], [K("ROPE", rj, 0)])
                    S.dma(ROPE[:, 1, :], rope_d[:, S_ + tc * TW:S_ + (tc + 1) * TW], [], [K("ROPE", rj, 1)])
                    for which in range(2):
                        ps, pk = ps_rot()
                        proj_fm(wv, wk, which * 256 + c2 * 128, tc, ps, pk)
                        raw, rawk = RAW[nr % 2], K("RAW", nr % 2)
                        tmp, tmpk = TMP[nr % 2], K("TMPr", nr % 2)
                        S.cp(raw[:], ps[:, :], [pk], [rawk], eng="act")
                        psw, pkw = ps_rot()
                        S.mm(psw[:, :], perm_f, raw[:], True, True, [rawk, K("c128")], [pkw])
                        S.tt(tmp[:], psw[:, :], ROPE[:, 1, rsl], ALU.mult, [pkw, K("ROPE", rj, 1)], [tmpk])
                        kf = KF[nr % 2]
                        dst, dk = kf[:], K("KF", nr % 2)
                        S.tt(dst, raw[:], ROPE[:, 0, rsl], ALU.mult, [rawk, K("ROPE", rj, 0)], [dk])
                        S.tt(dst, dst, tmp[:], ALU.add, [dk, tmpk], [dk])
                        if which == 0:
                            S.act(QA[0:64, 2 * c2, sl], kf[0:64, :], AF.Copy, [dk], [K("QA", 2 * c2, tc)], scale=0.125)
                            S.act(QA[64:128, 2 * c2 + 1, sl], kf[64:128, :], AF.Copy, [dk], [K("QA", 2 * c2 + 1, tc)],
                                  scale=0.125)
                        else:
                            S.cp(KA[0:64, 2 * c2, sl], kf[0:64, :], [dk], [K("KA", 2 * c2, tc)], eng="act")
                            S.cp(KA[64:128, 2 * c2 + 1, sl], kf[64:128, :], [dk], [K("KA", 2 * c2 + 1, tc)], eng="act")
                            for hb in range(2):
                                nblk = tc * 2 + hb
                                S.add("dve", (lambda e, o=KM[:, c2, nblk:nblk + 1], i_=kf[:, hb * 256:(hb + 1) * 256]:
                                              e.reduce_sum(out=o, in_=i_, axis=AX.X)), [dk], [K("KM", c2, nblk)])
                        nr += 1
            for i in range(NT):
                ps, pk = ps_rot()
                for kc in range(KC):
                    S.mm(ps[:, 0:256], hT[:, kc, i * 128:(i + 1) * 128], wv2[:, kc, :], kc == 0, kc == KC - 1,
                         wk2 + [K("hT", kc, i // 4)], [pk])
                S.cp(VB[:, i, :], ps[:, 0:256], [pk], [K("VB", i)], eng=("dve" if i % 2 else "act"))
            S.cp(KMb[:], KM[:], [K("KM", c2_, nb_) for c2_ in range(2) for nb_ in range(8)], [K("KMb")])
            str_.close()
            S.barrier()
            PTb = [sb(f"PTb{j}", [128, 512], BF16, stack=st) for j in range(3)]
            RD = [sb(f"RD{j}", [128, 512], stack=st) for j in range(2)]
            GT = sb("GT", [128, 128], stack=st)
            BT_ = sb("BIASTM", [128, 128], stack=st)
            MX = [sb(f"MX{j}", [128, 8], stack=st) for j in range(2)]
            for h in range(4):
                hh, c2 = h % 2, h // 2
                pr = slice(hh * 64, hh * 64 + 64)
                o_ = 64 if hh == 0 else 0
                psg, pkg = ps_rot()
                for i in range(NT):
                    S.mm(psg[:, i * 8:(i + 1) * 8], QA[pr, h, i * 128:(i + 1) * 128], KMb[pr, c2, :], True, True,
                         [K("QA", h, i // 4), K("KMb")], [pkg])
                S.stt(GT[:], psg[:, 0:128], 8.0 / 256.0, GC[:, 0, :], ALU.mult, ALU.add, [pkg, K("GC")], [K("GT")])
                for i in range(NT):
                    mx, mxk = MX[i % 2], K("MX", i % 2)
                    S.add("dve", (lambda e, o=mx[:], i_=GT[:, i * 8:(i + 1) * 8]: e.max(out=o, in_=i_)),
                          [K("GT")], [mxk])
                    bsl = BT_[:, i * 8:(i + 1) * 8]
                    S.ts(bsl, GT[:, i * 8:(i + 1) * 8], mx[:, 2:3], None, ALU.is_ge, None, [K("GT"), mxk],
                         [K("BIASTM", i)])
                    S.tt(bsl, bsl, GC[:, 1, i * 8:(i + 1) * 8], ALU.mult, [K("BIASTM", i), K("GC")], [K("BIASTM", i)])
                    S.tt(bsl, bsl, GC[:, 2, i * 8:(i + 1) * 8], ALU.add, [K("BIASTM", i), K("GC")], [K("BIASTM", i)])
                    S.ts(bsl, bsl, -1.0, -NEG, ALU.add, ALU.mult, [K("BIASTM", i)], [K("BIASTM", i)])
                for q4 in range(4):
                    pst, pkt = ps_rot()
                    for q in range(4):
                        i = q4 * 4 + q
                        S.tr(pst[0:8, q * 128:(q + 1) * 128], BT_[:, i * 8:(i + 1) * 8], ident,
                             [K("BIASTM", i), K("c128")], [pkt])
                    S.cp(QA[o_:o_ + 8, h, q4 * 512:(q4 + 1) * 512], pst[0:8, :], [pkt], [K("QAsel", h, q4)])
            its = []
            grp = 0
            for h in range(4):
                for c in range(NTC):
                    nkt = 4 * c + 4
                    for kt in range(nkt):
                        its.append(dict(h=h, c=c, kt=kt, nkt=nkt, grp=grp, i=len(its)))
                    grp += 1

            def m_s1(m):
                h, c, kt = m["h"], m["c"], m["kt"]
                d = kt - 4 * c
                qsl = slice(c * TW, (c + 1) * TW)
                ksl = slice(kt * 128, (kt + 1) * 128)
                off = 128 * max(d, 0)
                cs = slice(off, TW)
                qs = slice(c * TW + off, (c + 1) * TW)
                psS, pkS = ps_rot()
                S.mm(psS[:, cs], KA[:, h, ksl], QA[:, h, qs], True, d < 0,
                     [K("KA", h, kt // 4), K("KAoh", h), K("QA", h, c), K("QAsel", h, c)], [pkS])
                if d >= 0:
                    S.mm(psS[:, cs], ident_b, CAUS[:, d, cs], False, True, [K("CAUS"), K("c128b")], [pkS])
                pt, ptk = PTb[m["i"] % 3], K("PTb", m["i"] % 3)
                S.act(pt[:, cs], psS[:, cs], AF.Exp, [pkS], [ptk])

            def m_s2(m):
                h, c, kt, nkt = m["h"], m["c"], m["kt"], m["nkt"]
                hh, c2 = h % 2, h // 2
                pr = slice(hh * 64, hh * 64 + 64)
                qsl = slice(c * TW, (c + 1) * TW)
                g2 = m["grp"] % 2
                psO, pkO = PS[4 + g2], K("psO", g2)
                psD, pkD = PS[6 + g2], K("psD", g2)
                pt, ptk = PTb[m["i"] % 3], K("PTb", m["i"] % 3)
                off = 128 * max(kt - 4 * c, 0)
                cs = slice(off, TW)
                S.mm(psO[:, cs], VB[:, kt, c2 * 128:(c2 + 1) * 128], pt[:, cs], kt == 0, kt == nkt - 1,
                     [K("VB", kt), ptk], [pkO])
                S.mm(psD[:, cs], ones_b, pt[:, cs], kt == 0, kt == nkt - 1, [ptk, K("c128b")], [pkD])
                if kt == nkt - 1:
                    rd, rdk = RD[g2], K("RD", g2)
                    S.add("dve", (lambda e, o=rd[pr, :], i_=psD[pr, :]: e.reciprocal(out=o, in_=i_)), [pkD], [rdk], cost=2.2)
                    S.tt(YG[pr, c2, qsl], psO[pr, :], rd[pr, :], ALU.mult, [pkO, rdk], [K("YG", c2, c)])

            n_ = len(its)
            for r in range(-1, n_):
                if r + 1 < n_:
                    m_s1(its[r + 1])
                if r >= 0:
                    m_s2(its[r])
            dump(f"yd{l}", YG[:], [K("YG", c, t) for c in range(2) for t in range(NTC)], [128, 2, S_])
            st.close()
            S.barrier()
            nrot[0] = 8
            group_out(l, 3, YG, sty)
        S.barrier()
        dump(f"x1_{l}", xT[:], ALLX, [128, KC, S_])
        if stop_after == f"D{l}":
            done = True
            break

        with ExitStack() as st:
            with ExitStack() as st2:
                rmsnorm_fm(xT, "xT", lambda kc: col(l, "xat_g", kc), hT, "hT", KC, S_, st2, tag="x")
            S.barrier()
            memT = sb("memT", [128, KC, 256], stack=st)
            MTb = sb("MTb", [128, KC, 256], BF16, stack=st)
            with ExitStack() as st2:
                load_fm(memT, mem_d, 2, "memT", st2)
                rmsnorm_fm(memT, "memT", lambda kc: col(l, "mem_g", kc), MTb, "MTb", KC, 256, st2, tag="m")
            S.barrier()
            KTb = sb("KTb", [128, KC, 256], BF16, stack=st)
            VX = sb("VX", [128, 2, D_], BF16, stack=st)
            QX = sb("QX", [128, KC, S_], BF16, stack=st)
            MTk = [K("MTb", kc, 0) for kc in range(KC)]
            for half in range(2):
                wv, wk, _ = load_w(wkv_d[l, :, half * 512:(half + 1) * 512], 8, 512)
                for o4 in range(4):
                    oc = half * 4 + o4
                    ps, pk = ps_rot()
                    for kc in range(KC):
                        S.mm(ps[:, 0:256], wv[:, kc, o4 * 128:(o4 + 1) * 128], MTb[:, kc, :], kc == 0, kc == KC - 1,
                             wk + [K("MTb", kc, 0)], [pk])
                    S.cp(KTb[:, oc, :], ps[:, 0:256], [pk], [K("KTb", oc)])
            for half in range(2):
                wv, wk, _ = load_w(wkv_d[l, :, 1024 + half * 512:1024 + (half + 1) * 512], 8, 512)
                for mt in range(2):
                    ps, pk = ps_rot()
                    for kc in range(KC):
                        S.mm(ps[:, :], MTb[:, kc, mt * 128:(mt + 1) * 128], wv[:, kc, :], kc == 0, kc == KC - 1,
                             wk + [K("MTb", kc, 0)], [pk])
                    S.cp(VX[:, mt, half * 512:(half + 1) * 512], ps[:, :], [pk], [K("VX", mt, half)], eng="act")
            for half in range(2):
                wv, wk, _ = load_w(wq_d[l, :, half * 512:(half + 1) * 512], 8, 512)
                for o4 in range(4):
                    oc = half * 4 + o4
                    for tc in range(NTC):
                        ps, pk = ps_rot()
                        proj_fm(wv, wk, o4 * 128, tc, ps, pk)
                        S.act(QX[:, oc, tc * TW:(tc + 1) * TW], ps[:, :], AF.Copy, [pk], [K("QX", oc, tc)],
                              scale=1.0 / 16.0)
            dump(f"X_MTb{l}", MTb[:], MTk, [128, KC, 256])
            dump(f"X_KTb{l}", KTb[:], [K("KTb", oc) for oc in range(KC)], [128, KC, 256])
            dump(f"X_QX{l}", QX[:], [K("QX", oc, tc) for oc in range(KC) for tc in range(NTC)], [128, KC, S_])
            dump(f"X_VX{l}", VX[:], [K("VX", mt, hf) for mt in range(2) for hf in range(2)], [128, 2, D_])
            PT = [sb(f"PTx{j}", [128, 512], BF16, stack=st) for j in range(4)]
            RDx = [sb(f"RDx{j}", [128, 512], stack=st) for j in range(2)]
            it = 0
            for hd in range(4):
                for tc in range(NTC):
                    sl = slice(tc * TW, (tc + 1) * TW)
                    pts = []
                    for mt in range(2):
                        ps, pk = ps_rot()
                        for j in range(2):
                            S.mm(ps[:, :], KTb[:, 2 * hd + j, mt * 128:(mt + 1) * 128], QX[:, 2 * hd + j, sl],
                                 j == 0, j == 1, [K("KTb", 2 * hd + j), K("QX", 2 * hd + j, tc)], [pk])
                        pt, ptk = PT[it % 4], K("PTx", it % 4)
                        S.act(pt[:], ps[:, :], AF.Exp, [pk], [ptk])
                        pts.append((pt, ptk))
                        it += 1
                    psD, pkD = ps_rot()
                    for mt in range(2):
                        S.mm(psD[:, :], ones_b, pts[mt][0][:], mt == 0, mt == 1, [pts[mt][1], K("c128b")], [pkD])
                    rd, rdk = RDx[(hd * NTC + tc) % 2], K("RDx", (hd * NTC + tc) % 2)
                    S.add("dve", (lambda e, o=rd[:], i_=psD[:, :]: e.reciprocal(out=o, in_=i_)), [pkD], [rdk], cost=2.2)
                    for j in range(2):
                        oc = 2 * hd + j
                        ps, pk = ps_rot()
                        for mt in range(2):
                            S.mm(ps[:, :], VX[:, mt, oc * 128:(oc + 1) * 128], pts[mt][0][:], mt == 0, mt == 1,
                                 [K("VX", mt, oc // 4), pts[mt][1]], [pk])
                        S.tt(hT[:, oc, sl], ps[:, :], rd[:], ALU.mult, [pk, rdk], [K("hT", oc, tc)])
            dump(f"X_OX{l}", hT[:], ALLH, [128, KC, S_])
            for half in range(2):
                wv, wk, _ = load_w(wo_d[l, :, half * 512:(half + 1) * 512], 8, 512)
                for o4 in range(4):
                    oc = half * 4 + o4
                    for tc in range(NTC):
                        ps, pk = ps_rot()
                        proj_fm(wv, wk, o4 * 128, tc, ps, pk)
                        S.tt(xT[:, oc, tc * TW:(tc + 1) * TW], xT[:, oc, tc * TW:(tc + 1) * TW], ps[:, :], ALU.add,
                             [K("xT", oc, tc), pk], [K("xT", oc, tc)])
        S.barrier()
        dump(f"x2_{l}", xT[:], ALLX, [128, KC, S_])
        if stop_after == f"X{l}":
            done = True
            break

        with ExitStack() as st:
            ENp = sb("ENp", [128, 16, 128], BF16, stack=st)
            S.add("dve", lambda e: e.memset(ENp[:], 0.0), [], [K("ENpz")])
            S.dma(ENp[0:16, :, :], en_d[:, :].rearrange("p (a b) -> p a b", a=16), [K("ENpz")], [K("ENf")], q="pool",
                  arena=True)
            CTh = sb("CTh", [128, S_], BF16, stack=st)
            CTl = sb("CTl", [128, S_], BF16, stack=st)
            S.add("dve", lambda e: e.memset(CTh[:], 0.0), [], [K("CThz")])
            S.add("dve", lambda e: e.memset(CTl[:], 0.0), [], [K("CTlz")])
            st_rt = ExitStack()
            WR = sb("WR", [128, KC, 20], stack=st_rt)
            S.dma(WR[:], wrt_d[l].rearrange("(kc p) n -> p kc n", p=128), [], [K("WR")])
            LG = sb("LG", [128, NT, 20], stack=st_rt)
            st_hf = ExitStack()
            HF = sb("HF", [128, KC, TW], stack=st_hf)

            def router_hook(tc, r):
                sl = slice(tc * TW, (tc + 1) * TW)
                for kc in range(KC):
                    S.stt(HF[:, kc, :], xT[:, kc, sl], col(l, "ffn_g", kc), r[:], ALU.mult, ALU.mult,
                          [K("xT", kc, tc), K("rsf", tc % 2), K("colp")], [K("HF", kc)])
                ps, pk = ps_rot()
                for q in range(4):
                    for kc in range(KC):
                        S.mm(ps[:, q * 20:(q + 1) * 20], HF[:, kc, q * 128:(q + 1) * 128], WR[:, kc, :], kc == 0,
                             kc == KC - 1, [K("HF", kc), K("WR")], [pk])
                S.cp(LG[:, tc * 4:tc * 4 + 4, :], ps[:, 0:80].rearrange("p (a b) -> p a b", a=4), [pk],
                     [K("LG", tc)])

            with ExitStack() as st2:
                rmsnorm_fm(xT, "xT", lambda kc: col(l, "ffn_g", kc), hT, "hT", KC, S_, st2, hook=router_hook, tag="f")
            st_hf.close()
            S.barrier()
            COMB = sb("COMB", [128, NT, 16], stack=st_rt)
            CT = sb("CT", [16, S_], stack=st_rt)
            sm = {nm: sb("rt_" + nm, [128, NT, w_] if w_ > 1 else [128, NT], stack=st_rt) for nm, w_ in
                  [("lg", 4), ("m", 1), ("eg", 4), ("sg", 1), ("oh", 4), ("le", 16), ("sel", 4), ("tmp", 4),
                   ("a", 1), ("eq", 4), ("sel2", 4), ("b", 1), ("t2", 4), ("ex", 4), ("dd", 1), ("wg", 4)]}

            def v(nm):
                return sm[nm][:]

            def kk(nm):
                return [K("rt", nm)]

            def B3(ap2, w_=4):
                return ap2.unsqueeze(2).to_broadcast([128, NT, w_])

            LGk = [K("LG", t) for t in range(NTC)]
            bgB = row(l, "bg").unsqueeze(1).to_broadcast([128, NT, 4])
            beB = row(l, "be").unsqueeze(1).to_broadcast([128, NT, 16])
            S.tt(v("lg"), LG[:, :, 0:4], bgB, ALU.add, LGk + [K("rowp")], kk("lg"))
            S.add("dve", lambda e: e.reduce_max(out=v("m"), in_=v("lg"), axis=AX.X), kk("lg"), kk("m"))
            S.tt(v("lg"), v("lg"), B3(v("m")), ALU.subtract, kk("lg") + kk("m"), kk("lg"))
            S.act(v("eg"), v("lg"), AF.Exp, kk("lg"), kk("eg"))
            S.add("dve", lambda e: e.reduce_sum(out=v("sg"), in_=v("eg"), axis=AX.X), kk("eg"), kk("sg"))
            S.add("dve", lambda e: e.reciprocal(out=v("sg"), in_=v("sg")), kk("sg"), kk("sg"))
            S.ts(v("oh"), v("lg"), 0.0, None, ALU.is_ge, None, kk("lg"), kk("oh"))
            S.tt(v("le"), LG[:, :, 4:20], beB, ALU.add, LGk + [K("rowp")], kk("le"))
            for g in range(4):
                ohg = sm["oh"][:, :, g:g + 1].to_broadcast([128, NT, 4])
                if g == 0:
                    S.tt(v("sel"), sm["le"][:, :, 0:4], ohg, ALU.mult, kk("le") + kk("oh"), kk("sel"))
                else:
                    S.tt(v("tmp"), sm["le"][:, :, 4 * g:4 * g + 4], ohg, ALU.mult, kk("le") + kk("oh"), kk("tmp"))
                    S.tt(v("sel"), v("sel"), v("tmp"), ALU.add, kk("sel") + kk("tmp"), kk("sel"))
            S.add("dve", lambda e: e.reduce_max(out=v("a"), in_=v("sel"), axis=AX.X), kk("sel"), kk("a"))
            S.tt(v("sel"), v("sel"), B3(v("a")), ALU.subtract, kk("sel") + kk("a"), kk("sel"))
            S.ts(v("eq"), v("sel"), 0.0, None, ALU.is_ge, None, kk("sel"), kk("eq"))
            S.stt(v("sel2"), v("eq"), -1e30, v("sel"), ALU.mult, ALU.add, kk("eq") + kk("sel"), kk("sel2"))
            S.add("dve", lambda e: e.reduce_max(out=v("b"), in_=v("sel2"), axis=AX.X), kk("sel2"), kk("b"))
            S.tt(v("t2"), v("sel"), B3(v("b")), ALU.is_ge, kk("sel") + kk("b"), kk("t2"))
            S.act(v("ex"), v("sel"), AF.Exp, kk("sel"), kk("ex"))
            S.act(v("dd"), v("b"), AF.Exp, kk("b"), kk("dd"))
            S.ts(v("dd"), v("dd"), 1.0, None, ALU.add, None, kk("dd"), kk("dd"))
            S.add("dve", lambda e: e.reciprocal(out=v("dd"), in_=v("dd")), kk("dd"), kk("dd"))
            S.tt(v("wg"), v("ex"), v("t2"), ALU.mult, kk("ex") + kk("t2"), kk("wg"))
            S.tt(v("wg"), v("wg"), B3(v("dd")), ALU.mult, kk("wg") + kk("dd"), kk("wg"))
            S.tt(v("wg"), v("wg"), B3(v("sg")), ALU.mult, kk("wg") + kk("sg"), kk("wg"))
            for g in range(4):
                ohg = sm["oh"][:, :, g:g + 1].to_broadcast([128, NT, 4])
                S.tt(COMB[:, :, 4 * g:4 * g + 4], v("wg"), ohg, ALU.mult, kk("wg") + kk("oh"),
                     [K("COMB", i, g) for i in range(NT)])
            for q4 in range(4):
                pst, pkt = ps_rot()
                for q in range(4):
                    i = q4 * 4 + q
                    S.tr(pst[0:16, q * 128:(q + 1) * 128], COMB[:, i, :], ident,
                         [K("COMB", i, g) for g in range(4)] + [K("c128")], [pkt])
                S.cp(CT[0:16, q4 * 512:(q4 + 1) * 512], pst[0:16, :], [pkt], [K("CT", q4)])
                S.cp(CTh[0:16, q4 * 512:(q4 + 1) * 512], CT[0:16, q4 * 512:(q4 + 1) * 512], [K("CT", q4), K("CThz")],
                     [K("CTh", q4)])
                S.tt(CT[0:16, q4 * 512:(q4 + 1) * 512], CT[0:16, q4 * 512:(q4 + 1) * 512],
                     CTh[0:16, q4 * 512:(q4 + 1) * 512], ALU.subtract, [K("CT", q4), K("CTh", q4)], [K("CT", q4)])
                S.cp(CTl[0:16, q4 * 512:(q4 + 1) * 512], CT[0:16, q4 * 512:(q4 + 1) * 512], [K("CT", q4), K("CTlz")],
                     [K("CTl", q4)])
            st_rt.close()
            S.barrier()
            HID = sb("HID", [128, 8, S_], BF16, stack=st)
            W2S = sb("W2S", [128, 8, D_], BF16, stack=st)
            CB = [sb(f"CB{j}", [128, 512], stack=st) for j in range(2)]
            SL = [sb(f"SL{j}", [128, 512], stack=st) for j in range(2)]
            n = 0
            for g in range(4):
                for el in range(4):
                    ex = 4 * g + el
                    wv, wk1_, slot = load_w(w1_d[l, ex], 8, 256, c0=0, tot=512)
                    _, wk3_, _ = load_w(w3_d[l, ex], 8, 256, slot=slot, c0=256, tot=512)
                    wk = wk1_ + wk3_
                    for tc in range(NTC):
                        sl = slice(tc * TW, (tc + 1) * TW)
                        psC, pkC = ps_rot()
                        S.mm(psC[:, :], ENp[:, ex, :], CTh[:, sl], True, False, [K("ENf"), K("CTh", tc), K("CThz")], [pkC])
                        S.mm(psC[:, :], ENp[:, ex, :], CTl[:, sl], False, True, [K("ENf"), K("CTl", tc), K("CTlz")], [pkC])
                        cb, cbk = CB[n % 2], K("CB", n % 2)
                        S.cp(cb[:], psC[:, :], [pkC], [cbk], eng="act")
                        for fc in range(2):
                            ps1, pk1 = ps_rot()
                            proj_fm(wv, wk, fc * 128, tc, ps1, pk1)
                            ps3, pk3 = ps_rot()
                            proj_fm(wv, wk, 256 + fc * 128, tc, ps3, pk3)
                            s_, sk = SL[(2 * n + fc) % 2], K("SL", (2 * n + fc) % 2)
                            S.act(s_[:], ps1[:, :], AF.Silu, [pk1], [sk])
                            S.tt(s_[:], s_[:], ps3[:, :], ALU.mult, [sk, pk3], [sk])
                            S.tt(HID[:, el * 2 + fc, sl], s_[:], cb[:], ALU.mult, [sk, cbk], [K("HID", el * 2 + fc, tc)])
                        n += 1
                    S.dma(W2S[:, 2 * el:2 * el + 2, :], w2_d[l, ex].rearrange("(fc p) n -> p fc n", p=128), [],
                          [K("W2S", el)], q="pool", arena=True)
                for oc in range(KC):
                    for tc in range(NTC):
                        ps, pk = ps_rot()
                        for j in range(8):
                            S.mm(ps[:, :], W2S[:, j, oc * 128:(oc + 1) * 128], HID[:, j, tc * TW:(tc + 1) * TW],
                                 j == 0, j == 7, [K("W2S", j // 2), K("HID", j, tc)], [pk])
                        S.tt(xT[:, oc, tc * TW:(tc + 1) * TW], xT[:, oc, tc * TW:(tc + 1) * TW], ps[:, :], ALU.add,
                             [K("xT", oc, tc), pk], [K("xT", oc, tc)])
        S.barrier()
        dump(f"x3_{l}", xT[:], ALLX, [128, KC, S_])
        if stop_after == f"M{l}":
            done = True
            break

    if stop_after is None:
        with ExitStack() as st:
            HF = sb("HFo", [128, KC, TW], stack=st)
            OB = [sb(f"OB{j}", [128, D_], stack=st) for j in range(2)]
            nob = [0]

            def final_hook(tc, r):
                sl = slice(tc * TW, (tc + 1) * TW)
                for kc in range(KC):
                    S.stt(HF[:, kc, :], xT[:, kc, sl], col(0, "fin_g", kc), r[:], ALU.mult, ALU.mult,
                          [K("xT", kc, tc), K("rsz", tc % 2), K("colp")], [K("HFo", kc)])
                for q in range(4):
                    ob, obk = OB[nob[0] % 2], K("OB", nob[0] % 2)
                    nob[0] += 1
                    for half in range(2):
                        ps, pk = ps_rot()
                        for j in range(4):
                            S.tr(ps[:, j * 128:(j + 1) * 128], HF[:, half * 4 + j, q * 128:(q + 1) * 128], ident,
                                 [K("HFo", half * 4 + j), K("c128")], [pk])
                        S.cp(ob[:, half * 512:(half + 1) * 512], ps[:, :], [pk], [obk],
                             eng=("act" if half else "dve"))
                    r0 = tc * TW + q * 128
                    i = S.dma(out_d[r0:r0 + 128, :], ob[:], [obk], [K("out", r0)])
                    S.final.append(i)

            rmsnorm_fm(xT, "xT", lambda kc: col(0, "fin_g", kc), None, None, KC, S_, st, hook=final_hook, tag="z")

    S.finalize(es)
    es.close()
    return nc, dump_d


def make_consts():
    c128 = np.zeros((128, 8, 128), np.float32)
    j = np.arange(128)[:, None]
    s = np.arange(128)[None, :]
    c128[:, 0] = np.eye(128)
    c128[:, 1] = 1.0
    c128[:, 2] = (j <= s)
    c128[:, 3] = -1.0 * (j >= s)
    c128[:, 4] = -1.0
    partner = np.where((np.arange(128) % 64) < 32, np.arange(128) + 32, np.arange(128) - 32)
    perm = np.zeros((128, 128), np.float32)
    perm[partner, np.arange(128)] = 1.0
    c128[:, 5] = perm
    c128[:, 6] = NEG * (s < j)
    cm = np.zeros((128, 12, 512), np.float32)
    p = np.arange(128)[:, None]
    f = np.arange(512)[None, :]
    for d in range(4):
        valid = (f - p) > d * 128
        cm[:, d] = valid
        cm[:, 4 + d] = NEG * (~valid)
        cm[:, 8 + d] = NEG * ((d * 128 + p) > f)
    half = 32
    inv = (np.float32(10000.0) ** (-(np.arange(half, dtype=np.float32)) / np.float32(half))).astype(np.float32)
    ang = np.arange(S_, dtype=np.float32)[None, :] * inv[:, None]
    cos = np.cos(ang).astype(np.float32)
    sin = np.sin(ang).astype(np.float32)
    rope = np.zeros((128, 2, S_), np.float32)
    for pp in range(128):
        d = pp % 64
        rope[pp, 0] = cos[d % 32]
        rope[pp, 1] = -sin[d % 32] if d < 32 else sin[d % 32]
    en = np.zeros((16, 16, 128), np.float32)
    for n in range(16):
        en[n, n, :] = 1.0
    gc = np.zeros((128, 3, 16, 8), np.float32)
    for i in range(16):
        own = i // 2
        for n in range(8):
            gc[:, 0, i, n] = 0.0 if n < own else -1e30
            gc[:, 1, i, n] = 1.0 if n < own else 0.0
            gc[:, 2, i, n] = 1.0 if n == own else 0.0
    blkoh = np.zeros((8, S_), np.float32)
    for n in range(8):
        blkoh[n, n * 256:(n + 1) * 256] = 1.0
    return dict(c128=c128.reshape(128, -1), cmask=cm.reshape(128, -1), rope=rope.reshape(128, -1),
                en=en.reshape(16, -1), gatec=gc.reshape(128, -1), blkoh=blkoh)


def colvec(v):
    v = np.asarray(v, np.float32)
    return np.ascontiguousarray(v.reshape(-1, 128).T)


def pack_params(inp):
    colp = np.zeros((128, L_, NCOL), np.float32)
    rowp = np.zeros((128, L_, NROW), np.float32)

    def put(l, name, arr):
        c0, w = COLS[name]
        assert arr.shape == (128, w), (name, arr.shape)
        colp[:, l, c0:c0 + w] = arr

    for l in range(L_):
        put(l, "mix_g", colvec(inp["mix_norm_g"][l]))
        put(l, "xat_g", colvec(inp["xattn_norm_g"][l]))
        put(l, "mem_g", colvec(inp["mem_norm_g"][l]))
        put(l, "ffn_g", colvec(inp["ffn_norm_g"][l]))
        put(l, "grp_g", colvec(inp["group_norm_g"][l]))
        cw = inp["lru_conv_w"][l]
        put(l, "lru_cw", np.concatenate([colvec(cw[k]) for k in range(4)], axis=1).reshape(128, 4, 2)
            .transpose(0, 2, 1).reshape(128, 8))
        put(l, "lru_cb", colvec(inp["lru_conv_b"][l]))
        put(l, "lru_br", colvec(inp["lru_br"][l]))
        put(l, "lru_bi", colvec(inp["lru_bi"][l]))
        put(l, "lru_lam", colvec(inp["lru_lambda"][l]))
        sw = inp["ssm_conv_w"][l]
        put(l, "ssm_cw", np.concatenate([colvec(sw[k]) for k in range(4)], axis=1).reshape(128, 4, 6)
            .transpose(0, 2, 1).reshape(128, 24))
        put(l, "ssm_cb", colvec(inp["ssm_conv_b"][l]))
        put(l, "ssm_d", colvec(np.repeat(inp["ssm_d"][l], 64)))
        put(l, "fin_g", colvec(inp["final_norm_g"]))
        r0, w = ROWS["dt_bias"]
        rowp[:, l, r0:r0 + w] = np.tile(inp["ssm_dt_bias"][l], 16)[None, :]
        r0, w = ROWS["a_log"]
        rowp[:, l, r0:r0 + w] = np.tile(inp["ssm_a_log"][l], 16)[None, :]
        r0, w = ROWS["bg"]
        rowp[:, l, r0:r0 + w] = inp["router_group_b"][l][None, :]
        r0, w = ROWS["be"]
        rowp[:, l, r0:r0 + w] = inp["router_expert_b"][l][None, :]
    wbd = np.zeros((L_, 2, 2, 128, 128), np.float32)
    for l in range(L_):
        for gi, nm in enumerate(("lru_wr", "lru_wi")):
            for c in range(2):
                for hh in range(2):
                    wbd[l, gi, c, hh * 64:(hh + 1) * 64, hh * 64:(hh + 1) * 64] = inp[nm][l, 2 * c + hh]
    router_w = np.concatenate([inp["router_group_w"], inp["router_expert_w"]], axis=2)
    return dict(colp=colp.reshape(128, -1), rowp=rowp.reshape(128, -1), lru_wbd=wbd,
                router_w=np.ascontiguousarray(router_w))


SHARED = ["w_in", "w_out", "xattn_wq", "xattn_wkv", "xattn_wo", "expert_w1", "expert_w3", "expert_w2"]


def make_in_maps(inputs, cores):
    inp = {k: np.asarray(v) for k, v in inputs.items()}
    consts = make_consts()
    packed = pack_params(inp)
    shared = {k: np.ascontiguousarray(inp[k], dtype=np.float32) for k in SHARED}
    maps = []
    for b in cores:
        m = dict(shared)
        m.update(consts)
        m.update(packed)
        m["x"] = np.ascontiguousarray(inp["x"][b])
        m["mem"] = np.ascontiguousarray(inp["mem"][b])
        maps.append(m)
    return maps


_CACHE = {}


def kernel(**inputs):
    if "nc" not in _CACHE:
        _CACHE["nc"] = build_program()[0]
    nc = _CACHE["nc"]
    maps = make_in_maps(inputs, list(range(8)))
    res = run_bass_kernel_spmd(nc, maps, core_ids=list(range(8)))
    return np.stack([r["out"] for r in res.results], axis=0).astype(np.float32)
```

```python
import numpy as np
from contextlib import ExitStack
import concourse.bass as bass
import concourse.mybir as mybir
from concourse.bass_utils import run_bass_kernel_spmd

F32 = mybir.dt.float32
BF16 = mybir.dt.bfloat16
F32R = mybir.dt.float32r
AF = mybir.ActivationFunctionType
ALU = mybir.AluOpType
AX = mybir.AxisListType

S_ = 2048
D_ = 1024
L_ = 2
KC = 8
NTC = 4
TW = 512
NT = 16
NEG = -30000.0
EPS = 1e-6

COLS = {}
_c = 0
for _n, _w in [("mix_g", 8), ("xat_g", 8), ("mem_g", 8), ("ffn_g", 8), ("grp_g", 8),
               ("lru_cw", 8), ("lru_cb", 2), ("lru_br", 2), ("lru_bi", 2), ("lru_lam", 2),
               ("ssm_cw", 24), ("ssm_cb", 6), ("ssm_d", 2), ("fin_g", 8)]:
    COLS[_n] = (_c, _w)
    _c += _w
NCOL = _c
ROWS = {}
_c = 0
for _n, _w in [("dt_bias", 64), ("a_log", 64), ("bg", 4), ("be", 16)]:
    ROWS[_n] = (_c, _w)
    _c += _w
NROW = _c


def _free(ap):
    n = 1
    for d in ap.shape[1:]:
        n *= int(d)
    return n


DEBUG_LINES = False


class Sched:
    ENG = ("pe", "act", "dve", "pool", "sp")
    GATED = ("pe", "act", "dve", "sp")

    def __init__(self, nc):
        self.nc = nc
        self.ops = []
        self.lastw = {}
        self.rd = {}
        self.by_eng = {e: [] for e in self.ENG}
        self.final = []
        self.psn = 0
        self.seg = 0
        self.act_inorder = set()

    def add(self, eng, fn, reads=(), writes=(), dma=False, cost=0.3, arena=False):
        i = len(self.ops)
        reads = list(reads)
        writes = list(writes)
        deps = {}
        for r in reads:
            w = self.lastw.get(r)
            if w is not None:
                deps.setdefault(w, set()).add("raw")
        for w_ in writes:
            w = self.lastw.get(w_)
            if w is not None:
                deps.setdefault(w, set()).add("waw")
            for r in self.rd.get(w_, ()):
                deps.setdefault(r, set()).add("war")
        ws = set(writes)
        for r in reads:
            if r not in ws:
                self.rd.setdefault(r, []).append(i)
        for w_ in writes:
            self.lastw[w_] = i
            self.rd[w_] = []
        deps.pop(i, None)
        fdeps = set()
        for d, kinds in deps.items():
            od = self.ops[d]
            if od["eng"] == eng and not od["dma"] and not dma:
                if eng == "pe":
                    continue
                if "raw" not in kinds:
                    continue
            fdeps.add(d)
        ln_ = 0
        if DEBUG_LINES:
            import sys as _sys
            f_ = _sys._getframe(1)
            while f_ is not None and f_.f_code.co_name != "build_program":
                f_ = f_.f_back
            ln_ = f_.f_lineno if f_ is not None else 0
        self.ops.append(dict(eng=eng, fn=fn, deps=fdeps, order=set(deps.keys()), dma=dma, signal=dma, token=None,
                             seg=self.seg, cost=cost, arena=arena, line=ln_))
        self.by_eng[eng].append(i)
        return i

    def barrier(self):
        self.seg += 1

    def schedule(self, W=24):
        ops = self.ops
        n = len(ops)
        succ = [[] for _ in range(n)]
        nun = [0] * n
        for i, op in enumerate(ops):
            nun[i] = len(op["order"])
            for d in op["order"]:
                succ[d].append(i)
        ready = [0.0] * n
        fin = [0.0] * n
        done = [False] * n
        head = {e: 0 for e in self.ENG}
        free = {e: 0.0 for e in self.ENG}
        lists = {e: list(self.by_eng[e]) for e in self.ENG}
        new_order = {e: [] for e in self.ENG}
        nseg = self.seg + 1
        rem = [0] * (nseg + 1)
        for i, op in enumerate(ops):
            if op["eng"] in self.GATED:
                rem[op["seg"]] += 1
        import os
        WDEF = {"pe": W, "act": W, "dve": W}
        WE = {e_: int(os.environ.get("KSCHED_W_" + e_.upper(), WDEF[e_])) for e_ in ("pe", "act", "dve")}
        segbusy = {}
        segend = {}
        cur = 0
        seg_t0 = 0.0
        tmax = 0.0
        nsched = 0
        while nsched < n:
            while cur < nseg and rem[cur] == 0:
                cur += 1
                seg_t0 = tmax
            best = None
            for e in self.ENG:
                lst = lists[e]
                h = head[e]
                while h < len(lst) and done[lst[h]]:
                    h += 1
                head[e] = h
                w = WE.get(e, 1)
                if e == "act" and cur in self.act_inorder:
                    w = 1
                seen = 0
                j = h
                while j < len(lst) and seen < w:
                    i = lst[j]
                    j += 1
                    if done[i]:
                        continue
                    op = ops[i]
                    if e != "pool":
                        if op["seg"] != cur:
                            break
                    elif op["arena"] and op["seg"] > cur:
                        break
                    seen += 1
                    if nun[i] == 0:
                        est = max(free[e], ready[i])
                        if e != "pool" or op["arena"]:
                            est = max(est, seg_t0)
                        key = (est, i)
                        if best is None or key < best[0]:
                            best = (key, e, i)
            if best is None:
                raise RuntimeError("scheduler stuck at seg %d" % cur)
            (est, _), e, i = best
            op = ops[i]
            if op["dma"]:
                free[e] = est + 0.06
                f = est + op["cost"]
            else:
                f = est + op["cost"]
                free[e] = f
            fin[i] = f
            tmax = max(tmax, f)
            if not op["dma"]:
                segbusy[(op["seg"], e)] = segbusy.get((op["seg"], e), 0.0) + op["cost"]
            segend[op["seg"]] = max(segend.get(op["seg"], 0.0), f)
            done[i] = True
            nsched += 1
            new_order[e].append(i)
            if e in self.GATED:
                rem[op["seg"]] -= 1
            for s_ in succ[i]:
                nun[s_] -= 1
                lat = 0.0 if (ops[s_]["eng"] == e and not op["dma"]) else 0.1
                if f + lat > ready[s_]:
                    ready[s_] = f + lat
        self.by_eng = new_order
        self.sim_time = tmax
        import os
        if os.environ.get("KSCHED_DEBUG"):
            print("sched: ops", n, "sim_time_us", round(tmax, 1), flush=True)
            prev = 0.0
            for sg in sorted(segend):
                print("  seg", sg, "dur", round(segend[sg] - prev, 1), {e_: round(segbusy.get((sg, e_), 0.0), 1)
                                                                         for e_ in ("pe", "act", "dve")})
                prev = segend[sg]
        pos = {}
        for e in self.ENG:
            for k, i in enumerate(new_order[e]):
                pos[i] = k
        bar = [set() for _ in range(nseg + 1)]
        for s_ in range(1, nseg + 1):
            for e in ("pe", "act", "dve"):
                last = None
                for i in new_order[e]:
                    if ops[i]["seg"] < s_:
                        last = i
                    else:
                        break
                if last is not None:
                    bar[s_].add(last)
            for i in new_order["sp"]:
                if ops[i]["seg"] == s_ - 1 and ops[i]["dma"]:
                    bar[s_].add(i)
        for e in self.GATED:
            lastseg = 0
            for i in new_order[e]:
                sg = ops[i]["seg"]
                if sg > lastseg:
                    for t in range(lastseg + 1, sg + 1):
                        ops[i]["deps"] |= bar[t]
                    lastseg = sg
        for i in new_order["pool"]:
            if ops[i]["arena"]:
                ops[i]["deps"] |= bar[ops[i]["seg"]]
        for i, op in enumerate(ops):
            keep = set()
            bestp = {}
            for d in op["deps"]:
                if d == i:
                    continue
                od = ops[d]
                if od["dma"]:
                    keep.add(d)
                else:
                    pe_ = od["eng"]
                    if pe_ not in bestp or pos[d] > pos[bestp[pe_]]:
                        bestp[pe_] = d
            keep |= set(bestp.values())
            op["deps"] = keep

    def mm(self, out, lhsT, rhs, start, stop, reads, writes):
        nfree = _free(rhs)
        c = max(nfree, 64) / 2400.0 * (4.0 if rhs.dtype == F32 else 1.0) + 0.004
        return self.add("pe", lambda e: e.matmul(out, lhsT, rhs, start=start, stop=stop), reads, writes, cost=c)

    def tr(self, out, in_, ident, reads, writes):
        return self.add("pe", lambda e: e.transpose(out, in_, ident), reads, writes, cost=0.25)

    def act(self, out, in_, func, reads, writes, bias=None, scale=None, accum=None):
        kw = {}
        if bias is not None:
            kw["bias"] = bias
        if scale is not None:
            kw["scale"] = scale
        if accum is not None:
            kw["accum_out"] = accum
        c = 0.22 + _free(out) / 1400.0
        return self.add("act", lambda e: e.activation(out=out, in_=in_, func=func, **kw), reads, writes, cost=c)

    def _vc(self, out):
        return 0.08 + _free(out) / 960.0

    def tt(self, out, in0, in1, op, reads, writes, eng="dve"):
        return self.add(eng, lambda e: e.tensor_tensor(out=out, in0=in0, in1=in1, op=op), reads, writes,
                        cost=self._vc(out))

    def ts(self, out, in0, s1, s2, op0, op1, reads, writes, eng="dve"):
        if s2 is None:
            return self.add(eng, lambda e: e.tensor_scalar(out, in0, s1, None, op0), reads, writes, cost=self._vc(out))
        return self.add(eng, lambda e: e.tensor_scalar(out, in0, s1, s2, op0, op1), reads, writes, cost=self._vc(out))

    def stt(self, out, in0, scalar, in1, op0, op1, reads, writes, eng="dve"):
        return self.add(eng, lambda e: e.scalar_tensor_tensor(out=out, in0=in0, scalar=scalar, in1=in1,
                                                             op0=op0, op1=op1), reads, writes, cost=self._vc(out))

    def cp(self, out, in_, reads, writes, eng="dve"):
        if eng == "act":
            return self.add("act", lambda e: e.activation(out=out, in_=in_, func=AF.Copy), reads, writes,
                            cost=0.22 + _free(out) / 1400.0)
        return self.add(eng, lambda e: e.tensor_copy(out=out, in_=in_), reads, writes, cost=self._vc(out))

    def recip(self, out, in_, reads, writes):
        return self.add("dve", lambda e: e.reciprocal(out=out, in_=in_), reads, writes, cost=0.1 + _free(out) / 240.0)

    def dma(self, out, in_, reads, writes, q="sp", arena=False):
        nbytes = 128 * _free(out) * 4
        return self.add(q, lambda e: e.dma_start(out=out, in_=in_), reads, writes, dma=True,
                        cost=2.0 + nbytes / 150e3, arena=arena)

    def finalize(self, es):
        nc = self.nc
        self.schedule()
        for op in self.ops:
            for d in op["deps"]:
                self.ops[d]["signal"] = True
        for d in self.final:
            self.ops[d]["signal"] = True
        csem = {e: es.enter_context(nc.semaphore("c_" + e)) for e in ("pe", "act", "dve", "pool")}
        NP = 16
        qsem = {q: [es.enter_context(nc.semaphore(f"q_{q}{k}")) for k in range(NP)] for q in ("sp", "pool")}
        cnt = {e: 0 for e in csem}
        qn = {q: 0 for q in qsem}
        quse = {q: [0] * NP for q in qsem}
        qprev = {q: [None] * NP for q in qsem}
        for i, op in enumerate(self.ops):
            if op["dma"]:
                q = op["eng"]
                k = qn[q] % NP
                qn[q] += 1
                quse[q][k] += 1
                op["token"] = (qsem[q][k], 16 * quse[q][k])
                if qprev[q][k] is not None:
                    op["deps"].add(qprev[q][k])
                qprev[q][k] = i
        for e in csem:
            for i in self.by_eng[e]:
                op = self.ops[i]
                if (not op["dma"]) and op["signal"]:
                    cnt[e] += 1
                    op["token"] = (csem[e], cnt[e])
        ops = self.ops
        final = list(self.final)

        def emit(engname, e):
            waited = {}
            for i in self.by_eng[engname]:
                op = ops[i]
                for d in sorted(op["deps"]):
                    sem, val = ops[d]["token"]
                    if waited.get(sem, 0) < val:
                        e.wait_ge(sem, val)
                        waited[sem] = val
                ins = op["fn"](e)
                if op["signal"]:
                    ins.then_inc(op["token"][0], 16 if op["dma"] else 1)
            if engname == "sp":
                for d in final:
                    sem, val = ops[d]["token"]
                    if waited.get(sem, 0) < val:
                        e.wait_ge(sem, val)
                        waited[sem] = val

        with nc.Block() as block:
            @block.tensor
            def _(e):
                emit("pe", e)

            @block.scalar
            def _(e):
                emit("act", e)

            @block.vector
            def _(e):
                emit("dve", e)

            @block.gpsimd
            def _(e):
                emit("pool", e)

            @block.sync
            def _(e):
                emit("sp", e)


def K(name, *idx):
    return (name,) + idx


def build_program(stop_after=None, dumps=(), n_layers=L_):
    nc = bass.Bass("TRN2", target_bir_lowering=False)
    S = Sched(nc)
    es = ExitStack()
    P = {}

    def din(name, shape):
        P[name] = nc.dram_tensor(name, list(shape), F32, kind="ExternalInput").ap()
        return P[name]

    x_d = din("x", [S_, D_])
    mem_d = din("mem", [256, D_])
    w_in_d = din("w_in", [L_, D_, 3076])
    w_out_d = din("w_out", [L_, D_, D_])
    wq_d = din("xattn_wq", [L_, D_, D_])
    wkv_d = din("xattn_wkv", [L_, D_, 2 * D_])
    wo_d = din("xattn_wo", [L_, D_, D_])
    w1_d = din("expert_w1", [L_, 16, D_, 256])
    w3_d = din("expert_w3", [L_, 16, D_, 256])
    w2_d = din("expert_w2", [L_, 16, 256, D_])
    wrt_d = din("router_w", [L_, D_, 20])
    lrubd_d = din("lru_wbd", [L_, 2, 2, 128, 128])
    colp_d = din("colp", [128, L_ * NCOL])
    rowp_d = din("rowp", [128, L_ * NROW])
    c128_d = din("c128", [128, 8 * 128])
    cmask_d = din("cmask", [128, 12 * 512])
    rope_d = din("rope", [128, 2 * S_])
    en_d = din("en", [16, 16 * 128])
    blkoh_d = din("blkoh", [8, S_])
    gate_c_d = din("gatec", [128, 3 * 128])
    out_d = nc.dram_tensor("out", [S_, D_], F32, kind="ExternalOutput").ap()
    dump_d = {}

    uid = [0]

    def sb(name, shape, dt=F32, stack=None):
        uid[0] += 1
        return (stack or es).enter_context(nc.sbuf_tensor(f"{name}_{uid[0]}", list(shape), dt))

    xT = sb("xT", [128, KC, S_])
    hT = sb("hT", [128, KC, S_], BF16)
    colp = sb("colp_s", [128, L_ * NCOL])
    rowp = sb("rowp_s", [128, L_ * NROW])
    c128 = sb("c128_s", [128, 8 * 128])
    c128b = sb("c128b_s", [128, 2 * 128], BF16)
    W = [sb(f"wslot{i}", [128, KC, 512], BF16) for i in range(3)]
    PS = [es.enter_context(nc.psum_tensor(f"ps{i}", [128, 512], F32)) for i in range(8)]
    wn = [0]

    ident = c128[:, 0:128]
    ones_f = c128[:, 128:256]
    tri_le = c128[:, 256:384]
    negu = c128[:, 384:512]
    negones = c128[:, 512:640]
    perm_f = c128[:, 640:768]
    ssdneg = c128[:, 768:896]
    ones_b = c128b[:, 128:256]
    ident_b = c128b[:, 0:128]

    cst = sb("cst", [128, 4])
    S.add("dve", lambda e: e.memset(cst[:, 0:1], EPS), [], [K("cst0")])
    S.add("dve", lambda e: e.memset(cst[:, 1:2], 1.0), [], [K("cst1")])
    S.add("dve", lambda e: e.memset(cst[:, 2:4], 0.0), [K("cst0"), K("cst1")], [K("cst")])
    S.dma(colp[:], colp_d[:, :], [], [K("colp")])
    S.dma(rowp[:], rowp_d[:, :], [], [K("rowp")])
    S.dma(c128[:], c128_d[:, :], [], [K("c128")])
    S.dma(c128b[:], c128_d[:, 0:256], [], [K("c128b")], q="pool")

    def col(l, name, j=0, n=1):
        c0, w = COLS[name]
        return colp[:, l * NCOL + c0 + j: l * NCOL + c0 + j + n]

    def row(l, name, j=0, n=None):
        c0, w = ROWS[name]
        n = w if n is None else n
        return rowp[:, l * NROW + c0 + j: l * NROW + c0 + j + n]

    nrot = [8]

    def ps_rot():
        k = S.psn % nrot[0]
        S.psn += 1
        return PS[k], K("ps", k)

    Wf = [w[:].rearrange("p a b -> p (a b)") for w in W]

    def load_w(src2d, nk, ncols, slot=None, c0=0, tot=None):
        tot = ncols if tot is None else tot
        if slot is None:
            k = wn[0] % 3
            wn[0] += 1
        else:
            k = slot
        v = Wf[k][:, 0:nk * tot].rearrange("p (a b) -> p a b", a=nk)
        step = 2 if nk >= 2 else 1
        allk = [K("w", k, j_, hf_) for j_ in range(4) for hf_ in range(2)]
        for h in range(0, nk, step):
            if nk == 8 and tot == 512:
                halves = [hf_ for hf_ in range(2) if c0 < (hf_ + 1) * 256 and c0 + ncols > hf_ * 256]
                wkeys_ = [K("w", k, h // 2, hf_) for hf_ in halves]
            else:
                wkeys_ = allk
            S.dma(v[:, h:h + step, c0:c0 + ncols],
                  src2d[h * 128:(h + step) * 128, :].rearrange("(kc p) n -> p kc n", p=128),
                  [], wkeys_, q="pool")
        return v, allk, k

    def dump(name, ap, reads, shape):
        if name in dumps:
            d = nc.dram_tensor("dbg_" + name, list(shape), ap.dtype, kind="ExternalOutput").ap()
            dump_d[name] = d
            i = S.dma(d, ap, reads, [K("dbg", name)])
            S.final.append(i)

    def load_fm(dst, src_d, ntile, dkey, st):
        xin = [sb(f"xin{j}", [128, D_], stack=st) for j in range(2)]
        for i in range(ntile):
            b = xin[i % 2]
            S.dma(b[:], src_d[i * 128:(i + 1) * 128, :], [], [K("xin", i % 2)])
            for half in range(2):
                ps, pk = ps_rot()
                for j in range(4):
                    kc = half * 4 + j
                    S.tr(ps[:, j * 128:(j + 1) * 128], b[:, kc * 128:(kc + 1) * 128], ident,
                         [K("xin", i % 2), K("c128")], [pk])
                S.cp(dst[:, half * 4:half * 4 + 4, i * 128:(i + 1) * 128],
                     ps[:].rearrange("p (a b) -> p a b", a=4), [pk],
                     [K(dkey, half * 4 + j, i // 4) for j in range(4)], eng=("dve" if half == 0 else "act"))

    st0 = ExitStack()
    load_fm(xT, x_d, NT, "xT", st0)

    def rmsnorm_fm(src, skey, gcol, dst, dkey, nk, T, st, dscale=1.0, hook=None, tag="n"):
        tw = min(T, TW)
        sq = [sb(f"sq_{tag}{j}", [128, nk, tw], BF16, stack=st) for j in range(2)]
        rs = [sb(f"rs_{tag}{j}", [128, tw], stack=st) for j in range(2)]
        for tc in range(T // tw):
            sl = slice(tc * tw, (tc + 1) * tw)
            q = sq[tc % 2]
            for kc in range(nk):
                S.act(q[:, kc, :], src[:, kc, sl], AF.Square, [K(skey, kc, tc)], [K("sq" + tag, tc % 2, kc)])
            ps, pk = ps_rot()
            for kc in range(nk):
                S.mm(ps[:, 0:tw], ones_b, q[:, kc, :], kc == 0, kc == nk - 1,
                     [K("sq" + tag, tc % 2, kc), K("c128b")], [pk])
            r = rs[tc % 2]
            S.act(r[:], ps[:, 0:tw], AF.Sqrt, [pk, K("cst")], [K("rs" + tag, tc % 2)],
                  bias=cst[:, 0:1], scale=1.0 / (nk * 128))
            S.add("dve", (lambda e, r=r: e.reciprocal(out=r[:], in_=r[:])),
                  [K("rs" + tag, tc % 2)], [K("rs" + tag, tc % 2)], cost=0.1 + tw / 240.0)
            if dst is not None:
                for kc in range(nk):
                    S.stt(dst[:, kc, sl], src[:, kc, sl], gcol(kc), r[:], ALU.mult, ALU.mult,
                          [K(skey, kc, tc), K("rs" + tag, tc % 2), K("colp")], [K(dkey, kc, tc)],
                          eng="dve")
            if hook is not None:
                hook(tc, r)

    ALLH = [K("hT", kc, tc) for kc in range(KC) for tc in range(NTC)]

    def hkeys(tc):
        return [K("hT", kc, tc) for kc in range(KC)]

    def proj_fm(wv, wkeys, c0, tc, ps, pk):
        for kc in range(KC):
            S.mm(ps[:, :], wv[:, kc, c0:c0 + 128], hT[:, kc, tc * TW:(tc + 1) * TW], kc == 0, kc == KC - 1,
                 wkeys + [K("hT", kc, tc)], [pk])

    def conv_chunk(ps, pk, tc, xw, xwk, cwcol, cbcol, dst, dkey):
        w_ = xw[tc % 2]
        wk = K(xwk, tc % 2)
        if tc == 0:
            S.add("dve", lambda e: e.memset(w_[:, 0:3], 0.0), [], [wk])
        else:
            S.cp(w_[:, 0:3], xw[(tc - 1) % 2][:, 512:515], [K(xwk, (tc - 1) % 2)], [wk])
        S.cp(w_[:, 3:515], ps[:, :], [pk], [wk], eng="act")
        S.ts(dst, w_[:, 3:515], cwcol(3), cbcol, ALU.mult, ALU.add, [wk, K("colp")], [dkey])
        for k in range(3):
            S.stt(dst, w_[:, k:k + 512], cwcol(k), dst, ALU.mult, ALU.add, [wk, dkey, K("colp")], [dkey])

    def group_out(l, g, YG, st):
        YB = sb("YB", [128, 2, S_], BF16, stack=st)
        rmsnorm_fm(YG, "YG", lambda kc: col(l, "grp_g", 2 * g + kc), YB, "YB", 2, S_, st, tag="g")
        dump(f"yn{l}_{g}", YB[:], [K("YB", c, t) for c in range(2) for t in range(NTC)], [128, 2, S_])
        for half in range(2):
            wv, wk, _ = load_w(w_out_d[l, g * 256:(g + 1) * 256, half * 512:(half + 1) * 512], 2, 512)
            for o4 in range(4):
                oc = half * 4 + o4
                for tc in range(NTC):
                    ps, pk = ps_rot()
                    for kc in range(2):
                        S.mm(ps[:, :], wv[:, kc, o4 * 128:(o4 + 1) * 128], YB[:, kc, tc * TW:(tc + 1) * TW],
                             kc == 0, kc == 1, wk + [K("YB", kc, tc)], [pk])
                    S.tt(xT[:, oc, tc * TW:(tc + 1) * TW], xT[:, oc, tc * TW:(tc + 1) * TW], ps[:, :], ALU.add,
                         [K("xT", oc, tc), pk], [K("xT", oc, tc)])

    ALLX = [K("xT", kc, tc) for kc in range(KC) for tc in range(NTC)]
    done = False
    for l in range(n_layers):
        with ExitStack() as st:
            rmsnorm_fm(xT, "xT", lambda kc: col(l, "mix_g", kc), hT, "hT", KC, S_, (st0 if l == 0 else st), tag="a")
        if l == 0:
            st0.close()
        S.barrier()
        if l == 0:
            dump("h0", hT[:], ALLH, [128, KC, S_])
        if stop_after == "norm0":
            break

        with ExitStack() as sty, ExitStack() as st:
            YG = sb("YG", [128, 2, S_], stack=sty)
            wv, wk, _ = load_w(w_in_d[l, :, 0:512], 8, 512)
            wbd = sb("wbd", [128, 4, 128], BF16, stack=st)
            S.dma(wbd[:], lrubd_d[l].rearrange("g c k m -> k (g c) m"), [], [K("wbd")], q="pool", arena=True)
            c8 = sb("c8", [128, 2], stack=st)
            dump(f"A_wbd{l}", wbd[:], [K("wbd")], [128, 4, 128])
            cy = sb("cy", [128, 2], stack=st)
            cz = sb("cz", [128, 2], stack=st)
            S.act(cy[:], col(l, "lru_lam", 0, 2), AF.Exp, [K("colp")], [K("cy")], scale=-1.0)
            S.ts(cz[:], cy[:], 2.0, None, ALU.add, None, [K("cy")], [K("cz")])
            S.add("dve", lambda e: e.reciprocal(out=cz[:], in_=cz[:]), [K("cz")], [K("cz")])
            S.tt(cz[:], cz[:], cy[:], ALU.mult, [K("cz"), K("cy")], [K("cz")])
            S.tt(cy[:], cz[:], cz[:], ALU.mult, [K("cz")], [K("cy")])
            S.ts(c8[:], cy[:], 1.0 / 9.0, 1.0 / 7.0, ALU.mult, ALU.add, [K("cy")], [K("c8")])
            for cc_ in (1.0 / 5.0, 1.0 / 3.0, 1.0):
                S.tt(c8[:], c8[:], cy[:], ALU.mult, [K("c8"), K("cy")], [K("c8")])
                S.ts(c8[:], c8[:], cc_, None, ALU.add, None, [K("c8")], [K("c8")])
            S.tt(c8[:], c8[:], cz[:], ALU.mult, [K("c8"), K("cz")], [K("c8")])
            S.ts(c8[:], c8[:], -16.0, None, ALU.mult, None, [K("c8")], [K("c8")])
            xw = [sb(f"xw{j}", [128, 515], stack=st) for j in range(2)]
            XC = sb("XC", [128, S_], stack=st)
            XCB = sb("XCB", [128, S_], BF16, stack=st)
            GG = sb("GG", [128, S_], stack=st)
            RA = sb("RA", [128, S_], stack=st)
            IU = sb("IU", [128, S_], stack=st)
            T2 = sb("T2", [128, S_], stack=st)
            XX = sb("XX", [128, S_], stack=st)
            for c in range(2):
                for tc in range(NTC):
                    sl = slice(tc * TW, (tc + 1) * TW)
                    ps, pk = ps_rot()
                    proj_fm(wv, wk, c * 128, tc, ps, pk)
                    conv_chunk(ps, pk, tc, xw, "xw", lambda k: col(l, "lru_cw", c * 4 + k), col(l, "lru_cb", c),
                               XC[:, sl], K("XC", tc))
                    ps, pk = ps_rot()
                    proj_fm(wv, wk, 256 + c * 128, tc, ps, pk)
                    S.cp(GG[:, sl], ps[:, :], [pk], [K("GG", tc)], eng="act")
                XCk = [K("XC", t) for t in range(NTC)]
                GGk = [K("GG", t) for t in range(NTC)]
                S.tt(T2[:], GG[:], GG[:], ALU.mult, GGk, [K("T2")])
                S.ts(T2[:], T2[:], 0.044715, 1.0, ALU.mult, ALU.add, [K("T2")], [K("T2")])
                S.tt(T2[:], T2[:], GG[:], ALU.mult, [K("T2")] + GGk, [K("T2")])
                S.act(T2[:], T2[:], AF.Sigmoid, [K("T2")], [K("T2")], scale=1.5957691216)
                S.tt(GG[:], GG[:], T2[:], ALU.mult, [K("T2")] + GGk, GGk)
                S.cp(XCB[:], XC[:], XCk, [K("XCB")], eng="act")
                for tc in range(NTC):
                    sl = slice(tc * TW, (tc + 1) * TW)
                    ps, pk = ps_rot()
                    S.mm(ps[:, :], wbd[:, c, :], XCB[:, sl], True, True, [K("wbd"), K("XCB")], [pk])
                    S.act(RA[:, sl], ps[:, :], AF.Sigmoid, [pk, K("colp")], [K("RA", tc)], bias=col(l, "lru_br", c))
                    ps, pk = ps_rot()
                    S.mm(ps[:, :], wbd[:, 2 + c, :], XCB[:, sl], True, True, [K("wbd"), K("XCB")], [pk])
                    S.act(IU[:, sl], ps[:, :], AF.Sigmoid, [pk, K("colp")], [K("IU", tc)], bias=col(l, "lru_bi", c))
                RAk = [K("RA", t) for t in range(NTC)]
                IUk = [K("IU", t) for t in range(NTC)]
                if c == 1:
                    dump(f"A_r{l}", RA[:], RAk, [128, S_])
                    dump(f"A_i{l}", IU[:], IUk, [128, S_])
                    dump(f"A_xcb{l}", XCB[:], [K("XCB")], [128, S_])
                    dump(f"A_xc{l}", XC[:], XCk, [128, S_])
                S.ts(XX[:], RA[:], c8[:, c:c + 1], 2.0, ALU.mult, ALU.mult, RAk + [K("c8")], [K("XX")])
                S.ts(T2[:], XX[:], 1.0 / 120.0, None, ALU.mult, None, [K("XX")], [K("T2")])
                for cc_ in (1.0 / 24.0, 1.0 / 6.0, 0.5, 1.0):
                    S.stt(T2[:], T2[:], cc_, XX[:], ALU.add, ALU.mult, [K("T2"), K("XX")], [K("T2")])
                S.act(T2[:], T2[:], AF.Sqrt, [K("T2")], [K("T2")], scale=-1.0)
                S.act(RA[:], XX[:], AF.Exp, [K("XX")], RAk, scale=0.5)
                S.tt(IU[:], IU[:], XC[:], ALU.mult, IUk + XCk, IUk)
                S.tt(IU[:], IU[:], T2[:], ALU.mult, IUk + [K("T2")], IUk)
                S.add("dve", lambda e: e.tensor_tensor_scan(out=XC[:], data0=RA[:], data1=IU[:], initial=0.0,
                                                            op0=ALU.mult, op1=ALU.add), RAk + IUk, XCk, cost=2.6)
                S.tt(YG[:, c, :], XC[:], GG[:], ALU.mult, XCk + GGk, [K("YG", c, t) for t in range(NTC)])
            for nm_, t_ in (("XC", XC), ("GG", GG), ("RA", RA), ("IU", IU), ("T2", T2)):
                dump(f"A_{nm_}{l}", t_[:], [K(nm_, t) for t in range(NTC)] + [K(nm_)], [128, S_])
            dump(f"ya{l}", YG[:], [K("YG", c, t) for c in range(2) for t in range(NTC)], [128, 2, S_])
            st.close()
            S.barrier()
            group_out(l, 0, YG, sty)
        S.barrier()
        if stop_after == f"A{l}":
            done = True
            break

        nrot[0] = 4
        with ExitStack() as sty, ExitStack() as st:
            YG = sb("YG", [128, 2, S_], stack=sty)
            QB = sb("QB", [128, 2, S_], BF16, stack=st)
            KZ = sb("KZ", [128, 4, S_], BF16, stack=st)
            S.add("dve", lambda e: e.memset(KZ[:], 0.0), [], [K("KZ", h_, t_) for h_ in range(4) for t_ in range(NTC)], cost=4.5)
            VB = sb("VB", [128, NT, 256], BF16, stack=st)
            M01 = sb("M01", [128, 4, 512], stack=st)
            NM = sb("NM", [128, 4, 512], BF16, stack=st)
            S.dma(M01[:], cmask_d[:, 0:2048].rearrange("p (a b) -> p a b", a=4), [], [K("M01")])
            S.dma(NM[:], cmask_d[:, 2048:4096].rearrange("p (a b) -> p a b", a=4), [], [K("NM")], q="pool", arena=True)
            wv, wk, _ = load_w(w_in_d[l, :, 512:1024], 8, 512)
            wv2, wk2, _ = load_w(w_in_d[l, :, 1024:1280], 8, 256)
            for c2 in range(2):
                for tc in range(NTC):
                    sl = slice(tc * TW, (tc + 1) * TW)
                    ps, pk = ps_rot()
                    proj_fm(wv, wk, c2 * 128, tc, ps, pk)
                    S.ts(QB[:, c2, sl], ps[:, :], 0.125, None, ALU.mult, None, [pk], [K("QB", c2, tc)])
                    ps, pk = ps_rot()
                    proj_fm(wv, wk, 256 + c2 * 128, tc, ps, pk)
                    S.cp(KZ[0:64, 2 * c2, sl], ps[0:64, :], [pk], [K("KZ", 2 * c2, tc)])
                    S.cp(KZ[64:128, 2 * c2 + 1, sl], ps[64:128, :], [pk], [K("KZ", 2 * c2 + 1, tc)])
            for i in range(NT):
                ps, pk = ps_rot()
                for kc in range(KC):
                    S.mm(ps[:, 0:256], hT[:, kc, i * 128:(i + 1) * 128], wv2[:, kc, :], kc == 0, kc == KC - 1,
                         wk2 + [K("hT", kc, i // 4)], [pk])
                S.cp(VB[:, i, :], ps[:, 0:256], [pk], [K("VB", i)], eng=("dve" if i % 2 else "act"))
            Eb = [sb(f"Eb{j}", [128, 512], stack=st) for j in range(2)]
            SPb = [sb(f"SPb{j}", [128, 512], F32R, stack=st) for j in range(3)]
            Wb = [sb(f"Wb{j}", [128, 512], BF16, stack=st) for j in range(2)]
            Rb = [sb(f"Rb{j}", [128, 512], F32R, stack=st) for j in range(2)]
            NUr = sb("NUr", [128, 2, 128], F32R, stack=st)
            S.cp(NUr[:, 0, :], negu, [K("c128")], [K("NUr")])
            S.cp(NUr[:, 1, :], negones, [K("c128")], [K("NUr")])
            items = []
            grp = 0
            for h in range(4):
                for c in range(NTC):
                    nb = 4 * c + 4
                    for idx, b in enumerate(range(nb - 1, -1, -1)):
                        items.append(dict(h=h, c=c, idx=idx, b=b, grp=grp, last=(b == 0)))
                    grp += 1
            for i_, itm in enumerate(items):
                itm["i"] = i_
                itm["pr"] = slice((itm["h"] % 2) * 64, (itm["h"] % 2) * 64 + 64)
                itm["c2"] = itm["h"] // 2
                itm["d"] = itm["b"] - 4 * itm["c"]
                itm["ksl"] = slice(itm["b"] * 128, (itm["b"] + 1) * 128)
                itm["qsl"] = slice(itm["c"] * TW, (itm["c"] + 1) * TW)
                itm["qk"] = [K("KZ", itm["h"], itm["b"] // 4), K("QB", itm["c2"], itm["c"])]

            def sb_s1(m):
                i = m["i"]
                pr, c2 = m["pr"], m["c2"]
                off = 128 * max(m["d"], 0)
                cs = slice(off, TW)
                qs = slice(m["c"] * TW + off, (m["c"] + 1) * TW)
                if m["idx"] == 0:
                    R0, R0k = Rb[m["grp"] % 2], K("Rb", m["grp"] % 2)
                    S.ts(R0[:], M01[:, 0, :], 0.0, None, ALU.mult, None, [K("M01")], [R0k])
                psZ, pkZ = ps_rot()
                S.mm(psZ[:, cs], KZ[:, m["h"], m["ksl"]], QB[:, c2, qs], True, True, m["qk"], [pkZ])
                E, Ek = Eb[i % 2], K("Eb", i % 2)
                SP, SPk = SPb[i % 3], K("SPb", i % 3)
                S.act(E[:, cs], psZ[:, cs], AF.Exp, [pkZ], [Ek])
                S.act(SP[:, cs], E[:, cs], AF.Ln, [Ek, K("cst")], [SPk], bias=cst[:, 1:2])
                if m["d"] >= 0:
                    S.tt(SP[:, cs], SP[:, cs].bitcast(F32), M01[:, m["d"], cs], ALU.mult, [SPk, K("M01")], [SPk])

            def sb_s2(m):
                i = m["i"]
                pr, c2, d = m["pr"], m["c2"], m["d"]
                off = 128 * max(d, 0)
                cs = slice(off, TW)
                qs = slice(m["c"] * TW + off, (m["c"] + 1) * TW)
                SP, SPk = SPb[i % 3], K("SPb", i % 3)
                Wt, Wk_ = Wb[i % 2], K("Wb", i % 2)
                R, Rk = Rb[m["grp"] % 2], K("Rb", m["grp"] % 2)
                psA, pkA = ps_rot()
                has_r = m["idx"] > 0
                S.mm(psA[:, cs], KZ[:, m["h"], m["ksl"]], QB[:, c2, qs], True, False, m["qk"], [pkA])
                S.mm(psA[:, cs], NUr[:, 0, :], SP[:, cs], False, (not has_r) and d < 0, [SPk, K("NUr")], [pkA])
                if has_r:
                    S.mm(psA[:, cs], NUr[:, 1, :], R[:, cs], False, d < 0, [Rk, K("NUr")], [pkA])
                if d >= 0:
                    S.mm(psA[:, cs], ident_b, NM[:, d, cs], False, True, [K("NM"), K("c128b")], [pkA])
                S.act(Wt[:, cs], psA[:, cs], AF.Exp, [pkA], [Wk_])
                if not m["last"]:
                    S.tt(R[:, cs], R[:, cs].bitcast(F32), SP[:, cs].bitcast(F32), ALU.add, [Rk, SPk], [Rk])

            def sb_s3(m):
                i = m["i"]
                pr, c2, h = m["pr"], m["c2"], m["h"]
                off = 128 * max(m["d"], 0)
                cs = slice(off, TW)
                Wt, Wk_ = Wb[i % 2], K("Wb", i % 2)
                psO, pkO = PS[4 + m["grp"] % 2], K("psO", m["grp"] % 2)
                S.mm(psO[:, cs], VB[:, m["b"], c2 * 128:(c2 + 1) * 128], Wt[:, cs], m["idx"] == 0, m["last"],
                     [K("VB", m["b"]), Wk_], [pkO])
                if m["last"]:
                    S.cp(YG[pr, c2, m["qsl"]], psO[pr, :], [pkO], [K("YG", c2, m["c"])], eng="act")

            n_it = len(items)
            for r in range(-2, n_it):
                if 0 <= r + 2 < n_it:
                    sb_s1(items[r + 2])
                if 0 <= r + 1 < n_it:
                    sb_s2(items[r + 1])
                if 0 <= r < n_it:
                    sb_s3(items[r])
            dump(f"yb{l}", YG[:], [K("YG", c, t) for c in range(2) for t in range(NTC)], [128, 2, S_])
            st.close()
            S.barrier()
            nrot[0] = 8
            group_out(l, 1, YG, sty)
        S.barrier()
        if stop_after == f"B{l}":
            done = True
            break

        nrot[0] = 4
        with ExitStack() as sty, ExitStack() as st:
            YG = sb("YG", [128, 2, S_], stack=sty)
            XS = sb("XS", [128, 2, S_], stack=st)
            BTb = sb("BTb", [128, 2, S_], BF16, stack=st)
            CTb = sb("CTb", [128, 2, S_], BF16, stack=st)
            BTM = sb("BTM", [128, NT, 256], BF16, stack=st)
            XTM = sb("XTM", [128, NT, 256], BF16, stack=st)
            wdt = sb("wdt", [128, KC, 4], BF16, stack=st)
            stc = ExitStack()
            xw = [sb(f"xwc{j}", [128, 515], stack=stc) for j in range(2)]
            CV = [sb(f"CV{j}", [128, 512], stack=stc) for j in range(2)]
            BF = [sb(f"BF{j}", [128, 512], stack=stc) for j in range(2)]
            S.dma(wdt[:], w_in_d[l, :, 2304:2308].rearrange("(kc p) n -> p kc n", p=128), [], [K("wdt")], q="pool", arena=True)
            wv1, wk1, _ = load_w(w_in_d[l, :, 1280:1792], 8, 512)
            wv2, wk2, _ = load_w(w_in_d[l, :, 1792:2304], 8, 512)
            n_cv = 0
            for j in range(6):
                if j < 2:
                    wv, wk, c0 = wv1, wk1, 256 + j * 128
                elif j < 4:
                    wv, wk, c0 = wv2, wk2, (j - 2) * 128
                else:
                    wv, wk, c0 = wv2, wk2, 256 + (j - 4) * 128
                for tc in range(NTC):
                    sl = slice(tc * TW, (tc + 1) * TW)
                    ps, pk = ps_rot()
                    proj_fm(wv, wk, c0, tc, ps, pk)
                    cv, cvk = CV[n_cv % 2], K("CV", n_cv % 2)
                    conv_chunk(ps, pk, tc, xw, "xwc", lambda k: col(l, "ssm_cw", j * 4 + k), col(l, "ssm_cb", j),
                               cv[:], cvk)
                    if j < 2:
                        S.act(XS[:, j, sl], cv[:], AF.Silu, [cvk], [K("XS", j, tc)])
                        ps, pk = ps_rot()
                        for q in range(4):
                            S.tr(ps[:, q * 128:(q + 1) * 128], XS[:, j, tc * TW + q * 128: tc * TW + (q + 1) * 128],
                                 ident, [K("XS", j, tc), K("c128")], [pk])
                        S.cp(XTM[:, tc * 4:tc * 4 + 4, j * 128:(j + 1) * 128],
                             ps[:].rearrange("p (a b) -> p a b", a=4), [pk], [K("XTM", tc, j)])
                    elif j < 4:
                        g = j - 2
                        bf, bfk = BF[n_cv % 2], K("BF", n_cv % 2)
                        S.act(bf[:], cv[:], AF.Silu, [cvk], [bfk])
                        S.cp(BTb[:, g, sl], bf[:], [bfk], [K("BTb", g, tc)])
                        ps, pk = ps_rot()
                        for q in range(4):
                            S.tr(ps[:, q * 128:(q + 1) * 128], bf[:, q * 128:(q + 1) * 128], ident,
                                 [bfk, K("c128")], [pk])
                        S.cp(BTM[:, tc * 4:tc * 4 + 4, g * 128:(g + 1) * 128],
                             ps[:].rearrange("p (a b) -> p a b", a=4), [pk], [K("BTM", tc, g)])
                    else:
                        S.act(CTb[:, j - 4, sl], cv[:], AF.Silu, [cvk], [K("CTb", j - 4, tc)])
                    n_cv += 1
            stc.close()
            S.barrier()
            DT = sb("DT", [128, 64], stack=st)
            AN = sb("AN", [128, 64], stack=st)
            AC = sb("AC", [128, 64], stack=st)
            ACS = sb("ACS", [128, 64], stack=st)
            TOT = sb("TOT", [128, 64], stack=st)
            DEC = sb("DEC", [128, 64], stack=st)
            ETOT = sb("ETOT", [128, 64], stack=st)
            DTD = sb("DTD", [128, 64], stack=st)
            psd, pkd = ps_rot()
            for i in range(NT):
                for kc in range(KC):
                    S.mm(psd[:, i * 4:(i + 1) * 4], hT[:, kc, i * 128:(i + 1) * 128], wdt[:, kc, :], kc == 0,
                         kc == KC - 1, [K("wdt"), K("hT", kc, i // 4)], [pkd])
            S.tt(DT[:], psd[:, 0:64], row(l, "dt_bias"), ALU.add, [pkd, K("rowp")], [K("DT")])
            S.act(DT[:], DT[:], AF.Exp, [K("DT")], [K("DT")])
            S.act(DT[:], DT[:], AF.Ln, [K("DT"), K("cst")], [K("DT")], bias=cst[:, 1:2])
            S.act(AN[:], row(l, "a_log"), AF.Exp, [K("rowp")], [K("AN")])
            S.ts(AN[:], AN[:], -1.0, None, ALU.mult, None, [K("AN")], [K("AN")])
            S.tt(AC[:], DT[:], AN[:], ALU.mult, [K("DT"), K("AN")], [K("AC")])
            ps, pk = ps_rot()
            S.mm(ps[:, 0:64], tri_le, AC[:], True, True, [K("AC"), K("c128")], [pk])
            S.cp(ACS[:], ps[:, 0:64], [pk], [K("ACS")])
            ps, pk = ps_rot()
            S.mm(ps[:, 0:64], ones_f, AC[:], True, True, [K("AC"), K("c128")], [pk])
            S.cp(TOT[:], ps[:, 0:64], [pk], [K("TOT")])
            S.tt(DEC[:], TOT[:], ACS[:], ALU.subtract, [K("TOT"), K("ACS")], [K("DEC")])
            S.act(DEC[:], DEC[:], AF.Exp, [K("DEC")], [K("DEC")])
            S.act(ETOT[:], TOT[:], AF.Exp, [K("TOT")], [K("ETOT")])
            S.tt(DTD[:], DT[:], DEC[:], ALU.mult, [K("DT"), K("DEC")], [K("DTD")])
            ST = sb("ST", [128, 4, 64], stack=st)
            S.add("dve", lambda e: e.memset(ST[:], 0.0), [], [K("ST", h) for h in range(4)])
            Rm = [sb(f"Rm{j}", [128, 128], stack=st) for j in range(2)]
            ARG = [sb(f"ARG{j}", [128, 128], stack=st) for j in range(2)]
            E1 = [sb(f"E1{j}", [128, 128], stack=st) for j in range(2)]
            GL = [sb(f"GL{j}", [128, 128], BF16, stack=st) for j in range(2)]
            CTp = [sb(f"CTp{j}", [128, 128], BF16, stack=st) for j in range(2)]
            XCp = [[sb(f"XCp{a}{j}", [128, 128], BF16, stack=st) for j in range(2)] for a in range(2)]
            STp = sb("STp", [128, 4, 128], BF16, stack=st)
            for a in range(2):
                for j in range(2):
                    S.add("dve", (lambda e, t_=XCp[a][j]: e.memset(t_[:], 0.0)), [], [K("XCp", a, j)])
            S.add("dve", lambda e: e.memset(STp[:], 0.0), [], [K("STp", h) for h in range(4)])
            XDh = [sb(f"XDh{j}", [128, 64], BF16, stack=st) for j in range(2)]
            n = 0
            for ci in range(NT):
                csl = slice(ci * 128, (ci + 1) * 128)
                tcx = ci // 4
                for g in range(2):
                    psG, pkG = ps_rot()
                    S.mm(psG[:, 0:128], BTb[:, g, csl], CTb[:, g, csl], True, True,
                         [K("BTb", g, tcx), K("CTb", g, tcx)], [pkG])
                    psY, pkY = PS[4 + (ci * 2 + g) % 2], K("psO", (ci * 2 + g) % 2)
                    for hh in range(2):
                        h = 2 * g + hh
                        pr = slice(hh * 64, hh * 64 + 64)
                        cidx = ci * 4 + h
                        k2 = n % 2
                        S.ts(Rm[k2][:], tri_le, AC[:, cidx:cidx + 1], None, ALU.mult, None,
                             [K("AC"), K("c128")], [K("Rm", k2)])
                        ps1, pk1 = ps_rot()
                        S.mm(ps1[:, 0:128], ones_f, Rm[k2][:], True, True, [K("Rm", k2), K("c128")], [pk1])
                        S.stt(ARG[k2][:], ps1[:, 0:128], ACS[:, cidx:cidx + 1], ssdneg, ALU.subtract, ALU.add,
                              [pk1, K("ACS"), K("c128")], [K("ARG", k2)])
                        S.act(ARG[k2][:], ARG[k2][:], AF.Exp, [K("ARG", k2)], [K("ARG", k2)])
                        S.tt(GL[k2][:], ARG[k2][:], psG[:, 0:128], ALU.mult, [K("ARG", k2), pkG], [K("GL", k2)])
                        S.act(E1[k2][:], ps1[:, 0:128], AF.Exp, [pk1, K("ARG", k2)], [K("E1", k2)])
                        S.tt(CTp[k2][:], CTb[:, g, csl], E1[k2][:], ALU.mult, [K("CTb", g, tcx), K("E1", k2)],
                             [K("CTp", k2)])
                        rot = (ci * 2 + g) % 2
                        xcp = XCp[hh][rot]
                        S.ts(xcp[:, hh * 64:(hh + 1) * 64], XTM[:, ci, h * 64:(h + 1) * 64], DT[:, cidx:cidx + 1], None,
                             ALU.mult, None, [K("XTM", tcx, g), K("DT")], [K("XCp", hh, rot)])
                        S.mm(psY[:, 0:128], xcp[:], GL[k2][:], hh == 0, False, [K("XCp", hh, rot), K("GL", k2)], [pkY])
                        S.mm(psY[:, 0:128], STp[:, h, :], CTp[k2][:], False, hh == 1, [K("STp", h), K("CTp", k2)], [pkY])
                        S.ts(XDh[k2][:], XTM[:, ci, h * 64:(h + 1) * 64], DTD[:, cidx:cidx + 1], None, ALU.mult, None,
                             [K("XTM", tcx, g), K("DTD")], [K("XDh", k2)])
                        psS, pkS = ps_rot()
                        S.mm(psS[:, 0:64], BTM[:, ci, g * 128:(g + 1) * 128], XDh[k2][:], True, True,
                             [K("BTM", tcx, g), K("XDh", k2)], [pkS])
                        S.stt(ST[:, h, :], ST[:, h, :], ETOT[:, cidx:cidx + 1], psS[:, 0:64], ALU.mult, ALU.add,
                              [K("ST", h), K("ETOT"), pkS], [K("ST", h)])
                        S.cp(STp[:, h, hh * 64:(hh + 1) * 64], ST[:, h, :], [K("ST", h)], [K("STp", h)], eng="act")
                        n += 1
                    S.cp(YG[:, g, csl], psY[:, 0:128], [pkY], [K("YG", g, tcx)], eng="act")
            ARG = [sb(f"ZT{j}", [128, 512], stack=st) for j in range(1)]
            for j in range(2):
                YGk = [K("YG", j, t) for t in range(NTC)]
                S.stt(YG[:, j, :], XS[:, j, :], col(l, "ssm_d", j), YG[:, j, :], ALU.mult, ALU.add,
                      [K("XS", j, t) for t in range(NTC)] + YGk + [K("colp")], YGk)
                for tc in range(NTC):
                    sl = slice(tc * TW, (tc + 1) * TW)
                    ps, pk = ps_rot()
                    proj_fm(wv1, wk1, j * 128, tc, ps, pk)
                    zt, ztk = ARG[0], K("ARGz", 0)
                    S.act(zt[:], ps[:, :], AF.Silu, [pk], [ztk])
                    S.tt(YG[:, j, sl], YG[:, j, sl], zt[:], ALU.mult, [K("YG", j, tc), ztk], [K("YG", j, tc)])
            dump(f"yc{l}", YG[:], [K("YG", c, t) for c in range(2) for t in range(NTC)], [128, 2, S_])
            st.close()
            S.barrier()
            nrot[0] = 8
            group_out(l, 2, YG, sty)
        S.barrier()
        if stop_after == f"C{l}":
            done = True
            break

        nrot[0] = 4
        with ExitStack() as sty, ExitStack() as st:
            YG = sb("YG", [128, 2, S_], stack=sty)
            QA = sb("QA", [128, 4, S_], BF16, stack=st)
            KA = sb("KA", [128, 4, S_], BF16, stack=st)
            allqa = [K("QA", h_, t_) for h_ in range(4) for t_ in range(NTC)]
            allka = [K("KA", h_, t_) for h_ in range(4) for t_ in range(NTC)]
            S.add("dve", lambda e: e.memset(QA[:], 0.0), [], allqa, cost=4.5)
            S.add("dve", lambda e: e.memset(KA[:], 0.0), [], allka, cost=4.5)
            for h_ in range(4):
                o_ = 64 if h_ % 2 == 0 else 0
                S.dma(KA[o_:o_ + 8, h_, :], blkoh_d[:, :], allka, [K("KAoh", h_)], q="pool", arena=True)
            VB = sb("VB", [128, NT, 256], BF16, stack=st)
            CAUS = sb("CAUS", [128, 4, 512], BF16, stack=st)
            S.dma(CAUS[:], cmask_d[:, 4096:6144].rearrange("p (a b) -> p a b", a=4), [], [K("CAUS")], q="pool", arena=True)
            GC = sb("GC", [128, 3, 128], stack=st)
            S.dma(GC[:], gate_c_d[:, :].rearrange("p (a b) -> p a b", a=3), [], [K("GC")])
            KM = sb("KM", [128, 2, 8], stack=st)
            KMb = sb("KMb", [128, 2, 8], BF16, stack=st)
            wv, wk, _ = load_w(w_in_d[l, :, 2308:2820], 8, 512)
            wv2, wk2, _ = load_w(w_in_d[l, :, 2820:3076], 8, 256)
            str_ = ExitStack()
            ROPEb = [sb(f"ROPE{j}", [128, 2, TW], stack=str_) for j in range(1)]
            RAW = [sb(f"RAW{j}", [128, 512], stack=str_) for j in range(2)]
            TMP = [sb(f"TMPr{j}", [128, 512], stack=str_) for j in range(2)]
            KF = [sb(f"KF{j}", [128, 512], stack=str_) for j in range(2)]
            nr = 0
            for c2 in range(2):
                for tc in range(NTC):
                    sl = slice(tc * TW, (tc + 1) * TW)
                    rj = 0
                    ROPE = ROPEb[rj]
                    rsl = slice(0, TW)
                    S.dma(ROPE[:, 0, :], rope_d[:, tc * TW:(tc + 1) * TW], [], [K("ROPE", rj, 0)])
                    S.dma(ROPE[:, 1, :], rope_d[:, S_ + tc * TW:S_ + (tc + 1) * TW], [], [K("ROPE", rj, 1)])
                    for which in range(2):
                        ps, pk = ps_rot()
                        proj_fm(wv, wk, which * 256 + c2 * 128, tc, ps, pk)
                        raw, rawk = RAW[nr % 2], K("RAW", nr % 2)
                        tmp, tmpk = TMP[nr % 2], K("TMPr", nr % 2)
                        S.cp(raw[:], ps[:, :], [pk], [rawk], eng="act")
                        psw, pkw = ps_rot()
                        S.mm(psw[:, :], perm_f, raw[:], True, True, [rawk, K("c128")], [pkw])
                        S.tt(tmp[:], psw[:, :], ROPE[:, 1, rsl], ALU.mult, [pkw, K("ROPE", rj, 1)], [tmpk])
                        kf = KF[nr % 2]
                        dst, dk = kf[:], K("KF", nr % 2)
                        S.tt(dst, raw[:], ROPE[:, 0, rsl], ALU.mult, [rawk, K("ROPE", rj, 0)], [dk])
                        S.tt(dst, dst, tmp[:], ALU.add, [dk, tmpk], [dk])
                        if which == 0:
                            S.act(QA[0:64, 2 * c2, sl], kf[0:64, :], AF.Copy, [dk], [K("QA", 2 * c2, tc)], scale=0.125)
                            S.act(QA[64:128, 2 * c2 + 1, sl], kf[64:128, :], AF.Copy, [dk], [K("QA", 2 * c2 + 1, tc)],
                                  scale=0.125)
                        else:
                            S.cp(KA[0:64, 2 * c2, sl], kf[0:64, :], [dk], [K("KA", 2 * c2, tc)], eng="act")
                            S.cp(KA[64:128, 2 * c2 + 1, sl], kf[64:128, :], [dk], [K("KA", 2 * c2 + 1, tc)], eng="act")
                            for hb in range(2):
                                nblk = tc * 2 + hb
                                S.add("dve", (lambda e, o=KM[:, c2, nblk:nblk + 1], i_=kf[:, hb * 256:(hb + 1) * 256]:
                                              e.reduce_sum(out=o, in_=i_, axis=AX.X)), [dk], [K("KM", c2, nblk)])
                        nr += 1
            for i in range(NT):
                ps, pk = ps_rot()
                for kc in range(KC):
                    S.mm(ps[:, 0:256], hT[:, kc, i * 128:(i + 1) * 128], wv2[:, kc, :], kc == 0, kc == KC - 1,
                         wk2 + [K("hT", kc, i // 4)], [pk])
                S.cp(VB[:, i, :], ps[:, 0:256], [pk], [K("VB", i)], eng=("dve" if i % 2 else "act"))
            S.cp(KMb[:], KM[:], [K("KM", c2_, nb_) for c2_ in range(2) for nb_ in range(8)], [K("KMb")])
            str_.close()
            S.barrier()
            PTb = [sb(f"PTb{j}", [128, 512], BF16, stack=st) for j in range(3)]
            RD = [sb(f"RD{j}", [128, 512], stack=st) for j in range(2)]
            GT = sb("GT", [128, 128], stack=st)
            BT_ = sb("BIASTM", [128, 128], stack=st)
            MX = [sb(f"MX{j}", [128, 8], stack=st) for j in range(2)]
            for h in range(4):
                hh, c2 = h % 2, h // 2
                pr = slice(hh * 64, hh * 64 + 64)
                o_ = 64 if hh == 0 else 0
                psg, pkg = ps_rot()
                for i in range(NT):
                    S.mm(psg[:, i * 8:(i + 1) * 8], QA[pr, h, i * 128:(i + 1) * 128], KMb[pr, c2, :], True, True,
                         [K("QA", h, i // 4), K("KMb")], [pkg])
                S.stt(GT[:], psg[:, 0:128], 8.0 / 256.0, GC[:, 0, :], ALU.mult, ALU.add, [pkg, K("GC")], [K("GT")])
                for i in range(NT):
                    mx, mxk = MX[i % 2], K("MX", i % 2)
                    S.add("dve", (lambda e, o=mx[:], i_=GT[:, i * 8:(i + 1) * 8]: e.max(out=o, in_=i_)),
                          [K("GT")], [mxk])
                    bsl = BT_[:, i * 8:(i + 1) * 8]
                    S.ts(bsl, GT[:, i * 8:(i + 1) * 8], mx[:, 2:3], None, ALU.is_ge, None, [K("GT"), mxk],
                         [K("BIASTM", i)])
                    S.tt(bsl, bsl, GC[:, 1, i * 8:(i + 1) * 8], ALU.mult, [K("BIASTM", i), K("GC")], [K("BIASTM", i)])
                    S.tt(bsl, bsl, GC[:, 2, i * 8:(i + 1) * 8], ALU.add, [K("BIASTM", i), K("GC")], [K("BIASTM", i)])
                    S.ts(bsl, bsl, -1.0, -NEG, ALU.add, ALU.mult, [K("BIASTM", i)], [K("BIASTM", i)])
                for q4 in range(4):
                    pst, pkt = ps_rot()
                    for q in range(4):
                        i = q4 * 4 + q
                        S.tr(pst[0:8, q * 128:(q + 1) * 128], BT_[:, i * 8:(i + 1) * 8], ident,
                             [K("BIASTM", i), K("c128")], [pkt])
                    S.cp(QA[o_:o_ + 8, h, q4 * 512:(q4 + 1) * 512], pst[0:8, :], [pkt], [K("QAsel", h, q4)])
            its = []
            grp = 0
            for h in range(4):
                for c in range(NTC):
                    nkt = 4 * c + 4
                    for kt in range(nkt):
                        its.append(dict(h=h, c=c, kt=kt, nkt=nkt, grp=grp, i=len(its)))
                    grp += 1

            def m_s1(m):
                h, c, kt = m["h"], m["c"], m["kt"]
                d = kt - 4 * c
                qsl = slice(c * TW, (c + 1) * TW)
                ksl = slice(kt * 128, (kt + 1) * 128)
                off = 128 * max(d, 0)
                cs = slice(off, TW)
                qs = slice(c * TW + off, (c + 1) * TW)
                psS, pkS = ps_rot()
                S.mm(psS[:, cs], KA[:, h, ksl], QA[:, h, qs], True, d < 0,
                     [K("KA", h, kt // 4), K("KAoh", h), K("QA", h, c), K("QAsel", h, c)], [pkS])
                if d >= 0:
                    S.mm(psS[:, cs], ident_b, CAUS[:, d, cs], False, True, [K("CAUS"), K("c128b")], [pkS])
                pt, ptk = PTb[m["i"] % 3], K("PTb", m["i"] % 3)
                S.act(pt[:, cs], psS[:, cs], AF.Exp, [pkS], [ptk])

            def m_s2(m):
                h, c, kt, nkt = m["h"], m["c"], m["kt"], m["nkt"]
                hh, c2 = h % 2, h // 2
                pr = slice(hh * 64, hh * 64 + 64)
                qsl = slice(c * TW, (c + 1) * TW)
                g2 = m["grp"] % 2
                psO, pkO = PS[4 + g2], K("psO", g2)
                psD, pkD = PS[6 + g2], K("psD", g2)
                pt, ptk = PTb[m["i"] % 3], K("PTb", m["i"] % 3)
                off = 128 * max(kt - 4 * c, 0)
                cs = slice(off, TW)
                S.mm(psO[:, cs], VB[:, kt, c2 * 128:(c2 + 1) * 128], pt[:, cs], kt == 0, kt == nkt - 1,
                     [K("VB", kt), ptk], [pkO])
                S.mm(psD[:, cs], ones_b, pt[:, cs], kt == 0, kt == nkt - 1, [ptk, K("c128b")], [pkD])
                if kt == nkt - 1:
                    rd, rdk = RD[g2], K("RD", g2)
                    S.add("dve", (lambda e, o=rd[pr, :], i_=psD[pr, :]: e.reciprocal(out=o, in_=i_)), [pkD], [rdk], cost=2.2)
                    S.tt(YG[pr, c2, qsl], psO[pr, :], rd[pr, :], ALU.mult, [pkO, rdk], [K("YG", c2, c)])

            n_ = len(its)
            for r in range(-1, n_):
                if r + 1 < n_:
                    m_s1(its[r + 1])
                if r >= 0:
                    m_s2(its[r])
            dump(f"yd{l}", YG[:], [K("YG", c, t) for c in range(2) for t in range(NTC)], [128, 2, S_])
            st.close()
            S.barrier()
            nrot[0] = 8
            group_out(l, 3, YG, sty)
        S.barrier()
        dump(f"x1_{l}", xT[:], ALLX, [128, KC, S_])
        if stop_after == f"D{l}":
            done = True
            break

        with ExitStack() as st:
            with ExitStack() as st2:
                rmsnorm_fm(xT, "xT", lambda kc: col(l, "xat_g", kc), hT, "hT", KC, S_, st2, tag="x")
            S.barrier()
            memT = sb("memT", [128, KC, 256], stack=st)
            MTb = sb("MTb", [128, KC, 256], BF16, stack=st)
            with ExitStack() as st2:
                load_fm(memT, mem_d, 2, "memT", st2)
                rmsnorm_fm(memT, "memT", lambda kc: col(l, "mem_g", kc), MTb, "MTb", KC, 256, st2, tag="m")
            S.barrier()
            KTb = sb("KTb", [128, KC, 256], BF16, stack=st)
            VX = sb("VX", [128, 2, D_], BF16, stack=st)
            QX = sb("QX", [128, KC, S_], BF16, stack=st)
            MTk = [K("MTb", kc, 0) for kc in range(KC)]
            for half in range(2):
                wv, wk, _ = load_w(wkv_d[l, :, half * 512:(half + 1) * 512], 8, 512)
                for o4 in range(4):
                    oc = half * 4 + o4
                    ps, pk = ps_rot()
                    for kc in range(KC):
                        S.mm(ps[:, 0:256], wv[:, kc, o4 * 128:(o4 + 1) * 128], MTb[:, kc, :], kc == 0, kc == KC - 1,
                             wk + [K("MTb", kc, 0)], [pk])
                    S.cp(KTb[:, oc, :], ps[:, 0:256], [pk], [K("KTb", oc)])
            for half in range(2):
                wv, wk, _ = load_w(wkv_d[l, :, 1024 + half * 512:1024 + (half + 1) * 512], 8, 512)
                for mt in range(2):
                    ps, pk = ps_rot()
                    for kc in range(KC):
                        S.mm(ps[:, :], MTb[:, kc, mt * 128:(mt + 1) * 128], wv[:, kc, :], kc == 0, kc == KC - 1,
                             wk + [K("MTb", kc, 0)], [pk])
                    S.cp(VX[:, mt, half * 512:(half + 1) * 512], ps[:, :], [pk], [K("VX", mt, half)], eng="act")
            for half in range(2):
                wv, wk, _ = load_w(wq_d[l, :, half * 512:(half + 1) * 512], 8, 512)
                for o4 in range(4):
                    oc = half * 4 + o4
                    for tc in range(NTC):
                        ps, pk = ps_rot()
                        proj_fm(wv, wk, o4 * 128, tc, ps, pk)
                        S.act(QX[:, oc, tc * TW:(tc + 1) * TW], ps[:, :], AF.Copy, [pk], [K("QX", oc, tc)],
                              scale=1.0 / 16.0)
            dump(f"X_MTb{l}", MTb[:], MTk, [128, KC, 256])
            dump(f"X_KTb{l}", KTb[:], [K("KTb", oc) for oc in range(KC)], [128, KC, 256])
            dump(f"X_QX{l}", QX[:], [K("QX", oc, tc) for oc in range(KC) for tc in range(NTC)], [128, KC, S_])
            dump(f"X_VX{l}", VX[:], [K("VX", mt, hf) for mt in range(2) for hf in range(2)], [128, 2, D_])
            PT = [sb(f"PTx{j}", [128, 512], BF16, stack=st) for j in range(4)]
            RDx = [sb(f"RDx{j}", [128, 512], stack=st) for j in range(2)]
            it = 0
            for hd in range(4):
                for tc in range(NTC):
                    sl = slice(tc * TW, (tc + 1) * TW)
                    pts = []
                    for mt in range(2):
                        ps, pk = ps_rot()
                        for j in range(2):
                            S.mm(ps[:, :], KTb[:, 2 * hd + j, mt * 128:(mt + 1) * 128], QX[:, 2 * hd + j, sl],
                                 j == 0, j == 1, [K("KTb", 2 * hd + j), K("QX", 2 * hd + j, tc)], [pk])
                        pt, ptk = PT[it % 4], K("PTx", it % 4)
                        S.act(pt[:], ps[:, :], AF.Exp, [pk], [ptk])
                        pts.append((pt, ptk))
                        it += 1
                    psD, pkD = ps_rot()
                    for mt in range(2):
                        S.mm(psD[:, :], ones_b, pts[mt][0][:], mt == 0, mt == 1, [pts[mt][1], K("c128b")], [pkD])
                    rd, rdk = RDx[(hd * NTC + tc) % 2], K("RDx", (hd * NTC + tc) % 2)
                    S.add("dve", (lambda e, o=rd[:], i_=psD[:, :]: e.reciprocal(out=o, in_=i_)), [pkD], [rdk], cost=2.2)
                    for j in range(2):
                        oc = 2 * hd + j
                        ps, pk = ps_rot()
                        for mt in range(2):
                            S.mm(ps[:, :], VX[:, mt, oc * 128:(oc + 1) * 128], pts[mt][0][:], mt == 0, mt == 1,
                                 [K("VX", mt, oc // 4), pts[mt][1]], [pk])
                        S.tt(hT[:, oc, sl], ps[:, :], rd[:], ALU.mult, [pk, rdk], [K("hT", oc, tc)])
            dump(f"X_OX{l}", hT[:], ALLH, [128, KC, S_])
            for half in range(2):
                wv, wk, _ = load_w(wo_d[l, :, half * 512:(half + 1) * 512], 8, 512)
                for o4 in range(4):
                    oc = half * 4 + o4
                    for tc in range(NTC):
                        ps, pk = ps_rot()
                        proj_fm(wv, wk, o4 * 128, tc, ps, pk)
                        S.tt(xT[:, oc, tc * TW:(tc + 1) * TW], xT[:, oc, tc * TW:(tc + 1) * TW], ps[:, :], ALU.add,
                             [K("xT", oc, tc), pk], [K("xT", oc, tc)])
        S.barrier()
        dump(f"x2_{l}", xT[:], ALLX, [128, KC, S_])
        if stop_after == f"X{l}":
            done = True
            break

        with ExitStack() as st:
            ENp = sb("ENp", [128, 16, 128], BF16, stack=st)
            S.add("dve", lambda e: e.memset(ENp[:], 0.0), [], [K("ENpz")])
            S.dma(ENp[0:16, :, :], en_d[:, :].rearrange("p (a b) -> p a b", a=16), [K("ENpz")], [K("ENf")], q="pool",
                  arena=True)
            CTh = sb("CTh", [128, S_], BF16, stack=st)
            CTl = sb("CTl", [128, S_], BF16, stack=st)
            S.add("dve", lambda e: e.memset(CTh[:], 0.0), [], [K("CThz")])
            S.add("dve", lambda e: e.memset(CTl[:], 0.0), [], [K("CTlz")])
            st_rt = ExitStack()
            WR = sb("WR", [128, KC, 20], stack=st_rt)
            S.dma(WR[:], wrt_d[l].rearrange("(kc p) n -> p kc n", p=128), [], [K("WR")])
            LG = sb("LG", [128, NT, 20], stack=st_rt)
            st_hf = ExitStack()
            HF = sb("HF", [128, KC, TW], stack=st_hf)

            def router_hook(tc, r):
                sl = slice(tc * TW, (tc + 1) * TW)
                for kc in range(KC):
                    S.stt(HF[:, kc, :], xT[:, kc, sl], col(l, "ffn_g", kc), r[:], ALU.mult, ALU.mult,
                          [K("xT", kc, tc), K("rsf", tc % 2), K("colp")], [K("HF", kc)])
                ps, pk = ps_rot()
                for q in range(4):
                    for kc in range(KC):
                        S.mm(ps[:, q * 20:(q + 1) * 20], HF[:, kc, q * 128:(q + 1) * 128], WR[:, kc, :], kc == 0,
                             kc == KC - 1, [K("HF", kc), K("WR")], [pk])
                S.cp(LG[:, tc * 4:tc * 4 + 4, :], ps[:, 0:80].rearrange("p (a b) -> p a b", a=4), [pk],
                     [K("LG", tc)])

            with ExitStack() as st2:
                rmsnorm_fm(xT, "xT", lambda kc: col(l, "ffn_g", kc), hT, "hT", KC, S_, st2, hook=router_hook, tag="f")
            st_hf.close()
            S.barrier()
            COMB = sb("COMB", [128, NT, 16], stack=st_rt)
            CT = sb("CT", [16, S_], stack=st_rt)
            sm = {nm: sb("rt_" + nm, [128, NT, w_] if w_ > 1 else [128, NT], stack=st_rt) for nm, w_ in
                  [("lg", 4), ("m", 1), ("eg", 4), ("sg", 1), ("oh", 4), ("le", 16), ("sel", 4), ("tmp", 4),
                   ("a", 1), ("eq", 4), ("sel2", 4), ("b", 1), ("t2", 4), ("ex", 4), ("dd", 1), ("wg", 4)]}

            def v(nm):
                return sm[nm][:]

            def kk(nm):
                return [K("rt", nm)]

            def B3(ap2, w_=4):
                return ap2.unsqueeze(2).to_broadcast([128, NT, w_])

            LGk = [K("LG", t) for t in range(NTC)]
            bgB = row(l, "bg").unsqueeze(1).to_broadcast([128, NT, 4])
            beB = row(l, "be").unsqueeze(1).to_broadcast([128, NT, 16])
            S.tt(v("lg"), LG[:, :, 0:4], bgB, ALU.add, LGk + [K("rowp")], kk("lg"))
            S.add("dve", lambda e: e.reduce_max(out=v("m"), in_=v("lg"), axis=AX.X), kk("lg"), kk("m"))
            S.tt(v("lg"), v("lg"), B3(v("m")), ALU.subtract, kk("lg") + kk("m"), kk("lg"))
            S.act(v("eg"), v("lg"), AF.Exp, kk("lg"), kk("eg"))
            S.add("dve", lambda e: e.reduce_sum(out=v("sg"), in_=v("eg"), axis=AX.X), kk("eg"), kk("sg"))
            S.add("dve", lambda e: e.reciprocal(out=v("sg"), in_=v("sg")), kk("sg"), kk("sg"))
            S.ts(v("oh"), v("lg"), 0.0, None, ALU.is_ge, None, kk("lg"), kk("oh"))
            S.tt(v("le"), LG[:, :, 4:20], beB, ALU.add, LGk + [K("rowp")], kk("le"))
            for g in range(4):
                ohg = sm["oh"][:, :, g:g + 1].to_broadcast([128, NT, 4])
                if g == 0:
                    S.tt(v("sel"), sm["le"][:, :, 0:4], ohg, ALU.mult, kk("le") + kk("oh"), kk("sel"))
                else:
                    S.tt(v("tmp"), sm["le"][:, :, 4 * g:4 * g + 4], ohg, ALU.mult, kk("le") + kk("oh"), kk("tmp"))
                    S.tt(v("sel"), v("sel"), v("tmp"), ALU.add, kk("sel") + kk("tmp"), kk("sel"))
            S.add("dve", lambda e: e.reduce_max(out=v("a"), in_=v("sel"), axis=AX.X), kk("sel"), kk("a"))
            S.tt(v("sel"), v("sel"), B3(v("a")), ALU.subtract, kk("sel") + kk("a"), kk("sel"))
            S.ts(v("eq"), v("sel"), 0.0, None, ALU.is_ge, None, kk("sel"), kk("eq"))
            S.stt(v("sel2"), v("eq"), -1e30, v("sel"), ALU.mult, ALU.add, kk("eq") + kk("sel"), kk("sel2"))
            S.add("dve", lambda e: e.reduce_max(out=v("b"), in_=v("sel2"), axis=AX.X), kk("sel2"), kk("b"))
            S.tt(v("t2"), v("sel"), B3(v("b")), ALU.is_ge, kk("sel") + kk("b"), kk("t2"))
            S.act(v("ex"), v("sel"), AF.Exp, kk("sel"), kk("ex"))
            S.act(v("dd"), v("b"), AF.Exp, kk("b"), kk("dd"))
            S.ts(v("dd"), v("dd"), 1.0, None, ALU.add, None, kk("dd"), kk("dd"))
            S.add("dve", lambda e: e.reciprocal(out=v("dd"), in_=v("dd")), kk("dd"), kk("dd"))
            S.tt(v("wg"), v("ex"), v("t2"), ALU.mult, kk("ex") + kk("t2"), kk("wg"))
            S.tt(v("wg"), v("wg"), B3(v("dd")), ALU.mult, kk("wg") + kk("dd"), kk("wg"))
            S.tt(v("wg"), v("wg"), B3(v("sg")), ALU.mult, kk("wg") + kk("sg"), kk("wg"))
            for g in range(4):
                ohg = sm["oh"][:, :, g:g + 1].to_broadcast([128, NT, 4])
                S.tt(COMB[:, :, 4 * g:4 * g + 4], v("wg"), ohg, ALU.mult, kk("wg") + kk("oh"),
                     [K("COMB", i, g) for i in range(NT)])
            for q4 in range(4):
                pst, pkt = ps_rot()
                for q in range(4):
                    i = q4 * 4 + q
                    S.tr(pst[0:16, q * 128:(q + 1) * 128], COMB[:, i, :], ident,
                         [K("COMB", i, g) for g in range(4)] + [K("c128")], [pkt])
                S.cp(CT[0:16, q4 * 512:(q4 + 1) * 512], pst[0:16, :], [pkt], [K("CT", q4)])
                S.cp(CTh[0:16, q4 * 512:(q4 + 1) * 512], CT[0:16, q4 * 512:(q4 + 1) * 512], [K("CT", q4), K("CThz")],
                     [K("CTh", q4)])
                S.tt(CT[0:16, q4 * 512:(q4 + 1) * 512], CT[0:16, q4 * 512:(q4 + 1) * 512],
                     CTh[0:16, q4 * 512:(q4 + 1) * 512], ALU.subtract, [K("CT", q4), K("CTh", q4)], [K("CT", q4)])
                S.cp(CTl[0:16, q4 * 512:(q4 + 1) * 512], CT[0:16, q4 * 512:(q4 + 1) * 512], [K("CT", q4), K("CTlz")],
                     [K("CTl", q4)])
            st_rt.close()
            S.barrier()
            HID = sb("HID", [128, 8, S_], BF16, stack=st)
            W2S = sb("W2S", [128, 8, D_], BF16, stack=st)
            CB = [sb(f"CB{j}", [128, 512], stack=st) for j in range(2)]
            SL = [sb(f"SL{j}", [128, 512], stack=st) for j in range(2)]
            n = 0
            for g in range(4):
                for el in range(4):
                    ex = 4 * g + el
                    wv, wk1_, slot = load_w(w1_d[l, ex], 8, 256, c0=0, tot=512)
                    _, wk3_, _ = load_w(w3_d[l, ex], 8, 256, slot=slot, c0=256, tot=512)
                    wk = wk1_ + wk3_
                    for tc in range(NTC):
                        sl = slice(tc * TW, (tc + 1) * TW)
                        psC, pkC = ps_rot()
                        S.mm(psC[:, :], ENp[:, ex, :], CTh[:, sl], True, False, [K("ENf"), K("CTh", tc), K("CThz")], [pkC])
                        S.mm(psC[:, :], ENp[:, ex, :], CTl[:, sl], False, True, [K("ENf"), K("CTl", tc), K("CTlz")], [pkC])
                        cb, cbk = CB[n % 2], K("CB", n % 2)
                        S.cp(cb[:], psC[:, :], [pkC], [cbk], eng="act")
                        for fc in range(2):
                            ps1, pk1 = ps_rot()
                            proj_fm(wv, wk, fc * 128, tc, ps1, pk1)
                            ps3, pk3 = ps_rot()
                            proj_fm(wv, wk, 256 + fc * 128, tc, ps3, pk3)
                            s_, sk = SL[(2 * n + fc) % 2], K("SL", (2 * n + fc) % 2)
                            S.act(s_[:], ps1[:, :], AF.Silu, [pk1], [sk])
                            S.tt(s_[:], s_[:], ps3[:, :], ALU.mult, [sk, pk3], [sk])
                            S.tt(HID[:, el * 2 + fc, sl], s_[:], cb[:], ALU.mult, [sk, cbk], [K("HID", el * 2 + fc, tc)])
                        n += 1
                    S.dma(W2S[:, 2 * el:2 * el + 2, :], w2_d[l, ex].rearrange("(fc p) n -> p fc n", p=128), [],
                          [K("W2S", el)], q="pool", arena=True)
                for oc in range(KC):
                    for tc in range(NTC):
                        ps, pk = ps_rot()
                        for j in range(8):
                            S.mm(ps[:, :], W2S[:, j, oc * 128:(oc + 1) * 128], HID[:, j, tc * TW:(tc + 1) * TW],
                                 j == 0, j == 7, [K("W2S", j // 2), K("HID", j, tc)], [pk])
                        S.tt(xT[:, oc, tc * TW:(tc + 1) * TW], xT[:, oc, tc * TW:(tc + 1) * TW], ps[:, :], ALU.add,
                             [K("xT", oc, tc), pk], [K("xT", oc, tc)])
        S.barrier()
        dump(f"x3_{l}", xT[:], ALLX, [128, KC, S_])
        if stop_after == f"M{l}":
            done = True
            break

    if stop_after is None:
        with ExitStack() as st:
            HF = sb("HFo", [128, KC, TW], stack=st)
            OB = [sb(f"OB{j}", [128, D_], stack=st) for j in range(2)]
            nob = [0]

            def final_hook(tc, r):
                sl = slice(tc * TW, (tc + 1) * TW)
                for kc in range(KC):
                    S.stt(HF[:, kc, :], xT[:, kc, sl], col(0, "fin_g", kc), r[:], ALU.mult, ALU.mult,
                          [K("xT", kc, tc), K("rsz", tc % 2), K("colp")], [K("HFo", kc)])
                for q in range(4):
                    ob, obk = OB[nob[0] % 2], K("OB", nob[0] % 2)
                    nob[0] += 1
                    for half in range(2):
                        ps, pk = ps_rot()
                        for j in range(4):
                            S.tr(ps[:, j * 128:(j + 1) * 128], HF[:, half * 4 + j, q * 128:(q + 1) * 128], ident,
                                 [K("HFo", half * 4 + j), K("c128")], [pk])
                        S.cp(ob[:, half * 512:(half + 1) * 512], ps[:, :], [pk], [obk],
                             eng=("act" if half else "dve"))
                    r0 = tc * TW + q * 128
                    i = S.dma(out_d[r0:r0 + 128, :], ob[:], [obk], [K("out", r0)])
                    S.final.append(i)

            rmsnorm_fm(xT, "xT", lambda kc: col(0, "fin_g", kc), None, None, KC, S_, st, hook=final_hook, tag="z")

    S.finalize(es)
    es.close()
    return nc, dump_d


def make_consts():
    c128 = np.zeros((128, 8, 128), np.float32)
    j = np.arange(128)[:, None]
    s = np.arange(128)[None, :]
    c128[:, 0] = np.eye(128)
    c128[:, 1] = 1.0
    c128[:, 2] = (j <= s)
    c128[:, 3] = -1.0 * (j >= s)
    c128[:, 4] = -1.0
    partner = np.where((np.arange(128) % 64) < 32, np.arange(128) + 32, np.arange(128) - 32)
    perm = np.zeros((128, 128), np.float32)
    perm[partner, np.arange(128)] = 1.0
    c128[:, 5] = perm
    c128[:, 6] = NEG * (s < j)
    cm = np.zeros((128, 12, 512), np.float32)
    p = np.arange(128)[:, None]
    f = np.arange(512)[None, :]
    for d in range(4):
        valid = (f - p) > d * 128
        cm[:, d] = valid
        cm[:, 4 + d] = NEG * (~valid)
        cm[:, 8 + d] = NEG * ((d * 128 + p) > f)
    half = 32
    inv = (np.float32(10000.0) ** (-(np.arange(half, dtype=np.float32)) / np.float32(half))).astype(np.float32)
    ang = np.arange(S_, dtype=np.float32)[None, :] * inv[:, None]
    cos = np.cos(ang).astype(np.float32)
    sin = np.sin(ang).astype(np.float32)
    rope = np.zeros((128, 2, S_), np.float32)
    for pp in range(128):
        d = pp % 64
        rope[pp, 0] = cos[d % 32]
        rope[pp, 1] = -sin[d % 32] if d < 32 else sin[d % 32]
    en = np.zeros((16, 16, 128), np.float32)
    for n in range(16):
        en[n, n, :] = 1.0
    gc = np.zeros((128, 3, 16, 8), np.float32)
    for i in range(16):
        own = i // 2
        for n in range(8):
            gc[:, 0, i, n] = 0.0 if n < own else -1e30
            gc[:, 1, i, n] = 1.0 if n < own else 0.0
            gc[:, 2, i, n] = 1.0 if n == own else 0.0
    blkoh = np.zeros((8, S_), np.float32)
    for n in range(8):
        blkoh[n, n * 256:(n + 1) * 256] = 1.0
    return dict(c128=c128.reshape(128, -1), cmask=cm.reshape(128, -1), rope=rope.reshape(128, -1),
                en=en.reshape(16, -1), gatec=gc.reshape(128, -1), blkoh=blkoh)


def colvec(v):
    v = np.asarray(v, np.float32)
    return np.ascontiguousarray(v.reshape(-1, 128).T)


def pack_params(inp):
    colp = np.zeros((128, L_, NCOL), np.float32)
    rowp = np.zeros((128, L_, NROW), np.float32)

    def put(l, name, arr):
        c0, w = COLS[name]
        assert arr.shape == (128, w), (name, arr.shape)
        colp[:, l, c0:c0 + w] = arr

    for l in range(L_):
        put(l, "mix_g", colvec(inp["mix_norm_g"][l]))
        put(l, "xat_g", colvec(inp["xattn_norm_g"][l]))
        put(l, "mem_g", colvec(inp["mem_norm_g"][l]))
        put(l, "ffn_g", colvec(inp["ffn_norm_g"][l]))
        put(l, "grp_g", colvec(inp["group_norm_g"][l]))
        cw = inp["lru_conv_w"][l]
        put(l, "lru_cw", np.concatenate([colvec(cw[k]) for k in range(4)], axis=1).reshape(128, 4, 2)
            .transpose(0, 2, 1).reshape(128, 8))
        put(l, "lru_cb", colvec(inp["lru_conv_b"][l]))
        put(l, "lru_br", colvec(inp["lru_br"][l]))
        put(l, "lru_bi", colvec(inp["lru_bi"][l]))
        put(l, "lru_lam", colvec(inp["lru_lambda"][l]))
        sw = inp["ssm_conv_w"][l]
        put(l, "ssm_cw", np.concatenate([colvec(sw[k]) for k in range(4)], axis=1).reshape(128, 4, 6)
            .transpose(0, 2, 1).reshape(128, 24))
        put(l, "ssm_cb", colvec(inp["ssm_conv_b"][l]))
        put(l, "ssm_d", colvec(np.repeat(inp["ssm_d"][l], 64)))
        put(l, "fin_g", colvec(inp["final_norm_g"]))
        r0, w = ROWS["dt_bias"]
        rowp[:, l, r0:r0 + w] = np.tile(inp["ssm_dt_bias"][l], 16)[None, :]
        r0, w = ROWS["a_log"]
        rowp[:, l, r0:r0 + w] = np.tile(inp["ssm_a_log"][l], 16)[None, :]
        r0, w = ROWS["bg"]
        rowp[:, l, r0:r0 + w] = inp["router_group_b"][l][None, :]
        r0, w = ROWS["be"]
        rowp[:, l, r0:r0 + w] = inp["router_expert_b"][l][None, :]
    wbd = np.zeros((L_, 2, 2, 128, 128), np.float32)
    for l in range(L_):
        for gi, nm in enumerate(("lru_wr", "lru_wi")):
            for c in range(2):
                for hh in range(2):
                    wbd[l, gi, c, hh * 64:(hh + 1) * 64, hh * 64:(hh + 1) * 64] = inp[nm][l, 2 * c + hh]
    router_w = np.concatenate([inp["router_group_w"], inp["router_expert_w"]], axis=2)
    return dict(colp=colp.reshape(128, -1), rowp=rowp.reshape(128, -1), lru_wbd=wbd,
                router_w=np.ascontiguousarray(router_w))


SHARED = ["w_in", "w_out", "xattn_wq", "xattn_wkv", "xattn_wo", "expert_w1", "expert_w3", "expert_w2"]


def make_in_maps(inputs, cores):
    inp = {k: np.asarray(v) for k, v in inputs.items()}
    consts = make_consts()
    packed = pack_params(inp)
    shared = {k: np.ascontiguousarray(inp[k], dtype=np.float32) for k in SHARED}
    maps = []
    for b in cores:
        m = dict(shared)
        m.update(consts)
        m.update(packed)
        m["x"] = np.ascontiguousarray(inp["x"][b])
        m["mem"] = np.ascontiguousarray(inp["mem"][b])
        maps.append(m)
    return maps


_CACHE = {}


def kernel(**inputs):
    if "nc" not in _CACHE:
        _CACHE["nc"] = build_program()[0]
    nc = _CACHE["nc"]
    maps = make_in_maps(inputs, list(range(8)))
    res = run_bass_kernel_spmd(nc, maps, core_ids=list(range(8)))
    return np.stack([r["out"] for r in res.results], axis=0).astype(np.float32)
```

```python
import numpy as np
from contextlib import ExitStack
import concourse.bass as bass
import concourse.mybir as mybir
from concourse.bass_utils import run_bass_kernel_spmd

F32 = mybir.dt.float32
BF16 = mybir.dt.bfloat16
F32R = mybir.dt.float32r
AF = mybir.ActivationFunctionType
ALU = mybir.AluOpType
AX = mybir.AxisListType

S_ = 2048
D_ = 1024
L_ = 2
KC = 8
NTC = 4
TW = 512
NT = 16
NEG = -30000.0
EPS = 1e-6

COLS = {}
_c = 0
for _n, _w in [("mix_g", 8), ("xat_g", 8), ("mem_g", 8), ("ffn_g", 8), ("grp_g", 8),
               ("lru_cw", 8), ("lru_cb", 2), ("lru_br", 2), ("lru_bi", 2), ("lru_lam", 2),
               ("ssm_cw", 24), ("ssm_cb", 6), ("ssm_d", 2), ("fin_g", 8)]:
    COLS[_n] = (_c, _w)
    _c += _w
NCOL = _c
ROWS = {}
_c = 0
for _n, _w in [("dt_bias", 64), ("a_log", 64), ("bg", 4), ("be", 16)]:
    ROWS[_n] = (_c, _w)
    _c += _w
NROW = _c


def _free(ap):
    n = 1
    for d in ap.shape[1:]:
        n *= int(d)
    return n


DEBUG_LINES = False


class Sched:
    ENG = ("pe", "act", "dve", "pool", "sp")
    GATED = ("pe", "act", "dve", "sp")

    def __init__(self, nc):
        self.nc = nc
        self.ops = []
        self.lastw = {}
        self.rd = {}
        self.by_eng = {e: [] for e in self.ENG}
        self.final = []
        self.psn = 0
        self.seg = 0
        self.act_inorder = set()

    def add(self, eng, fn, reads=(), writes=(), dma=False, cost=0.3, arena=False):
        i = len(self.ops)
        reads = list(reads)
        writes = list(writes)
        deps = {}
        for r in reads:
            w = self.lastw.get(r)
            if w is not None:
                deps.setdefault(w, set()).add("raw")
        for w_ in writes:
            w = self.lastw.get(w_)
            if w is not None:
                deps.setdefault(w, set()).add("waw")
            for r in self.rd.get(w_, ()):
                deps.setdefault(r, set()).add("war")
        ws = set(writes)
        for r in reads:
            if r not in ws:
                self.rd.setdefault(r, []).append(i)
        for w_ in writes:
            self.lastw[w_] = i
            self.rd[w_] = []
        deps.pop(i, None)
        fdeps = set()
        for d, kinds in deps.items():
            od = self.ops[d]
            if od["eng"] == eng and not od["dma"] and not dma:
                if eng == "pe":
                    continue
                if "raw" not in kinds:
                    continue
            fdeps.add(d)
        ln_ = 0
        if DEBUG_LINES:
            import sys as _sys
            f_ = _sys._getframe(1)
            while f_ is not None and f_.f_code.co_name != "build_program":
                f_ = f_.f_back
            ln_ = f_.f_lineno if f_ is not None else 0
        self.ops.append(dict(eng=eng, fn=fn, deps=fdeps, order=set(deps.keys()), dma=dma, signal=dma, token=None,
                             seg=self.seg, cost=cost, arena=arena, line=ln_))
        self.by_eng[eng].append(i)
        return i

    def barrier(self):
        self.seg += 1

    def schedule(self, W=24):
        ops = self.ops
        n = len(ops)
        succ = [[] for _ in range(n)]
        nun = [0] * n
        for i, op in enumerate(ops):
            nun[i] = len(op["order"])
            for d in op["order"]:
                succ[d].append(i)
        ready = [0.0] * n
        fin = [0.0] * n
        done = [False] * n
        head = {e: 0 for e in self.ENG}
        free = {e: 0.0 for e in self.ENG}
        lists = {e: list(self.by_eng[e]) for e in self.ENG}
        new_order = {e: [] for e in self.ENG}
        nseg = self.seg + 1
        rem = [0] * (nseg + 1)
        for i, op in enumerate(ops):
            if op["eng"] in self.GATED:
                rem[op["seg"]] += 1
        import os
        WDEF = {"pe": W, "act": W, "dve": W}
        WE = {e_: int(os.environ.get("KSCHED_W_" + e_.upper(), WDEF[e_])) for e_ in ("pe", "act", "dve")}
        segbusy = {}
        segend = {}
        cur = 0
        seg_t0 = 0.0
        tmax = 0.0
        nsched = 0
        while nsched < n:
            while cur < nseg and rem[cur] == 0:
                cur += 1
                seg_t0 = tmax
            best = None
            for e in self.ENG:
                lst = lists[e]
                h = head[e]
                while h < len(lst) and done[lst[h]]:
                    h += 1
                head[e] = h
                w = WE.get(e, 1)
                if e == "act" and cur in self.act_inorder:
                    w = 1
                seen = 0
                j = h
                while j < len(lst) and seen < w:
                    i = lst[j]
                    j += 1
                    if done[i]:
                        continue
                    op = ops[i]
                    if e != "pool":
                        if op["seg"] != cur:
                            break
                    elif op["arena"] and op["seg"] > cur:
                        break
                    seen += 1
                    if nun[i] == 0:
                        est = max(free[e], ready[i])
                        if e != "pool" or op["arena"]:
                            est = max(est, seg_t0)
                        key = (est, i)
                        if best is None or key < best[0]:
                            best = (key, e, i)
            if best is None:
                raise RuntimeError("scheduler stuck at seg %d" % cur)
            (est, _), e, i = best
            op = ops[i]
            if op["dma"]:
                free[e] = est + 0.06
                f = est + op["cost"]
            else:
                f = est + op["cost"]
                free[e] = f
            fin[i] = f
            tmax = max(tmax, f)
            if not op["dma"]:
                segbusy[(op["seg"], e)] = segbusy.get((op["seg"], e), 0.0) + op["cost"]
            segend[op["seg"]] = max(segend.get(op["seg"], 0.0), f)
            done[i] = True
            nsched += 1
            new_order[e].append(i)
            if e in self.GATED:
                rem[op["seg"]] -= 1
            for s_ in succ[i]:
                nun[s_] -= 1
                lat = 0.0 if (ops[s_]["eng"] == e and not op["dma"]) else 0.1
                if f + lat > ready[s_]:
                    ready[s_] = f + lat
        self.by_eng = new_order
        self.sim_time = tmax
        import os
        if os.environ.get("KSCHED_DEBUG"):
            print("sched: ops", n, "sim_time_us", round(tmax, 1), flush=True)
            prev = 0.0
            for sg in sorted(segend):
                print("  seg", sg, "dur", round(segend[sg] - prev, 1), {e_: round(segbusy.get((sg, e_), 0.0), 1)
                                                                         for e_ in ("pe", "act", "dve")})
                prev = segend[sg]
        pos = {}
        for e in self.ENG:
            for k, i in enumerate(new_order[e]):
                pos[i] = k
        bar = [set() for _ in range(nseg + 1)]
        for s_ in range(1, nseg + 1):
            for e in ("pe", "act", "dve"):
                last = None
                for i in new_order[e]:
                    if ops[i]["seg"] < s_:
                        last = i
                    else:
                        break
                if last is not None:
                    bar[s_].add(last)
            for i in new_order["sp"]:
                if ops[i]["seg"] == s_ - 1 and ops[i]["dma"]:
                    bar[s_].add(i)
        for e in self.GATED:
            lastseg = 0
            for i in new_order[e]:
                sg = ops[i]["seg"]
                if sg > lastseg:
                    for t in range(lastseg + 1, sg + 1):
                        ops[i]["deps"] |= bar[t]
                    lastseg = sg
        for i in new_order["pool"]:
            if ops[i]["arena"]:
                ops[i]["deps"] |= bar[ops[i]["seg"]]
        for i, op in enumerate(ops):
            keep = set()
            bestp = {}
            for d in op["deps"]:
                if d == i:
                    continue
                od = ops[d]
                if od["dma"]:
                    keep.add(d)
                else:
                    pe_ = od["eng"]
                    if pe_ not in bestp or pos[d] > pos[bestp[pe_]]:
                        bestp[pe_] = d
            keep |= set(bestp.values())
            op["deps"] = keep

    def mm(self, out, lhsT, rhs, start, stop, reads, writes):
        nfree = _free(rhs)
        c = max(nfree, 64) / 2400.0 * (4.0 if rhs.dtype == F32 else 1.0) + 0.004
        return self.add("pe", lambda e: e.matmul(out, lhsT, rhs, start=start, stop=stop), reads, writes, cost=c)

    def tr(self, out, in_, ident, reads, writes):
        return self.add("pe", lambda e: e.transpose(out, in_, ident), reads, writes, cost=0.25)

    def act(self, out, in_, func, reads, writes, bias=None, scale=None, accum=None):
        kw = {}
        if bias is not None:
            kw["bias"] = bias
        if scale is not None:
            kw["scale"] = scale
        if accum is not None:
            kw["accum_out"] = accum
        c = 0.22 + _free(out) / 1400.0
        return self.add("act", lambda e: e.activation(out=out, in_=in_, func=func, **kw), reads, writes, cost=c)

    def _vc(self, out):
        return 0.08 + _free(out) / 960.0

    def tt(self, out, in0, in1, op, reads, writes, eng="dve"):
        return self.add(eng, lambda e: e.tensor_tensor(out=out, in0=in0, in1=in1, op=op), reads, writes,
                        cost=self._vc(out))

    def ts(self, out, in0, s1, s2, op0, op1, reads, writes, eng="dve"):
        if s2 is None:
            return self.add(eng, lambda e: e.tensor_scalar(out, in0, s1, None, op0), reads, writes, cost=self._vc(out))
        return self.add(eng, lambda e: e.tensor_scalar(out, in0, s1, s2, op0, op1), reads, writes, cost=self._vc(out))

    def stt(self, out, in0, scalar, in1, op0, op1, reads, writes, eng="dve"):
        return self.add(eng, lambda e: e.scalar_tensor_tensor(out=out, in0=in0, scalar=scalar, in1=in1,
                                                             op0=op0, op1=op1), reads, writes, cost=self._vc(out))

    def cp(self, out, in_, reads, writes, eng="dve"):
        if eng == "act":
            return self.add("act", lambda e: e.activation(out=out, in_=in_, func=AF.Copy), reads, writes,
                            cost=0.22 + _free(out) / 1400.0)
        return self.add(eng, lambda e: e.tensor_copy(out=out, in_=in_), reads, writes, cost=self._vc(out))

    def recip(self, out, in_, reads, writes):
        return self.add("dve", lambda e: e.reciprocal(out=out, in_=in_), reads, writes, cost=0.1 + _free(out) / 240.0)

    def dma(self, out, in_, reads, writes, q="sp", arena=False):
        nbytes = 128 * _free(out) * 4
        return self.add(q, lambda e: e.dma_start(out=out, in_=in_), reads, writes, dma=True,
                        cost=2.0 + nbytes / 150e3, arena=arena)

    def finalize(self, es):
        nc = self.nc
        self.schedule()
        for op in self.ops:
            for d in op["deps"]:
                self.ops[d]["signal"] = True
        for d in self.final:
            self.ops[d]["signal"] = True
        csem = {e: es.enter_context(nc.semaphore("c_" + e)) for e in ("pe", "act", "dve", "pool")}
        NP = 16
        qsem = {q: [es.enter_context(nc.semaphore(f"q_{q}{k}")) for k in range(NP)] for q in ("sp", "pool")}
        cnt = {e: 0 for e in csem}
        qn = {q: 0 for q in qsem}
        quse = {q: [0] * NP for q in qsem}
        qprev = {q: [None] * NP for q in qsem}
        for i, op in enumerate(self.ops):
            if op["dma"]:
                q = op["eng"]
                k = qn[q] % NP
                qn[q] += 1
                quse[q][k] += 1
                op["token"] = (qsem[q][k], 16 * quse[q][k])
                if qprev[q][k] is not None:
                    op["deps"].add(qprev[q][k])
                qprev[q][k] = i
        for e in csem:
            for i in self.by_eng[e]:
                op = self.ops[i]
                if (not op["dma"]) and op["signal"]:
                    cnt[e] += 1
                    op["token"] = (csem[e], cnt[e])
        ops = self.ops
        final = list(self.final)

        def emit(engname, e):
            waited = {}
            for i in self.by_eng[engname]:
                op = ops[i]
                for d in sorted(op["deps"]):
                    sem, val = ops[d]["token"]
                    if waited.get(sem, 0) < val:
                        e.wait_ge(sem, val)
                        waited[sem] = val
                ins = op["fn"](e)
                if op["signal"]:
                    ins.then_inc(op["token"][0], 16 if op["dma"] else 1)
            if engname == "sp":
                for d in final:
                    sem, val = ops[d]["token"]
                    if waited.get(sem, 0) < val:
                        e.wait_ge(sem, val)
                        waited[sem] = val

        with nc.Block() as block:
            @block.tensor
            def _(e):
                emit("pe", e)

            @block.scalar
            def _(e):
                emit("act", e)

            @block.vector
            def _(e):
                emit("dve", e)

            @block.gpsimd
            def _(e):
                emit("pool", e)

            @block.sync
            def _(e):
                emit("sp", e)


def K(name, *idx):
    return (name,) + idx


def build_program(stop_after=None, dumps=(), n_layers=L_):
    nc = bass.Bass("TRN2", target_bir_lowering=False)
    S = Sched(nc)
    es = ExitStack()
    P = {}

    def din(name, shape):
        P[name] = nc.dram_tensor(name, list(shape), F32, kind="ExternalInput").ap()
        return P[name]

    x_d = din("x", [S_, D_])
    mem_d = din("mem", [256, D_])
    w_in_d = din("w_in", [L_, D_, 3076])
    w_out_d = din("w_out", [L_, D_, D_])
    wq_d = din("xattn_wq", [L_, D_, D_])
    wkv_d = din("xattn_wkv", [L_, D_, 2 * D_])
    wo_d = din("xattn_wo", [L_, D_, D_])
    w1_d = din("expert_w1", [L_, 16, D_, 256])
    w3_d = din("expert_w3", [L_, 16, D_, 256])
    w2_d = din("expert_w2", [L_, 16, 256, D_])
    wrt_d = din("router_w", [L_, D_, 20])
    lrubd_d = din("lru_wbd", [L_, 2, 2, 128, 128])
    colp_d = din("colp", [128, L_ * NCOL])
    rowp_d = din("rowp", [128, L_ * NROW])
    c128_d = din("c128", [128, 8 * 128])
    cmask_d = din("cmask", [128, 12 * 512])
    rope_d = din("rope", [128, 2 * S_])
    en_d = din("en", [16, 16 * 128])
    blkoh_d = din("blkoh", [8, S_])
    gate_c_d = din("gatec", [128, 3 * 128])
    out_d = nc.dram_tensor("out", [S_, D_], F32, kind="ExternalOutput").ap()
    dump_d = {}

    uid = [0]

    def sb(name, shape, dt=F32, stack=None):
        uid[0] += 1
        return (stack or es).enter_context(nc.sbuf_tensor(f"{name}_{uid[0]}", list(shape), dt))

    xT = sb("xT", [128, KC, S_])
    hT = sb("hT", [128, KC, S_], BF16)
    colp = sb("colp_s", [128, L_ * NCOL])
    rowp = sb("rowp_s", [128, L_ * NROW])
    c128 = sb("c128_s", [128, 8 * 128])
    c128b = sb("c128b_s", [128, 2 * 128], BF16)
    W = [sb(f"wslot{i}", [128, KC, 512], BF16) for i in range(3)]
    PS = [es.enter_context(nc.psum_tensor(f"ps{i}", [128, 512], F32)) for i in range(8)]
    wn = [0]

    ident = c128[:, 0:128]
    ones_f = c128[:, 128:256]
    tri_le = c128[:, 256:384]
    negu = c128[:, 384:512]
    negones = c128[:, 512:640]
    perm_f = c128[:, 640:768]
    ssdneg = c128[:, 768:896]
    ones_b = c128b[:, 128:256]
    ident_b = c128b[:, 0:128]

    cst = sb("cst", [128, 4])
    S.add("dve", lambda e: e.memset(cst[:, 0:1], EPS), [], [K("cst0")])
    S.add("dve", lambda e: e.memset(cst[:, 1:2], 1.0), [], [K("cst1")])
    S.add("dve", lambda e: e.memset(cst[:, 2:4], 0.0), [K("cst0"), K("cst1")], [K("cst")])
    S.dma(colp[:], colp_d[:, :], [], [K("colp")])
    S.dma(rowp[:], rowp_d[:, :], [], [K("rowp")])
    S.dma(c128[:], c128_d[:, :], [], [K("c128")])
    S.dma(c128b[:], c128_d[:, 0:256], [], [K("c128b")], q="pool")

    def col(l, name, j=0, n=1):
        c0, w = COLS[name]
        return colp[:, l * NCOL + c0 + j: l * NCOL + c0 + j + n]

    def row(l, name, j=0, n=None):
        c0, w = ROWS[name]
        n = w if n is None else n
        return rowp[:, l * NROW + c0 + j: l * NROW + c0 + j + n]

    nrot = [8]

    def ps_rot():
        k = S.psn % nrot[0]
        S.psn += 1
        return PS[k], K("ps", k)

    Wf = [w[:].rearrange("p a b -> p (a b)") for w in W]

    def load_w(src2d, nk, ncols, slot=None, c0=0, tot=None):
        tot = ncols if tot is None else tot
        if slot is None:
            k = wn[0] % 3
            wn[0] += 1
        else:
            k = slot
        v = Wf[k][:, 0:nk * tot].rearrange("p (a b) -> p a b", a=nk)
        step = 2 if nk >= 2 else 1
        allk = [K("w", k, j_, hf_) for j_ in range(4) for hf_ in range(2)]
        for h in range(0, nk, step):
            if nk == 8 and tot == 512:
                halves = [hf_ for hf_ in range(2) if c0 < (hf_ + 1) * 256 and c0 + ncols > hf_ * 256]
                wkeys_ = [K("w", k, h // 2, hf_) for hf_ in halves]
            else:
                wkeys_ = allk
            S.dma(v[:, h:h + step, c0:c0 + ncols],
                  src2d[h * 128:(h + step) * 128, :].rearrange("(kc p) n -> p kc n", p=128),
                  [], wkeys_, q="pool")
        return v, allk, k

    def dump(name, ap, reads, shape):
        if name in dumps:
            d = nc.dram_tensor("dbg_" + name, list(shape), ap.dtype, kind="ExternalOutput").ap()
            dump_d[name] = d
            i = S.dma(d, ap, reads, [K("dbg", name)])
            S.final.append(i)

    def load_fm(dst, src_d, ntile, dkey, st):
        xin = [sb(f"xin{j}", [128, D_], stack=st) for j in range(2)]
        for i in range(ntile):
            b = xin[i % 2]
            S.dma(b[:], src_d[i * 128:(i + 1) * 128, :], [], [K("xin", i % 2)])
            for half in range(2):
                ps, pk = ps_rot()
                for j in range(4):
                    kc = half * 4 + j
                    S.tr(ps[:, j * 128:(j + 1) * 128], b[:, kc * 128:(kc + 1) * 128], ident,
                         [K("xin", i % 2), K("c128")], [pk])
                S.cp(dst[:, half * 4:half * 4 + 4, i * 128:(i + 1) * 128],
                     ps[:].rearrange("p (a b) -> p a b", a=4), [pk],
                     [K(dkey, half * 4 + j, i // 4) for j in range(4)], eng=("dve" if half == 0 else "act"))

    st0 = ExitStack()
    load_fm(xT, x_d, NT, "xT", st0)

    def rmsnorm_fm(src, skey, gcol, dst, dkey, nk, T, st, dscale=1.0, hook=None, tag="n"):
        tw = min(T, TW)
        sq = [sb(f"sq_{tag}{j}", [128, nk, tw], BF16, stack=st) for j in range(2)]
        rs = [sb(f"rs_{tag}{j}", [128, tw], stack=st) for j in range(2)]
        for tc in range(T // tw):
            sl = slice(tc * tw, (tc + 1) * tw)
            q = sq[tc % 2]
            for kc in range(nk):
                S.act(q[:, kc, :], src[:, kc, sl], AF.Square, [K(skey, kc, tc)], [K("sq" + tag, tc % 2, kc)])
            ps, pk = ps_rot()
            for kc in range(nk):
                S.mm(ps[:, 0:tw], ones_b, q[:, kc, :], kc == 0, kc == nk - 1,
                     [K("sq" + tag, tc % 2, kc), K("c128b")], [pk])
            r = rs[tc % 2]
            S.act(r[:], ps[:, 0:tw], AF.Sqrt, [pk, K("cst")], [K("rs" + tag, tc % 2)],
                  bias=cst[:, 0:1], scale=1.0 / (nk * 128))
            S.add("dve", (lambda e, r=r: e.reciprocal(out=r[:], in_=r[:])),
                  [K("rs" + tag, tc % 2)], [K("rs" + tag, tc % 2)], cost=0.1 + tw / 240.0)
            if dst is not None:
                for kc in range(nk):
                    S.stt(dst[:, kc, sl], src[:, kc, sl], gcol(kc), r[:], ALU.mult, ALU.mult,
                          [K(skey, kc, tc), K("rs" + tag, tc % 2), K("colp")], [K(dkey, kc, tc)],
                          eng="dve")
            if hook is not None:
                hook(tc, r)

    ALLH = [K("hT", kc, tc) for kc in range(KC) for tc in range(NTC)]

    def hkeys(tc):
        return [K("hT", kc, tc) for kc in range(KC)]

    def proj_fm(wv, wkeys, c0, tc, ps, pk):
        for kc in range(KC):
            S.mm(ps[:, :], wv[:, kc, c0:c0 + 128], hT[:, kc, tc * TW:(tc + 1) * TW], kc == 0, kc == KC - 1,
                 wkeys + [K("hT", kc, tc)], [pk])

    def conv_chunk(ps, pk, tc, xw, xwk, cwcol, cbcol, dst, dkey):
        w_ = xw[tc % 2]
        wk = K(xwk, tc % 2)
        if tc == 0:
            S.add("dve", lambda e: e.memset(w_[:, 0:3], 0.0), [], [wk])
        else:
            S.cp(w_[:, 0:3], xw[(tc - 1) % 2][:, 512:515], [K(xwk, (tc - 1) % 2)], [wk])
        S.cp(w_[:, 3:515], ps[:, :], [pk], [wk], eng="act")
        S.ts(dst, w_[:, 3:515], cwcol(3), cbcol, ALU.mult, ALU.add, [wk, K("colp")], [dkey])
        for k in range(3):
            S.stt(dst, w_[:, k:k + 512], cwcol(k), dst, ALU.mult, ALU.add, [wk, dkey, K("colp")], [dkey])

    def group_out(l, g, YG, st):
        YB = sb("YB", [128, 2, S_], BF16, stack=st)
        rmsnorm_fm(YG, "YG", lambda kc: col(l, "grp_g", 2 * g + kc), YB, "YB", 2, S_, st, tag="g")
        dump(f"yn{l}_{g}", YB[:], [K("YB", c, t) for c in range(2) for t in range(NTC)], [128, 2, S_])
        for half in range(2):
            wv, wk, _ = load_w(w_out_d[l, g * 256:(g + 1) * 256, half * 512:(half + 1) * 512], 2, 512)
            for o4 in range(4):
                oc = half * 4 + o4
                for tc in range(NTC):
                    ps, pk = ps_rot()
                    for kc in range(2):
                        S.mm(ps[:, :], wv[:, kc, o4 * 128:(o4 + 1) * 128], YB[:, kc, tc * TW:(tc + 1) * TW],
                             kc == 0, kc == 1, wk + [K("YB", kc, tc)], [pk])
                    S.tt(xT[:, oc, tc * TW:(tc + 1) * TW], xT[:, oc, tc * TW:(tc + 1) * TW], ps[:, :], ALU.add,
                         [K("xT", oc, tc), pk], [K("xT", oc, tc)])

    ALLX = [K("xT", kc, tc) for kc in range(KC) for tc in range(NTC)]
    done = False
    for l in range(n_layers):
        with ExitStack() as st:
            rmsnorm_fm(xT, "xT", lambda kc: col(l, "mix_g", kc), hT, "hT", KC, S_, (st0 if l == 0 else st), tag="a")
        if l == 0:
            st0.close()
        S.barrier()
        if l == 0:
            dump("h0", hT[:], ALLH, [128, KC, S_])
        if stop_after == "norm0":
            break

        with ExitStack() as sty, ExitStack() as st:
            YG = sb("YG", [128, 2, S_], stack=sty)
            wv, wk, _ = load_w(w_in_d[l, :, 0:512], 8, 512)
            wbd = sb("wbd", [128, 4, 128], BF16, stack=st)
            S.dma(wbd[:], lrubd_d[l].rearrange("g c k m -> k (g c) m"), [], [K("wbd")], q="pool", arena=True)
            c8 = sb("c8", [128, 2], stack=st)
            dump(f"A_wbd{l}", wbd[:], [K("wbd")], [128, 4, 128])
            cy = sb("cy", [128, 2], stack=st)
            cz = sb("cz", [128, 2], stack=st)
            S.act(cy[:], col(l, "lru_lam", 0, 2), AF.Exp, [K("colp")], [K("cy")], scale=-1.0)
            S.ts(cz[:], cy[:], 2.0, None, ALU.add, None, [K("cy")], [K("cz")])
            S.add("dve", lambda e: e.reciprocal(out=cz[:], in_=cz[:]), [K("cz")], [K("cz")])
            S.tt(cz[:], cz[:], cy[:], ALU.mult, [K("cz"), K("cy")], [K("cz")])
            S.tt(cy[:], cz[:], cz[:], ALU.mult, [K("cz")], [K("cy")])
            S.ts(c8[:], cy[:], 1.0 / 9.0, 1.0 / 7.0, ALU.mult, ALU.add, [K("cy")], [K("c8")])
            for cc_ in (1.0 / 5.0, 1.0 / 3.0, 1.0):
                S.tt(c8[:], c8[:], cy[:], ALU.mult, [K("c8"), K("cy")], [K("c8")])
                S.ts(c8[:], c8[:], cc_, None, ALU.add, None, [K("c8")], [K("c8")])
            S.tt(c8[:], c8[:], cz[:], ALU.mult, [K("c8"), K("cz")], [K("c8")])
            S.ts(c8[:], c8[:], -16.0, None, ALU.mult, None, [K("c8")], [K("c8")])
            xw = [sb(f"xw{j}", [128, 515], stack=st) for j in range(2)]
            XC = sb("XC", [128, S_], stack=st)
            XCB = sb("XCB", [128, S_], BF16, stack=st)
            GG = sb("GG", [128, S_], stack=st)
            RA = sb("RA", [128, S_], stack=st)
            IU = sb("IU", [128, S_], stack=st)
            T2 = sb("T2", [128, S_], stack=st)
            XX = sb("XX", [128, S_], stack=st)
            for c in range(2):
                for tc in range(NTC):
                    sl = slice(tc * TW, (tc + 1) * TW)
                    ps, pk = ps_rot()
                    proj_fm(wv, wk, c * 128, tc, ps, pk)
                    conv_chunk(ps, pk, tc, xw, "xw", lambda k: col(l, "lru_cw", c * 4 + k), col(l, "lru_cb", c),
                               XC[:, sl], K("XC", tc))
                    ps, pk = ps_rot()
                    proj_fm(wv, wk, 256 + c * 128, tc, ps, pk)
                    S.cp(GG[:, sl], ps[:, :], [pk], [K("GG", tc)], eng="act")
                XCk = [K("XC", t) for t in range(NTC)]
                GGk = [K("GG", t) for t in range(NTC)]
                S.tt(T2[:], GG[:], GG[:], ALU.mult, GGk, [K("T2")])
                S.ts(T2[:], T2[:], 0.044715, 1.0, ALU.mult, ALU.add, [K("T2")], [K("T2")])
                S.tt(T2[:], T2[:], GG[:], ALU.mult, [K("T2")] + GGk, [K("T2")])
                S.act(T2[:], T2[:], AF.Sigmoid, [K("T2")], [K("T2")], scale=1.5957691216)
                S.tt(GG[:], GG[:], T2[:], ALU.mult, [K("T2")] + GGk, GGk)
                S.cp(XCB[:], XC[:], XCk, [K("XCB")], eng="act")
                for tc in range(NTC):
                    sl = slice(tc * TW, (tc + 1) * TW)
                    ps, pk = ps_rot()
                    S.mm(ps[:, :], wbd[:, c, :], XCB[:, sl], True, True, [K("wbd"), K("XCB")], [pk])
                    S.act(RA[:, sl], ps[:, :], AF.Sigmoid, [pk, K("colp")], [K("RA", tc)], bias=col(l, "lru_br", c))
                    ps, pk = ps_rot()
                    S.mm(ps[:, :], wbd[:, 2 + c, :], XCB[:, sl], True, True, [K("wbd"), K("XCB")], [pk])
                    S.act(IU[:, sl], ps[:, :], AF.Sigmoid, [pk, K("colp")], [K("IU", tc)], bias=col(l, "lru_bi", c))
                RAk = [K("RA", t) for t in range(NTC)]
                IUk = [K("IU", t) for t in range(NTC)]
                if c == 1:
                    dump(f"A_r{l}", RA[:], RAk, [128, S_])
                    dump(f"A_i{l}", IU[:], IUk, [128, S_])
                    dump(f"A_xcb{l}", XCB[:], [K("XCB")], [128, S_])
                    dump(f"A_xc{l}", XC[:], XCk, [128, S_])
                S.ts(XX[:], RA[:], c8[:, c:c + 1], 2.0, ALU.mult, ALU.mult, RAk + [K("c8")], [K("XX")])
                S.ts(T2[:], XX[:], 1.0 / 120.0, None, ALU.mult, None, [K("XX")], [K("T2")])
                for cc_ in (1.0 / 24.0, 1.0 / 6.0, 0.5, 1.0):
                    S.stt(T2[:], T2[:], cc_, XX[:], ALU.add, ALU.mult, [K("T2"), K("XX")], [K("T2")])
                S.act(T2[:], T2[:], AF.Sqrt, [K("T2")], [K("T2")], scale=-1.0)
                S.act(RA[:], XX[:], AF.Exp, [K("XX")], RAk, scale=0.5)
                S.tt(IU[:], IU[:], XC[:], ALU.mult, IUk + XCk, IUk)
                S.tt(IU[:], IU[:], T2[:], ALU.mult, IUk + [K("T2")], IUk)
                S.add("dve", lambda e: e.tensor_tensor_scan(out=XC[:], data0=RA[:], data1=IU[:], initial=0.0,
                                                            op0=ALU.mult, op1=ALU.add), RAk + IUk, XCk, cost=2.6)
                S.tt(YG[:, c, :], XC[:], GG[:], ALU.mult, XCk + GGk, [K("YG", c, t) for t in range(NTC)])
            for nm_, t_ in (("XC", XC), ("GG", GG), ("RA", RA), ("IU", IU), ("T2", T2)):
                dump(f"A_{nm_}{l}", t_[:], [K(nm_, t) for t in range(NTC)] + [K(nm_)], [128, S_])
            dump(f"ya{l}", YG[:], [K("YG", c, t) for c in range(2) for t in range(NTC)], [128, 2, S_])
            st.close()
            S.barrier()
            group_out(l, 0, YG, sty)
        S.barrier()
        if stop_after == f"A{l}":
            done = True
            break

        nrot[0] = 4
        with ExitStack() as sty, ExitStack() as st:
            YG = sb("YG", [128, 2, S_], stack=sty)
            QB = sb("QB", [128, 2, S_], BF16, stack=st)
            KZ = sb("KZ", [128, 4, S_], BF16, stack=st)
            S.add("dve", lambda e: e.memset(KZ[:], 0.0), [], [K("KZ", h_, t_) for h_ in range(4) for t_ in range(NTC)], cost=4.5)
            VB = sb("VB", [128, NT, 256], BF16, stack=st)
            M01 = sb("M01", [128, 4, 512], stack=st)
            NM = sb("NM", [128, 4, 512], BF16, stack=st)
            S.dma(M01[:], cmask_d[:, 0:2048].rearrange("p (a b) -> p a b", a=4), [], [K("M01")])
            S.dma(NM[:], cmask_d[:, 2048:4096].rearrange("p (a b) -> p a b", a=4), [], [K("NM")], q="pool", arena=True)
            wv, wk, _ = load_w(w_in_d[l, :, 512:1024], 8, 512)
            wv2, wk2, _ = load_w(w_in_d[l, :, 1024:1280], 8, 256)
            for c2 in range(2):
                for tc in range(NTC):
                    sl = slice(tc * TW, (tc + 1) * TW)
                    ps, pk = ps_rot()
                    proj_fm(wv, wk, c2 * 128, tc, ps, pk)
                    S.ts(QB[:, c2, sl], ps[:, :], 0.125, None, ALU.mult, None, [pk], [K("QB", c2, tc)])
                    ps, pk = ps_rot()
                    proj_fm(wv, wk, 256 + c2 * 128, tc, ps, pk)
                    S.cp(KZ[0:64, 2 * c2, sl], ps[0:64, :], [pk], [K("KZ", 2 * c2, tc)])
                    S.cp(KZ[64:128, 2 * c2 + 1, sl], ps[64:128, :], [pk], [K("KZ", 2 * c2 + 1, tc)])
            for i in range(NT):
                ps, pk = ps_rot()
                for kc in range(KC):
                    S.mm(ps[:, 0:256], hT[:, kc, i * 128:(i + 1) * 128], wv2[:, kc, :], kc == 0, kc == KC - 1,
                         wk2 + [K("hT", kc, i // 4)], [pk])
                S.cp(VB[:, i, :], ps[:, 0:256], [pk], [K("VB", i)], eng=("dve" if i % 2 else "act"))
            Eb = [sb(f"Eb{j}", [128, 512], stack=st) for j in range(2)]
            SPb = [sb(f"SPb{j}", [128, 512], F32R, stack=st) for j in range(3)]
            Wb = [sb(f"Wb{j}", [128, 512], BF16, stack=st) for j in range(2)]
            Rb = [sb(f"Rb{j}", [128, 512], F32R, stack=st) for j in range(2)]
            NUr = sb("NUr", [128, 2, 128], F32R, stack=st)
            S.cp(NUr[:, 0, :], negu, [K("c128")], [K("NUr")])
            S.cp(NUr[:, 1, :], negones, [K("c128")], [K("NUr")])
            items = []
            grp = 0
            for h in range(4):
                for c in range(NTC):
                    nb = 4 * c + 4
                    for idx, b in enumerate(range(nb - 1, -1, -1)):
                        items.append(dict(h=h, c=c, idx=idx, b=b, grp=grp, last=(b == 0)))
                    grp += 1
            for i_, itm in enumerate(items):
                itm["i"] = i_
                itm["pr"] = slice((itm["h"] % 2) * 64, (itm["h"] % 2) * 64 + 64)
                itm["c2"] = itm["h"] // 2
                itm["d"] = itm["b"] - 4 * itm["c"]
                itm["ksl"] = slice(itm["b"] * 128, (itm["b"] + 1) * 128)
                itm["qsl"] = slice(itm["c"] * TW, (itm["c"] + 1) * TW)
                itm["qk"] = [K("KZ", itm["h"], itm["b"] // 4), K("QB", itm["c2"], itm["c"])]

            def sb_s1(m):
                i = m["i"]
                pr, c2 = m["pr"], m["c2"]
                off = 128 * max(m["d"], 0)
                cs = slice(off, TW)
                qs = slice(m["c"] * TW + off, (m["c"] + 1) * TW)
                if m["idx"] == 0:
                    R0, R0k = Rb[m["grp"] % 2], K("Rb", m["grp"] % 2)
                    S.ts(R0[:], M01[:, 0, :], 0.0, None, ALU.mult, None, [K("M01")], [R0k])
                psZ, pkZ = ps_rot()
                S.mm(psZ[:, cs], KZ[:, m["h"], m["ksl"]], QB[:, c2, qs], True, True, m["qk"], [pkZ])
                E, Ek = Eb[i % 2], K("Eb", i % 2)
                SP, SPk = SPb[i % 3], K("SPb", i % 3)
                S.act(E[:, cs], psZ[:, cs], AF.Exp, [pkZ], [Ek])
                S.act(SP[:, cs], E[:, cs], AF.Ln, [Ek, K("cst")], [SPk], bias=cst[:, 1:2])
                if m["d"] >= 0:
                    S.tt(SP[:, cs], SP[:, cs].bitcast(F32), M01[:, m["d"], cs], ALU.mult, [SPk, K("M01")], [SPk])

            def sb_s2(m):
                i = m["i"]
                pr, c2, d = m["pr"], m["c2"], m["d"]
                off = 128 * max(d, 0)
                cs = slice(off, TW)
                qs = slice(m["c"] * TW + off, (m["c"] + 1) * TW)
                SP, SPk = SPb[i % 3], K("SPb", i % 3)
                Wt, Wk_ = Wb[i % 2], K("Wb", i % 2)
                R, Rk = Rb[m["grp"] % 2], K("Rb", m["grp"] % 2)
                psA, pkA = ps_rot()
                has_r = m["idx"] > 0
                S.mm(psA[:, cs], KZ[:, m["h"], m["ksl"]], QB[:, c2, qs], True, False, m["qk"], [pkA])
                S.mm(psA[:, cs], NUr[:, 0, :], SP[:, cs], False, (not has_r) and d < 0, [SPk, K("NUr")], [pkA])
                if has_r:
                    S.mm(psA[:, cs], NUr[:, 1, :], R[:, cs], False, d < 0, [Rk, K("NUr")], [pkA])
                if d >= 0:
                    S.mm(psA[:, cs], ident_b, NM[:, d, cs], False, True, [K("NM"), K("c128b")], [pkA])
                S.act(Wt[:, cs], psA[:, cs], AF.Exp, [pkA], [Wk_])
                if not m["last"]:
                    S.tt(R[:, cs], R[:, cs].bitcast(F32), SP[:, cs].bitcast(F32), ALU.add, [Rk, SPk], [Rk])

            def sb_s3(m):
                i = m["i"]
                pr, c2, h = m["pr"], m["c2"], m["h"]
                off = 128 * max(m["d"], 0)
                cs = slice(off, TW)
                Wt, Wk_ = Wb[i % 2], K("Wb", i % 2)
                psO, pkO = PS[4 + m["grp"] % 2], K("psO", m["grp"] % 2)
                S.mm(psO[:, cs], VB[:, m["b"], c2 * 128:(c2 + 1) * 128], Wt[:, cs], m["idx"] == 0, m["last"],
                     [K("VB", m["b"]), Wk_], [pkO])
                if m["last"]:
                    S.cp(YG[pr, c2, m["qsl"]], psO[pr, :], [pkO], [K("YG", c2, m["c"])])

            n_it = len(items)
            for r in range(-2, n_it):
                if 0 <= r + 2 < n_it:
                    sb_s1(items[r + 2])
                if 0 <= r + 1 < n_it:
                    sb_s2(items[r + 1])
                if 0 <= r < n_it:
                    sb_s3(items[r])
            dump(f"yb{l}", YG[:], [K("YG", c, t) for c in range(2) for t in range(NTC)], [128, 2, S_])
            st.close()
            S.barrier()
            nrot[0] = 8
            group_out(l, 1, YG, sty)
        S.barrier()
        if stop_after == f"B{l}":
            done = True
            break

        nrot[0] = 4
        with ExitStack() as sty, ExitStack() as st:
            YG = sb("YG", [128, 2, S_], stack=sty)
            XS = sb("XS", [128, 2, S_], stack=st)
            BTb = sb("BTb", [128, 2, S_], BF16, stack=st)
            CTb = sb("CTb", [128, 2, S_], BF16, stack=st)
            BTM = sb("BTM", [128, NT, 256], BF16, stack=st)
            XTM = sb("XTM", [128, NT, 256], BF16, stack=st)
            wdt = sb("wdt", [128, KC, 4], BF16, stack=st)
            stc = ExitStack()
            xw = [sb(f"xwc{j}", [128, 515], stack=stc) for j in range(2)]
            CV = [sb(f"CV{j}", [128, 512], stack=stc) for j in range(2)]
            BF = [sb(f"BF{j}", [128, 512], stack=stc) for j in range(2)]
            S.dma(wdt[:], w_in_d[l, :, 2304:2308].rearrange("(kc p) n -> p kc n", p=128), [], [K("wdt")], q="pool", arena=True)
            wv1, wk1, _ = load_w(w_in_d[l, :, 1280:1792], 8, 512)
            wv2, wk2, _ = load_w(w_in_d[l, :, 1792:2304], 8, 512)
            n_cv = 0
            for j in range(6):
                if j < 2:
                    wv, wk, c0 = wv1, wk1, 256 + j * 128
                elif j < 4:
                    wv, wk, c0 = wv2, wk2, (j - 2) * 128
                else:
                    wv, wk, c0 = wv2, wk2, 256 + (j - 4) * 128
                for tc in range(NTC):
                    sl = slice(tc * TW, (tc + 1) * TW)
                    ps, pk = ps_rot()
                    proj_fm(wv, wk, c0, tc, ps, pk)
                    cv, cvk = CV[n_cv % 2], K("CV", n_cv % 2)
                    conv_chunk(ps, pk, tc, xw, "xwc", lambda k: col(l, "ssm_cw", j * 4 + k), col(l, "ssm_cb", j),
                               cv[:], cvk)
                    if j < 2:
                        S.act(XS[:, j, sl], cv[:], AF.Silu, [cvk], [K("XS", j, tc)])
                        ps, pk = ps_rot()
                        for q in range(4):
                            S.tr(ps[:, q * 128:(q + 1) * 128], XS[:, j, tc * TW + q * 128: tc * TW + (q + 1) * 128],
                                 ident, [K("XS", j, tc), K("c128")], [pk])
                        S.cp(XTM[:, tc * 4:tc * 4 + 4, j * 128:(j + 1) * 128],
                             ps[:].rearrange("p (a b) -> p a b", a=4), [pk], [K("XTM", tc, j)])
                    elif j < 4:
                        g = j - 2
                        bf, bfk = BF[n_cv % 2], K("BF", n_cv % 2)
                        S.act(bf[:], cv[:], AF.Silu, [cvk], [bfk])
                        S.cp(BTb[:, g, sl], bf[:], [bfk], [K("BTb", g, tc)])
                        ps, pk = ps_rot()
                        for q in range(4):
                            S.tr(ps[:, q * 128:(q + 1) * 128], bf[:, q * 128:(q + 1) * 128], ident,
                                 [bfk, K("c128")], [pk])
                        S.cp(BTM[:, tc * 4:tc * 4 + 4, g * 128:(g + 1) * 128],
                             ps[:].rearrange("p (a b) -> p a b", a=4), [pk], [K("BTM", tc, g)])
                    else:
                        S.act(CTb[:, j - 4, sl], cv[:], AF.Silu, [cvk], [K("CTb", j - 4, tc)])
                    n_cv += 1
            stc.close()
            S.barrier()
            DT = sb("DT", [128, 64], stack=st)
            AN = sb("AN", [128, 64], stack=st)
            AC = sb("AC", [128, 64], stack=st)
            ACS = sb("ACS", [128, 64], stack=st)
            TOT = sb("TOT", [128, 64], stack=st)
            DEC = sb("DEC", [128, 64], stack=st)
            ETOT = sb("ETOT", [128, 64], stack=st)
            DTD = sb("DTD", [128, 64], stack=st)
            psd, pkd = ps_rot()
            for i in range(NT):
                for kc in range(KC):
                    S.mm(psd[:, i * 4:(i + 1) * 4], hT[:, kc, i * 128:(i + 1) * 128], wdt[:, kc, :], kc == 0,
                         kc == KC - 1, [K("wdt"), K("hT", kc, i // 4)], [pkd])
            S.tt(DT[:], psd[:, 0:64], row(l, "dt_bias"), ALU.add, [pkd, K("rowp")], [K("DT")])
            S.act(DT[:], DT[:], AF.Exp, [K("DT")], [K("DT")])
            S.act(DT[:], DT[:], AF.Ln, [K("DT"), K("cst")], [K("DT")], bias=cst[:, 1:2])
            S.act(AN[:], row(l, "a_log"), AF.Exp, [K("rowp")], [K("AN")])
            S.ts(AN[:], AN[:], -1.0, None, ALU.mult, None, [K("AN")], [K("AN")])
            S.tt(AC[:], DT[:], AN[:], ALU.mult, [K("DT"), K("AN")], [K("AC")])
            ps, pk = ps_rot()
            S.mm(ps[:, 0:64], tri_le, AC[:], True, True, [K("AC"), K("c128")], [pk])
            S.cp(ACS[:], ps[:, 0:64], [pk], [K("ACS")])
            ps, pk = ps_rot()
            S.mm(ps[:, 0:64], ones_f, AC[:], True, True, [K("AC"), K("c128")], [pk])
            S.cp(TOT[:], ps[:, 0:64], [pk], [K("TOT")])
            S.tt(DEC[:], TOT[:], ACS[:], ALU.subtract, [K("TOT"), K("ACS")], [K("DEC")])
            S.act(DEC[:], DEC[:], AF.Exp, [K("DEC")], [K("DEC")])
            S.act(ETOT[:], TOT[:], AF.Exp, [K("TOT")], [K("ETOT")])
            S.tt(DTD[:], DT[:], DEC[:], ALU.mult, [K("DT"), K("DEC")], [K("DTD")])
            ST = sb("ST", [128, 4, 64], stack=st)
            S.add("dve", lambda e: e.memset(ST[:], 0.0), [], [K("ST", h) for h in range(4)])
            Rm = [sb(f"Rm{j}", [128, 128], stack=st) for j in range(2)]
            ARG = [sb(f"ARG{j}", [128, 128], stack=st) for j in range(2)]
            E1 = [sb(f"E1{j}", [128, 128], stack=st) for j in range(2)]
            GL = [sb(f"GL{j}", [128, 128], BF16, stack=st) for j in range(2)]
            CTp = [sb(f"CTp{j}", [128, 128], BF16, stack=st) for j in range(2)]
            XCp = [[sb(f"XCp{a}{j}", [128, 128], BF16, stack=st) for j in range(2)] for a in range(2)]
            STp = sb("STp", [128, 4, 128], BF16, stack=st)
            for a in range(2):
                for j in range(2):
                    S.add("dve", (lambda e, t_=XCp[a][j]: e.memset(t_[:], 0.0)), [], [K("XCp", a, j)])
            S.add("dve", lambda e: e.memset(STp[:], 0.0), [], [K("STp", h) for h in range(4)])
            XDh = [sb(f"XDh{j}", [128, 64], BF16, stack=st) for j in range(2)]
            n = 0
            for ci in range(NT):
                csl = slice(ci * 128, (ci + 1) * 128)
                tcx = ci // 4
                for g in range(2):
                    psG, pkG = ps_rot()
                    S.mm(psG[:, 0:128], BTb[:, g, csl], CTb[:, g, csl], True, True,
                         [K("BTb", g, tcx), K("CTb", g, tcx)], [pkG])
                    psY, pkY = PS[4 + (ci * 2 + g) % 2], K("psO", (ci * 2 + g) % 2)
                    for hh in range(2):
                        h = 2 * g + hh
                        pr = slice(hh * 64, hh * 64 + 64)
                        cidx = ci * 4 + h
                        k2 = n % 2
                        S.ts(Rm[k2][:], tri_le, AC[:, cidx:cidx + 1], None, ALU.mult, None,
                             [K("AC"), K("c128")], [K("Rm", k2)])
                        ps1, pk1 = ps_rot()
                        S.mm(ps1[:, 0:128], ones_f, Rm[k2][:], True, True, [K("Rm", k2), K("c128")], [pk1])
                        S.stt(ARG[k2][:], ps1[:, 0:128], ACS[:, cidx:cidx + 1], ssdneg, ALU.subtract, ALU.add,
                              [pk1, K("ACS"), K("c128")], [K("ARG", k2)])
                        S.act(ARG[k2][:], ARG[k2][:], AF.Exp, [K("ARG", k2)], [K("ARG", k2)])
                        S.tt(GL[k2][:], ARG[k2][:], psG[:, 0:128], ALU.mult, [K("ARG", k2), pkG], [K("GL", k2)])
                        S.act(E1[k2][:], ps1[:, 0:128], AF.Exp, [pk1, K("ARG", k2)], [K("E1", k2)])
                        S.tt(CTp[k2][:], CTb[:, g, csl], E1[k2][:], ALU.mult, [K("CTb", g, tcx), K("E1", k2)],
                             [K("CTp", k2)])
                        rot = (ci * 2 + g) % 2
                        xcp = XCp[hh][rot]
                        S.ts(xcp[:, hh * 64:(hh + 1) * 64], XTM[:, ci, h * 64:(h + 1) * 64], DT[:, cidx:cidx + 1], None,
                             ALU.mult, None, [K("XTM", tcx, g), K("DT")], [K("XCp", hh, rot)])
                        S.mm(psY[:, 0:128], xcp[:], GL[k2][:], hh == 0, False, [K("XCp", hh, rot), K("GL", k2)], [pkY])
                        S.mm(psY[:, 0:128], STp[:, h, :], CTp[k2][:], False, hh == 1, [K("STp", h), K("CTp", k2)], [pkY])
                        S.ts(XDh[k2][:], XTM[:, ci, h * 64:(h + 1) * 64], DTD[:, cidx:cidx + 1], None, ALU.mult, None,
                             [K("XTM", tcx, g), K("DTD")], [K("XDh", k2)])
                        psS, pkS = ps_rot()
                        S.mm(psS[:, 0:64], BTM[:, ci, g * 128:(g + 1) * 128], XDh[k2][:], True, True,
                             [K("BTM", tcx, g), K("XDh", k2)], [pkS])
                        S.stt(ST[:, h, :], ST[:, h, :], ETOT[:, cidx:cidx + 1], psS[:, 0:64], ALU.mult, ALU.add,
                              [K("ST", h), K("ETOT"), pkS], [K("ST", h)])
                        S.cp(STp[:, h, hh * 64:(hh + 1) * 64], ST[:, h, :], [K("ST", h)], [K("STp", h)], eng="act")
                        n += 1
                    S.cp(YG[:, g, csl], psY[:, 0:128], [pkY], [K("YG", g, tcx)], eng="act")
            ARG = [sb(f"ZT{j}", [128, 512], stack=st) for j in range(1)]
            for j in range(2):
                YGk = [K("YG", j, t) for t in range(NTC)]
                S.stt(YG[:, j, :], XS[:, j, :], col(l, "ssm_d", j), YG[:, j, :], ALU.mult, ALU.add,
                      [K("XS", j, t) for t in range(NTC)] + YGk + [K("colp")], YGk)
                for tc in range(NTC):
                    sl = slice(tc * TW, (tc + 1) * TW)
                    ps, pk = ps_rot()
                    proj_fm(wv1, wk1, j * 128, tc, ps, pk)
                    zt, ztk = ARG[0], K("ARGz", 0)
                    S.act(zt[:], ps[:, :], AF.Silu, [pk], [ztk])
                    S.tt(YG[:, j, sl], YG[:, j, sl], zt[:], ALU.mult, [K("YG", j, tc), ztk], [K("YG", j, tc)])
            dump(f"yc{l}", YG[:], [K("YG", c, t) for c in range(2) for t in range(NTC)], [128, 2, S_])
            st.close()
            S.barrier()
            nrot[0] = 8
            group_out(l, 2, YG, sty)
        S.barrier()
        if stop_after == f"C{l}":
            done = True
            break

        nrot[0] = 4
        with ExitStack() as sty, ExitStack() as st:
            YG = sb("YG", [128, 2, S_], stack=sty)
            QA = sb("QA", [128, 4, S_], BF16, stack=st)
            KA = sb("KA", [128, 4, S_], BF16, stack=st)
            allqa = [K("QA", h_, t_) for h_ in range(4) for t_ in range(NTC)]
            allka = [K("KA", h_, t_) for h_ in range(4) for t_ in range(NTC)]
            S.add("dve", lambda e: e.memset(QA[:], 0.0), [], allqa, cost=4.5)
            S.add("dve", lambda e: e.memset(KA[:], 0.0), [], allka, cost=4.5)
            for h_ in range(4):
                o_ = 64 if h_ % 2 == 0 else 0
                S.dma(KA[o_:o_ + 8, h_, :], blkoh_d[:, :], allka, [K("KAoh", h_)], q="pool", arena=True)
            VB = sb("VB", [128, NT, 256], BF16, stack=st)
            CAUS = sb("CAUS", [128, 4, 512], BF16, stack=st)
            S.dma(CAUS[:], cmask_d[:, 4096:6144].rearrange("p (a b) -> p a b", a=4), [], [K("CAUS")], q="pool", arena=True)
            GC = sb("GC", [128, 3, 128], stack=st)
            S.dma(GC[:], gate_c_d[:, :].rearrange("p (a b) -> p a b", a=3), [], [K("GC")])
            KM = sb("KM", [128, 2, 8], stack=st)
            KMb = sb("KMb", [128, 2, 8], BF16, stack=st)
            wv, wk, _ = load_w(w_in_d[l, :, 2308:2820], 8, 512)
            wv2, wk2, _ = load_w(w_in_d[l, :, 2820:3076], 8, 256)
            str_ = ExitStack()
            ROPEb = [sb(f"ROPE{j}", [128, 2, TW], stack=str_) for j in range(1)]
            RAW = [sb(f"RAW{j}", [128, 512], stack=str_) for j in range(2)]
            TMP = [sb(f"TMPr{j}", [128, 512], stack=str_) for j in range(2)]
            KF = [sb(f"KF{j}", [128, 512], stack=str_) for j in range(2)]
            nr = 0
            for c2 in range(2):
                for tc in range(NTC):
                    sl = slice(tc * TW, (tc + 1) * TW)
                    rj = 0
                    ROPE = ROPEb[rj]
                    rsl = slice(0, TW)
                    S.dma(ROPE[:, 0, :], rope_d[:, tc * TW:(tc + 1) * TW], [], [K("ROPE", rj, 0)])
                    S.dma(ROPE[:, 1, :], rope_d[:, S_ + tc * TW:S_ + (tc + 1) * TW], [], [K("ROPE", rj, 1)])
                    for which in range(2):
                        ps, pk = ps_rot()
                        proj_fm(wv, wk, which * 256 + c2 * 128, tc, ps, pk)
                        raw, rawk = RAW[nr % 2], K("RAW", nr % 2)
                        tmp, tmpk = TMP[nr % 2], K("TMPr", nr % 2)
                        S.cp(raw[:], ps[:, :], [pk], [rawk], eng="act")
                        psw, pkw = ps_rot()
                        S.mm(psw[:, :], perm_f, raw[:], True, True, [rawk, K("c128")], [pkw])
                        S.tt(tmp[:], psw[:, :], ROPE[:, 1, rsl], ALU.mult, [pkw, K("ROPE", rj, 1)], [tmpk])
                        kf = KF[nr % 2]
                        dst, dk = kf[:], K("KF", nr % 2)
                        S.tt(dst, raw[:], ROPE[:, 0, rsl], ALU.mult, [rawk, K("ROPE", rj, 0)], [dk])
                        S.tt(dst, dst, tmp[:], ALU.add, [dk, tmpk], [dk])
                        if which == 0:
                            S.act(QA[0:64, 2 * c2, sl], kf[0:64, :], AF.Copy, [dk], [K("QA", 2 * c2, tc)], scale=0.125)
                            S.act(QA[64:128, 2 * c2 + 1, sl], kf[64:128, :], AF.Copy, [dk], [K("QA", 2 * c2 + 1, tc)],
                                  scale=0.125)
                        else:
                            S.cp(KA[0:64, 2 * c2, sl], kf[0:64, :], [dk], [K("KA", 2 * c2, tc)], eng="act")
                            S.cp(KA[64:128, 2 * c2 + 1, sl], kf[64:128, :], [dk], [K("KA", 2 * c2 + 1, tc)], eng="act")
                            for hb in range(2):
                                nblk = tc * 2 + hb
                                S.add("dve", (lambda e, o=KM[:, c2, nblk:nblk + 1], i_=kf[:, hb * 256:(hb + 1) * 256]:
                                              e.reduce_sum(out=o, in_=i_, axis=AX.X)), [dk], [K("KM", c2, nblk)])
                        nr += 1
            for i in range(NT):
                ps, pk = ps_rot()
                for kc in range(KC):
                    S.mm(ps[:, 0:256], hT[:, kc, i * 128:(i + 1) * 128], wv2[:, kc, :], kc == 0, kc == KC - 1,
                         wk2 + [K("hT", kc, i // 4)], [pk])
                S.cp(VB[:, i, :], ps[:, 0:256], [pk], [K("VB", i)], eng=("dve" if i % 2 else "act"))
            S.cp(KMb[:], KM[:], [K("KM", c2_, nb_) for c2_ in range(2) for nb_ in range(8)], [K("KMb")])
            str_.close()
            S.barrier()
            PTb = [sb(f"PTb{j}", [128, 512], BF16, stack=st) for j in range(3)]
            RD = [sb(f"RD{j}", [128, 512], stack=st) for j in range(2)]
            GT = sb("GT", [128, 128], stack=st)
            BT_ = sb("BIASTM", [128, 128], stack=st)
            MX = [sb(f"MX{j}", [128, 8], stack=st) for j in range(2)]
            for h in range(4):
                hh, c2 = h % 2, h // 2
                pr = slice(hh * 64, hh * 64 + 64)
                o_ = 64 if hh == 0 else 0
                psg, pkg = ps_rot()
                for i in range(NT):
                    S.mm(psg[:, i * 8:(i + 1) * 8], QA[pr, h, i * 128:(i + 1) * 128], KMb[pr, c2, :], True, True,
                         [K("QA", h, i // 4), K("KMb")], [pkg])
                S.stt(GT[:], psg[:, 0:128], 8.0 / 256.0, GC[:, 0, :], ALU.mult, ALU.add, [pkg, K("GC")], [K("GT")])
                for i in range(NT):
                    mx, mxk = MX[i % 2], K("MX", i % 2)
                    S.add("dve", (lambda e, o=mx[:], i_=GT[:, i * 8:(i + 1) * 8]: e.max(out=o, in_=i_)),
                          [K("GT")], [mxk])
                    bsl = BT_[:, i * 8:(i + 1) * 8]
                    S.ts(bsl, GT[:, i * 8:(i + 1) * 8], mx[:, 2:3], None, ALU.is_ge, None, [K("GT"), mxk],
                         [K("BIASTM", i)])
                    S.tt(bsl, bsl, GC[:, 1, i * 8:(i + 1) * 8], ALU.mult, [K("BIASTM", i), K("GC")], [K("BIASTM", i)])
                    S.tt(bsl, bsl, GC[:, 2, i * 8:(i + 1) * 8], ALU.add, [K("BIASTM", i), K("GC")], [K("BIASTM", i)])
                    S.ts(bsl, bsl, -1.0, -NEG, ALU.add, ALU.mult, [K("BIASTM", i)], [K("BIASTM", i)])
                for q4 in range(4):
                    pst, pkt = ps_rot()
                    for q in range(4):
                        i = q4 * 4 + q
                        S.tr(pst[0:8, q * 128:(q + 1) * 128], BT_[:, i * 8:(i + 1) * 8], ident,
                             [K("BIASTM", i), K("c128")], [pkt])
                    S.cp(QA[o_:o_ + 8, h, q4 * 512:(q4 + 1) * 512], pst[0:8, :], [pkt], [K("QAsel", h, q4)])
            its = []
            grp = 0
            for h in range(4):
                for c in range(NTC):
                    nkt = 4 * c + 4
                    for kt in range(nkt):
                        its.append(dict(h=h, c=c, kt=kt, nkt=nkt, grp=grp, i=len(its)))
                    grp += 1

            def m_s1(m):
                h, c, kt = m["h"], m["c"], m["kt"]
                d = kt - 4 * c
                qsl = slice(c * TW, (c + 1) * TW)
                ksl = slice(kt * 128, (kt + 1) * 128)
                off = 128 * max(d, 0)
                cs = slice(off, TW)
                qs = slice(c * TW + off, (c + 1) * TW)
                psS, pkS = ps_rot()
                S.mm(psS[:, cs], KA[:, h, ksl], QA[:, h, qs], True, d < 0,
                     [K("KA", h, kt // 4), K("KAoh", h), K("QA", h, c), K("QAsel", h, c)], [pkS])
                if d >= 0:
                    S.mm(psS[:, cs], ident_b, CAUS[:, d, cs], False, True, [K("CAUS"), K("c128b")], [pkS])
                pt, ptk = PTb[m["i"] % 3], K("PTb", m["i"] % 3)
                S.act(pt[:, cs], psS[:, cs], AF.Exp, [pkS], [ptk])

            def m_s2(m):
                h, c, kt, nkt = m["h"], m["c"], m["kt"], m["nkt"]
                hh, c2 = h % 2, h // 2
                pr = slice(hh * 64, hh * 64 + 64)
                qsl = slice(c * TW, (c + 1) * TW)
                g2 = m["grp"] % 2
                psO, pkO = PS[4 + g2], K("psO", g2)
                psD, pkD = PS[6 + g2], K("psD", g2)
                pt, ptk = PTb[m["i"] % 3], K("PTb", m["i"] % 3)
                off = 128 * max(kt - 4 * c, 0)
                cs = slice(off, TW)
                S.mm(psO[:, cs], VB[:, kt, c2 * 128:(c2 + 1) * 128], pt[:, cs], kt == 0, kt == nkt - 1,
                     [K("VB", kt), ptk], [pkO])
                S.mm(psD[:, cs], ones_b, pt[:, cs], kt == 0, kt == nkt - 1, [ptk, K("c128b")], [pkD])
                if kt == nkt - 1:
                    rd, rdk = RD[g2], K("RD", g2)
                    S.add("dve", (lambda e, o=rd[pr, :], i_=psD[pr, :]: e.reciprocal(out=o, in_=i_)), [pkD], [rdk], cost=2.2)
                    S.tt(YG[pr, c2, qsl], psO[pr, :], rd[pr, :], ALU.mult, [pkO, rdk], [K("YG", c2, c)])

            n_ = len(its)
            for r in range(-1, n_):
                if r + 1 < n_:
                    m_s1(its[r + 1])
                if r >= 0:
                    m_s2(its[r])
            dump(f"yd{l}", YG[:], [K("YG", c, t) for c in range(2) for t in range(NTC)], [128, 2, S_])
            st.close()
            S.barrier()
            nrot[0] = 8
            group_out(l, 3, YG, sty)
        S.barrier()
        dump(f"x1_{l}", xT[:], ALLX, [128, KC, S_])
        if stop_after == f"D{l}":
            done = True
            break

        with ExitStack() as st:
            memT = sb("memT", [128, KC, 256], stack=st)
            MTb = sb("MTb", [128, KC, 256], BF16, stack=st)
            with ExitStack() as st2:
                rmsnorm_fm(xT, "xT", lambda kc: col(l, "xat_g", kc), hT, "hT", KC, S_, st2, tag="x")
                load_fm(memT, mem_d, 2, "memT", st2)
                rmsnorm_fm(memT, "memT", lambda kc: col(l, "mem_g", kc), MTb, "MTb", KC, 256, st2, tag="m")
            S.barrier()
            KTb = sb("KTb", [128, KC, 256], BF16, stack=st)
            VX = sb("VX", [128, 2, D_], BF16, stack=st)
            QX = sb("QX", [128, KC, S_], BF16, stack=st)
            MTk = [K("MTb", kc, 0) for kc in range(KC)]
            for half in range(2):
                wv, wk, _ = load_w(wkv_d[l, :, half * 512:(half + 1) * 512], 8, 512)
                for o4 in range(4):
                    oc = half * 4 + o4
                    ps, pk = ps_rot()
                    for kc in range(KC):
                        S.mm(ps[:, 0:256], wv[:, kc, o4 * 128:(o4 + 1) * 128], MTb[:, kc, :], kc == 0, kc == KC - 1,
                             wk + [K("MTb", kc, 0)], [pk])
                    S.cp(KTb[:, oc, :], ps[:, 0:256], [pk], [K("KTb", oc)])
            for half in range(2):
                wv, wk, _ = load_w(wkv_d[l, :, 1024 + half * 512:1024 + (half + 1) * 512], 8, 512)
                for mt in range(2):
                    ps, pk = ps_rot()
                    for kc in range(KC):
                        S.mm(ps[:, :], MTb[:, kc, mt * 128:(mt + 1) * 128], wv[:, kc, :], kc == 0, kc == KC - 1,
                             wk + [K("MTb", kc, 0)], [pk])
                    S.cp(VX[:, mt, half * 512:(half + 1) * 512], ps[:, :], [pk], [K("VX", mt, half)], eng="act")
            for half in range(2):
                wv, wk, _ = load_w(wq_d[l, :, half * 512:(half + 1) * 512], 8, 512)
                for o4 in range(4):
                    oc = half * 4 + o4
                    for tc in range(NTC):
                        ps, pk = ps_rot()
                        proj_fm(wv, wk, o4 * 128, tc, ps, pk)
                        S.act(QX[:, oc, tc * TW:(tc + 1) * TW], ps[:, :], AF.Copy, [pk], [K("QX", oc, tc)],
                              scale=1.0 / 16.0)
            dump(f"X_MTb{l}", MTb[:], MTk, [128, KC, 256])
            dump(f"X_KTb{l}", KTb[:], [K("KTb", oc) for oc in range(KC)], [128, KC, 256])
            dump(f"X_QX{l}", QX[:], [K("QX", oc, tc) for oc in range(KC) for tc in range(NTC)], [128, KC, S_])
            dump(f"X_VX{l}", VX[:], [K("VX", mt, hf) for mt in range(2) for hf in range(2)], [128, 2, D_])
            PT = [sb(f"PTx{j}", [128, 512], BF16, stack=st) for j in range(4)]
            RDx = [sb(f"RDx{j}", [128, 512], stack=st) for j in range(2)]
            it = 0
            for hd in range(4):
                for tc in range(NTC):
                    sl = slice(tc * TW, (tc + 1) * TW)
                    pts = []
                    for mt in range(2):
                        ps, pk = ps_rot()
                        for j in range(2):
                            S.mm(ps[:, :], KTb[:, 2 * hd + j, mt * 128:(mt + 1) * 128], QX[:, 2 * hd + j, sl],
                                 j == 0, j == 1, [K("KTb", 2 * hd + j), K("QX", 2 * hd + j, tc)], [pk])
                        pt, ptk = PT[it % 4], K("PTx", it % 4)
                        S.act(pt[:], ps[:, :], AF.Exp, [pk], [ptk])
                        pts.append((pt, ptk))
                        it += 1
                    psD, pkD = ps_rot()
                    for mt in range(2):
                        S.mm(psD[:, :], ones_b, pts[mt][0][:], mt == 0, mt == 1, [pts[mt][1], K("c128b")], [pkD])
                    rd, rdk = RDx[(hd * NTC + tc) % 2], K("RDx", (hd * NTC + tc) % 2)
                    S.add("dve", (lambda e, o=rd[:], i_=psD[:, :]: e.reciprocal(out=o, in_=i_)), [pkD], [rdk], cost=2.2)
                    for j in range(2):
                        oc = 2 * hd + j
                        ps, pk = ps_rot()
                        for mt in range(2):
                            S.mm(ps[:, :], VX[:, mt, oc * 128:(oc + 1) * 128], pts[mt][0][:], mt == 0, mt == 1,
                                 [K("VX", mt, oc // 4), pts[mt][1]], [pk])
                        S.tt(hT[:, oc, sl], ps[:, :], rd[:], ALU.mult, [pk, rdk], [K("hT", oc, tc)])
            dump(f"X_OX{l}", hT[:], ALLH, [128, KC, S_])
            for half in range(2):
                wv, wk, _ = load_w(wo_d[l, :, half * 512:(half + 1) * 512], 8, 512)
                for o4 in range(4):
                    oc = half * 4 + o4
                    for tc in range(NTC):
                        ps, pk = ps_rot()
                        proj_fm(wv, wk, o4 * 128, tc, ps, pk)
                        S.tt(xT[:, oc, tc * TW:(tc + 1) * TW], xT[:, oc, tc * TW:(tc + 1) * TW], ps[:, :], ALU.add,
                             [K("xT", oc, tc), pk], [K("xT", oc, tc)])
        S.barrier()
        dump(f"x2_{l}", xT[:], ALLX, [128, KC, S_])
        if stop_after == f"X{l}":
            done = True
            break

        with ExitStack() as st:
            ENp = sb("ENp", [128, 16, 128], BF16, stack=st)
            S.add("dve", lambda e: e.memset(ENp[:], 0.0), [], [K("ENpz")])
            S.dma(ENp[0:16, :, :], en_d[:, :].rearrange("p (a b) -> p a b", a=16), [K("ENpz")], [K("ENf")], q="pool",
                  arena=True)
            CTh = sb("CTh", [128, S_], BF16, stack=st)
            CTl = sb("CTl", [128, S_], BF16, stack=st)
            S.add("dve", lambda e: e.memset(CTh[:], 0.0), [], [K("CThz")])
            S.add("dve", lambda e: e.memset(CTl[:], 0.0), [], [K("CTlz")])
            st_rt = ExitStack()
            WR = sb("WR", [128, KC, 20], stack=st_rt)
            S.dma(WR[:], wrt_d[l].rearrange("(kc p) n -> p kc n", p=128), [], [K("WR")])
            LG = sb("LG", [128, NT, 20], stack=st_rt)
            st_hf = ExitStack()
            HF = sb("HF", [128, KC, TW], stack=st_hf)

            def router_hook(tc, r):
                sl = slice(tc * TW, (tc + 1) * TW)
                for kc in range(KC):
                    S.stt(HF[:, kc, :], xT[:, kc, sl], col(l, "ffn_g", kc), r[:], ALU.mult, ALU.mult,
                          [K("xT", kc, tc), K("rsf", tc % 2), K("colp")], [K("HF", kc)])
                ps, pk = ps_rot()
                for q in range(4):
                    for kc in range(KC):
                        S.mm(ps[:, q * 20:(q + 1) * 20], HF[:, kc, q * 128:(q + 1) * 128], WR[:, kc, :], kc == 0,
                             kc == KC - 1, [K("HF", kc), K("WR")], [pk])
                S.cp(LG[:, tc * 4:tc * 4 + 4, :], ps[:, 0:80].rearrange("p (a b) -> p a b", a=4), [pk],
                     [K("LG", tc)])

            with ExitStack() as st2:
                rmsnorm_fm(xT, "xT", lambda kc: col(l, "ffn_g", kc), hT, "hT", KC, S_, st2, hook=router_hook, tag="f")
            st_hf.close()
            S.barrier()
            COMB = sb("COMB", [128, NT, 16], stack=st_rt)
            CT = sb("CT", [16, S_], stack=st_rt)
            sm = {nm: sb("rt_" + nm, [128, NT, w_] if w_ > 1 else [128, NT], stack=st_rt) for nm, w_ in
                  [("lg", 4), ("m", 1), ("eg", 4), ("sg", 1), ("oh", 4), ("le", 16), ("sel", 4), ("tmp", 4),
                   ("a", 1), ("eq", 4), ("sel2", 4), ("b", 1), ("t2", 4), ("ex", 4), ("dd", 1), ("wg", 4)]}

            def v(nm):
                return sm[nm][:]

            def kk(nm):
                return [K("rt", nm)]

            def B3(ap2, w_=4):
                return ap2.unsqueeze(2).to_broadcast([128, NT, w_])

            LGk = [K("LG", t) for t in range(NTC)]
            bgB = row(l, "bg").unsqueeze(1).to_broadcast([128, NT, 4])
            beB = row(l, "be").unsqueeze(1).to_broadcast([128, NT, 16])
            S.tt(v("lg"), LG[:, :, 0:4], bgB, ALU.add, LGk + [K("rowp")], kk("lg"))
            S.add("dve", lambda e: e.reduce_max(out=v("m"), in_=v("lg"), axis=AX.X), kk("lg"), kk("m"))
            S.tt(v("lg"), v("lg"), B3(v("m")), ALU.subtract, kk("lg") + kk("m"), kk("lg"))
            S.act(v("eg"), v("lg"), AF.Exp, kk("lg"), kk("eg"))
            S.add("dve", lambda e: e.reduce_sum(out=v("sg"), in_=v("eg"), axis=AX.X), kk("eg"), kk("sg"))
            S.add("dve", lambda e: e.reciprocal(out=v("sg"), in_=v("sg")), kk("sg"), kk("sg"))
            S.ts(v("oh"), v("lg"), 0.0, None, ALU.is_ge, None, kk("lg"), kk("oh"))
            S.tt(v("le"), LG[:, :, 4:20], beB, ALU.add, LGk + [K("rowp")], kk("le"))
            for g in range(4):
                ohg = sm["oh"][:, :, g:g + 1].to_broadcast([128, NT, 4])
                if g == 0:
                    S.tt(v("sel"), sm["le"][:, :, 0:4], ohg, ALU.mult, kk("le") + kk("oh"), kk("sel"))
                else:
                    S.tt(v("tmp"), sm["le"][:, :, 4 * g:4 * g + 4], ohg, ALU.mult, kk("le") + kk("oh"), kk("tmp"))
                    S.tt(v("sel"), v("sel"), v("tmp"), ALU.add, kk("sel") + kk("tmp"), kk("sel"))
            S.add("dve", lambda e: e.reduce_max(out=v("a"), in_=v("sel"), axis=AX.X), kk("sel"), kk("a"))
            S.tt(v("sel"), v("sel"), B3(v("a")), ALU.subtract, kk("sel") + kk("a"), kk("sel"))
            S.ts(v("eq"), v("sel"), 0.0, None, ALU.is_ge, None, kk("sel"), kk("eq"))
            S.stt(v("sel2"), v("eq"), -1e30, v("sel"), ALU.mult, ALU.add, kk("eq") + kk("sel"), kk("sel2"))
            S.add("dve", lambda e: e.reduce_max(out=v("b"), in_=v("sel2"), axis=AX.X), kk("sel2"), kk("b"))
            S.tt(v("t2"), v("sel"), B3(v("b")), ALU.is_ge, kk("sel") + kk("b"), kk("t2"))
            S.act(v("ex"), v("sel"), AF.Exp, kk("sel"), kk("ex"))
            S.act(v("dd"), v("b"), AF.Exp, kk("b"), kk("dd"))
            S.ts(v("dd"), v("dd"), 1.0, None, ALU.add, None, kk("dd"), kk("dd"))
            S.add("dve", lambda e: e.reciprocal(out=v("dd"), in_=v("dd")), kk("dd"), kk("dd"))
            S.tt(v("wg"), v("ex"), v("t2"), ALU.mult, kk("ex") + kk("t2"), kk("wg"))
            S.tt(v("wg"), v("wg"), B3(v("dd")), ALU.mult, kk("wg") + kk("dd"), kk("wg"))
            S.tt(v("wg"), v("wg"), B3(v("sg")), ALU.mult, kk("wg") + kk("sg"), kk("wg"))
            for g in range(4):
                ohg = sm["oh"][:, :, g:g + 1].to_broadcast([128, NT, 4])
                S.tt(COMB[:, :, 4 * g:4 * g + 4], v("wg"), ohg, ALU.mult, kk("wg") + kk("oh"),
                     [K("COMB", i, g) for i in range(NT)])
            for q4 in range(4):
                pst, pkt = ps_rot()
                for q in range(4):
                    i = q4 * 4 + q
                    S.tr(pst[0:16, q * 128:(q + 1) * 128], COMB[:, i, :], ident,
                         [K("COMB", i, g) for g in range(4)] + [K("c128")], [pkt])
                S.cp(CT[0:16, q4 * 512:(q4 + 1) * 512], pst[0:16, :], [pkt], [K("CT", q4)])
                S.cp(CTh[0:16, q4 * 512:(q4 + 1) * 512], CT[0:16, q4 * 512:(q4 + 1) * 512], [K("CT", q4), K("CThz")],
                     [K("CTh", q4)])
                S.tt(CT[0:16, q4 * 512:(q4 + 1) * 512], CT[0:16, q4 * 512:(q4 + 1) * 512],
                     CTh[0:16, q4 * 512:(q4 + 1) * 512], ALU.subtract, [K("CT", q4), K("CTh", q4)], [K("CT", q4)])
                S.cp(CTl[0:16, q4 * 512:(q4 + 1) * 512], CT[0:16, q4 * 512:(q4 + 1) * 512], [K("CT", q4), K("CTlz")],
                     [K("CTl", q4)])
            st_rt.close()
            S.barrier()
            HID = sb("HID", [128, 8, S_], BF16, stack=st)
            W2S = sb("W2S", [128, 8, D_], BF16, stack=st)
            CB = [sb(f"CB{j}", [128, 512], stack=st) for j in range(2)]
            SL = [sb(f"SL{j}", [128, 512], stack=st) for j in range(2)]
            n = 0
            for g in range(4):
                for el in range(4):
                    ex = 4 * g + el
                    wv, wk1_, slot = load_w(w1_d[l, ex], 8, 256, c0=0, tot=512)
                    _, wk3_, _ = load_w(w3_d[l, ex], 8, 256, slot=slot, c0=256, tot=512)
                    wk = wk1_ + wk3_
                    for tc in range(NTC):
                        sl = slice(tc * TW, (tc + 1) * TW)
                        psC, pkC = ps_rot()
                        S.mm(psC[:, :], ENp[:, ex, :], CTh[:, sl], True, False, [K("ENf"), K("CTh", tc), K("CThz")], [pkC])
                        S.mm(psC[:, :], ENp[:, ex, :], CTl[:, sl], False, True, [K("ENf"), K("CTl", tc), K("CTlz")], [pkC])
                        cb, cbk = CB[n % 2], K("CB", n % 2)
                        S.cp(cb[:], psC[:, :], [pkC], [cbk], eng="act")
                        for fc in range(2):
                            ps1, pk1 = ps_rot()
                            proj_fm(wv, wk, fc * 128, tc, ps1, pk1)
                            ps3, pk3 = ps_rot()
                            proj_fm(wv, wk, 256 + fc * 128, tc, ps3, pk3)
                            s_, sk = SL[(2 * n + fc) % 2], K("SL", (2 * n + fc) % 2)
                            S.act(s_[:], ps1[:, :], AF.Silu, [pk1], [sk])
                            S.tt(s_[:], s_[:], ps3[:, :], ALU.mult, [sk, pk3], [sk])
                            S.tt(HID[:, el * 2 + fc, sl], s_[:], cb[:], ALU.mult, [sk, cbk], [K("HID", el * 2 + fc, tc)])
                        n += 1
                    S.dma(W2S[:, 2 * el:2 * el + 2, :], w2_d[l, ex].rearrange("(fc p) n -> p fc n", p=128), [],
                          [K("W2S", el)], q="pool", arena=True)
                for oc in range(KC):
                    for tc in range(NTC):
                        ps, pk = ps_rot()
                        for j in range(8):
                            S.mm(ps[:, :], W2S[:, j, oc * 128:(oc + 1) * 128], HID[:, j, tc * TW:(tc + 1) * TW],
                                 j == 0, j == 7, [K("W2S", j // 2), K("HID", j, tc)], [pk])
                        S.tt(xT[:, oc, tc * TW:(tc + 1) * TW], xT[:, oc, tc * TW:(tc + 1) * TW], ps[:, :], ALU.add,
                             [K("xT", oc, tc), pk], [K("xT", oc, tc)])
        S.barrier()
        dump(f"x3_{l}", xT[:], ALLX, [128, KC, S_])
        if stop_after == f"M{l}":
            done = True
            break

    if stop_after is None:
        with ExitStack() as st:
            HF = sb("HFo", [128, KC, TW], stack=st)
            OB = [sb(f"OB{j}", [128, D_], stack=st) for j in range(2)]
            nob = [0]

            def final_hook(tc, r):
                sl = slice(tc * TW, (tc + 1) * TW)
                for kc in range(KC):
                    S.stt(HF[:, kc, :], xT[:, kc, sl], col(0, "fin_g", kc), r[:], ALU.mult, ALU.mult,
                          [K("xT", kc, tc), K("rsz", tc % 2), K("colp")], [K("HFo", kc)])
                for q in range(4):
                    ob, obk = OB[nob[0] % 2], K("OB", nob[0] % 2)
                    nob[0] += 1
                    for half in range(2):
                        ps, pk = ps_rot()
                        for j in range(4):
                            S.tr(ps[:, j * 128:(j + 1) * 128], HF[:, half * 4 + j, q * 128:(q + 1) * 128], ident,
                                 [K("HFo", half * 4 + j), K("c128")], [pk])
                        S.cp(ob[:, half * 512:(half + 1) * 512], ps[:, :], [pk], [obk],
                             eng=("act" if half else "dve"))
                    r0 = tc * TW + q * 128
                    i = S.dma(out_d[r0:r0 + 128, :], ob[:], [obk], [K("out", r0)])
                    S.final.append(i)

            rmsnorm_fm(xT, "xT", lambda kc: col(0, "fin_g", kc), None, None, KC, S_, st, hook=final_hook, tag="z")

    S.finalize(es)
    es.close()
    return nc, dump_d


def make_consts():
    c128 = np.zeros((128, 8, 128), np.float32)
    j = np.arange(128)[:, None]
    s = np.arange(128)[None, :]
    c128[:, 0] = np.eye(128)
    c128[:, 1] = 1.0
    c128[:, 2] = (j <= s)
    c128[:, 3] = -1.0 * (j >= s)
    c128[:, 4] = -1.0
    partner = np.where((np.arange(128) % 64) < 32, np.arange(128) + 32, np.arange(128) - 32)
    perm = np.zeros((128, 128), np.float32)
    perm[partner, np.arange(128)] = 1.0
    c128[:, 5] = perm
    c128[:, 6] = NEG * (s < j)
    cm = np.zeros((128, 12, 512), np.float32)
    p = np.arange(128)[:, None]
    f = np.arange(512)[None, :]
    for d in range(4):
        valid = (f - p) > d * 128
        cm[:, d] = valid
        cm[:, 4 + d] = NEG * (~valid)
        cm[:, 8 + d] = NEG * ((d * 128 + p) > f)
    half = 32
    inv = (np.float32(10000.0) ** (-(np.arange(half, dtype=np.float32)) / np.float32(half))).astype(np.float32)
    ang = np.arange(S_, dtype=np.float32)[None, :] * inv[:, None]
    cos = np.cos(ang).astype(np.float32)
    sin = np.sin(ang).astype(np.float32)
    rope = np.zeros((128, 2, S_), np.float32)
    for pp in range(128):
        d = pp % 64
        rope[pp, 0] = cos[d % 32]
        rope[pp, 1] = -sin[d % 32] if d < 32 else sin[d % 32]
    en = np.zeros((16, 16, 128), np.float32)
    for n in range(16):
        en[n, n, :] = 1.0
    gc = np.zeros((128, 3, 16, 8), np.float32)
    for i in range(16):
        own = i // 2
        for n in range(8):
            gc[:, 0, i, n] = 0.0 if n < own else -1e30
            gc[:, 1, i, n] = 1.0 if n < own else 0.0
            gc[:, 2, i, n] = 1.0 if n == own else 0.0
    blkoh = np.zeros((8, S_), np.float32)
    for n in range(8):
        blkoh[n, n * 256:(n + 1) * 256] = 1.0
    return dict(c128=c128.reshape(128, -1), cmask=cm.reshape(128, -1), rope=rope.reshape(128, -1),
                en=en.reshape(16, -1), gatec=gc.reshape(128, -1), blkoh=blkoh)


def colvec(v):
    v = np.asarray(v, np.float32)
    return np.ascontiguousarray(v.reshape(-1, 128).T)


def pack_params(inp):
    colp = np.zeros((128, L_, NCOL), np.float32)
    rowp = np.zeros((128, L_, NROW), np.float32)

    def put(l, name, arr):
        c0, w = COLS[name]
        assert arr.shape == (128, w), (name, arr.shape)
        colp[:, l, c0:c0 + w] = arr

    for l in range(L_):
        put(l, "mix_g", colvec(inp["mix_norm_g"][l]))
        put(l, "xat_g", colvec(inp["xattn_norm_g"][l]))
        put(l, "mem_g", colvec(inp["mem_norm_g"][l]))
        put(l, "ffn_g", colvec(inp["ffn_norm_g"][l]))
        put(l, "grp_g", colvec(inp["group_norm_g"][l]))
        cw = inp["lru_conv_w"][l]
        put(l, "lru_cw", np.concatenate([colvec(cw[k]) for k in range(4)], axis=1).reshape(128, 4, 2)
            .transpose(0, 2, 1).reshape(128, 8))
        put(l, "lru_cb", colvec(inp["lru_conv_b"][l]))
        put(l, "lru_br", colvec(inp["lru_br"][l]))
        put(l, "lru_bi", colvec(inp["lru_bi"][l]))
        put(l, "lru_lam", colvec(inp["lru_lambda"][l]))
        sw = inp["ssm_conv_w"][l]
        put(l, "ssm_cw", np.concatenate([colvec(sw[k]) for k in range(4)], axis=1).reshape(128, 4, 6)
            .transpose(0, 2, 1).reshape(128, 24))
        put(l, "ssm_cb", colvec(inp["ssm_conv_b"][l]))
        put(l, "ssm_d", colvec(np.repeat(inp["ssm_d"][l], 64)))
        put(l, "fin_g", colvec(inp["final_norm_g"]))
        r0, w = ROWS["dt_bias"]
        rowp[:, l, r0:r0 + w] = np.tile(inp["ssm_dt_bias"][l], 16)[None, :]
        r0, w = ROWS["a_log"]
        rowp[:, l, r0:r0 + w] = np.tile(inp["ssm_a_log"][l], 16)[None, :]
        r0, w = ROWS["bg"]
        rowp[:, l, r0:r0 + w] = inp["router_group_b"][l][None, :]
        r0, w = ROWS["be"]
        rowp[:, l, r0:r0 + w] = inp["router_expert_b"][l][None, :]
    wbd = np.zeros((L_, 2, 2, 128, 128), np.float32)
    for l in range(L_):
        for gi, nm in enumerate(("lru_wr", "lru_wi")):
            for c in range(2):
                for hh in range(2):
                    wbd[l, gi, c, hh * 64:(hh + 1) * 64, hh * 64:(hh + 1) * 64] = inp[nm][l, 2 * c + hh]
    router_w = np.concatenate([inp["router_group_w"], inp["router_expert_w"]], axis=2)
    return dict(colp=colp.reshape(128, -1), rowp=rowp.reshape(128, -1), lru_wbd=wbd,
                router_w=np.ascontiguousarray(router_w))


SHARED = ["w_in", "w_out", "xattn_wq", "xattn_wkv", "xattn_wo", "expert_w1", "expert_w3", "expert_w2"]


def make_in_maps(inputs, cores):
    inp = {k: np.asarray(v) for k, v in inputs.items()}
    consts = make_consts()
    packed = pack_params(inp)
    shared = {k: np.ascontiguousarray(inp[k], dtype=np.float32) for k in SHARED}
    maps = []
    for b in cores:
        m = dict(shared)
        m.update(consts)
        m.update(packed)
        m["x"] = np.ascontiguousarray(inp["x"][b])
        m["mem"] = np.ascontiguousarray(inp["mem"][b])
        maps.append(m)
    return maps


_CACHE = {}


def kernel(**inputs):
    if "nc" not in _CACHE:
        _CACHE["nc"] = build_program()[0]
    nc = _CACHE["nc"]
    maps = make_in_maps(inputs, list(range(8)))
    res = run_bass_kernel_spmd(nc, maps, core_ids=list(range(8)))
    return np.stack([r["out"] for r in res.results], axis=0).astype(np.float32)
```

```python
import numpy as np
from contextlib import ExitStack
import concourse.bass as bass
import concourse.mybir as mybir
from concourse.bass_utils import run_bass_kernel_spmd

F32 = mybir.dt.float32
BF16 = mybir.dt.bfloat16
F32R = mybir.dt.float32r
AF = mybir.ActivationFunctionType
ALU = mybir.AluOpType
AX = mybir.AxisListType

S_ = 2048
D_ = 1024
L_ = 2
KC = 8
NTC = 4
TW = 512
NT = 16
NEG = -30000.0
EPS = 1e-6

COLS = {}
_c = 0
for _n, _w in [("mix_g", 8), ("xat_g", 8), ("mem_g", 8), ("ffn_g", 8), ("grp_g", 8),
               ("lru_cw", 8), ("lru_cb", 2), ("lru_br", 2), ("lru_bi", 2), ("lru_lam", 2),
               ("ssm_cw", 24), ("ssm_cb", 6), ("ssm_d", 2), ("fin_g", 8)]:
    COLS[_n] = (_c, _w)
    _c += _w
NCOL = _c
ROWS = {}
_c = 0
for _n, _w in [("dt_bias", 64), ("a_log", 64), ("bg", 4), ("be", 16)]:
    ROWS[_n] = (_c, _w)
    _c += _w
NROW = _c


def _free(ap):
    n = 1
    for d in ap.shape[1:]:
        n *= int(d)
    return n


DEBUG_LINES = False


class Sched:
    ENG = ("pe", "act", "dve", "pool", "sp")
    GATED = ("pe", "act", "dve", "sp")

    def __init__(self, nc):
        self.nc = nc
        self.ops = []
        self.lastw = {}
        self.rd = {}
        self.by_eng = {e: [] for e in self.ENG}
        self.final = []
        self.psn = 0
        self.seg = 0
        self.act_inorder = set()

    def add(self, eng, fn, reads=(), writes=(), dma=False, cost=0.3, arena=False):
        i = len(self.ops)
        reads = list(reads)
        writes = list(writes)
        deps = {}
        for r in reads:
            w = self.lastw.get(r)
            if w is not None:
                deps.setdefault(w, set()).add("raw")
        for w_ in writes:
            w = self.lastw.get(w_)
            if w is not None:
                deps.setdefault(w, set()).add("waw")
            for r in self.rd.get(w_, ()):
                deps.setdefault(r, set()).add("war")
        ws = set(writes)
        for r in reads:
            if r not in ws:
                self.rd.setdefault(r, []).append(i)
        for w_ in writes:
            self.lastw[w_] = i
            self.rd[w_] = []
        deps.pop(i, None)
        fdeps = set()
        for d, kinds in deps.items():
            od = self.ops[d]
            if od["eng"] == eng and not od["dma"] and not dma:
                if eng == "pe":
                    continue
                if "raw" not in kinds:
                    continue
            fdeps.add(d)
        ln_ = 0
        if DEBUG_LINES:
            import sys as _sys
            f_ = _sys._getframe(1)
            while f_ is not None and f_.f_code.co_name != "build_program":
                f_ = f_.f_back
            ln_ = f_.f_lineno if f_ is not None else 0
        self.ops.append(dict(eng=eng, fn=fn, deps=fdeps, order=set(deps.keys()), dma=dma, signal=dma, token=None,
                             seg=self.seg, cost=cost, arena=arena, line=ln_))
        self.by_eng[eng].append(i)
        return i

    def barrier(self):
        self.seg += 1

    def schedule(self, W=24):
        ops = self.ops
        n = len(ops)
        succ = [[] for _ in range(n)]
        nun = [0] * n
        for i, op in enumerate(ops):
            nun[i] = len(op["order"])
            for d in op["order"]:
                succ[d].append(i)
        ready = [0.0] * n
        fin = [0.0] * n
        done = [False] * n
        head = {e: 0 for e in self.ENG}
        free = {e: 0.0 for e in self.ENG}
        lists = {e: list(self.by_eng[e]) for e in self.ENG}
        new_order = {e: [] for e in self.ENG}
        nseg = self.seg + 1
        rem = [0] * (nseg + 1)
        for i, op in enumerate(ops):
            if op["eng"] in self.GATED:
                rem[op["seg"]] += 1
        import os
        WDEF = {"pe": W, "act": W, "dve": W}
        WE = {e_: int(os.environ.get("KSCHED_W_" + e_.upper(), WDEF[e_])) for e_ in ("pe", "act", "dve")}
        segbusy = {}
        segend = {}
        cur = 0
        seg_t0 = 0.0
        tmax = 0.0
        nsched = 0
        while nsched < n:
            while cur < nseg and rem[cur] == 0:
                cur += 1
                seg_t0 = tmax
            best = None
            for e in self.ENG:
                lst = lists[e]
                h = head[e]
                while h < len(lst) and done[lst[h]]:
                    h += 1
                head[e] = h
                w = WE.get(e, 1)
                if e == "act" and cur in self.act_inorder:
                    w = 1
                seen = 0
                j = h
                while j < len(lst) and seen < w:
                    i = lst[j]
                    j += 1
                    if done[i]:
                        continue
                    op = ops[i]
                    if e != "pool":
                        if op["seg"] != cur:
                            break
                    elif op["arena"] and op["seg"] > cur:
                        break
                    seen += 1
                    if nun[i] == 0:
                        est = max(free[e], ready[i])
                        if e != "pool" or op["arena"]:
                            est = max(est, seg_t0)
                        key = (est, i)
                        if best is None or key < best[0]:
                            best = (key, e, i)
            if best is None:
                raise RuntimeError("scheduler stuck at seg %d" % cur)
            (est, _), e, i = best
            op = ops[i]
            if op["dma"]:
                free[e] = est + 0.06
                f = est + op["cost"]
            else:
                f = est + op["cost"]
                free[e] = f
            fin[i] = f
            tmax = max(tmax, f)
            if not op["dma"]:
                segbusy[(op["seg"], e)] = segbusy.get((op["seg"], e), 0.0) + op["cost"]
            segend[op["seg"]] = max(segend.get(op["seg"], 0.0), f)
            done[i] = True
            nsched += 1
            new_order[e].append(i)
            if e in self.GATED:
                rem[op["seg"]] -= 1
            for s_ in succ[i]:
                nun[s_] -= 1
                lat = 0.0 if (ops[s_]["eng"] == e and not op["dma"]) else 0.1
                if f + lat > ready[s_]:
                    ready[s_] = f + lat
        self.by_eng = new_order
        self.sim_time = tmax
        import os
        if os.environ.get("KSCHED_DEBUG"):
            print("sched: ops", n, "sim_time_us", round(tmax, 1), flush=True)
            prev = 0.0
            for sg in sorted(segend):
                print("  seg", sg, "dur", round(segend[sg] - prev, 1), {e_: round(segbusy.get((sg, e_), 0.0), 1)
                                                                         for e_ in ("pe", "act", "dve")})
                prev = segend[sg]
        pos = {}
        for e in self.ENG:
            for k, i in enumerate(new_order[e]):
                pos[i] = k
        bar = [set() for _ in range(nseg + 1)]
        for s_ in range(1, nseg + 1):
            for e in ("pe", "act", "dve"):
                last = None
                for i in new_order[e]:
                    if ops[i]["seg"] < s_:
                        last = i
                    else:
                        break
                if last is not None:
                    bar[s_].add(last)
            for i in new_order["sp"]:
                if ops[i]["seg"] == s_ - 1 and ops[i]["dma"]:
                    bar[s_].add(i)
        for e in self.GATED:
            lastseg = 0
            for i in new_order[e]:
                sg = ops[i]["seg"]
                if sg > lastseg:
                    for t in range(lastseg + 1, sg + 1):
                        ops[i]["deps"] |= bar[t]
                    lastseg = sg
        for i in new_order["pool"]:
            if ops[i]["arena"]:
                ops[i]["deps"] |= bar[ops[i]["seg"]]
        for i, op in enumerate(ops):
            keep = set()
            bestp = {}
            for d in op["deps"]:
                if d == i:
                    continue
                od = ops[d]
                if od["dma"]:
                    keep.add(d)
                else:
                    pe_ = od["eng"]
                    if pe_ not in bestp or pos[d] > pos[bestp[pe_]]:
                        bestp[pe_] = d
            keep |= set(bestp.values())
            op["deps"] = keep

    def mm(self, out, lhsT, rhs, start, stop, reads, writes):
        nfree = _free(rhs)
        c = max(nfree, 64) / 2400.0 * (4.0 if rhs.dtype == F32 else 1.0) + 0.004
        return self.add("pe", lambda e: e.matmul(out, lhsT, rhs, start=start, stop=stop), reads, writes, cost=c)

    def tr(self, out, in_, ident, reads, writes):
        return self.add("pe", lambda e: e.transpose(out, in_, ident), reads, writes, cost=0.25)

    def act(self, out, in_, func, reads, writes, bias=None, scale=None, accum=None):
        kw = {}
        if bias is not None:
            kw["bias"] = bias
        if scale is not None:
            kw["scale"] = scale
        if accum is not None:
            kw["accum_out"] = accum
        c = 0.22 + _free(out) / 1400.0
        return self.add("act", lambda e: e.activation(out=out, in_=in_, func=func, **kw), reads, writes, cost=c)

    def _vc(self, out):
        return 0.08 + _free(out) / 960.0

    def tt(self, out, in0, in1, op, reads, writes, eng="dve"):
        return self.add(eng, lambda e: e.tensor_tensor(out=out, in0=in0, in1=in1, op=op), reads, writes,
                        cost=self._vc(out))

    def ts(self, out, in0, s1, s2, op0, op1, reads, writes, eng="dve"):
        if s2 is None:
            return self.add(eng, lambda e: e.tensor_scalar(out, in0, s1, None, op0), reads, writes, cost=self._vc(out))
        return self.add(eng, lambda e: e.tensor_scalar(out, in0, s1, s2, op0, op1), reads, writes, cost=self._vc(out))

    def stt(self, out, in0, scalar, in1, op0, op1, reads, writes, eng="dve"):
        return self.add(eng, lambda e: e.scalar_tensor_tensor(out=out, in0=in0, scalar=scalar, in1=in1,
                                                             op0=op0, op1=op1), reads, writes, cost=self._vc(out))

    def cp(self, out, in_, reads, writes, eng="dve"):
        if eng == "act":
            return self.add("act", lambda e: e.activation(out=out, in_=in_, func=AF.Copy), reads, writes,
                            cost=0.22 + _free(out) / 1400.0)
        return self.add(eng, lambda e: e.tensor_copy(out=out, in_=in_), reads, writes, cost=self._vc(out))

    def recip(self, out, in_, reads, writes):
        return self.add("dve", lambda e: e.reciprocal(out=out, in_=in_), reads, writes, cost=0.1 + _free(out) / 240.0)

    def dma(self, out, in_, reads, writes, q="sp", arena=False):
        nbytes = 128 * _free(out) * 4
        return self.add(q, lambda e: e.dma_start(out=out, in_=in_), reads, writes, dma=True,
                        cost=2.0 + nbytes / 150e3, arena=arena)

    def finalize(self, es):
        nc = self.nc
        self.schedule()
        for op in self.ops:
            for d in op["deps"]:
                self.ops[d]["signal"] = True
        for d in self.final:
            self.ops[d]["signal"] = True
        csem = {e: es.enter_context(nc.semaphore("c_" + e)) for e in ("pe", "act", "dve", "pool")}
        NP = 16
        qsem = {q: [es.enter_context(nc.semaphore(f"q_{q}{k}")) for k in range(NP)] for q in ("sp", "pool")}
        cnt = {e: 0 for e in csem}
        qn = {q: 0 for q in qsem}
        quse = {q: [0] * NP for q in qsem}
        qprev = {q: [None] * NP for q in qsem}
        for i, op in enumerate(self.ops):
            if op["dma"]:
                q = op["eng"]
                k = qn[q] % NP
                qn[q] += 1
                quse[q][k] += 1
                op["token"] = (qsem[q][k], 16 * quse[q][k])
                if qprev[q][k] is not None:
                    op["deps"].add(qprev[q][k])
                qprev[q][k] = i
        for e in csem:
            for i in self.by_eng[e]:
                op = self.ops[i]
                if (not op["dma"]) and op["signal"]:
                    cnt[e] += 1
                    op["token"] = (csem[e], cnt[e])
        ops = self.ops
        final = list(self.final)

        def emit(engname, e):
            waited = {}
            for i in self.by_eng[engname]:
                op = ops[i]
                for d in sorted(op["deps"]):
                    sem, val = ops[d]["token"]
                    if waited.get(sem, 0) < val:
                        e.wait_ge(sem, val)
                        waited[sem] = val
                ins = op["fn"](e)
                if op["signal"]:
                    ins.then_inc(op["token"][0], 16 if op["dma"] else 1)
            if engname == "sp":
                for d in final:
                    sem, val = ops[d]["token"]
                    if waited.get(sem, 0) < val:
                        e.wait_ge(sem, val)
                        waited[sem] = val

        with nc.Block() as block:
            @block.tensor
            def _(e):
                emit("pe", e)

            @block.scalar
            def _(e):
                emit("act", e)

            @block.vector
            def _(e):
                emit("dve", e)

            @block.gpsimd
            def _(e):
                emit("pool", e)

            @block.sync
            def _(e):
                emit("sp", e)


def K(name, *idx):
    return (name,) + idx


def build_program(stop_after=None, dumps=(), n_layers=L_):
    nc = bass.Bass("TRN2", target_bir_lowering=False)
    S = Sched(nc)
    es = ExitStack()
    P = {}

    def din(name, shape):
        P[name] = nc.dram_tensor(name, list(shape), F32, kind="ExternalInput").ap()
        return P[name]

    x_d = din("x", [S_, D_])
    mem_d = din("mem", [256, D_])
    w_in_d = din("w_in", [L_, D_, 3076])
    w_out_d = din("w_out", [L_, D_, D_])
    wq_d = din("xattn_wq", [L_, D_, D_])
    wkv_d = din("xattn_wkv", [L_, D_, 2 * D_])
    wo_d = din("xattn_wo", [L_, D_, D_])
    w1_d = din("expert_w1", [L_, 16, D_, 256])
    w3_d = din("expert_w3", [L_, 16, D_, 256])
    w2_d = din("expert_w2", [L_, 16, 256, D_])
    wrt_d = din("router_w", [L_, D_, 20])
    lrubd_d = din("lru_wbd", [L_, 2, 2, 128, 128])
    colp_d = din("colp", [128, L_ * NCOL])
    rowp_d = din("rowp", [128, L_ * NROW])
    c128_d = din("c128", [128, 8 * 128])
    cmask_d = din("cmask", [128, 12 * 512])
    rope_d = din("rope", [128, 2 * S_])
    en_d = din("en", [16, 16 * 128])
    blkoh_d = din("blkoh", [8, S_])
    gate_c_d = din("gatec", [128, 3 * 128])
    out_d = nc.dram_tensor("out", [S_, D_], F32, kind="ExternalOutput").ap()
    dump_d = {}

    uid = [0]

    def sb(name, shape, dt=F32, stack=None):
        uid[0] += 1
        return (stack or es).enter_context(nc.sbuf_tensor(f"{name}_{uid[0]}", list(shape), dt))

    xT = sb("xT", [128, KC, S_])
    hT = sb("hT", [128, KC, S_], BF16)
    colp = sb("colp_s", [128, L_ * NCOL])
    rowp = sb("rowp_s", [128, L_ * NROW])
    c128 = sb("c128_s", [128, 8 * 128])
    c128b = sb("c128b_s", [128, 2 * 128], BF16)
    W = [sb(f"wslot{i}", [128, KC, 512], BF16) for i in range(3)]
    PS = [es.enter_context(nc.psum_tensor(f"ps{i}", [128, 512], F32)) for i in range(8)]
    wn = [0]

    ident = c128[:, 0:128]
    ones_f = c128[:, 128:256]
    tri_le = c128[:, 256:384]
    negu = c128[:, 384:512]
    negones = c128[:, 512:640]
    perm_f = c128[:, 640:768]
    ssdneg = c128[:, 768:896]
    ones_b = c128b[:, 128:256]
    ident_b = c128b[:, 0:128]

    cst = sb("cst", [128, 4])
    S.add("dve", lambda e: e.memset(cst[:, 0:1], EPS), [], [K("cst0")])
    S.add("dve", lambda e: e.memset(cst[:, 1:2], 1.0), [], [K("cst1")])
    S.add("dve", lambda e: e.memset(cst[:, 2:4], 0.0), [K("cst0"), K("cst1")], [K("cst")])
    S.dma(colp[:], colp_d[:, :], [], [K("colp")])
    S.dma(rowp[:], rowp_d[:, :], [], [K("rowp")])
    S.dma(c128[:], c128_d[:, :], [], [K("c128")])
    S.dma(c128b[:], c128_d[:, 0:256], [], [K("c128b")], q="pool")

    def col(l, name, j=0, n=1):
        c0, w = COLS[name]
        return colp[:, l * NCOL + c0 + j: l * NCOL + c0 + j + n]

    def row(l, name, j=0, n=None):
        c0, w = ROWS[name]
        n = w if n is None else n
        return rowp[:, l * NROW + c0 + j: l * NROW + c0 + j + n]

    nrot = [8]

    def ps_rot():
        k = S.psn % nrot[0]
        S.psn += 1
        return PS[k], K("ps", k)

    Wf = [w[:].rearrange("p a b -> p (a b)") for w in W]

    def load_w(src2d, nk, ncols, slot=None, c0=0, tot=None):
        tot = ncols if tot is None else tot
        if slot is None:
            k = wn[0] % 3
            wn[0] += 1
        else:
            k = slot
        v = Wf[k][:, 0:nk * tot].rearrange("p (a b) -> p a b", a=nk)
        step = 2 if nk >= 2 else 1
        allk = [K("w", k, j_, hf_) for j_ in range(4) for hf_ in range(2)]
        for h in range(0, nk, step):
            if nk == 8 and tot == 512:
                halves = [hf_ for hf_ in range(2) if c0 < (hf_ + 1) * 256 and c0 + ncols > hf_ * 256]
                wkeys_ = [K("w", k, h // 2, hf_) for hf_ in halves]
            else:
                wkeys_ = allk
            S.dma(v[:, h:h + step, c0:c0 + ncols],
                  src2d[h * 128:(h + step) * 128, :].rearrange("(kc p) n -> p kc n", p=128),
                  [], wkeys_, q="pool")
        return v, allk, k

    def dump(name, ap, reads, shape):
        if name in dumps:
            d = nc.dram_tensor("dbg_" + name, list(shape), ap.dtype, kind="ExternalOutput").ap()
            dump_d[name] = d
            i = S.dma(d, ap, reads, [K("dbg", name)])
            S.final.append(i)

    def load_fm(dst, src_d, ntile, dkey, st):
        xin = [sb(f"xin{j}", [128, D_], stack=st) for j in range(2)]
        for i in range(ntile):
            b = xin[i % 2]
            S.dma(b[:], src_d[i * 128:(i + 1) * 128, :], [], [K("xin", i % 2)])
            for half in range(2):
                ps, pk = ps_rot()
                for j in range(4):
                    kc = half * 4 + j
                    S.tr(ps[:, j * 128:(j + 1) * 128], b[:, kc * 128:(kc + 1) * 128], ident,
                         [K("xin", i % 2), K("c128")], [pk])
                S.cp(dst[:, half * 4:half * 4 + 4, i * 128:(i + 1) * 128],
                     ps[:].rearrange("p (a b) -> p a b", a=4), [pk],
                     [K(dkey, half * 4 + j, i // 4) for j in range(4)], eng=("dve" if half == 0 else "act"))

    st0 = ExitStack()
    load_fm(xT, x_d, NT, "xT", st0)

    def rmsnorm_fm(src, skey, gcol, dst, dkey, nk, T, st, dscale=1.0, hook=None, tag="n"):
        tw = min(T, TW)
        sq = [sb(f"sq_{tag}{j}", [128, nk, tw], BF16, stack=st) for j in range(2)]
        rs = [sb(f"rs_{tag}{j}", [128, tw], stack=st) for j in range(2)]
        for tc in range(T // tw):
            sl = slice(tc * tw, (tc + 1) * tw)
            q = sq[tc % 2]
            for kc in range(nk):
                S.act(q[:, kc, :], src[:, kc, sl], AF.Square, [K(skey, kc, tc)], [K("sq" + tag, tc % 2, kc)],
                      scale=float((nk * 128) ** -0.5))
            ps, pk = ps_rot()
            for kc in range(nk):
                S.mm(ps[:, 0:tw], ones_b, q[:, kc, :], kc == 0, kc == nk - 1,
                     [K("sq" + tag, tc % 2, kc), K("c128b")], [pk])
            r = rs[tc % 2]
            S.act(r[:], ps[:, 0:tw], AF.Ln, [pk, K("cst")], [K("rs" + tag, tc % 2)], bias=cst[:, 0:1])
            S.act(r[:], r[:], AF.Exp, [K("rs" + tag, tc % 2)], [K("rs" + tag, tc % 2)], scale=-0.5)
            if dst is not None:
                for kc in range(nk):
                    S.stt(dst[:, kc, sl], src[:, kc, sl], gcol(kc), r[:], ALU.mult, ALU.mult,
                          [K(skey, kc, tc), K("rs" + tag, tc % 2), K("colp")], [K(dkey, kc, tc)],
                          eng="dve")
            if hook is not None:
                hook(tc, r)

    ALLH = [K("hT", kc, tc) for kc in range(KC) for tc in range(NTC)]

    def hkeys(tc):
        return [K("hT", kc, tc) for kc in range(KC)]

    def proj_fm(wv, wkeys, c0, tc, ps, pk):
        for kc in range(KC):
            S.mm(ps[:, :], wv[:, kc, c0:c0 + 128], hT[:, kc, tc * TW:(tc + 1) * TW], kc == 0, kc == KC - 1,
                 wkeys + [K("hT", kc, tc)], [pk])

    def conv_chunk(ps, pk, tc, xw, xwk, cwcol, cbcol, dst, dkey):
        w_ = xw[tc % 2]
        wk = K(xwk, tc % 2)
        if tc == 0:
            S.add("dve", lambda e: e.memset(w_[:, 0:3], 0.0), [], [wk])
        else:
            S.cp(w_[:, 0:3], xw[(tc - 1) % 2][:, 512:515], [K(xwk, (tc - 1) % 2)], [wk])
        S.cp(w_[:, 3:515], ps[:, :], [pk], [wk], eng="act")
        S.ts(dst, w_[:, 3:515], cwcol(3), cbcol, ALU.mult, ALU.add, [wk, K("colp")], [dkey])
        for k in range(3):
            S.stt(dst, w_[:, k:k + 512], cwcol(k), dst, ALU.mult, ALU.add, [wk, dkey, K("colp")], [dkey])

    def group_out(l, g, YG, st):
        YB = sb("YB", [128, 2, S_], BF16, stack=st)
        rmsnorm_fm(YG, "YG", lambda kc: col(l, "grp_g", 2 * g + kc), YB, "YB", 2, S_, st, tag="g")
        dump(f"yn{l}_{g}", YB[:], [K("YB", c, t) for c in range(2) for t in range(NTC)], [128, 2, S_])
        for half in range(2):
            wv, wk, _ = load_w(w_out_d[l, g * 256:(g + 1) * 256, half * 512:(half + 1) * 512], 2, 512)
            for o4 in range(4):
                oc = half * 4 + o4
                for tc in range(NTC):
                    ps, pk = ps_rot()
                    for kc in range(2):
                        S.mm(ps[:, :], wv[:, kc, o4 * 128:(o4 + 1) * 128], YB[:, kc, tc * TW:(tc + 1) * TW],
                             kc == 0, kc == 1, wk + [K("YB", kc, tc)], [pk])
                    S.tt(xT[:, oc, tc * TW:(tc + 1) * TW], xT[:, oc, tc * TW:(tc + 1) * TW], ps[:, :], ALU.add,
                         [K("xT", oc, tc), pk], [K("xT", oc, tc)])

    ALLX = [K("xT", kc, tc) for kc in range(KC) for tc in range(NTC)]
    done = False
    for l in range(n_layers):
        with ExitStack() as st:
            rmsnorm_fm(xT, "xT", lambda kc: col(l, "mix_g", kc), hT, "hT", KC, S_, (st0 if l == 0 else st), tag="a")
        if l == 0:
            st0.close()
        S.barrier()
        if l == 0:
            dump("h0", hT[:], ALLH, [128, KC, S_])
        if stop_after == "norm0":
            break

        with ExitStack() as sty, ExitStack() as st:
            YG = sb("YG", [128, 2, S_], stack=sty)
            wv, wk, _ = load_w(w_in_d[l, :, 0:512], 8, 512)
            wbd = sb("wbd", [128, 4, 128], BF16, stack=st)
            S.dma(wbd[:], lrubd_d[l].rearrange("g c k m -> k (g c) m"), [], [K("wbd")], q="pool", arena=True)
            c8 = sb("c8", [128, 2], stack=st)
            dump(f"A_wbd{l}", wbd[:], [K("wbd")], [128, 4, 128])
            cy = sb("cy", [128, 2], stack=st)
            cz = sb("cz", [128, 2], stack=st)
            S.act(cy[:], col(l, "lru_lam", 0, 2), AF.Exp, [K("colp")], [K("cy")], scale=-1.0)
            S.ts(cz[:], cy[:], 2.0, None, ALU.add, None, [K("cy")], [K("cz")])
            S.add("dve", lambda e: e.reciprocal(out=cz[:], in_=cz[:]), [K("cz")], [K("cz")])
            S.tt(cz[:], cz[:], cy[:], ALU.mult, [K("cz"), K("cy")], [K("cz")])
            S.tt(cy[:], cz[:], cz[:], ALU.mult, [K("cz")], [K("cy")])
            S.ts(c8[:], cy[:], 1.0 / 9.0, 1.0 / 7.0, ALU.mult, ALU.add, [K("cy")], [K("c8")])
            for cc_ in (1.0 / 5.0, 1.0 / 3.0, 1.0):
                S.tt(c8[:], c8[:], cy[:], ALU.mult, [K("c8"), K("cy")], [K("c8")])
                S.ts(c8[:], c8[:], cc_, None, ALU.add, None, [K("c8")], [K("c8")])
            S.tt(c8[:], c8[:], cz[:], ALU.mult, [K("c8"), K("cz")], [K("c8")])
            S.ts(c8[:], c8[:], -16.0, None, ALU.mult, None, [K("c8")], [K("c8")])
            xw = [sb(f"xw{j}", [128, 515], stack=st) for j in range(2)]
            XC = sb("XC", [128, S_], stack=st)
            XCB = sb("XCB", [128, S_], BF16, stack=st)
            GG = sb("GG", [128, S_], stack=st)
            RA = sb("RA", [128, S_], stack=st)
            IU = sb("IU", [128, S_], stack=st)
            T2 = sb("T2", [128, S_], stack=st)
            XX = sb("XX", [128, S_], stack=st)
            for c in range(2):
                for tc in range(NTC):
                    sl = slice(tc * TW, (tc + 1) * TW)
                    ps, pk = ps_rot()
                    proj_fm(wv, wk, c * 128, tc, ps, pk)
                    conv_chunk(ps, pk, tc, xw, "xw", lambda k: col(l, "lru_cw", c * 4 + k), col(l, "lru_cb", c),
                               XC[:, sl], K("XC", tc))
                    ps, pk = ps_rot()
                    proj_fm(wv, wk, 256 + c * 128, tc, ps, pk)
                    S.cp(GG[:, sl], ps[:, :], [pk], [K("GG", tc)], eng="act")
                XCk = [K("XC", t) for t in range(NTC)]
                GGk = [K("GG", t) for t in range(NTC)]
                S.tt(T2[:], GG[:], GG[:], ALU.mult, GGk, [K("T2")])
                S.ts(T2[:], T2[:], 0.044715, 1.0, ALU.mult, ALU.add, [K("T2")], [K("T2")])
                S.tt(T2[:], T2[:], GG[:], ALU.mult, [K("T2")] + GGk, [K("T2")])
                S.act(T2[:], T2[:], AF.Sigmoid, [K("T2")], [K("T2")], scale=1.5957691216)
                S.tt(GG[:], GG[:], T2[:], ALU.mult, [K("T2")] + GGk, GGk)
                S.cp(XCB[:], XC[:], XCk, [K("XCB")], eng="act")
                for tc in range(NTC):
                    sl = slice(tc * TW, (tc + 1) * TW)
                    ps, pk = ps_rot()
                    S.mm(ps[:, :], wbd[:, c, :], XCB[:, sl], True, True, [K("wbd"), K("XCB")], [pk])
                    S.act(RA[:, sl], ps[:, :], AF.Sigmoid, [pk, K("colp")], [K("RA", tc)], bias=col(l, "lru_br", c))
                    ps, pk = ps_rot()
                    S.mm(ps[:, :], wbd[:, 2 + c, :], XCB[:, sl], True, True, [K("wbd"), K("XCB")], [pk])
                    S.act(IU[:, sl], ps[:, :], AF.Sigmoid, [pk, K("colp")], [K("IU", tc)], bias=col(l, "lru_bi", c))
                RAk = [K("RA", t) for t in range(NTC)]
                IUk = [K("IU", t) for t in range(NTC)]
                if c == 1:
                    dump(f"A_r{l}", RA[:], RAk, [128, S_])
                    dump(f"A_i{l}", IU[:], IUk, [128, S_])
                    dump(f"A_xcb{l}", XCB[:], [K("XCB")], [128, S_])
                    dump(f"A_xc{l}", XC[:], XCk, [128, S_])
                S.ts(XX[:], RA[:], c8[:, c:c + 1], 2.0, ALU.mult, ALU.mult, RAk + [K("c8")], [K("XX")])
                S.ts(T2[:], XX[:], 1.0 / 120.0, None, ALU.mult, None, [K("XX")], [K("T2")])
                for cc_ in (1.0 / 24.0, 1.0 / 6.0, 0.5, 1.0):
                    S.stt(T2[:], T2[:], cc_, XX[:], ALU.add, ALU.mult, [K("T2"), K("XX")], [K("T2")])
                S.act(T2[:], T2[:], AF.Sqrt, [K("T2")], [K("T2")], scale=-1.0)
                S.act(RA[:], XX[:], AF.Exp, [K("XX")], RAk, scale=0.5)
                S.tt(IU[:], IU[:], XC[:], ALU.mult, IUk + XCk, IUk)
                S.tt(IU[:], IU[:], T2[:], ALU.mult, IUk + [K("T2")], IUk)
                S.add("dve", lambda e: e.tensor_tensor_scan(out=XC[:], data0=RA[:], data1=IU[:], initial=0.0,
                                                            op0=ALU.mult, op1=ALU.add), RAk + IUk, XCk, cost=2.6)
                S.tt(YG[:, c, :], XC[:], GG[:], ALU.mult, XCk + GGk, [K("YG", c, t) for t in range(NTC)])
            for nm_, t_ in (("XC", XC), ("GG", GG), ("RA", RA), ("IU", IU), ("T2", T2)):
                dump(f"A_{nm_}{l}", t_[:], [K(nm_, t) for t in range(NTC)] + [K(nm_)], [128, S_])
            dump(f"ya{l}", YG[:], [K("YG", c, t) for c in range(2) for t in range(NTC)], [128, 2, S_])
            st.close()
            S.barrier()
            group_out(l, 0, YG, sty)
        S.barrier()
        if stop_after == f"A{l}":
            done = True
            break

        nrot[0] = 4
        with ExitStack() as sty, ExitStack() as st:
            YG = sb("YG", [128, 2, S_], stack=sty)
            QB = sb("QB", [128, 2, S_], BF16, stack=st)
            KZ = sb("KZ", [128, 4, S_], BF16, stack=st)
            S.add("dve", lambda e: e.memset(KZ[:], 0.0), [], [K("KZ", h_, t_) for h_ in range(4) for t_ in range(NTC)], cost=4.5)
            VB = sb("VB", [128, NT, 256], BF16, stack=st)
            M01 = sb("M01", [128, 4, 512], stack=st)
            NM = sb("NM", [128, 4, 512], BF16, stack=st)
            S.dma(M01[:], cmask_d[:, 0:2048].rearrange("p (a b) -> p a b", a=4), [], [K("M01")])
            S.dma(NM[:], cmask_d[:, 2048:4096].rearrange("p (a b) -> p a b", a=4), [], [K("NM")], q="pool", arena=True)
            wv, wk, _ = load_w(w_in_d[l, :, 512:1024], 8, 512)
            wv2, wk2, _ = load_w(w_in_d[l, :, 1024:1280], 8, 256)
            for c2 in range(2):
                for tc in range(NTC):
                    sl = slice(tc * TW, (tc + 1) * TW)
                    ps, pk = ps_rot()
                    proj_fm(wv, wk, c2 * 128, tc, ps, pk)
                    S.ts(QB[:, c2, sl], ps[:, :], 0.125, None, ALU.mult, None, [pk], [K("QB", c2, tc)])
                    ps, pk = ps_rot()
                    proj_fm(wv, wk, 256 + c2 * 128, tc, ps, pk)
                    S.cp(KZ[0:64, 2 * c2, sl], ps[0:64, :], [pk], [K("KZ", 2 * c2, tc)])
                    S.cp(KZ[64:128, 2 * c2 + 1, sl], ps[64:128, :], [pk], [K("KZ", 2 * c2 + 1, tc)])
            for i in range(NT):
                ps, pk = ps_rot()
                for kc in range(KC):
                    S.mm(ps[:, 0:256], hT[:, kc, i * 128:(i + 1) * 128], wv2[:, kc, :], kc == 0, kc == KC - 1,
                         wk2 + [K("hT", kc, i // 4)], [pk])
                S.cp(VB[:, i, :], ps[:, 0:256], [pk], [K("VB", i)], eng=("dve" if i % 2 else "act"))
            Eb = [sb(f"Eb{j}", [128, 512], stack=st) for j in range(2)]
            SPb = [sb(f"SPb{j}", [128, 512], F32R, stack=st) for j in range(3)]
            Wb = [sb(f"Wb{j}", [128, 512], BF16, stack=st) for j in range(2)]
            Rb = [sb(f"Rb{j}", [128, 512], F32R, stack=st) for j in range(2)]
            NUr = sb("NUr", [128, 2, 128], F32R, stack=st)
            S.cp(NUr[:, 0, :], negu, [K("c128")], [K("NUr")])
            S.cp(NUr[:, 1, :], negones, [K("c128")], [K("NUr")])
            items = []
            grp = 0
            for h in range(4):
                for c in range(NTC):
                    nb = 4 * c + 4
                    for idx, b in enumerate(range(nb - 1, -1, -1)):
                        items.append(dict(h=h, c=c, idx=idx, b=b, grp=grp, last=(b == 0)))
                    grp += 1
            for i_, itm in enumerate(items):
                itm["i"] = i_
                itm["pr"] = slice((itm["h"] % 2) * 64, (itm["h"] % 2) * 64 + 64)
                itm["c2"] = itm["h"] // 2
                itm["d"] = itm["b"] - 4 * itm["c"]
                itm["ksl"] = slice(itm["b"] * 128, (itm["b"] + 1) * 128)
                itm["qsl"] = slice(itm["c"] * TW, (itm["c"] + 1) * TW)
                itm["qk"] = [K("KZ", itm["h"], itm["b"] // 4), K("QB", itm["c2"], itm["c"])]

            def sb_s1(m):
                i = m["i"]
                pr, c2 = m["pr"], m["c2"]
                off = 128 * max(m["d"], 0)
                cs = slice(off, TW)
                qs = slice(m["c"] * TW + off, (m["c"] + 1) * TW)
                if m["idx"] == 0:
                    R0, R0k = Rb[m["grp"] % 2], K("Rb", m["grp"] % 2)
                    S.ts(R0[:], M01[:, 0, :], 0.0, None, ALU.mult, None, [K("M01")], [R0k])
                psZ, pkZ = ps_rot()
                S.mm(psZ[:, cs], KZ[:, m["h"], m["ksl"]], QB[:, c2, qs], True, True, m["qk"], [pkZ])
                E, Ek = Eb[i % 2], K("Eb", i % 2)
                SP, SPk = SPb[i % 3], K("SPb", i % 3)
                S.act(E[:, cs], psZ[:, cs], AF.Exp, [pkZ], [Ek])
                S.act(SP[:, cs], E[:, cs], AF.Ln, [Ek, K("cst")], [SPk], bias=cst[:, 1:2])
                if m["d"] >= 0:
                    S.tt(SP[:, cs], SP[:, cs].bitcast(F32), M01[:, m["d"], cs], ALU.mult, [SPk, K("M01")], [SPk])

            def sb_s2(m):
                i = m["i"]
                pr, c2, d = m["pr"], m["c2"], m["d"]
                off = 128 * max(d, 0)
                cs = slice(off, TW)
                qs = slice(m["c"] * TW + off, (m["c"] + 1) * TW)
                SP, SPk = SPb[i % 3], K("SPb", i % 3)
                Wt, Wk_ = Wb[i % 2], K("Wb", i % 2)
                R, Rk = Rb[m["grp"] % 2], K("Rb", m["grp"] % 2)
                psA, pkA = ps_rot()
                has_r = m["idx"] > 0
                S.mm(psA[:, cs], KZ[:, m["h"], m["ksl"]], QB[:, c2, qs], True, False, m["qk"], [pkA])
                S.mm(psA[:, cs], NUr[:, 0, :], SP[:, cs], False, (not has_r) and d < 0, [SPk, K("NUr")], [pkA])
                if has_r:
                    S.mm(psA[:, cs], NUr[:, 1, :], R[:, cs], False, d < 0, [Rk, K("NUr")], [pkA])
                if d >= 0:
                    S.mm(psA[:, cs], ident_b, NM[:, d, cs], False, True, [K("NM"), K("c128b")], [pkA])
                S.act(Wt[:, cs], psA[:, cs], AF.Exp, [pkA], [Wk_])
                if not m["last"]:
                    S.tt(R[:, cs], R[:, cs].bitcast(F32), SP[:, cs].bitcast(F32), ALU.add, [Rk, SPk], [Rk])

            def sb_s3(m):
                i = m["i"]
                pr, c2, h = m["pr"], m["c2"], m["h"]
                off = 128 * max(m["d"], 0)
                cs = slice(off, TW)
                Wt, Wk_ = Wb[i % 2], K("Wb", i % 2)
                psO, pkO = PS[4 + m["grp"] % 2], K("psO", m["grp"] % 2)
                S.mm(psO[:, cs], VB[:, m["b"], c2 * 128:(c2 + 1) * 128], Wt[:, cs], m["idx"] == 0, m["last"],
                     [K("VB", m["b"]), Wk_], [pkO])
                if m["last"]:
                    S.cp(YG[pr, c2, m["qsl"]], psO[pr, :], [pkO], [K("YG", c2, m["c"])])

            n_it = len(items)
            for r in range(-2, n_it):
                if 0 <= r + 2 < n_it:
                    sb_s1(items[r + 2])
                if 0 <= r + 1 < n_it:
                    sb_s2(items[r + 1])
                if 0 <= r < n_it:
                    sb_s3(items[r])
            dump(f"yb{l}", YG[:], [K("YG", c, t) for c in range(2) for t in range(NTC)], [128, 2, S_])
            st.close()
            S.barrier()
            nrot[0] = 8
            group_out(l, 1, YG, sty)
        S.barrier()
        if stop_after == f"B{l}":
            done = True
            break

        nrot[0] = 4
        with ExitStack() as sty, ExitStack() as st:
            YG = sb("YG", [128, 2, S_], stack=sty)
            XS = sb("XS", [128, 2, S_], stack=st)
            BTb = sb("BTb", [128, 2, S_], BF16, stack=st)
            CTb = sb("CTb", [128, 2, S_], BF16, stack=st)
            BTM = sb("BTM", [128, NT, 256], BF16, stack=st)
            XTM = sb("XTM", [128, NT, 256], BF16, stack=st)
            wdt = sb("wdt", [128, KC, 4], BF16, stack=st)
            stc = ExitStack()
            xw = [sb(f"xwc{j}", [128, 515], stack=stc) for j in range(2)]
            CV = [sb(f"CV{j}", [128, 512], stack=stc) for j in range(2)]
            BF = [sb(f"BF{j}", [128, 512], stack=stc) for j in range(2)]
            S.dma(wdt[:], w_in_d[l, :, 2304:2308].rearrange("(kc p) n -> p kc n", p=128), [], [K("wdt")], q="pool", arena=True)
            wv1, wk1, _ = load_w(w_in_d[l, :, 1280:1792], 8, 512)
            wv2, wk2, _ = load_w(w_in_d[l, :, 1792:2304], 8, 512)
            n_cv = 0
            for j in range(6):
                if j < 2:
                    wv, wk, c0 = wv1, wk1, 256 + j * 128
                elif j < 4:
                    wv, wk, c0 = wv2, wk2, (j - 2) * 128
                else:
                    wv, wk, c0 = wv2, wk2, 256 + (j - 4) * 128
                for tc in range(NTC):
                    sl = slice(tc * TW, (tc + 1) * TW)
                    ps, pk = ps_rot()
                    proj_fm(wv, wk, c0, tc, ps, pk)
                    cv, cvk = CV[n_cv % 2], K("CV", n_cv % 2)
                    conv_chunk(ps, pk, tc, xw, "xwc", lambda k: col(l, "ssm_cw", j * 4 + k), col(l, "ssm_cb", j),
                               cv[:], cvk)
                    if j < 2:
                        S.act(XS[:, j, sl], cv[:], AF.Silu, [cvk], [K("XS", j, tc)])
                        ps, pk = ps_rot()
                        for q in range(4):
                            S.tr(ps[:, q * 128:(q + 1) * 128], XS[:, j, tc * TW + q * 128: tc * TW + (q + 1) * 128],
                                 ident, [K("XS", j, tc), K("c128")], [pk])
                        S.cp(XTM[:, tc * 4:tc * 4 + 4, j * 128:(j + 1) * 128],
                             ps[:].rearrange("p (a b) -> p a b", a=4), [pk], [K("XTM", tc, j)])
                    elif j < 4:
                        g = j - 2
                        bf, bfk = BF[n_cv % 2], K("BF", n_cv % 2)
                        S.act(bf[:], cv[:], AF.Silu, [cvk], [bfk])
                        S.cp(BTb[:, g, sl], bf[:], [bfk], [K("BTb", g, tc)])
                        ps, pk = ps_rot()
                        for q in range(4):
                            S.tr(ps[:, q * 128:(q + 1) * 128], bf[:, q * 128:(q + 1) * 128], ident,
                                 [bfk, K("c128")], [pk])
                        S.cp(BTM[:, tc * 4:tc * 4 + 4, g * 128:(g + 1) * 128],
                             ps[:].rearrange("p (a b) -> p a b", a=4), [pk], [K("BTM", tc, g)])
                    else:
                        S.act(CTb[:, j - 4, sl], cv[:], AF.Silu, [cvk], [K("CTb", j - 4, tc)])
                    n_cv += 1
            stc.close()
            S.barrier()
            DT = sb("DT", [128, 64], stack=st)
            AN = sb("AN", [128, 64], stack=st)
            AC = sb("AC", [128, 64], stack=st)
            ACS = sb("ACS", [128, 64], stack=st)
            TOT = sb("TOT", [128, 64], stack=st)
            DEC = sb("DEC", [128, 64], stack=st)
            ETOT = sb("ETOT", [128, 64], stack=st)
            DTD = sb("DTD", [128, 64], stack=st)
            psd, pkd = ps_rot()
            for i in range(NT):
                for kc in range(KC):
                    S.mm(psd[:, i * 4:(i + 1) * 4], hT[:, kc, i * 128:(i + 1) * 128], wdt[:, kc, :], kc == 0,
                         kc == KC - 1, [K("wdt"), K("hT", kc, i // 4)], [pkd])
            S.tt(DT[:], psd[:, 0:64], row(l, "dt_bias"), ALU.add, [pkd, K("rowp")], [K("DT")])
            S.act(DT[:], DT[:], AF.Exp, [K("DT")], [K("DT")])
            S.act(DT[:], DT[:], AF.Ln, [K("DT"), K("cst")], [K("DT")], bias=cst[:, 1:2])
            S.act(AN[:], row(l, "a_log"), AF.Exp, [K("rowp")], [K("AN")])
            S.ts(AN[:], AN[:], -1.0, None, ALU.mult, None, [K("AN")], [K("AN")])
            S.tt(AC[:], DT[:], AN[:], ALU.mult, [K("DT"), K("AN")], [K("AC")])
            ps, pk = ps_rot()
            S.mm(ps[:, 0:64], tri_le, AC[:], True, True, [K("AC"), K("c128")], [pk])
            S.cp(ACS[:], ps[:, 0:64], [pk], [K("ACS")])
            ps, pk = ps_rot()
            S.mm(ps[:, 0:64], ones_f, AC[:], True, True, [K("AC"), K("c128")], [pk])
            S.cp(TOT[:], ps[:, 0:64], [pk], [K("TOT")])
            S.tt(DEC[:], TOT[:], ACS[:], ALU.subtract, [K("TOT"), K("ACS")], [K("DEC")])
            S.act(DEC[:], DEC[:], AF.Exp, [K("DEC")], [K("DEC")])
            S.act(ETOT[:], TOT[:], AF.Exp, [K("TOT")], [K("ETOT")])
            S.tt(DTD[:], DT[:], DEC[:], ALU.mult, [K("DT"), K("DEC")], [K("DTD")])
            ST = sb("ST", [128, 4, 64], stack=st)
            S.add("dve", lambda e: e.memset(ST[:], 0.0), [], [K("ST", h) for h in range(4)])
            Rm = [sb(f"Rm{j}", [128, 128], stack=st) for j in range(2)]
            ARG = [sb(f"ARG{j}", [128, 128], stack=st) for j in range(2)]
            E1 = [sb(f"E1{j}", [128, 128], stack=st) for j in range(2)]
            GL = [sb(f"GL{j}", [128, 128], BF16, stack=st) for j in range(2)]
            CTp = [sb(f"CTp{j}", [128, 128], BF16, stack=st) for j in range(2)]
            XCp = [[sb(f"XCp{a}{j}", [128, 128], BF16, stack=st) for j in range(2)] for a in range(2)]
            STp = sb("STp", [128, 4, 128], BF16, stack=st)
            for a in range(2):
                for j in range(2):
                    S.add("dve", (lambda e, t_=XCp[a][j]: e.memset(t_[:], 0.0)), [], [K("XCp", a, j)])
            S.add("dve", lambda e: e.memset(STp[:], 0.0), [], [K("STp", h) for h in range(4)])
            XDh = [sb(f"XDh{j}", [128, 64], BF16, stack=st) for j in range(2)]
            n = 0
            for ci in range(NT):
                csl = slice(ci * 128, (ci + 1) * 128)
                tcx = ci // 4
                for g in range(2):
                    psG, pkG = ps_rot()
                    S.mm(psG[:, 0:128], BTb[:, g, csl], CTb[:, g, csl], True, True,
                         [K("BTb", g, tcx), K("CTb", g, tcx)], [pkG])
                    psY, pkY = PS[4 + (ci * 2 + g) % 2], K("psO", (ci * 2 + g) % 2)
                    for hh in range(2):
                        h = 2 * g + hh
                        pr = slice(hh * 64, hh * 64 + 64)
                        cidx = ci * 4 + h
                        k2 = n % 2
                        S.ts(Rm[k2][:], tri_le, AC[:, cidx:cidx + 1], None, ALU.mult, None,
                             [K("AC"), K("c128")], [K("Rm", k2)])
                        ps1, pk1 = ps_rot()
                        S.mm(ps1[:, 0:128], ones_f, Rm[k2][:], True, True, [K("Rm", k2), K("c128")], [pk1])
                        S.stt(ARG[k2][:], ps1[:, 0:128], ACS[:, cidx:cidx + 1], ssdneg, ALU.subtract, ALU.add,
                              [pk1, K("ACS"), K("c128")], [K("ARG", k2)])
                        S.act(ARG[k2][:], ARG[k2][:], AF.Exp, [K("ARG", k2)], [K("ARG", k2)])
                        S.tt(GL[k2][:], ARG[k2][:], psG[:, 0:128], ALU.mult, [K("ARG", k2), pkG], [K("GL", k2)])
                        S.act(E1[k2][:], ps1[:, 0:128], AF.Exp, [pk1, K("ARG", k2)], [K("E1", k2)])
                        S.tt(CTp[k2][:], CTb[:, g, csl], E1[k2][:], ALU.mult, [K("CTb", g, tcx), K("E1", k2)],
                             [K("CTp", k2)])
                        rot = (ci * 2 + g) % 2
                        xcp = XCp[hh][rot]
                        S.ts(xcp[:, hh * 64:(hh + 1) * 64], XTM[:, ci, h * 64:(h + 1) * 64], DT[:, cidx:cidx + 1], None,
                             ALU.mult, None, [K("XTM", tcx, g), K("DT")], [K("XCp", hh, rot)])
                        S.mm(psY[:, 0:128], xcp[:], GL[k2][:], hh == 0, False, [K("XCp", hh, rot), K("GL", k2)], [pkY])
                        S.mm(psY[:, 0:128], STp[:, h, :], CTp[k2][:], False, hh == 1, [K("STp", h), K("CTp", k2)], [pkY])
                        S.ts(XDh[k2][:], XTM[:, ci, h * 64:(h + 1) * 64], DTD[:, cidx:cidx + 1], None, ALU.mult, None,
                             [K("XTM", tcx, g), K("DTD")], [K("XDh", k2)])
                        psS, pkS = ps_rot()
                        S.mm(psS[:, 0:64], BTM[:, ci, g * 128:(g + 1) * 128], XDh[k2][:], True, True,
                             [K("BTM", tcx, g), K("XDh", k2)], [pkS])
                        S.stt(ST[:, h, :], ST[:, h, :], ETOT[:, cidx:cidx + 1], psS[:, 0:64], ALU.mult, ALU.add,
                              [K("ST", h), K("ETOT"), pkS], [K("ST", h)])
                        S.cp(STp[:, h, hh * 64:(hh + 1) * 64], ST[:, h, :], [K("ST", h)], [K("STp", h)], eng="act")
                        n += 1
                    S.cp(YG[:, g, csl], psY[:, 0:128], [pkY], [K("YG", g, tcx)], eng="act")
            ARG = [sb(f"ZT{j}", [128, 512], stack=st) for j in range(1)]
            for j in range(2):
                YGk = [K("YG", j, t) for t in range(NTC)]
                S.stt(YG[:, j, :], XS[:, j, :], col(l, "ssm_d", j), YG[:, j, :], ALU.mult, ALU.add,
                      [K("XS", j, t) for t in range(NTC)] + YGk + [K("colp")], YGk)
                for tc in range(NTC):
                    sl = slice(tc * TW, (tc + 1) * TW)
                    ps, pk = ps_rot()
                    proj_fm(wv1, wk1, j * 128, tc, ps, pk)
                    zt, ztk = ARG[0], K("ARGz", 0)
                    S.act(zt[:], ps[:, :], AF.Silu, [pk], [ztk])
                    S.tt(YG[:, j, sl], YG[:, j, sl], zt[:], ALU.mult, [K("YG", j, tc), ztk], [K("YG", j, tc)])
            dump(f"yc{l}", YG[:], [K("YG", c, t) for c in range(2) for t in range(NTC)], [128, 2, S_])
            st.close()
            S.barrier()
            nrot[0] = 8
            group_out(l, 2, YG, sty)
        S.barrier()
        if stop_after == f"C{l}":
            done = True
            break

        nrot[0] = 4
        with ExitStack() as sty, ExitStack() as st:
            YG = sb("YG", [128, 2, S_], stack=sty)
            QA = sb("QA", [128, 4, S_], BF16, stack=st)
            KA = sb("KA", [128, 4, S_], BF16, stack=st)
            allqa = [K("QA", h_, t_) for h_ in range(4) for t_ in range(NTC)]
            allka = [K("KA", h_, t_) for h_ in range(4) for t_ in range(NTC)]
            S.add("dve", lambda e: e.memset(QA[:], 0.0), [], allqa, cost=4.5)
            S.add("dve", lambda e: e.memset(KA[:], 0.0), [], allka, cost=4.5)
            for h_ in range(4):
                o_ = 64 if h_ % 2 == 0 else 0
                S.dma(KA[o_:o_ + 8, h_, :], blkoh_d[:, :], allka, [K("KAoh", h_)], q="pool", arena=True)
            VB = sb("VB", [128, NT, 256], BF16, stack=st)
            CAUS = sb("CAUS", [128, 4, 512], BF16, stack=st)
            S.dma(CAUS[:], cmask_d[:, 4096:6144].rearrange("p (a b) -> p a b", a=4), [], [K("CAUS")], q="pool", arena=True)
            GC = sb("GC", [128, 3, 128], stack=st)
            S.dma(GC[:], gate_c_d[:, :].rearrange("p (a b) -> p a b", a=3), [], [K("GC")])
            KM = sb("KM", [128, 2, 8], stack=st)
            KMb = sb("KMb", [128, 2, 8], BF16, stack=st)
            wv, wk, _ = load_w(w_in_d[l, :, 2308:2820], 8, 512)
            wv2, wk2, _ = load_w(w_in_d[l, :, 2820:3076], 8, 256)
            str_ = ExitStack()
            ROPEb = [sb(f"ROPE{j}", [128, 2, TW], stack=str_) for j in range(1)]
            RAW = [sb(f"RAW{j}", [128, 512], stack=str_) for j in range(2)]
            TMP = [sb(f"TMPr{j}", [128, 512], stack=str_) for j in range(2)]
            KF = [sb(f"KF{j}", [128, 512], stack=str_) for j in range(2)]
            nr = 0
            for c2 in range(2):
                for tc in range(NTC):
                    sl = slice(tc * TW, (tc + 1) * TW)
                    rj = 0
                    ROPE = ROPEb[rj]
                    rsl = slice(0, TW)
                    S.dma(ROPE[:, 0, :], rope_d[:, tc * TW:(tc + 1) * TW], [], [K("ROPE", rj, 0)])
                    S.dma(ROPE[:, 1, :], rope_d[:, S_ + tc * TW:S_ + (tc + 1) * TW], [], [K("ROPE", rj, 1)])
                    for which in range(2):
                        ps, pk = ps_rot()
                        proj_fm(wv, wk, which * 256 + c2 * 128, tc, ps, pk)
                        raw, rawk = RAW[nr % 2], K("RAW", nr % 2)
                        tmp, tmpk = TMP[nr % 2], K("TMPr", nr % 2)
                        S.cp(raw[:], ps[:, :], [pk], [rawk], eng="act")
                        psw, pkw = ps_rot()
                        S.mm(psw[:, :], perm_f, raw[:], True, True, [rawk, K("c128")], [pkw])
                        S.tt(tmp[:], psw[:, :], ROPE[:, 1, rsl], ALU.mult, [pkw, K("ROPE", rj, 1)], [tmpk])
                        kf = KF[nr % 2]
                        dst, dk = kf[:], K("KF", nr % 2)
                        S.tt(dst, raw[:], ROPE[:, 0, rsl], ALU.mult, [rawk, K("ROPE", rj, 0)], [dk])
                        S.tt(dst, dst, tmp[:], ALU.add, [dk, tmpk], [dk])
                        if which == 0:
                            S.act(QA[0:64, 2 * c2, sl], kf[0:64, :], AF.Copy, [dk], [K("QA", 2 * c2, tc)], scale=0.125)
                            S.act(QA[64:128, 2 * c2 + 1, sl], kf[64:128, :], AF.Copy, [dk], [K("QA", 2 * c2 + 1, tc)],
                                  scale=0.125)
                        else:
                            S.cp(KA[0:64, 2 * c2, sl], kf[0:64, :], [dk], [K("KA", 2 * c2, tc)], eng="act")
                            S.cp(KA[64:128, 2 * c2 + 1, sl], kf[64:128, :], [dk], [K("KA", 2 * c2 + 1, tc)], eng="act")
                            for hb in range(2):
                                nblk = tc * 2 + hb
                                S.add("dve", (lambda e, o=KM[:, c2, nblk:nblk + 1], i_=kf[:, hb * 256:(hb + 1) * 256]:
                                              e.reduce_sum(out=o, in_=i_, axis=AX.X)), [dk], [K("KM", c2, nblk)])
                        nr += 1
            for i in range(NT):
                ps, pk = ps_rot()
                for kc in range(KC):
                    S.mm(ps[:, 0:256], hT[:, kc, i * 128:(i + 1) * 128], wv2[:, kc, :], kc == 0, kc == KC - 1,
                         wk2 + [K("hT", kc, i // 4)], [pk])
                S.cp(VB[:, i, :], ps[:, 0:256], [pk], [K("VB", i)], eng=("dve" if i % 2 else "act"))
            S.cp(KMb[:], KM[:], [K("KM", c2_, nb_) for c2_ in range(2) for nb_ in range(8)], [K("KMb")])
            str_.close()
            S.barrier()
            PTb = [sb(f"PTb{j}", [128, 512], BF16, stack=st) for j in range(3)]
            RD = [sb(f"RD{j}", [128, 512], stack=st) for j in range(2)]
            GT = sb("GT", [128, 128], stack=st)
            BT_ = sb("BIASTM", [128, 128], stack=st)
            MX = [sb(f"MX{j}", [128, 8], stack=st) for j in range(2)]
            for h in range(4):
                hh, c2 = h % 2, h // 2
                pr = slice(hh * 64, hh * 64 + 64)
                o_ = 64 if hh == 0 else 0
                psg, pkg = ps_rot()
                for i in range(NT):
                    S.mm(psg[:, i * 8:(i + 1) * 8], QA[pr, h, i * 128:(i + 1) * 128], KMb[pr, c2, :], True, True,
                         [K("QA", h, i // 4), K("KMb")], [pkg])
                S.stt(GT[:], psg[:, 0:128], 8.0 / 256.0, GC[:, 0, :], ALU.mult, ALU.add, [pkg, K("GC")], [K("GT")])
                for i in range(NT):
                    mx, mxk = MX[i % 2], K("MX", i % 2)
                    S.add("dve", (lambda e, o=mx[:], i_=GT[:, i * 8:(i + 1) * 8]: e.max(out=o, in_=i_)),
                          [K("GT")], [mxk])
                    bsl = BT_[:, i * 8:(i + 1) * 8]
                    S.ts(bsl, GT[:, i * 8:(i + 1) * 8], mx[:, 2:3], None, ALU.is_ge, None, [K("GT"), mxk],
                         [K("BIASTM", i)])
                    S.tt(bsl, bsl, GC[:, 1, i * 8:(i + 1) * 8], ALU.mult, [K("BIASTM", i), K("GC")], [K("BIASTM", i)])
                    S.tt(bsl, bsl, GC[:, 2, i * 8:(i + 1) * 8], ALU.add, [K("BIASTM", i), K("GC")], [K("BIASTM", i)])
                    S.ts(bsl, bsl, -1.0, -NEG, ALU.add, ALU.mult, [K("BIASTM", i)], [K("BIASTM", i)])
                for q4 in range(4):
                    pst, pkt = ps_rot()
                    for q in range(4):
                        i = q4 * 4 + q
                        S.tr(pst[0:8, q * 128:(q + 1) * 128], BT_[:, i * 8:(i + 1) * 8], ident,
                             [K("BIASTM", i), K("c128")], [pkt])
                    S.cp(QA[o_:o_ + 8, h, q4 * 512:(q4 + 1) * 512], pst[0:8, :], [pkt], [K("QAsel", h, q4)])
            its = []
            grp = 0
            for h in range(4):
                for c in range(NTC):
                    nkt = 4 * c + 4
                    for kt in range(nkt):
                        its.append(dict(h=h, c=c, kt=kt, nkt=nkt, grp=grp, i=len(its)))
                    grp += 1

            def m_s1(m):
                h, c, kt = m["h"], m["c"], m["kt"]
                d = kt - 4 * c
                qsl = slice(c * TW, (c + 1) * TW)
                ksl = slice(kt * 128, (kt + 1) * 128)
                off = 128 * max(d, 0)
                cs = slice(off, TW)
                qs = slice(c * TW + off, (c + 1) * TW)
                psS, pkS = ps_rot()
                S.mm(psS[:, cs], KA[:, h, ksl], QA[:, h, qs], True, d < 0,
                     [K("KA", h, kt // 4), K("KAoh", h), K("QA", h, c), K("QAsel", h, c)], [pkS])
                if d >= 0:
                    S.mm(psS[:, cs], ident_b, CAUS[:, d, cs], False, True, [K("CAUS"), K("c128b")], [pkS])
                pt, ptk = PTb[m["i"] % 3], K("PTb", m["i"] % 3)
                S.act(pt[:, cs], psS[:, cs], AF.Exp, [pkS], [ptk])

            def m_s2(m):
                h, c, kt, nkt = m["h"], m["c"], m["kt"], m["nkt"]
                hh, c2 = h % 2, h // 2
                pr = slice(hh * 64, hh * 64 + 64)
                qsl = slice(c * TW, (c + 1) * TW)
                g2 = m["grp"] % 2
                psO, pkO = PS[4 + g2], K("psO", g2)
                psD, pkD = PS[6 + g2], K("psD", g2)
                pt, ptk = PTb[m["i"] % 3], K("PTb", m["i"] % 3)
                off = 128 * max(kt - 4 * c, 0)
                cs = slice(off, TW)
                S.mm(psO[:, cs], VB[:, kt, c2 * 128:(c2 + 1) * 128], pt[:, cs], kt == 0, kt == nkt - 1,
                     [K("VB", kt), ptk], [pkO])
                S.mm(psD[:, cs], ones_b, pt[:, cs], kt == 0, kt == nkt - 1, [ptk, K("c128b")], [pkD])
                if kt == nkt - 1:
                    rd, rdk = RD[g2], K("RD", g2)
                    S.add("dve", (lambda e, o=rd[pr, :], i_=psD[pr, :]: e.reciprocal(out=o, in_=i_)), [pkD], [rdk], cost=2.2)
                    S.tt(YG[pr, c2, qsl], psO[pr, :], rd[pr, :], ALU.mult, [pkO, rdk], [K("YG", c2, c)])

            n_ = len(its)
            for r in range(-1, n_):
                if r + 1 < n_:
                    m_s1(its[r + 1])
                if r >= 0:
                    m_s2(its[r])
            dump(f"yd{l}", YG[:], [K("YG", c, t) for c in range(2) for t in range(NTC)], [128, 2, S_])
            st.close()
            S.barrier()
            nrot[0] = 8
            group_out(l, 3, YG, sty)
        S.barrier()
        dump(f"x1_{l}", xT[:], ALLX, [128, KC, S_])
        if stop_after == f"D{l}":
            done = True
            break

        with ExitStack() as st:
            memT = sb("memT", [128, KC, 256], stack=st)
            MTb = sb("MTb", [128, KC, 256], BF16, stack=st)
            with ExitStack() as st2:
                rmsnorm_fm(xT, "xT", lambda kc: col(l, "xat_g", kc), hT, "hT", KC, S_, st2, tag="x")
                load_fm(memT, mem_d, 2, "memT", st2)
                rmsnorm_fm(memT, "memT", lambda kc: col(l, "mem_g", kc), MTb, "MTb", KC, 256, st2, tag="m")
            S.barrier()
            KTb = sb("KTb", [128, KC, 256], BF16, stack=st)
            VX = sb("VX", [128, 2, D_], BF16, stack=st)
            QX = sb("QX", [128, KC, S_], BF16, stack=st)
            MTk = [K("MTb", kc, 0) for kc in range(KC)]
            for half in range(2):
                wv, wk, _ = load_w(wkv_d[l, :, half * 512:(half + 1) * 512], 8, 512)
                for o4 in range(4):
                    oc = half * 4 + o4
                    ps, pk = ps_rot()
                    for kc in range(KC):
                        S.mm(ps[:, 0:256], wv[:, kc, o4 * 128:(o4 + 1) * 128], MTb[:, kc, :], kc == 0, kc == KC - 1,
                             wk + [K("MTb", kc, 0)], [pk])
                    S.cp(KTb[:, oc, :], ps[:, 0:256], [pk], [K("KTb", oc)])
            for half in range(2):
                wv, wk, _ = load_w(wkv_d[l, :, 1024 + half * 512:1024 + (half + 1) * 512], 8, 512)
                for mt in range(2):
                    ps, pk = ps_rot()
                    for kc in range(KC):
                        S.mm(ps[:, :], MTb[:, kc, mt * 128:(mt + 1) * 128], wv[:, kc, :], kc == 0, kc == KC - 1,
                             wk + [K("MTb", kc, 0)], [pk])
                    S.cp(VX[:, mt, half * 512:(half + 1) * 512], ps[:, :], [pk], [K("VX", mt, half)], eng="act")
            for half in range(2):
                wv, wk, _ = load_w(wq_d[l, :, half * 512:(half + 1) * 512], 8, 512)
                for o4 in range(4):
                    oc = half * 4 + o4
                    for tc in range(NTC):
                        ps, pk = ps_rot()
                        proj_fm(wv, wk, o4 * 128, tc, ps, pk)
                        S.act(QX[:, oc, tc * TW:(tc + 1) * TW], ps[:, :], AF.Copy, [pk], [K("QX", oc, tc)],
                              scale=1.0 / 16.0)
            dump(f"X_MTb{l}", MTb[:], MTk, [128, KC, 256])
            dump(f"X_KTb{l}", KTb[:], [K("KTb", oc) for oc in range(KC)], [128, KC, 256])
            dump(f"X_QX{l}", QX[:], [K("QX", oc, tc) for oc in range(KC) for tc in range(NTC)], [128, KC, S_])
            dump(f"X_VX{l}", VX[:], [K("VX", mt, hf) for mt in range(2) for hf in range(2)], [128, 2, D_])
            PT = [sb(f"PTx{j}", [128, 512], BF16, stack=st) for j in range(4)]
            RDx = [sb(f"RDx{j}", [128, 512], stack=st) for j in range(2)]
            it = 0
            for hd in range(4):
                for tc in range(NTC):
                    sl = slice(tc * TW, (tc + 1) * TW)
                    pts = []
                    for mt in range(2):
                        ps, pk = ps_rot()
                        for j in range(2):
                            S.mm(ps[:, :], KTb[:, 2 * hd + j, mt * 128:(mt + 1) * 128], QX[:, 2 * hd + j, sl],
                                 j == 0, j == 1, [K("KTb", 2 * hd + j), K("QX", 2 * hd + j, tc)], [pk])
                        pt, ptk = PT[it % 4], K("PTx", it % 4)
                        S.act(pt[:], ps[:, :], AF.Exp, [pk], [ptk])
                        pts.append((pt, ptk))
                        it += 1
                    psD, pkD = ps_rot()
                    for mt in range(2):
                        S.mm(psD[:, :], ones_b, pts[mt][0][:], mt == 0, mt == 1, [pts[mt][1], K("c128b")], [pkD])
                    rd, rdk = RDx[(hd * NTC + tc) % 2], K("RDx", (hd * NTC + tc) % 2)
                    S.add("dve", (lambda e, o=rd[:], i_=psD[:, :]: e.reciprocal(out=o, in_=i_)), [pkD], [rdk], cost=2.2)
                    for j in range(2):
                        oc = 2 * hd + j
                        ps, pk = ps_rot()
                        for mt in range(2):
                            S.mm(ps[:, :], VX[:, mt, oc * 128:(oc + 1) * 128], pts[mt][0][:], mt == 0, mt == 1,
                                 [K("VX", mt, oc // 4), pts[mt][1]], [pk])
                        S.tt(hT[:, oc, sl], ps[:, :], rd[:], ALU.mult, [pk, rdk], [K("hT", oc, tc)])
            dump(f"X_OX{l}", hT[:], ALLH, [128, KC, S_])
            for half in range(2):
                wv, wk, _ = load_w(wo_d[l, :, half * 512:(half + 1) * 512], 8, 512)
                for o4 in range(4):
                    oc = half * 4 + o4
                    for tc in range(NTC):
                        ps, pk = ps_rot()
                        proj_fm(wv, wk, o4 * 128, tc, ps, pk)
                        S.tt(xT[:, oc, tc * TW:(tc + 1) * TW], xT[:, oc, tc * TW:(tc + 1) * TW], ps[:, :], ALU.add,
                             [K("xT", oc, tc), pk], [K("xT", oc, tc)])
        S.barrier()
        dump(f"x2_{l}", xT[:], ALLX, [128, KC, S_])
        if stop_after == f"X{l}":
            done = True
            break

        with ExitStack() as st:
            ENp = sb("ENp", [128, 16, 128], BF16, stack=st)
            S.add("dve", lambda e: e.memset(ENp[:], 0.0), [], [K("ENpz")])
            S.dma(ENp[0:16, :, :], en_d[:, :].rearrange("p (a b) -> p a b", a=16), [K("ENpz")], [K("ENf")], q="pool",
                  arena=True)
            CTh = sb("CTh", [128, S_], BF16, stack=st)
            CTl = sb("CTl", [128, S_], BF16, stack=st)
            S.add("dve", lambda e: e.memset(CTh[:], 0.0), [], [K("CThz")])
            S.add("dve", lambda e: e.memset(CTl[:], 0.0), [], [K("CTlz")])
            st_rt = ExitStack()
            WR = sb("WR", [128, KC, 20], stack=st_rt)
            S.dma(WR[:], wrt_d[l].rearrange("(kc p) n -> p kc n", p=128), [], [K("WR")])
            LG = sb("LG", [128, NT, 20], stack=st_rt)
            st_hf = ExitStack()
            HF = sb("HF", [128, KC, TW], stack=st_hf)

            def router_hook(tc, r):
                sl = slice(tc * TW, (tc + 1) * TW)
                for kc in range(KC):
                    S.stt(HF[:, kc, :], xT[:, kc, sl], col(l, "ffn_g", kc), r[:], ALU.mult, ALU.mult,
                          [K("xT", kc, tc), K("rsf", tc % 2), K("colp")], [K("HF", kc)])
                ps, pk = ps_rot()
                for q in range(4):
                    for kc in range(KC):
                        S.mm(ps[:, q * 20:(q + 1) * 20], HF[:, kc, q * 128:(q + 1) * 128], WR[:, kc, :], kc == 0,
                             kc == KC - 1, [K("HF", kc), K("WR")], [pk])
                S.cp(LG[:, tc * 4:tc * 4 + 4, :], ps[:, 0:80].rearrange("p (a b) -> p a b", a=4), [pk],
                     [K("LG", tc)])

            with ExitStack() as st2:
                rmsnorm_fm(xT, "xT", lambda kc: col(l, "ffn_g", kc), hT, "hT", KC, S_, st2, hook=router_hook, tag="f")
            st_hf.close()
            S.barrier()
            COMB = sb("COMB", [128, NT, 16], stack=st_rt)
            CT = sb("CT", [16, S_], stack=st_rt)
            sm = {nm: sb("rt_" + nm, [128, NT, w_] if w_ > 1 else [128, NT], stack=st_rt) for nm, w_ in
                  [("lg", 4), ("m", 1), ("eg", 4), ("sg", 1), ("oh", 4), ("le", 16), ("sel", 4), ("tmp", 4),
                   ("a", 1), ("eq", 4), ("sel2", 4), ("b", 1), ("t2", 4), ("ex", 4), ("dd", 1), ("wg", 4)]}

            def v(nm):
                return sm[nm][:]

            def kk(nm):
                return [K("rt", nm)]

            def B3(ap2, w_=4):
                return ap2.unsqueeze(2).to_broadcast([128, NT, w_])

            LGk = [K("LG", t) for t in range(NTC)]
            bgB = row(l, "bg").unsqueeze(1).to_broadcast([128, NT, 4])
            beB = row(l, "be").unsqueeze(1).to_broadcast([128, NT, 16])
            S.tt(v("lg"), LG[:, :, 0:4], bgB, ALU.add, LGk + [K("rowp")], kk("lg"))
            S.add("dve", lambda e: e.reduce_max(out=v("m"), in_=v("lg"), axis=AX.X), kk("lg"), kk("m"))
            S.tt(v("lg"), v("lg"), B3(v("m")), ALU.subtract, kk("lg") + kk("m"), kk("lg"))
            S.act(v("eg"), v("lg"), AF.Exp, kk("lg"), kk("eg"))
            S.add("dve", lambda e: e.reduce_sum(out=v("sg"), in_=v("eg"), axis=AX.X), kk("eg"), kk("sg"))
            S.add("dve", lambda e: e.reciprocal(out=v("sg"), in_=v("sg")), kk("sg"), kk("sg"))
            S.ts(v("oh"), v("lg"), 0.0, None, ALU.is_ge, None, kk("lg"), kk("oh"))
            S.tt(v("le"), LG[:, :, 4:20], beB, ALU.add, LGk + [K("rowp")], kk("le"))
            for g in range(4):
                ohg = sm["oh"][:, :, g:g + 1].to_broadcast([128, NT, 4])
                if g == 0:
                    S.tt(v("sel"), sm["le"][:, :, 0:4], ohg, ALU.mult, kk("le") + kk("oh"), kk("sel"))
                else:
                    S.tt(v("tmp"), sm["le"][:, :, 4 * g:4 * g + 4], ohg, ALU.mult, kk("le") + kk("oh"), kk("tmp"))
                    S.tt(v("sel"), v("sel"), v("tmp"), ALU.add, kk("sel") + kk("tmp"), kk("sel"))
            S.add("dve", lambda e: e.reduce_max(out=v("a"), in_=v("sel"), axis=AX.X), kk("sel"), kk("a"))
            S.tt(v("sel"), v("sel"), B3(v("a")), ALU.subtract, kk("sel") + kk("a"), kk("sel"))
            S.ts(v("eq"), v("sel"), 0.0, None, ALU.is_ge, None, kk("sel"), kk("eq"))
            S.stt(v("sel2"), v("eq"), -1e30, v("sel"), ALU.mult, ALU.add, kk("eq") + kk("sel"), kk("sel2"))
            S.add("dve", lambda e: e.reduce_max(out=v("b"), in_=v("sel2"), axis=AX.X), kk("sel2"), kk("b"))
            S.tt(v("t2"), v("sel"), B3(v("b")), ALU.is_ge, kk("sel") + kk("b"), kk("t2"))
            S.act(v("ex"), v("sel"), AF.Exp, kk("sel"), kk("ex"))
            S.act(v("dd"), v("b"), AF.Exp, kk("b"), kk("dd"))
            S.ts(v("dd"), v("dd"), 1.0, None, ALU.add, None, kk("dd"), kk("dd"))
            S.add("dve", lambda e: e.reciprocal(out=v("dd"), in_=v("dd")), kk("dd"), kk("dd"))
            S.tt(v("wg"), v("ex"), v("t2"), ALU.mult, kk("ex") + kk("t2"), kk("wg"))
            S.tt(v("wg"), v("wg"), B3(v("dd")), ALU.mult, kk("wg") + kk("dd"), kk("wg"))
            S.tt(v("wg"), v("wg"), B3(v("sg")), ALU.mult, kk("wg") + kk("sg"), kk("wg"))
            for g in range(4):
                ohg = sm["oh"][:, :, g:g + 1].to_broadcast([128, NT, 4])
                S.tt(COMB[:, :, 4 * g:4 * g + 4], v("wg"), ohg, ALU.mult, kk("wg") + kk("oh"),
                     [K("COMB", i, g) for i in range(NT)])
            for q4 in range(4):
                pst, pkt = ps_rot()
                for q in range(4):
                    i = q4 * 4 + q
                    S.tr(pst[0:16, q * 128:(q + 1) * 128], COMB[:, i, :], ident,
                         [K("COMB", i, g) for g in range(4)] + [K("c128")], [pkt])
                S.cp(CT[0:16, q4 * 512:(q4 + 1) * 512], pst[0:16, :], [pkt], [K("CT", q4)])
                S.cp(CTh[0:16, q4 * 512:(q4 + 1) * 512], CT[0:16, q4 * 512:(q4 + 1) * 512], [K("CT", q4), K("CThz")],
                     [K("CTh", q4)])
                S.tt(CT[0:16, q4 * 512:(q4 + 1) * 512], CT[0:16, q4 * 512:(q4 + 1) * 512],
                     CTh[0:16, q4 * 512:(q4 + 1) * 512], ALU.subtract, [K("CT", q4), K("CTh", q4)], [K("CT", q4)])
                S.cp(CTl[0:16, q4 * 512:(q4 + 1) * 512], CT[0:16, q4 * 512:(q4 + 1) * 512], [K("CT", q4), K("CTlz")],
                     [K("CTl", q4)])
            st_rt.close()
            S.barrier()
            HID = sb("HID", [128, 8, S_], BF16, stack=st)
            W2S = sb("W2S", [128, 8, D_], BF16, stack=st)
            CB = [sb(f"CB{j}", [128, 512], stack=st) for j in range(2)]
            SL = [sb(f"SL{j}", [128, 512], stack=st) for j in range(2)]
            n = 0
            for g in range(4):
                for el in range(4):
                    ex = 4 * g + el
                    wv, wk1_, slot = load_w(w1_d[l, ex], 8, 256, c0=0, tot=512)
                    _, wk3_, _ = load_w(w3_d[l, ex], 8, 256, slot=slot, c0=256, tot=512)
                    wk = wk1_ + wk3_
                    for tc in range(NTC):
                        sl = slice(tc * TW, (tc + 1) * TW)
                        psC, pkC = ps_rot()
                        S.mm(psC[:, :], ENp[:, ex, :], CTh[:, sl], True, False, [K("ENf"), K("CTh", tc), K("CThz")], [pkC])
                        S.mm(psC[:, :], ENp[:, ex, :], CTl[:, sl], False, True, [K("ENf"), K("CTl", tc), K("CTlz")], [pkC])
                        cb, cbk = CB[n % 2], K("CB", n % 2)
                        S.cp(cb[:], psC[:, :], [pkC], [cbk], eng="act")
                        for fc in range(2):
                            ps1, pk1 = ps_rot()
                            proj_fm(wv, wk, fc * 128, tc, ps1, pk1)
                            ps3, pk3 = ps_rot()
                            proj_fm(wv, wk, 256 + fc * 128, tc, ps3, pk3)
                            s_, sk = SL[(2 * n + fc) % 2], K("SL", (2 * n + fc) % 2)
                            S.act(s_[:], ps1[:, :], AF.Silu, [pk1], [sk])
                            S.tt(s_[:], s_[:], ps3[:, :], ALU.mult, [sk, pk3], [sk])
                            S.tt(HID[:, el * 2 + fc, sl], s_[:], cb[:], ALU.mult, [sk, cbk], [K("HID", el * 2 + fc, tc)])
                        n += 1
                    S.dma(W2S[:, 2 * el:2 * el + 2, :], w2_d[l, ex].rearrange("(fc p) n -> p fc n", p=128), [],
                          [K("W2S", el)], q="pool", arena=True)
                for oc in range(KC):
                    for tc in range(NTC):
                        ps, pk = ps_rot()
                        for j in range(8):
                            S.mm(ps[:, :], W2S[:, j, oc * 128:(oc + 1) * 128], HID[:, j, tc * TW:(tc + 1) * TW],
                                 j == 0, j == 7, [K("W2S", j // 2), K("HID", j, tc)], [pk])
                        S.tt(xT[:, oc, tc * TW:(tc + 1) * TW], xT[:, oc, tc * TW:(tc + 1) * TW], ps[:, :], ALU.add,
                             [K("xT", oc, tc), pk], [K("xT", oc, tc)])
        S.barrier()
        dump(f"x3_{l}", xT[:], ALLX, [128, KC, S_])
        if stop_after == f"M{l}":
            done = True
            break

    if stop_after is None:
        with ExitStack() as st:
            HF = sb("HFo", [128, KC, TW], stack=st)
            OB = [sb(f"OB{j}", [128, D_], stack=st) for j in range(2)]
            nob = [0]

            def final_hook(tc, r):
                sl = slice(tc * TW, (tc + 1) * TW)
                for kc in range(KC):
                    S.stt(HF[:, kc, :], xT[:, kc, sl], col(0, "fin_g", kc), r[:], ALU.mult, ALU.mult,
                          [K("xT", kc, tc), K("rsz", tc % 2), K("colp")], [K("HFo", kc)])
                for q in range(4):
                    ob, obk = OB[nob[0] % 2], K("OB", nob[0] % 2)
                    nob[0] += 1
                    for half in range(2):
                        ps, pk = ps_rot()
                        for j in range(4):
                            S.tr(ps[:, j * 128:(j + 1) * 128], HF[:, half * 4 + j, q * 128:(q + 1) * 128], ident,
                                 [K("HFo", half * 4 + j), K("c128")], [pk])
                        S.cp(ob[:, half * 512:(half + 1) * 512], ps[:, :], [pk], [obk],
                             eng=("act" if half else "dve"))
                    r0 = tc * TW + q * 128
                    i = S.dma(out_d[r0:r0 + 128, :], ob[:], [obk], [K("out", r0)])
                    S.final.append(i)

            rmsnorm_fm(xT, "xT", lambda kc: col(0, "fin_g", kc), None, None, KC, S_, st, hook=final_hook, tag="z")

    S.finalize(es)
    es.close()
    return nc, dump_d


def make_consts():
    c128 = np.zeros((128, 8, 128), np.float32)
    j = np.arange(128)[:, None]
    s = np.arange(128)[None, :]
    c128[:, 0] = np.eye(128)
    c128[:, 1] = 1.0
    c128[:, 2] = (j <= s)
    c128[:, 3] = -1.0 * (j >= s)
    c128[:, 4] = -1.0
    partner = np.where((np.arange(128) % 64) < 32, np.arange(128) + 32, np.arange(128) - 32)
    perm = np.zeros((128, 128), np.float32)
    perm[partner, np.arange(128)] = 1.0
    c128[:, 5] = perm
    c128[:, 6] = NEG * (s < j)
    cm = np.zeros((128, 12, 512), np.float32)
    p = np.arange(128)[:, None]
    f = np.arange(512)[None, :]
    for d in range(4):
        valid = (f - p) > d * 128
        cm[:, d] = valid
        cm[:, 4 + d] = NEG * (~valid)
        cm[:, 8 + d] = NEG * ((d * 128 + p) > f)
    half = 32
    inv = (np.float32(10000.0) ** (-(np.arange(half, dtype=np.float32)) / np.float32(half))).astype(np.float32)
    ang = np.arange(S_, dtype=np.float32)[None, :] * inv[:, None]
    cos = np.cos(ang).astype(np.float32)
    sin = np.sin(ang).astype(np.float32)
    rope = np.zeros((128, 2, S_), np.float32)
    for pp in range(128):
        d = pp % 64
        rope[pp, 0] = cos[d % 32]
        rope[pp, 1] = -sin[d % 32] if d < 32 else sin[d % 32]
    en = np.zeros((16, 16, 128), np.float32)
    for n in range(16):
        en[n, n, :] = 1.0
    gc = np.zeros((128, 3, 16, 8), np.float32)
    for i in range(16):
        own = i // 2
        for n in range(8):
            gc[:, 0, i, n] = 0.0 if n < own else -1e30
            gc[:, 1, i, n] = 1.0 if n < own else 0.0
            gc[:, 2, i, n] = 1.0 if n == own else 0.0
    blkoh = np.zeros((8, S_), np.float32)
    for n in range(8):
        blkoh[n, n * 256:(n + 1) * 256] = 1.0
    return dict(c128=c128.reshape(128, -1), cmask=cm.reshape(128, -1), rope=rope.reshape(128, -1),
                en=en.reshape(16, -1), gatec=gc.reshape(128, -1), blkoh=blkoh)


def colvec(v):
    v = np.asarray(v, np.float32)
    return np.ascontiguousarray(v.reshape(-1, 128).T)


def pack_params(inp):
    colp = np.zeros((128, L_, NCOL), np.float32)
    rowp = np.zeros((128, L_, NROW), np.float32)

    def put(l, name, arr):
        c0, w = COLS[name]
        assert arr.shape == (128, w), (name, arr.shape)
        colp[:, l, c0:c0 + w] = arr

    for l in range(L_):
        put(l, "mix_g", colvec(inp["mix_norm_g"][l]))
        put(l, "xat_g", colvec(inp["xattn_norm_g"][l]))
        put(l, "mem_g", colvec(inp["mem_norm_g"][l]))
        put(l, "ffn_g", colvec(inp["ffn_norm_g"][l]))
        put(l, "grp_g", colvec(inp["group_norm_g"][l]))
        cw = inp["lru_conv_w"][l]
        put(l, "lru_cw", np.concatenate([colvec(cw[k]) for k in range(4)], axis=1).reshape(128, 4, 2)
            .transpose(0, 2, 1).reshape(128, 8))
        put(l, "lru_cb", colvec(inp["lru_conv_b"][l]))
        put(l, "lru_br", colvec(inp["lru_br"][l]))
        put(l, "lru_bi", colvec(inp["lru_bi"][l]))
        put(l, "lru_lam", colvec(inp["lru_lambda"][l]))
        sw = inp["ssm_conv_w"][l]
        put(l, "ssm_cw", np.concatenate([colvec(sw[k]) for k in range(4)], axis=1).reshape(128, 4, 6)
            .transpose(0, 2, 1).reshape(128, 24))
        put(l, "ssm_cb", colvec(inp["ssm_conv_b"][l]))
        put(l, "ssm_d", colvec(np.repeat(inp["ssm_d"][l], 64)))
        put(l, "fin_g", colvec(inp["final_norm_g"]))
        r0, w = ROWS["dt_bias"]
        rowp[:, l, r0:r0 + w] = np.tile(inp["ssm_dt_bias"][l], 16)[None, :]
        r0, w = ROWS["a_log"]
        rowp[:, l, r0:r0 + w] = np.tile(inp["ssm_a_log"][l], 16)[None, :]
        r0, w = ROWS["bg"]
        rowp[:, l, r0:r0 + w] = inp["router_group_b"][l][None, :]
        r0, w = ROWS["be"]
        rowp[:, l, r0:r0 + w] = inp["router_expert_b"][l][None, :]
    wbd = np.zeros((L_, 2, 2, 128, 128), np.float32)
    for l in range(L_):
        for gi, nm in enumerate(("lru_wr", "lru_wi")):
            for c in range(2):
                for hh in range(2):
                    wbd[l, gi, c, hh * 64:(hh + 1) * 64, hh * 64:(hh + 1) * 64] = inp[nm][l, 2 * c + hh]
    router_w = np.concatenate([inp["router_group_w"], inp["router_expert_w"]], axis=2)
    return dict(colp=colp.reshape(128, -1), rowp=rowp.reshape(128, -1), lru_wbd=wbd,
                router_w=np.ascontiguousarray(router_w))


SHARED = ["w_in", "w_out", "xattn_wq", "xattn_wkv", "xattn_wo", "expert_w1", "expert_w3", "expert_w2"]


def make_in_maps(inputs, cores):
    inp = {k: np.asarray(v) for k, v in inputs.items()}
    consts = make_consts()
    packed = pack_params(inp)
    shared = {k: np.ascontiguousarray(inp[k], dtype=np.float32) for k in SHARED}
    maps = []
    for b in cores:
        m = dict(shared)
        m.update(consts)
        m.update(packed)
        m["x"] = np.ascontiguousarray(inp["x"][b])
        m["mem"] = np.ascontiguousarray(inp["mem"][b])
        maps.append(m)
    return maps


_CACHE = {}


def kernel(**inputs):
    if "nc" not in _CACHE:
        _CACHE["nc"] = build_program()[0]
    nc = _CACHE["nc"]
    maps = make_in_maps(inputs, list(range(8)))
    res = run_bass_kernel_spmd(nc, maps, core_ids=list(range(8)))
    return np.stack([r["out"] for r in res.results], axis=0).astype(np.float32)
```

```python
import numpy as np
from contextlib import ExitStack
import concourse.bass as bass
import concourse.mybir as mybir
from concourse.bass_utils import run_bass_kernel_spmd

F32 = mybir.dt.float32
BF16 = mybir.dt.bfloat16
F32R = mybir.dt.float32r
AF = mybir.ActivationFunctionType
ALU = mybir.AluOpType
AX = mybir.AxisListType

S_ = 2048
D_ = 1024
L_ = 2
KC = 8
NTC = 4
TW = 512
NT = 16
NEG = -30000.0
EPS = 1e-6

COLS = {}
_c = 0
for _n, _w in [("mix_g", 8), ("xat_g", 8), ("mem_g", 8), ("ffn_g", 8), ("grp_g", 8),
               ("lru_cw", 8), ("lru_cb", 2), ("lru_br", 2), ("lru_bi", 2), ("lru_lam", 2),
               ("ssm_cw", 24), ("ssm_cb", 6), ("ssm_d", 2), ("fin_g", 8)]:
    COLS[_n] = (_c, _w)
    _c += _w
NCOL = _c
ROWS = {}
_c = 0
for _n, _w in [("dt_bias", 64), ("a_log", 64), ("bg", 4), ("be", 16)]:
    ROWS[_n] = (_c, _w)
    _c += _w
NROW = _c


def _free(ap):
    n = 1
    for d in ap.shape[1:]:
        n *= int(d)
    return n


DEBUG_LINES = False


class Sched:
    ENG = ("pe", "act", "dve", "pool", "sp")
    GATED = ("pe", "act", "dve", "sp")

    def __init__(self, nc):
        self.nc = nc
        self.ops = []
        self.lastw = {}
        self.rd = {}
        self.by_eng = {e: [] for e in self.ENG}
        self.final = []
        self.psn = 0
        self.seg = 0
        self.act_inorder = set()

    def add(self, eng, fn, reads=(), writes=(), dma=False, cost=0.3, arena=False):
        i = len(self.ops)
        reads = list(reads)
        writes = list(writes)
        deps = {}
        for r in reads:
            w = self.lastw.get(r)
            if w is not None:
                deps.setdefault(w, set()).add("raw")
        for w_ in writes:
            w = self.lastw.get(w_)
            if w is not None:
                deps.setdefault(w, set()).add("waw")
            for r in self.rd.get(w_, ()):
                deps.setdefault(r, set()).add("war")
        ws = set(writes)
        for r in reads:
            if r not in ws:
                self.rd.setdefault(r, []).append(i)
        for w_ in writes:
            self.lastw[w_] = i
            self.rd[w_] = []
        deps.pop(i, None)
        fdeps = set()
        for d, kinds in deps.items():
            od = self.ops[d]
            if od["eng"] == eng and not od["dma"] and not dma:
                if eng == "pe":
                    continue
                if "raw" not in kinds:
                    continue
            fdeps.add(d)
        ln_ = 0
        if DEBUG_LINES:
            import sys as _sys
            f_ = _sys._getframe(1)
            while f_ is not None and f_.f_code.co_name != "build_program":
                f_ = f_.f_back
            ln_ = f_.f_lineno if f_ is not None else 0
        self.ops.append(dict(eng=eng, fn=fn, deps=fdeps, order=set(deps.keys()), dma=dma, signal=dma, token=None,
                             seg=self.seg, cost=cost, arena=arena, line=ln_))
        self.by_eng[eng].append(i)
        return i

    def barrier(self):
        self.seg += 1

    def schedule(self, W=24):
        ops = self.ops
        n = len(ops)
        succ = [[] for _ in range(n)]
        nun = [0] * n
        for i, op in enumerate(ops):
            nun[i] = len(op["order"])
            for d in op["order"]:
                succ[d].append(i)
        ready = [0.0] * n
        fin = [0.0] * n
        done = [False] * n
        head = {e: 0 for e in self.ENG}
        free = {e: 0.0 for e in self.ENG}
        lists = {e: list(self.by_eng[e]) for e in self.ENG}
        new_order = {e: [] for e in self.ENG}
        nseg = self.seg + 1
        rem = [0] * (nseg + 1)
        for i, op in enumerate(ops):
            if op["eng"] in self.GATED:
                rem[op["seg"]] += 1
        import os
        WDEF = {"pe": W, "act": W, "dve": W}
        WE = {e_: int(os.environ.get("KSCHED_W_" + e_.upper(), WDEF[e_])) for e_ in ("pe", "act", "dve")}
        segbusy = {}
        segend = {}
        cur = 0
        seg_t0 = 0.0
        tmax = 0.0
        nsched = 0
        while nsched < n:
            while cur < nseg and rem[cur] == 0:
                cur += 1
                seg_t0 = tmax
            best = None
            for e in self.ENG:
                lst = lists[e]
                h = head[e]
                while h < len(lst) and done[lst[h]]:
                    h += 1
                head[e] = h
                w = WE.get(e, 1)
                if e == "act" and cur in self.act_inorder:
                    w = 1
                seen = 0
                j = h
                while j < len(lst) and seen < w:
                    i = lst[j]
                    j += 1
                    if done[i]:
                        continue
                    op = ops[i]
                    if e != "pool":
                        if op["seg"] != cur:
                            break
                    elif op["arena"] and op["seg"] > cur:
                        break
                    seen += 1
                    if nun[i] == 0:
                        est = max(free[e], ready[i])
                        if e != "pool" or op["arena"]:
                            est = max(est, seg_t0)
                        key = (est, i)
                        if best is None or key < best[0]:
                            best = (key, e, i)
            if best is None:
                raise RuntimeError("scheduler stuck at seg %d" % cur)
            (est, _), e, i = best
            op = ops[i]
            if op["dma"]:
                free[e] = est + 0.06
                f = est + op["cost"]
            else:
                f = est + op["cost"]
                free[e] = f
            fin[i] = f
            tmax = max(tmax, f)
            if not op["dma"]:
                segbusy[(op["seg"], e)] = segbusy.get((op["seg"], e), 0.0) + op["cost"]
            segend[op["seg"]] = max(segend.get(op["seg"], 0.0), f)
            done[i] = True
            nsched += 1
            new_order[e].append(i)
            if e in self.GATED:
                rem[op["seg"]] -= 1
            for s_ in succ[i]:
                nun[s_] -= 1
                lat = 0.0 if (ops[s_]["eng"] == e and not op["dma"]) else 0.1
                if f + lat > ready[s_]:
                    ready[s_] = f + lat
        self.by_eng = new_order
        self.sim_time = tmax
        import os
        if os.environ.get("KSCHED_DEBUG"):
            print("sched: ops", n, "sim_time_us", round(tmax, 1), flush=True)
            prev = 0.0
            for sg in sorted(segend):
                print("  seg", sg, "dur", round(segend[sg] - prev, 1), {e_: round(segbusy.get((sg, e_), 0.0), 1)
                                                                         for e_ in ("pe", "act", "dve")})
                prev = segend[sg]
        pos = {}
        for e in self.ENG:
            for k, i in enumerate(new_order[e]):
                pos[i] = k
        bar = [set() for _ in range(nseg + 1)]
        for s_ in range(1, nseg + 1):
            for e in ("pe", "act", "dve"):
                last = None
                for i in new_order[e]:
                    if ops[i]["seg"] < s_:
                        last = i
                    else:
                        break
                if last is not None:
                    bar[s_].add(last)
            for i in new_order["sp"]:
                if ops[i]["seg"] == s_ - 1 and ops[i]["dma"]:
                    bar[s_].add(i)
        for e in self.GATED:
            lastseg = 0
            for i in new_order[e]:
                sg = ops[i]["seg"]
                if sg > lastseg:
                    for t in range(lastseg + 1, sg + 1):
                        ops[i]["deps"] |= bar[t]
                    lastseg = sg
        for i in new_order["pool"]:
            if ops[i]["arena"]:
                ops[i]["deps"] |= bar[ops[i]["seg"]]
        for i, op in enumerate(ops):
            keep = set()
            bestp = {}
            for d in op["deps"]:
                if d == i:
                    continue
                od = ops[d]
                if od["dma"]:
                    keep.add(d)
                else:
                    pe_ = od["eng"]
                    if pe_ not in bestp or pos[d] > pos[bestp[pe_]]:
                        bestp[pe_] = d
            keep |= set(bestp.values())
            op["deps"] = keep

    def mm(self, out, lhsT, rhs, start, stop, reads, writes):
        nfree = _free(rhs)
        c = max(nfree, 64) / 2400.0 * (4.0 if rhs.dtype == F32 else 1.0) + 0.004
        return self.add("pe", lambda e: e.matmul(out, lhsT, rhs, start=start, stop=stop), reads, writes, cost=c)

    def tr(self, out, in_, ident, reads, writes):
        return self.add("pe", lambda e: e.transpose(out, in_, ident), reads, writes, cost=0.25)

    def act(self, out, in_, func, reads, writes, bias=None, scale=None, accum=None):
        kw = {}
        if bias is not None:
            kw["bias"] = bias
        if scale is not None:
            kw["scale"] = scale
        if accum is not None:
            kw["accum_out"] = accum
        c = 0.22 + _free(out) / 1400.0
        return self.add("act", lambda e: e.activation(out=out, in_=in_, func=func, **kw), reads, writes, cost=c)

    def _vc(self, out):
        return 0.08 + _free(out) / 960.0

    def tt(self, out, in0, in1, op, reads, writes, eng="dve"):
        return self.add(eng, lambda e: e.tensor_tensor(out=out, in0=in0, in1=in1, op=op), reads, writes,
                        cost=self._vc(out))

    def ts(self, out, in0, s1, s2, op0, op1, reads, writes, eng="dve"):
        if s2 is None:
            return self.add(eng, lambda e: e.tensor_scalar(out, in0, s1, None, op0), reads, writes, cost=self._vc(out))
        return self.add(eng, lambda e: e.tensor_scalar(out, in0, s1, s2, op0, op1), reads, writes, cost=self._vc(out))

    def stt(self, out, in0, scalar, in1, op0, op1, reads, writes, eng="dve"):
        return self.add(eng, lambda e: e.scalar_tensor_tensor(out=out, in0=in0, scalar=scalar, in1=in1,
                                                             op0=op0, op1=op1), reads, writes, cost=self._vc(out))

    def cp(self, out, in_, reads, writes, eng="dve"):
        if eng == "act":
            return self.add("act", lambda e: e.activation(out=out, in_=in_, func=AF.Copy), reads, writes,
                            cost=0.22 + _free(out) / 1400.0)
        return self.add(eng, lambda e: e.tensor_copy(out=out, in_=in_), reads, writes, cost=self._vc(out))

    def recip(self, out, in_, reads, writes):
        return self.add("dve", lambda e: e.reciprocal(out=out, in_=in_), reads, writes, cost=0.1 + _free(out) / 240.0)

    def dma(self, out, in_, reads, writes, q="sp", arena=False):
        nbytes = 128 * _free(out) * 4
        return self.add(q, lambda e: e.dma_start(out=out, in_=in_), reads, writes, dma=True,
                        cost=2.0 + nbytes / 150e3, arena=arena)

    def finalize(self, es):
        nc = self.nc
        self.schedule()
        for op in self.ops:
            for d in op["deps"]:
                self.ops[d]["signal"] = True
        for d in self.final:
            self.ops[d]["signal"] = True
        csem = {e: es.enter_context(nc.semaphore("c_" + e)) for e in ("pe", "act", "dve", "pool")}
        NP = 16
        qsem = {q: [es.enter_context(nc.semaphore(f"q_{q}{k}")) for k in range(NP)] for q in ("sp", "pool")}
        cnt = {e: 0 for e in csem}
        qn = {q: 0 for q in qsem}
        quse = {q: [0] * NP for q in qsem}
        qprev = {q: [None] * NP for q in qsem}
        for i, op in enumerate(self.ops):
            if op["dma"]:
                q = op["eng"]
                k = qn[q] % NP
                qn[q] += 1
                quse[q][k] += 1
                op["token"] = (qsem[q][k], 16 * quse[q][k])
                if qprev[q][k] is not None:
                    op["deps"].add(qprev[q][k])
                qprev[q][k] = i
        for e in csem:
            for i in self.by_eng[e]:
                op = self.ops[i]
                if (not op["dma"]) and op["signal"]:
                    cnt[e] += 1
                    op["token"] = (csem[e], cnt[e])
        ops = self.ops
        final = list(self.final)

        def emit(engname, e):
            waited = {}
            for i in self.by_eng[engname]:
                op = ops[i]
                for d in sorted(op["deps"]):
                    sem, val = ops[d]["token"]
                    if waited.get(sem, 0) < val:
                        e.wait_ge(sem, val)
                        waited[sem] = val
                ins = op["fn"](e)
                if op["signal"]:
                    ins.then_inc(op["token"][0], 16 if op["dma"] else 1)
            if engname == "sp":
                for d in final:
                    sem, val = ops[d]["token"]
                    if waited.get(sem, 0) < val:
                        e.wait_ge(sem, val)
                        waited[sem] = val

        with nc.Block() as block:
            @block.tensor
            def _(e):
                emit("pe", e)

            @block.scalar
            def _(e):
                emit("act", e)

            @block.vector
            def _(e):
                emit("dve", e)

            @block.gpsimd
            def _(e):
                emit("pool", e)

            @block.sync
            def _(e):
                emit("sp", e)


def K(name, *idx):
    return (name,) + idx


def build_program(stop_after=None, dumps=(), n_layers=L_):
    nc = bass.Bass("TRN2", target_bir_lowering=False)
    S = Sched(nc)
    es = ExitStack()
    P = {}

    def din(name, shape):
        P[name] = nc.dram_tensor(name, list(shape), F32, kind="ExternalInput").ap()
        return P[name]

    x_d = din("x", [S_, D_])
    mem_d = din("mem", [256, D_])
    w_in_d = din("w_in", [L_, D_, 3076])
    w_out_d = din("w_out", [L_, D_, D_])
    wq_d = din("xattn_wq", [L_, D_, D_])
    wkv_d = din("xattn_wkv", [L_, D_, 2 * D_])
    wo_d = din("xattn_wo", [L_, D_, D_])
    w1_d = din("expert_w1", [L_, 16, D_, 256])
    w3_d = din("expert_w3", [L_, 16, D_, 256])
    w2_d = din("expert_w2", [L_, 16, 256, D_])
    wrt_d = din("router_w", [L_, D_, 20])
    lrubd_d = din("lru_wbd", [L_, 2, 2, 128, 128])
    colp_d = din("colp", [128, L_ * NCOL])
    rowp_d = din("rowp", [128, L_ * NROW])
    c128_d = din("c128", [128, 8 * 128])
    cmask_d = din("cmask", [128, 12 * 512])
    rope_d = din("rope", [128, 2 * S_])
    en_d = din("en", [16, 16 * 128])
    blkoh_d = din("blkoh", [8, S_])
    gate_c_d = din("gatec", [128, 3 * 128])
    out_d = nc.dram_tensor("out", [S_, D_], F32, kind="ExternalOutput").ap()
    dump_d = {}

    uid = [0]

    def sb(name, shape, dt=F32, stack=None):
        uid[0] += 1
        return (stack or es).enter_context(nc.sbuf_tensor(f"{name}_{uid[0]}", list(shape), dt))

    xT = sb("xT", [128, KC, S_])
    hT = sb("hT", [128, KC, S_], BF16)
    colp = sb("colp_s", [128, L_ * NCOL])
    rowp = sb("rowp_s", [128, L_ * NROW])
    c128 = sb("c128_s", [128, 8 * 128])
    c128b = sb("c128b_s", [128, 2 * 128], BF16)
    W = [sb(f"wslot{i}", [128, KC, 512], BF16) for i in range(3)]
    PS = [es.enter_context(nc.psum_tensor(f"ps{i}", [128, 512], F32)) for i in range(8)]
    wn = [0]

    ident = c128[:, 0:128]
    ones_f = c128[:, 128:256]
    tri_le = c128[:, 256:384]
    negu = c128[:, 384:512]
    negones = c128[:, 512:640]
    perm_f = c128[:, 640:768]
    ssdneg = c128[:, 768:896]
    ones_b = c128b[:, 128:256]
    ident_b = c128b[:, 0:128]

    cst = sb("cst", [128, 4])
    S.add("dve", lambda e: e.memset(cst[:, 0:1], EPS), [], [K("cst0")])
    S.add("dve", lambda e: e.memset(cst[:, 1:2], 1.0), [], [K("cst1")])
    S.add("dve", lambda e: e.memset(cst[:, 2:4], 0.0), [K("cst0"), K("cst1")], [K("cst")])
    S.dma(colp[:], colp_d[:, :], [], [K("colp")])
    S.dma(rowp[:], rowp_d[:, :], [], [K("rowp")])
    S.dma(c128[:], c128_d[:, :], [], [K("c128")])
    S.dma(c128b[:], c128_d[:, 0:256], [], [K("c128b")], q="pool")

    def col(l, name, j=0, n=1):
        c0, w = COLS[name]
        return colp[:, l * NCOL + c0 + j: l * NCOL + c0 + j + n]

    def row(l, name, j=0, n=None):
        c0, w = ROWS[name]
        n = w if n is None else n
        return rowp[:, l * NROW + c0 + j: l * NROW + c0 + j + n]

    nrot = [8]

    def ps_rot():
        k = S.psn % nrot[0]
        S.psn += 1
        return PS[k], K("ps", k)

    Wf = [w[:].rearrange("p a b -> p (a b)") for w in W]

    def load_w(src2d, nk, ncols, slot=None, c0=0, tot=None):
        tot = ncols if tot is None else tot
        if slot is None:
            k = wn[0] % 3
            wn[0] += 1
        else:
            k = slot
        v = Wf[k][:, 0:nk * tot].rearrange("p (a b) -> p a b", a=nk)
        step = 2 if nk >= 2 else 1
        allk = [K("w", k, j_, hf_) for j_ in range(4) for hf_ in range(2)]
        for h in range(0, nk, step):
            if nk == 8 and tot == 512:
                halves = [hf_ for hf_ in range(2) if c0 < (hf_ + 1) * 256 and c0 + ncols > hf_ * 256]
                wkeys_ = [K("w", k, h // 2, hf_) for hf_ in halves]
            else:
                wkeys_ = allk
            S.dma(v[:, h:h + step, c0:c0 + ncols],
                  src2d[h * 128:(h + step) * 128, :].rearrange("(kc p) n -> p kc n", p=128),
                  [], wkeys_, q="pool")
        return v, allk, k

    def dump(name, ap, reads, shape):
        if name in dumps:
            d = nc.dram_tensor("dbg_" + name, list(shape), ap.dtype, kind="ExternalOutput").ap()
            dump_d[name] = d
            i = S.dma(d, ap, reads, [K("dbg", name)])
            S.final.append(i)

    def load_fm(dst, src_d, ntile, dkey, st):
        xin = [sb(f"xin{j}", [128, D_], stack=st) for j in range(2)]
        for i in range(ntile):
            b = xin[i % 2]
            S.dma(b[:], src_d[i * 128:(i + 1) * 128, :], [], [K("xin", i % 2)])
            for half in range(2):
                ps, pk = ps_rot()
                for j in range(4):
                    kc = half * 4 + j
                    S.tr(ps[:, j * 128:(j + 1) * 128], b[:, kc * 128:(kc + 1) * 128], ident,
                         [K("xin", i % 2), K("c128")], [pk])
                S.cp(dst[:, half * 4:half * 4 + 4, i * 128:(i + 1) * 128],
                     ps[:].rearrange("p (a b) -> p a b", a=4), [pk],
                     [K(dkey, half * 4 + j, i // 4) for j in range(4)], eng=("dve" if half == 0 else "act"))

    st0 = ExitStack()
    load_fm(xT, x_d, NT, "xT", st0)

    def rmsnorm_fm(src, skey, gcol, dst, dkey, nk, T, st, dscale=1.0, hook=None, tag="n"):
        tw = min(T, TW)
        sq = [sb(f"sq_{tag}{j}", [128, nk, tw], BF16, stack=st) for j in range(2)]
        rs = [sb(f"rs_{tag}{j}", [128, tw], stack=st) for j in range(2)]
        for tc in range(T // tw):
            sl = slice(tc * tw, (tc + 1) * tw)
            q = sq[tc % 2]
            for kc in range(nk):
                S.act(q[:, kc, :], src[:, kc, sl], AF.Square, [K(skey, kc, tc)], [K("sq" + tag, tc % 2, kc)],
                      scale=float((nk * 128) ** -0.5))
            ps, pk = ps_rot()
            for kc in range(nk):
                S.mm(ps[:, 0:tw], ones_b, q[:, kc, :], kc == 0, kc == nk - 1,
                     [K("sq" + tag, tc % 2, kc), K("c128b")], [pk])
            r = rs[tc % 2]
            S.act(r[:], ps[:, 0:tw], AF.Ln, [pk, K("cst")], [K("rs" + tag, tc % 2)], bias=cst[:, 0:1])
            S.act(r[:], r[:], AF.Exp, [K("rs" + tag, tc % 2)], [K("rs" + tag, tc % 2)], scale=-0.5)
            if dst is not None:
                for kc in range(nk):
                    S.stt(dst[:, kc, sl], src[:, kc, sl], gcol(kc), r[:], ALU.mult, ALU.mult,
                          [K(skey, kc, tc), K("rs" + tag, tc % 2), K("colp")], [K(dkey, kc, tc)],
                          eng="dve")
            if hook is not None:
                hook(tc, r)

    ALLH = [K("hT", kc, tc) for kc in range(KC) for tc in range(NTC)]

    def hkeys(tc):
        return [K("hT", kc, tc) for kc in range(KC)]

    def proj_fm(wv, wkeys, c0, tc, ps, pk):
        for kc in range(KC):
            S.mm(ps[:, :], wv[:, kc, c0:c0 + 128], hT[:, kc, tc * TW:(tc + 1) * TW], kc == 0, kc == KC - 1,
                 wkeys + [K("hT", kc, tc)], [pk])

    def conv_chunk(ps, pk, tc, xw, xwk, cwcol, cbcol, dst, dkey):
        w_ = xw[tc % 2]
        wk = K(xwk, tc % 2)
        if tc == 0:
            S.add("dve", lambda e: e.memset(w_[:, 0:3], 0.0), [], [wk])
        else:
            S.cp(w_[:, 0:3], xw[(tc - 1) % 2][:, 512:515], [K(xwk, (tc - 1) % 2)], [wk])
        S.cp(w_[:, 3:515], ps[:, :], [pk], [wk], eng="act")
        S.ts(dst, w_[:, 3:515], cwcol(3), cbcol, ALU.mult, ALU.add, [wk, K("colp")], [dkey])
        for k in range(3):
            S.stt(dst, w_[:, k:k + 512], cwcol(k), dst, ALU.mult, ALU.add, [wk, dkey, K("colp")], [dkey])

    def group_out(l, g, YG, st):
        YB = sb("YB", [128, 2, S_], BF16, stack=st)
        rmsnorm_fm(YG, "YG", lambda kc: col(l, "grp_g", 2 * g + kc), YB, "YB", 2, S_, st, tag="g")
        dump(f"yn{l}_{g}", YB[:], [K("YB", c, t) for c in range(2) for t in range(NTC)], [128, 2, S_])
        for half in range(2):
            wv, wk, _ = load_w(w_out_d[l, g * 256:(g + 1) * 256, half * 512:(half + 1) * 512], 2, 512)
            for o4 in range(4):
                oc = half * 4 + o4
                for tc in range(NTC):
                    ps, pk = ps_rot()
                    for kc in range(2):
                        S.mm(ps[:, :], wv[:, kc, o4 * 128:(o4 + 1) * 128], YB[:, kc, tc * TW:(tc + 1) * TW],
                             kc == 0, kc == 1, wk + [K("YB", kc, tc)], [pk])
                    S.tt(xT[:, oc, tc * TW:(tc + 1) * TW], xT[:, oc, tc * TW:(tc + 1) * TW], ps[:, :], ALU.add,
                         [K("xT", oc, tc), pk], [K("xT", oc, tc)])

    ALLX = [K("xT", kc, tc) for kc in range(KC) for tc in range(NTC)]
    done = False
    for l in range(n_layers):
        with ExitStack() as st:
            rmsnorm_fm(xT, "xT", lambda kc: col(l, "mix_g", kc), hT, "hT", KC, S_, (st0 if l == 0 else st), tag="a")
        if l == 0:
            st0.close()
        S.barrier()
        if l == 0:
            dump("h0", hT[:], ALLH, [128, KC, S_])
        if stop_after == "norm0":
            break

        with ExitStack() as sty, ExitStack() as st:
            YG = sb("YG", [128, 2, S_], stack=sty)
            wv, wk, _ = load_w(w_in_d[l, :, 0:512], 8, 512)
            wbd = sb("wbd", [128, 4, 128], BF16, stack=st)
            S.dma(wbd[:], lrubd_d[l].rearrange("g c k m -> k (g c) m"), [], [K("wbd")], q="pool", arena=True)
            c8 = sb("c8", [128, 2], stack=st)
            dump(f"A_wbd{l}", wbd[:], [K("wbd")], [128, 4, 128])
            cy = sb("cy", [128, 2], stack=st)
            cz = sb("cz", [128, 2], stack=st)
            S.act(cy[:], col(l, "lru_lam", 0, 2), AF.Exp, [K("colp")], [K("cy")], scale=-1.0)
            S.ts(cz[:], cy[:], 2.0, None, ALU.add, None, [K("cy")], [K("cz")])
            S.add("dve", lambda e: e.reciprocal(out=cz[:], in_=cz[:]), [K("cz")], [K("cz")])
            S.tt(cz[:], cz[:], cy[:], ALU.mult, [K("cz"), K("cy")], [K("cz")])
            S.tt(cy[:], cz[:], cz[:], ALU.mult, [K("cz")], [K("cy")])
            S.ts(c8[:], cy[:], 1.0 / 9.0, 1.0 / 7.0, ALU.mult, ALU.add, [K("cy")], [K("c8")])
            for cc_ in (1.0 / 5.0, 1.0 / 3.0, 1.0):
                S.tt(c8[:], c8[:], cy[:], ALU.mult, [K("c8"), K("cy")], [K("c8")])
                S.ts(c8[:], c8[:], cc_, None, ALU.add, None, [K("c8")], [K("c8")])
            S.tt(c8[:], c8[:], cz[:], ALU.mult, [K("c8"), K("cz")], [K("c8")])
            S.ts(c8[:], c8[:], -16.0, None, ALU.mult, None, [K("c8")], [K("c8")])
            xw = [sb(f"xw{j}", [128, 515], stack=st) for j in range(2)]
            XC = sb("XC", [128, S_], stack=st)
            XCB = sb("XCB", [128, S_], BF16, stack=st)
            GG = sb("GG", [128, S_], stack=st)
            RA = sb("RA", [128, S_], stack=st)
            IU = sb("IU", [128, S_], stack=st)
            T2 = sb("T2", [128, S_], stack=st)
            XX = sb("XX", [128, S_], stack=st)
            for c in range(2):
                for tc in range(NTC):
                    sl = slice(tc * TW, (tc + 1) * TW)
                    ps, pk = ps_rot()
                    proj_fm(wv, wk, c * 128, tc, ps, pk)
                    conv_chunk(ps, pk, tc, xw, "xw", lambda k: col(l, "lru_cw", c * 4 + k), col(l, "lru_cb", c),
                               XC[:, sl], K("XC", tc))
                    ps, pk = ps_rot()
                    proj_fm(wv, wk, 256 + c * 128, tc, ps, pk)
                    S.cp(GG[:, sl], ps[:, :], [pk], [K("GG", tc)], eng="act")
                XCk = [K("XC", t) for t in range(NTC)]
                GGk = [K("GG", t) for t in range(NTC)]
                S.tt(T2[:], GG[:], GG[:], ALU.mult, GGk, [K("T2")])
                S.ts(T2[:], T2[:], 0.044715, 1.0, ALU.mult, ALU.add, [K("T2")], [K("T2")])
                S.tt(T2[:], T2[:], GG[:], ALU.mult, [K("T2")] + GGk, [K("T2")])
                S.act(T2[:], T2[:], AF.Sigmoid, [K("T2")], [K("T2")], scale=1.5957691216)
                S.tt(GG[:], GG[:], T2[:], ALU.mult, [K("T2")] + GGk, GGk)
                S.cp(XCB[:], XC[:], XCk, [K("XCB")], eng="act")
                for tc in range(NTC):
                    sl = slice(tc * TW, (tc + 1) * TW)
                    ps, pk = ps_rot()
                    S.mm(ps[:, :], wbd[:, c, :], XCB[:, sl], True, True, [K("wbd"), K("XCB")], [pk])
                    S.act(RA[:, sl], ps[:, :], AF.Sigmoid, [pk, K("colp")], [K("RA", tc)], bias=col(l, "lru_br", c))
                    ps, pk = ps_rot()
                    S.mm(ps[:, :], wbd[:, 2 + c, :], XCB[:, sl], True, True, [K("wbd"), K("XCB")], [pk])
                    S.act(IU[:, sl], ps[:, :], AF.Sigmoid, [pk, K("colp")], [K("IU", tc)], bias=col(l, "lru_bi", c))
                RAk = [K("RA", t) for t in range(NTC)]
                IUk = [K("IU", t) for t in range(NTC)]
                if c == 1:
                    dump(f"A_r{l}", RA[:], RAk, [128, S_])
                    dump(f"A_i{l}", IU[:], IUk, [128, S_])
                    dump(f"A_xcb{l}", XCB[:], [K("XCB")], [128, S_])
                    dump(f"A_xc{l}", XC[:], XCk, [128, S_])
                S.ts(XX[:], RA[:], c8[:, c:c + 1], 2.0, ALU.mult, ALU.mult, RAk + [K("c8")], [K("XX")])
                S.ts(T2[:], XX[:], 1.0 / 120.0, None, ALU.mult, None, [K("XX")], [K("T2")])
                for cc_ in (1.0 / 24.0, 1.0 / 6.0, 0.5, 1.0):
                    S.stt(T2[:], T2[:], cc_, XX[:], ALU.add, ALU.mult, [K("T2"), K("XX")], [K("T2")])
                S.act(T2[:], T2[:], AF.Sqrt, [K("T2")], [K("T2")], scale=-1.0)
                S.act(RA[:], XX[:], AF.Exp, [K("XX")], RAk, scale=0.5)
                S.tt(IU[:], IU[:], XC[:], ALU.mult, IUk + XCk, IUk)
                S.tt(IU[:], IU[:], T2[:], ALU.mult, IUk + [K("T2")], IUk)
                S.add("dve", lambda e: e.tensor_tensor_scan(out=XC[:], data0=RA[:], data1=IU[:], initial=0.0,
                                                            op0=ALU.mult, op1=ALU.add), RAk + IUk, XCk, cost=2.6)
                S.tt(YG[:, c, :], XC[:], GG[:], ALU.mult, XCk + GGk, [K("YG", c, t) for t in range(NTC)])
            for nm_, t_ in (("XC", XC), ("GG", GG), ("RA", RA), ("IU", IU), ("T2", T2)):
                dump(f"A_{nm_}{l}", t_[:], [K(nm_, t) for t in range(NTC)] + [K(nm_)], [128, S_])
            dump(f"ya{l}", YG[:], [K("YG", c, t) for c in range(2) for t in range(NTC)], [128, 2, S_])
            st.close()
            S.barrier()
            group_out(l, 0, YG, sty)
        S.barrier()
        if stop_after == f"A{l}":
            done = True
            break

        nrot[0] = 4
        with ExitStack() as sty, ExitStack() as st:
            YG = sb("YG", [128, 2, S_], stack=sty)
            QB = sb("QB", [128, 2, S_], BF16, stack=st)
            KZ = sb("KZ", [128, 4, S_], BF16, stack=st)
            S.add("dve", lambda e: e.memset(KZ[:], 0.0), [], [K("KZ", h_, t_) for h_ in range(4) for t_ in range(NTC)], cost=4.5)
            VB = sb("VB", [128, NT, 256], BF16, stack=st)
            M01 = sb("M01", [128, 4, 512], stack=st)
            NM = sb("NM", [128, 4, 512], BF16, stack=st)
            S.dma(M01[:], cmask_d[:, 0:2048].rearrange("p (a b) -> p a b", a=4), [], [K("M01")])
            S.dma(NM[:], cmask_d[:, 2048:4096].rearrange("p (a b) -> p a b", a=4), [], [K("NM")], q="pool", arena=True)
            wv, wk, _ = load_w(w_in_d[l, :, 512:1024], 8, 512)
            wv2, wk2, _ = load_w(w_in_d[l, :, 1024:1280], 8, 256)
            for c2 in range(2):
                for tc in range(NTC):
                    sl = slice(tc * TW, (tc + 1) * TW)
                    ps, pk = ps_rot()
                    proj_fm(wv, wk, c2 * 128, tc, ps, pk)
                    S.ts(QB[:, c2, sl], ps[:, :], 0.125, None, ALU.mult, None, [pk], [K("QB", c2, tc)])
                    ps, pk = ps_rot()
                    proj_fm(wv, wk, 256 + c2 * 128, tc, ps, pk)
                    S.cp(KZ[0:64, 2 * c2, sl], ps[0:64, :], [pk], [K("KZ", 2 * c2, tc)])
                    S.cp(KZ[64:128, 2 * c2 + 1, sl], ps[64:128, :], [pk], [K("KZ", 2 * c2 + 1, tc)])
            for i in range(NT):
                ps, pk = ps_rot()
                for kc in range(KC):
                    S.mm(ps[:, 0:256], hT[:, kc, i * 128:(i + 1) * 128], wv2[:, kc, :], kc == 0, kc == KC - 1,
                         wk2 + [K("hT", kc, i // 4)], [pk])
                S.cp(VB[:, i, :], ps[:, 0:256], [pk], [K("VB", i)], eng=("dve" if i % 2 else "act"))
            Eb = [sb(f"Eb{j}", [128, 512], stack=st) for j in range(2)]
            SPb = [sb(f"SPb{j}", [128, 512], F32R, stack=st) for j in range(3)]
            Wb = [sb(f"Wb{j}", [128, 512], BF16, stack=st) for j in range(2)]
            Rb = [sb(f"Rb{j}", [128, 512], F32R, stack=st) for j in range(2)]
            NUr = sb("NUr", [128, 2, 128], F32R, stack=st)
            S.cp(NUr[:, 0, :], negu, [K("c128")], [K("NUr")])
            S.cp(NUr[:, 1, :], negones, [K("c128")], [K("NUr")])
            items = []
            grp = 0
            for h in range(4):
                for c in range(NTC):
                    nb = 4 * c + 4
                    for idx, b in enumerate(range(nb - 1, -1, -1)):
                        items.append(dict(h=h, c=c, idx=idx, b=b, grp=grp, last=(b == 0)))
                    grp += 1
            for i_, itm in enumerate(items):
                itm["i"] = i_
                itm["pr"] = slice((itm["h"] % 2) * 64, (itm["h"] % 2) * 64 + 64)
                itm["c2"] = itm["h"] // 2
                itm["d"] = itm["b"] - 4 * itm["c"]
                itm["ksl"] = slice(itm["b"] * 128, (itm["b"] + 1) * 128)
                itm["qsl"] = slice(itm["c"] * TW, (itm["c"] + 1) * TW)
                itm["qk"] = [K("KZ", itm["h"], itm["b"] // 4), K("QB", itm["c2"], itm["c"])]

            def sb_s1(m):
                i = m["i"]
                pr, c2 = m["pr"], m["c2"]
                off = 128 * max(m["d"], 0)
                cs = slice(off, TW)
                qs = slice(m["c"] * TW + off, (m["c"] + 1) * TW)
                if m["idx"] == 0:
                    R0, R0k = Rb[m["grp"] % 2], K("Rb", m["grp"] % 2)
                    S.ts(R0[:], M01[:, 0, :], 0.0, None, ALU.mult, None, [K("M01")], [R0k])
                psZ, pkZ = ps_rot()
                S.mm(psZ[:, cs], KZ[:, m["h"], m["ksl"]], QB[:, c2, qs], True, True, m["qk"], [pkZ])
                E, Ek = Eb[i % 2], K("Eb", i % 2)
                SP, SPk = SPb[i % 3], K("SPb", i % 3)
                S.act(E[:, cs], psZ[:, cs], AF.Exp, [pkZ], [Ek])
                S.act(SP[:, cs], E[:, cs], AF.Ln, [Ek, K("cst")], [SPk], bias=cst[:, 1:2])
                if m["d"] >= 0:
                    S.tt(SP[:, cs], SP[:, cs].bitcast(F32), M01[:, m["d"], cs], ALU.mult, [SPk, K("M01")], [SPk])

            def sb_s2(m):
                i = m["i"]
                pr, c2, d = m["pr"], m["c2"], m["d"]
                off = 128 * max(d, 0)
                cs = slice(off, TW)
                qs = slice(m["c"] * TW + off, (m["c"] + 1) * TW)
                SP, SPk = SPb[i % 3], K("SPb", i % 3)
                Wt, Wk_ = Wb[i % 2], K("Wb", i % 2)
                R, Rk = Rb[m["grp"] % 2], K("Rb", m["grp"] % 2)
                psA, pkA = ps_rot()
                has_r = m["idx"] > 0
                S.mm(psA[:, cs], KZ[:, m["h"], m["ksl"]], QB[:, c2, qs], True, False, m["qk"], [pkA])
                S.mm(psA[:, cs], NUr[:, 0, :], SP[:, cs], False, (not has_r) and d < 0, [SPk, K("NUr")], [pkA])
                if has_r:
                    S.mm(psA[:, cs], NUr[:, 1, :], R[:, cs], False, d < 0, [Rk, K("NUr")], [pkA])
                if d >= 0:
                    S.mm(psA[:, cs], ident_b, NM[:, d, cs], False, True, [K("NM"), K("c128b")], [pkA])
                S.act(Wt[:, cs], psA[:, cs], AF.Exp, [pkA], [Wk_])
                if not m["last"]:
                    S.tt(R[:, cs], R[:, cs].bitcast(F32), SP[:, cs].bitcast(F32), ALU.add, [Rk, SPk], [Rk])

            def sb_s3(m):
                i = m["i"]
                pr, c2, h = m["pr"], m["c2"], m["h"]
                off = 128 * max(m["d"], 0)
                cs = slice(off, TW)
                Wt, Wk_ = Wb[i % 2], K("Wb", i % 2)
                psO, pkO = PS[4 + m["grp"] % 2], K("psO", m["grp"] % 2)
                S.mm(psO[:, cs], VB[:, m["b"], c2 * 128:(c2 + 1) * 128], Wt[:, cs], m["idx"] == 0, m["last"],
                     [K("VB", m["b"]), Wk_], [pkO])
                if m["last"]:
                    S.cp(YG[pr, c2, m["qsl"]], psO[pr, :], [pkO], [K("YG", c2, m["c"])])

            n_it = len(items)
            for r in range(-2, n_it):
                if 0 <= r + 2 < n_it:
                    sb_s1(items[r + 2])
                if 0 <= r + 1 < n_it:
                    sb_s2(items[r + 1])
                if 0 <= r < n_it:
                    sb_s3(items[r])
            dump(f"yb{l}", YG[:], [K("YG", c, t) for c in range(2) for t in range(NTC)], [128, 2, S_])
            st.close()
            S.barrier()
            nrot[0] = 8
            group_out(l, 1, YG, sty)
        S.barrier()
        if stop_after == f"B{l}":
            done = True
            break

        nrot[0] = 4
        with ExitStack() as sty, ExitStack() as st:
            YG = sb("YG", [128, 2, S_], stack=sty)
            XS = sb("XS", [128, 2, S_], stack=st)
            BTb = sb("BTb", [128, 2, S_], BF16, stack=st)
            CTb = sb("CTb", [128, 2, S_], BF16, stack=st)
            BTM = sb("BTM", [128, NT, 256], BF16, stack=st)
            XTM = sb("XTM", [128, NT, 256], BF16, stack=st)
            wdt = sb("wdt", [128, KC, 4], BF16, stack=st)
            stc = ExitStack()
            xw = [sb(f"xwc{j}", [128, 515], stack=stc) for j in range(2)]
            CV = [sb(f"CV{j}", [128, 512], stack=stc) for j in range(2)]
            BF = [sb(f"BF{j}", [128, 512], stack=stc) for j in range(2)]
            S.dma(wdt[:], w_in_d[l, :, 2304:2308].rearrange("(kc p) n -> p kc n", p=128), [], [K("wdt")], q="pool", arena=True)
            wv1, wk1, _ = load_w(w_in_d[l, :, 1280:1792], 8, 512)
            wv2, wk2, _ = load_w(w_in_d[l, :, 1792:2304], 8, 512)
            n_cv = 0
            for j in range(6):
                if j < 2:
                    wv, wk, c0 = wv1, wk1, 256 + j * 128
                elif j < 4:
                    wv, wk, c0 = wv2, wk2, (j - 2) * 128
                else:
                    wv, wk, c0 = wv2, wk2, 256 + (j - 4) * 128
                for tc in range(NTC):
                    sl = slice(tc * TW, (tc + 1) * TW)
                    ps, pk = ps_rot()
                    proj_fm(wv, wk, c0, tc, ps, pk)
                    cv, cvk = CV[n_cv % 2], K("CV", n_cv % 2)
                    conv_chunk(ps, pk, tc, xw, "xwc", lambda k: col(l, "ssm_cw", j * 4 + k), col(l, "ssm_cb", j),
                               cv[:], cvk)
                    if j < 2:
                        S.act(XS[:, j, sl], cv[:], AF.Silu, [cvk], [K("XS", j, tc)])
                        ps, pk = ps_rot()
                        for q in range(4):
                            S.tr(ps[:, q * 128:(q + 1) * 128], XS[:, j, tc * TW + q * 128: tc * TW + (q + 1) * 128],
                                 ident, [K("XS", j, tc), K("c128")], [pk])
                        S.cp(XTM[:, tc * 4:tc * 4 + 4, j * 128:(j + 1) * 128],
                             ps[:].rearrange("p (a b) -> p a b", a=4), [pk], [K("XTM", tc, j)])
                    elif j < 4:
                        g = j - 2
                        bf, bfk = BF[n_cv % 2], K("BF", n_cv % 2)
                        S.act(bf[:], cv[:], AF.Silu, [cvk], [bfk])
                        S.cp(BTb[:, g, sl], bf[:], [bfk], [K("BTb", g, tc)])
                        ps, pk = ps_rot()
                        for q in range(4):
                            S.tr(ps[:, q * 128:(q + 1) * 128], bf[:, q * 128:(q + 1) * 128], ident,
                                 [bfk, K("c128")], [pk])
                        S.cp(BTM[:, tc * 4:tc * 4 + 4, g * 128:(g + 1) * 128],
                             ps[:].rearrange("p (a b) -> p a b", a=4), [pk], [K("BTM", tc, g)])
                    else:
                        S.act(CTb[:, j - 4, sl], cv[:], AF.Silu, [cvk], [K("CTb", j - 4, tc)])
                    n_cv += 1
            stc.close()
            S.barrier()
            DT = sb("DT", [128, 64], stack=st)
            AN = sb("AN", [128, 64], stack=st)
            AC = sb("AC", [128, 64], stack=st)
            ACS = sb("ACS", [128, 64], stack=st)
            TOT = sb("TOT", [128, 64], stack=st)
            DEC = sb("DEC", [128, 64], stack=st)
            ETOT = sb("ETOT", [128, 64], stack=st)
            DTD = sb("DTD", [128, 64], stack=st)
            psd, pkd = ps_rot()
            for i in range(NT):
                for kc in range(KC):
                    S.mm(psd[:, i * 4:(i + 1) * 4], hT[:, kc, i * 128:(i + 1) * 128], wdt[:, kc, :], kc == 0,
                         kc == KC - 1, [K("wdt"), K("hT", kc, i // 4)], [pkd])
            S.tt(DT[:], psd[:, 0:64], row(l, "dt_bias"), ALU.add, [pkd, K("rowp")], [K("DT")])
            S.act(DT[:], DT[:], AF.Exp, [K("DT")], [K("DT")])
            S.act(DT[:], DT[:], AF.Ln, [K("DT"), K("cst")], [K("DT")], bias=cst[:, 1:2])
            S.act(AN[:], row(l, "a_log"), AF.Exp, [K("rowp")], [K("AN")])
            S.ts(AN[:], AN[:], -1.0, None, ALU.mult, None, [K("AN")], [K("AN")])
            S.tt(AC[:], DT[:], AN[:], ALU.mult, [K("DT"), K("AN")], [K("AC")])
            ps, pk = ps_rot()
            S.mm(ps[:, 0:64], tri_le, AC[:], True, True, [K("AC"), K("c128")], [pk])
            S.cp(ACS[:], ps[:, 0:64], [pk], [K("ACS")])
            ps, pk = ps_rot()
            S.mm(ps[:, 0:64], ones_f, AC[:], True, True, [K("AC"), K("c128")], [pk])
            S.cp(TOT[:], ps[:, 0:64], [pk], [K("TOT")])
            S.tt(DEC[:], TOT[:], ACS[:], ALU.subtract, [K("TOT"), K("ACS")], [K("DEC")])
            S.act(DEC[:], DEC[:], AF.Exp, [K("DEC")], [K("DEC")])
            S.act(ETOT[:], TOT[:], AF.Exp, [K("TOT")], [K("ETOT")])
            S.tt(DTD[:], DT[:], DEC[:], ALU.mult, [K("DT"), K("DEC")], [K("DTD")])
            ST = sb("ST", [128, 4, 64], stack=st)
            S.add("dve", lambda e: e.memset(ST[:], 0.0), [], [K("ST", h) for h in range(4)])
            Rm = [sb(f"Rm{j}", [128, 128], stack=st) for j in range(2)]
            ARG = [sb(f"ARG{j}", [128, 128], stack=st) for j in range(2)]
            E1 = [sb(f"E1{j}", [128, 128], stack=st) for j in range(2)]
            GL = [sb(f"GL{j}", [128, 128], BF16, stack=st) for j in range(2)]
            CTp = [sb(f"CTp{j}", [128, 128], BF16, stack=st) for j in range(2)]
            XCp = [[sb(f"XCp{a}{j}", [128, 128], BF16, stack=st) for j in range(2)] for a in range(2)]
            STp = sb("STp", [128, 4, 128], BF16, stack=st)
            for a in range(2):
                for j in range(2):
                    S.add("dve", (lambda e, t_=XCp[a][j]: e.memset(t_[:], 0.0)), [], [K("XCp", a, j)])
            S.add("dve", lambda e: e.memset(STp[:], 0.0), [], [K("STp", h) for h in range(4)])
            XDh = [sb(f"XDh{j}", [128, 64], BF16, stack=st) for j in range(2)]
            n = 0
            for ci in range(NT):
                csl = slice(ci * 128, (ci + 1) * 128)
                tcx = ci // 4
                for g in range(2):
                    psG, pkG = ps_rot()
                    S.mm(psG[:, 0:128], BTb[:, g, csl], CTb[:, g, csl], True, True,
                         [K("BTb", g, tcx), K("CTb", g, tcx)], [pkG])
                    psY, pkY = PS[4 + (ci * 2 + g) % 2], K("psO", (ci * 2 + g) % 2)
                    for hh in range(2):
                        h = 2 * g + hh
                        pr = slice(hh * 64, hh * 64 + 64)
                        cidx = ci * 4 + h
                        k2 = n % 2
                        S.ts(Rm[k2][:], tri_le, AC[:, cidx:cidx + 1], None, ALU.mult, None,
                             [K("AC"), K("c128")], [K("Rm", k2)])
                        ps1, pk1 = ps_rot()
                        S.mm(ps1[:, 0:128], ones_f, Rm[k2][:], True, True, [K("Rm", k2), K("c128")], [pk1])
                        S.stt(ARG[k2][:], ps1[:, 0:128], ACS[:, cidx:cidx + 1], ssdneg, ALU.subtract, ALU.add,
                              [pk1, K("ACS"), K("c128")], [K("ARG", k2)])
                        S.act(ARG[k2][:], ARG[k2][:], AF.Exp, [K("ARG", k2)], [K("ARG", k2)])
                        S.tt(GL[k2][:], ARG[k2][:], psG[:, 0:128], ALU.mult, [K("ARG", k2), pkG], [K("GL", k2)])
                        S.act(E1[k2][:], ps1[:, 0:128], AF.Exp, [pk1, K("ARG", k2)], [K("E1", k2)])
                        S.tt(CTp[k2][:], CTb[:, g, csl], E1[k2][:], ALU.mult, [K("CTb", g, tcx), K("E1", k2)],
                             [K("CTp", k2)])
                        rot = (ci * 2 + g) % 2
                        xcp = XCp[hh][rot]
                        S.ts(xcp[:, hh * 64:(hh + 1) * 64], XTM[:, ci, h * 64:(h + 1) * 64], DT[:, cidx:cidx + 1], None,
                             ALU.mult, None, [K("XTM", tcx, g), K("DT")], [K("XCp", hh, rot)])
                        S.mm(psY[:, 0:128], xcp[:], GL[k2][:], hh == 0, False, [K("XCp", hh, rot), K("GL", k2)], [pkY])
                        S.mm(psY[:, 0:128], STp[:, h, :], CTp[k2][:], False, hh == 1, [K("STp", h), K("CTp", k2)], [pkY])
                        S.ts(XDh[k2][:], XTM[:, ci, h * 64:(h + 1) * 64], DTD[:, cidx:cidx + 1], None, ALU.mult, None,
                             [K("XTM", tcx, g), K("DTD")], [K("XDh", k2)])
                        psS, pkS = ps_rot()
                        S.mm(psS[:, 0:64], BTM[:, ci, g * 128:(g + 1) * 128], XDh[k2][:], True, True,
                             [K("BTM", tcx, g), K("XDh", k2)], [pkS])
                        S.stt(ST[:, h, :], ST[:, h, :], ETOT[:, cidx:cidx + 1], psS[:, 0:64], ALU.mult, ALU.add,
                              [K("ST", h), K("ETOT"), pkS], [K("ST", h)])
                        S.cp(STp[:, h, hh * 64:(hh + 1) * 64], ST[:, h, :], [K("ST", h)], [K("STp", h)], eng="act")
                        n += 1
                    S.cp(YG[:, g, csl], psY[:, 0:128], [pkY], [K("YG", g, tcx)], eng="act")
            ARG = [sb(f"ZT{j}", [128, 512], stack=st) for j in range(1)]
            for j in range(2):
                YGk = [K("YG", j, t) for t in range(NTC)]
                S.stt(YG[:, j, :], XS[:, j, :], col(l, "ssm_d", j), YG[:, j, :], ALU.mult, ALU.add,
                      [K("XS", j, t) for t in range(NTC)] + YGk + [K("colp")], YGk)
                for tc in range(NTC):
                    sl = slice(tc * TW, (tc + 1) * TW)
                    ps, pk = ps_rot()
                    proj_fm(wv1, wk1, j * 128, tc, ps, pk)
                    zt, ztk = ARG[0], K("ARGz", 0)
                    S.act(zt[:], ps[:, :], AF.Silu, [pk], [ztk])
                    S.tt(YG[:, j, sl], YG[:, j, sl], zt[:], ALU.mult, [K("YG", j, tc), ztk], [K("YG", j, tc)])
            dump(f"yc{l}", YG[:], [K("YG", c, t) for c in range(2) for t in range(NTC)], [128, 2, S_])
            st.close()
            S.barrier()
            nrot[0] = 8
            group_out(l, 2, YG, sty)
        S.barrier()
        if stop_after == f"C{l}":
            done = True
            break

        nrot[0] = 4
        with ExitStack() as sty, ExitStack() as st:
            YG = sb("YG", [128, 2, S_], stack=sty)
            QA = sb("QA", [128, 4, S_], BF16, stack=st)
            KA = sb("KA", [128, 4, S_], BF16, stack=st)
            allqa = [K("QA", h_, t_) for h_ in range(4) for t_ in range(NTC)]
            allka = [K("KA", h_, t_) for h_ in range(4) for t_ in range(NTC)]
            S.add("dve", lambda e: e.memset(QA[:], 0.0), [], allqa, cost=4.5)
            S.add("dve", lambda e: e.memset(KA[:], 0.0), [], allka, cost=4.5)
            for h_ in range(4):
                o_ = 64 if h_ % 2 == 0 else 0
                S.dma(KA[o_:o_ + 8, h_, :], blkoh_d[:, :], allka, [K("KAoh", h_)], q="pool", arena=True)
            VB = sb("VB", [128, NT, 256], BF16, stack=st)
            CAUS = sb("CAUS", [128, 4, 512], BF16, stack=st)
            S.dma(CAUS[:], cmask_d[:, 4096:6144].rearrange("p (a b) -> p a b", a=4), [], [K("CAUS")], q="pool", arena=True)
            GC = sb("GC", [128, 3, 128], stack=st)
            S.dma(GC[:], gate_c_d[:, :].rearrange("p (a b) -> p a b", a=3), [], [K("GC")])
            KM = sb("KM", [128, 2, 8], stack=st)
            KMb = sb("KMb", [128, 2, 8], BF16, stack=st)
            wv, wk, _ = load_w(w_in_d[l, :, 2308:2820], 8, 512)
            wv2, wk2, _ = load_w(w_in_d[l, :, 2820:3076], 8, 256)
            str_ = ExitStack()
            ROPEb = [sb(f"ROPE{j}", [128, 2, TW], stack=str_) for j in range(1)]
            RAW = [sb(f"RAW{j}", [128, 512], stack=str_) for j in range(2)]
            TMP = [sb(f"TMPr{j}", [128, 512], stack=str_) for j in range(2)]
            KF = [sb(f"KF{j}", [128, 512], stack=str_) for j in range(2)]
            nr = 0
            for c2 in range(2):
                for tc in range(NTC):
                    sl = slice(tc * TW, (tc + 1) * TW)
                    rj = 0
                    ROPE = ROPEb[rj]
                    rsl = slice(0, TW)
                    S.dma(ROPE[:, 0, :], rope_d[:, tc * TW:(tc + 1) * TW], [], [K("ROPE", rj, 0)])
                    S.dma(ROPE[:, 1, :], rope_d[:, S_ + tc * TW:S_ + (tc + 1) * TW], [], [K("ROPE", rj, 1)])
                    for which in range(2):
                        ps, pk = ps_rot()
                        proj_fm(wv, wk, which * 256 + c2 * 128, tc, ps, pk)
                        raw, rawk = RAW[nr % 2], K("RAW", nr % 2)
                        tmp, tmpk = TMP[nr % 2], K("TMPr", nr % 2)
                        S.cp(raw[:], ps[:, :], [pk], [rawk], eng="act")
                        psw, pkw = ps_rot()
                        S.mm(psw[:, :], perm_f, raw[:], True, True, [rawk, K("c128")], [pkw])
                        S.tt(tmp[:], psw[:, :], ROPE[:, 1, rsl], ALU.mult, [pkw, K("ROPE", rj, 1)], [tmpk])
                        kf = KF[nr % 2]
                        dst, dk = kf[:], K("KF", nr % 2)
                        S.tt(dst, raw[:], ROPE[:, 0, rsl], ALU.mult, [rawk, K("ROPE", rj, 0)], [dk])
                        S.tt(dst, dst, tmp[:], ALU.add, [dk, tmpk], [dk])
                        if which == 0:
                            S.act(QA[0:64, 2 * c2, sl], kf[0:64, :], AF.Copy, [dk], [K("QA", 2 * c2, tc)], scale=0.125)
                            S.act(QA[64:128, 2 * c2 + 1, sl], kf[64:128, :], AF.Copy, [dk], [K("QA", 2 * c2 + 1, tc)],
                                  scale=0.125)
                        else:
                            S.cp(KA[0:64, 2 * c2, sl], kf[0:64, :], [dk], [K("KA", 2 * c2, tc)], eng="act")
                            S.cp(KA[64:128, 2 * c2 + 1, sl], kf[64:128, :], [dk], [K("KA", 2 * c2 + 1, tc)], eng="act")
                            for hb in range(2):
                                nblk = tc * 2 + hb
                                S.add("dve", (lambda e, o=KM[:, c2, nblk:nblk + 1], i_=kf[:, hb * 256:(hb + 1) * 256]:
                                              e.reduce_sum(out=o, in_=i_, axis=AX.X)), [dk], [K("KM", c2, nblk)])
                        nr += 1
            for i in range(NT):
                ps, pk = ps_rot()
                for kc in range(KC):
                    S.mm(ps[:, 0:256], hT[:, kc, i * 128:(i + 1) * 128], wv2[:, kc, :], kc == 0, kc == KC - 1,
                         wk2 + [K("hT", kc, i // 4)], [pk])
                S.cp(VB[:, i, :], ps[:, 0:256], [pk], [K("VB", i)], eng=("dve" if i % 2 else "act"))
            S.cp(KMb[:], KM[:], [K("KM", c2_, nb_) for c2_ in range(2) for nb_ in range(8)], [K("KMb")])
            str_.close()
            S.barrier()
            PTb = [sb(f"PTb{j}", [128, 512], BF16, stack=st) for j in range(3)]
            RD = [sb(f"RD{j}", [128, 512], stack=st) for j in range(2)]
            GT = sb("GT", [128, 128], stack=st)
            BT_ = sb("BIASTM", [128, 128], stack=st)
            MX = [sb(f"MX{j}", [128, 8], stack=st) for j in range(2)]
            for h in range(4):
                hh, c2 = h % 2, h // 2
                pr = slice(hh * 64, hh * 64 + 64)
                o_ = 64 if hh == 0 else 0
                psg, pkg = ps_rot()
                for i in range(NT):
                    S.mm(psg[:, i * 8:(i + 1) * 8], QA[pr, h, i * 128:(i + 1) * 128], KMb[pr, c2, :], True, True,
                         [K("QA", h, i // 4), K("KMb")], [pkg])
                S.stt(GT[:], psg[:, 0:128], 8.0 / 256.0, GC[:, 0, :], ALU.mult, ALU.add, [pkg, K("GC")], [K("GT")])
                for i in range(NT):
                    mx, mxk = MX[i % 2], K("MX", i % 2)
                    S.add("dve", (lambda e, o=mx[:], i_=GT[:, i * 8:(i + 1) * 8]: e.max(out=o, in_=i_)),
                          [K("GT")], [mxk])
                    bsl = BT_[:, i * 8:(i + 1) * 8]
                    S.ts(bsl, GT[:, i * 8:(i + 1) * 8], mx[:, 2:3], None, ALU.is_ge, None, [K("GT"), mxk],
                         [K("BIASTM", i)])
                    S.tt(bsl, bsl, GC[:, 1, i * 8:(i + 1) * 8], ALU.mult, [K("BIASTM", i), K("GC")], [K("BIASTM", i)])
                    S.tt(bsl, bsl, GC[:, 2, i * 8:(i + 1) * 8], ALU.add, [K("BIASTM", i), K("GC")], [K("BIASTM", i)])
                    S.ts(bsl, bsl, -1.0, -NEG, ALU.add, ALU.mult, [K("BIASTM", i)], [K("BIASTM", i)])
                for q4 in range(4):
                    pst, pkt = ps_rot()
                    for q in range(4):
                        i = q4 * 4 + q
                        S.tr(pst[0:8, q * 128:(q + 1) * 128], BT_[:, i * 8:(i + 1) * 8], ident,
                             [K("BIASTM", i), K("c128")], [pkt])
                    S.cp(QA[o_:o_ + 8, h, q4 * 512:(q4 + 1) * 512], pst[0:8, :], [pkt], [K("QAsel", h, q4)])
            its = []
            grp = 0
            for h in range(4):
                for c in range(NTC):
                    nkt = 4 * c + 4
                    for kt in range(nkt):
                        its.append(dict(h=h, c=c, kt=kt, nkt=nkt, grp=grp, i=len(its)))
                    grp += 1

            def m_s1(m):
                h, c, kt = m["h"], m["c"], m["kt"]
                d = kt - 4 * c
                qsl = slice(c * TW, (c + 1) * TW)
                ksl = slice(kt * 128, (kt + 1) * 128)
                off = 128 * max(d, 0)
                cs = slice(off, TW)
                qs = slice(c * TW + off, (c + 1) * TW)
                psS, pkS = ps_rot()
                S.mm(psS[:, cs], KA[:, h, ksl], QA[:, h, qs], True, d < 0,
                     [K("KA", h, kt // 4), K("KAoh", h), K("QA", h, c), K("QAsel", h, c)], [pkS])
                if d >= 0:
                    S.mm(psS[:, cs], ident_b, CAUS[:, d, cs], False, True, [K("CAUS"), K("c128b")], [pkS])
                pt, ptk = PTb[m["i"] % 3], K("PTb", m["i"] % 3)
                S.act(pt[:, cs], psS[:, cs], AF.Exp, [pkS], [ptk])

            def m_s2(m):
                h, c, kt, nkt = m["h"], m["c"], m["kt"], m["nkt"]
                hh, c2 = h % 2, h // 2
                pr = slice(hh * 64, hh * 64 + 64)
                qsl = slice(c * TW, (c + 1) * TW)
                g2 = m["grp"] % 2
                psO, pkO = PS[4 + g2], K("psO", g2)
                psD, pkD = PS[6 + g2], K("psD", g2)
                pt, ptk = PTb[m["i"] % 3], K("PTb", m["i"] % 3)
                off = 128 * max(kt - 4 * c, 0)
                cs = slice(off, TW)
                S.mm(psO[:, cs], VB[:, kt, c2 * 128:(c2 + 1) * 128], pt[:, cs], kt == 0, kt == nkt - 1,
                     [K("VB", kt), ptk], [pkO])
                S.mm(psD[:, cs], ones_b, pt[:, cs], kt == 0, kt == nkt - 1, [ptk, K("c128b")], [pkD])
                if kt == nkt - 1:
                    rd, rdk = RD[g2], K("RD", g2)
                    S.act(rd[pr, :], psD[pr, :], AF.Ln, [pkD], [rdk])
                    S.act(rd[pr, :], rd[pr, :], AF.Exp, [rdk], [rdk], scale=-1.0)
                    S.tt(YG[pr, c2, qsl], psO[pr, :], rd[pr, :], ALU.mult, [pkO, rdk], [K("YG", c2, c)])

            n_ = len(its)
            for r in range(-1, n_):
                if r + 1 < n_:
                    m_s1(its[r + 1])
                if r >= 0:
                    m_s2(its[r])
            dump(f"yd{l}", YG[:], [K("YG", c, t) for c in range(2) for t in range(NTC)], [128, 2, S_])
            st.close()
            S.barrier()
            nrot[0] = 8
            group_out(l, 3, YG, sty)
        S.barrier()
        dump(f"x1_{l}", xT[:], ALLX, [128, KC, S_])
        if stop_after == f"D{l}":
            done = True
            break

        with ExitStack() as st:
            memT = sb("memT", [128, KC, 256], stack=st)
            MTb = sb("MTb", [128, KC, 256], BF16, stack=st)
            with ExitStack() as st2:
                rmsnorm_fm(xT, "xT", lambda kc: col(l, "xat_g", kc), hT, "hT", KC, S_, st2, tag="x")
                load_fm(memT, mem_d, 2, "memT", st2)
                rmsnorm_fm(memT, "memT", lambda kc: col(l, "mem_g", kc), MTb, "MTb", KC, 256, st2, tag="m")
            S.barrier()
            KTb = sb("KTb", [128, KC, 256], BF16, stack=st)
            VX = sb("VX", [128, 2, D_], BF16, stack=st)
            QX = sb("QX", [128, KC, S_], BF16, stack=st)
            MTk = [K("MTb", kc, 0) for kc in range(KC)]
            for half in range(2):
                wv, wk, _ = load_w(wkv_d[l, :, half * 512:(half + 1) * 512], 8, 512)
                for o4 in range(4):
                    oc = half * 4 + o4
                    ps, pk = ps_rot()
                    for kc in range(KC):
                        S.mm(ps[:, 0:256], wv[:, kc, o4 * 128:(o4 + 1) * 128], MTb[:, kc, :], kc == 0, kc == KC - 1,
                             wk + [K("MTb", kc, 0)], [pk])
                    S.cp(KTb[:, oc, :], ps[:, 0:256], [pk], [K("KTb", oc)])
            for half in range(2):
                wv, wk, _ = load_w(wkv_d[l, :, 1024 + half * 512:1024 + (half + 1) * 512], 8, 512)
                for mt in range(2):
                    ps, pk = ps_rot()
                    for kc in range(KC):
                        S.mm(ps[:, :], MTb[:, kc, mt * 128:(mt + 1) * 128], wv[:, kc, :], kc == 0, kc == KC - 1,
                             wk + [K("MTb", kc, 0)], [pk])
                    S.cp(VX[:, mt, half * 512:(half + 1) * 512], ps[:, :], [pk], [K("VX", mt, half)], eng="act")
            for half in range(2):
                wv, wk, _ = load_w(wq_d[l, :, half * 512:(half + 1) * 512], 8, 512)
                for o4 in range(4):
                    oc = half * 4 + o4
                    for tc in range(NTC):
                        ps, pk = ps_rot()
                        proj_fm(wv, wk, o4 * 128, tc, ps, pk)
                        S.act(QX[:, oc, tc * TW:(tc + 1) * TW], ps[:, :], AF.Copy, [pk], [K("QX", oc, tc)],
                              scale=1.0 / 16.0)
            dump(f"X_MTb{l}", MTb[:], MTk, [128, KC, 256])
            dump(f"X_KTb{l}", KTb[:], [K("KTb", oc) for oc in range(KC)], [128, KC, 256])
            dump(f"X_QX{l}", QX[:], [K("QX", oc, tc) for oc in range(KC) for tc in range(NTC)], [128, KC, S_])
            dump(f"X_VX{l}", VX[:], [K("VX", mt, hf) for mt in range(2) for hf in range(2)], [128, 2, D_])
            PT = [sb(f"PTx{j}", [128, 512], BF16, stack=st) for j in range(4)]
            RDx = [sb(f"RDx{j}", [128, 512], stack=st) for j in range(2)]
            it = 0
            for hd in range(4):
                for tc in range(NTC):
                    sl = slice(tc * TW, (tc + 1) * TW)
                    pts = []
                    for mt in range(2):
                        ps, pk = ps_rot()
                        for j in range(2):
                            S.mm(ps[:, :], KTb[:, 2 * hd + j, mt * 128:(mt + 1) * 128], QX[:, 2 * hd + j, sl],
                                 j == 0, j == 1, [K("KTb", 2 * hd + j), K("QX", 2 * hd + j, tc)], [pk])
                        pt, ptk = PT[it % 4], K("PTx", it % 4)
                        S.act(pt[:], ps[:, :], AF.Exp, [pk], [ptk])
                        pts.append((pt, ptk))
                        it += 1
                    psD, pkD = ps_rot()
                    for mt in range(2):
                        S.mm(psD[:, :], ones_b, pts[mt][0][:], mt == 0, mt == 1, [pts[mt][1], K("c128b")], [pkD])
                    rd, rdk = RDx[(hd * NTC + tc) % 2], K("RDx", (hd * NTC + tc) % 2)
                    S.act(rd[:], psD[:, :], AF.Ln, [pkD], [rdk])
                    S.act(rd[:], rd[:], AF.Exp, [rdk], [rdk], scale=-1.0)
                    for j in range(2):
                        oc = 2 * hd + j
                        ps, pk = ps_rot()
                        for mt in range(2):
                            S.mm(ps[:, :], VX[:, mt, oc * 128:(oc + 1) * 128], pts[mt][0][:], mt == 0, mt == 1,
                                 [K("VX", mt, oc // 4), pts[mt][1]], [pk])
                        S.tt(hT[:, oc, sl], ps[:, :], rd[:], ALU.mult, [pk, rdk], [K("hT", oc, tc)])
            dump(f"X_OX{l}", hT[:], ALLH, [128, KC, S_])
            for half in range(2):
                wv, wk, _ = load_w(wo_d[l, :, half * 512:(half + 1) * 512], 8, 512)
                for o4 in range(4):
                    oc = half * 4 + o4
                    for tc in range(NTC):
                        ps, pk = ps_rot()
                        proj_fm(wv, wk, o4 * 128, tc, ps, pk)
                        S.tt(xT[:, oc, tc * TW:(tc + 1) * TW], xT[:, oc, tc * TW:(tc + 1) * TW], ps[:, :], ALU.add,
                             [K("xT", oc, tc), pk], [K("xT", oc, tc)])
        S.barrier()
        dump(f"x2_{l}", xT[:], ALLX, [128, KC, S_])
        if stop_after == f"X{l}":
            done = True
            break

        with ExitStack() as st:
            ENp = sb("ENp", [128, 16, 128], BF16, stack=st)
            S.add("dve", lambda e: e.memset(ENp[:], 0.0), [], [K("ENpz")])
            S.dma(ENp[0:16, :, :], en_d[:, :].rearrange("p (a b) -> p a b", a=16), [K("ENpz")], [K("ENf")], q="pool",
                  arena=True)
            CTh = sb("CTh", [128, S_], BF16, stack=st)
            CTl = sb("CTl", [128, S_], BF16, stack=st)
            S.add("dve", lambda e: e.memset(CTh[:], 0.0), [], [K("CThz")])
            S.add("dve", lambda e: e.memset(CTl[:], 0.0), [], [K("CTlz")])
            st_rt = ExitStack()
            WR = sb("WR", [128, KC, 20], stack=st_rt)
            S.dma(WR[:], wrt_d[l].rearrange("(kc p) n -> p kc n", p=128), [], [K("WR")])
            LG = sb("LG", [128, NT, 20], stack=st_rt)
            st_hf = ExitStack()
            HF = sb("HF", [128, KC, TW], stack=st_hf)

            def router_hook(tc, r):
                sl = slice(tc * TW, (tc + 1) * TW)
                for kc in range(KC):
                    S.stt(HF[:, kc, :], xT[:, kc, sl], col(l, "ffn_g", kc), r[:], ALU.mult, ALU.mult,
                          [K("xT", kc, tc), K("rsf", tc % 2), K("colp")], [K("HF", kc)])
                ps, pk = ps_rot()
                for q in range(4):
                    for kc in range(KC):
                        S.mm(ps[:, q * 20:(q + 1) * 20], HF[:, kc, q * 128:(q + 1) * 128], WR[:, kc, :], kc == 0,
                             kc == KC - 1, [K("HF", kc), K("WR")], [pk])
                S.cp(LG[:, tc * 4:tc * 4 + 4, :], ps[:, 0:80].rearrange("p (a b) -> p a b", a=4), [pk],
                     [K("LG", tc)])

            with ExitStack() as st2:
                rmsnorm_fm(xT, "xT", lambda kc: col(l, "ffn_g", kc), hT, "hT", KC, S_, st2, hook=router_hook, tag="f")
            st_hf.close()
            S.barrier()
            COMB = sb("COMB", [128, NT, 16], stack=st_rt)
            CT = sb("CT", [16, S_], stack=st_rt)
            sm = {nm: sb("rt_" + nm, [128, NT, w_] if w_ > 1 else [128, NT], stack=st_rt) for nm, w_ in
                  [("lg", 4), ("m", 1), ("eg", 4), ("sg", 1), ("oh", 4), ("le", 16), ("sel", 4), ("tmp", 4),
                   ("a", 1), ("eq", 4), ("sel2", 4), ("b", 1), ("t2", 4), ("ex", 4), ("dd", 1), ("wg", 4)]}

            def v(nm):
                return sm[nm][:]

            def kk(nm):
                return [K("rt", nm)]

            def B3(ap2, w_=4):
                return ap2.unsqueeze(2).to_broadcast([128, NT, w_])

            LGk = [K("LG", t) for t in range(NTC)]
            bgB = row(l, "bg").unsqueeze(1).to_broadcast([128, NT, 4])
            beB = row(l, "be").unsqueeze(1).to_broadcast([128, NT, 16])
            S.tt(v("lg"), LG[:, :, 0:4], bgB, ALU.add, LGk + [K("rowp")], kk("lg"))
            S.add("dve", lambda e: e.reduce_max(out=v("m"), in_=v("lg"), axis=AX.X), kk("lg"), kk("m"))
            S.tt(v("lg"), v("lg"), B3(v("m")), ALU.subtract, kk("lg") + kk("m"), kk("lg"))
            S.act(v("eg"), v("lg"), AF.Exp, kk("lg"), kk("eg"))
            S.add("dve", lambda e: e.reduce_sum(out=v("sg"), in_=v("eg"), axis=AX.X), kk("eg"), kk("sg"))
            S.add("dve", lambda e: e.reciprocal(out=v("sg"), in_=v("sg")), kk("sg"), kk("sg"))
            S.ts(v("oh"), v("lg"), 0.0, None, ALU.is_ge, None, kk("lg"), kk("oh"))
            S.tt(v("le"), LG[:, :, 4:20], beB, ALU.add, LGk + [K("rowp")], kk("le"))
            for g in range(4):
                ohg = sm["oh"][:, :, g:g + 1].to_broadcast([128, NT, 4])
                if g == 0:
                    S.tt(v("sel"), sm["le"][:, :, 0:4], ohg, ALU.mult, kk("le") + kk("oh"), kk("sel"))
                else:
                    S.tt(v("tmp"), sm["le"][:, :, 4 * g:4 * g + 4], ohg, ALU.mult, kk("le") + kk("oh"), kk("tmp"))
                    S.tt(v("sel"), v("sel"), v("tmp"), ALU.add, kk("sel") + kk("tmp"), kk("sel"))
            S.add("dve", lambda e: e.reduce_max(out=v("a"), in_=v("sel"), axis=AX.X), kk("sel"), kk("a"))
            S.tt(v("sel"), v("sel"), B3(v("a")), ALU.subtract, kk("sel") + kk("a"), kk("sel"))
            S.ts(v("eq"), v("sel"), 0.0, None, ALU.is_ge, None, kk("sel"), kk("eq"))
            S.stt(v("sel2"), v("eq"), -1e30, v("sel"), ALU.mult, ALU.add, kk("eq") + kk("sel"), kk("sel2"))
            S.add("dve", lambda e: e.reduce_max(out=v("b"), in_=v("sel2"), axis=AX.X), kk("sel2"), kk("b"))
            S.tt(v("t2"), v("sel"), B3(v("b")), ALU.is_ge, kk("sel") + kk("b"), kk("t2"))
            S.act(v("ex"), v("sel"), AF.Exp, kk("sel"), kk("ex"))
            S.act(v("dd"), v("b"), AF.Exp, kk("b"), kk("dd"))
            S.ts(v("dd"), v("dd"), 1.0, None, ALU.add, None, kk("dd"), kk("dd"))
            S.add("dve", lambda e: e.reciprocal(out=v("dd"), in_=v("dd")), kk("dd"), kk("dd"))
            S.tt(v("wg"), v("ex"), v("t2"), ALU.mult, kk("ex") + kk("t2"), kk("wg"))
            S.tt(v("wg"), v("wg"), B3(v("dd")), ALU.mult, kk("wg") + kk("dd"), kk("wg"))
            S.tt(v("wg"), v("wg"), B3(v("sg")), ALU.mult, kk("wg") + kk("sg"), kk("wg"))
            for g in range(4):
                ohg = sm["oh"][:, :, g:g + 1].to_broadcast([128, NT, 4])
                S.tt(COMB[:, :, 4 * g:4 * g + 4], v("wg"), ohg, ALU.mult, kk("wg") + kk("oh"),
                     [K("COMB", i, g) for i in range(NT)])
            for q4 in range(4):
                pst, pkt = ps_rot()
                for q in range(4):
                    i = q4 * 4 + q
                    S.tr(pst[0:16, q * 128:(q + 1) * 128], COMB[:, i, :], ident,
                         [K("COMB", i, g) for g in range(4)] + [K("c128")], [pkt])
                S.cp(CT[0:16, q4 * 512:(q4 + 1) * 512], pst[0:16, :], [pkt], [K("CT", q4)])
                S.cp(CTh[0:16, q4 * 512:(q4 + 1) * 512], CT[0:16, q4 * 512:(q4 + 1) * 512], [K("CT", q4), K("CThz")],
                     [K("CTh", q4)])
                S.tt(CT[0:16, q4 * 512:(q4 + 1) * 512], CT[0:16, q4 * 512:(q4 + 1) * 512],
                     CTh[0:16, q4 * 512:(q4 + 1) * 512], ALU.subtract, [K("CT", q4), K("CTh", q4)], [K("CT", q4)])
                S.cp(CTl[0:16, q4 * 512:(q4 + 1) * 512], CT[0:16, q4 * 512:(q4 + 1) * 512], [K("CT", q4), K("CTlz")],
                     [K("CTl", q4)])
            st_rt.close()
            S.barrier()
            HID = sb("HID", [128, 8, S_], BF16, stack=st)
            W2S = sb("W2S", [128, 8, D_], BF16, stack=st)
            CB = [sb(f"CB{j}", [128, 512], stack=st) for j in range(2)]
            SL = [sb(f"SL{j}", [128, 512], stack=st) for j in range(2)]
            n = 0
            for g in range(4):
                for el in range(4):
                    ex = 4 * g + el
                    wv, wk1_, slot = load_w(w1_d[l, ex], 8, 256, c0=0, tot=512)
                    _, wk3_, _ = load_w(w3_d[l, ex], 8, 256, slot=slot, c0=256, tot=512)
                    wk = wk1_ + wk3_
                    for tc in range(NTC):
                        sl = slice(tc * TW, (tc + 1) * TW)
                        psC, pkC = ps_rot()
                        S.mm(psC[:, :], ENp[:, ex, :], CTh[:, sl], True, False, [K("ENf"), K("CTh", tc), K("CThz")], [pkC])
                        S.mm(psC[:, :], ENp[:, ex, :], CTl[:, sl], False, True, [K("ENf"), K("CTl", tc), K("CTlz")], [pkC])
                        cb, cbk = CB[n % 2], K("CB", n % 2)
                        S.cp(cb[:], psC[:, :], [pkC], [cbk], eng="act")
                        for fc in range(2):
                            ps1, pk1 = ps_rot()
                            proj_fm(wv, wk, fc * 128, tc, ps1, pk1)
                            ps3, pk3 = ps_rot()
                            proj_fm(wv, wk, 256 + fc * 128, tc, ps3, pk3)
                            s_, sk = SL[(2 * n + fc) % 2], K("SL", (2 * n + fc) % 2)
                            S.act(s_[:], ps1[:, :], AF.Silu, [pk1], [sk])
                            S.tt(s_[:], s_[:], ps3[:, :], ALU.mult, [sk, pk3], [sk])
                            S.tt(HID[:, el * 2 + fc, sl], s_[:], cb[:], ALU.mult, [sk, cbk], [K("HID", el * 2 + fc, tc)])
                        n += 1
                    S.dma(W2S[:, 2 * el:2 * el + 2, :], w2_d[l, ex].rearrange("(fc p) n -> p fc n", p=128), [],
                          [K("W2S", el)], q="pool", arena=True)
                for oc in range(KC):
                    for tc in range(NTC):
                        ps, pk = ps_rot()
                        for j in range(8):
                            S.mm(ps[:, :], W2S[:, j, oc * 128:(oc + 1) * 128], HID[:, j, tc * TW:(tc + 1) * TW],
                                 j == 0, j == 7, [K("W2S", j // 2), K("HID", j, tc)], [pk])
                        S.tt(xT[:, oc, tc * TW:(tc + 1) * TW], xT[:, oc, tc * TW:(tc + 1) * TW], ps[:, :], ALU.add,
                             [K("xT", oc, tc), pk], [K("xT", oc, tc)])
        S.barrier()
        dump(f"x3_{l}", xT[:], ALLX, [128, KC, S_])
        if stop_after == f"M{l}":
            done = True
            break

    if stop_after is None:
        with ExitStack() as st:
            HF = sb("HFo", [128, KC, TW], stack=st)
            OB = [sb(f"OB{j}", [128, D_], stack=st) for j in range(2)]
            nob = [0]

            def final_hook(tc, r):
                sl = slice(tc * TW, (tc + 1) * TW)
                for kc in range(KC):
                    S.stt(HF[:, kc, :], xT[:, kc, sl], col(0, "fin_g", kc), r[:], ALU.mult, ALU.mult,
                          [K("xT", kc, tc), K("rsz", tc % 2), K("colp")], [K("HFo", kc)])
                for q in range(4):
                    ob, obk = OB[nob[0] % 2], K("OB", nob[0] % 2)
                    nob[0] += 1
                    for half in range(2):
                        ps, pk = ps_rot()
                        for j in range(4):
                            S.tr(ps[:, j * 128:(j + 1) * 128], HF[:, half * 4 + j, q * 128:(q + 1) * 128], ident,
                                 [K("HFo", half * 4 + j), K("c128")], [pk])
                        S.cp(ob[:, half * 512:(half + 1) * 512], ps[:, :], [pk], [obk],
                             eng=("act" if half else "dve"))
                    r0 = tc * TW + q * 128
                    i = S.dma(out_d[r0:r0 + 128, :], ob[:], [obk], [K("out", r0)])
                    S.final.append(i)

            rmsnorm_fm(xT, "xT", lambda kc: col(0, "fin_g", kc), None, None, KC, S_, st, hook=final_hook, tag="z")

    S.finalize(es)
    es.close()
    return nc, dump_d


def make_consts():
    c128 = np.zeros((128, 8, 128), np.float32)
    j = np.arange(128)[:, None]
    s = np.arange(128)[None, :]
    c128[:, 0] = np.eye(128)
    c128[:, 1] = 1.0
    c128[:, 2] = (j <= s)
    c128[:, 3] = -1.0 * (j >= s)
    c128[:, 4] = -1.0
    partner = np.where((np.arange(128) % 64) < 32, np.arange(128) + 32, np.arange(128) - 32)
    perm = np.zeros((128, 128), np.float32)
    perm[partner, np.arange(128)] = 1.0
    c128[:, 5] = perm
    c128[:, 6] = NEG * (s < j)
    cm = np.zeros((128, 12, 512), np.float32)
    p = np.arange(128)[:, None]
    f = np.arange(512)[None, :]
    for d in range(4):
        valid = (f - p) > d * 128
        cm[:, d] = valid
        cm[:, 4 + d] = NEG * (~valid)
        cm[:, 8 + d] = NEG * ((d * 128 + p) > f)
    half = 32
    inv = (np.float32(10000.0) ** (-(np.arange(half, dtype=np.float32)) / np.float32(half))).astype(np.float32)
    ang = np.arange(S_, dtype=np.float32)[None, :] * inv[:, None]
    cos = np.cos(ang).astype(np.float32)
    sin = np.sin(ang).astype(np.float32)
    rope = np.zeros((128, 2, S_), np.float32)
    for pp in range(128):
        d = pp % 64
        rope[pp, 0] = cos[d % 32]
        rope[pp, 1] = -sin[d % 32] if d < 32 else sin[d % 32]
    en = np.zeros((16, 16, 128), np.float32)
    for n in range(16):
        en[n, n, :] = 1.0
    gc = np.zeros((128, 3, 16, 8), np.float32)
    for i in range(16):
        own = i // 2
        for n in range(8):
            gc[:, 0, i, n] = 0.0 if n < own else -1e30
            gc[:, 1, i, n] = 1.0 if n < own else 0.0
            gc[:, 2, i, n] = 1.0 if n == own else 0.0
    blkoh = np.zeros((8, S_), np.float32)
    for n in range(8):
        blkoh[n, n * 256:(n + 1) * 256] = 1.0
    return dict(c128=c128.reshape(128, -1), cmask=cm.reshape(128, -1), rope=rope.reshape(128, -1),
                en=en.reshape(16, -1), gatec=gc.reshape(128, -1), blkoh=blkoh)


def colvec(v):
    v = np.asarray(v, np.float32)
    return np.ascontiguousarray(v.reshape(-1, 128).T)


def pack_params(inp):
    colp = np.zeros((128, L_, NCOL), np.float32)
    rowp = np.zeros((128, L_, NROW), np.float32)

    def put(l, name, arr):
        c0, w = COLS[name]
        assert arr.shape == (128, w), (name, arr.shape)
        colp[:, l, c0:c0 + w] = arr

    for l in range(L_):
        put(l, "mix_g", colvec(inp["mix_norm_g"][l]))
        put(l, "xat_g", colvec(inp["xattn_norm_g"][l]))
        put(l, "mem_g", colvec(inp["mem_norm_g"][l]))
        put(l, "ffn_g", colvec(inp["ffn_norm_g"][l]))
        put(l, "grp_g", colvec(inp["group_norm_g"][l]))
        cw = inp["lru_conv_w"][l]
        put(l, "lru_cw", np.concatenate([colvec(cw[k]) for k in range(4)], axis=1).reshape(128, 4, 2)
            .transpose(0, 2, 1).reshape(128, 8))
        put(l, "lru_cb", colvec(inp["lru_conv_b"][l]))
        put(l, "lru_br", colvec(inp["lru_br"][l]))
        put(l, "lru_bi", colvec(inp["lru_bi"][l]))
        put(l, "lru_lam", colvec(inp["lru_lambda"][l]))
        sw = inp["ssm_conv_w"][l]
        put(l, "ssm_cw", np.concatenate([colvec(sw[k]) for k in range(4)], axis=1).reshape(128, 4, 6)
            .transpose(0, 2, 1).reshape(128, 24))
        put(l, "ssm_cb", colvec(inp["ssm_conv_b"][l]))
        put(l, "ssm_d", colvec(np.repeat(inp["ssm_d"][l], 64)))
        put(l, "fin_g", colvec(inp["final_norm_g"]))
        r0, w = ROWS["dt_bias"]
        rowp[:, l, r0:r0 + w] = np.tile(inp["ssm_dt_bias"][l], 16)[None, :]
        r0, w = ROWS["a_log"]
        rowp[:, l, r0:r0 + w] = np.tile(inp["ssm_a_log"][l], 16)[None, :]
        r0, w = ROWS["bg"]
        rowp[:, l, r0:r0 + w] = inp["router_group_b"][l][None, :]
        r0, w = ROWS["be"]
        rowp[:, l, r0:r0 + w] = inp["router_expert_b"][l][None, :]
    wbd = np.zeros((L_, 2, 2, 128, 128), np.float32)
    for l in range(L_):
        for gi, nm in enumerate(("lru_wr", "lru_wi")):
            for c in range(2):
                for hh in range(2):
                    wbd[l, gi, c, hh * 64:(hh + 1) * 64, hh * 64:(hh + 1) * 64] = inp[nm][l, 2 * c + hh]
    router_w = np.concatenate([inp["router_group_w"], inp["router_expert_w"]], axis=2)
    return dict(colp=colp.reshape(128, -1), rowp=rowp.reshape(128, -1), lru_wbd=wbd,
                router_w=np.ascontiguousarray(router_w))


SHARED = ["w_in", "w_out", "xattn_wq", "xattn_wkv", "xattn_wo", "expert_w1", "expert_w3", "expert_w2"]


def make_in_maps(inputs, cores):
    inp = {k: np.asarray(v) for k, v in inputs.items()}
    consts = make_consts()
    packed = pack_params(inp)
    shared = {k: np.ascontiguousarray(inp[k], dtype=np.float32) for k in SHARED}
    maps = []
    for b in cores:
        m = dict(shared)
        m.update(consts)
        m.update(packed)
        m["x"] = np.ascontiguousarray(inp["x"][b])
        m["mem"] = np.ascontiguousarray(inp["mem"][b])
        maps.append(m)
    return maps


_CACHE = {}


def kernel(**inputs):
    if "nc" not in _CACHE:
        _CACHE["nc"] = build_program()[0]
    nc = _CACHE["nc"]
    maps = make_in_maps(inputs, list(range(8)))
    res = run_bass_kernel_spmd(nc, maps, core_ids=list(range(8)))
    return np.stack([r["out"] for r in res.results], axis=0).astype(np.float32)
```
